# Optimizing a Trainium2 kernel written in Bass

```python
import math
import jax
import jax.numpy as jnp
from jax import lax
import numpy as np

D_MODEL = 1024
BATCH = 8
SEQ = 2048
DEPTH = 2

GRID_W = 64
CTX_LEN = 256
HEAD_DIM = 64
ROPE_BASE = 10000.0
ROPE_FREQS = HEAD_DIM // 4
LN_EPS = 1e-5
RMS_EPS = 1e-5
NEG_INF = -1e30

SWA_HEADS = 8
SWA_KV_HEADS = 2
SWA_GROUP = SWA_HEADS // SWA_KV_HEADS
SWA_WINDOW = 128
SWA_BLOCK = 128
SWA_Q_W = SWA_HEADS * HEAD_DIM
SWA_KV_W = SWA_KV_HEADS * HEAD_DIM

SSM_WIDTH = D_MODEL // 2
SSM_GROUP = 16
SSM_GROUPS = SSM_WIDTH // SSM_GROUP
SSM_STATE = 64
SSM_DT_MIN = 1e-3
SSM_DT_MAX = 1e-1

AB_IN_W = SWA_Q_W + 2 * SWA_KV_W + SSM_WIDTH
AB_OUT_W = SWA_Q_W + SSM_WIDTH

DIF_HEADS = D_MODEL // (2 * HEAD_DIM)
DIF_QK_W = DIF_HEADS * 2 * HEAD_DIM
DIF_V_HEAD = 2 * HEAD_DIM
DIF_V_W = DIF_HEADS * DIF_V_HEAD
DIF_BLOCK = 128

MOE_GROUPS = 4
MOE_EXPERTS_PER_GROUP = 8
MOE_EXPERTS = MOE_GROUPS * MOE_EXPERTS_PER_GROUP
MOE_TOPK = 2
MOE_HIDDEN = D_MODEL // 4

DEEPNORM_ALPHA = (2 * DEPTH) ** 0.25
DEEPNORM_BETA = (8 * DEPTH) ** -0.25
N_EVEN = (DEPTH + 1) // 2
N_ODD = DEPTH // 2

kernel_name = 'hybrid_swa_s5_diffattn_hmoe_dit'


def layer_norm(x, g, b):
    xf = x.astype(jnp.float32)
    mu = jnp.mean(xf, -1, keepdims=True)
    var = jnp.mean(jnp.square(xf - mu), -1, keepdims=True)
    return ((xf - mu) * lax.rsqrt(var + LN_EPS) * g + b).astype(x.dtype)


def axial_rope_tables(n_tokens):
    rows = n_tokens // GRID_W
    row = jnp.repeat(jnp.arange(rows, dtype=jnp.float32), GRID_W)
    col = jnp.tile(jnp.arange(GRID_W, dtype=jnp.float32), rows)
    inv = ROPE_BASE ** (-jnp.arange(ROPE_FREQS, dtype=jnp.float32) / ROPE_FREQS)
    ang_r = row[:, None] * inv[None, :]
    ang_c = col[:, None] * inv[None, :]
    return (jnp.cos(ang_r), jnp.sin(ang_r), jnp.cos(ang_c), jnp.sin(ang_c))


def apply_axial_rope(t, rope):
    cos_r, sin_r, cos_c, sin_c = rope
    n = t.shape[1]
    bshape = (n,) + (1,) * (t.ndim - 3) + (ROPE_FREQS,)
    tf = t.astype(jnp.float32)
    half = HEAD_DIM // 2

    def rot(u, cs, sn):
        cs = cs.reshape(bshape)
        sn = sn.reshape(bshape)
        u1, u2 = u[..., :ROPE_FREQS], u[..., ROPE_FREQS:]
        return jnp.concatenate([u1 * cs - u2 * sn, u2 * cs + u1 * sn], -1)

    out = jnp.concatenate([rot(tf[..., :half], cos_r, sin_r), rot(tf[..., half:], cos_c, sin_c)], -1)
    return out.astype(t.dtype)


def windowed_gqa_sink(q, k, v, qc, kc, vc, sink, need_ctx):
    bsz, n = q.shape[:2]
    ctx_len = kc.shape[1]
    nb = n // SWA_BLOCK
    span = 3 * SWA_BLOCK
    scale = HEAD_DIM ** -0.5
    qb = q.reshape(bsz, nb, SWA_BLOCK, SWA_KV_HEADS, SWA_GROUP, HEAD_DIM)
    pad = ((0, 0), (SWA_BLOCK, SWA_BLOCK), (0, 0), (0, 0))
    kp = jnp.pad(k, pad).reshape(bsz, nb + 2, SWA_BLOCK, SWA_KV_HEADS, HEAD_DIM)
    vp = jnp.pad(v, pad).reshape(bsz, nb + 2, SWA_BLOCK, SWA_KV_HEADS, HEAD_DIM)
    kb = jnp.concatenate([kp[:, :-2], kp[:, 1:-1], kp[:, 2:]], axis=2)
    vb = jnp.concatenate([vp[:, :-2], vp[:, 1:-1], vp[:, 2:]], axis=2)
    qi = jnp.arange(SWA_BLOCK)[:, None] + SWA_BLOCK
    ki = jnp.arange(span)[None, :]
    kabs = (jnp.arange(nb) * SWA_BLOCK - SWA_BLOCK)[:, None] + jnp.arange(span)[None, :]
    mask = (jnp.abs(qi - ki) <= SWA_WINDOW)[None] & ((kabs >= 0) & (kabs < n))[:, None, :]
    s_loc = jnp.einsum('bnqhgd,bnkhd->bnhgqk', qb, kb, preferred_element_type=jnp.float32) * scale
    s_loc = jnp.where(mask[None, :, None, None], s_loc, NEG_INF)
    s_ctx = jnp.einsum('bnqhgd,blhd->bnhgql', qb, kc, preferred_element_type=jnp.float32) * scale
    sk = sink.astype(jnp.float32).reshape(SWA_KV_HEADS, SWA_GROUP)
    s_sink = jnp.broadcast_to(sk[None, None, :, :, None, None], s_loc.shape[:-1] + (1,))
    p = jax.nn.softmax(jnp.concatenate([s_loc, s_ctx, s_sink], -1), -1).astype(v.dtype)
    o = (jnp.einsum('bnhgqk,bnkhd->bnqhgd', p[..., :span], vb)
         + jnp.einsum('bnhgql,blhd->bnqhgd', p[..., span:span + ctx_len], vc))
    o = o.reshape(bsz, n, SWA_Q_W)
    oc = None
    if need_ctx:
        sc = jnp.einsum('blhgd,bmhd->bhglm', qc, kc, preferred_element_type=jnp.float32) * scale
        sc_sink = jnp.broadcast_to(sk[None, :, :, None, None], sc.shape[:-1] + (1,))
        pc = jax.nn.softmax(jnp.concatenate([sc, sc_sink], -1), -1).astype(vc.dtype)
        oc = jnp.einsum('bhglm,bmhd->blhgd', pc[..., :ctx_len], vc).reshape(bsz, ctx_len, SWA_Q_W)
    return o, oc


def s5_discretize(a_re, a_im, log_step, b_re, b_im):
    dt = jnp.exp(log_step.astype(jnp.float32))[:, None]
    ar = a_re.astype(jnp.float32)
    ai = a_im.astype(jnp.float32)
    mag = jnp.exp(dt * ar)
    abar_re = mag * jnp.cos(dt * ai)
    abar_im = mag * jnp.sin(dt * ai)
    den = ar * ar + ai * ai
    nr = abar_re - 1.0
    coef_re = (nr * ar + abar_im * ai) / den
    coef_im = (abar_im * ar - nr * ai) / den
    br = b_re.astype(jnp.float32)
    bi = b_im.astype(jnp.float32)
    bb_re = coef_re[..., None] * br - coef_im[..., None] * bi
    bb_im = coef_re[..., None] * bi + coef_im[..., None] * br
    return abar_re, abar_im, bb_re, bb_im


def complex_linear_combine(e1, e2):
    a1r, a1i, b1r, b1i = e1
    a2r, a2i, b2r, b2i = e2
    return (a2r * a1r - a2i * a1i,
            a2r * a1i + a2i * a1r,
            a2r * b1r - a2i * b1i + b2r,
            a2r * b1i + a2i * b1r + b2i)


def s5_scan(u, abar_re, abar_im, bb_re, bb_im, reverse):
    t = u.shape[1]
    bu_re = jnp.einsum('btgc,gpc->btgp', u, bb_re)
    bu_im = jnp.einsum('btgc,gpc->btgp', u, bb_im)
    a_re = jnp.broadcast_to(abar_re, (1, t) + abar_re.shape)
    a_im = jnp.broadcast_to(abar_im, (1, t) + abar_im.shape)
    return lax.associative_scan(complex_linear_combine, (a_re, a_im, bu_re, bu_im), reverse=reverse, axis=1)


def s5_readout(s_re, s_im, c_re, c_im):
    return (jnp.einsum('btgp,gcp->btgc', s_re, c_re.astype(jnp.float32))
            - jnp.einsum('btgp,gcp->btgc', s_im, c_im.astype(jnp.float32)))


def s5_bidirectional(u, uc, a_re, a_im, log_step, b_re, b_im, c_re, c_im, d_skip, glu_w, glu_b, need_ctx):
    bsz, n = u.shape[:2]
    ctx_len = uc.shape[1]
    ug = u.astype(jnp.float32).reshape(bsz, n, SSM_GROUPS, SSM_GROUP)
    ucg = uc.astype(jnp.float32).reshape(bsz, ctx_len, SSM_GROUPS, SSM_GROUP)
    dg = d_skip.astype(jnp.float32).reshape(SSM_GROUPS, SSM_GROUP)
    y = ug * dg
    yc = ucg * dg if need_ctx else None
    for direction, reverse in ((0, False), (1, True)):
        ab_re, ab_im, bb_re, bb_im = s5_discretize(a_re[direction], a_im[direction], log_step[direction],
                                                   b_re[direction], b_im[direction])
        _, _, sc_re, sc_im = s5_scan(ucg, ab_re, ab_im, bb_re, bb_im, reverse)
        last = 0 if reverse else ctx_len - 1
        s0_re = sc_re[:, last:last + 1]
        s0_im = sc_im[:, last:last + 1]
        acc_re, acc_im, sl_re, sl_im = s5_scan(ug, ab_re, ab_im, bb_re, bb_im, reverse)
        s_re = acc_re * s0_re - acc_im * s0_im + sl_re
        s_im = acc_re * s0_im + acc_im * s0_re + sl_im
        y = y + s5_readout(s_re, s_im, c_re[direction], c_im[direction])
        if need_ctx:
            yc = yc + s5_readout(sc_re, sc_im, c_re[direction], c_im[direction])

    def glu(t):
        g = jax.nn.gelu(t.reshape(t.shape[:2] + (SSM_WIDTH,)))
        return g * jax.nn.sigmoid(g @ glu_w.astype(jnp.float32) + glu_b.astype(jnp.float32))

    out = glu(y).astype(u.dtype)
    outc = glu(yc).astype(uc.dtype) if need_ctx else None
    return out, outc


def mixer_swa_ssm(h, hc, rope, w_in, w_out, sink, a_re, a_im, log_step, b_re, b_im, c_re, c_im,
                  d_skip, glu_w, glu_b, need_ctx):
    splits = [SWA_Q_W, SWA_Q_W + SWA_KV_W, SWA_Q_W + 2 * SWA_KV_W]

    def proj(t):
        s = t.shape[:2]
        q, k, v, u = jnp.split(t @ w_in, splits, -1)
        return (q.reshape(s + (SWA_KV_HEADS, SWA_GROUP, HEAD_DIM)),
                k.reshape(s + (SWA_KV_HEADS, HEAD_DIM)),
                v.reshape(s + (SWA_KV_HEADS, HEAD_DIM)), u)

    q, k, v, u = proj(h)
    qc, kc, vc, uc = proj(hc)
    q = apply_axial_rope(q, rope)
    k = apply_axial_rope(k, rope)
    att, att_c = windowed_gqa_sink(q, k, v, qc, kc, vc, sink, need_ctx)
    ssm, ssm_c = s5_bidirectional(u, uc, a_re, a_im, log_step, b_re, b_im, c_re, c_im,
                                  d_skip, glu_w, glu_b, need_ctx)
    y = jnp.concatenate([att, ssm], -1) @ w_out
    yc = jnp.concatenate([att_c, ssm_c], -1) @ w_out if need_ctx else None
    return y, yc


def dif_head_out(o, g, lam_init):
    of = o.astype(jnp.float32)
    of = of * lax.rsqrt(jnp.mean(of * of, -1, keepdims=True) + RMS_EPS) * g.astype(jnp.float32)
    return (of * (1.0 - lam_init)).astype(o.dtype).reshape(o.shape[:2] + (DIF_V_W,))


def mixer_diff(h, hc, rope, w_in, w_out, lq1, lk1, lq2, lk2, subln_g, lam_init, need_ctx):
    bsz, n = h.shape[:2]
    scale = HEAD_DIM ** -0.5

    def proj(t):
        s = t.shape[:2]
        q, k, v = jnp.split(t @ w_in, [DIF_QK_W, 2 * DIF_QK_W], -1)
        return (q.reshape(s + (DIF_HEADS, 2, HEAD_DIM)), k.reshape(s + (DIF_HEADS, 2, HEAD_DIM)),
                v.reshape(s + (DIF_HEADS, DIF_V_HEAD)))

    q, k, v = proj(h)
    qc, kc, vc = proj(hc)
    q = apply_axial_rope(q, rope)
    k = apply_axial_rope(k, rope)
    lam = (jnp.exp(jnp.sum(lq1.astype(jnp.float32) * lk1.astype(jnp.float32)))
           - jnp.exp(jnp.sum(lq2.astype(jnp.float32) * lk2.astype(jnp.float32))) + lam_init)

    def attend(qq, kk, vv):
        s = jnp.einsum('bqhcd,bkhcd->bhcqk', qq, kk, preferred_element_type=jnp.float32) * scale
        p = jax.nn.softmax(s, -1)
        pd = (p[:, :, 0] - lam * p[:, :, 1]).astype(vv.dtype)
        return jnp.einsum('bhqk,bkhe->bqhe', pd, vv)

    kall = jnp.concatenate([k, kc], 1)
    vall = jnp.concatenate([v, vc], 1)
    nb = n // DIF_BLOCK
    qb = jnp.moveaxis(q.reshape(bsz, nb, DIF_BLOCK, DIF_HEADS, 2, HEAD_DIM), 1, 0)
    o = lax.map(lambda qq: attend(qq, kall, vall), qb)
    o = jnp.moveaxis(o, 0, 1).reshape(bsz, n, DIF_HEADS, DIF_V_HEAD)
    y = dif_head_out(o, subln_g, lam_init) @ w_out
    yc = dif_head_out(attend(qc, kc, vc), subln_g, lam_init) @ w_out if need_ctx else None
    return y, yc


def hier_moe(t, wg, bg, we, be, w1, w3, w2):
    pg = jax.nn.softmax((t @ wg).astype(jnp.float32) + bg.astype(jnp.float32), -1)
    gp, gi = lax.top_k(pg, 1)
    le = jnp.einsum('td,gde->tge', t, we).astype(jnp.float32) + be.astype(jnp.float32)
    le_sel = jnp.take_along_axis(le, gi[:, :, None], axis=1)[:, 0]
    tv, ti = lax.top_k(le_sel, MOE_TOPK)
    w_sel = jax.nn.softmax(tv, -1) * gp
    gate_e = jnp.sum(jax.nn.one_hot(ti, MOE_EXPERTS_PER_GROUP, dtype=jnp.float32) * w_sel[..., None], 1)
    gates = (jax.nn.one_hot(gi[:, 0], MOE_GROUPS, dtype=jnp.float32)[:, :, None]
             * gate_e[:, None, :]).astype(t.dtype)
    out = jnp.zeros_like(t)
    for g in range(MOE_GROUPS):
        sl = slice(g * MOE_EXPERTS_PER_GROUP, (g + 1) * MOE_EXPERTS_PER_GROUP)
        h1 = jnp.einsum('td,edf->tef', t, w1[sl])
        h3 = jnp.einsum('td,edf->tef', t, w3[sl])
        hh = jax.nn.silu(h1) * h3 * gates[:, g, :, None]
        out = out + jnp.einsum('tef,efd->td', hh, w2[sl])
    return out


def setup_inputs(seed: int = 0) -> dict:
    key = jax.random.key(seed)
    counter = [0]

    def nk():
        counter[0] += 1
        return jax.random.fold_in(key, counter[0])

    def nrm(shape, scale):
        return jax.random.normal(nk(), shape, jnp.float32) * scale

    D = D_MODEL
    NE, NO = N_EVEN, N_ODD
    G, P, C = SSM_GROUPS, SSM_STATE, SSM_GROUP
    E, F = MOE_EXPERTS, MOE_HIDDEN
    return {
        'x': nrm((BATCH, SEQ, D), 1.0),
        'c': nrm((BATCH, D), 1.0),
        'ctx': nrm((BATCH, CTX_LEN, D), 1.0),
        'c_ctx': nrm((D,), 1.0),
        'mod_w': nrm((DEPTH, D, 6 * D), D ** -0.5),
        'mod_b': nrm((DEPTH, 6 * D), 0.02),
        'ln1_g': 1.0 + nrm((DEPTH, D), 0.02),
        'ln1_b': nrm((DEPTH, D), 0.02),
        'ln2_g': 1.0 + nrm((DEPTH, D), 0.02),
        'ln2_b': nrm((DEPTH, D), 0.02),
        'swa_ssm_w_in': nrm((NE, D, AB_IN_W), D ** -0.5),
        'swa_ssm_w_out': nrm((NE, AB_OUT_W, D), AB_OUT_W ** -0.5 * DEEPNORM_BETA),
        'swa_sink': nrm((NE, SWA_HEADS), 0.5),
        'ssm_a_re': -0.5 + nrm((NE, 2, G, P), 0.01),
        'ssm_a_im': jnp.pi * jnp.arange(P, dtype=jnp.float32) + nrm((NE, 2, G, P), 0.01),
        'ssm_log_step': jax.random.uniform(nk(), (NE, 2, G), jnp.float32,
                                           math.log(SSM_DT_MIN), math.log(SSM_DT_MAX)),
        'ssm_b_re': nrm((NE, 2, G, P, C), (2 * C) ** -0.5),
        'ssm_b_im': nrm((NE, 2, G, P, C), (2 * C) ** -0.5),
        'ssm_c_re': nrm((NE, 2, G, C, P), P ** -0.5),
        'ssm_c_im': nrm((NE, 2, G, C, P), P ** -0.5),
        'ssm_d': nrm((NE, SSM_WIDTH), 1.0),
        'ssm_glu_w': nrm((NE, SSM_WIDTH, SSM_WIDTH), SSM_WIDTH ** -0.5),
        'ssm_glu_b': nrm((NE, SSM_WIDTH), 0.02),
        'dif_w_in': nrm((NO, D, 2 * DIF_QK_W + DIF_V_W), D ** -0.5),
        'dif_w_out': nrm((NO, DIF_V_W, D), DIF_V_W ** -0.5 * DEEPNORM_BETA),
        'dif_lam_q1': nrm((NO, HEAD_DIM), 0.1),
        'dif_lam_k1': nrm((NO, HEAD_DIM), 0.1),
        'dif_lam_q2': nrm((NO, HEAD_DIM), 0.1),
        'dif_lam_k2': nrm((NO, HEAD_DIM), 0.1),
        'dif_subln_g': 1.0 + nrm((NO, DIF_V_HEAD), 0.02),
        'moe_wg': nrm((DEPTH, D, MOE_GROUPS), D ** -0.5),
        'moe_bg': nrm((DEPTH, MOE_GROUPS), 0.01),
        'moe_we': nrm((DEPTH, MOE_GROUPS, D, MOE_EXPERTS_PER_GROUP), D ** -0.5),
        'moe_be': nrm((DEPTH, MOE_GROUPS, MOE_EXPERTS_PER_GROUP), 0.01),
        'moe_w1': nrm((DEPTH, E, D, F), D ** -0.5),
        'moe_w3': nrm((DEPTH, E, D, F), D ** -0.5),
        'moe_w2': nrm((DEPTH, E, F, D), F ** -0.5 * DEEPNORM_BETA),
    }


def reference(x, c, ctx, c_ctx, mod_w, mod_b, ln1_g, ln1_b, ln2_g, ln2_b,
              swa_ssm_w_in, swa_ssm_w_out, swa_sink,
              ssm_a_re, ssm_a_im, ssm_log_step, ssm_b_re, ssm_b_im, ssm_c_re, ssm_c_im,
              ssm_d, ssm_glu_w, ssm_glu_b,
              dif_w_in, dif_w_out, dif_lam_q1, dif_lam_k1, dif_lam_q2, dif_lam_k2, dif_subln_g,
              moe_wg, moe_bg, moe_we, moe_be, moe_w1, moe_w3, moe_w2):
    bsz, n, d = x.shape
    ctx_len = ctx.shape[1]
    rope = axial_rope_tables(n)
    xl, xc = x, ctx
    for layer in range(DEPTH):
        need_ctx = layer < DEPTH - 1
        i = layer // 2
        m_lat = jnp.split(jax.nn.silu(c) @ mod_w[layer] + mod_b[layer], 6, -1)
        sh1, sc1, g1, sh2, sc2, g2 = [m[:, None, :] for m in m_lat]
        csh1, csc1, cg1, csh2, csc2, cg2 = jnp.split(jax.nn.silu(c_ctx) @ mod_w[layer] + mod_b[layer], 6, -1)
        hl = xl * (1.0 + sc1) + sh1
        hc = xc * (1.0 + csc1) + csh1
        if layer % 2 == 0:
            yl, yc = mixer_swa_ssm(hl, hc, rope, swa_ssm_w_in[i], swa_ssm_w_out[i], swa_sink[i],
                                   ssm_a_re[i], ssm_a_im[i], ssm_log_step[i], ssm_b_re[i], ssm_b_im[i],
                                   ssm_c_re[i], ssm_c_im[i], ssm_d[i], ssm_glu_w[i], ssm_glu_b[i], need_ctx)
        else:
            lam_init = 0.8 - 0.6 * math.exp(-0.3 * layer)
            yl, yc = mixer_diff(hl, hc, rope, dif_w_in[i], dif_w_out[i], dif_lam_q1[i], dif_lam_k1[i],
                                dif_lam_q2[i], dif_lam_k2[i], dif_subln_g[i], lam_init, need_ctx)
        xl = layer_norm(DEEPNORM_ALPHA * xl + g1 * yl, ln1_g[layer], ln1_b[layer])
        hl = xl * (1.0 + sc2) + sh2
        if need_ctx:
            xc = layer_norm(DEEPNORM_ALPHA * xc + cg1 * yc, ln1_g[layer], ln1_b[layer])
            hc = xc * (1.0 + csc2) + csh2
            tok = jnp.concatenate([hl.reshape(-1, d), hc.reshape(-1, d)], 0)
        else:
            tok = hl.reshape(-1, d)
        f = hier_moe(tok, moe_wg[layer], moe_bg[layer], moe_we[layer], moe_be[layer],
                     moe_w1[layer], moe_w3[layer], moe_w2[layer])
        xl = layer_norm(DEEPNORM_ALPHA * xl + g2 * f[:bsz * n].reshape(bsz, n, d), ln2_g[layer], ln2_b[layer])
        if need_ctx:
            xc = layer_norm(DEEPNORM_ALPHA * xc + cg2 * f[bsz * n:].reshape(bsz, ctx_len, d),
                            ln2_g[layer], ln2_b[layer])
    return xl
```

```python
import math
from contextlib import ExitStack

import numpy as np
import concourse.bass as bass
import concourse.mybir as mybir
from concourse.bass_utils import run_bass_kernel_spmd

F32 = mybir.dt.float32
BF16 = mybir.dt.bfloat16
AF = mybir.ActivationFunctionType
ALU = mybir.AluOpType
AX = mybir.AxisListType

D = 1024
SEQ = 2048
CTX = 256
NT = SEQ + CTX
NTILE = NT // 128
NLT = SEQ // 128
ALPHA = 4 ** 0.25
LN_EPS = 1e-5


class Buf:
    __slots__ = ("w", "r")

    def __init__(self):
        self.w = None
        self.r = {}


class EngState:
    def __init__(self, name, eng, sem):
        self.name = name
        self.eng = eng
        self.sem = sem
        self.count = 0
        self.waited = {}
        self.slots = []
        self.slot_i = 0


class K:
    def __init__(self, nc, stack):
        self.nc = nc
        self.E = {}
        for name, eng in (("pe", nc.tensor), ("dve", nc.vector), ("act", nc.scalar),
                          ("pool", nc.gpsimd), ("sp", nc.sync)):
            sem = stack.enter_context(nc.semaphore("s_" + name))
            self.E[name] = EngState(name, eng, sem)
        self.semkey = {}
        for qn, n in (("sp", 12), ("pool", 12), ("act", 6)):
            for i in range(n):
                sem = stack.enter_context(nc.semaphore(f"d_{qn}{i}"))
                self.E[qn].slots.append([sem, 0])
        self.uid = 0

    def _key(self, sem):
        return id(sem)

    def _wait(self, E, deps, skip_self=False):
        best = {}
        for sem, val in deps:
            if skip_self and sem is E.sem:
                continue
            k = id(sem)
            if k not in best or best[k][1] < val:
                best[k] = (sem, val)
        for k, (sem, val) in best.items():
            if E.waited.get(k, 0) < val:
                E.eng.wait_ge(sem, val)
                E.waited[k] = val

    def _deps(self, reads, writes):
        deps = []
        for b in reads:
            if b.w is not None:
                deps.append(b.w)
        for b in writes:
            if b.w is not None:
                deps.append(b.w)
            deps.extend(b.r.values())
        return deps

    def _mark(self, tok, reads, writes):
        sem, val = tok
        for b in reads:
            b.r[id(sem)] = tok
        for b in writes:
            b.w = tok
            b.r = {}

    def op(self, en, fn, reads=(), writes=()):
        E = self.E[en]
        self._wait(E, self._deps(reads, writes), skip_self=(en == "pe"))
        ins = fn(E.eng)
        E.count += 1
        ins.then_inc(E.sem, 1)
        tok = (E.sem, E.count)
        self._mark(tok, reads, writes)
        return tok

    def dma(self, qn, out, in_, reads=(), writes=(), **kw):
        E = self.E[qn]
        self._wait(E, self._deps(reads, writes))
        slot = E.slots[E.slot_i % len(E.slots)]
        E.slot_i += 1
        if slot[1] > 0:
            self._wait(E, [(slot[0], slot[1] * 16)])
        ins = E.eng.dma_start(out=out, in_=in_, **kw)
        slot[1] += 1
        ins.then_inc(slot[0], 16)
        tok = (slot[0], slot[1] * 16)
        self._mark(tok, reads, writes)
        return tok

    def all_tokens(self):
        toks = []
        for E in self.E.values():
            if E.count:
                toks.append((E.sem, E.count))
            for sem, c in E.slots:
                if c:
                    toks.append((sem, c * 16))
        return toks

    def barrier(self):
        toks = self.all_tokens()
        for E in self.E.values():
            self._wait(E, toks, skip_self=False)


def build(dbg=(), inject=(), phases=None):
    nc = bass.Bass("TRN2", target_bir_lowering=False)
    dbg = set(dbg)
    inject = set(inject)
    ALLP = {"mod", "l0proj", "l0att", "l0ssm", "l0ssmpost", "l0out", "moe0", "l1proj", "l1att", "moe1"}
    phases = ALLP if phases is None else set(phases)

    def din(name, shape):
        return nc.dram_tensor(name, list(shape), F32, kind="ExternalInput").ap()

    def dscr(name, shape, dt=F32):
        kind = "ExternalOutput" if name in dbg else ("ExternalInput" if name in inject else "Internal")
        return nc.dram_tensor(name, list(shape), dt, kind=kind).ap()

    x_in = din("x", [SEQ, D])
    ctx_in = din("ctx", [CTX, D])
    c_in = din("c", [1, D])
    cc_in = din("c_ctx", [1, D])
    mod_w = din("mod_w", [2, D, 6 * D])
    mod_b = din("mod_b", [2, 6 * D])
    ln1_g = din("ln1_g", [2, D]); ln1_b = din("ln1_b", [2, D])
    ln2_g = din("ln2_g", [2, D]); ln2_b = din("ln2_b", [2, D])
    w_in0 = din("swa_ssm_w_in", [D, 1280])
    w_out0 = din("swa_ssm_w_out", [D, D])
    swa_sink = din("swa_sink", [1, 8])
    a_re = din("ssm_a_re", [2, 32, 64]); a_im = din("ssm_a_im", [2, 32, 64])
    log_step = din("ssm_log_step", [2, 32])
    b_re = din("ssm_b_re", [2, 32, 64, 16]); b_im = din("ssm_b_im", [2, 32, 64, 16])
    c_re = din("ssm_c_re", [2, 32, 16, 64]); c_im = din("ssm_c_im", [2, 32, 16, 64])
    ssm_d = din("ssm_d", [1, 512])
    glu_w = din("ssm_glu_w", [512, 512]); glu_b = din("ssm_glu_b", [1, 512])
    dif_w_in = din("dif_w_in", [D, 3072]); dif_w_out = din("dif_w_out", [D, D])
    lam_q1 = din("dif_lam_q1", [1, 64]); lam_k1 = din("dif_lam_k1", [1, 64])
    lam_q2 = din("dif_lam_q2", [1, 64]); lam_k2 = din("dif_lam_k2", [1, 64])
    subln_g = din("dif_subln_g", [1, 128])
    moe_wg = din("moe_wg", [2, D, 4]); moe_bg = din("moe_bg", [2, 4])
    moe_we = din("moe_we", [2, 4, D, 8]); moe_be = din("moe_be", [2, 32])
    moe_w1 = din("moe_w1", [2, 32, D, 256]); moe_w3 = din("moe_w3", [2, 32, D, 256])
    moe_w2 = din("moe_w2", [2, 32, 256, D])
    rope_cs = din("rope_cs", [SEQ, 64])
    ident_in = din("ident", [128, 128])
    sel_in = din("sel", [32, 32, 128])
    kval_in = din("kval", [64, 1024]); mrow_in = din("mrow", [64, 288])
    maskF_in = din("maskF", [128, 128]); maskB_in = din("maskB", [128, 128])
    maskL_in = din("maskL", [128, 128]); maskR_in = din("maskR", [128, 128])
    out = nc.dram_tensor("out", [SEQ, D], F32, kind="ExternalOutput").ap()

    modrow = dscr("modrow", [2, 2, 6 * D])

    with ExitStack() as gs:
        k = K(nc, gs)

        def sb(st, name, shape, dt=F32):
            k.uid += 1
            return st.enter_context(nc.sbuf_tensor(f"sb{k.uid}_{name}", list(shape), dt))

        def ps(st, name, shape, dt=F32):
            k.uid += 1
            return st.enter_context(nc.psum_tensor(f"ps{k.uid}_{name}", list(shape), dt))

        def phase(name):
            if name in phases:
                with ExitStack() as st_:
                    yield st_

        ident_f = sb(gs, "ident_f", [128, 128]); B_ident_f = Buf()
        ident_b = sb(gs, "ident_b", [128, 128], BF16); B_ident_b = Buf()
        k.dma("sp", ident_f[:], ident_in[:, :], writes=[B_ident_f])
        k.op("dve", lambda e: e.tensor_copy(out=ident_b[:], in_=ident_f[:]),
             reads=[B_ident_f], writes=[B_ident_b])

        for st in phase("mod"):
            cT = sb(st, "cT", [128, 8, 2]); B_cT = Buf()
            with nc.allow_non_contiguous_dma(reason="tiny column loads"):
                k.dma("sp", cT[:, :, 0], c_in[0, :].rearrange("(k p) -> p k", p=128), writes=[B_cT])
                k.dma("sp", cT[:, :, 1], cc_in[0, :].rearrange("(k p) -> p k", p=128), writes=[B_cT])
            sT = sb(st, "sT", [128, 8, 2]); B_sT = Buf()
            k.op("act", lambda e: e.activation(out=sT[:], in_=cT[:], func=AF.Silu),
                 reads=[B_cT], writes=[B_sT])
            wt = [sb(st, f"modw{i}", [128, 8, 512]) for i in range(2)]
            B_wt = [Buf(), Buf()]
            mb = sb(st, "modb", [2, 6 * D]); B_mb = Buf()
            mrow = sb(st, "mrow", [2, 6 * D]); B_mrow = Buf()
            pm = [ps(st, f"pmod{i}", [2, 512]) for i in range(2)]
            B_pm = [Buf(), Buf()]
            it = 0
            for l in range(2):
                k.dma("sp", mb[0:1, :], mod_b[l:l + 1, :], writes=[B_mb])
                k.dma("sp", mb[1:2, :], mod_b[l:l + 1, :], writes=[B_mb])
                for cb in range(12):
                    i = it % 2
                    it += 1
                    k.dma("sp" if cb % 2 == 0 else "act", wt[i][:],
                          mod_w[l, :, cb * 512:(cb + 1) * 512].rearrange("(k p) n -> p k n", p=128),
                          writes=[B_wt[i]])
                    for kc in range(8):
                        k.op("pe", lambda e, kc=kc, i=i: e.matmul(
                            pm[i][:], lhsT=sT[:, kc, :], rhs=wt[i][:, kc, :],
                            start=(kc == 0), stop=(kc == 7)),
                            reads=[B_sT, B_wt[i]], writes=[B_pm[i]])
                    k.op("dve", lambda e, i=i, cb=cb: e.tensor_tensor(
                        out=mrow[:, cb * 512:(cb + 1) * 512], in0=pm[i][:],
                        in1=mb[:, cb * 512:(cb + 1) * 512], op=ALU.add),
                        reads=[B_pm[i], B_mb], writes=[B_mrow])
                k.dma("sp", modrow[l], mrow[:], reads=[B_mrow], writes=[])
            k.barrier()


        def load_bc(st, name, src_row_ap, n, q="sp"):
            t = sb(st, name, [128, n]); B = Buf()
            k.dma(q, t[:], src_row_ap.partition_broadcast(128), writes=[B])
            return t, B

        def mod_bc(st, name, l, r, chunk, plus1=False):
            t, B = load_bc(st, name, modrow[l, r:r + 1, chunk * D:(chunk + 1) * D], D)
            if plus1:
                k.op("pool", lambda e: e.tensor_scalar(out=t[:], in0=t[:], scalar1=1.0, scalar2=None,
                                                       op0=ALU.add), reads=[B], writes=[B])
            return t, B

        u_scr = dscr("u_scr", [NT, 512])
        y_scr = dscr("y_scr", [NT, 512], BF16)
        x1_scr = dscr("x1_scr", [NT, D])
        x2_scr = dscr("x2_scr", [NT, D])
        dbg_q = dscr("dbg_q", [NT, 1280])

        def src_rows(ti, l):
            if l == 0:
                return x_in[ti * 128:(ti + 1) * 128, :] if ti < NLT else ctx_in[(ti - NLT) * 128:(ti - NLT + 1) * 128, :]
            return x2_scr[ti * 128:(ti + 1) * 128, :]

        def rope_apply(st_bufs, src_ps, B_src, dst, B_dst, nh, rt, B_rt, tmp1, tmp2, B_tmp):
            S = src_ps.rearrange("p (h a b f) -> p h a b f", h=nh, a=2, b=2, f=16)
            O = dst.rearrange("p (h a b f) -> p h a b f", h=nh, a=2, b=2, f=16)
            T1 = tmp1.rearrange("p (h a b f) -> p h a b f", h=nh, a=2, b=2, f=16)
            T2 = tmp2.rearrange("p (h a b f) -> p h a b f", h=nh, a=2, b=2, f=16)
            for a in range(2):
                cos = rt[:, a * 32:a * 32 + 16].rearrange("p (x y f) -> p x y f", x=1, y=1).to_broadcast([128, nh, 2, 16])
                sin = rt[:, a * 32 + 16:a * 32 + 32].rearrange("p (x y f) -> p x y f", x=1, y=1).to_broadcast([128, nh, 2, 16])
                k.op("dve", lambda e, a=a, cos=cos: e.tensor_tensor(out=T1[:, :, a], in0=S[:, :, a], in1=cos, op=ALU.mult),
                     reads=[B_src, B_rt], writes=[B_tmp])
                k.op("dve", lambda e, a=a, sin=sin: e.tensor_tensor(out=T2[:, :, a], in0=S[:, :, a, ::-1, :], in1=sin, op=ALU.mult),
                     reads=[B_src, B_rt], writes=[B_tmp])
                k.op("dve", lambda e, a=a: e.tensor_tensor(out=O[:, :, a, 0, :], in0=T1[:, :, a, 0, :], in1=T2[:, :, a, 0, :], op=ALU.subtract),
                     reads=[B_tmp], writes=[B_dst])
                k.op("dve", lambda e, a=a: e.tensor_tensor(out=O[:, :, a, 1, :], in0=T1[:, :, a, 1, :], in1=T2[:, :, a, 1, :], op=ALU.add),
                     reads=[B_tmp], writes=[B_dst])


        def ln_epilogue(wk, y_parts, B_y, xo, B_xo, g_t, lng_t, lnb_t, dst_rows):
            tmp, B_tmp, z, B_z, stt, B_stt, o, B_o = wk
            for hf in range(2):
                sl = slice(hf * 512, (hf + 1) * 512)
                k.op("dve", lambda e, hf=hf, sl=sl: e.tensor_tensor(out=tmp[:, sl], in0=y_parts[hf], in1=g_t[0][:, sl], op=ALU.mult),
                     reads=[B_y[hf], g_t[1]], writes=[B_tmp])
            k.op("dve", lambda e: e.scalar_tensor_tensor(out=z[:], in0=xo[:], scalar=ALPHA, in1=tmp[:], op0=ALU.mult, op1=ALU.add),
                 reads=[B_xo, B_tmp], writes=[B_z])
            for hf in range(2):
                k.op("dve", lambda e, hf=hf: e.bn_stats(out=stt[:, hf * 6:(hf + 1) * 6], in_=z[:, hf * 512:(hf + 1) * 512]),
                     reads=[B_z], writes=[B_stt])
            k.op("dve", lambda e: e.bn_aggr(out=stt[:, 12:14], in_=stt[:, 0:12]), reads=[B_stt], writes=[B_stt])
            k.op("dve", lambda e: e.tensor_scalar(out=stt[:, 15:16], in0=stt[:, 13:14], scalar1=LN_EPS, scalar2=None, op0=ALU.add),
                 reads=[B_stt], writes=[B_stt])
            k.op("act", lambda e: e.sqrt(out=stt[:, 15:16], in_=stt[:, 15:16]), reads=[B_stt], writes=[B_stt])
            k.op("dve", lambda e: e.reciprocal(out=stt[:, 14:15], in_=stt[:, 15:16]), reads=[B_stt], writes=[B_stt])
            k.op("dve", lambda e: e.tensor_scalar(out=tmp[:], in0=z[:], scalar1=stt[:, 12:13], scalar2=stt[:, 14:15], op0=ALU.subtract, op1=ALU.mult),
                 reads=[B_z, B_stt], writes=[B_tmp])
            k.op("pool", lambda e: e.tensor_tensor(out=o[:], in0=tmp[:], in1=lng_t[0][:], op=ALU.mult),
                 reads=[B_tmp, lng_t[1]], writes=[B_o])
            k.op("pool", lambda e: e.tensor_tensor(out=o[:], in0=o[:], in1=lnb_t[0][:], op=ALU.add),
                 reads=[B_o, lnb_t[1]], writes=[B_o])
            k.dma("sp", dst_rows, o[:], reads=[B_o])

        def ln_work(st, pfx):
            tmp = sb(st, pfx + "_tmp", [128, D]); z = sb(st, pfx + "_z", [128, D])
            stt = sb(st, pfx + "_stt", [128, 16]); o = sb(st, pfx + "_o", [128, D])
            return (tmp, Buf(), z, Buf(), stt, Buf(), o, Buf())

        def outproj_ln1(st, l, catT_, B_catT_, w_out_dram, ntiles):
            w_bf = sb(st, "w_out_bf", [128, 8, D], BF16); B_w = Buf()
            for kc in range(8):
                k.dma("pool", w_bf[:, kc, :], w_out_dram[kc * 128:(kc + 1) * 128, :], writes=[B_w])
            g1 = [mod_bc(st, f"g1_{r}", l, r, 2) for r in range(2)]
            lng = load_bc(st, "ln1g", ln1_g[l:l + 1, :], D)
            lnb = load_bc(st, "ln1b", ln1_b[l:l + 1, :], D)
            wk = ln_work(st, "e1")
            xo = [sb(st, f"xo{i}", [128, D]) for i in range(2)]; B_xo = [Buf(), Buf()]
            py = [ps(st, f"py{i}", [128, 512]) for i in range(4)]; B_py = [Buf() for _ in range(4)]
            for ti in range(ntiles):
                i = ti % 2
                r = 0 if ti < NLT else 1
                T0 = ti * 128
                k.dma("sp", xo[i][:], src_rows(ti, l), writes=[B_xo[i]])
                for hf in range(2):
                    pi = i * 2 + hf
                    for kc in range(8):
                        k.op("pe", lambda e, kc=kc, hf=hf, pi=pi, T0=T0: e.matmul(
                            py[pi][:], lhsT=catT_[:, kc, T0:T0 + 128], rhs=w_bf[:, kc, hf * 512:(hf + 1) * 512],
                            start=(kc == 0), stop=(kc == 7)), reads=[B_catT_, B_w], writes=[B_py[pi]])
                ln_epilogue(wk, [py[i * 2][:], py[i * 2 + 1][:]], [B_py[i * 2], B_py[i * 2 + 1]], xo[i], B_xo[i],
                            g1[r], lng, lnb, x1_scr[T0:T0 + 128, :])
            k.barrier()

        def moe_phase(st, l, ntiles, dst):
            ntok = ntiles * 128
            h2T = sb(st, "h2T", [128, 8, ntok], BF16); B_h2T = Buf()
            gateT = sb(st, "gateT", [32, ntok], BF16); B_gateT = Buf()
            f_acc = sb(st, "f_acc", [128, ntiles, D]); B_facc = [Buf() for _ in range(ntiles)]
            sel = sb(st, "sel", [32, 32, 128], BF16); B_sel = Buf()
            k.dma("pool", sel[:], sel_in[:, :, :], writes=[B_sel])
            with ExitStack() as s1:
                Wr = sb(s1, "Wr", [128, 8, 36]); B_Wr = Buf()
                with nc.allow_non_contiguous_dma(reason="small router weights"):
                    k.dma("sp", Wr[:, :, 0:4], moe_wg[l].rearrange("(k p) n -> p k n", p=128), writes=[B_Wr])
                    for g in range(4):
                        k.dma("sp", Wr[:, :, 4 + g * 8:12 + g * 8], moe_we[l, g].rearrange("(k p) n -> p k n", p=128), writes=[B_Wr])
                Whi = sb(s1, "Whi", [128, 8, 36], BF16); Wlo = sb(s1, "Wlo", [128, 8, 36], BF16); B_Wsp = Buf()
                k.op("dve", lambda e: e.tensor_copy(out=Whi[:], in_=Wr[:]), reads=[B_Wr], writes=[B_Wsp])
                k.op("dve", lambda e: e.tensor_tensor(out=Wlo[:], in0=Wr[:], in1=Whi[:], op=ALU.subtract), reads=[B_Wr, B_Wsp], writes=[B_Wsp])
                rb = sb(s1, "rb", [128, 36]); B_rb = Buf()
                k.dma("sp", rb[:, 0:4], moe_bg[l:l + 1, :].partition_broadcast(128), writes=[B_rb])
                k.dma("sp", rb[:, 4:36], moe_be[l:l + 1, :].partition_broadcast(128), writes=[B_rb])
                sc2p = [mod_bc(s1, f"sc2p_{r}", l, r, 4, plus1=True) for r in range(2)]
                sh2 = [mod_bc(s1, f"sh2_{r}", l, r, 3) for r in range(2)]
                xt = [sb(s1, f"mx{i}", [128, D]) for i in range(2)]; B_xt = [Buf(), Buf()]
                hf32 = sb(s1, "mh", [128, D]); B_h = Buf()
                hhi = sb(s1, "hhi", [128, D], BF16); hlo = sb(s1, "hlo", [128, D], BF16); B_hs = Buf()
                hloT = sb(s1, "hloT", [128, 8, 128], BF16); B_hloT = Buf()
                pTh = ps(s1, "mpTh", [128, 8, 128], BF16); B_pTh = Buf()
                pTl = ps(s1, "mpTl", [128, 8, 128], BF16); B_pTl = Buf()
                pr = ps(s1, "mpr", [128, 512]); B_pr = Buf()
                pg = ps(s1, "mpg", [32, 1024], BF16); B_pg = Buf()
                lg = sb(s1, "lg", [128, 36]); B_lg = Buf()
                sm = sb(s1, "rsm", [128, 160]); B_sm = Buf()
                gates = sb(s1, "gates", [128, 32]); B_gates = Buf()
                gates_bf = sb(s1, "gates_bf", [128, 32], BF16); B_gbf = Buf()
                for ti in range(ntiles):
                    i = ti % 2
                    r = 0 if ti < NLT else 1
                    T0 = ti * 128
                    k.dma("sp", xt[i][:], x1_scr[T0:T0 + 128, :], writes=[B_xt[i]])
                    k.op("dve", lambda e, i=i, r=r: e.tensor_tensor(out=hf32[:], in0=xt[i][:], in1=sc2p[r][0][:], op=ALU.mult),
                         reads=[B_xt[i], sc2p[r][1]], writes=[B_h])
                    k.op("pool", lambda e, r=r: e.tensor_tensor(out=hf32[:], in0=hf32[:], in1=sh2[r][0][:], op=ALU.add),
                         reads=[B_h, sh2[r][1]], writes=[B_h])
                    k.op("pool", lambda e: e.tensor_copy(out=hhi[:], in_=hf32[:]), reads=[B_h], writes=[B_hs])
                    k.op("dve", lambda e: e.tensor_tensor(out=hlo[:], in0=hf32[:], in1=hhi[:], op=ALU.subtract), reads=[B_h, B_hs], writes=[B_hs])
                    for kc in range(8):
                        k.op("pe", lambda e, kc=kc: e.transpose(out=pTh[:, kc, :], in_=hhi[:, kc * 128:(kc + 1) * 128], identity=ident_b[:]),
                             reads=[B_hs, B_ident_b], writes=[B_pTh])
                    for kc in range(8):
                        k.op("pe", lambda e, kc=kc: e.transpose(out=pTl[:, kc, :], in_=hlo[:, kc * 128:(kc + 1) * 128], identity=ident_b[:]),
                             reads=[B_hs, B_ident_b], writes=[B_pTl])
                    k.op("act", lambda e, T0=T0: e.copy(out=h2T[:, :, T0:T0 + 128], in_=pTh[:]), reads=[B_pTh], writes=[B_h2T])
                    k.op("dve", lambda e: e.tensor_copy(out=hloT[:], in_=pTl[:]), reads=[B_pTl], writes=[B_hloT])
                    n_mm = 24
                    j = 0
                    for (A, BA, W) in ((None, B_h2T, Whi), (hloT, B_hloT, Whi), (None, B_h2T, Wlo)):
                        for kc in range(8):
                            lhs = h2T[:, kc, T0:T0 + 128] if A is None else A[:, kc, :]
                            k.op("pe", lambda e, lhs=lhs, W=W, kc=kc, j=j: e.matmul(pr[:, 0:36], lhsT=lhs, rhs=W[:, kc, :], start=(j == 0), stop=(j == 23)),
                                 reads=[BA, B_Wsp], writes=[B_pr])
                            j += 1
                    R = [B_lg, B_sm]
                    def dv(fn, reads=R, writes=(B_sm,)):
                        k.op("dve", fn, reads=list(reads), writes=list(writes))
                    k.op("dve", lambda e: e.tensor_tensor(out=lg[:], in0=pr[:, 0:36], in1=rb[:], op=ALU.add), reads=[B_pr, B_rb], writes=[B_lg])
                    dv(lambda e: e.reduce_max(out=sm[:, 0:1], in_=lg[:, 0:4], axis=AX.X))
                    dv(lambda e: e.tensor_scalar(out=sm[:, 1:2], in0=sm[:, 0:1], scalar1=-1.0, scalar2=None, op0=ALU.mult))
                    k.op("act", lambda e: e.activation(out=sm[:, 56:60], in_=lg[:, 0:4], func=AF.Exp, bias=sm[:, 1:2], scale=1.0, accum_out=sm[:, 2:3]),
                         reads=R, writes=[B_sm])
                    dv(lambda e: e.reciprocal(out=sm[:, 3:4], in_=sm[:, 2:3]))
                    dv(lambda e: e.tensor_scalar(out=sm[:, 4:8], in0=lg[:, 0:4], scalar1=sm[:, 0:1], scalar2=None, op0=ALU.is_equal))
                    le = lg[:, 4:36].rearrange("p (g e) -> p g e", g=4)
                    tmp48 = sm[:, 64:96].rearrange("p (g e) -> p g e", g=4)
                    ohb = sm[:, 4:8].rearrange("p (g x) -> p g x", x=1).to_broadcast([128, 4, 8])
                    dv(lambda e: e.tensor_tensor(out=tmp48, in0=le, in1=ohb, op=ALU.mult))
                    dv(lambda e: e.tensor_reduce(out=sm[:, 8:16], in_=sm[:, 64:96].rearrange("p (g e) -> p e g", g=4), axis=AX.X, op=ALU.add))
                    dv(lambda e: e.reduce_max(out=sm[:, 16:17], in_=sm[:, 8:16], axis=AX.X))
                    dv(lambda e: e.tensor_scalar(out=sm[:, 24:32], in0=sm[:, 8:16], scalar1=sm[:, 16:17], scalar2=None, op0=ALU.is_equal))
                    dv(lambda e: e.scalar_tensor_tensor(out=sm[:, 32:40], in0=sm[:, 24:32], scalar=-1e30, in1=sm[:, 8:16], op0=ALU.mult, op1=ALU.add))
                    dv(lambda e: e.reduce_max(out=sm[:, 17:18], in_=sm[:, 32:40], axis=AX.X))
                    dv(lambda e: e.tensor_scalar(out=sm[:, 40:48], in0=sm[:, 32:40], scalar1=sm[:, 17:18], scalar2=None, op0=ALU.is_equal))
                    dv(lambda e: e.tensor_tensor(out=sm[:, 18:19], in0=sm[:, 17:18], in1=sm[:, 16:17], op=ALU.subtract))
                    k.op("act", lambda e: e.activation(out=sm[:, 19:20], in_=sm[:, 18:19], func=AF.Exp), reads=R, writes=[B_sm])
                    dv(lambda e: e.tensor_scalar(out=sm[:, 20:21], in0=sm[:, 19:20], scalar1=1.0, scalar2=None, op0=ALU.add))
                    dv(lambda e: e.reciprocal(out=sm[:, 20:21], in_=sm[:, 20:21]))
                    dv(lambda e: e.tensor_tensor(out=sm[:, 21:22], in0=sm[:, 19:20], in1=sm[:, 20:21], op=ALU.mult))
                    dv(lambda e: e.tensor_tensor(out=sm[:, 22:23], in0=sm[:, 20:21], in1=sm[:, 3:4], op=ALU.mult))
                    dv(lambda e: e.tensor_tensor(out=sm[:, 23:24], in0=sm[:, 21:22], in1=sm[:, 3:4], op=ALU.mult))
                    dv(lambda e: e.tensor_scalar(out=sm[:, 48:56], in0=sm[:, 24:32], scalar1=sm[:, 22:23], scalar2=None, op0=ALU.mult))
                    dv(lambda e: e.scalar_tensor_tensor(out=sm[:, 48:56], in0=sm[:, 40:48], scalar=sm[:, 23:24], in1=sm[:, 48:56], op0=ALU.mult, op1=ALU.add))
                    geb = sm[:, 48:56].rearrange("p (x e) -> p x e", x=1).to_broadcast([128, 4, 8])
                    k.op("dve", lambda e: e.tensor_tensor(out=gates[:].rearrange("p (g e) -> p g e", g=4), in0=ohb, in1=geb, op=ALU.mult),
                         reads=R, writes=[B_gates])
                    k.op("dve", lambda e: e.tensor_copy(out=gates_bf[:], in_=gates[:]), reads=[B_gates], writes=[B_gbf])
                    k.op("pe", lambda e: e.transpose(out=pg[:, 0:128], in_=gates_bf[:], identity=ident_b[:]),
                         reads=[B_gbf, B_ident_b], writes=[B_pg])
                    k.op("act", lambda e, T0=T0: e.copy(out=gateT[:, T0:T0 + 128], in_=pg[:, 0:128]), reads=[B_pg], writes=[B_gateT])
                    if "dbg_gates" in dbg:
                        if ti == 0:
                            dbg_gates = dscr("dbg_gates", [NT, 32])
                        k.dma("sp", dbg_gates[T0:T0 + 128, :], gates[:], reads=[B_gates])
                k.barrier()
            if "stop_router" in dbg:
                return
            with ExitStack() as s2:
                w13 = [sb(s2, f"w13_{i}", [128, 8, 512], BF16) for i in range(2)]; B_w13 = [Buf(), Buf()]
                w2 = [sb(s2, f"w2_{i}", [128, 2, D], BF16) for i in range(2)]; B_w2 = [Buf(), Buf()]
                hh = [sb(s2, f"hh{i}", [128, 2, ntok], BF16) for i in range(2)]; B_hh = [Buf(), Buf()]
                s1t = [sb(s2, f"s1t{i}", [128, 512]) for i in range(2)]; B_s1t = [Buf(), Buf()]
                t3 = [sb(s2, f"t3{i}", [128, 512]) for i in range(2)]; B_t3 = [Buf(), Buf()]
                ph1 = [ps(s2, f"ph1_{i}", [128, 512]) for i in range(2)]; B_ph1 = [Buf(), Buf()]
                ph3 = [ps(s2, f"ph3_{i}", [128, 512]) for i in range(2)]; B_ph3 = [Buf(), Buf()]
                pgb = ps(s2, "pgb", [128, 512]); B_pgb = Buf()
                pf = [ps(s2, f"pf{i}", [128, 512]) for i in range(2)]; B_pf = [Buf(), Buf()]
                blocks = [(b0, min(512, ntok - b0)) for b0 in range(0, ntok, 512)]
                it = 0; itf = 0
                for e_ in range(32):
                    wi = e_ % 2
                    k.dma("pool", w13[wi][:, :, 0:256], moe_w1[l, e_].rearrange("(k p) f -> p k f", p=128), writes=[B_w13[wi]])
                    k.dma("pool", w13[wi][:, :, 256:512], moe_w3[l, e_].rearrange("(k p) f -> p k f", p=128), writes=[B_w13[wi]])
                    k.dma("pool", w2[wi][:], moe_w2[l, e_].rearrange("(c p) d -> p c d", p=128), writes=[B_w2[wi]])
                    for (b0, bn) in blocks:
                        k.op("pe", lambda e, e_=e_, b0=b0, bn=bn: e.matmul(pgb[:, 0:bn], lhsT=sel[:, e_, :], rhs=gateT[:, b0:b0 + bn], start=True, stop=True),
                             reads=[B_sel, B_gateT], writes=[B_pgb])
                        for fc in range(2):
                            i = it % 2; it += 1
                            for kc in range(8):
                                k.op("pe", lambda e, kc=kc, fc=fc, i=i, wi=wi, b0=b0, bn=bn: e.matmul(
                                    ph1[i][:, 0:bn], lhsT=w13[wi][:, kc, fc * 128:(fc + 1) * 128], rhs=h2T[:, kc, b0:b0 + bn],
                                    start=(kc == 0), stop=(kc == 7)), reads=[B_w13[wi], B_h2T], writes=[B_ph1[i]])
                            for kc in range(8):
                                k.op("pe", lambda e, kc=kc, fc=fc, i=i, wi=wi, b0=b0, bn=bn: e.matmul(
                                    ph3[i][:, 0:bn], lhsT=w13[wi][:, kc, 256 + fc * 128:256 + (fc + 1) * 128], rhs=h2T[:, kc, b0:b0 + bn],
                                    start=(kc == 0), stop=(kc == 7)), reads=[B_w13[wi], B_h2T], writes=[B_ph3[i]])
                            k.op("act", lambda e, i=i, bn=bn: e.activation(out=s1t[i][:, 0:bn], in_=ph1[i][:, 0:bn], func=AF.Silu),
                                 reads=[B_ph1[i]], writes=[B_s1t[i]])
                            k.op("dve", lambda e, i=i, bn=bn: e.tensor_tensor(out=t3[i][:, 0:bn], in0=s1t[i][:, 0:bn], in1=ph3[i][:, 0:bn], op=ALU.mult),
                                 reads=[B_s1t[i], B_ph3[i]], writes=[B_t3[i]])
                            k.op("dve", lambda e, i=i, bn=bn, fc=fc, wi=wi, b0=b0: e.tensor_tensor(out=hh[wi][:, fc, b0:b0 + bn], in0=t3[i][:, 0:bn], in1=pgb[:, 0:bn], op=ALU.mult),
                                 reads=[B_t3[i], B_pgb], writes=[B_hh[wi]])
                    for tt in range(ntiles):
                        for dc in range(2):
                            j = itf % 2; itf += 1
                            for fc in range(2):
                                k.op("pe", lambda e, fc=fc, j=j, wi=wi, tt=tt, dc=dc: e.matmul(
                                    pf[j][:], lhsT=hh[wi][:, fc, tt * 128:(tt + 1) * 128], rhs=w2[wi][:, fc, dc * 512:(dc + 1) * 512],
                                    start=(fc == 0), stop=(fc == 1)), reads=[B_hh[wi], B_w2[wi]], writes=[B_pf[j]])
                            if e_ == 0:
                                k.op("dve", lambda e, j=j, tt=tt, dc=dc: e.tensor_copy(out=f_acc[:, tt, dc * 512:(dc + 1) * 512], in_=pf[j][:]),
                                     reads=[B_pf[j]], writes=[B_facc[tt]])
                            else:
                                k.op("dve", lambda e, j=j, tt=tt, dc=dc: e.tensor_tensor(out=f_acc[:, tt, dc * 512:(dc + 1) * 512],
                                     in0=f_acc[:, tt, dc * 512:(dc + 1) * 512], in1=pf[j][:], op=ALU.add),
                                     reads=[B_pf[j], B_facc[tt]], writes=[B_facc[tt]])
                k.barrier()
            if "stop_experts" in dbg:
                return
            with ExitStack() as s3:
                g2 = [mod_bc(s3, f"g2_{r}", l, r, 5) for r in range(2)]
                lng = load_bc(s3, "ln2g", ln2_g[l:l + 1, :], D)
                lnb = load_bc(s3, "ln2b", ln2_b[l:l + 1, :], D)
                wk = ln_work(s3, "e2")
                xo = [sb(s3, f"x1o{i}", [128, D]) for i in range(2)]; B_xo = [Buf(), Buf()]
                for ti in range(ntiles):
                    i = ti % 2
                    r = 0 if ti < NLT else 1
                    T0 = ti * 128
                    k.dma("sp", xo[i][:], x1_scr[T0:T0 + 128, :], writes=[B_xo[i]])
                    if "dbg_f" in dbg:
                        if ti == 0:
                            dbg_f = dscr("dbg_f", [NT, D])
                        k.dma("sp", dbg_f[T0:T0 + 128, :], f_acc[:, ti, :], reads=[B_facc[ti]])
                    ln_epilogue(wk, [f_acc[:, ti, 0:512], f_acc[:, ti, 512:1024]], [B_facc[ti], B_facc[ti]], xo[i], B_xo[i],
                                g2[r], lng, lnb, dst[T0:T0 + 128, :])
                k.barrier()


        def ssm_phase(st, catT_, B_catT_):
            PI = math.pi
            TWO_PI = 2.0 * math.pi
            def bc3(ap2, n):
                P_, G_ = ap2.shape
                return ap2.rearrange("p (g x) -> p g x", x=1).to_broadcast([P_, G_, n])
            I32 = mybir.dt.int32
            INV2PI = 1.0 / TWO_PI

            def sincos(ang_ap, shape, s_out, c_out, tmps, B_in, B_out, B_tmp):
                y, yi, yf = tmps
                dvt = lambda fn: k.op("dve", fn, reads=[B_in, B_tmp, B_out], writes=[B_tmp])
                dvt(lambda e: e.tensor_scalar(out=y, in0=ang_ap, scalar1=INV2PI, scalar2=32.5, op0=ALU.mult, op1=ALU.add))
                dvt(lambda e: e.tensor_copy(out=yi, in_=y))
                dvt(lambda e: e.tensor_copy(out=yf, in_=yi))
                dvt(lambda e: e.tensor_tensor(out=y, in0=y, in1=yf, op=ALU.subtract))
                dvt(lambda e: e.scalar_tensor_tensor(out=yf, in0=y, scalar=0.0, in1=y, op0=ALU.is_lt, op1=ALU.add))
                k.op("act", lambda e: e.activation(out=s_out, in_=yf, func=AF.Sin, bias=negpi[0:shape[0], :], scale=TWO_PI),
                     reads=[B_tmp, B_np], writes=[B_out])
                dvt(lambda e: e.tensor_scalar(out=y, in0=yf, scalar1=0.25, scalar2=None, op0=ALU.add))
                dvt(lambda e: e.scalar_tensor_tensor(out=yf, in0=y, scalar=1.0, in1=y, op0=ALU.is_ge, op1=ALU.subtract))
                k.op("act", lambda e: e.activation(out=c_out, in_=yf, func=AF.Sin, bias=negpi[0:shape[0], :], scale=-TWO_PI),
                     reads=[B_tmp, B_np], writes=[B_out])

            ar = sb(st, "ar", [64, 64]); ai = sb(st, "ai", [64, 64]); ls = sb(st, "ls", [64, 64]); B_par = Buf()
            with nc.allow_non_contiguous_dma(reason="ssm params"):
                k.dma("sp", ar[:], a_re.rearrange("d g p -> p (d g)"), writes=[B_par])
                k.dma("sp", ai[:], a_im.rearrange("d g p -> p (d g)"), writes=[B_par])
            k.dma("sp", ls[:], log_step.rearrange("(x d) g -> x (d g)", x=1).partition_broadcast(64), writes=[B_par])
            negpi = sb(st, "negpi", [128, 1]); B_np = Buf()
            k.op("dve", lambda e: e.memset(negpi[:], -PI), writes=[B_np])
            kv = sb(st, "kv", [64, 16, 64]); B_kv = Buf()
            k.dma("sp", kv[:], kval_in.rearrange("p (k g) -> p k g", k=16), writes=[B_kv])
            mrow = sb(st, "mrow", [64, 288]); B_mrow = Buf()
            k.dma("sp", mrow[:], mrow_in[:, :], writes=[B_mrow])
            maskF = sb(st, "maskF", [128, 128]); maskB = sb(st, "maskB", [128, 128]); B_mk = Buf()
            k.dma("sp", maskF[:], maskF_in[:, :], writes=[B_mk])
            k.dma("sp", maskB[:], maskB_in[:, :], writes=[B_mk])
            dar = sb(st, "dar", [64, 64]); dai = sb(st, "dai", [64, 64]); B_d = Buf()
            k.op("act", lambda e: e.activation(out=ls[:], in_=ls[:], func=AF.Exp), reads=[B_par], writes=[B_par])
            k.op("dve", lambda e: e.tensor_tensor(out=dar[:], in0=ls[:], in1=ar[:], op=ALU.mult), reads=[B_par], writes=[B_d])
            k.op("dve", lambda e: e.tensor_tensor(out=dai[:], in0=ls[:], in1=ai[:], op=ALU.mult), reads=[B_par, B_d], writes=[B_d])
            LR = sb(st, "LR", [64, 16, 64]); LI = sb(st, "LI", [64, 16, 64]); MG = sb(st, "MG", [64, 16, 64]); B_L = Buf()
            th8 = sb(st, "th8", [64, 64]); B_th8 = Buf()
            k.op("dve", lambda e: e.tensor_scalar(out=th8[:], in0=dai[:], scalar1=8.0, scalar2=None, op0=ALU.mult), reads=[B_d], writes=[B_th8])
            with ExitStack() as t0:
                ang = sb(t0, "ang", [64, 16, 64]); a2 = sb(t0, "a2", [64, 16, 64]); B_ang = Buf()
                dai_b = dai[:].rearrange("p (x g) -> p x g", x=1).to_broadcast([64, 16, 64])
                dar_b = dar[:].rearrange("p (x g) -> p x g", x=1).to_broadcast([64, 16, 64])
                k.op("dve", lambda e: e.tensor_tensor(out=MG[:], in0=kv[:], in1=dar_b, op=ALU.mult), reads=[B_kv, B_d], writes=[B_L])
                k.op("act", lambda e: e.activation(out=MG[:], in_=MG[:], func=AF.Exp), reads=[B_L], writes=[B_L])
                k.op("dve", lambda e: e.tensor_tensor(out=ang[:], in0=kv[:], in1=dai_b, op=ALU.mult), reads=[B_kv, B_d], writes=[B_ang])
                a3 = sb(t0, "a3", [64, 16, 64], I32); a4 = sb(t0, "a4", [64, 16, 64])
                sincos(ang[:], [64, 16, 64], LI[:], LR[:], (a2[:], a3[:], a4[:]), B_ang, B_L, B_ang)
                k.op("dve", lambda e: e.tensor_tensor(out=LR[:], in0=LR[:], in1=MG[:], op=ALU.mult), reads=[B_L], writes=[B_L])
                k.op("dve", lambda e: e.tensor_tensor(out=LI[:], in0=LI[:], in1=MG[:], op=ALU.mult), reads=[B_L], writes=[B_L])
                k.barrier()
            cre = sb(st, "cre", [64, 64]); cim = sb(st, "cim", [64, 64]); B_c = Buf()
            with ExitStack() as t0:
                nr = sb(t0, "nr", [64, 64]); den = sb(t0, "den", [64, 64]); tq = sb(t0, "tq", [64, 64]); B_t = Buf()
                L1r = LR[:, 8, :]; L1i = LI[:, 8, :]
                dv = lambda fn: k.op("dve", fn, reads=[B_t, B_L, B_par, B_c], writes=[B_t, B_c])
                dv(lambda e: e.tensor_scalar(out=nr[:], in0=L1r, scalar1=-1.0, scalar2=None, op0=ALU.add))
                dv(lambda e: e.tensor_tensor(out=den[:], in0=ar[:], in1=ar[:], op=ALU.mult))
                dv(lambda e: e.tensor_tensor(out=tq[:], in0=ai[:], in1=ai[:], op=ALU.mult))
                dv(lambda e: e.tensor_tensor(out=den[:], in0=den[:], in1=tq[:], op=ALU.add))
                dv(lambda e: e.reciprocal(out=den[:], in_=den[:]))
                dv(lambda e: e.tensor_tensor(out=cre[:], in0=nr[:], in1=ar[:], op=ALU.mult))
                dv(lambda e: e.tensor_tensor(out=tq[:], in0=L1i, in1=ai[:], op=ALU.mult))
                dv(lambda e: e.tensor_tensor(out=cre[:], in0=cre[:], in1=tq[:], op=ALU.add))
                dv(lambda e: e.tensor_tensor(out=cre[:], in0=cre[:], in1=den[:], op=ALU.mult))
                dv(lambda e: e.tensor_tensor(out=cim[:], in0=L1i, in1=ar[:], op=ALU.mult))
                dv(lambda e: e.tensor_tensor(out=tq[:], in0=nr[:], in1=ai[:], op=ALU.mult))
                dv(lambda e: e.tensor_tensor(out=cim[:], in0=cim[:], in1=tq[:], op=ALU.subtract))
                dv(lambda e: e.tensor_tensor(out=cim[:], in0=cim[:], in1=den[:], op=ALU.mult))
                k.barrier()
            UT_all = sb(st, "UT_all", [128, 32, 288], BF16); B_UT = Buf()
            NB = ((0, 128), (128, 128), (256, 32))
            u8v = u_scr.rearrange("(n j) f -> n (j f)", j=8)
            with ExitStack() as t0:
                U8 = sb(t0, "U8", [128, 3, 4096]); B_U8 = Buf()
                U8b = sb(t0, "U8b", [128, 3, 4096], BF16); B_U8b = Buf()
                pU = [ps(t0, f"pU{i}", [128, 1024], BF16) for i in range(2)]; B_pU = [Buf(), Buf()]
                for bi, (n0, nb) in enumerate(NB):
                    k.dma("sp", U8[0:nb, bi, :], u8v[n0:n0 + nb, :], writes=[B_U8])
                    ov = U8b[0:nb, bi, :].rearrange("p (g j c) -> p j g c", g=32, j=8, c=16)
                    iv = U8[0:nb, bi, :].rearrange("p (j g c) -> p j g c", g=32, j=8, c=16)
                    k.op("pool" if bi == 1 else "act", (lambda e, ov=ov, iv=iv: e.tensor_copy(out=ov, in_=iv)) if bi == 1 else
                         (lambda e, ov=ov, iv=iv: e.copy(out=ov, in_=iv)), reads=[B_U8], writes=[B_U8b])
                for g in range(32):
                    i = g % 2
                    for bi, (n0, nb) in enumerate(NB):
                        src = U8b[0:nb, bi, g * 128:(g + 1) * 128]
                        k.op("pe", lambda e, i=i, src=src, n0=n0, nb=nb: e.transpose(out=pU[i][:, n0:n0 + nb], in_=src, identity=ident_b[0:nb, 0:nb]),
                             reads=[B_U8b, B_ident_b], writes=[B_pU[i]])
                    k.op("act", lambda e, i=i, g=g: e.copy(out=UT_all[:, g, :], in_=pU[i][:, 0:288]), reads=[B_pU[i]], writes=[B_UT])
                k.barrier()
            COr = sb(st, "COr", [64, 2, 32, 128], BF16); COi = sb(st, "COi", [64, 2, 32, 128], BF16); B_CO = Buf()
            T_all = sb(st, "T_all", [128, 32, 128], BF16); B_T = Buf()
            WinT = sb(st, "WinT", [128, 2, 32, 128], BF16); B_WinT = Buf()
            with ExitStack() as t0:
                BLr = sb(t0, "BLr", [64, 32, 128], BF16); BLi = sb(t0, "BLi", [64, 32, 128], BF16); B_BL = Buf()
                CTr = sb(t0, "CTr", [64, 32, 128], BF16); CTi = sb(t0, "CTi", [64, 32, 128], BF16); B_CT = Buf()
                Br = sb(t0, "Br", [64, 64, 16]); Bi = sb(t0, "Bi", [64, 64, 16]); B_B = Buf()
                Cr = sb(t0, "Cr", [64, 64, 16]); Ci = sb(t0, "Ci", [64, 64, 16]); B_C = Buf()
                Bbr = sb(t0, "Bbr", [64, 64, 16]); Bbi = sb(t0, "Bbi", [64, 64, 16]); B_Bb = Buf()
                with nc.allow_non_contiguous_dma(reason="ssm B/C tables"):
                    for d in range(2):
                        k.dma("sp", Br[:, d * 32:(d + 1) * 32, :], b_re[d].rearrange("g p c -> p g c"), writes=[B_B])
                        k.dma("sp", Bi[:, d * 32:(d + 1) * 32, :], b_im[d].rearrange("g p c -> p g c"), writes=[B_B])
                        for gb in range(4):
                            sl = slice(d * 32 + gb * 8, d * 32 + gb * 8 + 8)
                            k.dma("sp", Cr[:, sl, :], c_re[d, gb * 8:(gb + 1) * 8].rearrange("g c p -> p g c"), writes=[B_C])
                            k.dma("act", Ci[:, sl, :], c_im[d, gb * 8:(gb + 1) * 8].rearrange("g c p -> p g c"), writes=[B_C])
                t1 = sb(t0, "t1", [64, 64, 16]); t2 = sb(t0, "t2", [64, 64, 16]); B_t12 = Buf()
                creb = bc3(cre[:], 16); cimb = bc3(cim[:], 16)
                dv = lambda fn: k.op("dve", fn, reads=[B_B, B_c, B_t12, B_Bb], writes=[B_t12, B_Bb])
                dv(lambda e: e.tensor_tensor(out=t1[:], in0=Br[:], in1=creb, op=ALU.mult))
                dv(lambda e: e.tensor_tensor(out=t2[:], in0=Bi[:], in1=cimb, op=ALU.mult))
                dv(lambda e: e.tensor_tensor(out=Bbr[:], in0=t1[:], in1=t2[:], op=ALU.subtract))
                dv(lambda e: e.tensor_tensor(out=t1[:], in0=Bi[:], in1=creb, op=ALU.mult))
                dv(lambda e: e.tensor_tensor(out=t2[:], in0=Br[:], in1=cimb, op=ALU.mult))
                dv(lambda e: e.tensor_tensor(out=Bbi[:], in0=t1[:], in1=t2[:], op=ALU.add))
                ta = sb(t0, "ta", [64, 32, 16]); tb_ = sb(t0, "tb", [64, 32, 16]); B_tab = Buf()
                tc_ = sb(t0, "tc", [64, 32, 16]); td_ = sb(t0, "td", [64, 32, 16]); B_tcd = Buf()
                pTd = [ps(t0, f"pTd{i}", [128, 512]) for i in range(2)]; B_pTd = [Buf(), Buf()]
                pW = [ps(t0, f"pW{i}", [128, 8, 128], BF16) for i in range(2)]; B_pW = [Buf(), Buf()]
                tt = sb(t0, "tt", [128, 128]); B_tt = Buf()
                for d in range(2):
                    dsl = slice(d * 32, (d + 1) * 32)
                    for j in range(8):
                        e_ = (7 - j) if d == 0 else j
                        lr = bc3(LR[:, e_ + 7, dsl], 16); li = bc3(LI[:, e_ + 7, dsl], 16)
                        o_r = BLr[:, :, j * 16:(j + 1) * 16]; o_i = BLi[:, :, j * 16:(j + 1) * 16]
                        dv2 = lambda fn: k.op("dve", fn, reads=[B_Bb, B_L, B_tab, B_BL], writes=[B_tab, B_BL])
                        dv2(lambda e, lr=lr: e.tensor_tensor(out=ta[:], in0=Bbr[:, dsl, :], in1=lr, op=ALU.mult))
                        dv2(lambda e, li=li: e.tensor_tensor(out=tb_[:], in0=Bbi[:, dsl, :], in1=li, op=ALU.mult))
                        dv2(lambda e, o_r=o_r: e.tensor_tensor(out=o_r, in0=ta[:], in1=tb_[:], op=ALU.subtract))
                        dv2(lambda e, lr=lr: e.tensor_tensor(out=ta[:], in0=Bbi[:, dsl, :], in1=lr, op=ALU.mult))
                        dv2(lambda e, li=li: e.tensor_tensor(out=tb_[:], in0=Bbr[:, dsl, :], in1=li, op=ALU.mult))
                        dv2(lambda e, o_i=o_i: e.tensor_tensor(out=o_i, in0=ta[:], in1=tb_[:], op=ALU.add))
                        f_ = (j - 7) if d == 0 else -j
                        for (kk, o_r, o_i, BO) in ((f_ + 7, CTr[:, :, j * 16:(j + 1) * 16], CTi[:, :, j * 16:(j + 1) * 16], B_CT),
                                                   (f_ + 15, COr[:, d, :, j * 16:(j + 1) * 16], COi[:, d, :, j * 16:(j + 1) * 16], B_CO)):
                            lr = bc3(LR[:, kk, dsl], 16); li = bc3(LI[:, kk, dsl], 16)
                            pl = lambda fn, BO=BO: k.op("pool", fn, reads=[B_C, B_L, B_tcd, BO], writes=[B_tcd, BO])
                            pl(lambda e, lr=lr: e.tensor_tensor(out=tc_[:], in0=Cr[:, dsl, :], in1=lr, op=ALU.mult))
                            pl(lambda e, li=li: e.tensor_tensor(out=td_[:], in0=Ci[:, dsl, :], in1=li, op=ALU.mult))
                            pl(lambda e, o_r=o_r: e.tensor_tensor(out=o_r, in0=tc_[:], in1=td_[:], op=ALU.subtract))
                            pl(lambda e, lr=lr: e.tensor_tensor(out=tc_[:], in0=Ci[:, dsl, :], in1=lr, op=ALU.mult))
                            pl(lambda e, li=li: e.tensor_tensor(out=td_[:], in0=Cr[:, dsl, :], in1=li, op=ALU.mult))
                            pl(lambda e: e.tensor_tensor(out=tc_[:], in0=tc_[:], in1=td_[:], op=ALU.add))
                            pl(lambda e, o_i=o_i: e.tensor_scalar(out=o_i, in0=tc_[:], scalar1=-1.0, scalar2=None, op0=ALU.mult))
                    for g in range(32):
                        i = g % 2
                        k.op("pe", lambda e, i=i, g=g: e.matmul(pTd[i][:, 0:128], lhsT=BLr[:, g, :], rhs=CTr[:, g, :], start=True, stop=False),
                             reads=[B_BL, B_CT], writes=[B_pTd[i]])
                        k.op("pe", lambda e, i=i, g=g: e.matmul(pTd[i][:, 0:128], lhsT=BLi[:, g, :], rhs=CTi[:, g, :], start=False, stop=True),
                             reads=[B_BL, B_CT], writes=[B_pTd[i]])
                        if d == 0:
                            k.op("dve", lambda e, i=i, g=g: e.tensor_tensor(out=T_all[:, g, :], in0=pTd[i][:, 0:128], in1=maskF[:], op=ALU.mult),
                                 reads=[B_pTd[i], B_mk], writes=[B_T])
                        else:
                            k.op("dve", lambda e, i=i: e.tensor_tensor(out=tt[:], in0=pTd[i][:, 0:128], in1=maskB[:], op=ALU.mult),
                                 reads=[B_pTd[i], B_mk], writes=[B_tt])
                            k.op("dve", lambda e, g=g: e.tensor_tensor(out=T_all[:, g, :], in0=T_all[:, g, :], in1=tt[:], op=ALU.add),
                                 reads=[B_tt, B_T], writes=[B_T])
                        k.op("pe", lambda e, i=i, g=g: e.transpose(out=pW[i][:, 0, 0:64], in_=BLr[:, g, :], identity=ident_b[0:64, 0:64]),
                             reads=[B_BL, B_ident_b], writes=[B_pW[i]])
                        k.op("pe", lambda e, i=i, g=g: e.transpose(out=pW[i][:, 0, 64:128], in_=BLi[:, g, :], identity=ident_b[0:64, 0:64]),
                             reads=[B_BL, B_ident_b], writes=[B_pW[i]])
                        k.op("act", lambda e, i=i, g=g, d=d: e.copy(out=WinT[:, d, g, :], in_=pW[i][:, 0, :]), reads=[B_pW[i]], writes=[B_WinT])
                k.barrier()
            with ExitStack() as t0:
                Yt = sb(t0, "Yt", [128, 3, 4096], BF16); B_Yt = Buf()
                pX = [ps(t0, f"pX{i}", [64, 512]) for i in range(2)]; B_pX = [Buf(), Buf()]
                pY = ps(t0, "pY", [128, 512]); B_pY = Buf()
                pYt = ps(t0, "pYt", [128, 8, 128], BF16); B_pYt = Buf()
                XS = sb(t0, "XS", [64, 2, 288]); B_XS = Buf()
                base = sb(t0, "base", [64, 288]); sarg = sb(t0, "sarg", [64, 288]); B_tr = Buf(); B_tr2 = Buf()
                sargi = sb(t0, "sargi", [64, 288], mybir.dt.int32); sargf = sb(t0, "sargf", [64, 288])
                sn = sb(t0, "sn", [64, 288]); cs = sb(t0, "cs", [64, 288]); B_sc = Buf()
                RR = sb(t0, "RR", [64, 2, 288]); B_RR = Buf()
                q1 = sb(t0, "q1", [64, 288]); q2 = sb(t0, "q2", [64, 288]); B_q = Buf()
                SS = sb(t0, "SS", [64, 2, 288]); B_SS = Buf()
                Sp = sb(t0, "Sp", [64, 2, 2, 288], BF16); B_Sp = Buf()
                Ysb = sb(t0, "Ysb", [128, 288], BF16); B_Ysb = Buf()
                k.op("dve", lambda e: e.memset(Sp[:], 0.0), writes=[B_Sp])
                for g in range(32):
                    for d in range(2):
                        dg = d * 32 + g
                        for c2 in range(2):
                            k.op("pe", lambda e, c2=c2, d=d, g=g: e.matmul(pX[c2][:, 0:288], lhsT=WinT[:, d, g, c2 * 64:(c2 + 1) * 64], rhs=UT_all[:, g, :],
                                                                           start=True, stop=True), reads=[B_WinT, B_UT], writes=[B_pX[c2]])
                            if d == 0:
                                k.op("act", lambda e, c2=c2: e.copy(out=XS[:, c2, :], in_=pX[c2][:, 0:288]), reads=[B_pX[c2]], writes=[B_XS])
                            else:
                                k.op("act", lambda e, c2=c2: e.copy(out=XS[:, c2, 0:32], in_=pX[c2][:, 31::-1]), reads=[B_pX[c2]], writes=[B_XS])
                                k.op("act", lambda e, c2=c2: e.copy(out=XS[:, c2, 32:288], in_=pX[c2][:, 287:31:-1]), reads=[B_pX[c2]], writes=[B_XS])
                        k.op("dve", lambda e, dg=dg: e.tensor_scalar(out=base[:], in0=mrow[:], scalar1=th8[:, dg:dg + 1], scalar2=None, op0=ALU.mult),
                             reads=[B_mrow, B_th8], writes=[B_tr])
                        sincos(base[:], [64, 288], sn[:], cs[:], (sarg[:], sargi[:], sargf[:]), B_tr, B_sc, B_tr2)
                        dv = lambda fn: k.op("dve", fn, reads=[B_XS, B_sc, B_q, B_RR, B_SS, B_L], writes=[B_q, B_RR, B_SS])
                        dv(lambda e: e.tensor_tensor(out=q1[:], in0=cs[:], in1=XS[:, 0, :], op=ALU.mult))
                        dv(lambda e: e.tensor_tensor(out=q2[:], in0=sn[:], in1=XS[:, 1, :], op=ALU.mult))
                        dv(lambda e: e.tensor_tensor(out=q1[:], in0=q1[:], in1=q2[:], op=ALU.add))
                        dv(lambda e, dg=dg: e.tensor_tensor_scan(out=RR[:, 0, :], data0=MG[:, 15, dg:dg + 1].to_broadcast([64, 288]), data1=q1[:], initial=0.0, op0=ALU.mult, op1=ALU.add))
                        dv(lambda e: e.tensor_tensor(out=q1[:], in0=cs[:], in1=XS[:, 1, :], op=ALU.mult))
                        dv(lambda e: e.tensor_tensor(out=q2[:], in0=sn[:], in1=XS[:, 0, :], op=ALU.mult))
                        dv(lambda e: e.tensor_tensor(out=q1[:], in0=q1[:], in1=q2[:], op=ALU.subtract))
                        dv(lambda e, dg=dg: e.tensor_tensor_scan(out=RR[:, 1, :], data0=MG[:, 15, dg:dg + 1].to_broadcast([64, 288]), data1=q1[:], initial=0.0, op0=ALU.mult, op1=ALU.add))
                        dv(lambda e: e.tensor_tensor(out=q1[:], in0=cs[:], in1=RR[:, 0, :], op=ALU.mult))
                        dv(lambda e: e.tensor_tensor(out=q2[:], in0=sn[:], in1=RR[:, 1, :], op=ALU.mult))
                        dv(lambda e: e.tensor_tensor(out=SS[:, 0, :], in0=q1[:], in1=q2[:], op=ALU.subtract))
                        dv(lambda e: e.tensor_tensor(out=q1[:], in0=cs[:], in1=RR[:, 1, :], op=ALU.mult))
                        dv(lambda e: e.tensor_tensor(out=q2[:], in0=sn[:], in1=RR[:, 0, :], op=ALU.mult))
                        dv(lambda e: e.tensor_tensor(out=SS[:, 1, :], in0=q1[:], in1=q2[:], op=ALU.add))
                        for c2 in range(2):
                            if d == 0:
                                k.op("act", lambda e, c2=c2: e.copy(out=Sp[:, 0, c2, 1:288], in_=SS[:, c2, 0:287]), reads=[B_SS], writes=[B_Sp])
                            else:
                                k.op("act", lambda e, c2=c2: e.copy(out=Sp[:, 1, c2, 0:31], in_=SS[:, c2, 30::-1]), reads=[B_SS], writes=[B_Sp])
                                k.op("act", lambda e, c2=c2: e.copy(out=Sp[:, 1, c2, 32:288], in_=SS[:, c2, 286:30:-1]), reads=[B_SS], writes=[B_Sp])
                    k.op("pe", lambda e, g=g: e.matmul(pY[:, 0:288], lhsT=T_all[:, g, :], rhs=UT_all[:, g, :], start=True, stop=False),
                         reads=[B_T, B_UT], writes=[B_pY])
                    for d in range(2):
                        k.op("pe", lambda e, g=g, d=d: e.matmul(pY[:, 0:288], lhsT=COr[:, d, g, :], rhs=Sp[:, d, 0, :], start=False, stop=False),
                             reads=[B_CO, B_Sp], writes=[B_pY])
                        k.op("pe", lambda e, g=g, d=d: e.matmul(pY[:, 0:288], lhsT=COi[:, d, g, :], rhs=Sp[:, d, 1, :], start=False, stop=(d == 1)),
                             reads=[B_CO, B_Sp], writes=[B_pY])
                    k.op("act", lambda e: e.copy(out=Ysb[:], in_=pY[:, 0:288]), reads=[B_pY], writes=[B_Ysb])
                    for bi, (n0, nb) in enumerate(NB):
                        k.op("pe", lambda e, bi=bi, n0=n0, nb=nb: e.transpose(out=pYt[0:nb, bi, :], in_=Ysb[:, n0:n0 + nb], identity=ident_b[:]),
                             reads=[B_Ysb, B_ident_b], writes=[B_pYt])
                    for bi, (n0, nb) in enumerate(NB):
                        dst = Yt[0:nb, bi, :].rearrange("p (j f) -> p j f", j=8)[:, :, g * 16:(g + 1) * 16]
                        k.op("dve", lambda e, bi=bi, nb=nb, dst=dst: e.tensor_copy(out=dst, in_=pYt[0:nb, bi, :].rearrange("p (j c) -> p j c", j=8)),
                             reads=[B_pYt], writes=[B_Yt])
                y8v = y_scr.rearrange("(n j) f -> n (j f)", j=8)
                for bi, (n0, nb) in enumerate(NB):
                    k.dma("sp", y8v[n0:n0 + nb, :], Yt[0:nb, bi, :], reads=[B_Yt])
                k.barrier()


        def ssm_post(st, catT_, B_catT_):
            GC = 2.0 * math.sqrt(2.0 / math.pi)
            gw = sb(st, "gluw", [128, 4, 512], BF16); B_gw = Buf()
            k.dma("pool", gw[:], glu_w.rearrange("(k p) n -> p k n", p=128), writes=[B_gw])
            gb = sb(st, "glub", [128, 4]); B_gb = Buf()
            with nc.allow_non_contiguous_dma(reason="tiny bias"):
                k.dma("sp", gb[:], glu_b[0, :].rearrange("(c p) -> p c", p=128), writes=[B_gb])
            dbc = load_bc(st, "dskip", ssm_d[0:1, :], 512)
            yt = [sb(st, f"py{i}", [128, 512], BF16) for i in range(2)]; B_yt = [Buf(), Buf()]
            ut = [sb(st, f"pu{i}", [128, 512]) for i in range(2)]; B_ut = [Buf(), Buf()]
            xx = sb(st, "pxx", [128, 512]); B_xx = Buf()
            ww = sb(st, "pww", [128, 512]); B_ww = Buf()
            sg = sb(st, "psg", [128, 512]); B_sg = Buf()
            g_bf = sb(st, "pg_bf", [128, 512], BF16); B_g = Buf()
            gT = sb(st, "pgT", [128, 4, 128], BF16); B_gT = Buf()
            s2 = sb(st, "ps2", [128, 4, 128]); B_s2 = Buf()
            pGT = ps(st, "pGT", [128, 8, 128], BF16); B_pGT = Buf()
            pz = ps(st, "pz", [128, 4, 128]); B_pz = Buf()
            for ti in range(NTILE):
                i = ti % 2
                T0 = ti * 128
                row0 = T0 + CTX if ti < NLT else T0 - SEQ
                k.dma("sp", yt[i][:], y_scr[row0:row0 + 128, :], writes=[B_yt[i]])
                k.dma("sp", ut[i][:], u_scr[row0:row0 + 128, :], writes=[B_ut[i]])
                k.op("dve", lambda e, i=i: e.tensor_tensor(out=xx[:], in0=ut[i][:], in1=dbc[0][:], op=ALU.mult), reads=[B_ut[i], dbc[1]], writes=[B_xx])
                k.op("dve", lambda e, i=i: e.tensor_tensor(out=xx[:], in0=xx[:], in1=yt[i][:], op=ALU.add), reads=[B_xx, B_yt[i]], writes=[B_xx])
                k.op("pool", lambda e: e.tensor_tensor(out=ww[:], in0=xx[:], in1=xx[:], op=ALU.mult), reads=[B_xx], writes=[B_ww])
                k.op("pool", lambda e: e.tensor_scalar(out=ww[:], in0=ww[:], scalar1=0.044715, scalar2=1.0, op0=ALU.mult, op1=ALU.add), reads=[B_ww], writes=[B_ww])
                k.op("pool", lambda e: e.tensor_tensor(out=ww[:], in0=ww[:], in1=xx[:], op=ALU.mult), reads=[B_ww, B_xx], writes=[B_ww])
                k.op("act", lambda e: e.activation(out=sg[:], in_=ww[:], func=AF.Sigmoid, scale=GC), reads=[B_ww], writes=[B_sg])
                k.op("dve", lambda e: e.tensor_tensor(out=g_bf[:], in0=xx[:], in1=sg[:], op=ALU.mult), reads=[B_xx, B_sg], writes=[B_g])
                if "dbg_g" in dbg:
                    if ti == 0:
                        dbg_g = dscr("dbg_g", [NT, 512], BF16)
                    k.dma("sp", dbg_g[T0:T0 + 128, :], g_bf[:], reads=[B_g])
                for kc in range(4):
                    k.op("pe", lambda e, kc=kc: e.transpose(out=pGT[:, kc, :], in_=g_bf[:, kc * 128:(kc + 1) * 128], identity=ident_b[:]),
                         reads=[B_g, B_ident_b], writes=[B_pGT])
                k.op("act", lambda e: e.copy(out=gT[:], in_=pGT[:, 0:4, :]), reads=[B_pGT], writes=[B_gT])
                for n_ in range(4):
                    for kc in range(4):
                        k.op("pe", lambda e, n_=n_, kc=kc: e.matmul(pz[:, n_, :], lhsT=gw[:, kc, n_ * 128:(n_ + 1) * 128], rhs=gT[:, kc, :],
                                                                    start=(kc == 0), stop=(kc == 3)), reads=[B_gw, B_gT], writes=[B_pz])
                for n_ in range(4):
                    k.op("act", lambda e, n_=n_: e.activation(out=s2[:, n_, :], in_=pz[:, n_, :], func=AF.Sigmoid, bias=gb[:, n_:n_ + 1], scale=1.0),
                         reads=[B_pz, B_gb], writes=[B_s2])
                k.op("dve", lambda e, T0=T0: e.tensor_tensor(out=catT_[:, 4:8, T0:T0 + 128], in0=gT[:], in1=s2[:], op=ALU.mult),
                     reads=[B_gT, B_s2], writes=[B_catT_])
            if "dbg_cat" in dbg:
                dbg_cat = dscr("dbg_cat", [128, 8, NT], BF16)
                k.dma("sp", dbg_cat, catT_[:], reads=[B_catT_])
            k.barrier()


        def layer1_mixer():
            LAM_INIT = 0.8 - 0.6 * math.exp(-0.3 * 1)
            SC = 0.125
            with ExitStack() as L1:
                qT2 = sb(L1, "qT2", [128, 8, SEQ], BF16); B_q2 = Buf()
                kT2 = sb(L1, "kT2", [128, 8, NT], BF16); B_k2 = Buf()
                v1 = sb(L1, "v1", [128, NTILE, D], BF16); B_v1 = Buf()
                for st in phase("l1proj"):
                    w_bf = sb(st, "dif_w_bf", [128, 8, 3072], BF16); B_w = Buf()
                    for kc in range(8):
                        k.dma("pool", w_bf[:, kc, :], dif_w_in[kc * 128:(kc + 1) * 128, :], writes=[B_w])
                    sc1p = [mod_bc(st, f"l1sc1p_{r}", 1, r, 1, plus1=True) for r in range(2)]
                    sh1 = [mod_bc(st, f"l1sh1_{r}", 1, r, 0) for r in range(2)]
                    xt = [sb(st, f"l1xt{i}", [128, D]) for i in range(2)]; B_xt = [Buf(), Buf()]
                    tmpf = sb(st, "l1tmpf", [128, D]); B_tmpf = Buf()
                    h_bf = sb(st, "l1h_bf", [128, D], BF16); B_hbf = Buf()
                    hT = [sb(st, f"l1hT{i}", [128, 8, 128], BF16) for i in range(2)]; B_hT = [Buf(), Buf()]
                    rt = [sb(st, f"l1rt{i}", [128, 64]) for i in range(2)]; B_rt = [Buf(), Buf()]
                    t1 = sb(st, "l1rope_t1", [128, 512]); t2 = sb(st, "l1rope_t2", [128, 512]); B_rtmp = Buf()
                    qk_bf = sb(st, "l1qk_bf", [128, 512], BF16); B_qk = Buf()
                    pT = ps(st, "l1pT", [128, 8, 128], BF16); B_pT = Buf()
                    pp = [ps(st, f"l1pp{i}", [128, 512]) for i in range(3)]; B_pp = [Buf(), Buf(), Buf()]
                    pq = [ps(st, f"l1pq{i}", [128, 8, 128], BF16) for i in range(2)]; B_pq = [Buf(), Buf()]
                    ib = 0
                    for ti in range(NTILE):
                        i = ti % 2
                        r = 0 if ti < NLT else 1
                        T0 = ti * 128
                        k.dma("sp", xt[i][:], src_rows(ti, 1), writes=[B_xt[i]])
                        if r == 0:
                            k.dma("sp", rt[i][:], rope_cs[T0:T0 + 128, :], writes=[B_rt[i]])
                        k.op("dve", lambda e, i=i, r=r: e.tensor_tensor(out=tmpf[:], in0=xt[i][:], in1=sc1p[r][0][:], op=ALU.mult),
                             reads=[B_xt[i], sc1p[r][1]], writes=[B_tmpf])
                        k.op("pool", lambda e, r=r: e.tensor_tensor(out=h_bf[:], in0=tmpf[:], in1=sh1[r][0][:], op=ALU.add),
                             reads=[B_tmpf, sh1[r][1]], writes=[B_hbf])
                        for kc in range(8):
                            k.op("pe", lambda e, kc=kc: e.transpose(out=pT[:, kc, :], in_=h_bf[:, kc * 128:(kc + 1) * 128], identity=ident_b[:]),
                                 reads=[B_hbf, B_ident_b], writes=[B_pT])
                        k.op("act", lambda e, i=i: e.copy(out=hT[i][:], in_=pT[:]), reads=[B_pT], writes=[B_hT[i]])
                        for cb in range(6):
                            if r == 1 and cb < 2:
                                continue
                            j = ib % 3; ib += 1
                            for kc in range(8):
                                k.op("pe", lambda e, kc=kc, j=j, cb=cb, i=i: e.matmul(
                                    pp[j][:], lhsT=hT[i][:, kc, :], rhs=w_bf[:, kc, cb * 512:(cb + 1) * 512],
                                    start=(kc == 0), stop=(kc == 7)), reads=[B_hT[i], B_w], writes=[B_pp[j]])
                            if cb >= 4:
                                c0 = (cb - 4) * 512
                                k.op("act", lambda e, j=j, ti=ti, c0=c0: e.copy(out=v1[:, ti, c0:c0 + 512], in_=pp[j][:]), reads=[B_pp[j]], writes=[B_v1])
                                continue
                            if r == 0:
                                rope_apply(None, pp[j][:], B_pp[j], qk_bf[:], B_qk, 8, rt[i], B_rt[i], t1[:], t2[:], B_rtmp)
                            else:
                                k.op("dve", lambda e, j=j: e.tensor_copy(out=qk_bf[:], in_=pp[j][:]), reads=[B_pp[j]], writes=[B_qk])
                            jq = cb % 2
                            for hh in range(4):
                                k.op("pe", lambda e, hh=hh, jq=jq: e.transpose(out=pq[jq][:, hh, :], in_=qk_bf[:, hh * 128:(hh + 1) * 128], identity=ident_b[:]),
                                     reads=[B_qk, B_ident_b], writes=[B_pq[jq]])
                            dstT, BD = (qT2, B_q2) if cb < 2 else (kT2, B_k2)
                            h0 = (cb % 2) * 4
                            k.op("act", lambda e, jq=jq, dstT=dstT, h0=h0, T0=T0: e.copy(out=dstT[:, h0:h0 + 4, T0:T0 + 128], in_=pq[jq][:, 0:4, :]),
                                 reads=[B_pq[jq]], writes=[BD])
                    k.barrier()
                for st in phase("l1att"):
                    w_bf = sb(st, "difwo_bf", [128, 8, D], BF16); B_w = Buf()
                    for kc in range(8):
                        k.dma("pool", w_bf[:, kc, :], dif_w_out[kc * 128:(kc + 1) * 128, :], writes=[B_w])
                    g1 = mod_bc(st, "l1g1", 1, 0, 2)
                    lng = load_bc(st, "l1ln1g", ln1_g[1:2, :], D)
                    lnb = load_bc(st, "l1ln1b", ln1_b[1:2, :], D)
                    wk = ln_work(st, "l1e1")
                    xo = [sb(st, f"l1xo{i}", [128, D]) for i in range(2)]; B_xo = [Buf(), Buf()]
                    lam = sb(st, "lam", [128, 8]); B_lam = Buf()
                    lq = [load_bc(st, f"lq{i}", a[0:1, :], 64) for i, a in enumerate((lam_q1, lam_k1, lam_q2, lam_k2))]
                    ltmp = sb(st, "ltmp", [128, 64]); B_lt = Buf()
                    for i2 in range(2):
                        k.op("dve", lambda e, i2=i2: e.tensor_tensor(out=ltmp[:], in0=lq[2 * i2][0][:], in1=lq[2 * i2 + 1][0][:], op=ALU.mult),
                             reads=[lq[2 * i2][1], lq[2 * i2 + 1][1], B_lt], writes=[B_lt])
                        k.op("dve", lambda e, i2=i2: e.reduce_sum(out=lam[:, i2:i2 + 1], in_=ltmp[:], axis=AX.X), reads=[B_lt, B_lam], writes=[B_lam])
                    k.op("act", lambda e: e.activation(out=lam[:, 2:4], in_=lam[:, 0:2], func=AF.Exp), reads=[B_lam], writes=[B_lam])
                    k.op("dve", lambda e: e.tensor_tensor(out=lam[:, 4:5], in0=lam[:, 2:3], in1=lam[:, 3:4], op=ALU.subtract), reads=[B_lam], writes=[B_lam])
                    k.op("dve", lambda e: e.tensor_scalar(out=lam[:, 5:6], in0=lam[:, 4:5], scalar1=LAM_INIT, scalar2=None, op0=ALU.add), reads=[B_lam], writes=[B_lam])
                    sg_bc = load_bc(st, "subg", subln_g[0:1, :], 128)
                    k.op("dve", lambda e: e.tensor_scalar(out=sg_bc[0][:], in0=sg_bc[0][:], scalar1=1.0 - LAM_INIT, scalar2=None, op0=ALU.mult),
                         reads=[sg_bc[1]], writes=[sg_bc[1]])
                    e_bf = [sb(st, f"e_bf{i}", [128, NT], BF16) for i in range(2)]; B_e = [Buf(), Buf()]
                    tl = sb(st, "tl", [128, NT], BF16); B_tl = Buf()
                    pd = sb(st, "pd", [128, NT], BF16); B_pd = Buf()
                    PTt = sb(st, "PTt", [128, NTILE, 128], BF16); B_PT = Buf()
                    stt = sb(st, "dstat", [128, 16]); B_st = Buf()
                    ao = sb(st, "ao", [128, D], BF16); B_ao = Buf()
                    aoT = sb(st, "aoT", [128, 8, 128], BF16); B_aoT = Buf()
                    osq = sb(st, "osq", [128, 128]); B_osq = Buf()
                    scp = ps(st, "scp", [128, 2560]); B_scp = Buf()
                    pPT = [ps(st, f"dpPT{i}", [128, 8, 128], BF16) for i in range(2)]; B_pPT = [Buf(), Buf()]
                    po = ps(st, "dpo", [128, 512]); B_po = Buf()
                    KB = [(b0, min(512, NT - b0)) for b0 in range(0, NT, 512)]
                    for ti in range(NLT):
                        T0 = ti * 128
                        i = ti % 2
                        k.dma("sp", xo[i][:], src_rows(ti, 1), writes=[B_xo[i]])
                        for h in range(8):
                            for c in range(2):
                                ps_ = slice(c * 64, (c + 1) * 64)
                                for (b0, bn) in KB:
                                    k.op("pe", lambda e, h=h, ps_=ps_, b0=b0, bn=bn, T0=T0: e.matmul(
                                        scp[:, b0:b0 + bn], lhsT=qT2[ps_, h, T0:T0 + 128], rhs=kT2[ps_, h, b0:b0 + bn], start=True, stop=True),
                                        reads=[B_q2, B_k2], writes=[B_scp])
                                o0 = c * 4
                                k.op("dve", lambda e, o0=o0: e.reduce_max(out=stt[:, o0:o0 + 1], in_=scp[:, 0:NT], axis=AX.X), reads=[B_scp, B_st], writes=[B_st])
                                k.op("dve", lambda e, o0=o0: e.tensor_scalar(out=stt[:, o0 + 1:o0 + 2], in0=stt[:, o0:o0 + 1], scalar1=-SC, scalar2=None, op0=ALU.mult),
                                     reads=[B_st], writes=[B_st])
                                k.op("act", lambda e, c=c, o0=o0: e.activation(out=e_bf[c][:], in_=scp[:, 0:NT], func=AF.Exp, bias=stt[:, o0 + 1:o0 + 2], scale=SC,
                                                                             accum_out=stt[:, o0 + 2:o0 + 3]), reads=[B_scp, B_st], writes=[B_e[c], B_st])
                                k.op("dve", lambda e, o0=o0: e.reciprocal(out=stt[:, o0 + 3:o0 + 4], in_=stt[:, o0 + 2:o0 + 3]), reads=[B_st], writes=[B_st])
                            k.op("dve", lambda e: e.tensor_tensor(out=stt[:, 8:9], in0=stt[:, 7:8], in1=lam[:, 5:6], op=ALU.mult), reads=[B_st, B_lam], writes=[B_st])
                            k.op("pool", lambda e: e.tensor_scalar(out=tl[:], in0=e_bf[1][:], scalar1=stt[:, 8:9], scalar2=None, op0=ALU.mult),
                                 reads=[B_e[1], B_st], writes=[B_tl])
                            k.op("dve", lambda e: e.scalar_tensor_tensor(out=pd[:], in0=e_bf[0][:], scalar=stt[:, 3:4], in1=tl[:], op0=ALU.mult, op1=ALU.subtract),
                                 reads=[B_e[0], B_st, B_tl], writes=[B_pd])
                            for b in range(NTILE):
                                jb = (b // 8) % 2
                                k.op("pe", lambda e, b=b, jb=jb: e.transpose(out=pPT[jb][:, b % 8, :], in_=pd[:, b * 128:(b + 1) * 128], identity=ident_b[:]),
                                     reads=[B_pd, B_ident_b], writes=[B_pPT[jb]])
                                if b % 8 == 7 or b == NTILE - 1:
                                    nb_ = b % 8 + 1
                                    b0_ = b - nb_ + 1
                                    k.op("act", lambda e, jb=jb, nb_=nb_, b0_=b0_: e.copy(out=PTt[:, b0_:b0_ + nb_, :], in_=pPT[jb][:, 0:nb_, :]),
                                         reads=[B_pPT[jb]], writes=[B_PT])
                            for b in range(NTILE):
                                k.op("pe", lambda e, b=b, h=h: e.matmul(po[:, 0:128], lhsT=PTt[:, b, :], rhs=v1[:, b, h * 128:(h + 1) * 128],
                                                                       start=(b == 0), stop=(b == NTILE - 1)), reads=[B_PT, B_v1], writes=[B_po])
                            k.op("act", lambda e: e.activation(out=osq[:], in_=po[:, 0:128], func=AF.Square, accum_out=stt[:, 9:10]), reads=[B_po, B_st], writes=[B_osq, B_st])
                            k.op("dve", lambda e: e.tensor_scalar(out=stt[:, 10:11], in0=stt[:, 9:10], scalar1=1.0 / 128.0, scalar2=1e-5, op0=ALU.mult, op1=ALU.add),
                                 reads=[B_st], writes=[B_st])
                            k.op("act", lambda e: e.sqrt(out=stt[:, 10:11], in_=stt[:, 10:11]), reads=[B_st], writes=[B_st])
                            k.op("dve", lambda e: e.reciprocal(out=stt[:, 11:12], in_=stt[:, 10:11]), reads=[B_st], writes=[B_st])
                            k.op("dve", lambda e, h=h: e.scalar_tensor_tensor(out=ao[:, h * 128:(h + 1) * 128], in0=po[:, 0:128], scalar=stt[:, 11:12], in1=sg_bc[0][:],
                                                                              op0=ALU.mult, op1=ALU.mult), reads=[B_po, B_st, sg_bc[1]], writes=[B_ao])
                        if "dbg_ao" in dbg:
                            if ti == 0:
                                dbg_ao = dscr("dbg_ao", [SEQ, D], BF16)
                            k.dma("sp", dbg_ao[T0:T0 + 128, :], ao[:], reads=[B_ao])
                        for kc in range(8):
                            k.op("pe", lambda e, kc=kc: e.transpose(out=pPT[0][:, kc, :], in_=ao[:, kc * 128:(kc + 1) * 128], identity=ident_b[:]),
                                 reads=[B_ao, B_ident_b], writes=[B_pPT[0]])
                        k.op("act", lambda e: e.copy(out=aoT[:], in_=pPT[0][:]), reads=[B_pPT[0]], writes=[B_aoT])
                        for hf in range(2):
                            for kc in range(8):
                                k.op("pe", lambda e, kc=kc, hf=hf: e.matmul(scp[:, hf * 512:(hf + 1) * 512], lhsT=aoT[:, kc, :], rhs=w_bf[:, kc, hf * 512:(hf + 1) * 512],
                                                                          start=(kc == 0), stop=(kc == 7)), reads=[B_aoT, B_w], writes=[B_scp])
                        ln_epilogue(wk, [scp[:, 0:512], scp[:, 512:1024]], [B_scp, B_scp], xo[i], B_xo[i], g1, lng, lnb, x1_scr[T0:T0 + 128, :])
                    k.barrier()

        with ExitStack() as L0:
            catT = sb(L0, "catT", [128, 8, NT], BF16); B_catT = Buf()
            LA = ExitStack()
            qT = sb(LA, "qT", [64, 8, NT], BF16); B_qT = Buf()
            kT = sb(LA, "kT", [64, 2, NT], BF16); B_kT = Buf()
            v_all = sb(LA, "v_all", [128, NTILE, 128], BF16); B_v = Buf()
            for st in phase("l0proj"):
                w_bf = sb(st, "w_in_bf", [128, 8, 1280], BF16); B_w = Buf()
                for kc in range(8):
                    k.dma("pool", w_bf[:, kc, :], w_in0[kc * 128:(kc + 1) * 128, :], writes=[B_w])
                sc1p = [None, None]; sh1 = [None, None]
                for r in range(2):
                    sh1[r] = mod_bc(st, f"sh1_{r}", 0, r, 0)
                    sc1p[r] = mod_bc(st, f"sc1p_{r}", 0, r, 1, plus1=True)
                xt = [sb(st, f"xt{i}", [128, D]) for i in range(2)]; B_xt = [Buf(), Buf()]
                tmpf = sb(st, "tmpf", [128, D]); B_tmpf = Buf()
                h_bf = sb(st, "h_bf", [128, D], BF16); B_hbf = Buf()
                hT = [sb(st, f"hT{i}", [128, 8, 128], BF16) for i in range(2)]; B_hT = [Buf(), Buf()]
                rt = [sb(st, f"rt{i}", [128, 64]) for i in range(2)]; B_rt = [Buf(), Buf()]
                t1 = sb(st, "rope_t1", [128, 640]); t2 = sb(st, "rope_t2", [128, 640]); B_rtmp = Buf()
                qk_bf = sb(st, "qk_bf", [128, 640], BF16); B_qk = Buf()
                ut = [sb(st, f"ut{i}", [128, 512]) for i in range(2)]; B_ut = [Buf(), Buf()]
                pT = ps(st, "pT", [128, 8, 128], BF16); B_pT = Buf()
                pp = [ps(st, f"pp{i}", [128, 512]) for i in range(3)]; B_pp = [Buf(), Buf(), Buf()]
                pq = ps(st, "pq", [64, 8, 128], BF16); B_pq = Buf()
                pk = ps(st, "pk", [64, 8, 128], BF16); B_pk = Buf()
                for ti in range(NTILE):
                    i = ti % 2
                    r = 0 if ti < NLT else 1
                    T0 = ti * 128
                    k.dma("sp", xt[i][:], src_rows(ti, 0), writes=[B_xt[i]])
                    if r == 0:
                        k.dma("sp", rt[i][:], rope_cs[T0:T0 + 128, :], writes=[B_rt[i]])
                    k.op("dve", lambda e, i=i, r=r: e.tensor_tensor(out=tmpf[:], in0=xt[i][:], in1=sc1p[r][0][:], op=ALU.mult),
                         reads=[B_xt[i], sc1p[r][1]], writes=[B_tmpf])
                    k.op("pool", lambda e, r=r: e.tensor_tensor(out=h_bf[:], in0=tmpf[:], in1=sh1[r][0][:], op=ALU.add),
                         reads=[B_tmpf, sh1[r][1]], writes=[B_hbf])
                    for kc in range(8):
                        k.op("pe", lambda e, kc=kc: e.transpose(out=pT[:, kc, :], in_=h_bf[:, kc * 128:(kc + 1) * 128], identity=ident_b[:]),
                             reads=[B_hbf, B_ident_b], writes=[B_pT])
                    k.op("act", lambda e, i=i: e.copy(out=hT[i][:], in_=pT[:]), reads=[B_pT], writes=[B_hT[i]])
                    for nb, (c0, c1) in enumerate(((0, 512), (512, 1024), (1024, 1280))):
                        for kc in range(8):
                            k.op("pe", lambda e, kc=kc, nb=nb, c0=c0, c1=c1, i=i: e.matmul(
                                pp[nb][:, 0:c1 - c0], lhsT=hT[i][:, kc, :], rhs=w_bf[:, kc, c0:c1],
                                start=(kc == 0), stop=(kc == 7)),
                                reads=[B_hT[i], B_w], writes=[B_pp[nb]])
                    if "dbg_q" in dbg:
                        if ti == 0:
                            dq = sb(st, "dq", [128, 1280]); B_dq = Buf()
                        for nb, (c0, c1) in enumerate(((0, 512), (512, 1024), (1024, 1280))):
                            k.op("dve", lambda e, nb=nb, c0=c0, c1=c1: e.tensor_copy(out=dq[:, c0:c1], in_=pp[nb][:, 0:c1 - c0]),
                                 reads=[B_pp[nb]], writes=[B_dq])
                        k.dma("sp", dbg_q[T0:T0 + 128, :], dq[:], reads=[B_dq])
                    if r == 0:
                        rope_apply(None, pp[0][:, 0:512], B_pp[0], qk_bf[:, 0:512], B_qk, 8, rt[i], B_rt[i], t1[:, 0:512], t2[:, 0:512], B_rtmp)
                        rope_apply(None, pp[1][:, 0:128], B_pp[1], qk_bf[:, 512:640], B_qk, 2, rt[i], B_rt[i], t1[:, 512:640], t2[:, 512:640], B_rtmp)
                    else:
                        k.op("dve", lambda e: e.tensor_copy(out=qk_bf[:, 0:512], in_=pp[0][:, 0:512]), reads=[B_pp[0]], writes=[B_qk])
                        k.op("dve", lambda e: e.tensor_copy(out=qk_bf[:, 512:640], in_=pp[1][:, 0:128]), reads=[B_pp[1]], writes=[B_qk])
                    for h in range(8):
                        k.op("pe", lambda e, h=h: e.transpose(out=pq[:, h, :], in_=qk_bf[:, h * 64:(h + 1) * 64], identity=ident_b[:]),
                             reads=[B_qk, B_ident_b], writes=[B_pq])
                    for h in range(2):
                        k.op("pe", lambda e, h=h: e.transpose(out=pk[:, h, :], in_=qk_bf[:, 512 + h * 64:512 + (h + 1) * 64], identity=ident_b[:]),
                             reads=[B_qk, B_ident_b], writes=[B_pk])
                    k.op("act", lambda e, T0=T0: e.copy(out=qT[:, :, T0:T0 + 128], in_=pq[:]), reads=[B_pq], writes=[B_qT])
                    k.op("act", lambda e, T0=T0: e.copy(out=kT[:, :, T0:T0 + 128], in_=pk[:, 0:2, :]), reads=[B_pk], writes=[B_kT])
                    k.op("act", lambda e, ti=ti: e.copy(out=v_all[:, ti, :], in_=pp[1][:, 128:256]), reads=[B_pp[1]], writes=[B_v])
                    k.op("act", lambda e, i=i: e.copy(out=ut[i][:, 0:256], in_=pp[1][:, 256:512]), reads=[B_pp[1]], writes=[B_ut[i]])
                    k.op("act", lambda e, i=i: e.copy(out=ut[i][:, 256:512], in_=pp[2][:, 0:256]), reads=[B_pp[2]], writes=[B_ut[i]])
                    urow = T0 + CTX if r == 0 else T0 - SEQ
                    k.dma("sp", u_scr[urow:urow + 128, :], ut[i][:], reads=[B_ut[i]])
                if "dbg_qT" in dbg:
                    dbg_qT = dscr("dbg_qT", [64, 8, NT], BF16)
                    k.dma("sp", dbg_qT, qT[:], reads=[B_qT])
                k.barrier()


            for st in phase("l0att"):
                SC = 0.125
                maskL = sb(st, "maskL_sb", [128, 128]); maskR = sb(st, "maskR_sb", [128, 128]); B_mask = Buf()
                k.dma("sp", maskL[:], maskL_in[:, :], writes=[B_mask])
                k.dma("sp", maskR[:], maskR_in[:, :], writes=[B_mask])
                sink_bc, B_sink = load_bc(st, "sink_bc", swa_sink[0:1, :], 8)
                sm = [sb(st, f"sm{i}", [128, 640]) for i in range(2)]; B_sm = [Buf(), Buf()]
                P = [sb(st, f"P{i}", [128, 640], BF16) for i in range(2)]; B_P = [Buf(), Buf()]
                PT = [sb(st, f"PT{i}", [128, 5, 128], BF16) for i in range(2)]; B_PT = [Buf(), Buf()]
                stat = [sb(st, f"stat{i}", [128, 8]) for i in range(2)]; B_stat = [Buf(), Buf()]
                att_bf = sb(st, "att_bf", [128, 512], BF16); B_att = Buf()
                ps_loc = [ps(st, f"ps_loc{i}", [128, 512]) for i in range(2)]; B_psl = [Buf(), Buf()]
                ps_ctx = [ps(st, f"ps_ctx{i}", [128, 512]) for i in range(2)]; B_psc = [Buf(), Buf()]
                pPT = ps(st, "pPT", [128, 8, 128], BF16); B_pPT = Buf()
                po = ps(st, "po", [128, 512]); B_po = Buf()
                pcat = ps(st, "pcat", [128, 8, 128], BF16); B_pcat = Buf()
                it = 0
                for ti in range(NTILE):
                    T0 = ti * 128
                    lat = ti < NLT
                    if lat:
                        j0 = max(0, ti - 1); j1 = min(NLT - 1, ti + 1)
                        nloc = (j1 - j0 + 1) * 128
                        blocks = list(range(j0, j1 + 1)) + [NLT, NLT + 1]
                    else:
                        nloc = 0
                        blocks = [NLT, NLT + 1]
                    n = nloc + 256
                    for h in range(8):
                        i = it % 2; it += 1
                        kvh = h // 4
                        if lat:
                            k.op("pe", lambda e, i=i, h=h, kvh=kvh, j0=j0, nloc=nloc, T0=T0: e.matmul(
                                ps_loc[i][:, 0:nloc], lhsT=qT[:, h, T0:T0 + 128], rhs=kT[:, kvh, j0 * 128:j0 * 128 + nloc],
                                start=True, stop=True), reads=[B_qT, B_kT], writes=[B_psl[i]])
                        k.op("pe", lambda e, i=i, h=h, kvh=kvh, T0=T0: e.matmul(
                            ps_ctx[i][:, 0:256], lhsT=qT[:, h, T0:T0 + 128], rhs=kT[:, kvh, SEQ:NT],
                            start=True, stop=True), reads=[B_qT, B_kT], writes=[B_psc[i]])
                        if lat:
                            for bi, j in enumerate(range(j0, j1 + 1)):
                                sl = slice(bi * 128, (bi + 1) * 128)
                                if j == ti:
                                    k.op("act", lambda e, i=i, sl=sl: e.mul(out=sm[i][:, sl], in_=ps_loc[i][:, sl], mul=SC),
                                         reads=[B_psl[i]], writes=[B_sm[i]])
                                else:
                                    mk = maskL if j < ti else maskR
                                    k.op("dve", lambda e, i=i, sl=sl, mk=mk: e.scalar_tensor_tensor(
                                        out=sm[i][:, sl], in0=ps_loc[i][:, sl], scalar=SC, in1=mk[:], op0=ALU.mult, op1=ALU.add),
                                        reads=[B_psl[i], B_mask], writes=[B_sm[i]])
                        k.op("act", lambda e, i=i, nloc=nloc: e.mul(out=sm[i][:, nloc:nloc + 256], in_=ps_ctx[i][:, 0:256], mul=SC),
                             reads=[B_psc[i]], writes=[B_sm[i]])
                        sti = stat[i]
                        k.op("dve", lambda e, i=i, n=n, sti=sti: e.reduce_max(out=sti[:, 0:1], in_=sm[i][:, 0:n], axis=AX.X),
                             reads=[B_sm[i]], writes=[B_stat[i]])
                        k.op("dve", lambda e, sti=sti, h=h: e.tensor_tensor(out=sti[:, 1:2], in0=sti[:, 0:1], in1=sink_bc[:, h:h + 1], op=ALU.max),
                             reads=[B_stat[i], B_sink], writes=[B_stat[i]])
                        k.op("dve", lambda e, sti=sti: e.tensor_scalar(out=sti[:, 2:3], in0=sti[:, 1:2], scalar1=-1.0, scalar2=None, op0=ALU.mult),
                             reads=[B_stat[i]], writes=[B_stat[i]])
                        k.op("act", lambda e, i=i, n=n, sti=sti: e.activation(out=P[i][:, 0:n], in_=sm[i][:, 0:n], func=AF.Exp,
                                                                             bias=sti[:, 2:3], scale=1.0, accum_out=sti[:, 3:4]),
                             reads=[B_sm[i], B_stat[i]], writes=[B_P[i], B_stat[i]])
                        k.op("act", lambda e, sti=sti, h=h: e.activation(out=sti[:, 4:5], in_=sink_bc[:, h:h + 1], func=AF.Exp,
                                                                        bias=sti[:, 2:3], scale=1.0),
                             reads=[B_sink, B_stat[i]], writes=[B_stat[i]])
                        k.op("dve", lambda e, sti=sti: e.tensor_tensor(out=sti[:, 5:6], in0=sti[:, 3:4], in1=sti[:, 4:5], op=ALU.add),
                             reads=[B_stat[i]], writes=[B_stat[i]])
                        k.op("dve", lambda e, sti=sti: e.reciprocal(out=sti[:, 6:7], in_=sti[:, 5:6]),
                             reads=[B_stat[i]], writes=[B_stat[i]])
                        nb = n // 128
                        for b in range(nb):
                            k.op("pe", lambda e, i=i, b=b: e.transpose(out=pPT[:, b, :], in_=P[i][:, b * 128:(b + 1) * 128], identity=ident_b[:]),
                                 reads=[B_P[i], B_ident_b], writes=[B_pPT])
                        k.op("pool" if False else "dve", lambda e, i=i, nb=nb: e.tensor_copy(out=PT[i][:, 0:nb, :], in_=pPT[:, 0:nb, :]),
                             reads=[B_pPT], writes=[B_PT[i]])
                        for b in range(nb):
                            k.op("pe", lambda e, i=i, b=b, h=h, kvh=kvh, vb=blocks[b], nb=nb: e.matmul(
                                po[:, h * 64:(h + 1) * 64], lhsT=PT[i][:, b, :], rhs=v_all[:, vb, kvh * 64:(kvh + 1) * 64],
                                start=(b == 0), stop=(b == nb - 1)), reads=[B_PT[i], B_v], writes=[B_po])
                        k.op("dve", lambda e, h=h, sti=sti: e.tensor_scalar(out=att_bf[:, h * 64:(h + 1) * 64], in0=po[:, h * 64:(h + 1) * 64],
                                                                           scalar1=sti[:, 6:7], scalar2=None, op0=ALU.mult),
                             reads=[B_po, B_stat[i]], writes=[B_att])
                    for cb in range(4):
                        k.op("pe", lambda e, cb=cb: e.transpose(out=pcat[:, cb, :], in_=att_bf[:, cb * 128:(cb + 1) * 128], identity=ident_b[:]),
                             reads=[B_att, B_ident_b], writes=[B_pcat])
                    k.op("act", lambda e, T0=T0: e.copy(out=catT[:, 0:4, T0:T0 + 128], in_=pcat[:, 0:4, :]), reads=[B_pcat], writes=[B_catT])
                    if "dbg_att" in dbg:
                        if ti == 0:
                            dbg_att = dscr("dbg_att", [NT, 512], BF16)
                        k.dma("sp", dbg_att[T0:T0 + 128, :], att_bf[:], reads=[B_att])
                k.barrier()


            k.barrier()
            LA.close()
            for st in phase("l0ssm"):
                ssm_phase(st, catT, B_catT)
            for st in phase("l0ssmpost"):
                ssm_post(st, catT, B_catT)

            for st in phase("l0out"):
                outproj_ln1(st, 0, catT, B_catT, w_out0, NTILE)


        for st in phase("moe0"):
            moe_phase(st, 0, NTILE, x2_scr)


        layer1_mixer()
        for st in phase("moe1"):
            moe_phase(st, 1, NLT, out)

        k.barrier()
    return nc


_CONSTS = None


def _consts():
    global _CONSTS
    if _CONSTS is None:
        t = np.arange(SEQ)
        row = (t // 64).astype(np.float32)
        col = (t % 64).astype(np.float32)
        inv = (10000.0 ** (-np.arange(16, dtype=np.float32) / 16)).astype(np.float32)
        ar = row[:, None] * inv[None, :]
        ac = col[:, None] * inv[None, :]
        rope = np.concatenate([np.cos(ar), np.sin(ar), np.cos(ac), np.sin(ac)], 1).astype(np.float32)
        qi = np.arange(128)[:, None]; kj = np.arange(128)[None, :]
        mL = np.where(kj >= qi, 0.0, -30000.0).astype(np.float32)
        mR = np.where(kj <= qi, 0.0, -30000.0).astype(np.float32)
        _CONSTS = {"rope_cs": rope, "ident": np.eye(128, dtype=np.float32), "maskL": mL, "maskR": mR}
        selm = np.zeros((32, 32, 128), np.float32)
        for e_ in range(32):
            selm[e_, e_, :] = 1.0
        _CONSTS["sel"] = selm
        _CONSTS["kval"] = np.ascontiguousarray(np.broadcast_to(np.repeat(np.arange(-7, 9, dtype=np.float32), 64)[None, :], (64, 1024)))
        _CONSTS["mrow"] = np.ascontiguousarray(np.broadcast_to(np.arange(288, dtype=np.float32)[None, :], (64, 288)))
        jj = np.arange(128) // 16
        _CONSTS["maskF"] = (jj[None, :] >= jj[:, None]).astype(np.float32)
        _CONSTS["maskB"] = (jj[None, :] <= jj[:, None]).astype(np.float32)
    return _CONSTS


def make_in_maps(inputs, cores):
    f = lambda a: np.ascontiguousarray(np.asarray(a, dtype=np.float32))
    shared = {}
    for name in ("mod_w", "mod_b", "ln1_g", "ln1_b", "ln2_g", "ln2_b", "swa_sink",
                 "ssm_d", "ssm_glu_b", "dif_lam_q1", "dif_lam_k1", "dif_lam_q2", "dif_lam_k2",
                 "dif_subln_g", "moe_wg", "moe_bg", "moe_we", "moe_w1", "moe_w3", "moe_w2"):
        shared[name] = f(inputs[name])
    for name in ("swa_ssm_w_in", "swa_ssm_w_out", "ssm_a_re", "ssm_a_im", "ssm_log_step",
                 "ssm_b_re", "ssm_b_im", "ssm_c_re", "ssm_c_im", "ssm_glu_w", "dif_w_in", "dif_w_out"):
        shared[name] = f(inputs[name])[0]
    shared["moe_be"] = f(inputs["moe_be"]).reshape(2, 32)
    shared["c_ctx"] = f(inputs["c_ctx"]).reshape(1, D)
    shared.update(_consts())
    maps = []
    for b in cores:
        m = dict(shared)
        m["x"] = f(inputs["x"][b])
        m["ctx"] = f(inputs["ctx"][b])
        m["c"] = f(inputs["c"][b]).reshape(1, D)
        maps.append(m)
    return maps


def kernel(**inputs):
    nc = build()
    maps = make_in_maps(inputs, range(8))
    res = run_bass_kernel_spmd(nc, maps, core_ids=list(range(8)))
    return np.stack([r["out"] for r in res.results], 0).astype(np.float32)
```

```python
import math
from contextlib import ExitStack

import numpy as np
import concourse.bass as bass
import concourse.mybir as mybir
from concourse.bass_utils import run_bass_kernel_spmd

F32 = mybir.dt.float32
BF16 = mybir.dt.bfloat16
AF = mybir.ActivationFunctionType
ALU = mybir.AluOpType
AX = mybir.AxisListType

D = 1024
SEQ = 2048
CTX = 256
NT = SEQ + CTX
NTILE = NT // 128
NLT = SEQ // 128
ALPHA = 4 ** 0.25
LN_EPS = 1e-5


class Buf:
    __slots__ = ("w", "r")

    def __init__(self):
        self.w = None
        self.r = {}


class EngState:
    def __init__(self, name, eng, sem):
        self.name = name
        self.eng = eng
        self.sem = sem
        self.count = 0
        self.waited = {}
        self.slots = []
        self.slot_i = 0


class K:
    def __init__(self, nc, stack):
        self.nc = nc
        self.E = {}
        for name, eng in (("pe", nc.tensor), ("dve", nc.vector), ("act", nc.scalar),
                          ("pool", nc.gpsimd), ("sp", nc.sync)):
            sem = stack.enter_context(nc.semaphore("s_" + name))
            self.E[name] = EngState(name, eng, sem)
        self.semkey = {}
        for qn, n in (("sp", 12), ("pool", 12), ("act", 6)):
            for i in range(n):
                sem = stack.enter_context(nc.semaphore(f"d_{qn}{i}"))
                self.E[qn].slots.append([sem, 0])
        self.uid = 0

    def _key(self, sem):
        return id(sem)

    def _wait(self, E, deps, skip_self=False):
        best = {}
        for sem, val in deps:
            if skip_self and sem is E.sem:
                continue
            k = id(sem)
            if k not in best or best[k][1] < val:
                best[k] = (sem, val)
        for k, (sem, val) in best.items():
            if E.waited.get(k, 0) < val:
                E.eng.wait_ge(sem, val)
                E.waited[k] = val

    def _deps(self, reads, writes):
        deps = []
        for b in reads:
            if b.w is not None:
                deps.append(b.w)
        for b in writes:
            if b.w is not None:
                deps.append(b.w)
            deps.extend(b.r.values())
        return deps

    def _mark(self, tok, reads, writes):
        sem, val = tok
        for b in reads:
            b.r[id(sem)] = tok
        for b in writes:
            b.w = tok
            b.r = {}

    def op(self, en, fn, reads=(), writes=()):
        E = self.E[en]
        self._wait(E, self._deps(reads, writes), skip_self=(en == "pe"))
        ins = fn(E.eng)
        E.count += 1
        ins.then_inc(E.sem, 1)
        tok = (E.sem, E.count)
        self._mark(tok, reads, writes)
        return tok

    def dma(self, qn, out, in_, reads=(), writes=(), **kw):
        E = self.E[qn]
        self._wait(E, self._deps(reads, writes))
        slot = E.slots[E.slot_i % len(E.slots)]
        E.slot_i += 1
        if slot[1] > 0:
            self._wait(E, [(slot[0], slot[1] * 16)])
        ins = E.eng.dma_start(out=out, in_=in_, **kw)
        slot[1] += 1
        ins.then_inc(slot[0], 16)
        tok = (slot[0], slot[1] * 16)
        self._mark(tok, reads, writes)
        return tok

    def all_tokens(self):
        toks = []
        for E in self.E.values():
            if E.count:
                toks.append((E.sem, E.count))
            for sem, c in E.slots:
                if c:
                    toks.append((sem, c * 16))
        return toks

    def barrier(self):
        toks = self.all_tokens()
        for E in self.E.values():
            self._wait(E, toks, skip_self=False)


def build(dbg=(), inject=(), phases=None):
    nc = bass.Bass("TRN2", target_bir_lowering=False)
    dbg = set(dbg)
    inject = set(inject)
    ALLP = {"mod", "l0proj", "l0att", "l0ssm", "l0ssmpost", "l0out", "moe0", "l1proj", "l1att", "moe1"}
    phases = ALLP if phases is None else set(phases)

    def din(name, shape):
        return nc.dram_tensor(name, list(shape), F32, kind="ExternalInput").ap()

    def dscr(name, shape, dt=F32):
        kind = "ExternalOutput" if name in dbg else ("ExternalInput" if name in inject else "Internal")
        return nc.dram_tensor(name, list(shape), dt, kind=kind).ap()

    x_in = din("x", [SEQ, D])
    ctx_in = din("ctx", [CTX, D])
    c_in = din("c", [1, D])
    cc_in = din("c_ctx", [1, D])
    mod_w = din("mod_w", [2, D, 6 * D])
    mod_b = din("mod_b", [2, 6 * D])
    ln1_g = din("ln1_g", [2, D]); ln1_b = din("ln1_b", [2, D])
    ln2_g = din("ln2_g", [2, D]); ln2_b = din("ln2_b", [2, D])
    w_in0 = din("swa_ssm_w_in", [D, 1280])
    w_out0 = din("swa_ssm_w_out", [D, D])
    swa_sink = din("swa_sink", [1, 8])
    a_re = din("ssm_a_re", [2, 32, 64]); a_im = din("ssm_a_im", [2, 32, 64])
    log_step = din("ssm_log_step", [2, 32])
    b_re = din("ssm_b_re", [2, 32, 64, 16]); b_im = din("ssm_b_im", [2, 32, 64, 16])
    c_re = din("ssm_c_re", [2, 32, 16, 64]); c_im = din("ssm_c_im", [2, 32, 16, 64])
    ssm_d = din("ssm_d", [1, 512])
    glu_w = din("ssm_glu_w", [512, 512]); glu_b = din("ssm_glu_b", [1, 512])
    dif_w_in = din("dif_w_in", [D, 3072]); dif_w_out = din("dif_w_out", [D, D])
    lam_q1 = din("dif_lam_q1", [1, 64]); lam_k1 = din("dif_lam_k1", [1, 64])
    lam_q2 = din("dif_lam_q2", [1, 64]); lam_k2 = din("dif_lam_k2", [1, 64])
    subln_g = din("dif_subln_g", [1, 128])
    moe_wg = din("moe_wg", [2, D, 4]); moe_bg = din("moe_bg", [2, 4])
    moe_we = din("moe_we", [2, 4, D, 8]); moe_be = din("moe_be", [2, 32])
    moe_w1 = din("moe_w1", [2, 32, D, 256]); moe_w3 = din("moe_w3", [2, 32, D, 256])
    moe_w2 = din("moe_w2", [2, 32, 256, D])
    rope_cs = din("rope_cs", [SEQ, 64])
    ident_in = din("ident", [128, 128])
    sel_in = din("sel", [32, 32, 128])
    kval_in = din("kval", [64, 1024]); mrow_in = din("mrow", [64, 288])
    maskF_in = din("maskF", [128, 128]); maskB_in = din("maskB", [128, 128])
    maskL_in = din("maskL", [128, 128]); maskR_in = din("maskR", [128, 128])
    out = nc.dram_tensor("out", [SEQ, D], F32, kind="ExternalOutput").ap()

    modrow = dscr("modrow", [2, 2, 6 * D])

    with ExitStack() as gs:
        k = K(nc, gs)

        def sb(st, name, shape, dt=F32):
            k.uid += 1
            return st.enter_context(nc.sbuf_tensor(f"sb{k.uid}_{name}", list(shape), dt))

        def ps(st, name, shape, dt=F32):
            k.uid += 1
            return st.enter_context(nc.psum_tensor(f"ps{k.uid}_{name}", list(shape), dt))

        def phase(name):
            if name in phases:
                with ExitStack() as st_:
                    yield st_

        ident_f = sb(gs, "ident_f", [128, 128]); B_ident_f = Buf()
        ident_b = sb(gs, "ident_b", [128, 128], BF16); B_ident_b = Buf()
        k.dma("sp", ident_f[:], ident_in[:, :], writes=[B_ident_f])
        k.op("dve", lambda e: e.tensor_copy(out=ident_b[:], in_=ident_f[:]),
             reads=[B_ident_f], writes=[B_ident_b])

        for st in phase("mod"):
            cT = sb(st, "cT", [128, 8, 2]); B_cT = Buf()
            with nc.allow_non_contiguous_dma(reason="tiny column loads"):
                k.dma("sp", cT[:, :, 0], c_in[0, :].rearrange("(k p) -> p k", p=128), writes=[B_cT])
                k.dma("sp", cT[:, :, 1], cc_in[0, :].rearrange("(k p) -> p k", p=128), writes=[B_cT])
            sT = sb(st, "sT", [128, 8, 2]); B_sT = Buf()
            k.op("act", lambda e: e.activation(out=sT[:], in_=cT[:], func=AF.Silu),
                 reads=[B_cT], writes=[B_sT])
            wt = [sb(st, f"modw{i}", [128, 8, 512]) for i in range(2)]
            B_wt = [Buf(), Buf()]
            mb = sb(st, "modb", [2, 6 * D]); B_mb = Buf()
            mrow = sb(st, "mrow", [2, 6 * D]); B_mrow = Buf()
            pm = [ps(st, f"pmod{i}", [2, 512]) for i in range(2)]
            B_pm = [Buf(), Buf()]
            it = 0
            for l in range(2):
                k.dma("sp", mb[0:1, :], mod_b[l:l + 1, :], writes=[B_mb])
                k.dma("sp", mb[1:2, :], mod_b[l:l + 1, :], writes=[B_mb])
                for cb in range(12):
                    i = it % 2
                    it += 1
                    k.dma("sp" if cb % 2 == 0 else "act", wt[i][:],
                          mod_w[l, :, cb * 512:(cb + 1) * 512].rearrange("(k p) n -> p k n", p=128),
                          writes=[B_wt[i]])
                    for kc in range(8):
                        k.op("pe", lambda e, kc=kc, i=i: e.matmul(
                            pm[i][:], lhsT=sT[:, kc, :], rhs=wt[i][:, kc, :],
                            start=(kc == 0), stop=(kc == 7)),
                            reads=[B_sT, B_wt[i]], writes=[B_pm[i]])
                    k.op("dve", lambda e, i=i, cb=cb: e.tensor_tensor(
                        out=mrow[:, cb * 512:(cb + 1) * 512], in0=pm[i][:],
                        in1=mb[:, cb * 512:(cb + 1) * 512], op=ALU.add),
                        reads=[B_pm[i], B_mb], writes=[B_mrow])
                k.dma("sp", modrow[l], mrow[:], reads=[B_mrow], writes=[])
            k.barrier()


        def load_bc(st, name, src_row_ap, n, q="sp"):
            t = sb(st, name, [128, n]); B = Buf()
            k.dma(q, t[:], src_row_ap.partition_broadcast(128), writes=[B])
            return t, B

        def mod_bc(st, name, l, r, chunk, plus1=False):
            t, B = load_bc(st, name, modrow[l, r:r + 1, chunk * D:(chunk + 1) * D], D)
            if plus1:
                k.op("pool", lambda e: e.tensor_scalar(out=t[:], in0=t[:], scalar1=1.0, scalar2=None,
                                                       op0=ALU.add), reads=[B], writes=[B])
            return t, B

        u_scr = dscr("u_scr", [NT, 512])
        y_scr = dscr("y_scr", [NT, 512], BF16)
        x1_scr = dscr("x1_scr", [NT, D])
        x2_scr = dscr("x2_scr", [NT, D])
        dbg_q = dscr("dbg_q", [NT, 1280])

        def src_rows(ti, l):
            if l == 0:
                return x_in[ti * 128:(ti + 1) * 128, :] if ti < NLT else ctx_in[(ti - NLT) * 128:(ti - NLT + 1) * 128, :]
            return x2_scr[ti * 128:(ti + 1) * 128, :]

        def rope_apply(st_bufs, src_ps, B_src, dst, B_dst, nh, rt, B_rt, tmp1, tmp2, B_tmp):
            S = src_ps.rearrange("p (h a b f) -> p h a b f", h=nh, a=2, b=2, f=16)
            O = dst.rearrange("p (h a b f) -> p h a b f", h=nh, a=2, b=2, f=16)
            T1 = tmp1.rearrange("p (h a b f) -> p h a b f", h=nh, a=2, b=2, f=16)
            T2 = tmp2.rearrange("p (h a b f) -> p h a b f", h=nh, a=2, b=2, f=16)
            for a in range(2):
                cos = rt[:, a * 32:a * 32 + 16].rearrange("p (x y f) -> p x y f", x=1, y=1).to_broadcast([128, nh, 2, 16])
                sin = rt[:, a * 32 + 16:a * 32 + 32].rearrange("p (x y f) -> p x y f", x=1, y=1).to_broadcast([128, nh, 2, 16])
                k.op("dve", lambda e, a=a, cos=cos: e.tensor_tensor(out=T1[:, :, a], in0=S[:, :, a], in1=cos, op=ALU.mult),
                     reads=[B_src, B_rt], writes=[B_tmp])
                k.op("dve", lambda e, a=a, sin=sin: e.tensor_tensor(out=T2[:, :, a], in0=S[:, :, a, ::-1, :], in1=sin, op=ALU.mult),
                     reads=[B_src, B_rt], writes=[B_tmp])
                k.op("dve", lambda e, a=a: e.tensor_tensor(out=O[:, :, a, 0, :], in0=T1[:, :, a, 0, :], in1=T2[:, :, a, 0, :], op=ALU.subtract),
                     reads=[B_tmp], writes=[B_dst])
                k.op("dve", lambda e, a=a: e.tensor_tensor(out=O[:, :, a, 1, :], in0=T1[:, :, a, 1, :], in1=T2[:, :, a, 1, :], op=ALU.add),
                     reads=[B_tmp], writes=[B_dst])


        def ln_epilogue(wk, y_parts, B_y, xo, B_xo, g_t, lng_t, lnb_t, dst_rows):
            tmp, B_tmp, z, B_z, stt, B_stt, o, B_o = wk
            for hf in range(2):
                sl = slice(hf * 512, (hf + 1) * 512)
                k.op("dve", lambda e, hf=hf, sl=sl: e.tensor_tensor(out=tmp[:, sl], in0=y_parts[hf], in1=g_t[0][:, sl], op=ALU.mult),
                     reads=[B_y[hf], g_t[1]], writes=[B_tmp])
            k.op("dve", lambda e: e.scalar_tensor_tensor(out=z[:], in0=xo[:], scalar=ALPHA, in1=tmp[:], op0=ALU.mult, op1=ALU.add),
                 reads=[B_xo, B_tmp], writes=[B_z])
            for hf in range(2):
                k.op("dve", lambda e, hf=hf: e.bn_stats(out=stt[:, hf * 6:(hf + 1) * 6], in_=z[:, hf * 512:(hf + 1) * 512]),
                     reads=[B_z], writes=[B_stt])
            k.op("dve", lambda e: e.bn_aggr(out=stt[:, 12:14], in_=stt[:, 0:12]), reads=[B_stt], writes=[B_stt])
            k.op("dve", lambda e: e.tensor_scalar(out=stt[:, 15:16], in0=stt[:, 13:14], scalar1=LN_EPS, scalar2=None, op0=ALU.add),
                 reads=[B_stt], writes=[B_stt])
            k.op("act", lambda e: e.sqrt(out=stt[:, 15:16], in_=stt[:, 15:16]), reads=[B_stt], writes=[B_stt])
            k.op("dve", lambda e: e.reciprocal(out=stt[:, 14:15], in_=stt[:, 15:16]), reads=[B_stt], writes=[B_stt])
            k.op("dve", lambda e: e.tensor_scalar(out=tmp[:], in0=z[:], scalar1=stt[:, 12:13], scalar2=stt[:, 14:15], op0=ALU.subtract, op1=ALU.mult),
                 reads=[B_z, B_stt], writes=[B_tmp])
            k.op("pool", lambda e: e.tensor_tensor(out=o[:], in0=tmp[:], in1=lng_t[0][:], op=ALU.mult),
                 reads=[B_tmp, lng_t[1]], writes=[B_o])
            k.op("pool", lambda e: e.tensor_tensor(out=o[:], in0=o[:], in1=lnb_t[0][:], op=ALU.add),
                 reads=[B_o, lnb_t[1]], writes=[B_o])
            k.dma("sp", dst_rows, o[:], reads=[B_o])

        def ln_work(st, pfx):
            tmp = sb(st, pfx + "_tmp", [128, D]); z = sb(st, pfx + "_z", [128, D])
            stt = sb(st, pfx + "_stt", [128, 16]); o = sb(st, pfx + "_o", [128, D])
            return (tmp, Buf(), z, Buf(), stt, Buf(), o, Buf())

        def outproj_ln1(st, l, catT_, B_catT_, w_out_dram, ntiles):
            w_bf = sb(st, "w_out_bf", [128, 8, D], BF16); B_w = Buf()
            for kc in range(8):
                k.dma("pool", w_bf[:, kc, :], w_out_dram[kc * 128:(kc + 1) * 128, :], writes=[B_w])
            g1 = [mod_bc(st, f"g1_{r}", l, r, 2) for r in range(2)]
            lng = load_bc(st, "ln1g", ln1_g[l:l + 1, :], D)
            lnb = load_bc(st, "ln1b", ln1_b[l:l + 1, :], D)
            wk = ln_work(st, "e1")
            xo = [sb(st, f"xo{i}", [128, D]) for i in range(2)]; B_xo = [Buf(), Buf()]
            py = [ps(st, f"py{i}", [128, 512]) for i in range(4)]; B_py = [Buf() for _ in range(4)]
            for ti in range(ntiles):
                i = ti % 2
                r = 0 if ti < NLT else 1
                T0 = ti * 128
                k.dma("sp", xo[i][:], src_rows(ti, l), writes=[B_xo[i]])
                for hf in range(2):
                    pi = i * 2 + hf
                    for kc in range(8):
                        k.op("pe", lambda e, kc=kc, hf=hf, pi=pi, T0=T0: e.matmul(
                            py[pi][:], lhsT=catT_[:, kc, T0:T0 + 128], rhs=w_bf[:, kc, hf * 512:(hf + 1) * 512],
                            start=(kc == 0), stop=(kc == 7)), reads=[B_catT_, B_w], writes=[B_py[pi]])
                ln_epilogue(wk, [py[i * 2][:], py[i * 2 + 1][:]], [B_py[i * 2], B_py[i * 2 + 1]], xo[i], B_xo[i],
                            g1[r], lng, lnb, x1_scr[T0:T0 + 128, :])
            k.barrier()

        def moe_phase(st, l, ntiles, dst):
            ntok = ntiles * 128
            h2T = sb(st, "h2T", [128, 8, ntok], BF16); B_h2T = Buf()
            gateT = sb(st, "gateT", [32, ntok], BF16); B_gateT = Buf()
            f_acc = sb(st, "f_acc", [128, ntiles, D]); B_facc = [Buf() for _ in range(ntiles)]
            sel = sb(st, "sel", [32, 32, 128], BF16); B_sel = Buf()
            k.dma("pool", sel[:], sel_in[:, :, :], writes=[B_sel])
            with ExitStack() as s1:
                Wr = sb(s1, "Wr", [128, 8, 36]); B_Wr = Buf()
                with nc.allow_non_contiguous_dma(reason="small router weights"):
                    k.dma("sp", Wr[:, :, 0:4], moe_wg[l].rearrange("(k p) n -> p k n", p=128), writes=[B_Wr])
                    for g in range(4):
                        k.dma("sp", Wr[:, :, 4 + g * 8:12 + g * 8], moe_we[l, g].rearrange("(k p) n -> p k n", p=128), writes=[B_Wr])
                Whi = sb(s1, "Whi", [128, 8, 36], BF16); Wlo = sb(s1, "Wlo", [128, 8, 36], BF16); B_Wsp = Buf()
                k.op("dve", lambda e: e.tensor_copy(out=Whi[:], in_=Wr[:]), reads=[B_Wr], writes=[B_Wsp])
                k.op("dve", lambda e: e.tensor_tensor(out=Wlo[:], in0=Wr[:], in1=Whi[:], op=ALU.subtract), reads=[B_Wr, B_Wsp], writes=[B_Wsp])
                rb = sb(s1, "rb", [128, 36]); B_rb = Buf()
                k.dma("sp", rb[:, 0:4], moe_bg[l:l + 1, :].partition_broadcast(128), writes=[B_rb])
                k.dma("sp", rb[:, 4:36], moe_be[l:l + 1, :].partition_broadcast(128), writes=[B_rb])
                sc2p = [mod_bc(s1, f"sc2p_{r}", l, r, 4, plus1=True) for r in range(2)]
                sh2 = [mod_bc(s1, f"sh2_{r}", l, r, 3) for r in range(2)]
                xt = [sb(s1, f"mx{i}", [128, D]) for i in range(2)]; B_xt = [Buf(), Buf()]
                hf32 = sb(s1, "mh", [128, D]); B_h = Buf()
                hhi = sb(s1, "hhi", [128, D], BF16); hlo = sb(s1, "hlo", [128, D], BF16); B_hs = Buf()
                hloT = sb(s1, "hloT", [128, 8, 128], BF16); B_hloT = Buf()
                pTh = ps(s1, "mpTh", [128, 8, 128], BF16); B_pTh = Buf()
                pTl = ps(s1, "mpTl", [128, 8, 128], BF16); B_pTl = Buf()
                pr = ps(s1, "mpr", [128, 512]); B_pr = Buf()
                pg = ps(s1, "mpg", [32, 1024], BF16); B_pg = Buf()
                lg = sb(s1, "lg", [128, 36]); B_lg = Buf()
                sm = sb(s1, "rsm", [128, 160]); B_sm = Buf()
                gates = sb(s1, "gates", [128, 32]); B_gates = Buf()
                gates_bf = sb(s1, "gates_bf", [128, 32], BF16); B_gbf = Buf()
                for ti in range(ntiles):
                    i = ti % 2
                    r = 0 if ti < NLT else 1
                    T0 = ti * 128
                    k.dma("sp", xt[i][:], x1_scr[T0:T0 + 128, :], writes=[B_xt[i]])
                    k.op("dve", lambda e, i=i, r=r: e.tensor_tensor(out=hf32[:], in0=xt[i][:], in1=sc2p[r][0][:], op=ALU.mult),
                         reads=[B_xt[i], sc2p[r][1]], writes=[B_h])
                    k.op("pool", lambda e, r=r: e.tensor_tensor(out=hf32[:], in0=hf32[:], in1=sh2[r][0][:], op=ALU.add),
                         reads=[B_h, sh2[r][1]], writes=[B_h])
                    k.op("pool", lambda e: e.tensor_copy(out=hhi[:], in_=hf32[:]), reads=[B_h], writes=[B_hs])
                    k.op("dve", lambda e: e.tensor_tensor(out=hlo[:], in0=hf32[:], in1=hhi[:], op=ALU.subtract), reads=[B_h, B_hs], writes=[B_hs])
                    for kc in range(8):
                        k.op("pe", lambda e, kc=kc: e.transpose(out=pTh[:, kc, :], in_=hhi[:, kc * 128:(kc + 1) * 128], identity=ident_b[:]),
                             reads=[B_hs, B_ident_b], writes=[B_pTh])
                    for kc in range(8):
                        k.op("pe", lambda e, kc=kc: e.transpose(out=pTl[:, kc, :], in_=hlo[:, kc * 128:(kc + 1) * 128], identity=ident_b[:]),
                             reads=[B_hs, B_ident_b], writes=[B_pTl])
                    k.op("act", lambda e, T0=T0: e.copy(out=h2T[:, :, T0:T0 + 128], in_=pTh[:]), reads=[B_pTh], writes=[B_h2T])
                    k.op("dve", lambda e: e.tensor_copy(out=hloT[:], in_=pTl[:]), reads=[B_pTl], writes=[B_hloT])
                    n_mm = 24
                    j = 0
                    for (A, BA, W) in ((None, B_h2T, Whi), (hloT, B_hloT, Whi), (None, B_h2T, Wlo)):
                        for kc in range(8):
                            lhs = h2T[:, kc, T0:T0 + 128] if A is None else A[:, kc, :]
                            k.op("pe", lambda e, lhs=lhs, W=W, kc=kc, j=j: e.matmul(pr[:, 0:36], lhsT=lhs, rhs=W[:, kc, :], start=(j == 0), stop=(j == 23)),
                                 reads=[BA, B_Wsp], writes=[B_pr])
                            j += 1
                    R = [B_lg, B_sm]
                    def dv(fn, reads=R, writes=(B_sm,)):
                        k.op("dve", fn, reads=list(reads), writes=list(writes))
                    k.op("dve", lambda e: e.tensor_tensor(out=lg[:], in0=pr[:, 0:36], in1=rb[:], op=ALU.add), reads=[B_pr, B_rb], writes=[B_lg])
                    dv(lambda e: e.reduce_max(out=sm[:, 0:1], in_=lg[:, 0:4], axis=AX.X))
                    dv(lambda e: e.tensor_scalar(out=sm[:, 1:2], in0=sm[:, 0:1], scalar1=-1.0, scalar2=None, op0=ALU.mult))
                    k.op("act", lambda e: e.activation(out=sm[:, 56:60], in_=lg[:, 0:4], func=AF.Exp, bias=sm[:, 1:2], scale=1.0, accum_out=sm[:, 2:3]),
                         reads=R, writes=[B_sm])
                    dv(lambda e: e.reciprocal(out=sm[:, 3:4], in_=sm[:, 2:3]))
                    dv(lambda e: e.tensor_scalar(out=sm[:, 4:8], in0=lg[:, 0:4], scalar1=sm[:, 0:1], scalar2=None, op0=ALU.is_equal))
                    le = lg[:, 4:36].rearrange("p (g e) -> p g e", g=4)
                    tmp48 = sm[:, 64:96].rearrange("p (g e) -> p g e", g=4)
                    ohb = sm[:, 4:8].rearrange("p (g x) -> p g x", x=1).to_broadcast([128, 4, 8])
                    dv(lambda e: e.tensor_tensor(out=tmp48, in0=le, in1=ohb, op=ALU.mult))
                    dv(lambda e: e.tensor_reduce(out=sm[:, 8:16], in_=sm[:, 64:96].rearrange("p (g e) -> p e g", g=4), axis=AX.X, op=ALU.add))
                    dv(lambda e: e.reduce_max(out=sm[:, 16:17], in_=sm[:, 8:16], axis=AX.X))
                    dv(lambda e: e.tensor_scalar(out=sm[:, 24:32], in0=sm[:, 8:16], scalar1=sm[:, 16:17], scalar2=None, op0=ALU.is_equal))
                    dv(lambda e: e.scalar_tensor_tensor(out=sm[:, 32:40], in0=sm[:, 24:32], scalar=-1e30, in1=sm[:, 8:16], op0=ALU.mult, op1=ALU.add))
                    dv(lambda e: e.reduce_max(out=sm[:, 17:18], in_=sm[:, 32:40], axis=AX.X))
                    dv(lambda e: e.tensor_scalar(out=sm[:, 40:48], in0=sm[:, 32:40], scalar1=sm[:, 17:18], scalar2=None, op0=ALU.is_equal))
                    dv(lambda e: e.tensor_tensor(out=sm[:, 18:19], in0=sm[:, 17:18], in1=sm[:, 16:17], op=ALU.subtract))
                    k.op("act", lambda e: e.activation(out=sm[:, 19:20], in_=sm[:, 18:19], func=AF.Exp), reads=R, writes=[B_sm])
                    dv(lambda e: e.tensor_scalar(out=sm[:, 20:21], in0=sm[:, 19:20], scalar1=1.0, scalar2=None, op0=ALU.add))
                    dv(lambda e: e.reciprocal(out=sm[:, 20:21], in_=sm[:, 20:21]))
                    dv(lambda e: e.tensor_tensor(out=sm[:, 21:22], in0=sm[:, 19:20], in1=sm[:, 20:21], op=ALU.mult))
                    dv(lambda e: e.tensor_tensor(out=sm[:, 22:23], in0=sm[:, 20:21], in1=sm[:, 3:4], op=ALU.mult))
                    dv(lambda e: e.tensor_tensor(out=sm[:, 23:24], in0=sm[:, 21:22], in1=sm[:, 3:4], op=ALU.mult))
                    dv(lambda e: e.tensor_scalar(out=sm[:, 48:56], in0=sm[:, 24:32], scalar1=sm[:, 22:23], scalar2=None, op0=ALU.mult))
                    dv(lambda e: e.scalar_tensor_tensor(out=sm[:, 48:56], in0=sm[:, 40:48], scalar=sm[:, 23:24], in1=sm[:, 48:56], op0=ALU.mult, op1=ALU.add))
                    geb = sm[:, 48:56].rearrange("p (x e) -> p x e", x=1).to_broadcast([128, 4, 8])
                    k.op("dve", lambda e: e.tensor_tensor(out=gates[:].rearrange("p (g e) -> p g e", g=4), in0=ohb, in1=geb, op=ALU.mult),
                         reads=R, writes=[B_gates])
                    k.op("dve", lambda e: e.tensor_copy(out=gates_bf[:], in_=gates[:]), reads=[B_gates], writes=[B_gbf])
                    k.op("pe", lambda e: e.transpose(out=pg[:, 0:128], in_=gates_bf[:], identity=ident_b[:]),
                         reads=[B_gbf, B_ident_b], writes=[B_pg])
                    k.op("act", lambda e, T0=T0: e.copy(out=gateT[:, T0:T0 + 128], in_=pg[:, 0:128]), reads=[B_pg], writes=[B_gateT])
                    if "dbg_gates" in dbg:
                        if ti == 0:
                            dbg_gates = dscr("dbg_gates", [NT, 32])
                        k.dma("sp", dbg_gates[T0:T0 + 128, :], gates[:], reads=[B_gates])
                k.barrier()
            if "stop_router" in dbg:
                return
            with ExitStack() as s2:
                w13 = [sb(s2, f"w13_{i}", [128, 8, 512], BF16) for i in range(2)]; B_w13 = [Buf(), Buf()]
                w2 = [sb(s2, f"w2_{i}", [128, 2, D], BF16) for i in range(2)]; B_w2 = [Buf(), Buf()]
                hh = [sb(s2, f"hh{i}", [128, 2, ntok], BF16) for i in range(2)]; B_hh = [Buf(), Buf()]
                stg13 = sb(s2, "stg13", [128, 8, 512]); B_stg13 = Buf()
                stg2 = sb(s2, "stg2", [128, 2, D]); B_stg2 = Buf()
                s1t = [sb(s2, f"s1t{i}", [128, 512]) for i in range(2)]; B_s1t = [Buf(), Buf()]
                t3 = [sb(s2, f"t3{i}", [128, 512]) for i in range(2)]; B_t3 = [Buf(), Buf()]
                ph1 = [ps(s2, f"ph1_{i}", [128, 512]) for i in range(2)]; B_ph1 = [Buf(), Buf()]
                ph3 = [ps(s2, f"ph3_{i}", [128, 512]) for i in range(2)]; B_ph3 = [Buf(), Buf()]
                pgb = ps(s2, "pgb", [128, 512]); B_pgb = Buf()
                pf = [ps(s2, f"pf{i}", [128, 512]) for i in range(2)]; B_pf = [Buf(), Buf()]
                blocks = [(b0, min(512, ntok - b0)) for b0 in range(0, ntok, 512)]
                it = 0; itf = 0
                for e_ in range(32):
                    wi = e_ % 2
                    k.dma("sp", stg13[:, :, 0:256], moe_w1[l, e_].rearrange("(k p) f -> p k f", p=128), writes=[B_stg13])
                    k.dma("act", stg13[:, :, 256:512], moe_w3[l, e_].rearrange("(k p) f -> p k f", p=128), writes=[B_stg13])
                    k.dma("sp", stg2[:], moe_w2[l, e_].rearrange("(c p) d -> p c d", p=128), writes=[B_stg2])
                    k.op("pool", lambda e, wi=wi: e.tensor_copy(out=w13[wi][:], in_=stg13[:]), reads=[B_stg13], writes=[B_w13[wi]])
                    k.op("pool", lambda e, wi=wi: e.tensor_copy(out=w2[wi][:], in_=stg2[:]), reads=[B_stg2], writes=[B_w2[wi]])
                    for (b0, bn) in blocks:
                        k.op("pe", lambda e, e_=e_, b0=b0, bn=bn: e.matmul(pgb[:, 0:bn], lhsT=sel[:, e_, :], rhs=gateT[:, b0:b0 + bn], start=True, stop=True),
                             reads=[B_sel, B_gateT], writes=[B_pgb])
                        for fc in range(2):
                            i = it % 2; it += 1
                            for kc in range(8):
                                k.op("pe", lambda e, kc=kc, fc=fc, i=i, wi=wi, b0=b0, bn=bn: e.matmul(
                                    ph1[i][:, 0:bn], lhsT=w13[wi][:, kc, fc * 128:(fc + 1) * 128], rhs=h2T[:, kc, b0:b0 + bn],
                                    start=(kc == 0), stop=(kc == 7)), reads=[B_w13[wi], B_h2T], writes=[B_ph1[i]])
                            for kc in range(8):
                                k.op("pe", lambda e, kc=kc, fc=fc, i=i, wi=wi, b0=b0, bn=bn: e.matmul(
                                    ph3[i][:, 0:bn], lhsT=w13[wi][:, kc, 256 + fc * 128:256 + (fc + 1) * 128], rhs=h2T[:, kc, b0:b0 + bn],
                                    start=(kc == 0), stop=(kc == 7)), reads=[B_w13[wi], B_h2T], writes=[B_ph3[i]])
                            k.op("act", lambda e, i=i, bn=bn: e.activation(out=s1t[i][:, 0:bn], in_=ph1[i][:, 0:bn], func=AF.Silu),
                                 reads=[B_ph1[i]], writes=[B_s1t[i]])
                            k.op("dve", lambda e, i=i, bn=bn: e.tensor_tensor(out=t3[i][:, 0:bn], in0=s1t[i][:, 0:bn], in1=ph3[i][:, 0:bn], op=ALU.mult),
                                 reads=[B_s1t[i], B_ph3[i]], writes=[B_t3[i]])
                            k.op("dve", lambda e, i=i, bn=bn, fc=fc, wi=wi, b0=b0: e.tensor_tensor(out=hh[wi][:, fc, b0:b0 + bn], in0=t3[i][:, 0:bn], in1=pgb[:, 0:bn], op=ALU.mult),
                                 reads=[B_t3[i], B_pgb], writes=[B_hh[wi]])
                    for tt in range(ntiles):
                        for dc in range(2):
                            j = itf % 2; itf += 1
                            for fc in range(2):
                                k.op("pe", lambda e, fc=fc, j=j, wi=wi, tt=tt, dc=dc: e.matmul(
                                    pf[j][:], lhsT=hh[wi][:, fc, tt * 128:(tt + 1) * 128], rhs=w2[wi][:, fc, dc * 512:(dc + 1) * 512],
                                    start=(fc == 0), stop=(fc == 1)), reads=[B_hh[wi], B_w2[wi]], writes=[B_pf[j]])
                            if e_ == 0:
                                k.op("dve", lambda e, j=j, tt=tt, dc=dc: e.tensor_copy(out=f_acc[:, tt, dc * 512:(dc + 1) * 512], in_=pf[j][:]),
                                     reads=[B_pf[j]], writes=[B_facc[tt]])
                            else:
                                k.op("dve", lambda e, j=j, tt=tt, dc=dc: e.tensor_tensor(out=f_acc[:, tt, dc * 512:(dc + 1) * 512],
                                     in0=f_acc[:, tt, dc * 512:(dc + 1) * 512], in1=pf[j][:], op=ALU.add),
                                     reads=[B_pf[j], B_facc[tt]], writes=[B_facc[tt]])
                k.barrier()
            if "stop_experts" in dbg:
                return
            with ExitStack() as s3:
                g2 = [mod_bc(s3, f"g2_{r}", l, r, 5) for r in range(2)]
                lng = load_bc(s3, "ln2g", ln2_g[l:l + 1, :], D)
                lnb = load_bc(s3, "ln2b", ln2_b[l:l + 1, :], D)
                wk = ln_work(s3, "e2")
                xo = [sb(s3, f"x1o{i}", [128, D]) for i in range(2)]; B_xo = [Buf(), Buf()]
                for ti in range(ntiles):
                    i = ti % 2
                    r = 0 if ti < NLT else 1
                    T0 = ti * 128
                    k.dma("sp", xo[i][:], x1_scr[T0:T0 + 128, :], writes=[B_xo[i]])
                    if "dbg_f" in dbg:
                        if ti == 0:
                            dbg_f = dscr("dbg_f", [NT, D])
                        k.dma("sp", dbg_f[T0:T0 + 128, :], f_acc[:, ti, :], reads=[B_facc[ti]])
                    ln_epilogue(wk, [f_acc[:, ti, 0:512], f_acc[:, ti, 512:1024]], [B_facc[ti], B_facc[ti]], xo[i], B_xo[i],
                                g2[r], lng, lnb, dst[T0:T0 + 128, :])
                k.barrier()


        def ssm_phase(st, catT_, B_catT_):
            PI = math.pi
            TWO_PI = 2.0 * math.pi
            def bc3(ap2, n):
                P_, G_ = ap2.shape
                return ap2.rearrange("p (g x) -> p g x", x=1).to_broadcast([P_, G_, n])
            I32 = mybir.dt.int32
            INV2PI = 1.0 / TWO_PI

            def sincos(ang_ap, shape, s_out, c_out, tmps, B_in, B_out, B_tmp):
                y, yi, yf = tmps
                dvt = lambda fn: k.op("dve", fn, reads=[B_in, B_tmp, B_out], writes=[B_tmp])
                dvt(lambda e: e.tensor_scalar(out=y, in0=ang_ap, scalar1=INV2PI, scalar2=32.5, op0=ALU.mult, op1=ALU.add))
                dvt(lambda e: e.tensor_copy(out=yi, in_=y))
                dvt(lambda e: e.tensor_copy(out=yf, in_=yi))
                dvt(lambda e: e.tensor_tensor(out=y, in0=y, in1=yf, op=ALU.subtract))
                dvt(lambda e: e.scalar_tensor_tensor(out=yf, in0=y, scalar=0.0, in1=y, op0=ALU.is_lt, op1=ALU.add))
                k.op("act", lambda e: e.activation(out=s_out, in_=yf, func=AF.Sin, bias=negpi[0:shape[0], :], scale=TWO_PI),
                     reads=[B_tmp, B_np], writes=[B_out])
                dvt(lambda e: e.tensor_scalar(out=y, in0=yf, scalar1=0.25, scalar2=None, op0=ALU.add))
                dvt(lambda e: e.scalar_tensor_tensor(out=yf, in0=y, scalar=1.0, in1=y, op0=ALU.is_ge, op1=ALU.subtract))
                k.op("act", lambda e: e.activation(out=c_out, in_=yf, func=AF.Sin, bias=negpi[0:shape[0], :], scale=-TWO_PI),
                     reads=[B_tmp, B_np], writes=[B_out])

            ar = sb(st, "ar", [64, 64]); ai = sb(st, "ai", [64, 64]); ls = sb(st, "ls", [64, 64]); B_par = Buf()
            with nc.allow_non_contiguous_dma(reason="ssm params"):
                k.dma("sp", ar[:], a_re.rearrange("d g p -> p (d g)"), writes=[B_par])
                k.dma("sp", ai[:], a_im.rearrange("d g p -> p (d g)"), writes=[B_par])
            k.dma("sp", ls[:], log_step.rearrange("(x d) g -> x (d g)", x=1).partition_broadcast(64), writes=[B_par])
            negpi = sb(st, "negpi", [128, 1]); B_np = Buf()
            k.op("dve", lambda e: e.memset(negpi[:], -PI), writes=[B_np])
            kv = sb(st, "kv", [64, 16, 64]); B_kv = Buf()
            k.dma("sp", kv[:], kval_in.rearrange("p (k g) -> p k g", k=16), writes=[B_kv])
            mrow = sb(st, "mrow", [64, 288]); B_mrow = Buf()
            k.dma("sp", mrow[:], mrow_in[:, :], writes=[B_mrow])
            maskF = sb(st, "maskF", [128, 128]); maskB = sb(st, "maskB", [128, 128]); B_mk = Buf()
            k.dma("sp", maskF[:], maskF_in[:, :], writes=[B_mk])
            k.dma("sp", maskB[:], maskB_in[:, :], writes=[B_mk])
            dar = sb(st, "dar", [64, 64]); dai = sb(st, "dai", [64, 64]); B_d = Buf()
            k.op("act", lambda e: e.activation(out=ls[:], in_=ls[:], func=AF.Exp), reads=[B_par], writes=[B_par])
            k.op("dve", lambda e: e.tensor_tensor(out=dar[:], in0=ls[:], in1=ar[:], op=ALU.mult), reads=[B_par], writes=[B_d])
            k.op("dve", lambda e: e.tensor_tensor(out=dai[:], in0=ls[:], in1=ai[:], op=ALU.mult), reads=[B_par, B_d], writes=[B_d])
            LR = sb(st, "LR", [64, 16, 64]); LI = sb(st, "LI", [64, 16, 64]); MG = sb(st, "MG", [64, 16, 64]); B_L = Buf()
            th8 = sb(st, "th8", [64, 64]); B_th8 = Buf()
            k.op("dve", lambda e: e.tensor_scalar(out=th8[:], in0=dai[:], scalar1=8.0, scalar2=None, op0=ALU.mult), reads=[B_d], writes=[B_th8])
            with ExitStack() as t0:
                ang = sb(t0, "ang", [64, 16, 64]); a2 = sb(t0, "a2", [64, 16, 64]); B_ang = Buf()
                dai_b = dai[:].rearrange("p (x g) -> p x g", x=1).to_broadcast([64, 16, 64])
                dar_b = dar[:].rearrange("p (x g) -> p x g", x=1).to_broadcast([64, 16, 64])
                k.op("dve", lambda e: e.tensor_tensor(out=MG[:], in0=kv[:], in1=dar_b, op=ALU.mult), reads=[B_kv, B_d], writes=[B_L])
                k.op("act", lambda e: e.activation(out=MG[:], in_=MG[:], func=AF.Exp), reads=[B_L], writes=[B_L])
                k.op("dve", lambda e: e.tensor_tensor(out=ang[:], in0=kv[:], in1=dai_b, op=ALU.mult), reads=[B_kv, B_d], writes=[B_ang])
                a3 = sb(t0, "a3", [64, 16, 64], I32); a4 = sb(t0, "a4", [64, 16, 64])
                sincos(ang[:], [64, 16, 64], LI[:], LR[:], (a2[:], a3[:], a4[:]), B_ang, B_L, B_ang)
                k.op("dve", lambda e: e.tensor_tensor(out=LR[:], in0=LR[:], in1=MG[:], op=ALU.mult), reads=[B_L], writes=[B_L])
                k.op("dve", lambda e: e.tensor_tensor(out=LI[:], in0=LI[:], in1=MG[:], op=ALU.mult), reads=[B_L], writes=[B_L])
                k.barrier()
            cre = sb(st, "cre", [64, 64]); cim = sb(st, "cim", [64, 64]); B_c = Buf()
            with ExitStack() as t0:
                nr = sb(t0, "nr", [64, 64]); den = sb(t0, "den", [64, 64]); tq = sb(t0, "tq", [64, 64]); B_t = Buf()
                L1r = LR[:, 8, :]; L1i = LI[:, 8, :]
                dv = lambda fn: k.op("dve", fn, reads=[B_t, B_L, B_par, B_c], writes=[B_t, B_c])
                dv(lambda e: e.tensor_scalar(out=nr[:], in0=L1r, scalar1=-1.0, scalar2=None, op0=ALU.add))
                dv(lambda e: e.tensor_tensor(out=den[:], in0=ar[:], in1=ar[:], op=ALU.mult))
                dv(lambda e: e.tensor_tensor(out=tq[:], in0=ai[:], in1=ai[:], op=ALU.mult))
                dv(lambda e: e.tensor_tensor(out=den[:], in0=den[:], in1=tq[:], op=ALU.add))
                dv(lambda e: e.reciprocal(out=den[:], in_=den[:]))
                dv(lambda e: e.tensor_tensor(out=cre[:], in0=nr[:], in1=ar[:], op=ALU.mult))
                dv(lambda e: e.tensor_tensor(out=tq[:], in0=L1i, in1=ai[:], op=ALU.mult))
                dv(lambda e: e.tensor_tensor(out=cre[:], in0=cre[:], in1=tq[:], op=ALU.add))
                dv(lambda e: e.tensor_tensor(out=cre[:], in0=cre[:], in1=den[:], op=ALU.mult))
                dv(lambda e: e.tensor_tensor(out=cim[:], in0=L1i, in1=ar[:], op=ALU.mult))
                dv(lambda e: e.tensor_tensor(out=tq[:], in0=nr[:], in1=ai[:], op=ALU.mult))
                dv(lambda e: e.tensor_tensor(out=cim[:], in0=cim[:], in1=tq[:], op=ALU.subtract))
                dv(lambda e: e.tensor_tensor(out=cim[:], in0=cim[:], in1=den[:], op=ALU.mult))
                k.barrier()
            UT_all = sb(st, "UT_all", [128, 32, 288], BF16); B_UT = Buf()
            NB = ((0, 128), (128, 128), (256, 32))
            u8v = u_scr.rearrange("(n j) f -> n (j f)", j=8)
            with ExitStack() as t0:
                U8 = sb(t0, "U8", [128, 3, 4096]); B_U8 = Buf()
                U8b = sb(t0, "U8b", [128, 3, 4096], BF16); B_U8b = Buf()
                pU = [ps(t0, f"pU{i}", [128, 1024], BF16) for i in range(2)]; B_pU = [Buf(), Buf()]
                for bi, (n0, nb) in enumerate(NB):
                    k.dma("sp", U8[0:nb, bi, :], u8v[n0:n0 + nb, :], writes=[B_U8])
                    ov = U8b[0:nb, bi, :].rearrange("p (g j c) -> p j g c", g=32, j=8, c=16)
                    iv = U8[0:nb, bi, :].rearrange("p (j g c) -> p j g c", g=32, j=8, c=16)
                    k.op("pool" if bi == 1 else "act", (lambda e, ov=ov, iv=iv: e.tensor_copy(out=ov, in_=iv)) if bi == 1 else
                         (lambda e, ov=ov, iv=iv: e.copy(out=ov, in_=iv)), reads=[B_U8], writes=[B_U8b])
                for g in range(32):
                    i = g % 2
                    for bi, (n0, nb) in enumerate(NB):
                        src = U8b[0:nb, bi, g * 128:(g + 1) * 128]
                        k.op("pe", lambda e, i=i, src=src, n0=n0, nb=nb: e.transpose(out=pU[i][:, n0:n0 + nb], in_=src, identity=ident_b[0:nb, 0:nb]),
                             reads=[B_U8b, B_ident_b], writes=[B_pU[i]])
                    k.op("act", lambda e, i=i, g=g: e.copy(out=UT_all[:, g, :], in_=pU[i][:, 0:288]), reads=[B_pU[i]], writes=[B_UT])
                k.barrier()
            COr = sb(st, "COr", [64, 2, 32, 128], BF16); COi = sb(st, "COi", [64, 2, 32, 128], BF16); B_CO = Buf()
            T_all = sb(st, "T_all", [128, 32, 128], BF16); B_T = Buf()
            WinT = sb(st, "WinT", [128, 2, 32, 128], BF16); B_WinT = Buf()
            with ExitStack() as t0:
                BLr = sb(t0, "BLr", [64, 32, 128], BF16); BLi = sb(t0, "BLi", [64, 32, 128], BF16); B_BL = Buf()
                CTr = sb(t0, "CTr", [64, 32, 128], BF16); CTi = sb(t0, "CTi", [64, 32, 128], BF16); B_CT = Buf()
                Br = sb(t0, "Br", [64, 64, 16]); Bi = sb(t0, "Bi", [64, 64, 16]); B_B = Buf()
                Cr = sb(t0, "Cr", [64, 64, 16]); Ci = sb(t0, "Ci", [64, 64, 16]); B_C = Buf()
                Bbr = sb(t0, "Bbr", [64, 64, 16]); Bbi = sb(t0, "Bbi", [64, 64, 16]); B_Bb = Buf()
                with nc.allow_non_contiguous_dma(reason="ssm B/C tables"):
                    for d in range(2):
                        k.dma("sp", Br[:, d * 32:(d + 1) * 32, :], b_re[d].rearrange("g p c -> p g c"), writes=[B_B])
                        k.dma("sp", Bi[:, d * 32:(d + 1) * 32, :], b_im[d].rearrange("g p c -> p g c"), writes=[B_B])
                        for gb in range(4):
                            sl = slice(d * 32 + gb * 8, d * 32 + gb * 8 + 8)
                            k.dma("sp", Cr[:, sl, :], c_re[d, gb * 8:(gb + 1) * 8].rearrange("g c p -> p g c"), writes=[B_C])
                            k.dma("act", Ci[:, sl, :], c_im[d, gb * 8:(gb + 1) * 8].rearrange("g c p -> p g c"), writes=[B_C])
                t1 = sb(t0, "t1", [64, 64, 16]); t2 = sb(t0, "t2", [64, 64, 16]); B_t12 = Buf()
                creb = bc3(cre[:], 16); cimb = bc3(cim[:], 16)
                dv = lambda fn: k.op("dve", fn, reads=[B_B, B_c, B_t12, B_Bb], writes=[B_t12, B_Bb])
                dv(lambda e: e.tensor_tensor(out=t1[:], in0=Br[:], in1=creb, op=ALU.mult))
                dv(lambda e: e.tensor_tensor(out=t2[:], in0=Bi[:], in1=cimb, op=ALU.mult))
                dv(lambda e: e.tensor_tensor(out=Bbr[:], in0=t1[:], in1=t2[:], op=ALU.subtract))
                dv(lambda e: e.tensor_tensor(out=t1[:], in0=Bi[:], in1=creb, op=ALU.mult))
                dv(lambda e: e.tensor_tensor(out=t2[:], in0=Br[:], in1=cimb, op=ALU.mult))
                dv(lambda e: e.tensor_tensor(out=Bbi[:], in0=t1[:], in1=t2[:], op=ALU.add))
                ta = sb(t0, "ta", [64, 32, 16]); tb_ = sb(t0, "tb", [64, 32, 16]); B_tab = Buf()
                tc_ = sb(t0, "tc", [64, 32, 16]); td_ = sb(t0, "td", [64, 32, 16]); B_tcd = Buf()
                pTd = [ps(t0, f"pTd{i}", [128, 512]) for i in range(2)]; B_pTd = [Buf(), Buf()]
                pW = [ps(t0, f"pW{i}", [128, 8, 128], BF16) for i in range(2)]; B_pW = [Buf(), Buf()]
                tt = sb(t0, "tt", [128, 128]); B_tt = Buf()
                for d in range(2):
                    dsl = slice(d * 32, (d + 1) * 32)
                    for j in range(8):
                        e_ = (7 - j) if d == 0 else j
                        lr = bc3(LR[:, e_ + 7, dsl], 16); li = bc3(LI[:, e_ + 7, dsl], 16)
                        o_r = BLr[:, :, j * 16:(j + 1) * 16]; o_i = BLi[:, :, j * 16:(j + 1) * 16]
                        dv2 = lambda fn: k.op("dve", fn, reads=[B_Bb, B_L, B_tab, B_BL], writes=[B_tab, B_BL])
                        dv2(lambda e, lr=lr: e.tensor_tensor(out=ta[:], in0=Bbr[:, dsl, :], in1=lr, op=ALU.mult))
                        dv2(lambda e, li=li: e.tensor_tensor(out=tb_[:], in0=Bbi[:, dsl, :], in1=li, op=ALU.mult))
                        dv2(lambda e, o_r=o_r: e.tensor_tensor(out=o_r, in0=ta[:], in1=tb_[:], op=ALU.subtract))
                        dv2(lambda e, lr=lr: e.tensor_tensor(out=ta[:], in0=Bbi[:, dsl, :], in1=lr, op=ALU.mult))
                        dv2(lambda e, li=li: e.tensor_tensor(out=tb_[:], in0=Bbr[:, dsl, :], in1=li, op=ALU.mult))
                        dv2(lambda e, o_i=o_i: e.tensor_tensor(out=o_i, in0=ta[:], in1=tb_[:], op=ALU.add))
                        f_ = (j - 7) if d == 0 else -j
                        for (kk, o_r, o_i, BO) in ((f_ + 7, CTr[:, :, j * 16:(j + 1) * 16], CTi[:, :, j * 16:(j + 1) * 16], B_CT),
                                                   (f_ + 15, COr[:, d, :, j * 16:(j + 1) * 16], COi[:, d, :, j * 16:(j + 1) * 16], B_CO)):
                            lr = bc3(LR[:, kk, dsl], 16); li = bc3(LI[:, kk, dsl], 16)
                            pl = lambda fn, BO=BO: k.op("pool", fn, reads=[B_C, B_L, B_tcd, BO], writes=[B_tcd, BO])
                            pl(lambda e, lr=lr: e.tensor_tensor(out=tc_[:], in0=Cr[:, dsl, :], in1=lr, op=ALU.mult))
                            pl(lambda e, li=li: e.tensor_tensor(out=td_[:], in0=Ci[:, dsl, :], in1=li, op=ALU.mult))
                            pl(lambda e, o_r=o_r: e.tensor_tensor(out=o_r, in0=tc_[:], in1=td_[:], op=ALU.subtract))
                            pl(lambda e, lr=lr: e.tensor_tensor(out=tc_[:], in0=Ci[:, dsl, :], in1=lr, op=ALU.mult))
                            pl(lambda e, li=li: e.tensor_tensor(out=td_[:], in0=Cr[:, dsl, :], in1=li, op=ALU.mult))
                            pl(lambda e: e.tensor_tensor(out=tc_[:], in0=tc_[:], in1=td_[:], op=ALU.add))
                            pl(lambda e, o_i=o_i: e.tensor_scalar(out=o_i, in0=tc_[:], scalar1=-1.0, scalar2=None, op0=ALU.mult))
                    for g in range(32):
                        i = g % 2
                        k.op("pe", lambda e, i=i, g=g: e.matmul(pTd[i][:, 0:128], lhsT=BLr[:, g, :], rhs=CTr[:, g, :], start=True, stop=False),
                             reads=[B_BL, B_CT], writes=[B_pTd[i]])
                        k.op("pe", lambda e, i=i, g=g: e.matmul(pTd[i][:, 0:128], lhsT=BLi[:, g, :], rhs=CTi[:, g, :], start=False, stop=True),
                             reads=[B_BL, B_CT], writes=[B_pTd[i]])
                        if d == 0:
                            k.op("dve", lambda e, i=i, g=g: e.tensor_tensor(out=T_all[:, g, :], in0=pTd[i][:, 0:128], in1=maskF[:], op=ALU.mult),
                                 reads=[B_pTd[i], B_mk], writes=[B_T])
                        else:
                            k.op("dve", lambda e, i=i: e.tensor_tensor(out=tt[:], in0=pTd[i][:, 0:128], in1=maskB[:], op=ALU.mult),
                                 reads=[B_pTd[i], B_mk], writes=[B_tt])
                            k.op("dve", lambda e, g=g: e.tensor_tensor(out=T_all[:, g, :], in0=T_all[:, g, :], in1=tt[:], op=ALU.add),
                                 reads=[B_tt, B_T], writes=[B_T])
                        k.op("pe", lambda e, i=i, g=g: e.transpose(out=pW[i][:, 0, 0:64], in_=BLr[:, g, :], identity=ident_b[0:64, 0:64]),
                             reads=[B_BL, B_ident_b], writes=[B_pW[i]])
                        k.op("pe", lambda e, i=i, g=g: e.transpose(out=pW[i][:, 0, 64:128], in_=BLi[:, g, :], identity=ident_b[0:64, 0:64]),
                             reads=[B_BL, B_ident_b], writes=[B_pW[i]])
                        k.op("act", lambda e, i=i, g=g, d=d: e.copy(out=WinT[:, d, g, :], in_=pW[i][:, 0, :]), reads=[B_pW[i]], writes=[B_WinT])
                k.barrier()
            with ExitStack() as t0:
                Yt = sb(t0, "Yt", [128, 3, 4096], BF16); B_Yt = Buf()
                pX = [ps(t0, f"pX{i}", [64, 512]) for i in range(2)]; B_pX = [Buf(), Buf()]
                pY = ps(t0, "pY", [128, 512]); B_pY = Buf()
                pYt = ps(t0, "pYt", [128, 8, 128], BF16); B_pYt = Buf()
                XS = sb(t0, "XS", [64, 2, 288]); B_XS = Buf()
                base = sb(t0, "base", [64, 288]); sarg = sb(t0, "sarg", [64, 288]); B_tr = Buf(); B_tr2 = Buf()
                sargi = sb(t0, "sargi", [64, 288], mybir.dt.int32); sargf = sb(t0, "sargf", [64, 288])
                sn = sb(t0, "sn", [64, 288]); cs = sb(t0, "cs", [64, 288]); B_sc = Buf()
                RR = sb(t0, "RR", [64, 2, 288]); B_RR = Buf()
                q1 = sb(t0, "q1", [64, 288]); q2 = sb(t0, "q2", [64, 288]); B_q = Buf()
                SS = sb(t0, "SS", [64, 2, 288]); B_SS = Buf()
                Sp = sb(t0, "Sp", [64, 2, 2, 288], BF16); B_Sp = Buf()
                Ysb = sb(t0, "Ysb", [128, 288], BF16); B_Ysb = Buf()
                k.op("dve", lambda e: e.memset(Sp[:], 0.0), writes=[B_Sp])
                for g in range(32):
                    for d in range(2):
                        dg = d * 32 + g
                        for c2 in range(2):
                            k.op("pe", lambda e, c2=c2, d=d, g=g: e.matmul(pX[c2][:, 0:288], lhsT=WinT[:, d, g, c2 * 64:(c2 + 1) * 64], rhs=UT_all[:, g, :],
                                                                           start=True, stop=True), reads=[B_WinT, B_UT], writes=[B_pX[c2]])
                            if d == 0:
                                k.op("act", lambda e, c2=c2: e.copy(out=XS[:, c2, :], in_=pX[c2][:, 0:288]), reads=[B_pX[c2]], writes=[B_XS])
                            else:
                                k.op("act", lambda e, c2=c2: e.copy(out=XS[:, c2, 0:32], in_=pX[c2][:, 31::-1]), reads=[B_pX[c2]], writes=[B_XS])
                                k.op("act", lambda e, c2=c2: e.copy(out=XS[:, c2, 32:288], in_=pX[c2][:, 287:31:-1]), reads=[B_pX[c2]], writes=[B_XS])
                        k.op("dve", lambda e, dg=dg: e.tensor_scalar(out=base[:], in0=mrow[:], scalar1=th8[:, dg:dg + 1], scalar2=None, op0=ALU.mult),
                             reads=[B_mrow, B_th8], writes=[B_tr])
                        sincos(base[:], [64, 288], sn[:], cs[:], (sarg[:], sargi[:], sargf[:]), B_tr, B_sc, B_tr2)
                        dv = lambda fn: k.op("dve", fn, reads=[B_XS, B_sc, B_q, B_RR, B_SS, B_L], writes=[B_q, B_RR, B_SS])
                        dv(lambda e: e.tensor_tensor(out=q1[:], in0=cs[:], in1=XS[:, 0, :], op=ALU.mult))
                        dv(lambda e: e.tensor_tensor(out=q2[:], in0=sn[:], in1=XS[:, 1, :], op=ALU.mult))
                        dv(lambda e: e.tensor_tensor(out=q1[:], in0=q1[:], in1=q2[:], op=ALU.add))
                        dv(lambda e, dg=dg: e.tensor_tensor_scan(out=RR[:, 0, :], data0=MG[:, 15, dg:dg + 1].to_broadcast([64, 288]), data1=q1[:], initial=0.0, op0=ALU.mult, op1=ALU.add))
                        dv(lambda e: e.tensor_tensor(out=q1[:], in0=cs[:], in1=XS[:, 1, :], op=ALU.mult))
                        dv(lambda e: e.tensor_tensor(out=q2[:], in0=sn[:], in1=XS[:, 0, :], op=ALU.mult))
                        dv(lambda e: e.tensor_tensor(out=q1[:], in0=q1[:], in1=q2[:], op=ALU.subtract))
                        dv(lambda e, dg=dg: e.tensor_tensor_scan(out=RR[:, 1, :], data0=MG[:, 15, dg:dg + 1].to_broadcast([64, 288]), data1=q1[:], initial=0.0, op0=ALU.mult, op1=ALU.add))
                        dv(lambda e: e.tensor_tensor(out=q1[:], in0=cs[:], in1=RR[:, 0, :], op=ALU.mult))
                        dv(lambda e: e.tensor_tensor(out=q2[:], in0=sn[:], in1=RR[:, 1, :], op=ALU.mult))
                        dv(lambda e: e.tensor_tensor(out=SS[:, 0, :], in0=q1[:], in1=q2[:], op=ALU.subtract))
                        dv(lambda e: e.tensor_tensor(out=q1[:], in0=cs[:], in1=RR[:, 1, :], op=ALU.mult))
                        dv(lambda e: e.tensor_tensor(out=q2[:], in0=sn[:], in1=RR[:, 0, :], op=ALU.mult))
                        dv(lambda e: e.tensor_tensor(out=SS[:, 1, :], in0=q1[:], in1=q2[:], op=ALU.add))
                        for c2 in range(2):
                            if d == 0:
                                k.op("act", lambda e, c2=c2: e.copy(out=Sp[:, 0, c2, 1:288], in_=SS[:, c2, 0:287]), reads=[B_SS], writes=[B_Sp])
                            else:
                                k.op("act", lambda e, c2=c2: e.copy(out=Sp[:, 1, c2, 0:31], in_=SS[:, c2, 30::-1]), reads=[B_SS], writes=[B_Sp])
                                k.op("act", lambda e, c2=c2: e.copy(out=Sp[:, 1, c2, 32:288], in_=SS[:, c2, 286:30:-1]), reads=[B_SS], writes=[B_Sp])
                    k.op("pe", lambda e, g=g: e.matmul(pY[:, 0:288], lhsT=T_all[:, g, :], rhs=UT_all[:, g, :], start=True, stop=False),
                         reads=[B_T, B_UT], writes=[B_pY])
                    for d in range(2):
                        k.op("pe", lambda e, g=g, d=d: e.matmul(pY[:, 0:288], lhsT=COr[:, d, g, :], rhs=Sp[:, d, 0, :], start=False, stop=False),
                             reads=[B_CO, B_Sp], writes=[B_pY])
                        k.op("pe", lambda e, g=g, d=d: e.matmul(pY[:, 0:288], lhsT=COi[:, d, g, :], rhs=Sp[:, d, 1, :], start=False, stop=(d == 1)),
                             reads=[B_CO, B_Sp], writes=[B_pY])
                    k.op("act", lambda e: e.copy(out=Ysb[:], in_=pY[:, 0:288]), reads=[B_pY], writes=[B_Ysb])
                    for bi, (n0, nb) in enumerate(NB):
                        k.op("pe", lambda e, bi=bi, n0=n0, nb=nb: e.transpose(out=pYt[0:nb, bi, :], in_=Ysb[:, n0:n0 + nb], identity=ident_b[:]),
                             reads=[B_Ysb, B_ident_b], writes=[B_pYt])
                    for bi, (n0, nb) in enumerate(NB):
                        dst = Yt[0:nb, bi, :].rearrange("p (j f) -> p j f", j=8)[:, :, g * 16:(g + 1) * 16]
                        k.op("dve", lambda e, bi=bi, nb=nb, dst=dst: e.tensor_copy(out=dst, in_=pYt[0:nb, bi, :].rearrange("p (j c) -> p j c", j=8)),
                             reads=[B_pYt], writes=[B_Yt])
                y8v = y_scr.rearrange("(n j) f -> n (j f)", j=8)
                for bi, (n0, nb) in enumerate(NB):
                    k.dma("sp", y8v[n0:n0 + nb, :], Yt[0:nb, bi, :], reads=[B_Yt])
                k.barrier()


        def ssm_post(st, catT_, B_catT_):
            GC = 2.0 * math.sqrt(2.0 / math.pi)
            gw = sb(st, "gluw", [128, 4, 512], BF16); B_gw = Buf()
            k.dma("pool", gw[:], glu_w.rearrange("(k p) n -> p k n", p=128), writes=[B_gw])
            gb = sb(st, "glub", [128, 4]); B_gb = Buf()
            with nc.allow_non_contiguous_dma(reason="tiny bias"):
                k.dma("sp", gb[:], glu_b[0, :].rearrange("(c p) -> p c", p=128), writes=[B_gb])
            dbc = load_bc(st, "dskip", ssm_d[0:1, :], 512)
            yt = [sb(st, f"py{i}", [128, 512], BF16) for i in range(2)]; B_yt = [Buf(), Buf()]
            ut = [sb(st, f"pu{i}", [128, 512]) for i in range(2)]; B_ut = [Buf(), Buf()]
            xx = sb(st, "pxx", [128, 512]); B_xx = Buf()
            ww = sb(st, "pww", [128, 512]); B_ww = Buf()
            sg = sb(st, "psg", [128, 512]); B_sg = Buf()
            g_bf = sb(st, "pg_bf", [128, 512], BF16); B_g = Buf()
            gT = sb(st, "pgT", [128, 4, 128], BF16); B_gT = Buf()
            s2 = sb(st, "ps2", [128, 4, 128]); B_s2 = Buf()
            pGT = ps(st, "pGT", [128, 8, 128], BF16); B_pGT = Buf()
            pz = ps(st, "pz", [128, 4, 128]); B_pz = Buf()
            for ti in range(NTILE):
                i = ti % 2
                T0 = ti * 128
                row0 = T0 + CTX if ti < NLT else T0 - SEQ
                k.dma("sp", yt[i][:], y_scr[row0:row0 + 128, :], writes=[B_yt[i]])
                k.dma("sp", ut[i][:], u_scr[row0:row0 + 128, :], writes=[B_ut[i]])
                k.op("dve", lambda e, i=i: e.tensor_tensor(out=xx[:], in0=ut[i][:], in1=dbc[0][:], op=ALU.mult), reads=[B_ut[i], dbc[1]], writes=[B_xx])
                k.op("dve", lambda e, i=i: e.tensor_tensor(out=xx[:], in0=xx[:], in1=yt[i][:], op=ALU.add), reads=[B_xx, B_yt[i]], writes=[B_xx])
                k.op("pool", lambda e: e.tensor_tensor(out=ww[:], in0=xx[:], in1=xx[:], op=ALU.mult), reads=[B_xx], writes=[B_ww])
                k.op("pool", lambda e: e.tensor_scalar(out=ww[:], in0=ww[:], scalar1=0.044715, scalar2=1.0, op0=ALU.mult, op1=ALU.add), reads=[B_ww], writes=[B_ww])
                k.op("pool", lambda e: e.tensor_tensor(out=ww[:], in0=ww[:], in1=xx[:], op=ALU.mult), reads=[B_ww, B_xx], writes=[B_ww])
                k.op("act", lambda e: e.activation(out=sg[:], in_=ww[:], func=AF.Sigmoid, scale=GC), reads=[B_ww], writes=[B_sg])
                k.op("dve", lambda e: e.tensor_tensor(out=g_bf[:], in0=xx[:], in1=sg[:], op=ALU.mult), reads=[B_xx, B_sg], writes=[B_g])
                if "dbg_g" in dbg:
                    if ti == 0:
                        dbg_g = dscr("dbg_g", [NT, 512], BF16)
                    k.dma("sp", dbg_g[T0:T0 + 128, :], g_bf[:], reads=[B_g])
                for kc in range(4):
                    k.op("pe", lambda e, kc=kc: e.transpose(out=pGT[:, kc, :], in_=g_bf[:, kc * 128:(kc + 1) * 128], identity=ident_b[:]),
                         reads=[B_g, B_ident_b], writes=[B_pGT])
                k.op("act", lambda e: e.copy(out=gT[:], in_=pGT[:, 0:4, :]), reads=[B_pGT], writes=[B_gT])
                for n_ in range(4):
                    for kc in range(4):
                        k.op("pe", lambda e, n_=n_, kc=kc: e.matmul(pz[:, n_, :], lhsT=gw[:, kc, n_ * 128:(n_ + 1) * 128], rhs=gT[:, kc, :],
                                                                    start=(kc == 0), stop=(kc == 3)), reads=[B_gw, B_gT], writes=[B_pz])
                for n_ in range(4):
                    k.op("act", lambda e, n_=n_: e.activation(out=s2[:, n_, :], in_=pz[:, n_, :], func=AF.Sigmoid, bias=gb[:, n_:n_ + 1], scale=1.0),
                         reads=[B_pz, B_gb], writes=[B_s2])
                k.op("dve", lambda e, T0=T0: e.tensor_tensor(out=catT_[:, 4:8, T0:T0 + 128], in0=gT[:], in1=s2[:], op=ALU.mult),
                     reads=[B_gT, B_s2], writes=[B_catT_])
            if "dbg_cat" in dbg:
                dbg_cat = dscr("dbg_cat", [128, 8, NT], BF16)
                k.dma("sp", dbg_cat, catT_[:], reads=[B_catT_])
            k.barrier()


        def layer1_mixer():
            LAM_INIT = 0.8 - 0.6 * math.exp(-0.3 * 1)
            SC = 0.125
            with ExitStack() as L1:
                qT2 = sb(L1, "qT2", [128, 8, SEQ], BF16); B_q2 = Buf()
                kT2 = sb(L1, "kT2", [128, 8, NT], BF16); B_k2 = Buf()
                v1 = sb(L1, "v1", [128, NTILE, D], BF16); B_v1 = Buf()
                for st in phase("l1proj"):
                    w_bf = sb(st, "dif_w_bf", [128, 8, 3072], BF16); B_w = Buf()
                    for kc in range(8):
                        k.dma("pool", w_bf[:, kc, :], dif_w_in[kc * 128:(kc + 1) * 128, :], writes=[B_w])
                    sc1p = [mod_bc(st, f"l1sc1p_{r}", 1, r, 1, plus1=True) for r in range(2)]
                    sh1 = [mod_bc(st, f"l1sh1_{r}", 1, r, 0) for r in range(2)]
                    xt = [sb(st, f"l1xt{i}", [128, D]) for i in range(2)]; B_xt = [Buf(), Buf()]
                    tmpf = sb(st, "l1tmpf", [128, D]); B_tmpf = Buf()
                    h_bf = sb(st, "l1h_bf", [128, D], BF16); B_hbf = Buf()
                    hT = [sb(st, f"l1hT{i}", [128, 8, 128], BF16) for i in range(2)]; B_hT = [Buf(), Buf()]
                    rt = [sb(st, f"l1rt{i}", [128, 64]) for i in range(2)]; B_rt = [Buf(), Buf()]
                    t1 = sb(st, "l1rope_t1", [128, 512]); t2 = sb(st, "l1rope_t2", [128, 512]); B_rtmp = Buf()
                    qk_bf = sb(st, "l1qk_bf", [128, 512], BF16); B_qk = Buf()
                    pT = ps(st, "l1pT", [128, 8, 128], BF16); B_pT = Buf()
                    pp = [ps(st, f"l1pp{i}", [128, 512]) for i in range(3)]; B_pp = [Buf(), Buf(), Buf()]
                    pq = [ps(st, f"l1pq{i}", [128, 8, 128], BF16) for i in range(2)]; B_pq = [Buf(), Buf()]
                    ib = 0
                    for ti in range(NTILE):
                        i = ti % 2
                        r = 0 if ti < NLT else 1
                        T0 = ti * 128
                        k.dma("sp", xt[i][:], src_rows(ti, 1), writes=[B_xt[i]])
                        if r == 0:
                            k.dma("sp", rt[i][:], rope_cs[T0:T0 + 128, :], writes=[B_rt[i]])
                        k.op("dve", lambda e, i=i, r=r: e.tensor_tensor(out=tmpf[:], in0=xt[i][:], in1=sc1p[r][0][:], op=ALU.mult),
                             reads=[B_xt[i], sc1p[r][1]], writes=[B_tmpf])
                        k.op("pool", lambda e, r=r: e.tensor_tensor(out=h_bf[:], in0=tmpf[:], in1=sh1[r][0][:], op=ALU.add),
                             reads=[B_tmpf, sh1[r][1]], writes=[B_hbf])
                        for kc in range(8):
                            k.op("pe", lambda e, kc=kc: e.transpose(out=pT[:, kc, :], in_=h_bf[:, kc * 128:(kc + 1) * 128], identity=ident_b[:]),
                                 reads=[B_hbf, B_ident_b], writes=[B_pT])
                        k.op("act", lambda e, i=i: e.copy(out=hT[i][:], in_=pT[:]), reads=[B_pT], writes=[B_hT[i]])
                        for cb in range(6):
                            if r == 1 and cb < 2:
                                continue
                            j = ib % 3; ib += 1
                            for kc in range(8):
                                k.op("pe", lambda e, kc=kc, j=j, cb=cb, i=i: e.matmul(
                                    pp[j][:], lhsT=hT[i][:, kc, :], rhs=w_bf[:, kc, cb * 512:(cb + 1) * 512],
                                    start=(kc == 0), stop=(kc == 7)), reads=[B_hT[i], B_w], writes=[B_pp[j]])
                            if cb >= 4:
                                c0 = (cb - 4) * 512
                                k.op("act", lambda e, j=j, ti=ti, c0=c0: e.copy(out=v1[:, ti, c0:c0 + 512], in_=pp[j][:]), reads=[B_pp[j]], writes=[B_v1])
                                continue
                            if r == 0:
                                rope_apply(None, pp[j][:], B_pp[j], qk_bf[:], B_qk, 8, rt[i], B_rt[i], t1[:], t2[:], B_rtmp)
                            else:
                                k.op("dve", lambda e, j=j: e.tensor_copy(out=qk_bf[:], in_=pp[j][:]), reads=[B_pp[j]], writes=[B_qk])
                            jq = cb % 2
                            for hh in range(4):
                                k.op("pe", lambda e, hh=hh, jq=jq: e.transpose(out=pq[jq][:, hh, :], in_=qk_bf[:, hh * 128:(hh + 1) * 128], identity=ident_b[:]),
                                     reads=[B_qk, B_ident_b], writes=[B_pq[jq]])
                            dstT, BD = (qT2, B_q2) if cb < 2 else (kT2, B_k2)
                            h0 = (cb % 2) * 4
                            k.op("act", lambda e, jq=jq, dstT=dstT, h0=h0, T0=T0: e.copy(out=dstT[:, h0:h0 + 4, T0:T0 + 128], in_=pq[jq][:, 0:4, :]),
                                 reads=[B_pq[jq]], writes=[BD])
                    k.barrier()
                for st in phase("l1att"):
                    w_bf = sb(st, "difwo_bf", [128, 8, D], BF16); B_w = Buf()
                    for kc in range(8):
                        k.dma("pool", w_bf[:, kc, :], dif_w_out[kc * 128:(kc + 1) * 128, :], writes=[B_w])
                    g1 = mod_bc(st, "l1g1", 1, 0, 2)
                    lng = load_bc(st, "l1ln1g", ln1_g[1:2, :], D)
                    lnb = load_bc(st, "l1ln1b", ln1_b[1:2, :], D)
                    wk = ln_work(st, "l1e1")
                    xo = [sb(st, f"l1xo{i}", [128, D]) for i in range(2)]; B_xo = [Buf(), Buf()]
                    lam = sb(st, "lam", [128, 8]); B_lam = Buf()
                    lq = [load_bc(st, f"lq{i}", a[0:1, :], 64) for i, a in enumerate((lam_q1, lam_k1, lam_q2, lam_k2))]
                    ltmp = sb(st, "ltmp", [128, 64]); B_lt = Buf()
                    for i2 in range(2):
                        k.op("dve", lambda e, i2=i2: e.tensor_tensor(out=ltmp[:], in0=lq[2 * i2][0][:], in1=lq[2 * i2 + 1][0][:], op=ALU.mult),
                             reads=[lq[2 * i2][1], lq[2 * i2 + 1][1], B_lt], writes=[B_lt])
                        k.op("dve", lambda e, i2=i2: e.reduce_sum(out=lam[:, i2:i2 + 1], in_=ltmp[:], axis=AX.X), reads=[B_lt, B_lam], writes=[B_lam])
                    k.op("act", lambda e: e.activation(out=lam[:, 2:4], in_=lam[:, 0:2], func=AF.Exp), reads=[B_lam], writes=[B_lam])
                    k.op("dve", lambda e: e.tensor_tensor(out=lam[:, 4:5], in0=lam[:, 2:3], in1=lam[:, 3:4], op=ALU.subtract), reads=[B_lam], writes=[B_lam])
                    k.op("dve", lambda e: e.tensor_scalar(out=lam[:, 5:6], in0=lam[:, 4:5], scalar1=LAM_INIT, scalar2=None, op0=ALU.add), reads=[B_lam], writes=[B_lam])
                    sg_bc = load_bc(st, "subg", subln_g[0:1, :], 128)
                    k.op("dve", lambda e: e.tensor_scalar(out=sg_bc[0][:], in0=sg_bc[0][:], scalar1=1.0 - LAM_INIT, scalar2=None, op0=ALU.mult),
                         reads=[sg_bc[1]], writes=[sg_bc[1]])
                    neglam = sb(st, "neglam", [128, 1]); B_nl = Buf()
                    k.op("dve", lambda e: e.tensor_scalar(out=neglam[:], in0=lam[:, 5:6], scalar1=-1.0, scalar2=None, op0=ALU.mult), reads=[B_lam], writes=[B_nl])
                    UN = ((0, 1024, 0, 8), (1024, 1280, 8, 10))
                    E = [sb(st, f"E{i}", [128, 1280], BF16) for i in range(2)]; B_E = [Buf(), Buf()]
                    PTt = [sb(st, f"PTt{i}", [128, 10, 128], BF16) for i in range(2)]; B_PT = [Buf(), Buf()]
                    stts = [sb(st, f"dstat{i}", [128, 32]) for i in range(2)]; B_sts = [Buf(), Buf()]
                    ao = sb(st, "ao", [128, D], BF16); B_ao = Buf()
                    aoT = sb(st, "aoT", [128, 8, 128], BF16); B_aoT = Buf()
                    osq = sb(st, "osq", [128, 128]); B_osq = Buf()
                    oacc = sb(st, "oacc", [128, 128]); B_oacc = Buf()
                    bufA = ps(st, "bufA", [128, 1024]); bufB = ps(st, "bufB", [128, 1536]); B_buf = [Buf(), Buf()]
                    bufs = [bufA, bufB]
                    pPT = [ps(st, f"dpPT{i}", [128, 8, 128], BF16) for i in range(2)]; B_pPT = [Buf(), Buf()]
                    po = [ps(st, f"dpo{i}", [128, 512]) for i in range(1)]; B_po = [Buf()]
                    cnt = {"pt": 0}

                    def qk_unit(T0, h, c, hf, stt, B_st):
                        k0, nk, _, _ = UN[hf]
                        ps_ = slice(c * 64, (c + 1) * 64)
                        u = c * 2 + hf
                        for b0 in range(0, nk, 512):
                            bn = min(512, nk - b0)
                            k.op("pe", lambda e, b0=b0, bn=bn: e.matmul(bufs[hf][:, b0:b0 + bn], lhsT=qT2[ps_, h, T0:T0 + 128],
                                                                      rhs=kT2[ps_, h, k0 + b0:k0 + b0 + bn], start=True, stop=True),
                                 reads=[B_q2, B_k2], writes=[B_buf[hf]])
                        k.op("dve", lambda e: e.reduce_max(out=stt[:, u:u + 1], in_=bufs[hf][:, 0:nk], axis=AX.X), reads=[B_buf[hf], B_st], writes=[B_st])
                        k.op("dve", lambda e: e.tensor_scalar(out=stt[:, 8 + u:9 + u], in0=stt[:, u:u + 1], scalar1=-SC, scalar2=None, op0=ALU.mult),
                             reads=[B_st], writes=[B_st])
                        k.op("act", lambda e: e.activation(out=E[hf][:, 0:nk], in_=bufs[hf][:, 0:nk], func=AF.Exp, bias=stt[:, 8 + u:9 + u], scale=SC,
                                                           accum_out=stt[:, 4 + u:5 + u]), reads=[B_buf[hf], B_st], writes=[B_E[hf], B_st])

                    def tpv_unit(h, c, hf):
                        _, nk, t0_, nt_ = UN[hf]
                        u = c * 2 + hf
                        groups = [(0, min(8, nt_))] + ([(8, nt_ - 8)] if nt_ > 8 else [])
                        for (g0, gn) in groups:
                            jb = cnt["pt"] % 2; cnt["pt"] += 1
                            for b in range(g0, g0 + gn):
                                k.op("pe", lambda e, b=b, jb=jb, g0=g0: e.transpose(out=pPT[jb][:, b - g0, :], in_=E[hf][:, b * 128:(b + 1) * 128], identity=ident_b[:]),
                                     reads=[B_E[hf], B_ident_b], writes=[B_pPT[jb]])
                            if False:
                                pass
                            else:
                                k.op("act", lambda e, jb=jb, g0=g0, gn=gn: e.copy(out=PTt[hf][:, g0:g0 + gn, :], in_=pPT[jb][:, 0:gn, :]),
                                     reads=[B_pPT[jb]], writes=[B_PT[hf]])
                        for b in range(nt_):
                            k.op("pe", lambda e, b=b: e.matmul(po[0][:, u * 128:(u + 1) * 128], lhsT=PTt[hf][:, b, :], rhs=v1[:, t0_ + b, h * 128:(h + 1) * 128],
                                                               start=(b == 0), stop=(b == nt_ - 1)), reads=[B_PT[hf], B_v1], writes=[B_po[0]])

                    def combine(h, stt, B_st):
                        dv = lambda fn, extra=(): k.op("dve", fn, reads=[B_st, B_nl] + list(extra), writes=[B_st])
                        m4 = stt[:, 0:4]; z4 = stt[:, 4:8]
                        dv(lambda e: e.tensor_tensor(out=stt[:, 12:14], in0=stt[:, 0:4:2], in1=stt[:, 1:4:2], op=ALU.max))
                        Mb = stt[:, 12:14].rearrange("p (c x) -> p c x", x=1).to_broadcast([128, 2, 2])
                        dv(lambda e: e.tensor_tensor(out=stt[:, 14:18].rearrange("p (c x) -> p c x", x=2), in0=m4.rearrange("p (c x) -> p c x", x=2), in1=Mb, op=ALU.subtract))
                        k.op("act", lambda e: e.activation(out=stt[:, 18:22], in_=stt[:, 14:18], func=AF.Exp, scale=SC), reads=[B_st], writes=[B_st])
                        dv(lambda e: e.tensor_tensor(out=stt[:, 22:26], in0=stt[:, 18:22], in1=z4, op=ALU.mult))
                        dv(lambda e: e.tensor_tensor(out=stt[:, 26:28], in0=stt[:, 22:26:2], in1=stt[:, 23:26:2], op=ALU.add))
                        dv(lambda e: e.reciprocal(out=stt[:, 26:28], in_=stt[:, 26:28]))
                        rZb = stt[:, 26:28].rearrange("p (c x) -> p c x", x=1).to_broadcast([128, 2, 2])
                        dv(lambda e: e.tensor_tensor(out=stt[:, 28:32].rearrange("p (c x) -> p c x", x=2), in0=stt[:, 18:22].rearrange("p (c x) -> p c x", x=2), in1=rZb, op=ALU.mult))
                        dv(lambda e: e.tensor_scalar(out=stt[:, 30:32], in0=stt[:, 30:32], scalar1=neglam[:, 0:1], scalar2=None, op0=ALU.mult))
                        k.op("dve", lambda e: e.tensor_scalar(out=oacc[:], in0=po[0][:, 0:128], scalar1=stt[:, 28:29], scalar2=None, op0=ALU.mult),
                             reads=[B_po[0], B_st, B_osq], writes=[B_oacc])
                        for u in range(1, 4):
                            k.op("dve", lambda e, u=u: e.scalar_tensor_tensor(out=oacc[:], in0=po[0][:, u * 128:(u + 1) * 128], scalar=stt[:, 28 + u:29 + u], in1=oacc[:],
                                                                              op0=ALU.mult, op1=ALU.add), reads=[B_po[0], B_st, B_oacc], writes=[B_oacc])
                        k.op("act", lambda e: e.activation(out=osq[:], in_=oacc[:], func=AF.Square, accum_out=stt[:, 9 + 0:10]), reads=[B_oacc, B_st], writes=[B_osq, B_st])
                        dv(lambda e: e.tensor_scalar(out=stt[:, 10:11], in0=stt[:, 9:10], scalar1=1.0 / 128.0, scalar2=1e-5, op0=ALU.mult, op1=ALU.add))
                        k.op("act", lambda e: e.sqrt(out=stt[:, 10:11], in_=stt[:, 10:11]), reads=[B_st], writes=[B_st])
                        dv(lambda e: e.reciprocal(out=stt[:, 11:12], in_=stt[:, 10:11]))
                        k.op("dve", lambda e: e.scalar_tensor_tensor(out=ao[:, h * 128:(h + 1) * 128], in0=oacc[:], scalar=stt[:, 11:12], in1=sg_bc[0][:],
                                                                     op0=ALU.mult, op1=ALU.mult), reads=[B_oacc, B_st, sg_bc[1]], writes=[B_ao])

                    for ti in range(NLT):
                        T0 = ti * 128
                        i = ti % 2
                        k.dma("sp", xo[i][:], src_rows(ti, 1), writes=[B_xo[i]])
                        units = []
                        for h in range(8):
                            for c in range(2):
                                for hf in range(2):
                                    units.append((h, c, hf, (c == 1 and hf == 1), stts[h % 2], B_sts[h % 2]))
                        qk_unit(T0, units[0][0], units[0][1], units[0][2], units[0][4], units[0][5])
                        for idx, un in enumerate(units):
                            if idx + 1 < len(units):
                                nx = units[idx + 1]
                                qk_unit(T0, nx[0], nx[1], nx[2], nx[4], nx[5])
                            tpv_unit(*un[:3])
                            if un[3]:
                                combine(un[0], un[4], un[5])
                        if "dbg_ao" in dbg:
                            if ti == 0:
                                dbg_ao = dscr("dbg_ao", [SEQ, D], BF16)
                            k.dma("sp", dbg_ao[T0:T0 + 128, :], ao[:], reads=[B_ao])
                        for kc in range(8):
                            k.op("pe", lambda e, kc=kc: e.transpose(out=pPT[0][:, kc, :], in_=ao[:, kc * 128:(kc + 1) * 128], identity=ident_b[:]),
                                 reads=[B_ao, B_ident_b], writes=[B_pPT[0]])
                        k.op("act", lambda e: e.copy(out=aoT[:], in_=pPT[0][:]), reads=[B_pPT[0]], writes=[B_aoT])
                        for hf in range(2):
                            for kc in range(8):
                                k.op("pe", lambda e, kc=kc, hf=hf: e.matmul(bufA[:, hf * 512:(hf + 1) * 512], lhsT=aoT[:, kc, :], rhs=w_bf[:, kc, hf * 512:(hf + 1) * 512],
                                                                          start=(kc == 0), stop=(kc == 7)), reads=[B_aoT, B_w], writes=[B_buf[0]])
                        ln_epilogue(wk, [bufA[:, 0:512], bufA[:, 512:1024]], [B_buf[0], B_buf[0]], xo[i], B_xo[i], g1, lng, lnb, x1_scr[T0:T0 + 128, :])
                    k.barrier()

        with ExitStack() as L0:
            catT = sb(L0, "catT", [128, 8, NT], BF16); B_catT = Buf()
            LA = ExitStack()
            qT = sb(LA, "qT", [64, 8, NT], BF16); B_qT = Buf()
            kT = sb(LA, "kT", [64, 2, NT], BF16); B_kT = Buf()
            v_all = sb(LA, "v_all", [128, NTILE, 128], BF16); B_v = Buf()
            for st in phase("l0proj"):
                w_bf = sb(st, "w_in_bf", [128, 8, 1280], BF16); B_w = Buf()
                for kc in range(8):
                    k.dma("pool", w_bf[:, kc, :], w_in0[kc * 128:(kc + 1) * 128, :], writes=[B_w])
                sc1p = [None, None]; sh1 = [None, None]
                for r in range(2):
                    sh1[r] = mod_bc(st, f"sh1_{r}", 0, r, 0)
                    sc1p[r] = mod_bc(st, f"sc1p_{r}", 0, r, 1, plus1=True)
                xt = [sb(st, f"xt{i}", [128, D]) for i in range(2)]; B_xt = [Buf(), Buf()]
                tmpf = sb(st, "tmpf", [128, D]); B_tmpf = Buf()
                h_bf = sb(st, "h_bf", [128, D], BF16); B_hbf = Buf()
                hT = [sb(st, f"hT{i}", [128, 8, 128], BF16) for i in range(2)]; B_hT = [Buf(), Buf()]
                rt = [sb(st, f"rt{i}", [128, 64]) for i in range(2)]; B_rt = [Buf(), Buf()]
                t1 = sb(st, "rope_t1", [128, 640]); t2 = sb(st, "rope_t2", [128, 640]); B_rtmp = Buf()
                qk_bf = sb(st, "qk_bf", [128, 640], BF16); B_qk = Buf()
                ut = [sb(st, f"ut{i}", [128, 512]) for i in range(2)]; B_ut = [Buf(), Buf()]
                pT = ps(st, "pT", [128, 8, 128], BF16); B_pT = Buf()
                pp = [ps(st, f"pp{i}", [128, 512]) for i in range(3)]; B_pp = [Buf(), Buf(), Buf()]
                pq = ps(st, "pq", [64, 8, 128], BF16); B_pq = Buf()
                pk = ps(st, "pk", [64, 8, 128], BF16); B_pk = Buf()
                for ti in range(NTILE):
                    i = ti % 2
                    r = 0 if ti < NLT else 1
                    T0 = ti * 128
                    k.dma("sp", xt[i][:], src_rows(ti, 0), writes=[B_xt[i]])
                    if r == 0:
                        k.dma("sp", rt[i][:], rope_cs[T0:T0 + 128, :], writes=[B_rt[i]])
                    k.op("dve", lambda e, i=i, r=r: e.tensor_tensor(out=tmpf[:], in0=xt[i][:], in1=sc1p[r][0][:], op=ALU.mult),
                         reads=[B_xt[i], sc1p[r][1]], writes=[B_tmpf])
                    k.op("pool", lambda e, r=r: e.tensor_tensor(out=h_bf[:], in0=tmpf[:], in1=sh1[r][0][:], op=ALU.add),
                         reads=[B_tmpf, sh1[r][1]], writes=[B_hbf])
                    for kc in range(8):
                        k.op("pe", lambda e, kc=kc: e.transpose(out=pT[:, kc, :], in_=h_bf[:, kc * 128:(kc + 1) * 128], identity=ident_b[:]),
                             reads=[B_hbf, B_ident_b], writes=[B_pT])
                    k.op("act", lambda e, i=i: e.copy(out=hT[i][:], in_=pT[:]), reads=[B_pT], writes=[B_hT[i]])
                    for nb, (c0, c1) in enumerate(((0, 512), (512, 1024), (1024, 1280))):
                        for kc in range(8):
                            k.op("pe", lambda e, kc=kc, nb=nb, c0=c0, c1=c1, i=i: e.matmul(
                                pp[nb][:, 0:c1 - c0], lhsT=hT[i][:, kc, :], rhs=w_bf[:, kc, c0:c1],
                                start=(kc == 0), stop=(kc == 7)),
                                reads=[B_hT[i], B_w], writes=[B_pp[nb]])
                    if "dbg_q" in dbg:
                        if ti == 0:
                            dq = sb(st, "dq", [128, 1280]); B_dq = Buf()
                        for nb, (c0, c1) in enumerate(((0, 512), (512, 1024), (1024, 1280))):
                            k.op("dve", lambda e, nb=nb, c0=c0, c1=c1: e.tensor_copy(out=dq[:, c0:c1], in_=pp[nb][:, 0:c1 - c0]),
                                 reads=[B_pp[nb]], writes=[B_dq])
                        k.dma("sp", dbg_q[T0:T0 + 128, :], dq[:], reads=[B_dq])
                    if r == 0:
                        rope_apply(None, pp[0][:, 0:512], B_pp[0], qk_bf[:, 0:512], B_qk, 8, rt[i], B_rt[i], t1[:, 0:512], t2[:, 0:512], B_rtmp)
                        rope_apply(None, pp[1][:, 0:128], B_pp[1], qk_bf[:, 512:640], B_qk, 2, rt[i], B_rt[i], t1[:, 512:640], t2[:, 512:640], B_rtmp)
                    else:
                        k.op("dve", lambda e: e.tensor_copy(out=qk_bf[:, 0:512], in_=pp[0][:, 0:512]), reads=[B_pp[0]], writes=[B_qk])
                        k.op("dve", lambda e: e.tensor_copy(out=qk_bf[:, 512:640], in_=pp[1][:, 0:128]), reads=[B_pp[1]], writes=[B_qk])
                    for h in range(8):
                        k.op("pe", lambda e, h=h: e.transpose(out=pq[:, h, :], in_=qk_bf[:, h * 64:(h + 1) * 64], identity=ident_b[:]),
                             reads=[B_qk, B_ident_b], writes=[B_pq])
                    for h in range(2):
                        k.op("pe", lambda e, h=h: e.transpose(out=pk[:, h, :], in_=qk_bf[:, 512 + h * 64:512 + (h + 1) * 64], identity=ident_b[:]),
                             reads=[B_qk, B_ident_b], writes=[B_pk])
                    k.op("act", lambda e, T0=T0: e.copy(out=qT[:, :, T0:T0 + 128], in_=pq[:]), reads=[B_pq], writes=[B_qT])
                    k.op("act", lambda e, T0=T0: e.copy(out=kT[:, :, T0:T0 + 128], in_=pk[:, 0:2, :]), reads=[B_pk], writes=[B_kT])
                    k.op("act", lambda e, ti=ti: e.copy(out=v_all[:, ti, :], in_=pp[1][:, 128:256]), reads=[B_pp[1]], writes=[B_v])
                    k.op("act", lambda e, i=i: e.copy(out=ut[i][:, 0:256], in_=pp[1][:, 256:512]), reads=[B_pp[1]], writes=[B_ut[i]])
                    k.op("act", lambda e, i=i: e.copy(out=ut[i][:, 256:512], in_=pp[2][:, 0:256]), reads=[B_pp[2]], writes=[B_ut[i]])
                    urow = T0 + CTX if r == 0 else T0 - SEQ
                    k.dma("sp", u_scr[urow:urow + 128, :], ut[i][:], reads=[B_ut[i]])
                if "dbg_qT" in dbg:
                    dbg_qT = dscr("dbg_qT", [64, 8, NT], BF16)
                    k.dma("sp", dbg_qT, qT[:], reads=[B_qT])
                k.barrier()


            for st in phase("l0att"):
                SC = 0.125
                maskL = sb(st, "maskL_sb", [128, 128]); maskR = sb(st, "maskR_sb", [128, 128]); B_mask = Buf()
                k.dma("sp", maskL[:], maskL_in[:, :], writes=[B_mask])
                k.dma("sp", maskR[:], maskR_in[:, :], writes=[B_mask])
                sink_bc, B_sink = load_bc(st, "sink_bc", swa_sink[0:1, :], 8)
                sm = [sb(st, f"sm{i}", [128, 640]) for i in range(2)]; B_sm = [Buf(), Buf()]
                P = [sb(st, f"P{i}", [128, 640], BF16) for i in range(2)]; B_P = [Buf(), Buf()]
                PT = [sb(st, f"PT{i}", [128, 5, 128], BF16) for i in range(2)]; B_PT = [Buf(), Buf()]
                stat = [sb(st, f"stat{i}", [128, 8]) for i in range(2)]; B_stat = [Buf(), Buf()]
                att_bf = sb(st, "att_bf", [128, 512], BF16); B_att = Buf()
                ps_loc = [ps(st, f"ps_loc{i}", [128, 512]) for i in range(2)]; B_psl = [Buf(), Buf()]
                ps_ctx = [ps(st, f"ps_ctx{i}", [128, 512]) for i in range(2)]; B_psc = [Buf(), Buf()]
                pPT = ps(st, "pPT", [128, 8, 128], BF16); B_pPT = Buf()
                po = ps(st, "po", [128, 512]); B_po = Buf()
                pcat = ps(st, "pcat", [128, 8, 128], BF16); B_pcat = Buf()
                it = 0
                for ti in range(NTILE):
                    T0 = ti * 128
                    lat = ti < NLT
                    if lat:
                        j0 = max(0, ti - 1); j1 = min(NLT - 1, ti + 1)
                        nloc = (j1 - j0 + 1) * 128
                        blocks = list(range(j0, j1 + 1)) + [NLT, NLT + 1]
                    else:
                        nloc = 0
                        blocks = [NLT, NLT + 1]
                    n = nloc + 256
                    for h in range(8):
                        i = it % 2; it += 1
                        kvh = h // 4
                        if lat:
                            k.op("pe", lambda e, i=i, h=h, kvh=kvh, j0=j0, nloc=nloc, T0=T0: e.matmul(
                                ps_loc[i][:, 0:nloc], lhsT=qT[:, h, T0:T0 + 128], rhs=kT[:, kvh, j0 * 128:j0 * 128 + nloc],
                                start=True, stop=True), reads=[B_qT, B_kT], writes=[B_psl[i]])
                        k.op("pe", lambda e, i=i, h=h, kvh=kvh, T0=T0: e.matmul(
                            ps_ctx[i][:, 0:256], lhsT=qT[:, h, T0:T0 + 128], rhs=kT[:, kvh, SEQ:NT],
                            start=True, stop=True), reads=[B_qT, B_kT], writes=[B_psc[i]])
                        if lat:
                            for bi, j in enumerate(range(j0, j1 + 1)):
                                sl = slice(bi * 128, (bi + 1) * 128)
                                if j == ti:
                                    k.op("act", lambda e, i=i, sl=sl: e.mul(out=sm[i][:, sl], in_=ps_loc[i][:, sl], mul=SC),
                                         reads=[B_psl[i]], writes=[B_sm[i]])
                                else:
                                    mk = maskL if j < ti else maskR
                                    k.op("dve", lambda e, i=i, sl=sl, mk=mk: e.scalar_tensor_tensor(
                                        out=sm[i][:, sl], in0=ps_loc[i][:, sl], scalar=SC, in1=mk[:], op0=ALU.mult, op1=ALU.add),
                                        reads=[B_psl[i], B_mask], writes=[B_sm[i]])
                        k.op("act", lambda e, i=i, nloc=nloc: e.mul(out=sm[i][:, nloc:nloc + 256], in_=ps_ctx[i][:, 0:256], mul=SC),
                             reads=[B_psc[i]], writes=[B_sm[i]])
                        sti = stat[i]
                        k.op("dve", lambda e, i=i, n=n, sti=sti: e.reduce_max(out=sti[:, 0:1], in_=sm[i][:, 0:n], axis=AX.X),
                             reads=[B_sm[i]], writes=[B_stat[i]])
                        k.op("dve", lambda e, sti=sti, h=h: e.tensor_tensor(out=sti[:, 1:2], in0=sti[:, 0:1], in1=sink_bc[:, h:h + 1], op=ALU.max),
                             reads=[B_stat[i], B_sink], writes=[B_stat[i]])
                        k.op("dve", lambda e, sti=sti: e.tensor_scalar(out=sti[:, 2:3], in0=sti[:, 1:2], scalar1=-1.0, scalar2=None, op0=ALU.mult),
                             reads=[B_stat[i]], writes=[B_stat[i]])
                        k.op("act", lambda e, i=i, n=n, sti=sti: e.activation(out=P[i][:, 0:n], in_=sm[i][:, 0:n], func=AF.Exp,
                                                                             bias=sti[:, 2:3], scale=1.0, accum_out=sti[:, 3:4]),
                             reads=[B_sm[i], B_stat[i]], writes=[B_P[i], B_stat[i]])
                        k.op("act", lambda e, sti=sti, h=h: e.activation(out=sti[:, 4:5], in_=sink_bc[:, h:h + 1], func=AF.Exp,
                                                                        bias=sti[:, 2:3], scale=1.0),
                             reads=[B_sink, B_stat[i]], writes=[B_stat[i]])
                        k.op("dve", lambda e, sti=sti: e.tensor_tensor(out=sti[:, 5:6], in0=sti[:, 3:4], in1=sti[:, 4:5], op=ALU.add),
                             reads=[B_stat[i]], writes=[B_stat[i]])
                        k.op("dve", lambda e, sti=sti: e.reciprocal(out=sti[:, 6:7], in_=sti[:, 5:6]),
                             reads=[B_stat[i]], writes=[B_stat[i]])
                        nb = n // 128
                        for b in range(nb):
                            k.op("pe", lambda e, i=i, b=b: e.transpose(out=pPT[:, b, :], in_=P[i][:, b * 128:(b + 1) * 128], identity=ident_b[:]),
                                 reads=[B_P[i], B_ident_b], writes=[B_pPT])
                        k.op("pool" if False else "dve", lambda e, i=i, nb=nb: e.tensor_copy(out=PT[i][:, 0:nb, :], in_=pPT[:, 0:nb, :]),
                             reads=[B_pPT], writes=[B_PT[i]])
                        for b in range(nb):
                            k.op("pe", lambda e, i=i, b=b, h=h, kvh=kvh, vb=blocks[b], nb=nb: e.matmul(
                                po[:, h * 64:(h + 1) * 64], lhsT=PT[i][:, b, :], rhs=v_all[:, vb, kvh * 64:(kvh + 1) * 64],
                                start=(b == 0), stop=(b == nb - 1)), reads=[B_PT[i], B_v], writes=[B_po])
                        k.op("dve", lambda e, h=h, sti=sti: e.tensor_scalar(out=att_bf[:, h * 64:(h + 1) * 64], in0=po[:, h * 64:(h + 1) * 64],
                                                                           scalar1=sti[:, 6:7], scalar2=None, op0=ALU.mult),
                             reads=[B_po, B_stat[i]], writes=[B_att])
                    for cb in range(4):
                        k.op("pe", lambda e, cb=cb: e.transpose(out=pcat[:, cb, :], in_=att_bf[:, cb * 128:(cb + 1) * 128], identity=ident_b[:]),
                             reads=[B_att, B_ident_b], writes=[B_pcat])
                    k.op("act", lambda e, T0=T0: e.copy(out=catT[:, 0:4, T0:T0 + 128], in_=pcat[:, 0:4, :]), reads=[B_pcat], writes=[B_catT])
                    if "dbg_att" in dbg:
                        if ti == 0:
                            dbg_att = dscr("dbg_att", [NT, 512], BF16)
                        k.dma("sp", dbg_att[T0:T0 + 128, :], att_bf[:], reads=[B_att])
                k.barrier()


            k.barrier()
            LA.close()
            for st in phase("l0ssm"):
                ssm_phase(st, catT, B_catT)
            for st in phase("l0ssmpost"):
                ssm_post(st, catT, B_catT)

            for st in phase("l0out"):
                outproj_ln1(st, 0, catT, B_catT, w_out0, NTILE)


        for st in phase("moe0"):
            moe_phase(st, 0, NTILE, x2_scr)


        layer1_mixer()
        for st in phase("moe1"):
            moe_phase(st, 1, NLT, out)

        k.barrier()
    return nc


_CONSTS = None


def _consts():
    global _CONSTS
    if _CONSTS is None:
        t = np.arange(SEQ)
        row = (t // 64).astype(np.float32)
        col = (t % 64).astype(np.float32)
        inv = (10000.0 ** (-np.arange(16, dtype=np.float32) / 16)).astype(np.float32)
        ar = row[:, None] * inv[None, :]
        ac = col[:, None] * inv[None, :]
        rope = np.concatenate([np.cos(ar), np.sin(ar), np.cos(ac), np.sin(ac)], 1).astype(np.float32)
        qi = np.arange(128)[:, None]; kj = np.arange(128)[None, :]
        mL = np.where(kj >= qi, 0.0, -30000.0).astype(np.float32)
        mR = np.where(kj <= qi, 0.0, -30000.0).astype(np.float32)
        _CONSTS = {"rope_cs": rope, "ident": np.eye(128, dtype=np.float32), "maskL": mL, "maskR": mR}
        selm = np.zeros((32, 32, 128), np.float32)
        for e_ in range(32):
            selm[e_, e_, :] = 1.0
        _CONSTS["sel"] = selm
        _CONSTS["kval"] = np.ascontiguousarray(np.broadcast_to(np.repeat(np.arange(-7, 9, dtype=np.float32), 64)[None, :], (64, 1024)))
        _CONSTS["mrow"] = np.ascontiguousarray(np.broadcast_to(np.arange(288, dtype=np.float32)[None, :], (64, 288)))
        jj = np.arange(128) // 16
        _CONSTS["maskF"] = (jj[None, :] >= jj[:, None]).astype(np.float32)
        _CONSTS["maskB"] = (jj[None, :] <= jj[:, None]).astype(np.float32)
    return _CONSTS


def make_in_maps(inputs, cores):
    f = lambda a: np.ascontiguousarray(np.asarray(a, dtype=np.float32))
    shared = {}
    for name in ("mod_w", "mod_b", "ln1_g", "ln1_b", "ln2_g", "ln2_b", "swa_sink",
                 "ssm_d", "ssm_glu_b", "dif_lam_q1", "dif_lam_k1", "dif_lam_q2", "dif_lam_k2",
                 "dif_subln_g", "moe_wg", "moe_bg", "moe_we", "moe_w1", "moe_w3", "moe_w2"):
        shared[name] = f(inputs[name])
    for name in ("swa_ssm_w_in", "swa_ssm_w_out", "ssm_a_re", "ssm_a_im", "ssm_log_step",
                 "ssm_b_re", "ssm_b_im", "ssm_c_re", "ssm_c_im", "ssm_glu_w", "dif_w_in", "dif_w_out"):
        shared[name] = f(inputs[name])[0]
    shared["moe_be"] = f(inputs["moe_be"]).reshape(2, 32)
    shared["c_ctx"] = f(inputs["c_ctx"]).reshape(1, D)
    shared.update(_consts())
    maps = []
    for b in cores:
        m = dict(shared)
        m["x"] = f(inputs["x"][b])
        m["ctx"] = f(inputs["ctx"][b])
        m["c"] = f(inputs["c"][b]).reshape(1, D)
        maps.append(m)
    return maps


def kernel(**inputs):
    nc = build()
    maps = make_in_maps(inputs, range(8))
    res = run_bass_kernel_spmd(nc, maps, core_ids=list(range(8)))
    return np.stack([r["out"] for r in res.results], 0).astype(np.float32)
```

```python
import math
from contextlib import ExitStack

import numpy as np
import concourse.bass as bass
import concourse.mybir as mybir
from concourse.bass_utils import run_bass_kernel_spmd

F32 = mybir.dt.float32
BF16 = mybir.dt.bfloat16
AF = mybir.ActivationFunctionType
ALU = mybir.AluOpType
AX = mybir.AxisListType

D = 1024
SEQ = 2048
CTX = 256
NT = SEQ + CTX
NTILE = NT // 128
NLT = SEQ // 128
ALPHA = 4 ** 0.25
LN_EPS = 1e-5


class Buf:
    __slots__ = ("w", "r")

    def __init__(self):
        self.w = None
        self.r = {}


class EngState:
    def __init__(self, name, eng, sem):
        self.name = name
        self.eng = eng
        self.sem = sem
        self.count = 0
        self.waited = {}
        self.slots = []
        self.slot_i = 0


class K:
    def __init__(self, nc, stack):
        self.nc = nc
        self.E = {}
        for name, eng in (("pe", nc.tensor), ("dve", nc.vector), ("act", nc.scalar),
                          ("pool", nc.gpsimd), ("sp", nc.sync)):
            sem = stack.enter_context(nc.semaphore("s_" + name))
            self.E[name] = EngState(name, eng, sem)
        self.semkey = {}
        for qn, n in (("sp", 12), ("pool", 12), ("act", 6)):
            for i in range(n):
                sem = stack.enter_context(nc.semaphore(f"d_{qn}{i}"))
                self.E[qn].slots.append([sem, 0])
        self.uid = 0

    def _key(self, sem):
        return id(sem)

    def _wait(self, E, deps, skip_self=False):
        best = {}
        for sem, val in deps:
            if skip_self and sem is E.sem:
                continue
            k = id(sem)
            if k not in best or best[k][1] < val:
                best[k] = (sem, val)
        for k, (sem, val) in best.items():
            if E.waited.get(k, 0) < val:
                E.eng.wait_ge(sem, val)
                E.waited[k] = val

    def _deps(self, reads, writes):
        deps = []
        for b in reads:
            if b.w is not None:
                deps.append(b.w)
        for b in writes:
            if b.w is not None:
                deps.append(b.w)
            deps.extend(b.r.values())
        return deps

    def _mark(self, tok, reads, writes):
        sem, val = tok
        for b in reads:
            b.r[id(sem)] = tok
        for b in writes:
            b.w = tok
            b.r = {}

    def op(self, en, fn, reads=(), writes=()):
        E = self.E[en]
        self._wait(E, self._deps(reads, writes), skip_self=(en == "pe"))
        ins = fn(E.eng)
        E.count += 1
        ins.then_inc(E.sem, 1)
        tok = (E.sem, E.count)
        self._mark(tok, reads, writes)
        return tok

    def dma(self, qn, out, in_, reads=(), writes=(), **kw):
        E = self.E[qn]
        self._wait(E, self._deps(reads, writes))
        slot = E.slots[E.slot_i % len(E.slots)]
        E.slot_i += 1
        if slot[1] > 0:
            self._wait(E, [(slot[0], slot[1] * 16)])
        ins = E.eng.dma_start(out=out, in_=in_, **kw)
        slot[1] += 1
        ins.then_inc(slot[0], 16)
        tok = (slot[0], slot[1] * 16)
        self._mark(tok, reads, writes)
        return tok

    def all_tokens(self):
        toks = []
        for E in self.E.values():
            if E.count:
                toks.append((E.sem, E.count))
            for sem, c in E.slots:
                if c:
                    toks.append((sem, c * 16))
        return toks

    def barrier(self):
        toks = self.all_tokens()
        for E in self.E.values():
            self._wait(E, toks, skip_self=False)


def build(dbg=(), inject=(), phases=None):
    nc = bass.Bass("TRN2", target_bir_lowering=False)
    dbg = set(dbg)
    inject = set(inject)
    ALLP = {"mod", "l0proj", "l0att", "l0ssm", "l0ssmpost", "l0out", "moe0", "l1proj", "l1att", "moe1"}
    phases = ALLP if phases is None else set(phases)

    def din(name, shape):
        return nc.dram_tensor(name, list(shape), F32, kind="ExternalInput").ap()

    def dscr(name, shape, dt=F32):
        kind = "ExternalOutput" if name in dbg else ("ExternalInput" if name in inject else "Internal")
        return nc.dram_tensor(name, list(shape), dt, kind=kind).ap()

    x_in = din("x", [SEQ, D])
    ctx_in = din("ctx", [CTX, D])
    c_in = din("c", [1, D])
    cc_in = din("c_ctx", [1, D])
    mod_w = din("mod_w", [2, D, 6 * D])
    mod_b = din("mod_b", [2, 6 * D])
    ln1_g = din("ln1_g", [2, D]); ln1_b = din("ln1_b", [2, D])
    ln2_g = din("ln2_g", [2, D]); ln2_b = din("ln2_b", [2, D])
    w_in0 = din("swa_ssm_w_in", [D, 1280])
    w_out0 = din("swa_ssm_w_out", [D, D])
    swa_sink = din("swa_sink", [1, 8])
    a_re = din("ssm_a_re", [2, 32, 64]); a_im = din("ssm_a_im", [2, 32, 64])
    log_step = din("ssm_log_step", [2, 32])
    b_re = din("ssm_b_re", [2, 32, 64, 16]); b_im = din("ssm_b_im", [2, 32, 64, 16])
    c_re = din("ssm_c_re", [2, 32, 16, 64]); c_im = din("ssm_c_im", [2, 32, 16, 64])
    ssm_d = din("ssm_d", [1, 512])
    glu_w = din("ssm_glu_w", [512, 512]); glu_b = din("ssm_glu_b", [1, 512])
    dif_w_in = din("dif_w_in", [D, 3072]); dif_w_out = din("dif_w_out", [D, D])
    lam_q1 = din("dif_lam_q1", [1, 64]); lam_k1 = din("dif_lam_k1", [1, 64])
    lam_q2 = din("dif_lam_q2", [1, 64]); lam_k2 = din("dif_lam_k2", [1, 64])
    subln_g = din("dif_subln_g", [1, 128])
    moe_wg = din("moe_wg", [2, D, 4]); moe_bg = din("moe_bg", [2, 4])
    moe_we = din("moe_we", [2, 4, D, 8]); moe_be = din("moe_be", [2, 32])
    moe_w1 = din("moe_w1", [2, 32, D, 256]); moe_w3 = din("moe_w3", [2, 32, D, 256])
    moe_w2 = din("moe_w2", [2, 32, 256, D])
    rope_cs = din("rope_cs", [SEQ, 64])
    ident_in = din("ident", [128, 128])
    sel_in = din("sel", [32, 32, 128])
    kval_in = din("kval", [64, 1024]); mrow_in = din("mrow", [64, 288])
    maskF_in = din("maskF", [128, 128]); maskB_in = din("maskB", [128, 128])
    maskL_in = din("maskL", [128, 128]); maskR_in = din("maskR", [128, 128])
    out = nc.dram_tensor("out", [SEQ, D], F32, kind="ExternalOutput").ap()

    modrow = dscr("modrow", [2, 2, 6 * D])

    with ExitStack() as gs:
        k = K(nc, gs)

        def sb(st, name, shape, dt=F32):
            k.uid += 1
            return st.enter_context(nc.sbuf_tensor(f"sb{k.uid}_{name}", list(shape), dt))

        def ps(st, name, shape, dt=F32):
            k.uid += 1
            return st.enter_context(nc.psum_tensor(f"ps{k.uid}_{name}", list(shape), dt))

        def phase(name):
            if name in phases:
                with ExitStack() as st_:
                    yield st_

        ident_f = sb(gs, "ident_f", [128, 128]); B_ident_f = Buf()
        ident_b = sb(gs, "ident_b", [128, 128], BF16); B_ident_b = Buf()
        k.dma("sp", ident_f[:], ident_in[:, :], writes=[B_ident_f])
        k.op("dve", lambda e: e.tensor_copy(out=ident_b[:], in_=ident_f[:]),
             reads=[B_ident_f], writes=[B_ident_b])

        for st in phase("mod"):
            cT = sb(st, "cT", [128, 8, 2]); B_cT = Buf()
            with nc.allow_non_contiguous_dma(reason="tiny column loads"):
                k.dma("sp", cT[:, :, 0], c_in[0, :].rearrange("(k p) -> p k", p=128), writes=[B_cT])
                k.dma("sp", cT[:, :, 1], cc_in[0, :].rearrange("(k p) -> p k", p=128), writes=[B_cT])
            sT = sb(st, "sT", [128, 8, 2]); B_sT = Buf()
            k.op("act", lambda e: e.activation(out=sT[:], in_=cT[:], func=AF.Silu),
                 reads=[B_cT], writes=[B_sT])
            wt = [sb(st, f"modw{i}", [128, 8, 512]) for i in range(2)]
            B_wt = [Buf(), Buf()]
            mb = sb(st, "modb", [2, 6 * D]); B_mb = Buf()
            mrow = sb(st, "mrow", [2, 6 * D]); B_mrow = Buf()
            pm = [ps(st, f"pmod{i}", [2, 512]) for i in range(2)]
            B_pm = [Buf(), Buf()]
            it = 0
            for l in range(2):
                k.dma("sp", mb[0:1, :], mod_b[l:l + 1, :], writes=[B_mb])
                k.dma("sp", mb[1:2, :], mod_b[l:l + 1, :], writes=[B_mb])
                for cb in range(12):
                    i = it % 2
                    it += 1
                    k.dma("sp" if cb % 2 == 0 else "act", wt[i][:],
                          mod_w[l, :, cb * 512:(cb + 1) * 512].rearrange("(k p) n -> p k n", p=128),
                          writes=[B_wt[i]])
                    for kc in range(8):
                        k.op("pe", lambda e, kc=kc, i=i: e.matmul(
                            pm[i][:], lhsT=sT[:, kc, :], rhs=wt[i][:, kc, :],
                            start=(kc == 0), stop=(kc == 7)),
                            reads=[B_sT, B_wt[i]], writes=[B_pm[i]])
                    k.op("dve", lambda e, i=i, cb=cb: e.tensor_tensor(
                        out=mrow[:, cb * 512:(cb + 1) * 512], in0=pm[i][:],
                        in1=mb[:, cb * 512:(cb + 1) * 512], op=ALU.add),
                        reads=[B_pm[i], B_mb], writes=[B_mrow])
                k.dma("sp", modrow[l], mrow[:], reads=[B_mrow], writes=[])
            k.barrier()


        def load_bc(st, name, src_row_ap, n, q="sp"):
            t = sb(st, name, [128, n]); B = Buf()
            k.dma(q, t[:], src_row_ap.partition_broadcast(128), writes=[B])
            return t, B

        def mod_bc(st, name, l, r, chunk, plus1=False):
            t, B = load_bc(st, name, modrow[l, r:r + 1, chunk * D:(chunk + 1) * D], D)
            if plus1:
                k.op("pool", lambda e: e.tensor_scalar(out=t[:], in0=t[:], scalar1=1.0, scalar2=None,
                                                       op0=ALU.add), reads=[B], writes=[B])
            return t, B

        u_scr = dscr("u_scr", [NT, 512])
        y_scr = dscr("y_scr", [NT, 512], BF16)
        x1_scr = dscr("x1_scr", [NT, D])
        x2_scr = dscr("x2_scr", [NT, D])
        dbg_q = dscr("dbg_q", [NT, 1280])

        def src_rows(ti, l):
            if l == 0:
                return x_in[ti * 128:(ti + 1) * 128, :] if ti < NLT else ctx_in[(ti - NLT) * 128:(ti - NLT + 1) * 128, :]
            return x2_scr[ti * 128:(ti + 1) * 128, :]

        def rope_apply(st_bufs, src_ps, B_src, dst, B_dst, nh, rt, B_rt, tmp1, tmp2, B_tmp):
            S = src_ps.rearrange("p (h a b f) -> p h a b f", h=nh, a=2, b=2, f=16)
            O = dst.rearrange("p (h a b f) -> p h a b f", h=nh, a=2, b=2, f=16)
            T1 = tmp1.rearrange("p (h a b f) -> p h a b f", h=nh, a=2, b=2, f=16)
            T2 = tmp2.rearrange("p (h a b f) -> p h a b f", h=nh, a=2, b=2, f=16)
            for a in range(2):
                cos = rt[:, a * 32:a * 32 + 16].rearrange("p (x y f) -> p x y f", x=1, y=1).to_broadcast([128, nh, 2, 16])
                sin = rt[:, a * 32 + 16:a * 32 + 32].rearrange("p (x y f) -> p x y f", x=1, y=1).to_broadcast([128, nh, 2, 16])
                k.op("dve", lambda e, a=a, cos=cos: e.tensor_tensor(out=T1[:, :, a], in0=S[:, :, a], in1=cos, op=ALU.mult),
                     reads=[B_src, B_rt], writes=[B_tmp])
                k.op("dve", lambda e, a=a, sin=sin: e.tensor_tensor(out=T2[:, :, a], in0=S[:, :, a, ::-1, :], in1=sin, op=ALU.mult),
                     reads=[B_src, B_rt], writes=[B_tmp])
                k.op("dve", lambda e, a=a: e.tensor_tensor(out=O[:, :, a, 0, :], in0=T1[:, :, a, 0, :], in1=T2[:, :, a, 0, :], op=ALU.subtract),
                     reads=[B_tmp], writes=[B_dst])
                k.op("dve", lambda e, a=a: e.tensor_tensor(out=O[:, :, a, 1, :], in0=T1[:, :, a, 1, :], in1=T2[:, :, a, 1, :], op=ALU.add),
                     reads=[B_tmp], writes=[B_dst])


        def ln_epilogue(wk, y_parts, B_y, xo, B_xo, g_t, lng_t, lnb_t, dst_rows):
            tmp, B_tmp, z, B_z, stt, B_stt, o, B_o = wk
            for hf in range(2):
                sl = slice(hf * 512, (hf + 1) * 512)
                k.op("dve", lambda e, hf=hf, sl=sl: e.tensor_tensor(out=tmp[:, sl], in0=y_parts[hf], in1=g_t[0][:, sl], op=ALU.mult),
                     reads=[B_y[hf], g_t[1]], writes=[B_tmp])
            k.op("dve", lambda e: e.scalar_tensor_tensor(out=z[:], in0=xo[:], scalar=ALPHA, in1=tmp[:], op0=ALU.mult, op1=ALU.add),
                 reads=[B_xo, B_tmp], writes=[B_z])
            for hf in range(2):
                k.op("dve", lambda e, hf=hf: e.bn_stats(out=stt[:, hf * 6:(hf + 1) * 6], in_=z[:, hf * 512:(hf + 1) * 512]),
                     reads=[B_z], writes=[B_stt])
            k.op("dve", lambda e: e.bn_aggr(out=stt[:, 12:14], in_=stt[:, 0:12]), reads=[B_stt], writes=[B_stt])
            k.op("dve", lambda e: e.tensor_scalar(out=stt[:, 15:16], in0=stt[:, 13:14], scalar1=LN_EPS, scalar2=None, op0=ALU.add),
                 reads=[B_stt], writes=[B_stt])
            k.op("act", lambda e: e.sqrt(out=stt[:, 15:16], in_=stt[:, 15:16]), reads=[B_stt], writes=[B_stt])
            k.op("dve", lambda e: e.reciprocal(out=stt[:, 14:15], in_=stt[:, 15:16]), reads=[B_stt], writes=[B_stt])
            k.op("dve", lambda e: e.tensor_scalar(out=tmp[:], in0=z[:], scalar1=stt[:, 12:13], scalar2=stt[:, 14:15], op0=ALU.subtract, op1=ALU.mult),
                 reads=[B_z, B_stt], writes=[B_tmp])
            k.op("pool", lambda e: e.tensor_tensor(out=o[:], in0=tmp[:], in1=lng_t[0][:], op=ALU.mult),
                 reads=[B_tmp, lng_t[1]], writes=[B_o])
            k.op("pool", lambda e: e.tensor_tensor(out=o[:], in0=o[:], in1=lnb_t[0][:], op=ALU.add),
                 reads=[B_o, lnb_t[1]], writes=[B_o])
            k.dma("sp", dst_rows, o[:], reads=[B_o])

        def ln_work(st, pfx):
            tmp = sb(st, pfx + "_tmp", [128, D]); z = sb(st, pfx + "_z", [128, D])
            stt = sb(st, pfx + "_stt", [128, 16]); o = sb(st, pfx + "_o", [128, D])
            return (tmp, Buf(), z, Buf(), stt, Buf(), o, Buf())

        def outproj_ln1(st, l, catT_, B_catT_, w_out_dram, ntiles):
            w_bf = sb(st, "w_out_bf", [128, 8, D], BF16); B_w = Buf()
            for kc in range(8):
                k.dma("pool", w_bf[:, kc, :], w_out_dram[kc * 128:(kc + 1) * 128, :], writes=[B_w])
            g1 = [mod_bc(st, f"g1_{r}", l, r, 2) for r in range(2)]
            lng = load_bc(st, "ln1g", ln1_g[l:l + 1, :], D)
            lnb = load_bc(st, "ln1b", ln1_b[l:l + 1, :], D)
            wk = ln_work(st, "e1")
            xo = [sb(st, f"xo{i}", [128, D]) for i in range(2)]; B_xo = [Buf(), Buf()]
            py = [ps(st, f"py{i}", [128, 512]) for i in range(4)]; B_py = [Buf() for _ in range(4)]
            for ti in range(ntiles):
                i = ti % 2
                r = 0 if ti < NLT else 1
                T0 = ti * 128
                k.dma("sp", xo[i][:], src_rows(ti, l), writes=[B_xo[i]])
                for hf in range(2):
                    pi = i * 2 + hf
                    for kc in range(8):
                        k.op("pe", lambda e, kc=kc, hf=hf, pi=pi, T0=T0: e.matmul(
                            py[pi][:], lhsT=catT_[:, kc, T0:T0 + 128], rhs=w_bf[:, kc, hf * 512:(hf + 1) * 512],
                            start=(kc == 0), stop=(kc == 7)), reads=[B_catT_, B_w], writes=[B_py[pi]])
                ln_epilogue(wk, [py[i * 2][:], py[i * 2 + 1][:]], [B_py[i * 2], B_py[i * 2 + 1]], xo[i], B_xo[i],
                            g1[r], lng, lnb, x1_scr[T0:T0 + 128, :])
            k.barrier()

        def moe_phase(st, l, ntiles, dst):
            ntok = ntiles * 128
            h2T = sb(st, "h2T", [128, 8, ntok], BF16); B_h2T = Buf()
            gateT = sb(st, "gateT", [32, ntok], BF16); B_gateT = Buf()
            f_acc = sb(st, "f_acc", [128, ntiles, D]); B_facc = [Buf() for _ in range(ntiles)]
            sel = sb(st, "sel", [32, 32, 128], BF16); B_sel = Buf()
            k.dma("pool", sel[:], sel_in[:, :, :], writes=[B_sel])
            with ExitStack() as s1:
                Wr = sb(s1, "Wr", [128, 8, 36]); B_Wr = Buf()
                with nc.allow_non_contiguous_dma(reason="small router weights"):
                    k.dma("sp", Wr[:, :, 0:4], moe_wg[l].rearrange("(k p) n -> p k n", p=128), writes=[B_Wr])
                    for g in range(4):
                        k.dma("sp", Wr[:, :, 4 + g * 8:12 + g * 8], moe_we[l, g].rearrange("(k p) n -> p k n", p=128), writes=[B_Wr])
                Whi = sb(s1, "Whi", [128, 8, 36], BF16); Wlo = sb(s1, "Wlo", [128, 8, 36], BF16); B_Wsp = Buf()
                k.op("dve", lambda e: e.tensor_copy(out=Whi[:], in_=Wr[:]), reads=[B_Wr], writes=[B_Wsp])
                k.op("dve", lambda e: e.tensor_tensor(out=Wlo[:], in0=Wr[:], in1=Whi[:], op=ALU.subtract), reads=[B_Wr, B_Wsp], writes=[B_Wsp])
                rb = sb(s1, "rb", [128, 36]); B_rb = Buf()
                k.dma("sp", rb[:, 0:4], moe_bg[l:l + 1, :].partition_broadcast(128), writes=[B_rb])
                k.dma("sp", rb[:, 4:36], moe_be[l:l + 1, :].partition_broadcast(128), writes=[B_rb])
                sc2p = [mod_bc(s1, f"sc2p_{r}", l, r, 4, plus1=True) for r in range(2)]
                sh2 = [mod_bc(s1, f"sh2_{r}", l, r, 3) for r in range(2)]
                xt = [sb(s1, f"mx{i}", [128, D]) for i in range(2)]; B_xt = [Buf(), Buf()]
                hf32 = sb(s1, "mh", [128, D]); B_h = Buf()
                hhi = sb(s1, "hhi", [128, D], BF16); hlo = sb(s1, "hlo", [128, D], BF16); B_hs = Buf()
                hloT = sb(s1, "hloT", [128, 8, 128], BF16); B_hloT = Buf()
                pTh = ps(s1, "mpTh", [128, 8, 128], BF16); B_pTh = Buf()
                pTl = ps(s1, "mpTl", [128, 8, 128], BF16); B_pTl = Buf()
                pr = ps(s1, "mpr", [128, 512]); B_pr = Buf()
                pg = ps(s1, "mpg", [32, 1024], BF16); B_pg = Buf()
                lg = sb(s1, "lg", [128, 36]); B_lg = Buf()
                sm = sb(s1, "rsm", [128, 160]); B_sm = Buf()
                gates = sb(s1, "gates", [128, 32]); B_gates = Buf()
                gates_bf = sb(s1, "gates_bf", [128, 32], BF16); B_gbf = Buf()
                for ti in range(ntiles):
                    i = ti % 2
                    r = 0 if ti < NLT else 1
                    T0 = ti * 128
                    k.dma("sp", xt[i][:], x1_scr[T0:T0 + 128, :], writes=[B_xt[i]])
                    k.op("dve", lambda e, i=i, r=r: e.tensor_tensor(out=hf32[:], in0=xt[i][:], in1=sc2p[r][0][:], op=ALU.mult),
                         reads=[B_xt[i], sc2p[r][1]], writes=[B_h])
                    k.op("pool", lambda e, r=r: e.tensor_tensor(out=hf32[:], in0=hf32[:], in1=sh2[r][0][:], op=ALU.add),
                         reads=[B_h, sh2[r][1]], writes=[B_h])
                    k.op("pool", lambda e: e.tensor_copy(out=hhi[:], in_=hf32[:]), reads=[B_h], writes=[B_hs])
                    k.op("dve", lambda e: e.tensor_tensor(out=hlo[:], in0=hf32[:], in1=hhi[:], op=ALU.subtract), reads=[B_h, B_hs], writes=[B_hs])
                    for kc in range(8):
                        k.op("pe", lambda e, kc=kc: e.transpose(out=pTh[:, kc, :], in_=hhi[:, kc * 128:(kc + 1) * 128], identity=ident_b[:]),
                             reads=[B_hs, B_ident_b], writes=[B_pTh])
                    for kc in range(8):
                        k.op("pe", lambda e, kc=kc: e.transpose(out=pTl[:, kc, :], in_=hlo[:, kc * 128:(kc + 1) * 128], identity=ident_b[:]),
                             reads=[B_hs, B_ident_b], writes=[B_pTl])
                    k.op("act", lambda e, T0=T0: e.copy(out=h2T[:, :, T0:T0 + 128], in_=pTh[:]), reads=[B_pTh], writes=[B_h2T])
                    k.op("dve", lambda e: e.tensor_copy(out=hloT[:], in_=pTl[:]), reads=[B_pTl], writes=[B_hloT])
                    n_mm = 24
                    j = 0
                    for (A, BA, W) in ((None, B_h2T, Whi), (hloT, B_hloT, Whi), (None, B_h2T, Wlo)):
                        for kc in range(8):
                            lhs = h2T[:, kc, T0:T0 + 128] if A is None else A[:, kc, :]
                            k.op("pe", lambda e, lhs=lhs, W=W, kc=kc, j=j: e.matmul(pr[:, 0:36], lhsT=lhs, rhs=W[:, kc, :], start=(j == 0), stop=(j == 23)),
                                 reads=[BA, B_Wsp], writes=[B_pr])
                            j += 1
                    R = [B_lg, B_sm]
                    def dv(fn, reads=R, writes=(B_sm,)):
                        k.op("dve", fn, reads=list(reads), writes=list(writes))
                    k.op("dve", lambda e: e.tensor_tensor(out=lg[:], in0=pr[:, 0:36], in1=rb[:], op=ALU.add), reads=[B_pr, B_rb], writes=[B_lg])
                    dv(lambda e: e.reduce_max(out=sm[:, 0:1], in_=lg[:, 0:4], axis=AX.X))
                    dv(lambda e: e.tensor_scalar(out=sm[:, 1:2], in0=sm[:, 0:1], scalar1=-1.0, scalar2=None, op0=ALU.mult))
                    k.op("act", lambda e: e.activation(out=sm[:, 56:60], in_=lg[:, 0:4], func=AF.Exp, bias=sm[:, 1:2], scale=1.0, accum_out=sm[:, 2:3]),
                         reads=R, writes=[B_sm])
                    dv(lambda e: e.reciprocal(out=sm[:, 3:4], in_=sm[:, 2:3]))
                    dv(lambda e: e.tensor_scalar(out=sm[:, 4:8], in0=lg[:, 0:4], scalar1=sm[:, 0:1], scalar2=None, op0=ALU.is_equal))
                    le = lg[:, 4:36].rearrange("p (g e) -> p g e", g=4)
                    tmp48 = sm[:, 64:96].rearrange("p (g e) -> p g e", g=4)
                    ohb = sm[:, 4:8].rearrange("p (g x) -> p g x", x=1).to_broadcast([128, 4, 8])
                    dv(lambda e: e.tensor_tensor(out=tmp48, in0=le, in1=ohb, op=ALU.mult))
                    dv(lambda e: e.tensor_reduce(out=sm[:, 8:16], in_=sm[:, 64:96].rearrange("p (g e) -> p e g", g=4), axis=AX.X, op=ALU.add))
                    dv(lambda e: e.reduce_max(out=sm[:, 16:17], in_=sm[:, 8:16], axis=AX.X))
                    dv(lambda e: e.tensor_scalar(out=sm[:, 24:32], in0=sm[:, 8:16], scalar1=sm[:, 16:17], scalar2=None, op0=ALU.is_equal))
                    dv(lambda e: e.scalar_tensor_tensor(out=sm[:, 32:40], in0=sm[:, 24:32], scalar=-1e30, in1=sm[:, 8:16], op0=ALU.mult, op1=ALU.add))
                    dv(lambda e: e.reduce_max(out=sm[:, 17:18], in_=sm[:, 32:40], axis=AX.X))
                    dv(lambda e: e.tensor_scalar(out=sm[:, 40:48], in0=sm[:, 32:40], scalar1=sm[:, 17:18], scalar2=None, op0=ALU.is_equal))
                    dv(lambda e: e.tensor_tensor(out=sm[:, 18:19], in0=sm[:, 17:18], in1=sm[:, 16:17], op=ALU.subtract))
                    k.op("act", lambda e: e.activation(out=sm[:, 19:20], in_=sm[:, 18:19], func=AF.Exp), reads=R, writes=[B_sm])
                    dv(lambda e: e.tensor_scalar(out=sm[:, 20:21], in0=sm[:, 19:20], scalar1=1.0, scalar2=None, op0=ALU.add))
                    dv(lambda e: e.reciprocal(out=sm[:, 20:21], in_=sm[:, 20:21]))
                    dv(lambda e: e.tensor_tensor(out=sm[:, 21:22], in0=sm[:, 19:20], in1=sm[:, 20:21], op=ALU.mult))
                    dv(lambda e: e.tensor_tensor(out=sm[:, 22:23], in0=sm[:, 20:21], in1=sm[:, 3:4], op=ALU.mult))
                    dv(lambda e: e.tensor_tensor(out=sm[:, 23:24], in0=sm[:, 21:22], in1=sm[:, 3:4], op=ALU.mult))
                    dv(lambda e: e.tensor_scalar(out=sm[:, 48:56], in0=sm[:, 24:32], scalar1=sm[:, 22:23], scalar2=None, op0=ALU.mult))
                    dv(lambda e: e.scalar_tensor_tensor(out=sm[:, 48:56], in0=sm[:, 40:48], scalar=sm[:, 23:24], in1=sm[:, 48:56], op0=ALU.mult, op1=ALU.add))
                    geb = sm[:, 48:56].rearrange("p (x e) -> p x e", x=1).to_broadcast([128, 4, 8])
                    k.op("dve", lambda e: e.tensor_tensor(out=gates[:].rearrange("p (g e) -> p g e", g=4), in0=ohb, in1=geb, op=ALU.mult),
                         reads=R, writes=[B_gates])
                    k.op("dve", lambda e: e.tensor_copy(out=gates_bf[:], in_=gates[:]), reads=[B_gates], writes=[B_gbf])
                    k.op("pe", lambda e: e.transpose(out=pg[:, 0:128], in_=gates_bf[:], identity=ident_b[:]),
                         reads=[B_gbf, B_ident_b], writes=[B_pg])
                    k.op("act", lambda e, T0=T0: e.copy(out=gateT[:, T0:T0 + 128], in_=pg[:, 0:128]), reads=[B_pg], writes=[B_gateT])
                    if "dbg_gates" in dbg:
                        if ti == 0:
                            dbg_gates = dscr("dbg_gates", [NT, 32])
                        k.dma("sp", dbg_gates[T0:T0 + 128, :], gates[:], reads=[B_gates])
                k.barrier()
            if "stop_router" in dbg:
                return
            with ExitStack() as s2:
                w13 = [sb(s2, f"w13_{i}", [128, 8, 512], BF16) for i in range(2)]; B_w13 = [Buf(), Buf()]
                w2 = [sb(s2, f"w2_{i}", [128, 2, D], BF16) for i in range(2)]; B_w2 = [Buf(), Buf()]
                hh = [sb(s2, f"hh{i}", [128, 2, ntok], BF16) for i in range(2)]; B_hh = [Buf(), Buf()]
                stg13 = sb(s2, "stg13", [128, 8, 512]); B_stg13 = Buf()
                stg2 = sb(s2, "stg2", [128, 2, D]); B_stg2 = Buf()
                s1t = [sb(s2, f"s1t{i}", [128, 512]) for i in range(2)]; B_s1t = [Buf(), Buf()]
                t3 = [sb(s2, f"t3{i}", [128, 512]) for i in range(2)]; B_t3 = [Buf(), Buf()]
                ph1 = [ps(s2, f"ph1_{i}", [128, 512]) for i in range(2)]; B_ph1 = [Buf(), Buf()]
                ph3 = [ps(s2, f"ph3_{i}", [128, 512]) for i in range(2)]; B_ph3 = [Buf(), Buf()]
                pgb = ps(s2, "pgb", [128, 512]); B_pgb = Buf()
                pf = [ps(s2, f"pf{i}", [128, 512]) for i in range(2)]; B_pf = [Buf(), Buf()]
                blocks = [(b0, min(512, ntok - b0)) for b0 in range(0, ntok, 512)]
                it = 0; itf = 0
                for e_ in range(32):
                    wi = e_ % 2
                    k.dma("sp", stg13[:, :, 0:256], moe_w1[l, e_].rearrange("(k p) f -> p k f", p=128), writes=[B_stg13])
                    k.dma("act", stg13[:, :, 256:512], moe_w3[l, e_].rearrange("(k p) f -> p k f", p=128), writes=[B_stg13])
                    k.dma("sp", stg2[:], moe_w2[l, e_].rearrange("(c p) d -> p c d", p=128), writes=[B_stg2])
                    k.op("pool", lambda e, wi=wi: e.tensor_copy(out=w13[wi][:], in_=stg13[:]), reads=[B_stg13], writes=[B_w13[wi]])
                    k.op("pool", lambda e, wi=wi: e.tensor_copy(out=w2[wi][:], in_=stg2[:]), reads=[B_stg2], writes=[B_w2[wi]])
                    for (b0, bn) in blocks:
                        k.op("pe", lambda e, e_=e_, b0=b0, bn=bn: e.matmul(pgb[:, 0:bn], lhsT=sel[:, e_, :], rhs=gateT[:, b0:b0 + bn], start=True, stop=True),
                             reads=[B_sel, B_gateT], writes=[B_pgb])
                        for fc in range(2):
                            i = it % 2; it += 1
                            for kc in range(8):
                                k.op("pe", lambda e, kc=kc, fc=fc, i=i, wi=wi, b0=b0, bn=bn: e.matmul(
                                    ph1[i][:, 0:bn], lhsT=w13[wi][:, kc, fc * 128:(fc + 1) * 128], rhs=h2T[:, kc, b0:b0 + bn],
                                    start=(kc == 0), stop=(kc == 7)), reads=[B_w13[wi], B_h2T], writes=[B_ph1[i]])
                            for kc in range(8):
                                k.op("pe", lambda e, kc=kc, fc=fc, i=i, wi=wi, b0=b0, bn=bn: e.matmul(
                                    ph3[i][:, 0:bn], lhsT=w13[wi][:, kc, 256 + fc * 128:256 + (fc + 1) * 128], rhs=h2T[:, kc, b0:b0 + bn],
                                    start=(kc == 0), stop=(kc == 7)), reads=[B_w13[wi], B_h2T], writes=[B_ph3[i]])
                            k.op("act", lambda e, i=i, bn=bn: e.activation(out=s1t[i][:, 0:bn], in_=ph1[i][:, 0:bn], func=AF.Silu),
                                 reads=[B_ph1[i]], writes=[B_s1t[i]])
                            k.op("dve", lambda e, i=i, bn=bn: e.tensor_tensor(out=t3[i][:, 0:bn], in0=s1t[i][:, 0:bn], in1=ph3[i][:, 0:bn], op=ALU.mult),
                                 reads=[B_s1t[i], B_ph3[i]], writes=[B_t3[i]])
                            k.op("dve", lambda e, i=i, bn=bn, fc=fc, wi=wi, b0=b0: e.tensor_tensor(out=hh[wi][:, fc, b0:b0 + bn], in0=t3[i][:, 0:bn], in1=pgb[:, 0:bn], op=ALU.mult),
                                 reads=[B_t3[i], B_pgb], writes=[B_hh[wi]])
                    for tt in range(ntiles):
                        for dc in range(2):
                            j = itf % 2; itf += 1
                            for fc in range(2):
                                k.op("pe", lambda e, fc=fc, j=j, wi=wi, tt=tt, dc=dc: e.matmul(
                                    pf[j][:], lhsT=hh[wi][:, fc, tt * 128:(tt + 1) * 128], rhs=w2[wi][:, fc, dc * 512:(dc + 1) * 512],
                                    start=(fc == 0), stop=(fc == 1)), reads=[B_hh[wi], B_w2[wi]], writes=[B_pf[j]])
                            if e_ == 0:
                                k.op("dve", lambda e, j=j, tt=tt, dc=dc: e.tensor_copy(out=f_acc[:, tt, dc * 512:(dc + 1) * 512], in_=pf[j][:]),
                                     reads=[B_pf[j]], writes=[B_facc[tt]])
                            else:
                                k.op("dve", lambda e, j=j, tt=tt, dc=dc: e.tensor_tensor(out=f_acc[:, tt, dc * 512:(dc + 1) * 512],
                                     in0=f_acc[:, tt, dc * 512:(dc + 1) * 512], in1=pf[j][:], op=ALU.add),
                                     reads=[B_pf[j], B_facc[tt]], writes=[B_facc[tt]])
                k.barrier()
            if "stop_experts" in dbg:
                return
            with ExitStack() as s3:
                g2 = [mod_bc(s3, f"g2_{r}", l, r, 5) for r in range(2)]
                lng = load_bc(s3, "ln2g", ln2_g[l:l + 1, :], D)
                lnb = load_bc(s3, "ln2b", ln2_b[l:l + 1, :], D)
                wk = ln_work(s3, "e2")
                xo = [sb(s3, f"x1o{i}", [128, D]) for i in range(2)]; B_xo = [Buf(), Buf()]
                for ti in range(ntiles):
                    i = ti % 2
                    r = 0 if ti < NLT else 1
                    T0 = ti * 128
                    k.dma("sp", xo[i][:], x1_scr[T0:T0 + 128, :], writes=[B_xo[i]])
                    if "dbg_f" in dbg:
                        if ti == 0:
                            dbg_f = dscr("dbg_f", [NT, D])
                        k.dma("sp", dbg_f[T0:T0 + 128, :], f_acc[:, ti, :], reads=[B_facc[ti]])
                    ln_epilogue(wk, [f_acc[:, ti, 0:512], f_acc[:, ti, 512:1024]], [B_facc[ti], B_facc[ti]], xo[i], B_xo[i],
                                g2[r], lng, lnb, dst[T0:T0 + 128, :])
                k.barrier()


        def ssm_phase(st, catT_, B_catT_):
            PI = math.pi
            TWO_PI = 2.0 * math.pi
            def bc3(ap2, n):
                P_, G_ = ap2.shape
                return ap2.rearrange("p (g x) -> p g x", x=1).to_broadcast([P_, G_, n])
            I32 = mybir.dt.int32
            INV2PI = 1.0 / TWO_PI

            def sincos(ang_ap, shape, s_out, c_out, tmps, B_in, B_out, B_tmp):
                y, yi, yf = tmps
                dvt = lambda fn: k.op("dve", fn, reads=[B_in, B_tmp, B_out], writes=[B_tmp])
                dvt(lambda e: e.tensor_scalar(out=y, in0=ang_ap, scalar1=INV2PI, scalar2=32.5, op0=ALU.mult, op1=ALU.add))
                dvt(lambda e: e.tensor_copy(out=yi, in_=y))
                dvt(lambda e: e.tensor_copy(out=yf, in_=yi))
                dvt(lambda e: e.tensor_tensor(out=y, in0=y, in1=yf, op=ALU.subtract))
                dvt(lambda e: e.scalar_tensor_tensor(out=yf, in0=y, scalar=0.0, in1=y, op0=ALU.is_lt, op1=ALU.add))
                k.op("act", lambda e: e.activation(out=s_out, in_=yf, func=AF.Sin, bias=negpi[0:shape[0], :], scale=TWO_PI),
                     reads=[B_tmp, B_np], writes=[B_out])
                dvt(lambda e: e.tensor_scalar(out=y, in0=yf, scalar1=0.25, scalar2=None, op0=ALU.add))
                dvt(lambda e: e.scalar_tensor_tensor(out=yf, in0=y, scalar=1.0, in1=y, op0=ALU.is_ge, op1=ALU.subtract))
                k.op("act", lambda e: e.activation(out=c_out, in_=yf, func=AF.Sin, bias=negpi[0:shape[0], :], scale=-TWO_PI),
                     reads=[B_tmp, B_np], writes=[B_out])

            ar = sb(st, "ar", [64, 64]); ai = sb(st, "ai", [64, 64]); ls = sb(st, "ls", [64, 64]); B_par = Buf()
            with nc.allow_non_contiguous_dma(reason="ssm params"):
                k.dma("sp", ar[:], a_re.rearrange("d g p -> p (d g)"), writes=[B_par])
                k.dma("sp", ai[:], a_im.rearrange("d g p -> p (d g)"), writes=[B_par])
            k.dma("sp", ls[:], log_step.rearrange("(x d) g -> x (d g)", x=1).partition_broadcast(64), writes=[B_par])
            negpi = sb(st, "negpi", [128, 1]); B_np = Buf()
            k.op("dve", lambda e: e.memset(negpi[:], -PI), writes=[B_np])
            kv = sb(st, "kv", [64, 16, 64]); B_kv = Buf()
            k.dma("sp", kv[:], kval_in.rearrange("p (k g) -> p k g", k=16), writes=[B_kv])
            mrow = sb(st, "mrow", [64, 288]); B_mrow = Buf()
            k.dma("sp", mrow[:], mrow_in[:, :], writes=[B_mrow])
            maskF = sb(st, "maskF", [128, 128]); maskB = sb(st, "maskB", [128, 128]); B_mk = Buf()
            k.dma("sp", maskF[:], maskF_in[:, :], writes=[B_mk])
            k.dma("sp", maskB[:], maskB_in[:, :], writes=[B_mk])
            dar = sb(st, "dar", [64, 64]); dai = sb(st, "dai", [64, 64]); B_d = Buf()
            k.op("act", lambda e: e.activation(out=ls[:], in_=ls[:], func=AF.Exp), reads=[B_par], writes=[B_par])
            k.op("dve", lambda e: e.tensor_tensor(out=dar[:], in0=ls[:], in1=ar[:], op=ALU.mult), reads=[B_par], writes=[B_d])
            k.op("dve", lambda e: e.tensor_tensor(out=dai[:], in0=ls[:], in1=ai[:], op=ALU.mult), reads=[B_par, B_d], writes=[B_d])
            LR = sb(st, "LR", [64, 16, 64]); LI = sb(st, "LI", [64, 16, 64]); MG = sb(st, "MG", [64, 16, 64]); B_L = Buf()
            th8 = sb(st, "th8", [64, 64]); B_th8 = Buf()
            k.op("dve", lambda e: e.tensor_scalar(out=th8[:], in0=dai[:], scalar1=8.0, scalar2=None, op0=ALU.mult), reads=[B_d], writes=[B_th8])
            with ExitStack() as t0:
                ang = sb(t0, "ang", [64, 16, 64]); a2 = sb(t0, "a2", [64, 16, 64]); B_ang = Buf()
                dai_b = dai[:].rearrange("p (x g) -> p x g", x=1).to_broadcast([64, 16, 64])
                dar_b = dar[:].rearrange("p (x g) -> p x g", x=1).to_broadcast([64, 16, 64])
                k.op("dve", lambda e: e.tensor_tensor(out=MG[:], in0=kv[:], in1=dar_b, op=ALU.mult), reads=[B_kv, B_d], writes=[B_L])
                k.op("act", lambda e: e.activation(out=MG[:], in_=MG[:], func=AF.Exp), reads=[B_L], writes=[B_L])
                k.op("dve", lambda e: e.tensor_tensor(out=ang[:], in0=kv[:], in1=dai_b, op=ALU.mult), reads=[B_kv, B_d], writes=[B_ang])
                a3 = sb(t0, "a3", [64, 16, 64], I32); a4 = sb(t0, "a4", [64, 16, 64])
                sincos(ang[:], [64, 16, 64], LI[:], LR[:], (a2[:], a3[:], a4[:]), B_ang, B_L, B_ang)
                k.op("dve", lambda e: e.tensor_tensor(out=LR[:], in0=LR[:], in1=MG[:], op=ALU.mult), reads=[B_L], writes=[B_L])
                k.op("dve", lambda e: e.tensor_tensor(out=LI[:], in0=LI[:], in1=MG[:], op=ALU.mult), reads=[B_L], writes=[B_L])
                k.barrier()
            cre = sb(st, "cre", [64, 64]); cim = sb(st, "cim", [64, 64]); B_c = Buf()
            with ExitStack() as t0:
                nr = sb(t0, "nr", [64, 64]); den = sb(t0, "den", [64, 64]); tq = sb(t0, "tq", [64, 64]); B_t = Buf()
                L1r = LR[:, 8, :]; L1i = LI[:, 8, :]
                dv = lambda fn: k.op("dve", fn, reads=[B_t, B_L, B_par, B_c], writes=[B_t, B_c])
                dv(lambda e: e.tensor_scalar(out=nr[:], in0=L1r, scalar1=-1.0, scalar2=None, op0=ALU.add))
                dv(lambda e: e.tensor_tensor(out=den[:], in0=ar[:], in1=ar[:], op=ALU.mult))
                dv(lambda e: e.tensor_tensor(out=tq[:], in0=ai[:], in1=ai[:], op=ALU.mult))
                dv(lambda e: e.tensor_tensor(out=den[:], in0=den[:], in1=tq[:], op=ALU.add))
                dv(lambda e: e.reciprocal(out=den[:], in_=den[:]))
                dv(lambda e: e.tensor_tensor(out=cre[:], in0=nr[:], in1=ar[:], op=ALU.mult))
                dv(lambda e: e.tensor_tensor(out=tq[:], in0=L1i, in1=ai[:], op=ALU.mult))
                dv(lambda e: e.tensor_tensor(out=cre[:], in0=cre[:], in1=tq[:], op=ALU.add))
                dv(lambda e: e.tensor_tensor(out=cre[:], in0=cre[:], in1=den[:], op=ALU.mult))
                dv(lambda e: e.tensor_tensor(out=cim[:], in0=L1i, in1=ar[:], op=ALU.mult))
                dv(lambda e: e.tensor_tensor(out=tq[:], in0=nr[:], in1=ai[:], op=ALU.mult))
                dv(lambda e: e.tensor_tensor(out=cim[:], in0=cim[:], in1=tq[:], op=ALU.subtract))
                dv(lambda e: e.tensor_tensor(out=cim[:], in0=cim[:], in1=den[:], op=ALU.mult))
                k.barrier()
            UT_all = sb(st, "UT_all", [128, 32, 288], BF16); B_UT = Buf()
            NB = ((0, 128), (128, 128), (256, 32))
            u8v = u_scr.rearrange("(n j) f -> n (j f)", j=8)
            with ExitStack() as t0:
                U8 = sb(t0, "U8", [128, 3, 4096]); B_U8 = Buf()
                U8b = sb(t0, "U8b", [128, 3, 4096], BF16); B_U8b = Buf()
                pU = [ps(t0, f"pU{i}", [128, 1024], BF16) for i in range(2)]; B_pU = [Buf(), Buf()]
                for bi, (n0, nb) in enumerate(NB):
                    k.dma("sp", U8[0:nb, bi, :], u8v[n0:n0 + nb, :], writes=[B_U8])
                    ov = U8b[0:nb, bi, :].rearrange("p (g j c) -> p j g c", g=32, j=8, c=16)
                    iv = U8[0:nb, bi, :].rearrange("p (j g c) -> p j g c", g=32, j=8, c=16)
                    k.op("pool" if bi == 1 else "act", (lambda e, ov=ov, iv=iv: e.tensor_copy(out=ov, in_=iv)) if bi == 1 else
                         (lambda e, ov=ov, iv=iv: e.copy(out=ov, in_=iv)), reads=[B_U8], writes=[B_U8b])
                for g in range(32):
                    i = g % 2
                    for bi, (n0, nb) in enumerate(NB):
                        src = U8b[0:nb, bi, g * 128:(g + 1) * 128]
                        k.op("pe", lambda e, i=i, src=src, n0=n0, nb=nb: e.transpose(out=pU[i][:, n0:n0 + nb], in_=src, identity=ident_b[0:nb, 0:nb]),
                             reads=[B_U8b, B_ident_b], writes=[B_pU[i]])
                    k.op("act", lambda e, i=i, g=g: e.copy(out=UT_all[:, g, :], in_=pU[i][:, 0:288]), reads=[B_pU[i]], writes=[B_UT])
                k.barrier()
            COr = sb(st, "COr", [64, 2, 32, 128], BF16); COi = sb(st, "COi", [64, 2, 32, 128], BF16); B_CO = Buf()
            T_all = sb(st, "T_all", [128, 32, 128], BF16); B_T = Buf()
            WinT = sb(st, "WinT", [128, 2, 32, 128], BF16); B_WinT = Buf()
            with ExitStack() as t0:
                BLr = sb(t0, "BLr", [64, 32, 128], BF16); BLi = sb(t0, "BLi", [64, 32, 128], BF16); B_BL = Buf()
                CTr = sb(t0, "CTr", [64, 32, 128], BF16); CTi = sb(t0, "CTi", [64, 32, 128], BF16); B_CT = Buf()
                Br = sb(t0, "Br", [64, 64, 16]); Bi = sb(t0, "Bi", [64, 64, 16]); B_B = Buf()
                Cr = sb(t0, "Cr", [64, 64, 16]); Ci = sb(t0, "Ci", [64, 64, 16]); B_C = Buf()
                Bbr = sb(t0, "Bbr", [64, 64, 16]); Bbi = sb(t0, "Bbi", [64, 64, 16]); B_Bb = Buf()
                with nc.allow_non_contiguous_dma(reason="ssm B/C tables"):
                    for d in range(2):
                        k.dma("sp", Br[:, d * 32:(d + 1) * 32, :], b_re[d].rearrange("g p c -> p g c"), writes=[B_B])
                        k.dma("sp", Bi[:, d * 32:(d + 1) * 32, :], b_im[d].rearrange("g p c -> p g c"), writes=[B_B])
                        for gb in range(4):
                            sl = slice(d * 32 + gb * 8, d * 32 + gb * 8 + 8)
                            k.dma("sp", Cr[:, sl, :], c_re[d, gb * 8:(gb + 1) * 8].rearrange("g c p -> p g c"), writes=[B_C])
                            k.dma("act", Ci[:, sl, :], c_im[d, gb * 8:(gb + 1) * 8].rearrange("g c p -> p g c"), writes=[B_C])
                t1 = sb(t0, "t1", [64, 64, 16]); t2 = sb(t0, "t2", [64, 64, 16]); B_t12 = Buf()
                creb = bc3(cre[:], 16); cimb = bc3(cim[:], 16)
                dv = lambda fn: k.op("dve", fn, reads=[B_B, B_c, B_t12, B_Bb], writes=[B_t12, B_Bb])
                dv(lambda e: e.tensor_tensor(out=t1[:], in0=Br[:], in1=creb, op=ALU.mult))
                dv(lambda e: e.tensor_tensor(out=t2[:], in0=Bi[:], in1=cimb, op=ALU.mult))
                dv(lambda e: e.tensor_tensor(out=Bbr[:], in0=t1[:], in1=t2[:], op=ALU.subtract))
                dv(lambda e: e.tensor_tensor(out=t1[:], in0=Bi[:], in1=creb, op=ALU.mult))
                dv(lambda e: e.tensor_tensor(out=t2[:], in0=Br[:], in1=cimb, op=ALU.mult))
                dv(lambda e: e.tensor_tensor(out=Bbi[:], in0=t1[:], in1=t2[:], op=ALU.add))
                ta = sb(t0, "ta", [64, 32, 16]); tb_ = sb(t0, "tb", [64, 32, 16]); B_tab = Buf()
                tc_ = sb(t0, "tc", [64, 32, 16]); td_ = sb(t0, "td", [64, 32, 16]); B_tcd = Buf()
                pTd = [ps(t0, f"pTd{i}", [128, 512]) for i in range(2)]; B_pTd = [Buf(), Buf()]
                pW = [ps(t0, f"pW{i}", [128, 8, 128], BF16) for i in range(2)]; B_pW = [Buf(), Buf()]
                tt = sb(t0, "tt", [128, 128]); B_tt = Buf()
                for d in range(2):
                    dsl = slice(d * 32, (d + 1) * 32)
                    for j in range(8):
                        e_ = (7 - j) if d == 0 else j
                        lr = bc3(LR[:, e_ + 7, dsl], 16); li = bc3(LI[:, e_ + 7, dsl], 16)
                        o_r = BLr[:, :, j * 16:(j + 1) * 16]; o_i = BLi[:, :, j * 16:(j + 1) * 16]
                        dv2 = lambda fn: k.op("dve", fn, reads=[B_Bb, B_L, B_tab, B_BL], writes=[B_tab, B_BL])
                        dv2(lambda e, lr=lr: e.tensor_tensor(out=ta[:], in0=Bbr[:, dsl, :], in1=lr, op=ALU.mult))
                        dv2(lambda e, li=li: e.tensor_tensor(out=tb_[:], in0=Bbi[:, dsl, :], in1=li, op=ALU.mult))
                        dv2(lambda e, o_r=o_r: e.tensor_tensor(out=o_r, in0=ta[:], in1=tb_[:], op=ALU.subtract))
                        dv2(lambda e, lr=lr: e.tensor_tensor(out=ta[:], in0=Bbi[:, dsl, :], in1=lr, op=ALU.mult))
                        dv2(lambda e, li=li: e.tensor_tensor(out=tb_[:], in0=Bbr[:, dsl, :], in1=li, op=ALU.mult))
                        dv2(lambda e, o_i=o_i: e.tensor_tensor(out=o_i, in0=ta[:], in1=tb_[:], op=ALU.add))
                        f_ = (j - 7) if d == 0 else -j
                        for (kk, o_r, o_i, BO) in ((f_ + 7, CTr[:, :, j * 16:(j + 1) * 16], CTi[:, :, j * 16:(j + 1) * 16], B_CT),
                                                   (f_ + 15, COr[:, d, :, j * 16:(j + 1) * 16], COi[:, d, :, j * 16:(j + 1) * 16], B_CO)):
                            lr = bc3(LR[:, kk, dsl], 16); li = bc3(LI[:, kk, dsl], 16)
                            pl = lambda fn, BO=BO: k.op("pool", fn, reads=[B_C, B_L, B_tcd, BO], writes=[B_tcd, BO])
                            pl(lambda e, lr=lr: e.tensor_tensor(out=tc_[:], in0=Cr[:, dsl, :], in1=lr, op=ALU.mult))
                            pl(lambda e, li=li: e.tensor_tensor(out=td_[:], in0=Ci[:, dsl, :], in1=li, op=ALU.mult))
                            pl(lambda e, o_r=o_r: e.tensor_tensor(out=o_r, in0=tc_[:], in1=td_[:], op=ALU.subtract))
                            pl(lambda e, lr=lr: e.tensor_tensor(out=tc_[:], in0=Ci[:, dsl, :], in1=lr, op=ALU.mult))
                            pl(lambda e, li=li: e.tensor_tensor(out=td_[:], in0=Cr[:, dsl, :], in1=li, op=ALU.mult))
                            pl(lambda e: e.tensor_tensor(out=tc_[:], in0=tc_[:], in1=td_[:], op=ALU.add))
                            pl(lambda e, o_i=o_i: e.tensor_scalar(out=o_i, in0=tc_[:], scalar1=-1.0, scalar2=None, op0=ALU.mult))
                    for g in range(32):
                        i = g % 2
                        k.op("pe", lambda e, i=i, g=g: e.matmul(pTd[i][:, 0:128], lhsT=BLr[:, g, :], rhs=CTr[:, g, :], start=True, stop=False),
                             reads=[B_BL, B_CT], writes=[B_pTd[i]])
                        k.op("pe", lambda e, i=i, g=g: e.matmul(pTd[i][:, 0:128], lhsT=BLi[:, g, :], rhs=CTi[:, g, :], start=False, stop=True),
                             reads=[B_BL, B_CT], writes=[B_pTd[i]])
                        if d == 0:
                            k.op("dve", lambda e, i=i, g=g: e.tensor_tensor(out=T_all[:, g, :], in0=pTd[i][:, 0:128], in1=maskF[:], op=ALU.mult),
                                 reads=[B_pTd[i], B_mk], writes=[B_T])
                        else:
                            k.op("dve", lambda e, i=i: e.tensor_tensor(out=tt[:], in0=pTd[i][:, 0:128], in1=maskB[:], op=ALU.mult),
                                 reads=[B_pTd[i], B_mk], writes=[B_tt])
                            k.op("dve", lambda e, g=g: e.tensor_tensor(out=T_all[:, g, :], in0=T_all[:, g, :], in1=tt[:], op=ALU.add),
                                 reads=[B_tt, B_T], writes=[B_T])
                        k.op("pe", lambda e, i=i, g=g: e.transpose(out=pW[i][:, 0, 0:64], in_=BLr[:, g, :], identity=ident_b[0:64, 0:64]),
                             reads=[B_BL, B_ident_b], writes=[B_pW[i]])
                        k.op("pe", lambda e, i=i, g=g: e.transpose(out=pW[i][:, 0, 64:128], in_=BLi[:, g, :], identity=ident_b[0:64, 0:64]),
                             reads=[B_BL, B_ident_b], writes=[B_pW[i]])
                        k.op("act", lambda e, i=i, g=g, d=d: e.copy(out=WinT[:, d, g, :], in_=pW[i][:, 0, :]), reads=[B_pW[i]], writes=[B_WinT])
                k.barrier()
            with ExitStack() as t0:
                Yt = sb(t0, "Yt", [128, 3, 4096], BF16); B_Yt = Buf()
                pX = [ps(t0, f"pX{i}", [64, 512]) for i in range(2)]; B_pX = [Buf(), Buf()]
                pY = ps(t0, "pY", [128, 512]); B_pY = Buf()
                pYt = ps(t0, "pYt", [128, 8, 128], BF16); B_pYt = Buf()
                XS = sb(t0, "XS", [64, 2, 288]); B_XS = Buf()
                base = sb(t0, "base", [64, 288]); sarg = sb(t0, "sarg", [64, 288]); B_tr = Buf(); B_tr2 = Buf()
                sargi = sb(t0, "sargi", [64, 288], mybir.dt.int32); sargf = sb(t0, "sargf", [64, 288])
                sn = sb(t0, "sn", [64, 288]); cs = sb(t0, "cs", [64, 288]); B_sc = Buf()
                RR = sb(t0, "RR", [64, 2, 288]); B_RR = Buf()
                q1 = sb(t0, "q1", [64, 288]); q2 = sb(t0, "q2", [64, 288]); B_q = Buf()
                SS = sb(t0, "SS", [64, 2, 288]); B_SS = Buf()
                Sp = sb(t0, "Sp", [64, 2, 2, 288], BF16); B_Sp = Buf()
                Ysb = sb(t0, "Ysb", [128, 288], BF16); B_Ysb = Buf()
                k.op("dve", lambda e: e.memset(Sp[:], 0.0), writes=[B_Sp])
                for g in range(32):
                    for d in range(2):
                        dg = d * 32 + g
                        for c2 in range(2):
                            k.op("pe", lambda e, c2=c2, d=d, g=g: e.matmul(pX[c2][:, 0:288], lhsT=WinT[:, d, g, c2 * 64:(c2 + 1) * 64], rhs=UT_all[:, g, :],
                                                                           start=True, stop=True), reads=[B_WinT, B_UT], writes=[B_pX[c2]])
                            if d == 0:
                                k.op("act", lambda e, c2=c2: e.copy(out=XS[:, c2, :], in_=pX[c2][:, 0:288]), reads=[B_pX[c2]], writes=[B_XS])
                            else:
                                k.op("act", lambda e, c2=c2: e.copy(out=XS[:, c2, 0:32], in_=pX[c2][:, 31::-1]), reads=[B_pX[c2]], writes=[B_XS])
                                k.op("act", lambda e, c2=c2: e.copy(out=XS[:, c2, 32:288], in_=pX[c2][:, 287:31:-1]), reads=[B_pX[c2]], writes=[B_XS])
                        k.op("dve", lambda e, dg=dg: e.tensor_scalar(out=base[:], in0=mrow[:], scalar1=th8[:, dg:dg + 1], scalar2=None, op0=ALU.mult),
                             reads=[B_mrow, B_th8], writes=[B_tr])
                        sincos(base[:], [64, 288], sn[:], cs[:], (sarg[:], sargi[:], sargf[:]), B_tr, B_sc, B_tr2)
                        dv = lambda fn: k.op("dve", fn, reads=[B_XS, B_sc, B_q, B_RR, B_SS, B_L], writes=[B_q, B_RR, B_SS])
                        dv(lambda e: e.tensor_tensor(out=q1[:], in0=cs[:], in1=XS[:, 0, :], op=ALU.mult))
                        dv(lambda e: e.tensor_tensor(out=q2[:], in0=sn[:], in1=XS[:, 1, :], op=ALU.mult))
                        dv(lambda e: e.tensor_tensor(out=q1[:], in0=q1[:], in1=q2[:], op=ALU.add))
                        dv(lambda e, dg=dg: e.tensor_tensor_scan(out=RR[:, 0, :], data0=MG[:, 15, dg:dg + 1].to_broadcast([64, 288]), data1=q1[:], initial=0.0, op0=ALU.mult, op1=ALU.add))
                        dv(lambda e: e.tensor_tensor(out=q1[:], in0=cs[:], in1=XS[:, 1, :], op=ALU.mult))
                        dv(lambda e: e.tensor_tensor(out=q2[:], in0=sn[:], in1=XS[:, 0, :], op=ALU.mult))
                        dv(lambda e: e.tensor_tensor(out=q1[:], in0=q1[:], in1=q2[:], op=ALU.subtract))
                        dv(lambda e, dg=dg: e.tensor_tensor_scan(out=RR[:, 1, :], data0=MG[:, 15, dg:dg + 1].to_broadcast([64, 288]), data1=q1[:], initial=0.0, op0=ALU.mult, op1=ALU.add))
                        dv(lambda e: e.tensor_tensor(out=q1[:], in0=cs[:], in1=RR[:, 0, :], op=ALU.mult))
                        dv(lambda e: e.tensor_tensor(out=q2[:], in0=sn[:], in1=RR[:, 1, :], op=ALU.mult))
                        dv(lambda e: e.tensor_tensor(out=SS[:, 0, :], in0=q1[:], in1=q2[:], op=ALU.subtract))
                        dv(lambda e: e.tensor_tensor(out=q1[:], in0=cs[:], in1=RR[:, 1, :], op=ALU.mult))
                        dv(lambda e: e.tensor_tensor(out=q2[:], in0=sn[:], in1=RR[:, 0, :], op=ALU.mult))
                        dv(lambda e: e.tensor_tensor(out=SS[:, 1, :], in0=q1[:], in1=q2[:], op=ALU.add))
                        for c2 in range(2):
                            if d == 0:
                                k.op("act", lambda e, c2=c2: e.copy(out=Sp[:, 0, c2, 1:288], in_=SS[:, c2, 0:287]), reads=[B_SS], writes=[B_Sp])
                            else:
                                k.op("act", lambda e, c2=c2: e.copy(out=Sp[:, 1, c2, 0:31], in_=SS[:, c2, 30::-1]), reads=[B_SS], writes=[B_Sp])
                                k.op("act", lambda e, c2=c2: e.copy(out=Sp[:, 1, c2, 32:288], in_=SS[:, c2, 286:30:-1]), reads=[B_SS], writes=[B_Sp])
                    k.op("pe", lambda e, g=g: e.matmul(pY[:, 0:288], lhsT=T_all[:, g, :], rhs=UT_all[:, g, :], start=True, stop=False),
                         reads=[B_T, B_UT], writes=[B_pY])
                    for d in range(2):
                        k.op("pe", lambda e, g=g, d=d: e.matmul(pY[:, 0:288], lhsT=COr[:, d, g, :], rhs=Sp[:, d, 0, :], start=False, stop=False),
                             reads=[B_CO, B_Sp], writes=[B_pY])
                        k.op("pe", lambda e, g=g, d=d: e.matmul(pY[:, 0:288], lhsT=COi[:, d, g, :], rhs=Sp[:, d, 1, :], start=False, stop=(d == 1)),
                             reads=[B_CO, B_Sp], writes=[B_pY])
                    k.op("act", lambda e: e.copy(out=Ysb[:], in_=pY[:, 0:288]), reads=[B_pY], writes=[B_Ysb])
                    for bi, (n0, nb) in enumerate(NB):
                        k.op("pe", lambda e, bi=bi, n0=n0, nb=nb: e.transpose(out=pYt[0:nb, bi, :], in_=Ysb[:, n0:n0 + nb], identity=ident_b[:]),
                             reads=[B_Ysb, B_ident_b], writes=[B_pYt])
                    for bi, (n0, nb) in enumerate(NB):
                        dst = Yt[0:nb, bi, :].rearrange("p (j f) -> p j f", j=8)[:, :, g * 16:(g + 1) * 16]
                        k.op("dve", lambda e, bi=bi, nb=nb, dst=dst: e.tensor_copy(out=dst, in_=pYt[0:nb, bi, :].rearrange("p (j c) -> p j c", j=8)),
                             reads=[B_pYt], writes=[B_Yt])
                y8v = y_scr.rearrange("(n j) f -> n (j f)", j=8)
                for bi, (n0, nb) in enumerate(NB):
                    k.dma("sp", y8v[n0:n0 + nb, :], Yt[0:nb, bi, :], reads=[B_Yt])
                k.barrier()


        def ssm_post(st, catT_, B_catT_):
            GC = 2.0 * math.sqrt(2.0 / math.pi)
            gw = sb(st, "gluw", [128, 4, 512], BF16); B_gw = Buf()
            k.dma("pool", gw[:], glu_w.rearrange("(k p) n -> p k n", p=128), writes=[B_gw])
            gb = sb(st, "glub", [128, 4]); B_gb = Buf()
            with nc.allow_non_contiguous_dma(reason="tiny bias"):
                k.dma("sp", gb[:], glu_b[0, :].rearrange("(c p) -> p c", p=128), writes=[B_gb])
            dbc = load_bc(st, "dskip", ssm_d[0:1, :], 512)
            yt = [sb(st, f"py{i}", [128, 512], BF16) for i in range(2)]; B_yt = [Buf(), Buf()]
            ut = [sb(st, f"pu{i}", [128, 512]) for i in range(2)]; B_ut = [Buf(), Buf()]
            xx = sb(st, "pxx", [128, 512]); B_xx = Buf()
            ww = sb(st, "pww", [128, 512]); B_ww = Buf()
            sg = sb(st, "psg", [128, 512]); B_sg = Buf()
            g_bf = sb(st, "pg_bf", [128, 512], BF16); B_g = Buf()
            gT = sb(st, "pgT", [128, 4, 128], BF16); B_gT = Buf()
            s2 = sb(st, "ps2", [128, 4, 128]); B_s2 = Buf()
            pGT = ps(st, "pGT", [128, 8, 128], BF16); B_pGT = Buf()
            pz = ps(st, "pz", [128, 4, 128]); B_pz = Buf()
            for ti in range(NTILE):
                i = ti % 2
                T0 = ti * 128
                row0 = T0 + CTX if ti < NLT else T0 - SEQ
                k.dma("sp", yt[i][:], y_scr[row0:row0 + 128, :], writes=[B_yt[i]])
                k.dma("sp", ut[i][:], u_scr[row0:row0 + 128, :], writes=[B_ut[i]])
                k.op("dve", lambda e, i=i: e.tensor_tensor(out=xx[:], in0=ut[i][:], in1=dbc[0][:], op=ALU.mult), reads=[B_ut[i], dbc[1]], writes=[B_xx])
                k.op("dve", lambda e, i=i: e.tensor_tensor(out=xx[:], in0=xx[:], in1=yt[i][:], op=ALU.add), reads=[B_xx, B_yt[i]], writes=[B_xx])
                k.op("pool", lambda e: e.tensor_tensor(out=ww[:], in0=xx[:], in1=xx[:], op=ALU.mult), reads=[B_xx], writes=[B_ww])
                k.op("pool", lambda e: e.tensor_scalar(out=ww[:], in0=ww[:], scalar1=0.044715, scalar2=1.0, op0=ALU.mult, op1=ALU.add), reads=[B_ww], writes=[B_ww])
                k.op("pool", lambda e: e.tensor_tensor(out=ww[:], in0=ww[:], in1=xx[:], op=ALU.mult), reads=[B_ww, B_xx], writes=[B_ww])
                k.op("act", lambda e: e.activation(out=sg[:], in_=ww[:], func=AF.Sigmoid, scale=GC), reads=[B_ww], writes=[B_sg])
                k.op("dve", lambda e: e.tensor_tensor(out=g_bf[:], in0=xx[:], in1=sg[:], op=ALU.mult), reads=[B_xx, B_sg], writes=[B_g])
                if "dbg_g" in dbg:
                    if ti == 0:
                        dbg_g = dscr("dbg_g", [NT, 512], BF16)
                    k.dma("sp", dbg_g[T0:T0 + 128, :], g_bf[:], reads=[B_g])
                for kc in range(4):
                    k.op("pe", lambda e, kc=kc: e.transpose(out=pGT[:, kc, :], in_=g_bf[:, kc * 128:(kc + 1) * 128], identity=ident_b[:]),
                         reads=[B_g, B_ident_b], writes=[B_pGT])
                k.op("act", lambda e: e.copy(out=gT[:], in_=pGT[:, 0:4, :]), reads=[B_pGT], writes=[B_gT])
                for n_ in range(4):
                    for kc in range(4):
                        k.op("pe", lambda e, n_=n_, kc=kc: e.matmul(pz[:, n_, :], lhsT=gw[:, kc, n_ * 128:(n_ + 1) * 128], rhs=gT[:, kc, :],
                                                                    start=(kc == 0), stop=(kc == 3)), reads=[B_gw, B_gT], writes=[B_pz])
                for n_ in range(4):
                    k.op("act", lambda e, n_=n_: e.activation(out=s2[:, n_, :], in_=pz[:, n_, :], func=AF.Sigmoid, bias=gb[:, n_:n_ + 1], scale=1.0),
                         reads=[B_pz, B_gb], writes=[B_s2])
                k.op("dve", lambda e, T0=T0: e.tensor_tensor(out=catT_[:, 4:8, T0:T0 + 128], in0=gT[:], in1=s2[:], op=ALU.mult),
                     reads=[B_gT, B_s2], writes=[B_catT_])
            if "dbg_cat" in dbg:
                dbg_cat = dscr("dbg_cat", [128, 8, NT], BF16)
                k.dma("sp", dbg_cat, catT_[:], reads=[B_catT_])
            k.barrier()


        def layer1_mixer():
            LAM_INIT = 0.8 - 0.6 * math.exp(-0.3 * 1)
            SC = 0.125
            with ExitStack() as L1:
                qT2 = sb(L1, "qT2", [128, 8, SEQ], BF16); B_q2 = Buf()
                kT2 = sb(L1, "kT2", [128, 8, NT], BF16); B_k2 = Buf()
                v1 = sb(L1, "v1", [128, NTILE, D], BF16); B_v1 = Buf()
                nmax = sb(L1, "nmax", [128, 32]); B_nmax = Buf()
                for st in phase("l1proj"):
                    w_bf = sb(st, "dif_w_bf", [128, 8, 3072], BF16); B_w = Buf()
                    for kc in range(8):
                        k.dma("pool", w_bf[:, kc, :], dif_w_in[kc * 128:(kc + 1) * 128, :], writes=[B_w])
                    sc1p = [mod_bc(st, f"l1sc1p_{r}", 1, r, 1, plus1=True) for r in range(2)]
                    sh1 = [mod_bc(st, f"l1sh1_{r}", 1, r, 0) for r in range(2)]
                    xt = [sb(st, f"l1xt{i}", [128, D]) for i in range(2)]; B_xt = [Buf(), Buf()]
                    tmpf = sb(st, "l1tmpf", [128, D]); B_tmpf = Buf()
                    h_bf = sb(st, "l1h_bf", [128, D], BF16); B_hbf = Buf()
                    hT = [sb(st, f"l1hT{i}", [128, 8, 128], BF16) for i in range(2)]; B_hT = [Buf(), Buf()]
                    rt = [sb(st, f"l1rt{i}", [128, 64]) for i in range(2)]; B_rt = [Buf(), Buf()]
                    t1 = sb(st, "l1rope_t1", [128, 512]); t2 = sb(st, "l1rope_t2", [128, 512]); B_rtmp = Buf()
                    qk_bf = sb(st, "l1qk_bf", [128, 512], BF16); B_qk = Buf()
                    pT = ps(st, "l1pT", [128, 8, 128], BF16); B_pT = Buf()
                    pp = [ps(st, f"l1pp{i}", [128, 512]) for i in range(3)]; B_pp = [Buf(), Buf(), Buf()]
                    pq = [ps(st, f"l1pq{i}", [128, 8, 128], BF16) for i in range(2)]; B_pq = [Buf(), Buf()]
                    sqt = sb(st, "l1sq", [128, 512]); rs8 = sb(st, "l1rs8", [128, 8]); B_sq = Buf()
                    k.op("dve", lambda e: e.memset(nmax[:], 0.0), writes=[B_nmax])
                    ib = 0
                    for ti in range(NTILE):
                        i = ti % 2
                        r = 0 if ti < NLT else 1
                        T0 = ti * 128
                        k.dma("sp", xt[i][:], src_rows(ti, 1), writes=[B_xt[i]])
                        if r == 0:
                            k.dma("sp", rt[i][:], rope_cs[T0:T0 + 128, :], writes=[B_rt[i]])
                        k.op("dve", lambda e, i=i, r=r: e.tensor_tensor(out=tmpf[:], in0=xt[i][:], in1=sc1p[r][0][:], op=ALU.mult),
                             reads=[B_xt[i], sc1p[r][1]], writes=[B_tmpf])
                        k.op("pool", lambda e, r=r: e.tensor_tensor(out=h_bf[:], in0=tmpf[:], in1=sh1[r][0][:], op=ALU.add),
                             reads=[B_tmpf, sh1[r][1]], writes=[B_hbf])
                        for kc in range(8):
                            k.op("pe", lambda e, kc=kc: e.transpose(out=pT[:, kc, :], in_=h_bf[:, kc * 128:(kc + 1) * 128], identity=ident_b[:]),
                                 reads=[B_hbf, B_ident_b], writes=[B_pT])
                        k.op("act", lambda e, i=i: e.copy(out=hT[i][:], in_=pT[:]), reads=[B_pT], writes=[B_hT[i]])
                        for cb in range(6):
                            if r == 1 and cb < 2:
                                continue
                            j = ib % 3; ib += 1
                            for kc in range(8):
                                k.op("pe", lambda e, kc=kc, j=j, cb=cb, i=i: e.matmul(
                                    pp[j][:], lhsT=hT[i][:, kc, :], rhs=w_bf[:, kc, cb * 512:(cb + 1) * 512],
                                    start=(kc == 0), stop=(kc == 7)), reads=[B_hT[i], B_w], writes=[B_pp[j]])
                            if cb >= 4:
                                c0 = (cb - 4) * 512
                                k.op("act", lambda e, j=j, ti=ti, c0=c0: e.copy(out=v1[:, ti, c0:c0 + 512], in_=pp[j][:]), reads=[B_pp[j]], writes=[B_v1])
                                continue
                            if r == 0:
                                rope_apply(None, pp[j][:], B_pp[j], qk_bf[:], B_qk, 8, rt[i], B_rt[i], t1[:], t2[:], B_rtmp)
                            else:
                                k.op("dve", lambda e, j=j: e.tensor_copy(out=qk_bf[:], in_=pp[j][:]), reads=[B_pp[j]], writes=[B_qk])
                            k.op("dve", lambda e: e.tensor_tensor(out=sqt[:], in0=qk_bf[:], in1=qk_bf[:], op=ALU.mult), reads=[B_qk, B_sq], writes=[B_sq])
                            k.op("dve", lambda e: e.tensor_reduce(out=rs8[:], in_=sqt[:].rearrange("p (m d) -> p m d", d=64), axis=AX.X, op=ALU.add), reads=[B_sq], writes=[B_sq])
                            k.op("dve", lambda e, cb=cb: e.tensor_tensor(out=nmax[:, cb * 8:(cb + 1) * 8], in0=nmax[:, cb * 8:(cb + 1) * 8], in1=rs8[:], op=ALU.max),
                                 reads=[B_sq, B_nmax], writes=[B_nmax])
                            jq = cb % 2
                            for hh in range(4):
                                k.op("pe", lambda e, hh=hh, jq=jq: e.transpose(out=pq[jq][:, hh, :], in_=qk_bf[:, hh * 128:(hh + 1) * 128], identity=ident_b[:]),
                                     reads=[B_qk, B_ident_b], writes=[B_pq[jq]])
                            dstT, BD = (qT2, B_q2) if cb < 2 else (kT2, B_k2)
                            h0 = (cb % 2) * 4
                            k.op("act", lambda e, jq=jq, dstT=dstT, h0=h0, T0=T0: e.copy(out=dstT[:, h0:h0 + 4, T0:T0 + 128], in_=pq[jq][:, 0:4, :]),
                                 reads=[B_pq[jq]], writes=[BD])
                    k.barrier()
                for st in phase("l1att"):
                    w_bf = sb(st, "difwo_bf", [128, 8, D], BF16); B_w = Buf()
                    for kc in range(8):
                        k.dma("pool", w_bf[:, kc, :], dif_w_out[kc * 128:(kc + 1) * 128, :], writes=[B_w])
                    g1 = mod_bc(st, "l1g1", 1, 0, 2)
                    lng = load_bc(st, "l1ln1g", ln1_g[1:2, :], D)
                    lnb = load_bc(st, "l1ln1b", ln1_b[1:2, :], D)
                    wk = ln_work(st, "l1e1")
                    xo = [sb(st, f"l1xo{i}", [128, D]) for i in range(2)]; B_xo = [Buf(), Buf()]
                    lam = sb(st, "lam", [128, 8]); B_lam = Buf()
                    lq = [load_bc(st, f"lq{i}", a[0:1, :], 64) for i, a in enumerate((lam_q1, lam_k1, lam_q2, lam_k2))]
                    ltmp = sb(st, "ltmp", [128, 64]); B_lt = Buf()
                    for i2 in range(2):
                        k.op("dve", lambda e, i2=i2: e.tensor_tensor(out=ltmp[:], in0=lq[2 * i2][0][:], in1=lq[2 * i2 + 1][0][:], op=ALU.mult),
                             reads=[lq[2 * i2][1], lq[2 * i2 + 1][1], B_lt], writes=[B_lt])
                        k.op("dve", lambda e, i2=i2: e.reduce_sum(out=lam[:, i2:i2 + 1], in_=ltmp[:], axis=AX.X), reads=[B_lt, B_lam], writes=[B_lam])
                    k.op("act", lambda e: e.activation(out=lam[:, 2:4], in_=lam[:, 0:2], func=AF.Exp), reads=[B_lam], writes=[B_lam])
                    k.op("dve", lambda e: e.tensor_tensor(out=lam[:, 4:5], in0=lam[:, 2:3], in1=lam[:, 3:4], op=ALU.subtract), reads=[B_lam], writes=[B_lam])
                    k.op("dve", lambda e: e.tensor_scalar(out=lam[:, 5:6], in0=lam[:, 4:5], scalar1=-1.0, scalar2=-LAM_INIT, op0=ALU.mult, op1=ALU.add), reads=[B_lam], writes=[B_lam])
                    sg_col = sb(st, "sg_col", [128, 1]); B_sg = Buf()
                    with nc.allow_non_contiguous_dma(reason="tiny"):
                        k.dma("sp", sg_col[:], subln_g[0, :].rearrange("(p x) -> p x", x=1), writes=[B_sg])
                    k.op("dve", lambda e: e.tensor_scalar(out=sg_col[:], in0=sg_col[:], scalar1=1.0 - LAM_INIT, scalar2=None, op0=ALU.mult), reads=[B_sg], writes=[B_sg])
                    ones_bf = sb(st, "ones_bf", [128, 128], BF16); B_ones = Buf()
                    k.op("dve", lambda e: e.memset(ones_bf[:], 1.0), writes=[B_ones])
                    negC = sb(st, "negC", [128, 16]); B_negC = Buf()
                    with ExitStack() as t0:
                        nb = sb(t0, "nmax_bf", [128, 32], BF16); B_nb = Buf()
                        k.op("dve", lambda e: e.tensor_scalar(out=nb[:], in0=nmax[:], scalar1=1.02, scalar2=None, op0=ALU.mult), reads=[B_nmax], writes=[B_nb])
                        pn = ps(t0, "pn", [16, 1024], BF16); B_pn = Buf()
                        k.op("pe", lambda e: e.transpose(out=pn[:, 0:128], in_=nb[:, 0:16], identity=ident_b[:]), reads=[B_nb, B_ident_b], writes=[B_pn])
                        k.op("pe", lambda e: e.transpose(out=pn[:, 128:256], in_=nb[:, 16:32], identity=ident_b[:]), reads=[B_nb, B_ident_b], writes=[B_pn])
                        r2 = sb(t0, "r2", [16, 8]); B_r2 = Buf()
                        k.op("dve", lambda e: e.reduce_max(out=r2[:, 0:1], in_=pn[:, 0:128], axis=AX.X), reads=[B_pn], writes=[B_r2])
                        k.op("dve", lambda e: e.reduce_max(out=r2[:, 1:2], in_=pn[:, 128:256], axis=AX.X), reads=[B_pn, B_r2], writes=[B_r2])
                        k.op("dve", lambda e: e.tensor_tensor(out=r2[:, 2:3], in0=r2[:, 0:1], in1=r2[:, 1:2], op=ALU.mult), reads=[B_r2], writes=[B_r2])
                        k.op("act", lambda e: e.sqrt(out=r2[:, 3:4], in_=r2[:, 2:3]), reads=[B_r2], writes=[B_r2])
                        k.op("dve", lambda e: e.tensor_scalar(out=r2[:, 4:5], in0=r2[:, 3:4], scalar1=-SC, scalar2=None, op0=ALU.mult), reads=[B_r2], writes=[B_r2])
                        dg = sb(t0, "dgC", [16, 16], BF16); B_dg = Buf()
                        k.op("dve", lambda e: e.tensor_scalar(out=dg[:], in0=ident_f[0:16, 0:16], scalar1=r2[:, 4:5], scalar2=None, op0=ALU.mult), reads=[B_r2, B_ident_f], writes=[B_dg])
                        pc = ps(t0, "pcb", [128, 512]); B_pc = Buf()
                        k.op("pe", lambda e: e.matmul(pc[:, 0:16], lhsT=ones_bf[0:16, :], rhs=dg[:], start=True, stop=True), reads=[B_ones, B_dg], writes=[B_pc])
                        k.op("dve", lambda e: e.tensor_copy(out=negC[:], in_=pc[:, 0:16]), reads=[B_pc], writes=[B_negC])
                        k.barrier()
                    ET = [sb(st, f"ET{i}", [128, 512], BF16) for i in range(4)]; B_ET = [Buf() for _ in range(4)]
                    aoT = sb(st, "aoT_all", [128, 8, 512], BF16); B_aoT = Buf()
                    rz = sb(st, "rz", [1, 2, 512]); B_rz = Buf()
                    rzb = sb(st, "rzb", [1, 4, 512], BF16); B_rzb = Buf()
                    bcs = sb(st, "bcs", [128, 512]); B_bcs = Buf()
                    oT = sb(st, "oT", [128, 512]); B_oT = Buf()
                    t5 = sb(st, "t5", [128, 512]); B_t5 = Buf()
                    sqb = sb(st, "sqb", [128, 512], BF16); B_sqb = Buf()
                    pS = [ps(st, f"pS{i}", [128, 512]) for i in range(2)]; B_pS = [Buf(), Buf()]
                    pO = [ps(st, f"pO{i}", [128, 512]) for i in range(2)]; B_pO = [Buf(), Buf()]
                    pZ = [ps(st, f"pZ{i}", [1, 512]) for i in range(2)]; B_pZ = [Buf(), Buf()]
                    pB = ps(st, "pB", [128, 512]); B_pB = Buf()
                    cnt = {"s": 0, "e": 0}

                    def bcast_row(hi, lo):
                        k.op("pe", lambda e: e.matmul(pB[:], lhsT=ones_bf[0:1, :], rhs=hi, start=True, stop=False), reads=[B_ones, B_rzb], writes=[B_pB])
                        k.op("pe", lambda e: e.matmul(pB[:], lhsT=ones_bf[0:1, :], rhs=lo, start=False, stop=True), reads=[B_ones, B_rzb], writes=[B_pB])
                        k.op("act", lambda e: e.copy(out=bcs[:], in_=pB[:]), reads=[B_pB], writes=[B_bcs])

                    def split_row(src, j):
                        k.op("dve", lambda e: e.tensor_copy(out=rzb[:, 2 * j, :], in_=src), reads=[B_rz], writes=[B_rzb])
                        k.op("dve", lambda e: e.tensor_tensor(out=rzb[:, 2 * j + 1, :], in0=src, in1=rzb[:, 2 * j, :], op=ALU.subtract), reads=[B_rz, B_rzb], writes=[B_rzb])

                    zs = sb(st, "zs", [1, 2, 512]); B_zs = Buf()

                    def qk_exp(Q0, h, c, b):
                        ps_ = slice(c * 64, (c + 1) * 64)
                        m = h * 2 + c
                        js = cnt["s"] % 2; cnt["s"] += 1
                        je = cnt["e"] % 4; cnt["e"] += 1
                        k.op("pe", lambda e: e.matmul(pS[js][:], lhsT=kT2[ps_, h, b * 128:(b + 1) * 128], rhs=qT2[ps_, h, Q0:Q0 + 512],
                                                      start=True, stop=True), reads=[B_k2, B_q2], writes=[B_pS[js]])
                        k.op("act", lambda e: e.activation(out=ET[je][:], in_=pS[js][:], func=AF.Exp, bias=negC[:, m:m + 1], scale=SC),
                             reads=[B_pS[js], B_negC], writes=[B_ET[je]])
                        return je

                    def pvz(h, c, b, je):
                        k.op("pe", lambda e: e.matmul(pO[c][:], lhsT=v1[:, b, h * 128:(h + 1) * 128], rhs=ET[je][:],
                                                      start=(b == 0), stop=(b == NTILE - 1)), reads=[B_v1, B_ET[je]], writes=[B_pO[c]])
                        if b == 0:
                            k.op("dve", lambda e: e.tensor_copy(out=Zacc[c][:], in_=ET[je][:]), reads=[B_ET[je]], writes=[B_Zacc[c]])
                        else:
                            k.op("dve", lambda e: e.tensor_tensor(out=Zacc[c][:], in0=Zacc[c][:], in1=ET[je][:], op=ALU.add), reads=[B_ET[je], B_Zacc[c]], writes=[B_Zacc[c]])

                    Zacc = [sb(st, f"Zacc{i}", [128, 512]) for i in range(2)]; B_Zacc = [Buf(), Buf()]
                    ones_f = sb(st, "ones_f", [128, 1]); B_onesf = Buf()
                    k.op("dve", lambda e: e.memset(ones_f[:], 1.0), writes=[B_onesf])

                    def bcast_recip(c):
                        k.op("pe", lambda e: e.matmul(pZ[c][:], lhsT=ones_f[:, 0:1], rhs=Zacc[c][:], start=True, stop=True), reads=[B_onesf, B_Zacc[c]], writes=[B_pZ[c]])
                        k.op("act", lambda e: e.copy(out=zs[:, c, :], in_=pZ[c][:]), reads=[B_pZ[c], B_zs], writes=[B_zs])
                        k.op("dve", lambda e: e.tensor_copy(out=rzb[:, 2 * c, :], in_=zs[:, c, :]), reads=[B_zs, B_rzb], writes=[B_rzb])
                        k.op("dve", lambda e: e.tensor_tensor(out=rzb[:, 2 * c + 1, :], in0=zs[:, c, :], in1=rzb[:, 2 * c, :], op=ALU.subtract), reads=[B_zs, B_rzb], writes=[B_rzb])
                        k.op("pe", lambda e: e.matmul(pB[:], lhsT=ones_bf[0:1, :], rhs=rzb[:, 2 * c, :], start=True, stop=False), reads=[B_ones, B_rzb], writes=[B_pB])
                        k.op("pe", lambda e: e.matmul(pB[:], lhsT=ones_bf[0:1, :], rhs=rzb[:, 2 * c + 1, :], start=False, stop=True), reads=[B_ones, B_rzb], writes=[B_pB])
                        k.op("dve", lambda e: e.reciprocal(out=bcs[:], in_=pB[:]), reads=[B_pB, B_bcs], writes=[B_bcs])

                    def epilogue(h):
                        bcast_recip(0)
                        k.op("dve", lambda e: e.tensor_tensor(out=oT[:], in0=pO[0][:], in1=bcs[:], op=ALU.mult), reads=[B_pO[0], B_bcs, B_oT], writes=[B_oT])
                        bcast_recip(1)
                        k.op("dve", lambda e: e.tensor_tensor(out=t5[:], in0=pO[1][:], in1=bcs[:], op=ALU.mult), reads=[B_pO[1], B_bcs, B_t5], writes=[B_t5])
                        k.op("dve", lambda e: e.scalar_tensor_tensor(out=oT[:], in0=t5[:], scalar=lam[:, 5:6], in1=oT[:], op0=ALU.mult, op1=ALU.add),
                             reads=[B_oT, B_t5, B_lam], writes=[B_oT])
                        k.op("dve", lambda e: e.tensor_tensor(out=sqb[:], in0=oT[:], in1=oT[:], op=ALU.mult), reads=[B_oT, B_sqb], writes=[B_sqb])
                        k.op("pe", lambda e: e.matmul(pZ[0][:], lhsT=ones_bf[:, 0:1], rhs=sqb[:], start=True, stop=True), reads=[B_ones, B_sqb], writes=[B_pZ[0]])
                        k.op("act", lambda e: e.activation(out=rz[:, 0, :], in_=pZ[0][:], func=AF.Ln, scale=1.0 / 128.0, bias=eps_t[0:1, :]), reads=[B_pZ[0], B_rz, B_eps], writes=[B_rz])
                        k.op("act", lambda e: e.activation(out=rz[:, 0, :], in_=rz[:, 0, :], func=AF.Exp, scale=-0.5), reads=[B_rz], writes=[B_rz])
                        split_row(rz[:, 0, :], 0)
                        bcast_row(rzb[:, 0, :], rzb[:, 1, :])
                        k.op("dve", lambda e: e.scalar_tensor_tensor(out=aoT[:, h, :], in0=oT[:], scalar=sg_col[:, 0:1], in1=bcs[:], op0=ALU.mult, op1=ALU.mult),
                             reads=[B_oT, B_sg, B_bcs], writes=[B_aoT])

                    eps_t = sb(st, "eps_t", [128, 1]); B_eps = Buf()
                    k.op("dve", lambda e: e.memset(eps_t[:], 1e-5), writes=[B_eps])
                    for qg in range(4):
                        Q0 = qg * 512
                        steps = [(h, c, b) for h in range(8) for c in range(2) for b in range(NTILE)]
                        je_next = qk_exp(Q0, *steps[0])
                        for si, (h, c, b) in enumerate(steps):
                            je_cur = je_next
                            if si + 1 < len(steps):
                                je_next = qk_exp(Q0, *steps[si + 1])
                            pvz(h, c, b, je_cur)
                            if c == 1 and b == NTILE - 1:
                                epilogue(h)
                        for tt in range(4):
                            ti = qg * 4 + tt
                            T0 = ti * 128
                            i = ti % 2
                            k.dma("sp", xo[i][:], src_rows(ti, 1), writes=[B_xo[i]])
                            for hf in range(2):
                                for h in range(8):
                                    k.op("pe", lambda e, h=h, hf=hf, tt=tt: e.matmul(pS[hf][:], lhsT=aoT[:, h, tt * 128:(tt + 1) * 128], rhs=w_bf[:, h, hf * 512:(hf + 1) * 512],
                                                                                start=(h == 0), stop=(h == 7)), reads=[B_aoT, B_w], writes=[B_pS[hf]])
                            ln_epilogue(wk, [pS[0][:], pS[1][:]], [B_pS[0], B_pS[1]], xo[i], B_xo[i], g1, lng, lnb, x1_scr[T0:T0 + 128, :])
                    k.barrier()

        with ExitStack() as L0:
            catT = sb(L0, "catT", [128, 8, NT], BF16); B_catT = Buf()
            LA = ExitStack()
            qT = sb(LA, "qT", [64, 8, NT], BF16); B_qT = Buf()
            kT = sb(LA, "kT", [64, 2, NT], BF16); B_kT = Buf()
            v_all = sb(LA, "v_all", [128, NTILE, 128], BF16); B_v = Buf()
            for st in phase("l0proj"):
                w_bf = sb(st, "w_in_bf", [128, 8, 1280], BF16); B_w = Buf()
                for kc in range(8):
                    k.dma("pool", w_bf[:, kc, :], w_in0[kc * 128:(kc + 1) * 128, :], writes=[B_w])
                sc1p = [None, None]; sh1 = [None, None]
                for r in range(2):
                    sh1[r] = mod_bc(st, f"sh1_{r}", 0, r, 0)
                    sc1p[r] = mod_bc(st, f"sc1p_{r}", 0, r, 1, plus1=True)
                xt = [sb(st, f"xt{i}", [128, D]) for i in range(2)]; B_xt = [Buf(), Buf()]
                tmpf = sb(st, "tmpf", [128, D]); B_tmpf = Buf()
                h_bf = sb(st, "h_bf", [128, D], BF16); B_hbf = Buf()
                hT = [sb(st, f"hT{i}", [128, 8, 128], BF16) for i in range(2)]; B_hT = [Buf(), Buf()]
                rt = [sb(st, f"rt{i}", [128, 64]) for i in range(2)]; B_rt = [Buf(), Buf()]
                t1 = sb(st, "rope_t1", [128, 640]); t2 = sb(st, "rope_t2", [128, 640]); B_rtmp = Buf()
                qk_bf = sb(st, "qk_bf", [128, 640], BF16); B_qk = Buf()
                ut = [sb(st, f"ut{i}", [128, 512]) for i in range(2)]; B_ut = [Buf(), Buf()]
                pT = ps(st, "pT", [128, 8, 128], BF16); B_pT = Buf()
                pp = [ps(st, f"pp{i}", [128, 512]) for i in range(3)]; B_pp = [Buf(), Buf(), Buf()]
                pq = ps(st, "pq", [64, 8, 128], BF16); B_pq = Buf()
                pk = ps(st, "pk", [64, 8, 128], BF16); B_pk = Buf()
                for ti in range(NTILE):
                    i = ti % 2
                    r = 0 if ti < NLT else 1
                    T0 = ti * 128
                    k.dma("sp", xt[i][:], src_rows(ti, 0), writes=[B_xt[i]])
                    if r == 0:
                        k.dma("sp", rt[i][:], rope_cs[T0:T0 + 128, :], writes=[B_rt[i]])
                    k.op("dve", lambda e, i=i, r=r: e.tensor_tensor(out=tmpf[:], in0=xt[i][:], in1=sc1p[r][0][:], op=ALU.mult),
                         reads=[B_xt[i], sc1p[r][1]], writes=[B_tmpf])
                    k.op("pool", lambda e, r=r: e.tensor_tensor(out=h_bf[:], in0=tmpf[:], in1=sh1[r][0][:], op=ALU.add),
                         reads=[B_tmpf, sh1[r][1]], writes=[B_hbf])
                    for kc in range(8):
                        k.op("pe", lambda e, kc=kc: e.transpose(out=pT[:, kc, :], in_=h_bf[:, kc * 128:(kc + 1) * 128], identity=ident_b[:]),
                             reads=[B_hbf, B_ident_b], writes=[B_pT])
                    k.op("act", lambda e, i=i: e.copy(out=hT[i][:], in_=pT[:]), reads=[B_pT], writes=[B_hT[i]])
                    for nb, (c0, c1) in enumerate(((0, 512), (512, 1024), (1024, 1280))):
                        for kc in range(8):
                            k.op("pe", lambda e, kc=kc, nb=nb, c0=c0, c1=c1, i=i: e.matmul(
                                pp[nb][:, 0:c1 - c0], lhsT=hT[i][:, kc, :], rhs=w_bf[:, kc, c0:c1],
                                start=(kc == 0), stop=(kc == 7)),
                                reads=[B_hT[i], B_w], writes=[B_pp[nb]])
                    if "dbg_q" in dbg:
                        if ti == 0:
                            dq = sb(st, "dq", [128, 1280]); B_dq = Buf()
                        for nb, (c0, c1) in enumerate(((0, 512), (512, 1024), (1024, 1280))):
                            k.op("dve", lambda e, nb=nb, c0=c0, c1=c1: e.tensor_copy(out=dq[:, c0:c1], in_=pp[nb][:, 0:c1 - c0]),
                                 reads=[B_pp[nb]], writes=[B_dq])
                        k.dma("sp", dbg_q[T0:T0 + 128, :], dq[:], reads=[B_dq])
                    if r == 0:
                        rope_apply(None, pp[0][:, 0:512], B_pp[0], qk_bf[:, 0:512], B_qk, 8, rt[i], B_rt[i], t1[:, 0:512], t2[:, 0:512], B_rtmp)
                        rope_apply(None, pp[1][:, 0:128], B_pp[1], qk_bf[:, 512:640], B_qk, 2, rt[i], B_rt[i], t1[:, 512:640], t2[:, 512:640], B_rtmp)
                    else:
                        k.op("dve", lambda e: e.tensor_copy(out=qk_bf[:, 0:512], in_=pp[0][:, 0:512]), reads=[B_pp[0]], writes=[B_qk])
                        k.op("dve", lambda e: e.tensor_copy(out=qk_bf[:, 512:640], in_=pp[1][:, 0:128]), reads=[B_pp[1]], writes=[B_qk])
                    for h in range(8):
                        k.op("pe", lambda e, h=h: e.transpose(out=pq[:, h, :], in_=qk_bf[:, h * 64:(h + 1) * 64], identity=ident_b[:]),
                             reads=[B_qk, B_ident_b], writes=[B_pq])
                    for h in range(2):
                        k.op("pe", lambda e, h=h: e.transpose(out=pk[:, h, :], in_=qk_bf[:, 512 + h * 64:512 + (h + 1) * 64], identity=ident_b[:]),
                             reads=[B_qk, B_ident_b], writes=[B_pk])
                    k.op("act", lambda e, T0=T0: e.copy(out=qT[:, :, T0:T0 + 128], in_=pq[:]), reads=[B_pq], writes=[B_qT])
                    k.op("act", lambda e, T0=T0: e.copy(out=kT[:, :, T0:T0 + 128], in_=pk[:, 0:2, :]), reads=[B_pk], writes=[B_kT])
                    k.op("act", lambda e, ti=ti: e.copy(out=v_all[:, ti, :], in_=pp[1][:, 128:256]), reads=[B_pp[1]], writes=[B_v])
                    k.op("act", lambda e, i=i: e.copy(out=ut[i][:, 0:256], in_=pp[1][:, 256:512]), reads=[B_pp[1]], writes=[B_ut[i]])
                    k.op("act", lambda e, i=i: e.copy(out=ut[i][:, 256:512], in_=pp[2][:, 0:256]), reads=[B_pp[2]], writes=[B_ut[i]])
                    urow = T0 + CTX if r == 0 else T0 - SEQ
                    k.dma("sp", u_scr[urow:urow + 128, :], ut[i][:], reads=[B_ut[i]])
                if "dbg_qT" in dbg:
                    dbg_qT = dscr("dbg_qT", [64, 8, NT], BF16)
                    k.dma("sp", dbg_qT, qT[:], reads=[B_qT])
                k.barrier()


            for st in phase("l0att"):
                SC = 0.125
                maskL = sb(st, "maskL_sb", [128, 128]); maskR = sb(st, "maskR_sb", [128, 128]); B_mask = Buf()
                k.dma("sp", maskL[:], maskL_in[:, :], writes=[B_mask])
                k.dma("sp", maskR[:], maskR_in[:, :], writes=[B_mask])
                sink_bc, B_sink = load_bc(st, "sink_bc", swa_sink[0:1, :], 8)
                sm = [sb(st, f"sm{i}", [128, 640]) for i in range(2)]; B_sm = [Buf(), Buf()]
                P = [sb(st, f"P{i}", [128, 640], BF16) for i in range(2)]; B_P = [Buf(), Buf()]
                PT = [sb(st, f"PT{i}", [128, 5, 128], BF16) for i in range(2)]; B_PT = [Buf(), Buf()]
                stat = [sb(st, f"stat{i}", [128, 8]) for i in range(2)]; B_stat = [Buf(), Buf()]
                att_bf = sb(st, "att_bf", [128, 512], BF16); B_att = Buf()
                ps_loc = [ps(st, f"ps_loc{i}", [128, 512]) for i in range(2)]; B_psl = [Buf(), Buf()]
                ps_ctx = [ps(st, f"ps_ctx{i}", [128, 512]) for i in range(2)]; B_psc = [Buf(), Buf()]
                pPT = ps(st, "pPT", [128, 8, 128], BF16); B_pPT = Buf()
                po = ps(st, "po", [128, 512]); B_po = Buf()
                pcat = ps(st, "pcat", [128, 8, 128], BF16); B_pcat = Buf()
                it = 0
                for ti in range(NTILE):
                    T0 = ti * 128
                    lat = ti < NLT
                    if lat:
                        j0 = max(0, ti - 1); j1 = min(NLT - 1, ti + 1)
                        nloc = (j1 - j0 + 1) * 128
                        blocks = list(range(j0, j1 + 1)) + [NLT, NLT + 1]
                    else:
                        nloc = 0
                        blocks = [NLT, NLT + 1]
                    n = nloc + 256
                    for h in range(8):
                        i = it % 2; it += 1
                        kvh = h // 4
                        if lat:
                            k.op("pe", lambda e, i=i, h=h, kvh=kvh, j0=j0, nloc=nloc, T0=T0: e.matmul(
                                ps_loc[i][:, 0:nloc], lhsT=qT[:, h, T0:T0 + 128], rhs=kT[:, kvh, j0 * 128:j0 * 128 + nloc],
                                start=True, stop=True), reads=[B_qT, B_kT], writes=[B_psl[i]])
                        k.op("pe", lambda e, i=i, h=h, kvh=kvh, T0=T0: e.matmul(
                            ps_ctx[i][:, 0:256], lhsT=qT[:, h, T0:T0 + 128], rhs=kT[:, kvh, SEQ:NT],
                            start=True, stop=True), reads=[B_qT, B_kT], writes=[B_psc[i]])
                        if lat:
                            for bi, j in enumerate(range(j0, j1 + 1)):
                                sl = slice(bi * 128, (bi + 1) * 128)
                                if j == ti:
                                    k.op("act", lambda e, i=i, sl=sl: e.mul(out=sm[i][:, sl], in_=ps_loc[i][:, sl], mul=SC),
                                         reads=[B_psl[i]], writes=[B_sm[i]])
                                else:
                                    mk = maskL if j < ti else maskR
                                    k.op("dve", lambda e, i=i, sl=sl, mk=mk: e.scalar_tensor_tensor(
                                        out=sm[i][:, sl], in0=ps_loc[i][:, sl], scalar=SC, in1=mk[:], op0=ALU.mult, op1=ALU.add),
                                        reads=[B_psl[i], B_mask], writes=[B_sm[i]])
                        k.op("act", lambda e, i=i, nloc=nloc: e.mul(out=sm[i][:, nloc:nloc + 256], in_=ps_ctx[i][:, 0:256], mul=SC),
                             reads=[B_psc[i]], writes=[B_sm[i]])
                        sti = stat[i]
                        k.op("dve", lambda e, i=i, n=n, sti=sti: e.reduce_max(out=sti[:, 0:1], in_=sm[i][:, 0:n], axis=AX.X),
                             reads=[B_sm[i]], writes=[B_stat[i]])
                        k.op("dve", lambda e, sti=sti, h=h: e.tensor_tensor(out=sti[:, 1:2], in0=sti[:, 0:1], in1=sink_bc[:, h:h + 1], op=ALU.max),
                             reads=[B_stat[i], B_sink], writes=[B_stat[i]])
                        k.op("dve", lambda e, sti=sti: e.tensor_scalar(out=sti[:, 2:3], in0=sti[:, 1:2], scalar1=-1.0, scalar2=None, op0=ALU.mult),
                             reads=[B_stat[i]], writes=[B_stat[i]])
                        k.op("act", lambda e, i=i, n=n, sti=sti: e.activation(out=P[i][:, 0:n], in_=sm[i][:, 0:n], func=AF.Exp,
                                                                             bias=sti[:, 2:3], scale=1.0, accum_out=sti[:, 3:4]),
                             reads=[B_sm[i], B_stat[i]], writes=[B_P[i], B_stat[i]])
                        k.op("act", lambda e, sti=sti, h=h: e.activation(out=sti[:, 4:5], in_=sink_bc[:, h:h + 1], func=AF.Exp,
                                                                        bias=sti[:, 2:3], scale=1.0),
                             reads=[B_sink, B_stat[i]], writes=[B_stat[i]])
                        k.op("dve", lambda e, sti=sti: e.tensor_tensor(out=sti[:, 5:6], in0=sti[:, 3:4], in1=sti[:, 4:5], op=ALU.add),
                             reads=[B_stat[i]], writes=[B_stat[i]])
                        k.op("dve", lambda e, sti=sti: e.reciprocal(out=sti[:, 6:7], in_=sti[:, 5:6]),
                             reads=[B_stat[i]], writes=[B_stat[i]])
                        nb = n // 128
                        for b in range(nb):
                            k.op("pe", lambda e, i=i, b=b: e.transpose(out=pPT[:, b, :], in_=P[i][:, b * 128:(b + 1) * 128], identity=ident_b[:]),
                                 reads=[B_P[i], B_ident_b], writes=[B_pPT])
                        k.op("pool" if False else "dve", lambda e, i=i, nb=nb: e.tensor_copy(out=PT[i][:, 0:nb, :], in_=pPT[:, 0:nb, :]),
                             reads=[B_pPT], writes=[B_PT[i]])
                        for b in range(nb):
                            k.op("pe", lambda e, i=i, b=b, h=h, kvh=kvh, vb=blocks[b], nb=nb: e.matmul(
                                po[:, h * 64:(h + 1) * 64], lhsT=PT[i][:, b, :], rhs=v_all[:, vb, kvh * 64:(kvh + 1) * 64],
                                start=(b == 0), stop=(b == nb - 1)), reads=[B_PT[i], B_v], writes=[B_po])
                        k.op("dve", lambda e, h=h, sti=sti: e.tensor_scalar(out=att_bf[:, h * 64:(h + 1) * 64], in0=po[:, h * 64:(h + 1) * 64],
                                                                           scalar1=sti[:, 6:7], scalar2=None, op0=ALU.mult),
                             reads=[B_po, B_stat[i]], writes=[B_att])
                    for cb in range(4):
                        k.op("pe", lambda e, cb=cb: e.transpose(out=pcat[:, cb, :], in_=att_bf[:, cb * 128:(cb + 1) * 128], identity=ident_b[:]),
                             reads=[B_att, B_ident_b], writes=[B_pcat])
                    k.op("act", lambda e, T0=T0: e.copy(out=catT[:, 0:4, T0:T0 + 128], in_=pcat[:, 0:4, :]), reads=[B_pcat], writes=[B_catT])
                    if "dbg_att" in dbg:
                        if ti == 0:
                            dbg_att = dscr("dbg_att", [NT, 512], BF16)
                        k.dma("sp", dbg_att[T0:T0 + 128, :], att_bf[:], reads=[B_att])
                k.barrier()


            k.barrier()
            LA.close()
            for st in phase("l0ssm"):
                ssm_phase(st, catT, B_catT)
            for st in phase("l0ssmpost"):
                ssm_post(st, catT, B_catT)

            for st in phase("l0out"):
                outproj_ln1(st, 0, catT, B_catT, w_out0, NTILE)


        for st in phase("moe0"):
            moe_phase(st, 0, NTILE, x2_scr)


        layer1_mixer()
        for st in phase("moe1"):
            moe_phase(st, 1, NLT, out)

        k.barrier()
    return nc


_CONSTS = None


def _consts():
    global _CONSTS
    if _CONSTS is None:
        t = np.arange(SEQ)
        row = (t // 64).astype(np.float32)
        col = (t % 64).astype(np.float32)
        inv = (10000.0 ** (-np.arange(16, dtype=np.float32) / 16)).astype(np.float32)
        ar = row[:, None] * inv[None, :]
        ac = col[:, None] * inv[None, :]
        rope = np.concatenate([np.cos(ar), np.sin(ar), np.cos(ac), np.sin(ac)], 1).astype(np.float32)
        qi = np.arange(128)[:, None]; kj = np.arange(128)[None, :]
        mL = np.where(kj >= qi, 0.0, -30000.0).astype(np.float32)
        mR = np.where(kj <= qi, 0.0, -30000.0).astype(np.float32)
        _CONSTS = {"rope_cs": rope, "ident": np.eye(128, dtype=np.float32), "maskL": mL, "maskR": mR}
        selm = np.zeros((32, 32, 128), np.float32)
        for e_ in range(32):
            selm[e_, e_, :] = 1.0
        _CONSTS["sel"] = selm
        _CONSTS["kval"] = np.ascontiguousarray(np.broadcast_to(np.repeat(np.arange(-7, 9, dtype=np.float32), 64)[None, :], (64, 1024)))
        _CONSTS["mrow"] = np.ascontiguousarray(np.broadcast_to(np.arange(288, dtype=np.float32)[None, :], (64, 288)))
        jj = np.arange(128) // 16
        _CONSTS["maskF"] = (jj[None, :] >= jj[:, None]).astype(np.float32)
        _CONSTS["maskB"] = (jj[None, :] <= jj[:, None]).astype(np.float32)
    return _CONSTS


def make_in_maps(inputs, cores):
    f = lambda a: np.ascontiguousarray(np.asarray(a, dtype=np.float32))
    shared = {}
    for name in ("mod_w", "mod_b", "ln1_g", "ln1_b", "ln2_g", "ln2_b", "swa_sink",
                 "ssm_d", "ssm_glu_b", "dif_lam_q1", "dif_lam_k1", "dif_lam_q2", "dif_lam_k2",
                 "dif_subln_g", "moe_wg", "moe_bg", "moe_we", "moe_w1", "moe_w3", "moe_w2"):
        shared[name] = f(inputs[name])
    for name in ("swa_ssm_w_in", "swa_ssm_w_out", "ssm_a_re", "ssm_a_im", "ssm_log_step",
                 "ssm_b_re", "ssm_b_im", "ssm_c_re", "ssm_c_im", "ssm_glu_w", "dif_w_in", "dif_w_out"):
        shared[name] = f(inputs[name])[0]
    shared["moe_be"] = f(inputs["moe_be"]).reshape(2, 32)
    shared["c_ctx"] = f(inputs["c_ctx"]).reshape(1, D)
    shared.update(_consts())
    maps = []
    for b in cores:
        m = dict(shared)
        m["x"] = f(inputs["x"][b])
        m["ctx"] = f(inputs["ctx"][b])
        m["c"] = f(inputs["c"][b]).reshape(1, D)
        maps.append(m)
    return maps


def kernel(**inputs):
    nc = build()
    maps = make_in_maps(inputs, range(8))
    res = run_bass_kernel_spmd(nc, maps, core_ids=list(range(8)))
    return np.stack([r["out"] for r in res.results], 0).astype(np.float32)
```

```python
import math
from contextlib import ExitStack

import numpy as np
import concourse.bass as bass
import concourse.mybir as mybir
from concourse.bass_utils import run_bass_kernel_spmd

F32 = mybir.dt.float32
BF16 = mybir.dt.bfloat16
AF = mybir.ActivationFunctionType
ALU = mybir.AluOpType
AX = mybir.AxisListType

D = 1024
SEQ = 2048
CTX = 256
NT = SEQ + CTX
NTILE = NT // 128
NLT = SEQ // 128
ALPHA = 4 ** 0.25
LN_EPS = 1e-5


class Buf:
    __slots__ = ("w", "r")

    def __init__(self):
        self.w = None
        self.r = {}


class EngState:
    def __init__(self, name, eng, sem):
        self.name = name
        self.eng = eng
        self.sem = sem
        self.count = 0
        self.waited = {}
        self.slots = []
        self.slot_i = 0


class K:
    def __init__(self, nc, stack):
        self.nc = nc
        self.E = {}
        for name, eng in (("pe", nc.tensor), ("dve", nc.vector), ("act", nc.scalar),
                          ("pool", nc.gpsimd), ("sp", nc.sync)):
            sem = stack.enter_context(nc.semaphore("s_" + name))
            self.E[name] = EngState(name, eng, sem)
        self.semkey = {}
        for qn, n in (("sp", 12), ("pool", 12), ("act", 6)):
            for i in range(n):
                sem = stack.enter_context(nc.semaphore(f"d_{qn}{i}"))
                self.E[qn].slots.append([sem, 0])
        self.uid = 0

    def _key(self, sem):
        return id(sem)

    def _wait(self, E, deps, skip_self=False):
        best = {}
        for sem, val in deps:
            if skip_self and sem is E.sem:
                continue
            k = id(sem)
            if k not in best or best[k][1] < val:
                best[k] = (sem, val)
        for k, (sem, val) in best.items():
            if E.waited.get(k, 0) < val:
                E.eng.wait_ge(sem, val)
                E.waited[k] = val

    def _deps(self, reads, writes):
        deps = []
        for b in reads:
            if b.w is not None:
                deps.append(b.w)
        for b in writes:
            if b.w is not None:
                deps.append(b.w)
            deps.extend(b.r.values())
        return deps

    def _mark(self, tok, reads, writes):
        sem, val = tok
        for b in reads:
            b.r[id(sem)] = tok
        for b in writes:
            b.w = tok
            b.r = {}

    def op(self, en, fn, reads=(), writes=()):
        E = self.E[en]
        self._wait(E, self._deps(reads, writes), skip_self=(en == "pe"))
        ins = fn(E.eng)
        E.count += 1
        ins.then_inc(E.sem, 1)
        tok = (E.sem, E.count)
        self._mark(tok, reads, writes)
        return tok

    def dma(self, qn, out, in_, reads=(), writes=(), **kw):
        E = self.E[qn]
        self._wait(E, self._deps(reads, writes))
        slot = E.slots[E.slot_i % len(E.slots)]
        E.slot_i += 1
        if slot[1] > 0:
            self._wait(E, [(slot[0], slot[1] * 16)])
        ins = E.eng.dma_start(out=out, in_=in_, **kw)
        slot[1] += 1
        ins.then_inc(slot[0], 16)
        tok = (slot[0], slot[1] * 16)
        self._mark(tok, reads, writes)
        return tok

    def all_tokens(self):
        toks = []
        for E in self.E.values():
            if E.count:
                toks.append((E.sem, E.count))
            for sem, c in E.slots:
                if c:
                    toks.append((sem, c * 16))
        return toks

    def barrier(self):
        toks = self.all_tokens()
        for E in self.E.values():
            self._wait(E, toks, skip_self=False)


def build(dbg=(), inject=(), phases=None):
    nc = bass.Bass("TRN2", target_bir_lowering=False)
    dbg = set(dbg)
    inject = set(inject)
    ALLP = {"mod", "l0proj", "l0att", "l0ssm", "l0ssmpost", "l0out", "moe0", "l1proj", "l1att", "moe1"}
    phases = ALLP if phases is None else set(phases)

    def din(name, shape):
        return nc.dram_tensor(name, list(shape), F32, kind="ExternalInput").ap()

    def dscr(name, shape, dt=F32):
        kind = "ExternalOutput" if name in dbg else ("ExternalInput" if name in inject else "Internal")
        return nc.dram_tensor(name, list(shape), dt, kind=kind).ap()

    x_in = din("x", [SEQ, D])
    ctx_in = din("ctx", [CTX, D])
    c_in = din("c", [1, D])
    cc_in = din("c_ctx", [1, D])
    mod_w = din("mod_w", [2, D, 6 * D])
    mod_b = din("mod_b", [2, 6 * D])
    ln1_g = din("ln1_g", [2, D]); ln1_b = din("ln1_b", [2, D])
    ln2_g = din("ln2_g", [2, D]); ln2_b = din("ln2_b", [2, D])
    w_in0 = din("swa_ssm_w_in", [D, 1280])
    w_out0 = din("swa_ssm_w_out", [D, D])
    swa_sink = din("swa_sink", [1, 8])
    a_re = din("ssm_a_re", [2, 32, 64]); a_im = din("ssm_a_im", [2, 32, 64])
    log_step = din("ssm_log_step", [2, 32])
    b_re = din("ssm_b_re", [2, 32, 64, 16]); b_im = din("ssm_b_im", [2, 32, 64, 16])
    c_re = din("ssm_c_re", [2, 32, 16, 64]); c_im = din("ssm_c_im", [2, 32, 16, 64])
    ssm_d = din("ssm_d", [1, 512])
    glu_w = din("ssm_glu_w", [512, 512]); glu_b = din("ssm_glu_b", [1, 512])
    dif_w_in = din("dif_w_in", [D, 3072]); dif_w_out = din("dif_w_out", [D, D])
    lam_q1 = din("dif_lam_q1", [1, 64]); lam_k1 = din("dif_lam_k1", [1, 64])
    lam_q2 = din("dif_lam_q2", [1, 64]); lam_k2 = din("dif_lam_k2", [1, 64])
    subln_g = din("dif_subln_g", [1, 128])
    moe_wg = din("moe_wg", [2, D, 4]); moe_bg = din("moe_bg", [2, 4])
    moe_we = din("moe_we", [2, 4, D, 8]); moe_be = din("moe_be", [2, 32])
    moe_w1 = din("moe_w1", [2, 32, D, 256]); moe_w3 = din("moe_w3", [2, 32, D, 256])
    moe_w2 = din("moe_w2", [2, 32, 256, D])
    rope_cs = din("rope_cs", [SEQ, 64])
    ident_in = din("ident", [128, 128])
    sel_in = din("sel", [32, 32, 128])
    kval_in = din("kval", [64, 1024]); mrow_in = din("mrow", [64, 288])
    maskF_in = din("maskF", [128, 128]); maskB_in = din("maskB", [128, 128])
    maskL_in = din("maskL", [128, 128]); maskR_in = din("maskR", [128, 128])
    out = nc.dram_tensor("out", [SEQ, D], F32, kind="ExternalOutput").ap()

    modrow = dscr("modrow", [2, 2, 6 * D])

    with ExitStack() as gs:
        k = K(nc, gs)

        def sb(st, name, shape, dt=F32):
            k.uid += 1
            return st.enter_context(nc.sbuf_tensor(f"sb{k.uid}_{name}", list(shape), dt))

        def ps(st, name, shape, dt=F32):
            k.uid += 1
            return st.enter_context(nc.psum_tensor(f"ps{k.uid}_{name}", list(shape), dt))

        def phase(name):
            if name in phases:
                with ExitStack() as st_:
                    yield st_

        ident_f = sb(gs, "ident_f", [128, 128]); B_ident_f = Buf()
        ident_b = sb(gs, "ident_b", [128, 128], BF16); B_ident_b = Buf()
        k.dma("sp", ident_f[:], ident_in[:, :], writes=[B_ident_f])
        k.op("dve", lambda e: e.tensor_copy(out=ident_b[:], in_=ident_f[:]),
             reads=[B_ident_f], writes=[B_ident_b])

        for st in phase("mod"):
            cT = sb(st, "cT", [128, 8, 2]); B_cT = Buf()
            with nc.allow_non_contiguous_dma(reason="tiny column loads"):
                k.dma("sp", cT[:, :, 0], c_in[0, :].rearrange("(k p) -> p k", p=128), writes=[B_cT])
                k.dma("sp", cT[:, :, 1], cc_in[0, :].rearrange("(k p) -> p k", p=128), writes=[B_cT])
            sT = sb(st, "sT", [128, 8, 2]); B_sT = Buf()
            k.op("act", lambda e: e.activation(out=sT[:], in_=cT[:], func=AF.Silu),
                 reads=[B_cT], writes=[B_sT])
            wt = [sb(st, f"modw{i}", [128, 8, 512]) for i in range(2)]
            B_wt = [Buf(), Buf()]
            mb = sb(st, "modb", [2, 6 * D]); B_mb = Buf()
            mrow = sb(st, "mrow", [2, 6 * D]); B_mrow = Buf()
            pm = [ps(st, f"pmod{i}", [2, 512]) for i in range(2)]
            B_pm = [Buf(), Buf()]
            it = 0
            for l in range(2):
                k.dma("sp", mb[0:1, :], mod_b[l:l + 1, :], writes=[B_mb])
                k.dma("sp", mb[1:2, :], mod_b[l:l + 1, :], writes=[B_mb])
                for cb in range(12):
                    i = it % 2
                    it += 1
                    k.dma("sp" if cb % 2 == 0 else "act", wt[i][:],
                          mod_w[l, :, cb * 512:(cb + 1) * 512].rearrange("(k p) n -> p k n", p=128),
                          writes=[B_wt[i]])
                    for kc in range(8):
                        k.op("pe", lambda e, kc=kc, i=i: e.matmul(
                            pm[i][:], lhsT=sT[:, kc, :], rhs=wt[i][:, kc, :],
                            start=(kc == 0), stop=(kc == 7)),
                            reads=[B_sT, B_wt[i]], writes=[B_pm[i]])
                    k.op("dve", lambda e, i=i, cb=cb: e.tensor_tensor(
                        out=mrow[:, cb * 512:(cb + 1) * 512], in0=pm[i][:],
                        in1=mb[:, cb * 512:(cb + 1) * 512], op=ALU.add),
                        reads=[B_pm[i], B_mb], writes=[B_mrow])
                k.dma("sp", modrow[l], mrow[:], reads=[B_mrow], writes=[])
            k.barrier()


        def load_bc(st, name, src_row_ap, n, q="sp"):
            t = sb(st, name, [128, n]); B = Buf()
            k.dma(q, t[:], src_row_ap.partition_broadcast(128), writes=[B])
            return t, B

        def mod_bc(st, name, l, r, chunk, plus1=False):
            t, B = load_bc(st, name, modrow[l, r:r + 1, chunk * D:(chunk + 1) * D], D)
            if plus1:
                k.op("pool", lambda e: e.tensor_scalar(out=t[:], in0=t[:], scalar1=1.0, scalar2=None,
                                                       op0=ALU.add), reads=[B], writes=[B])
            return t, B

        u_scr = dscr("u_scr", [NT, 512])
        y_scr = dscr("y_scr", [NT, 512], BF16)
        x1_scr = dscr("x1_scr", [NT, D])
        x2_scr = dscr("x2_scr", [NT, D])
        dbg_q = dscr("dbg_q", [NT, 1280])

        def src_rows(ti, l):
            if l == 0:
                return x_in[ti * 128:(ti + 1) * 128, :] if ti < NLT else ctx_in[(ti - NLT) * 128:(ti - NLT + 1) * 128, :]
            return x2_scr[ti * 128:(ti + 1) * 128, :]

        def rope_apply(st_bufs, src_ps, B_src, dst, B_dst, nh, rt, B_rt, tmp1, tmp2, B_tmp):
            S = src_ps.rearrange("p (h a b f) -> p h a b f", h=nh, a=2, b=2, f=16)
            O = dst.rearrange("p (h a b f) -> p h a b f", h=nh, a=2, b=2, f=16)
            T1 = tmp1.rearrange("p (h a b f) -> p h a b f", h=nh, a=2, b=2, f=16)
            T2 = tmp2.rearrange("p (h a b f) -> p h a b f", h=nh, a=2, b=2, f=16)
            for a in range(2):
                cos = rt[:, a * 32:a * 32 + 16].rearrange("p (x y f) -> p x y f", x=1, y=1).to_broadcast([128, nh, 2, 16])
                sin = rt[:, a * 32 + 16:a * 32 + 32].rearrange("p (x y f) -> p x y f", x=1, y=1).to_broadcast([128, nh, 2, 16])
                k.op("dve", lambda e, a=a, cos=cos: e.tensor_tensor(out=T1[:, :, a], in0=S[:, :, a], in1=cos, op=ALU.mult),
                     reads=[B_src, B_rt], writes=[B_tmp])
                k.op("dve", lambda e, a=a, sin=sin: e.tensor_tensor(out=T2[:, :, a], in0=S[:, :, a, ::-1, :], in1=sin, op=ALU.mult),
                     reads=[B_src, B_rt], writes=[B_tmp])
                k.op("dve", lambda e, a=a: e.tensor_tensor(out=O[:, :, a, 0, :], in0=T1[:, :, a, 0, :], in1=T2[:, :, a, 0, :], op=ALU.subtract),
                     reads=[B_tmp], writes=[B_dst])
                k.op("dve", lambda e, a=a: e.tensor_tensor(out=O[:, :, a, 1, :], in0=T1[:, :, a, 1, :], in1=T2[:, :, a, 1, :], op=ALU.add),
                     reads=[B_tmp], writes=[B_dst])


        def ln_epilogue(wk, y_parts, B_y, xo, B_xo, g_t, lng_t, lnb_t, dst_rows):
            tmp, B_tmp, z, B_z, stt, B_stt, o, B_o = wk
            for hf in range(2):
                sl = slice(hf * 512, (hf + 1) * 512)
                k.op("dve", lambda e, hf=hf, sl=sl: e.tensor_tensor(out=tmp[:, sl], in0=y_parts[hf], in1=g_t[0][:, sl], op=ALU.mult),
                     reads=[B_y[hf], g_t[1]], writes=[B_tmp])
            k.op("dve", lambda e: e.scalar_tensor_tensor(out=z[:], in0=xo[:], scalar=ALPHA, in1=tmp[:], op0=ALU.mult, op1=ALU.add),
                 reads=[B_xo, B_tmp], writes=[B_z])
            for hf in range(2):
                k.op("dve", lambda e, hf=hf: e.bn_stats(out=stt[:, hf * 6:(hf + 1) * 6], in_=z[:, hf * 512:(hf + 1) * 512]),
                     reads=[B_z], writes=[B_stt])
            k.op("dve", lambda e: e.bn_aggr(out=stt[:, 12:14], in_=stt[:, 0:12]), reads=[B_stt], writes=[B_stt])
            k.op("dve", lambda e: e.tensor_scalar(out=stt[:, 15:16], in0=stt[:, 13:14], scalar1=LN_EPS, scalar2=None, op0=ALU.add),
                 reads=[B_stt], writes=[B_stt])
            k.op("act", lambda e: e.sqrt(out=stt[:, 15:16], in_=stt[:, 15:16]), reads=[B_stt], writes=[B_stt])
            k.op("dve", lambda e: e.reciprocal(out=stt[:, 14:15], in_=stt[:, 15:16]), reads=[B_stt], writes=[B_stt])
            k.op("dve", lambda e: e.tensor_scalar(out=tmp[:], in0=z[:], scalar1=stt[:, 12:13], scalar2=stt[:, 14:15], op0=ALU.subtract, op1=ALU.mult),
                 reads=[B_z, B_stt], writes=[B_tmp])
            k.op("pool", lambda e: e.tensor_tensor(out=o[:], in0=tmp[:], in1=lng_t[0][:], op=ALU.mult),
                 reads=[B_tmp, lng_t[1]], writes=[B_o])
            k.op("pool", lambda e: e.tensor_tensor(out=o[:], in0=o[:], in1=lnb_t[0][:], op=ALU.add),
                 reads=[B_o, lnb_t[1]], writes=[B_o])
            k.dma("sp", dst_rows, o[:], reads=[B_o])

        def ln_work(st, pfx):
            tmp = sb(st, pfx + "_tmp", [128, D]); z = sb(st, pfx + "_z", [128, D])
            stt = sb(st, pfx + "_stt", [128, 16]); o = sb(st, pfx + "_o", [128, D])
            return (tmp, Buf(), z, Buf(), stt, Buf(), o, Buf())

        def outproj_ln1(st, l, catT_, B_catT_, w_out_dram, ntiles):
            w_bf = sb(st, "w_out_bf", [128, 8, D], BF16); B_w = Buf()
            for kc in range(8):
                k.dma("pool", w_bf[:, kc, :], w_out_dram[kc * 128:(kc + 1) * 128, :], writes=[B_w])
            g1 = [mod_bc(st, f"g1_{r}", l, r, 2) for r in range(2)]
            lng = load_bc(st, "ln1g", ln1_g[l:l + 1, :], D)
            lnb = load_bc(st, "ln1b", ln1_b[l:l + 1, :], D)
            wk = ln_work(st, "e1")
            xo = [sb(st, f"xo{i}", [128, D]) for i in range(2)]; B_xo = [Buf(), Buf()]
            py = [ps(st, f"py{i}", [128, 512]) for i in range(4)]; B_py = [Buf() for _ in range(4)]
            for ti in range(ntiles):
                i = ti % 2
                r = 0 if ti < NLT else 1
                T0 = ti * 128
                k.dma("sp", xo[i][:], src_rows(ti, l), writes=[B_xo[i]])
                for hf in range(2):
                    pi = i * 2 + hf
                    for kc in range(8):
                        k.op("pe", lambda e, kc=kc, hf=hf, pi=pi, T0=T0: e.matmul(
                            py[pi][:], lhsT=catT_[:, kc, T0:T0 + 128], rhs=w_bf[:, kc, hf * 512:(hf + 1) * 512],
                            start=(kc == 0), stop=(kc == 7)), reads=[B_catT_, B_w], writes=[B_py[pi]])
                ln_epilogue(wk, [py[i * 2][:], py[i * 2 + 1][:]], [B_py[i * 2], B_py[i * 2 + 1]], xo[i], B_xo[i],
                            g1[r], lng, lnb, x1_scr[T0:T0 + 128, :])
            k.barrier()

        def moe_phase(st, l, ntiles, dst):
            ntok = ntiles * 128
            h2T = sb(st, "h2T", [128, 8, ntok], BF16); B_h2T = Buf()
            gateT = sb(st, "gateT", [32, ntok], BF16); B_gateT = Buf()
            f_acc = sb(st, "f_acc", [128, ntiles, D]); B_facc = [Buf() for _ in range(ntiles)]
            sel = sb(st, "sel", [32, 32, 128], BF16); B_sel = Buf()
            k.dma("pool", sel[:], sel_in[:, :, :], writes=[B_sel])
            with ExitStack() as s1:
                Wr = sb(s1, "Wr", [128, 8, 36]); B_Wr = Buf()
                with nc.allow_non_contiguous_dma(reason="small router weights"):
                    k.dma("sp", Wr[:, :, 0:4], moe_wg[l].rearrange("(k p) n -> p k n", p=128), writes=[B_Wr])
                    for g in range(4):
                        k.dma("sp", Wr[:, :, 4 + g * 8:12 + g * 8], moe_we[l, g].rearrange("(k p) n -> p k n", p=128), writes=[B_Wr])
                Whi = sb(s1, "Whi", [128, 8, 36], BF16); Wlo = sb(s1, "Wlo", [128, 8, 36], BF16); B_Wsp = Buf()
                k.op("dve", lambda e: e.tensor_copy(out=Whi[:], in_=Wr[:]), reads=[B_Wr], writes=[B_Wsp])
                k.op("dve", lambda e: e.tensor_tensor(out=Wlo[:], in0=Wr[:], in1=Whi[:], op=ALU.subtract), reads=[B_Wr, B_Wsp], writes=[B_Wsp])
                rb = sb(s1, "rb", [128, 36]); B_rb = Buf()
                k.dma("sp", rb[:, 0:4], moe_bg[l:l + 1, :].partition_broadcast(128), writes=[B_rb])
                k.dma("sp", rb[:, 4:36], moe_be[l:l + 1, :].partition_broadcast(128), writes=[B_rb])
                sc2p = [mod_bc(s1, f"sc2p_{r}", l, r, 4, plus1=True) for r in range(2)]
                sh2 = [mod_bc(s1, f"sh2_{r}", l, r, 3) for r in range(2)]
                xt = [sb(s1, f"mx{i}", [128, D]) for i in range(2)]; B_xt = [Buf(), Buf()]
                hf32 = sb(s1, "mh", [128, D]); B_h = Buf()
                hhi = sb(s1, "hhi", [128, D], BF16); hlo = sb(s1, "hlo", [128, D], BF16); B_hs = Buf()
                hloT = sb(s1, "hloT", [128, 8, 128], BF16); B_hloT = Buf()
                pTh = ps(s1, "mpTh", [128, 8, 128], BF16); B_pTh = Buf()
                pTl = ps(s1, "mpTl", [128, 8, 128], BF16); B_pTl = Buf()
                pr = ps(s1, "mpr", [128, 512]); B_pr = Buf()
                pg = ps(s1, "mpg", [32, 1024], BF16); B_pg = Buf()
                lg = sb(s1, "lg", [128, 36]); B_lg = Buf()
                sm = sb(s1, "rsm", [128, 160]); B_sm = Buf()
                gates = sb(s1, "gates", [128, 32]); B_gates = Buf()
                gates_bf = sb(s1, "gates_bf", [128, 32], BF16); B_gbf = Buf()
                for ti in range(ntiles):
                    i = ti % 2
                    r = 0 if ti < NLT else 1
                    T0 = ti * 128
                    k.dma("sp", xt[i][:], x1_scr[T0:T0 + 128, :], writes=[B_xt[i]])
                    k.op("dve", lambda e, i=i, r=r: e.tensor_tensor(out=hf32[:], in0=xt[i][:], in1=sc2p[r][0][:], op=ALU.mult),
                         reads=[B_xt[i], sc2p[r][1]], writes=[B_h])
                    k.op("pool", lambda e, r=r: e.tensor_tensor(out=hf32[:], in0=hf32[:], in1=sh2[r][0][:], op=ALU.add),
                         reads=[B_h, sh2[r][1]], writes=[B_h])
                    k.op("pool", lambda e: e.tensor_copy(out=hhi[:], in_=hf32[:]), reads=[B_h], writes=[B_hs])
                    k.op("dve", lambda e: e.tensor_tensor(out=hlo[:], in0=hf32[:], in1=hhi[:], op=ALU.subtract), reads=[B_h, B_hs], writes=[B_hs])
                    for kc in range(8):
                        k.op("pe", lambda e, kc=kc: e.transpose(out=pTh[:, kc, :], in_=hhi[:, kc * 128:(kc + 1) * 128], identity=ident_b[:]),
                             reads=[B_hs, B_ident_b], writes=[B_pTh])
                    for kc in range(8):
                        k.op("pe", lambda e, kc=kc: e.transpose(out=pTl[:, kc, :], in_=hlo[:, kc * 128:(kc + 1) * 128], identity=ident_b[:]),
                             reads=[B_hs, B_ident_b], writes=[B_pTl])
                    k.op("act", lambda e, T0=T0: e.copy(out=h2T[:, :, T0:T0 + 128], in_=pTh[:]), reads=[B_pTh], writes=[B_h2T])
                    k.op("dve", lambda e: e.tensor_copy(out=hloT[:], in_=pTl[:]), reads=[B_pTl], writes=[B_hloT])
                    n_mm = 24
                    j = 0
                    for (A, BA, W) in ((None, B_h2T, Whi), (hloT, B_hloT, Whi), (None, B_h2T, Wlo)):
                        for kc in range(8):
                            lhs = h2T[:, kc, T0:T0 + 128] if A is None else A[:, kc, :]
                            k.op("pe", lambda e, lhs=lhs, W=W, kc=kc, j=j: e.matmul(pr[:, 0:36], lhsT=lhs, rhs=W[:, kc, :], start=(j == 0), stop=(j == 23)),
                                 reads=[BA, B_Wsp], writes=[B_pr])
                            j += 1
                    R = [B_lg, B_sm]
                    def dv(fn, reads=R, writes=(B_sm,)):
                        k.op("dve", fn, reads=list(reads), writes=list(writes))
                    k.op("dve", lambda e: e.tensor_tensor(out=lg[:], in0=pr[:, 0:36], in1=rb[:], op=ALU.add), reads=[B_pr, B_rb], writes=[B_lg])
                    dv(lambda e: e.reduce_max(out=sm[:, 0:1], in_=lg[:, 0:4], axis=AX.X))
                    dv(lambda e: e.tensor_scalar(out=sm[:, 1:2], in0=sm[:, 0:1], scalar1=-1.0, scalar2=None, op0=ALU.mult))
                    k.op("act", lambda e: e.activation(out=sm[:, 56:60], in_=lg[:, 0:4], func=AF.Exp, bias=sm[:, 1:2], scale=1.0, accum_out=sm[:, 2:3]),
                         reads=R, writes=[B_sm])
                    dv(lambda e: e.reciprocal(out=sm[:, 3:4], in_=sm[:, 2:3]))
                    dv(lambda e: e.tensor_scalar(out=sm[:, 4:8], in0=lg[:, 0:4], scalar1=sm[:, 0:1], scalar2=None, op0=ALU.is_equal))
                    le = lg[:, 4:36].rearrange("p (g e) -> p g e", g=4)
                    tmp48 = sm[:, 64:96].rearrange("p (g e) -> p g e", g=4)
                    ohb = sm[:, 4:8].rearrange("p (g x) -> p g x", x=1).to_broadcast([128, 4, 8])
                    dv(lambda e: e.tensor_tensor(out=tmp48, in0=le, in1=ohb, op=ALU.mult))
                    dv(lambda e: e.tensor_reduce(out=sm[:, 8:16], in_=sm[:, 64:96].rearrange("p (g e) -> p e g", g=4), axis=AX.X, op=ALU.add))
                    dv(lambda e: e.reduce_max(out=sm[:, 16:17], in_=sm[:, 8:16], axis=AX.X))
                    dv(lambda e: e.tensor_scalar(out=sm[:, 24:32], in0=sm[:, 8:16], scalar1=sm[:, 16:17], scalar2=None, op0=ALU.is_equal))
                    dv(lambda e: e.scalar_tensor_tensor(out=sm[:, 32:40], in0=sm[:, 24:32], scalar=-1e30, in1=sm[:, 8:16], op0=ALU.mult, op1=ALU.add))
                    dv(lambda e: e.reduce_max(out=sm[:, 17:18], in_=sm[:, 32:40], axis=AX.X))
                    dv(lambda e: e.tensor_scalar(out=sm[:, 40:48], in0=sm[:, 32:40], scalar1=sm[:, 17:18], scalar2=None, op0=ALU.is_equal))
                    dv(lambda e: e.tensor_tensor(out=sm[:, 18:19], in0=sm[:, 17:18], in1=sm[:, 16:17], op=ALU.subtract))
                    k.op("act", lambda e: e.activation(out=sm[:, 19:20], in_=sm[:, 18:19], func=AF.Exp), reads=R, writes=[B_sm])
                    dv(lambda e: e.tensor_scalar(out=sm[:, 20:21], in0=sm[:, 19:20], scalar1=1.0, scalar2=None, op0=ALU.add))
                    dv(lambda e: e.reciprocal(out=sm[:, 20:21], in_=sm[:, 20:21]))
                    dv(lambda e: e.tensor_tensor(out=sm[:, 21:22], in0=sm[:, 19:20], in1=sm[:, 20:21], op=ALU.mult))
                    dv(lambda e: e.tensor_tensor(out=sm[:, 22:23], in0=sm[:, 20:21], in1=sm[:, 3:4], op=ALU.mult))
                    dv(lambda e: e.tensor_tensor(out=sm[:, 23:24], in0=sm[:, 21:22], in1=sm[:, 3:4], op=ALU.mult))
                    dv(lambda e: e.tensor_scalar(out=sm[:, 48:56], in0=sm[:, 24:32], scalar1=sm[:, 22:23], scalar2=None, op0=ALU.mult))
                    dv(lambda e: e.scalar_tensor_tensor(out=sm[:, 48:56], in0=sm[:, 40:48], scalar=sm[:, 23:24], in1=sm[:, 48:56], op0=ALU.mult, op1=ALU.add))
                    geb = sm[:, 48:56].rearrange("p (x e) -> p x e", x=1).to_broadcast([128, 4, 8])
                    k.op("dve", lambda e: e.tensor_tensor(out=gates[:].rearrange("p (g e) -> p g e", g=4), in0=ohb, in1=geb, op=ALU.mult),
                         reads=R, writes=[B_gates])
                    k.op("dve", lambda e: e.tensor_copy(out=gates_bf[:], in_=gates[:]), reads=[B_gates], writes=[B_gbf])
                    k.op("pe", lambda e: e.transpose(out=pg[:, 0:128], in_=gates_bf[:], identity=ident_b[:]),
                         reads=[B_gbf, B_ident_b], writes=[B_pg])
                    k.op("act", lambda e, T0=T0: e.copy(out=gateT[:, T0:T0 + 128], in_=pg[:, 0:128]), reads=[B_pg], writes=[B_gateT])
                    if "dbg_gates" in dbg:
                        if ti == 0:
                            dbg_gates = dscr("dbg_gates", [NT, 32])
                        k.dma("sp", dbg_gates[T0:T0 + 128, :], gates[:], reads=[B_gates])
                k.barrier()
            if "stop_router" in dbg:
                return
            with ExitStack() as s2:
                w13 = [sb(s2, f"w13_{i}", [128, 8, 512], BF16) for i in range(2)]; B_w13 = [Buf(), Buf()]
                w2 = [sb(s2, f"w2_{i}", [128, 2, D], BF16) for i in range(2)]; B_w2 = [Buf(), Buf()]
                hh = [sb(s2, f"hh{i}", [128, 2, ntok], BF16) for i in range(2)]; B_hh = [Buf(), Buf()]
                stg13 = sb(s2, "stg13", [128, 8, 512]); B_stg13 = Buf()
                stg2 = sb(s2, "stg2", [128, 2, D]); B_stg2 = Buf()
                s1t = [sb(s2, f"s1t{i}", [128, 512]) for i in range(2)]; B_s1t = [Buf(), Buf()]
                t3 = [sb(s2, f"t3{i}", [128, 512]) for i in range(2)]; B_t3 = [Buf(), Buf()]
                ph1 = [ps(s2, f"ph1_{i}", [128, 512]) for i in range(2)]; B_ph1 = [Buf(), Buf()]
                ph3 = [ps(s2, f"ph3_{i}", [128, 512]) for i in range(2)]; B_ph3 = [Buf(), Buf()]
                pgbs = [ps(s2, f"pgb{i}", [128, 512]) for i in range(2)]; B_pgbs = [Buf(), Buf()]
                ibk = 0
                pf = [ps(s2, f"pf{i}", [128, 512]) for i in range(2)]; B_pf = [Buf(), Buf()]
                blocks = [(b0, min(512, ntok - b0)) for b0 in range(0, ntok, 512)]
                it = 0; itf = 0

                def w_stage13(ee):
                    k.dma("sp", stg13[:, :, 0:256], moe_w1[l, ee].rearrange("(k p) f -> p k f", p=128), writes=[B_stg13])
                    k.dma("sp", stg13[:, :, 256:512], moe_w3[l, ee].rearrange("(k p) f -> p k f", p=128), writes=[B_stg13])

                def w_stage2(ee):
                    k.dma("sp", stg2[:], moe_w2[l, ee].rearrange("(c p) d -> p c d", p=128), writes=[B_stg2])

                def w_cast13(ee):
                    wj = ee % 2
                    k.op("pool", lambda e: e.tensor_copy(out=w13[wj][:], in_=stg13[:]), reads=[B_stg13], writes=[B_w13[wj]])

                def w_cast2(ee):
                    wj = ee % 2
                    k.op("pool", lambda e: e.tensor_copy(out=w2[wj][:], in_=stg2[:]), reads=[B_stg2], writes=[B_w2[wj]])

                def w_tail(e_):
                    if e_ + 1 < 32:
                        w_cast2(e_ + 1)
                    if e_ + 2 < 32:
                        w_stage2(e_ + 2)
                for e_ in range(32):
                    wi = e_ % 2
                    if e_ == 0:
                        w_stage13(0); w_stage2(0); w_cast13(0); w_cast2(0); w_stage13(1); w_stage2(1)
                    if e_ + 1 < 32:
                        w_cast13(e_ + 1)
                    if e_ + 2 < 32:
                        w_stage13(e_ + 2)
                    for (b0, bn) in blocks:
                        pgb = pgbs[ibk % 2]; B_pgb = B_pgbs[ibk % 2]; ibk += 1
                        k.op("pe", lambda e, e_=e_, b0=b0, bn=bn, pgb=pgb: e.matmul(pgb[:, 0:bn], lhsT=sel[:, e_, :], rhs=gateT[:, b0:b0 + bn], start=True, stop=True),
                             reads=[B_sel, B_gateT], writes=[B_pgb])
                        for fc in range(2):
                            i = it % 2; it += 1
                            for kc in range(8):
                                k.op("pe", lambda e, kc=kc, fc=fc, i=i, wi=wi, b0=b0, bn=bn: e.matmul(
                                    ph1[i][:, 0:bn], lhsT=w13[wi][:, kc, fc * 128:(fc + 1) * 128], rhs=h2T[:, kc, b0:b0 + bn],
                                    start=(kc == 0), stop=(kc == 7)), reads=[B_w13[wi], B_h2T], writes=[B_ph1[i]])
                            for kc in range(8):
                                k.op("pe", lambda e, kc=kc, fc=fc, i=i, wi=wi, b0=b0, bn=bn: e.matmul(
                                    ph3[i][:, 0:bn], lhsT=w13[wi][:, kc, 256 + fc * 128:256 + (fc + 1) * 128], rhs=h2T[:, kc, b0:b0 + bn],
                                    start=(kc == 0), stop=(kc == 7)), reads=[B_w13[wi], B_h2T], writes=[B_ph3[i]])
                            k.op("act", lambda e, i=i, bn=bn: e.activation(out=s1t[i][:, 0:bn], in_=ph1[i][:, 0:bn], func=AF.Silu),
                                 reads=[B_ph1[i]], writes=[B_s1t[i]])
                            k.op("dve", lambda e, i=i, bn=bn: e.tensor_tensor(out=t3[i][:, 0:bn], in0=s1t[i][:, 0:bn], in1=ph3[i][:, 0:bn], op=ALU.mult),
                                 reads=[B_s1t[i], B_ph3[i]], writes=[B_t3[i]])
                            k.op("dve", lambda e, i=i, bn=bn, fc=fc, wi=wi, b0=b0, pgb=pgb: e.tensor_tensor(out=hh[wi][:, fc, b0:b0 + bn], in0=t3[i][:, 0:bn], in1=pgb[:, 0:bn], op=ALU.mult),
                                 reads=[B_t3[i], B_pgb], writes=[B_hh[wi]])
                    if e_ % 2 == 0:
                        w_tail(e_)
                        continue
                    for tt in range(ntiles):
                        for dc in range(2):
                            j = itf % 2; itf += 1
                            for q_ in range(4):
                                wq = q_ // 2; fc = q_ % 2
                                k.op("pe", lambda e, fc=fc, j=j, wq=wq, tt=tt, dc=dc, q_=q_: e.matmul(
                                    pf[j][:], lhsT=hh[wq][:, fc, tt * 128:(tt + 1) * 128], rhs=w2[wq][:, fc, dc * 512:(dc + 1) * 512],
                                    start=(q_ == 0), stop=(q_ == 3)), reads=[B_hh[wq], B_w2[wq]], writes=[B_pf[j]])
                            if e_ == 1:
                                k.op("dve", lambda e, j=j, tt=tt, dc=dc: e.tensor_copy(out=f_acc[:, tt, dc * 512:(dc + 1) * 512], in_=pf[j][:]),
                                     reads=[B_pf[j]], writes=[B_facc[tt]])
                            else:
                                k.op("dve", lambda e, j=j, tt=tt, dc=dc: e.tensor_tensor(out=f_acc[:, tt, dc * 512:(dc + 1) * 512],
                                     in0=f_acc[:, tt, dc * 512:(dc + 1) * 512], in1=pf[j][:], op=ALU.add),
                                     reads=[B_pf[j], B_facc[tt]], writes=[B_facc[tt]])
                    w_tail(e_)
                k.barrier()
            if "stop_experts" in dbg:
                return
            with ExitStack() as s3:
                g2 = [mod_bc(s3, f"g2_{r}", l, r, 5) for r in range(2)]
                lng = load_bc(s3, "ln2g", ln2_g[l:l + 1, :], D)
                lnb = load_bc(s3, "ln2b", ln2_b[l:l + 1, :], D)
                wk = ln_work(s3, "e2")
                xo = [sb(s3, f"x1o{i}", [128, D]) for i in range(2)]; B_xo = [Buf(), Buf()]
                for ti in range(ntiles):
                    i = ti % 2
                    r = 0 if ti < NLT else 1
                    T0 = ti * 128
                    k.dma("sp", xo[i][:], x1_scr[T0:T0 + 128, :], writes=[B_xo[i]])
                    if "dbg_f" in dbg:
                        if ti == 0:
                            dbg_f = dscr("dbg_f", [NT, D])
                        k.dma("sp", dbg_f[T0:T0 + 128, :], f_acc[:, ti, :], reads=[B_facc[ti]])
                    ln_epilogue(wk, [f_acc[:, ti, 0:512], f_acc[:, ti, 512:1024]], [B_facc[ti], B_facc[ti]], xo[i], B_xo[i],
                                g2[r], lng, lnb, dst[T0:T0 + 128, :])
                k.barrier()


        def ssm_phase(st, catT_, B_catT_):
            PI = math.pi
            TWO_PI = 2.0 * math.pi
            def bc3(ap2, n):
                P_, G_ = ap2.shape
                return ap2.rearrange("p (g x) -> p g x", x=1).to_broadcast([P_, G_, n])
            I32 = mybir.dt.int32
            INV2PI = 1.0 / TWO_PI

            def sincos(ang_ap, shape, s_out, c_out, tmps, B_in, B_out, B_tmp):
                y, yi, yf = tmps
                dvt = lambda fn: k.op("dve", fn, reads=[B_in, B_tmp, B_out], writes=[B_tmp])
                dvt(lambda e: e.tensor_scalar(out=y, in0=ang_ap, scalar1=INV2PI, scalar2=32.5, op0=ALU.mult, op1=ALU.add))
                dvt(lambda e: e.tensor_copy(out=yi, in_=y))
                dvt(lambda e: e.tensor_copy(out=yf, in_=yi))
                dvt(lambda e: e.tensor_tensor(out=y, in0=y, in1=yf, op=ALU.subtract))
                dvt(lambda e: e.scalar_tensor_tensor(out=yf, in0=y, scalar=0.0, in1=y, op0=ALU.is_lt, op1=ALU.add))
                k.op("act", lambda e: e.activation(out=s_out, in_=yf, func=AF.Sin, bias=negpi[0:shape[0], :], scale=TWO_PI),
                     reads=[B_tmp, B_np], writes=[B_out])
                dvt(lambda e: e.tensor_scalar(out=y, in0=yf, scalar1=0.25, scalar2=None, op0=ALU.add))
                dvt(lambda e: e.scalar_tensor_tensor(out=yf, in0=y, scalar=1.0, in1=y, op0=ALU.is_ge, op1=ALU.subtract))
                k.op("act", lambda e: e.activation(out=c_out, in_=yf, func=AF.Sin, bias=negpi[0:shape[0], :], scale=-TWO_PI),
                     reads=[B_tmp, B_np], writes=[B_out])

            ar = sb(st, "ar", [64, 64]); ai = sb(st, "ai", [64, 64]); ls = sb(st, "ls", [64, 64]); B_par = Buf()
            with nc.allow_non_contiguous_dma(reason="ssm params"):
                k.dma("sp", ar[:], a_re.rearrange("d g p -> p (d g)"), writes=[B_par])
                k.dma("sp", ai[:], a_im.rearrange("d g p -> p (d g)"), writes=[B_par])
            k.dma("sp", ls[:], log_step.rearrange("(x d) g -> x (d g)", x=1).partition_broadcast(64), writes=[B_par])
            negpi = sb(st, "negpi", [128, 1]); B_np = Buf()
            k.op("dve", lambda e: e.memset(negpi[:], -PI), writes=[B_np])
            kv = sb(st, "kv", [64, 16, 64]); B_kv = Buf()
            k.dma("sp", kv[:], kval_in.rearrange("p (k g) -> p k g", k=16), writes=[B_kv])
            mrow = sb(st, "mrow", [64, 288]); B_mrow = Buf()
            k.dma("sp", mrow[:], mrow_in[:, :], writes=[B_mrow])
            maskF = sb(st, "maskF", [128, 128]); maskB = sb(st, "maskB", [128, 128]); B_mk = Buf()
            k.dma("sp", maskF[:], maskF_in[:, :], writes=[B_mk])
            k.dma("sp", maskB[:], maskB_in[:, :], writes=[B_mk])
            dar = sb(st, "dar", [64, 64]); dai = sb(st, "dai", [64, 64]); B_d = Buf()
            k.op("act", lambda e: e.activation(out=ls[:], in_=ls[:], func=AF.Exp), reads=[B_par], writes=[B_par])
            k.op("dve", lambda e: e.tensor_tensor(out=dar[:], in0=ls[:], in1=ar[:], op=ALU.mult), reads=[B_par], writes=[B_d])
            k.op("dve", lambda e: e.tensor_tensor(out=dai[:], in0=ls[:], in1=ai[:], op=ALU.mult), reads=[B_par, B_d], writes=[B_d])
            LR = sb(st, "LR", [64, 16, 64]); LI = sb(st, "LI", [64, 16, 64]); MG = sb(st, "MG", [64, 16, 64]); B_L = Buf()
            th8 = sb(st, "th8", [64, 64]); B_th8 = Buf()
            k.op("dve", lambda e: e.tensor_scalar(out=th8[:], in0=dai[:], scalar1=8.0, scalar2=None, op0=ALU.mult), reads=[B_d], writes=[B_th8])
            with ExitStack() as t0:
                ang = sb(t0, "ang", [64, 16, 64]); a2 = sb(t0, "a2", [64, 16, 64]); B_ang = Buf()
                dai_b = dai[:].rearrange("p (x g) -> p x g", x=1).to_broadcast([64, 16, 64])
                dar_b = dar[:].rearrange("p (x g) -> p x g", x=1).to_broadcast([64, 16, 64])
                k.op("dve", lambda e: e.tensor_tensor(out=MG[:], in0=kv[:], in1=dar_b, op=ALU.mult), reads=[B_kv, B_d], writes=[B_L])
                k.op("act", lambda e: e.activation(out=MG[:], in_=MG[:], func=AF.Exp), reads=[B_L], writes=[B_L])
                k.op("dve", lambda e: e.tensor_tensor(out=ang[:], in0=kv[:], in1=dai_b, op=ALU.mult), reads=[B_kv, B_d], writes=[B_ang])
                a3 = sb(t0, "a3", [64, 16, 64], I32); a4 = sb(t0, "a4", [64, 16, 64])
                sincos(ang[:], [64, 16, 64], LI[:], LR[:], (a2[:], a3[:], a4[:]), B_ang, B_L, B_ang)
                k.op("dve", lambda e: e.tensor_tensor(out=LR[:], in0=LR[:], in1=MG[:], op=ALU.mult), reads=[B_L], writes=[B_L])
                k.op("dve", lambda e: e.tensor_tensor(out=LI[:], in0=LI[:], in1=MG[:], op=ALU.mult), reads=[B_L], writes=[B_L])
                k.barrier()
            cre = sb(st, "cre", [64, 64]); cim = sb(st, "cim", [64, 64]); B_c = Buf()
            with ExitStack() as t0:
                nr = sb(t0, "nr", [64, 64]); den = sb(t0, "den", [64, 64]); tq = sb(t0, "tq", [64, 64]); B_t = Buf()
                L1r = LR[:, 8, :]; L1i = LI[:, 8, :]
                dv = lambda fn: k.op("dve", fn, reads=[B_t, B_L, B_par, B_c], writes=[B_t, B_c])
                dv(lambda e: e.tensor_scalar(out=nr[:], in0=L1r, scalar1=-1.0, scalar2=None, op0=ALU.add))
                dv(lambda e: e.tensor_tensor(out=den[:], in0=ar[:], in1=ar[:], op=ALU.mult))
                dv(lambda e: e.tensor_tensor(out=tq[:], in0=ai[:], in1=ai[:], op=ALU.mult))
                dv(lambda e: e.tensor_tensor(out=den[:], in0=den[:], in1=tq[:], op=ALU.add))
                dv(lambda e: e.reciprocal(out=den[:], in_=den[:]))
                dv(lambda e: e.tensor_tensor(out=cre[:], in0=nr[:], in1=ar[:], op=ALU.mult))
                dv(lambda e: e.tensor_tensor(out=tq[:], in0=L1i, in1=ai[:], op=ALU.mult))
                dv(lambda e: e.tensor_tensor(out=cre[:], in0=cre[:], in1=tq[:], op=ALU.add))
                dv(lambda e: e.tensor_tensor(out=cre[:], in0=cre[:], in1=den[:], op=ALU.mult))
                dv(lambda e: e.tensor_tensor(out=cim[:], in0=L1i, in1=ar[:], op=ALU.mult))
                dv(lambda e: e.tensor_tensor(out=tq[:], in0=nr[:], in1=ai[:], op=ALU.mult))
                dv(lambda e: e.tensor_tensor(out=cim[:], in0=cim[:], in1=tq[:], op=ALU.subtract))
                dv(lambda e: e.tensor_tensor(out=cim[:], in0=cim[:], in1=den[:], op=ALU.mult))
                k.barrier()
            UT_all = sb(st, "UT_all", [128, 32, 288], BF16); B_UT = Buf()
            NB = ((0, 128), (128, 128), (256, 32))
            u8v = u_scr.rearrange("(n j) f -> n (j f)", j=8)
            with ExitStack() as t0:
                U8 = sb(t0, "U8", [128, 3, 4096]); B_U8 = Buf()
                U8b = sb(t0, "U8b", [128, 3, 4096], BF16); B_U8b = Buf()
                pU = [ps(t0, f"pU{i}", [128, 1024], BF16) for i in range(2)]; B_pU = [Buf(), Buf()]
                for bi, (n0, nb) in enumerate(NB):
                    k.dma("sp", U8[0:nb, bi, :], u8v[n0:n0 + nb, :], writes=[B_U8])
                    ov = U8b[0:nb, bi, :].rearrange("p (g j c) -> p j g c", g=32, j=8, c=16)
                    iv = U8[0:nb, bi, :].rearrange("p (j g c) -> p j g c", g=32, j=8, c=16)
                    k.op("pool" if bi == 1 else "act", (lambda e, ov=ov, iv=iv: e.tensor_copy(out=ov, in_=iv)) if bi == 1 else
                         (lambda e, ov=ov, iv=iv: e.copy(out=ov, in_=iv)), reads=[B_U8], writes=[B_U8b])
                for g in range(32):
                    i = g % 2
                    for bi, (n0, nb) in enumerate(NB):
                        src = U8b[0:nb, bi, g * 128:(g + 1) * 128]
                        k.op("pe", lambda e, i=i, src=src, n0=n0, nb=nb: e.transpose(out=pU[i][:, n0:n0 + nb], in_=src, identity=ident_b[0:nb, 0:nb]),
                             reads=[B_U8b, B_ident_b], writes=[B_pU[i]])
                    k.op("act", lambda e, i=i, g=g: e.copy(out=UT_all[:, g, :], in_=pU[i][:, 0:288]), reads=[B_pU[i]], writes=[B_UT])
                k.barrier()
            COr = sb(st, "COr", [64, 2, 32, 128], BF16); COi = sb(st, "COi", [64, 2, 32, 128], BF16); B_CO = Buf()
            T_all = sb(st, "T_all", [128, 32, 128], BF16); B_T = Buf()
            WinT = sb(st, "WinT", [128, 2, 32, 128], BF16); B_WinT = Buf()
            with ExitStack() as t0:
                BLr = sb(t0, "BLr", [64, 32, 128], BF16); BLi = sb(t0, "BLi", [64, 32, 128], BF16); B_BL = Buf()
                CTr = sb(t0, "CTr", [64, 32, 128], BF16); CTi = sb(t0, "CTi", [64, 32, 128], BF16); B_CT = Buf()
                Br = sb(t0, "Br", [64, 64, 16]); Bi = sb(t0, "Bi", [64, 64, 16]); B_B = Buf()
                Cr = sb(t0, "Cr", [64, 64, 16]); Ci = sb(t0, "Ci", [64, 64, 16]); B_C = Buf()
                Bbr = sb(t0, "Bbr", [64, 64, 16]); Bbi = sb(t0, "Bbi", [64, 64, 16]); B_Bb = Buf()
                with nc.allow_non_contiguous_dma(reason="ssm B/C tables"):
                    for d in range(2):
                        k.dma("sp", Br[:, d * 32:(d + 1) * 32, :], b_re[d].rearrange("g p c -> p g c"), writes=[B_B])
                        k.dma("sp", Bi[:, d * 32:(d + 1) * 32, :], b_im[d].rearrange("g p c -> p g c"), writes=[B_B])
                        for gb in range(4):
                            sl = slice(d * 32 + gb * 8, d * 32 + gb * 8 + 8)
                            k.dma("sp", Cr[:, sl, :], c_re[d, gb * 8:(gb + 1) * 8].rearrange("g c p -> p g c"), writes=[B_C])
                            k.dma("act", Ci[:, sl, :], c_im[d, gb * 8:(gb + 1) * 8].rearrange("g c p -> p g c"), writes=[B_C])
                NLR = sb(t0, "NLR", [64, 16, 64]); NLI = sb(t0, "NLI", [64, 16, 64])
                k.op("dve", lambda e: e.tensor_scalar(out=NLR[:], in0=LR[:], scalar1=-1.0, scalar2=None, op0=ALU.mult), reads=[B_L], writes=[B_L])
                k.op("dve", lambda e: e.tensor_scalar(out=NLI[:], in0=LI[:], scalar1=-1.0, scalar2=None, op0=ALU.mult), reads=[B_L], writes=[B_L])
                ta = sb(t0, "ta", [64, 32, 16]); tb_ = sb(t0, "tb", [64, 32, 16]); B_tab = Buf()
                tc_ = sb(t0, "tc", [64, 32, 16]); td_ = sb(t0, "td", [64, 32, 16]); B_tcd = Buf()
                for d in range(2):
                    dsl = slice(d * 32, (d + 1) * 32)
                    creb = bc3(cre[:, dsl], 16); cimb = bc3(cim[:, dsl], 16)
                    dv = lambda fn: k.op("dve", fn, reads=[B_B, B_c, B_tab, B_Bb], writes=[B_tab, B_Bb])
                    dv(lambda e, dsl=dsl, creb=creb: e.tensor_tensor(out=ta[:], in0=Br[:, dsl, :], in1=creb, op=ALU.mult))
                    dv(lambda e, dsl=dsl, cimb=cimb: e.tensor_tensor(out=tb_[:], in0=Bi[:, dsl, :], in1=cimb, op=ALU.mult))
                    dv(lambda e, dsl=dsl: e.tensor_tensor(out=Bbr[:, dsl, :], in0=ta[:], in1=tb_[:], op=ALU.subtract))
                    dv(lambda e, dsl=dsl, creb=creb: e.tensor_tensor(out=ta[:], in0=Bi[:, dsl, :], in1=creb, op=ALU.mult))
                    dv(lambda e, dsl=dsl, cimb=cimb: e.tensor_tensor(out=tb_[:], in0=Br[:, dsl, :], in1=cimb, op=ALU.mult))
                    dv(lambda e, dsl=dsl: e.tensor_tensor(out=Bbi[:, dsl, :], in0=ta[:], in1=tb_[:], op=ALU.add))
                pTd = [ps(t0, f"pTd{i}", [128, 512]) for i in range(2)]; B_pTd = [Buf(), Buf()]
                pW = [ps(t0, f"pW{i}", [128, 8, 128], BF16) for i in range(2)]; B_pW = [Buf(), Buf()]
                tt = sb(t0, "tt", [128, 128]); B_tt = Buf()
                for d in range(2):
                    dsl = slice(d * 32, (d + 1) * 32)
                    for j in range(8):
                        e_ = (7 - j) if d == 0 else j
                        lr = bc3(LR[:, e_ + 7, dsl], 16); li = bc3(LI[:, e_ + 7, dsl], 16)
                        o_r = BLr[:, :, j * 16:(j + 1) * 16]; o_i = BLi[:, :, j * 16:(j + 1) * 16]
                        dv2 = lambda fn: k.op("dve", fn, reads=[B_Bb, B_L, B_tab, B_BL], writes=[B_tab, B_BL])
                        dv2(lambda e, lr=lr: e.tensor_tensor(out=ta[:], in0=Bbr[:, dsl, :], in1=lr, op=ALU.mult))
                        dv2(lambda e, li=li: e.tensor_tensor(out=tb_[:], in0=Bbi[:, dsl, :], in1=li, op=ALU.mult))
                        dv2(lambda e, o_r=o_r: e.tensor_tensor(out=o_r, in0=ta[:], in1=tb_[:], op=ALU.subtract))
                        dv2(lambda e, lr=lr: e.tensor_tensor(out=ta[:], in0=Bbi[:, dsl, :], in1=lr, op=ALU.mult))
                        dv2(lambda e, li=li: e.tensor_tensor(out=tb_[:], in0=Bbr[:, dsl, :], in1=li, op=ALU.mult))
                        dv2(lambda e, o_i=o_i: e.tensor_tensor(out=o_i, in0=ta[:], in1=tb_[:], op=ALU.add))
                        f_ = (j - 7) if d == 0 else -j
                        for (kk, o_r, o_i, BO) in ((f_ + 7, CTr[:, :, j * 16:(j + 1) * 16], CTi[:, :, j * 16:(j + 1) * 16], B_CT),
                                                   (f_ + 15, COr[:, d, :, j * 16:(j + 1) * 16], COi[:, d, :, j * 16:(j + 1) * 16], B_CO)):
                            lr = bc3(LR[:, kk, dsl], 16); li = bc3(LI[:, kk, dsl], 16)
                            pl = lambda fn, BO=BO: k.op("pool", fn, reads=[B_C, B_L, B_tcd, BO], writes=[B_tcd, BO])
                            pl(lambda e, lr=lr: e.tensor_tensor(out=tc_[:], in0=Cr[:, dsl, :], in1=lr, op=ALU.mult))
                            pl(lambda e, li=li: e.tensor_tensor(out=td_[:], in0=Ci[:, dsl, :], in1=li, op=ALU.mult))
                            pl(lambda e, o_r=o_r: e.tensor_tensor(out=o_r, in0=tc_[:], in1=td_[:], op=ALU.subtract))
                            nlr = bc3(NLR[:, kk, dsl], 16); nli = bc3(NLI[:, kk, dsl], 16)
                            pl(lambda e, nlr=nlr: e.tensor_tensor(out=tc_[:], in0=Ci[:, dsl, :], in1=nlr, op=ALU.mult))
                            pl(lambda e, nli=nli: e.tensor_tensor(out=td_[:], in0=Cr[:, dsl, :], in1=nli, op=ALU.mult))
                            pl(lambda e, o_i=o_i: e.tensor_tensor(out=o_i, in0=tc_[:], in1=td_[:], op=ALU.add))
                    for g in range(32):
                        i = g % 2
                        k.op("pe", lambda e, i=i, g=g: e.matmul(pTd[i][:, 0:128], lhsT=BLr[:, g, :], rhs=CTr[:, g, :], start=True, stop=False),
                             reads=[B_BL, B_CT], writes=[B_pTd[i]])
                        k.op("pe", lambda e, i=i, g=g: e.matmul(pTd[i][:, 0:128], lhsT=BLi[:, g, :], rhs=CTi[:, g, :], start=False, stop=True),
                             reads=[B_BL, B_CT], writes=[B_pTd[i]])
                        if d == 0:
                            k.op("dve", lambda e, i=i, g=g: e.tensor_tensor(out=T_all[:, g, :], in0=pTd[i][:, 0:128], in1=maskF[:], op=ALU.mult),
                                 reads=[B_pTd[i], B_mk], writes=[B_T])
                        else:
                            k.op("dve", lambda e, i=i: e.tensor_tensor(out=tt[:], in0=pTd[i][:, 0:128], in1=maskB[:], op=ALU.mult),
                                 reads=[B_pTd[i], B_mk], writes=[B_tt])
                            k.op("dve", lambda e, g=g: e.tensor_tensor(out=T_all[:, g, :], in0=T_all[:, g, :], in1=tt[:], op=ALU.add),
                                 reads=[B_tt, B_T], writes=[B_T])
                        k.op("pe", lambda e, i=i, g=g: e.transpose(out=pW[i][:, 0, 0:64], in_=BLr[:, g, :], identity=ident_b[0:64, 0:64]),
                             reads=[B_BL, B_ident_b], writes=[B_pW[i]])
                        k.op("pe", lambda e, i=i, g=g: e.transpose(out=pW[i][:, 0, 64:128], in_=BLi[:, g, :], identity=ident_b[0:64, 0:64]),
                             reads=[B_BL, B_ident_b], writes=[B_pW[i]])
                        k.op("act", lambda e, i=i, g=g, d=d: e.copy(out=WinT[:, d, g, :], in_=pW[i][:, 0, :]), reads=[B_pW[i]], writes=[B_WinT])
                k.barrier()
            with ExitStack() as t0:
                Yt = sb(t0, "Yt", [128, 3, 4096], BF16); B_Yt = Buf()
                pXd = [[ps(t0, f"pX{d}{i}", [64, 512]) for i in range(2)] for d in range(2)]
                B_pXd = [[Buf(), Buf()], [Buf(), Buf()]]
                pY = ps(t0, "pY", [128, 512]); B_pY = Buf()
                pYt = ps(t0, "pYt", [128, 8, 128], BF16); B_pYt = Buf()
                W = []
                for d in range(2):
                    W.append(dict(
                        XS=sb(t0, f"XS{d}", [64, 2, 288]), B_XS=Buf(),
                        base=sb(t0, f"base{d}", [64, 288]), sarg=sb(t0, f"sarg{d}", [64, 288]), B_tr=Buf(), B_tr2=Buf(),
                        sargi=sb(t0, f"sargi{d}", [64, 288], mybir.dt.int32), sargf=sb(t0, f"sargf{d}", [64, 288]),
                        sn=sb(t0, f"sn{d}", [64, 288]), cs=sb(t0, f"cs{d}", [64, 288]), B_sc=Buf(),
                        RR=sb(t0, f"RR{d}", [64, 2, 288]), B_RR=Buf(),
                        q1=sb(t0, f"q1{d}", [64, 288]), q2=sb(t0, f"q2{d}", [64, 288]), B_q=Buf(),
                        SS=sb(t0, f"SS{d}", [64, 2, 288]), B_SS=Buf()))
                Sp = sb(t0, "Sp", [64, 2, 2, 288], BF16); B_Spd = [Buf(), Buf()]
                Ysb = sb(t0, "Ysb", [128, 288], BF16); B_Ysb = Buf()
                k.op("dve", lambda e: e.memset(Sp[:], 0.0), writes=[B_Spd[0], B_Spd[1]])

                def chain(d, g):
                    w = W[d]
                    XS, base, sarg, sargi, sargf, sn, cs, RR, q1, q2, SS = (w[n] for n in ("XS", "base", "sarg", "sargi", "sargf", "sn", "cs", "RR", "q1", "q2", "SS"))
                    B_XS, B_tr, B_tr2, B_sc, B_RR, B_q, B_SS = (w[n] for n in ("B_XS", "B_tr", "B_tr2", "B_sc", "B_RR", "B_q", "B_SS"))
                    pX = pXd[d]; B_pX = B_pXd[d]; B_Sp = B_Spd[d]
                    dg = d * 32 + g
                    for c2 in range(2):
                        k.op("pe", lambda e, c2=c2: e.matmul(pX[c2][:, 0:288], lhsT=WinT[:, d, g, c2 * 64:(c2 + 1) * 64], rhs=UT_all[:, g, :],
                                                             start=True, stop=True), reads=[B_WinT, B_UT], writes=[B_pX[c2]])
                        if d == 0:
                            k.op("act", lambda e, c2=c2: e.copy(out=XS[:, c2, :], in_=pX[c2][:, 0:288]), reads=[B_pX[c2]], writes=[B_XS])
                        else:
                            k.op("act", lambda e, c2=c2: e.copy(out=XS[:, c2, 0:32], in_=pX[c2][:, 31::-1]), reads=[B_pX[c2]], writes=[B_XS])
                            k.op("act", lambda e, c2=c2: e.copy(out=XS[:, c2, 32:288], in_=pX[c2][:, 287:31:-1]), reads=[B_pX[c2]], writes=[B_XS])
                    k.op("dve", lambda e: e.tensor_scalar(out=base[:], in0=mrow[:], scalar1=th8[:, dg:dg + 1], scalar2=None, op0=ALU.mult),
                         reads=[B_mrow, B_th8], writes=[B_tr])
                    sincos(base[:], [64, 288], sn[:], cs[:], (sarg[:], sargi[:], sargf[:]), B_tr, B_sc, B_tr2)
                    dv = lambda fn: k.op("dve", fn, reads=[B_XS, B_sc, B_q, B_RR, B_SS, B_L], writes=[B_q, B_RR, B_SS])
                    dv(lambda e: e.tensor_tensor(out=q1[:], in0=cs[:], in1=XS[:, 0, :], op=ALU.mult))
                    dv(lambda e: e.tensor_tensor(out=q2[:], in0=sn[:], in1=XS[:, 1, :], op=ALU.mult))
                    dv(lambda e: e.tensor_tensor(out=q1[:], in0=q1[:], in1=q2[:], op=ALU.add))
                    dv(lambda e: e.tensor_tensor_scan(out=RR[:, 0, :], data0=MG[:, 15, dg:dg + 1].to_broadcast([64, 288]), data1=q1[:], initial=0.0, op0=ALU.mult, op1=ALU.add))
                    dv(lambda e: e.tensor_tensor(out=q1[:], in0=cs[:], in1=XS[:, 1, :], op=ALU.mult))
                    dv(lambda e: e.tensor_tensor(out=q2[:], in0=sn[:], in1=XS[:, 0, :], op=ALU.mult))
                    dv(lambda e: e.tensor_tensor(out=q1[:], in0=q1[:], in1=q2[:], op=ALU.subtract))
                    dv(lambda e: e.tensor_tensor_scan(out=RR[:, 1, :], data0=MG[:, 15, dg:dg + 1].to_broadcast([64, 288]), data1=q1[:], initial=0.0, op0=ALU.mult, op1=ALU.add))
                    dv(lambda e: e.tensor_tensor(out=q1[:], in0=cs[:], in1=RR[:, 0, :], op=ALU.mult))
                    dv(lambda e: e.tensor_tensor(out=q2[:], in0=sn[:], in1=RR[:, 1, :], op=ALU.mult))
                    dv(lambda e: e.tensor_tensor(out=SS[:, 0, :], in0=q1[:], in1=q2[:], op=ALU.subtract))
                    dv(lambda e: e.tensor_tensor(out=q1[:], in0=cs[:], in1=RR[:, 1, :], op=ALU.mult))
                    dv(lambda e: e.tensor_tensor(out=q2[:], in0=sn[:], in1=RR[:, 0, :], op=ALU.mult))
                    dv(lambda e: e.tensor_tensor(out=SS[:, 1, :], in0=q1[:], in1=q2[:], op=ALU.add))
                    for c2 in range(2):
                        if d == 0:
                            k.op("act", lambda e, c2=c2: e.copy(out=Sp[:, 0, c2, 1:288], in_=SS[:, c2, 0:287]), reads=[B_SS], writes=[B_Sp])
                        else:
                            k.op("act", lambda e, c2=c2: e.copy(out=Sp[:, 1, c2, 0:31], in_=SS[:, c2, 30::-1]), reads=[B_SS], writes=[B_Sp])
                            k.op("act", lambda e, c2=c2: e.copy(out=Sp[:, 1, c2, 32:288], in_=SS[:, c2, 286:30:-1]), reads=[B_SS], writes=[B_Sp])

                B_Sp = B_Spd[0]
                for g in range(32):
                    recs = []
                    orig_op = k.op
                    for d in range(2):
                        rec = []
                        k.op = lambda *a, rec=rec, **kw: rec.append((a, kw))
                        chain(d, g)
                        recs.append(rec)
                    k.op = orig_op
                    for i_ in range(max(len(r_) for r_ in recs)):
                        for r_ in recs:
                            if i_ < len(r_):
                                a_, kw_ = r_[i_]
                                k.op(*a_, **kw_)
                    k.op("pe", lambda e, g=g: e.matmul(pY[:, 0:288], lhsT=T_all[:, g, :], rhs=UT_all[:, g, :], start=True, stop=False),
                         reads=[B_T, B_UT], writes=[B_pY])
                    for d in range(2):
                        k.op("pe", lambda e, g=g, d=d: e.matmul(pY[:, 0:288], lhsT=COr[:, d, g, :], rhs=Sp[:, d, 0, :], start=False, stop=False),
                             reads=[B_CO, B_Spd[d]], writes=[B_pY])
                        k.op("pe", lambda e, g=g, d=d: e.matmul(pY[:, 0:288], lhsT=COi[:, d, g, :], rhs=Sp[:, d, 1, :], start=False, stop=(d == 1)),
                             reads=[B_CO, B_Spd[d]], writes=[B_pY])
                    k.op("act", lambda e: e.copy(out=Ysb[:], in_=pY[:, 0:288]), reads=[B_pY], writes=[B_Ysb])
                    for bi, (n0, nb) in enumerate(NB):
                        k.op("pe", lambda e, bi=bi, n0=n0, nb=nb: e.transpose(out=pYt[0:nb, bi, :], in_=Ysb[:, n0:n0 + nb], identity=ident_b[:]),
                             reads=[B_Ysb, B_ident_b], writes=[B_pYt])
                    for bi, (n0, nb) in enumerate(NB):
                        dst = Yt[0:nb, bi, :].rearrange("p (j f) -> p j f", j=8)[:, :, g * 16:(g + 1) * 16]
                        k.op("dve", lambda e, bi=bi, nb=nb, dst=dst: e.tensor_copy(out=dst, in_=pYt[0:nb, bi, :].rearrange("p (j c) -> p j c", j=8)),
                             reads=[B_pYt], writes=[B_Yt])
                y8v = y_scr.rearrange("(n j) f -> n (j f)", j=8)
                for bi, (n0, nb) in enumerate(NB):
                    k.dma("sp", y8v[n0:n0 + nb, :], Yt[0:nb, bi, :], reads=[B_Yt])
                k.barrier()


        def ssm_post(st, catT_, B_catT_):
            GC = 2.0 * math.sqrt(2.0 / math.pi)
            gw = sb(st, "gluw", [128, 4, 512], BF16); B_gw = Buf()
            k.dma("pool", gw[:], glu_w.rearrange("(k p) n -> p k n", p=128), writes=[B_gw])
            gb = sb(st, "glub", [128, 4]); B_gb = Buf()
            with nc.allow_non_contiguous_dma(reason="tiny bias"):
                k.dma("sp", gb[:], glu_b[0, :].rearrange("(c p) -> p c", p=128), writes=[B_gb])
            dbc = load_bc(st, "dskip", ssm_d[0:1, :], 512)
            yt = [sb(st, f"py{i}", [128, 512], BF16) for i in range(2)]; B_yt = [Buf(), Buf()]
            ut = [sb(st, f"pu{i}", [128, 512]) for i in range(2)]; B_ut = [Buf(), Buf()]
            xx = sb(st, "pxx", [128, 512]); B_xx = Buf()
            ww = sb(st, "pww", [128, 512]); B_ww = Buf()
            sg = sb(st, "psg", [128, 512]); B_sg = Buf()
            g_bf = sb(st, "pg_bf", [128, 512], BF16); B_g = Buf()
            gT = sb(st, "pgT", [128, 4, 128], BF16); B_gT = Buf()
            s2 = sb(st, "ps2", [128, 4, 128]); B_s2 = Buf()
            pGT = ps(st, "pGT", [128, 8, 128], BF16); B_pGT = Buf()
            pz = ps(st, "pz", [128, 4, 128]); B_pz = Buf()
            for ti in range(NTILE):
                i = ti % 2
                T0 = ti * 128
                row0 = T0 + CTX if ti < NLT else T0 - SEQ
                k.dma("sp", yt[i][:], y_scr[row0:row0 + 128, :], writes=[B_yt[i]])
                k.dma("sp", ut[i][:], u_scr[row0:row0 + 128, :], writes=[B_ut[i]])
                k.op("dve", lambda e, i=i: e.tensor_tensor(out=xx[:], in0=ut[i][:], in1=dbc[0][:], op=ALU.mult), reads=[B_ut[i], dbc[1]], writes=[B_xx])
                k.op("dve", lambda e, i=i: e.tensor_tensor(out=xx[:], in0=xx[:], in1=yt[i][:], op=ALU.add), reads=[B_xx, B_yt[i]], writes=[B_xx])
                k.op("pool", lambda e: e.tensor_tensor(out=ww[:], in0=xx[:], in1=xx[:], op=ALU.mult), reads=[B_xx], writes=[B_ww])
                k.op("pool", lambda e: e.tensor_scalar(out=ww[:], in0=ww[:], scalar1=0.044715, scalar2=1.0, op0=ALU.mult, op1=ALU.add), reads=[B_ww], writes=[B_ww])
                k.op("pool", lambda e: e.tensor_tensor(out=ww[:], in0=ww[:], in1=xx[:], op=ALU.mult), reads=[B_ww, B_xx], writes=[B_ww])
                k.op("act", lambda e: e.activation(out=sg[:], in_=ww[:], func=AF.Sigmoid, scale=GC), reads=[B_ww], writes=[B_sg])
                k.op("dve", lambda e: e.tensor_tensor(out=g_bf[:], in0=xx[:], in1=sg[:], op=ALU.mult), reads=[B_xx, B_sg], writes=[B_g])
                if "dbg_g" in dbg:
                    if ti == 0:
                        dbg_g = dscr("dbg_g", [NT, 512], BF16)
                    k.dma("sp", dbg_g[T0:T0 + 128, :], g_bf[:], reads=[B_g])
                for kc in range(4):
                    k.op("pe", lambda e, kc=kc: e.transpose(out=pGT[:, kc, :], in_=g_bf[:, kc * 128:(kc + 1) * 128], identity=ident_b[:]),
                         reads=[B_g, B_ident_b], writes=[B_pGT])
                k.op("act", lambda e: e.copy(out=gT[:], in_=pGT[:, 0:4, :]), reads=[B_pGT], writes=[B_gT])
                for n_ in range(4):
                    for kc in range(4):
                        k.op("pe", lambda e, n_=n_, kc=kc: e.matmul(pz[:, n_, :], lhsT=gw[:, kc, n_ * 128:(n_ + 1) * 128], rhs=gT[:, kc, :],
                                                                    start=(kc == 0), stop=(kc == 3)), reads=[B_gw, B_gT], writes=[B_pz])
                for n_ in range(4):
                    k.op("act", lambda e, n_=n_: e.activation(out=s2[:, n_, :], in_=pz[:, n_, :], func=AF.Sigmoid, bias=gb[:, n_:n_ + 1], scale=1.0),
                         reads=[B_pz, B_gb], writes=[B_s2])
                k.op("dve", lambda e, T0=T0: e.tensor_tensor(out=catT_[:, 4:8, T0:T0 + 128], in0=gT[:], in1=s2[:], op=ALU.mult),
                     reads=[B_gT, B_s2], writes=[B_catT_])
            if "dbg_cat" in dbg:
                dbg_cat = dscr("dbg_cat", [128, 8, NT], BF16)
                k.dma("sp", dbg_cat, catT_[:], reads=[B_catT_])
            k.barrier()


        def layer1_mixer():
            LAM_INIT = 0.8 - 0.6 * math.exp(-0.3 * 1)
            SC = 0.125
            with ExitStack() as L1:
                qT2 = sb(L1, "qT2", [128, 8, SEQ], BF16); B_q2 = Buf()
                kT2 = sb(L1, "kT2", [128, 8, NT], BF16); B_k2 = Buf()
                v1 = sb(L1, "v1", [128, NTILE, D], BF16); B_v1 = Buf()
                nmax = sb(L1, "nmax", [128, 32]); B_nmax = Buf()
                for st in phase("l1proj"):
                    w_bf = sb(st, "dif_w_bf", [128, 8, 3072], BF16); B_w = Buf()
                    for kc in range(8):
                        k.dma("pool", w_bf[:, kc, :], dif_w_in[kc * 128:(kc + 1) * 128, :], writes=[B_w])
                    sc1p = [mod_bc(st, f"l1sc1p_{r}", 1, r, 1, plus1=True) for r in range(2)]
                    sh1 = [mod_bc(st, f"l1sh1_{r}", 1, r, 0) for r in range(2)]
                    xt = [sb(st, f"l1xt{i}", [128, D]) for i in range(2)]; B_xt = [Buf(), Buf()]
                    tmpf = sb(st, "l1tmpf", [128, D]); B_tmpf = Buf()
                    h_bf = sb(st, "l1h_bf", [128, D], BF16); B_hbf = Buf()
                    hT = [sb(st, f"l1hT{i}", [128, 8, 128], BF16) for i in range(2)]; B_hT = [Buf(), Buf()]
                    rt = [sb(st, f"l1rt{i}", [128, 64]) for i in range(2)]; B_rt = [Buf(), Buf()]
                    t1 = sb(st, "l1rope_t1", [128, 512]); t2 = sb(st, "l1rope_t2", [128, 512]); B_rtmp = Buf()
                    qk_bf = sb(st, "l1qk_bf", [128, 512], BF16); B_qk = Buf()
                    pT = ps(st, "l1pT", [128, 8, 128], BF16); B_pT = Buf()
                    pp = [ps(st, f"l1pp{i}", [128, 512]) for i in range(3)]; B_pp = [Buf(), Buf(), Buf()]
                    pq = [ps(st, f"l1pq{i}", [128, 8, 128], BF16) for i in range(2)]; B_pq = [Buf(), Buf()]
                    sqt = sb(st, "l1sq", [128, 512]); rs8 = sb(st, "l1rs8", [128, 8]); B_sq = Buf()
                    k.op("dve", lambda e: e.memset(nmax[:], 0.0), writes=[B_nmax])
                    ib = 0
                    for ti in range(NTILE):
                        i = ti % 2
                        r = 0 if ti < NLT else 1
                        T0 = ti * 128
                        k.dma("sp", xt[i][:], src_rows(ti, 1), writes=[B_xt[i]])
                        if r == 0:
                            k.dma("sp", rt[i][:], rope_cs[T0:T0 + 128, :], writes=[B_rt[i]])
                        k.op("dve", lambda e, i=i, r=r: e.tensor_tensor(out=tmpf[:], in0=xt[i][:], in1=sc1p[r][0][:], op=ALU.mult),
                             reads=[B_xt[i], sc1p[r][1]], writes=[B_tmpf])
                        k.op("pool", lambda e, r=r: e.tensor_tensor(out=h_bf[:], in0=tmpf[:], in1=sh1[r][0][:], op=ALU.add),
                             reads=[B_tmpf, sh1[r][1]], writes=[B_hbf])
                        for kc in range(8):
                            k.op("pe", lambda e, kc=kc: e.transpose(out=pT[:, kc, :], in_=h_bf[:, kc * 128:(kc + 1) * 128], identity=ident_b[:]),
                                 reads=[B_hbf, B_ident_b], writes=[B_pT])
                        k.op("act", lambda e, i=i: e.copy(out=hT[i][:], in_=pT[:]), reads=[B_pT], writes=[B_hT[i]])
                        for cb in range(6):
                            if r == 1 and cb < 2:
                                continue
                            j = ib % 3; ib += 1
                            for kc in range(8):
                                k.op("pe", lambda e, kc=kc, j=j, cb=cb, i=i: e.matmul(
                                    pp[j][:], lhsT=hT[i][:, kc, :], rhs=w_bf[:, kc, cb * 512:(cb + 1) * 512],
                                    start=(kc == 0), stop=(kc == 7)), reads=[B_hT[i], B_w], writes=[B_pp[j]])
                            if cb >= 4:
                                c0 = (cb - 4) * 512
                                k.op("act", lambda e, j=j, ti=ti, c0=c0: e.copy(out=v1[:, ti, c0:c0 + 512], in_=pp[j][:]), reads=[B_pp[j]], writes=[B_v1])
                                continue
                            if r == 0:
                                rope_apply(None, pp[j][:], B_pp[j], qk_bf[:], B_qk, 8, rt[i], B_rt[i], t1[:], t2[:], B_rtmp)
                            else:
                                k.op("dve", lambda e, j=j: e.tensor_copy(out=qk_bf[:], in_=pp[j][:]), reads=[B_pp[j]], writes=[B_qk])
                            k.op("dve", lambda e: e.tensor_tensor(out=sqt[:], in0=qk_bf[:], in1=qk_bf[:], op=ALU.mult), reads=[B_qk, B_sq], writes=[B_sq])
                            k.op("dve", lambda e: e.tensor_reduce(out=rs8[:], in_=sqt[:].rearrange("p (m d) -> p m d", d=64), axis=AX.X, op=ALU.add), reads=[B_sq], writes=[B_sq])
                            k.op("dve", lambda e, cb=cb: e.tensor_tensor(out=nmax[:, cb * 8:(cb + 1) * 8], in0=nmax[:, cb * 8:(cb + 1) * 8], in1=rs8[:], op=ALU.max),
                                 reads=[B_sq, B_nmax], writes=[B_nmax])
                            jq = cb % 2
                            for hh in range(4):
                                k.op("pe", lambda e, hh=hh, jq=jq: e.transpose(out=pq[jq][:, hh, :], in_=qk_bf[:, hh * 128:(hh + 1) * 128], identity=ident_b[:]),
                                     reads=[B_qk, B_ident_b], writes=[B_pq[jq]])
                            dstT, BD = (qT2, B_q2) if cb < 2 else (kT2, B_k2)
                            h0 = (cb % 2) * 4
                            k.op("act", lambda e, jq=jq, dstT=dstT, h0=h0, T0=T0: e.copy(out=dstT[:, h0:h0 + 4, T0:T0 + 128], in_=pq[jq][:, 0:4, :]),
                                 reads=[B_pq[jq]], writes=[BD])
                    k.barrier()
                for st in phase("l1att"):
                    w_bf = sb(st, "difwo_bf", [128, 8, D], BF16); B_w = Buf()
                    for kc in range(8):
                        k.dma("pool", w_bf[:, kc, :], dif_w_out[kc * 128:(kc + 1) * 128, :], writes=[B_w])
                    g1 = mod_bc(st, "l1g1", 1, 0, 2)
                    lng = load_bc(st, "l1ln1g", ln1_g[1:2, :], D)
                    lnb = load_bc(st, "l1ln1b", ln1_b[1:2, :], D)
                    wk = ln_work(st, "l1e1")
                    xo = [sb(st, f"l1xo{i}", [128, D]) for i in range(2)]; B_xo = [Buf(), Buf()]
                    lam = sb(st, "lam", [128, 8]); B_lam = Buf()
                    lq = [load_bc(st, f"lq{i}", a[0:1, :], 64) for i, a in enumerate((lam_q1, lam_k1, lam_q2, lam_k2))]
                    ltmp = sb(st, "ltmp", [128, 64]); B_lt = Buf()
                    for i2 in range(2):
                        k.op("dve", lambda e, i2=i2: e.tensor_tensor(out=ltmp[:], in0=lq[2 * i2][0][:], in1=lq[2 * i2 + 1][0][:], op=ALU.mult),
                             reads=[lq[2 * i2][1], lq[2 * i2 + 1][1], B_lt], writes=[B_lt])
                        k.op("dve", lambda e, i2=i2: e.reduce_sum(out=lam[:, i2:i2 + 1], in_=ltmp[:], axis=AX.X), reads=[B_lt, B_lam], writes=[B_lam])
                    k.op("act", lambda e: e.activation(out=lam[:, 2:4], in_=lam[:, 0:2], func=AF.Exp), reads=[B_lam], writes=[B_lam])
                    k.op("dve", lambda e: e.tensor_tensor(out=lam[:, 4:5], in0=lam[:, 2:3], in1=lam[:, 3:4], op=ALU.subtract), reads=[B_lam], writes=[B_lam])
                    k.op("dve", lambda e: e.tensor_scalar(out=lam[:, 5:6], in0=lam[:, 4:5], scalar1=-1.0, scalar2=-LAM_INIT, op0=ALU.mult, op1=ALU.add), reads=[B_lam], writes=[B_lam])
                    sg_col = sb(st, "sg_col", [128, 1]); B_sg = Buf()
                    with nc.allow_non_contiguous_dma(reason="tiny"):
                        k.dma("sp", sg_col[:], subln_g[0, :].rearrange("(p x) -> p x", x=1), writes=[B_sg])
                    k.op("dve", lambda e: e.tensor_scalar(out=sg_col[:], in0=sg_col[:], scalar1=1.0 - LAM_INIT, scalar2=None, op0=ALU.mult), reads=[B_sg], writes=[B_sg])
                    ones_bf = sb(st, "ones_bf", [128, 128], BF16); B_ones = Buf()
                    k.op("dve", lambda e: e.memset(ones_bf[:], 1.0), writes=[B_ones])
                    negC = sb(st, "negC", [128, 16]); B_negC = Buf()
                    with ExitStack() as t0:
                        nb = sb(t0, "nmax_bf", [128, 32], BF16); B_nb = Buf()
                        k.op("dve", lambda e: e.tensor_scalar(out=nb[:], in0=nmax[:], scalar1=1.02, scalar2=None, op0=ALU.mult), reads=[B_nmax], writes=[B_nb])
                        pn = ps(t0, "pn", [16, 1024], BF16); B_pn = Buf()
                        k.op("pe", lambda e: e.transpose(out=pn[:, 0:128], in_=nb[:, 0:16], identity=ident_b[:]), reads=[B_nb, B_ident_b], writes=[B_pn])
                        k.op("pe", lambda e: e.transpose(out=pn[:, 128:256], in_=nb[:, 16:32], identity=ident_b[:]), reads=[B_nb, B_ident_b], writes=[B_pn])
                        r2 = sb(t0, "r2", [16, 8]); B_r2 = Buf()
                        k.op("dve", lambda e: e.reduce_max(out=r2[:, 0:1], in_=pn[:, 0:128], axis=AX.X), reads=[B_pn], writes=[B_r2])
                        k.op("dve", lambda e: e.reduce_max(out=r2[:, 1:2], in_=pn[:, 128:256], axis=AX.X), reads=[B_pn, B_r2], writes=[B_r2])
                        k.op("dve", lambda e: e.tensor_tensor(out=r2[:, 2:3], in0=r2[:, 0:1], in1=r2[:, 1:2], op=ALU.mult), reads=[B_r2], writes=[B_r2])
                        k.op("act", lambda e: e.sqrt(out=r2[:, 3:4], in_=r2[:, 2:3]), reads=[B_r2], writes=[B_r2])
                        k.op("dve", lambda e: e.tensor_scalar(out=r2[:, 4:5], in0=r2[:, 3:4], scalar1=-SC, scalar2=None, op0=ALU.mult), reads=[B_r2], writes=[B_r2])
                        dg = sb(t0, "dgC", [16, 16], BF16); B_dg = Buf()
                        k.op("dve", lambda e: e.tensor_scalar(out=dg[:], in0=ident_f[0:16, 0:16], scalar1=r2[:, 4:5], scalar2=None, op0=ALU.mult), reads=[B_r2, B_ident_f], writes=[B_dg])
                        pc = ps(t0, "pcb", [128, 512]); B_pc = Buf()
                        k.op("pe", lambda e: e.matmul(pc[:, 0:16], lhsT=ones_bf[0:16, :], rhs=dg[:], start=True, stop=True), reads=[B_ones, B_dg], writes=[B_pc])
                        k.op("dve", lambda e: e.tensor_copy(out=negC[:], in_=pc[:, 0:16]), reads=[B_pc], writes=[B_negC])
                        k.barrier()
                    ET = [sb(st, f"ET{i}", [128, 512], BF16) for i in range(4)]; B_ET = [Buf() for _ in range(4)]
                    aoT = sb(st, "aoT_all", [128, 8, 512], BF16); B_aoT = Buf()
                    rz = sb(st, "rz", [1, 2, 512]); B_rz = Buf()
                    rzb = sb(st, "rzb", [1, 4, 512], BF16); B_rzb = Buf()
                    bcs = sb(st, "bcs", [128, 512]); B_bcs = Buf()
                    oT = sb(st, "oT", [128, 512]); B_oT = Buf()
                    t5 = sb(st, "t5", [128, 512]); B_t5 = Buf()
                    sqb = sb(st, "sqb", [128, 512], BF16); B_sqb = Buf()
                    pS = [ps(st, f"pS{i}", [128, 512]) for i in range(2)]; B_pS = [Buf(), Buf()]
                    pO4 = [ps(st, f"pO{i}", [128, 512]) for i in range(4)]; B_pO4 = [Buf() for _ in range(4)]
                    pZ1 = ps(st, "pZ", [1, 512]); B_pZ1 = Buf()
                    pZ = [pZ1, pZ1]; B_pZ = [B_pZ1, B_pZ1]
                    pB = ps(st, "pB", [128, 512]); B_pB = Buf()
                    cnt = {"s": 0, "e": 0}

                    def bcast_row(hi, lo):
                        k.op("pe", lambda e: e.matmul(pB[:], lhsT=ones_bf[0:1, :], rhs=hi, start=True, stop=False), reads=[B_ones, B_rzb], writes=[B_pB])
                        k.op("pe", lambda e: e.matmul(pB[:], lhsT=ones_bf[0:1, :], rhs=lo, start=False, stop=True), reads=[B_ones, B_rzb], writes=[B_pB])
                        k.op("act", lambda e: e.copy(out=bcs[:], in_=pB[:]), reads=[B_pB], writes=[B_bcs])

                    def split_row(src, j):
                        k.op("dve", lambda e: e.tensor_copy(out=rzb[:, 2 * j, :], in_=src), reads=[B_rz], writes=[B_rzb])
                        k.op("dve", lambda e: e.tensor_tensor(out=rzb[:, 2 * j + 1, :], in0=src, in1=rzb[:, 2 * j, :], op=ALU.subtract), reads=[B_rz, B_rzb], writes=[B_rzb])

                    zs = sb(st, "zs", [1, 2, 512]); B_zs = Buf()

                    def qk_exp(Q0, h, c, b):
                        ps_ = slice(c * 64, (c + 1) * 64)
                        m = h * 2 + c
                        js = cnt["s"] % 2; cnt["s"] += 1
                        je = cnt["e"] % 4; cnt["e"] += 1
                        k.op("pe", lambda e: e.matmul(pS[js][:], lhsT=kT2[ps_, h, b * 128:(b + 1) * 128], rhs=qT2[ps_, h, Q0:Q0 + 512],
                                                      start=True, stop=True), reads=[B_k2, B_q2], writes=[B_pS[js]])
                        k.op("act", lambda e: e.activation(out=ET[je][:], in_=pS[js][:], func=AF.Exp, bias=negC[:, m:m + 1], scale=SC),
                             reads=[B_pS[js], B_negC], writes=[B_ET[je]])
                        return je

                    def pvz(h, c, b, je):
                        pO = pO4[(h % 2) * 2:(h % 2) * 2 + 2]; B_pO = B_pO4[(h % 2) * 2:(h % 2) * 2 + 2]
                        Zacc = Zacc4[(h % 2) * 2:(h % 2) * 2 + 2]; B_Zacc = B_Zacc4[(h % 2) * 2:(h % 2) * 2 + 2]
                        k.op("pe", lambda e: e.matmul(pO[c][:], lhsT=v1[:, b, h * 128:(h + 1) * 128], rhs=ET[je][:],
                                                      start=(b == 0), stop=(b == NTILE - 1)), reads=[B_v1, B_ET[je]], writes=[B_pO[c]])
                        if b == 0:
                            k.op("dve", lambda e: e.tensor_copy(out=Zacc[c][:], in_=ET[je][:]), reads=[B_ET[je]], writes=[B_Zacc[c]])
                        else:
                            k.op("dve", lambda e: e.tensor_tensor(out=Zacc[c][:], in0=Zacc[c][:], in1=ET[je][:], op=ALU.add), reads=[B_ET[je], B_Zacc[c]], writes=[B_Zacc[c]])

                    Zacc4 = [sb(st, f"Zacc{i}", [128, 512]) for i in range(4)]; B_Zacc4 = [Buf() for _ in range(4)]
                    ones_f = sb(st, "ones_f", [128, 1]); B_onesf = Buf()
                    k.op("dve", lambda e: e.memset(ones_f[:], 1.0), writes=[B_onesf])

                    def bcast_recip(c, Zacc, B_Zacc):
                        k.op("pe", lambda e: e.matmul(pZ[c][:], lhsT=ones_f[:, 0:1], rhs=Zacc[c][:], start=True, stop=True), reads=[B_onesf, B_Zacc[c]], writes=[B_pZ[c]])
                        k.op("act", lambda e: e.copy(out=zs[:, c, :], in_=pZ[c][:]), reads=[B_pZ[c], B_zs], writes=[B_zs])
                        k.op("dve", lambda e: e.tensor_copy(out=rzb[:, 2 * c, :], in_=zs[:, c, :]), reads=[B_zs, B_rzb], writes=[B_rzb])
                        k.op("dve", lambda e: e.tensor_tensor(out=rzb[:, 2 * c + 1, :], in0=zs[:, c, :], in1=rzb[:, 2 * c, :], op=ALU.subtract), reads=[B_zs, B_rzb], writes=[B_rzb])
                        k.op("pe", lambda e: e.matmul(pB[:], lhsT=ones_bf[0:1, :], rhs=rzb[:, 2 * c, :], start=True, stop=False), reads=[B_ones, B_rzb], writes=[B_pB])
                        k.op("pe", lambda e: e.matmul(pB[:], lhsT=ones_bf[0:1, :], rhs=rzb[:, 2 * c + 1, :], start=False, stop=True), reads=[B_ones, B_rzb], writes=[B_pB])
                        k.op("dve", lambda e: e.reciprocal(out=bcs[:], in_=pB[:]), reads=[B_pB, B_bcs], writes=[B_bcs])

                    def epilogue(h):
                        pO = pO4[(h % 2) * 2:(h % 2) * 2 + 2]; B_pO = B_pO4[(h % 2) * 2:(h % 2) * 2 + 2]
                        Zacc = Zacc4[(h % 2) * 2:(h % 2) * 2 + 2]; B_Zacc = B_Zacc4[(h % 2) * 2:(h % 2) * 2 + 2]
                        bcast_recip(0, Zacc, B_Zacc)
                        k.op("dve", lambda e: e.tensor_tensor(out=oT[:], in0=pO[0][:], in1=bcs[:], op=ALU.mult), reads=[B_pO[0], B_bcs, B_oT], writes=[B_oT])
                        bcast_recip(1, Zacc, B_Zacc)
                        k.op("dve", lambda e: e.tensor_tensor(out=t5[:], in0=pO[1][:], in1=bcs[:], op=ALU.mult), reads=[B_pO[1], B_bcs, B_t5], writes=[B_t5])
                        k.op("dve", lambda e: e.scalar_tensor_tensor(out=oT[:], in0=t5[:], scalar=lam[:, 5:6], in1=oT[:], op0=ALU.mult, op1=ALU.add),
                             reads=[B_oT, B_t5, B_lam], writes=[B_oT])
                        k.op("dve", lambda e: e.tensor_tensor(out=sqb[:], in0=oT[:], in1=oT[:], op=ALU.mult), reads=[B_oT, B_sqb], writes=[B_sqb])
                        k.op("pe", lambda e: e.matmul(pZ[0][:], lhsT=ones_bf[:, 0:1], rhs=sqb[:], start=True, stop=True), reads=[B_ones, B_sqb], writes=[B_pZ[0]])
                        k.op("act", lambda e: e.activation(out=rz[:, 0, :], in_=pZ[0][:], func=AF.Ln, scale=1.0 / 128.0, bias=eps_t[0:1, :]), reads=[B_pZ[0], B_rz, B_eps], writes=[B_rz])
                        k.op("act", lambda e: e.activation(out=rz[:, 0, :], in_=rz[:, 0, :], func=AF.Exp, scale=-0.5), reads=[B_rz], writes=[B_rz])
                        split_row(rz[:, 0, :], 0)
                        bcast_row(rzb[:, 0, :], rzb[:, 1, :])
                        k.op("dve", lambda e: e.scalar_tensor_tensor(out=aoT[:, h, :], in0=oT[:], scalar=sg_col[:, 0:1], in1=bcs[:], op0=ALU.mult, op1=ALU.mult),
                             reads=[B_oT, B_sg, B_bcs], writes=[B_aoT])

                    eps_t = sb(st, "eps_t", [128, 1]); B_eps = Buf()
                    k.op("dve", lambda e: e.memset(eps_t[:], 1e-5), writes=[B_eps])
                    for qg in range(4):
                        Q0 = qg * 512
                        steps = [(h, c, b) for h in range(8) for c in range(2) for b in range(NTILE)]
                        je_next = qk_exp(Q0, *steps[0])
                        pending = []
                        for si, (h, c, b) in enumerate(steps):
                            je_cur = je_next
                            if si + 1 < len(steps):
                                je_next = qk_exp(Q0, *steps[si + 1])
                            pvz(h, c, b, je_cur)
                            if pending:
                                a_, kw_ = pending.pop(0)
                                k.op(*a_, **kw_)
                            if c == 1 and b == NTILE - 1:
                                while pending:
                                    a_, kw_ = pending.pop(0)
                                    k.op(*a_, **kw_)
                                orig_op = k.op
                                rec = []
                                k.op = lambda *a, rec=rec, **kw: rec.append((a, kw))
                                epilogue(h)
                                k.op = orig_op
                                pending = rec
                        while pending:
                            a_, kw_ = pending.pop(0)
                            k.op(*a_, **kw_)
                        for tt in range(4):
                            ti = qg * 4 + tt
                            T0 = ti * 128
                            i = ti % 2
                            k.dma("sp", xo[i][:], src_rows(ti, 1), writes=[B_xo[i]])
                            for hf in range(2):
                                for h in range(8):
                                    k.op("pe", lambda e, h=h, hf=hf, tt=tt: e.matmul(pS[hf][:], lhsT=aoT[:, h, tt * 128:(tt + 1) * 128], rhs=w_bf[:, h, hf * 512:(hf + 1) * 512],
                                                                                start=(h == 0), stop=(h == 7)), reads=[B_aoT, B_w], writes=[B_pS[hf]])
                            ln_epilogue(wk, [pS[0][:], pS[1][:]], [B_pS[0], B_pS[1]], xo[i], B_xo[i], g1, lng, lnb, x1_scr[T0:T0 + 128, :])
                    k.barrier()

        with ExitStack() as L0:
            catT = sb(L0, "catT", [128, 8, NT], BF16); B_catT = Buf()
            LA = ExitStack()
            qT = sb(LA, "qT", [64, 8, NT], BF16); B_qT = Buf()
            kT = sb(LA, "kT", [64, 2, NT], BF16); B_kT = Buf()
            v_all = sb(LA, "v_all", [128, NTILE, 128], BF16); B_v = Buf()
            for st in phase("l0proj"):
                w_bf = sb(st, "w_in_bf", [128, 8, 1280], BF16); B_w = Buf()
                for kc in range(8):
                    k.dma("pool", w_bf[:, kc, :], w_in0[kc * 128:(kc + 1) * 128, :], writes=[B_w])
                sc1p = [None, None]; sh1 = [None, None]
                for r in range(2):
                    sh1[r] = mod_bc(st, f"sh1_{r}", 0, r, 0)
                    sc1p[r] = mod_bc(st, f"sc1p_{r}", 0, r, 1, plus1=True)
                xt = [sb(st, f"xt{i}", [128, D]) for i in range(2)]; B_xt = [Buf(), Buf()]
                tmpf = sb(st, "tmpf", [128, D]); B_tmpf = Buf()
                h_bf = sb(st, "h_bf", [128, D], BF16); B_hbf = Buf()
                hT = [sb(st, f"hT{i}", [128, 8, 128], BF16) for i in range(2)]; B_hT = [Buf(), Buf()]
                rt = [sb(st, f"rt{i}", [128, 64]) for i in range(2)]; B_rt = [Buf(), Buf()]
                t1 = sb(st, "rope_t1", [128, 640]); t2 = sb(st, "rope_t2", [128, 640]); B_rtmp = Buf()
                qk_bf = sb(st, "qk_bf", [128, 640], BF16); B_qk = Buf()
                ut = [sb(st, f"ut{i}", [128, 512]) for i in range(2)]; B_ut = [Buf(), Buf()]
                pT = ps(st, "pT", [128, 8, 128], BF16); B_pT = Buf()
                pp = [ps(st, f"pp{i}", [128, 512]) for i in range(3)]; B_pp = [Buf(), Buf(), Buf()]
                pq = ps(st, "pq", [64, 8, 128], BF16); B_pq = Buf()
                pk = ps(st, "pk", [64, 8, 128], BF16); B_pk = Buf()
                for ti in range(NTILE):
                    i = ti % 2
                    r = 0 if ti < NLT else 1
                    T0 = ti * 128
                    k.dma("sp", xt[i][:], src_rows(ti, 0), writes=[B_xt[i]])
                    if r == 0:
                        k.dma("sp", rt[i][:], rope_cs[T0:T0 + 128, :], writes=[B_rt[i]])
                    k.op("dve", lambda e, i=i, r=r: e.tensor_tensor(out=tmpf[:], in0=xt[i][:], in1=sc1p[r][0][:], op=ALU.mult),
                         reads=[B_xt[i], sc1p[r][1]], writes=[B_tmpf])
                    k.op("pool", lambda e, r=r: e.tensor_tensor(out=h_bf[:], in0=tmpf[:], in1=sh1[r][0][:], op=ALU.add),
                         reads=[B_tmpf, sh1[r][1]], writes=[B_hbf])
                    for kc in range(8):
                        k.op("pe", lambda e, kc=kc: e.transpose(out=pT[:, kc, :], in_=h_bf[:, kc * 128:(kc + 1) * 128], identity=ident_b[:]),
                             reads=[B_hbf, B_ident_b], writes=[B_pT])
                    k.op("act", lambda e, i=i: e.copy(out=hT[i][:], in_=pT[:]), reads=[B_pT], writes=[B_hT[i]])
                    for nb, (c0, c1) in enumerate(((0, 512), (512, 1024), (1024, 1280))):
                        for kc in range(8):
                            k.op("pe", lambda e, kc=kc, nb=nb, c0=c0, c1=c1, i=i: e.matmul(
                                pp[nb][:, 0:c1 - c0], lhsT=hT[i][:, kc, :], rhs=w_bf[:, kc, c0:c1],
                                start=(kc == 0), stop=(kc == 7)),
                                reads=[B_hT[i], B_w], writes=[B_pp[nb]])
                    if "dbg_q" in dbg:
                        if ti == 0:
                            dq = sb(st, "dq", [128, 1280]); B_dq = Buf()
                        for nb, (c0, c1) in enumerate(((0, 512), (512, 1024), (1024, 1280))):
                            k.op("dve", lambda e, nb=nb, c0=c0, c1=c1: e.tensor_copy(out=dq[:, c0:c1], in_=pp[nb][:, 0:c1 - c0]),
                                 reads=[B_pp[nb]], writes=[B_dq])
                        k.dma("sp", dbg_q[T0:T0 + 128, :], dq[:], reads=[B_dq])
                    if r == 0:
                        rope_apply(None, pp[0][:, 0:512], B_pp[0], qk_bf[:, 0:512], B_qk, 8, rt[i], B_rt[i], t1[:, 0:512], t2[:, 0:512], B_rtmp)
                        rope_apply(None, pp[1][:, 0:128], B_pp[1], qk_bf[:, 512:640], B_qk, 2, rt[i], B_rt[i], t1[:, 512:640], t2[:, 512:640], B_rtmp)
                    else:
                        k.op("dve", lambda e: e.tensor_copy(out=qk_bf[:, 0:512], in_=pp[0][:, 0:512]), reads=[B_pp[0]], writes=[B_qk])
                        k.op("dve", lambda e: e.tensor_copy(out=qk_bf[:, 512:640], in_=pp[1][:, 0:128]), reads=[B_pp[1]], writes=[B_qk])
                    for h in range(8):
                        k.op("pe", lambda e, h=h: e.transpose(out=pq[:, h, :], in_=qk_bf[:, h * 64:(h + 1) * 64], identity=ident_b[:]),
                             reads=[B_qk, B_ident_b], writes=[B_pq])
                    for h in range(2):
                        k.op("pe", lambda e, h=h: e.transpose(out=pk[:, h, :], in_=qk_bf[:, 512 + h * 64:512 + (h + 1) * 64], identity=ident_b[:]),
                             reads=[B_qk, B_ident_b], writes=[B_pk])
                    k.op("act", lambda e, T0=T0: e.copy(out=qT[:, :, T0:T0 + 128], in_=pq[:]), reads=[B_pq], writes=[B_qT])
                    k.op("act", lambda e, T0=T0: e.copy(out=kT[:, :, T0:T0 + 128], in_=pk[:, 0:2, :]), reads=[B_pk], writes=[B_kT])
                    k.op("act", lambda e, ti=ti: e.copy(out=v_all[:, ti, :], in_=pp[1][:, 128:256]), reads=[B_pp[1]], writes=[B_v])
                    k.op("act", lambda e, i=i: e.copy(out=ut[i][:, 0:256], in_=pp[1][:, 256:512]), reads=[B_pp[1]], writes=[B_ut[i]])
                    k.op("act", lambda e, i=i: e.copy(out=ut[i][:, 256:512], in_=pp[2][:, 0:256]), reads=[B_pp[2]], writes=[B_ut[i]])
                    urow = T0 + CTX if r == 0 else T0 - SEQ
                    k.dma("sp", u_scr[urow:urow + 128, :], ut[i][:], reads=[B_ut[i]])
                if "dbg_qT" in dbg:
                    dbg_qT = dscr("dbg_qT", [64, 8, NT], BF16)
                    k.dma("sp", dbg_qT, qT[:], reads=[B_qT])
                k.barrier()


            for st in phase("l0att"):
                SC = 0.125
                maskL = sb(st, "maskL_sb", [128, 128]); maskR = sb(st, "maskR_sb", [128, 128]); B_mask = Buf()
                k.dma("sp", maskL[:], maskL_in[:, :], writes=[B_mask])
                k.dma("sp", maskR[:], maskR_in[:, :], writes=[B_mask])
                sink_bc, B_sink = load_bc(st, "sink_bc", swa_sink[0:1, :], 8)
                sm = [sb(st, f"sm{i}", [128, 640]) for i in range(2)]; B_sm = [Buf(), Buf()]
                P = [sb(st, f"P{i}", [128, 640], BF16) for i in range(2)]; B_P = [Buf(), Buf()]
                PT = [sb(st, f"PT{i}", [128, 5, 128], BF16) for i in range(2)]; B_PT = [Buf(), Buf()]
                stat = [sb(st, f"stat{i}", [128, 8]) for i in range(2)]; B_stat = [Buf(), Buf()]
                att_bf = sb(st, "att_bf", [128, 512], BF16); B_att = Buf()
                ps_loc = [ps(st, f"ps_loc{i}", [128, 512]) for i in range(2)]; B_psl = [Buf(), Buf()]
                ps_ctx = [ps(st, f"ps_ctx{i}", [128, 512]) for i in range(2)]; B_psc = [Buf(), Buf()]
                pPT = ps(st, "pPT", [128, 8, 128], BF16); B_pPT = Buf()
                po = ps(st, "po", [128, 512]); B_po = Buf()
                pcat = ps(st, "pcat", [128, 8, 128], BF16); B_pcat = Buf()
                it = 0
                for ti in range(NTILE):
                    T0 = ti * 128
                    lat = ti < NLT
                    if lat:
                        j0 = max(0, ti - 1); j1 = min(NLT - 1, ti + 1)
                        nloc = (j1 - j0 + 1) * 128
                        blocks = list(range(j0, j1 + 1)) + [NLT, NLT + 1]
                    else:
                        nloc = 0
                        blocks = [NLT, NLT + 1]
                    n = nloc + 256
                    for h in range(8):
                        i = it % 2; it += 1
                        kvh = h // 4
                        if lat:
                            k.op("pe", lambda e, i=i, h=h, kvh=kvh, j0=j0, nloc=nloc, T0=T0: e.matmul(
                                ps_loc[i][:, 0:nloc], lhsT=qT[:, h, T0:T0 + 128], rhs=kT[:, kvh, j0 * 128:j0 * 128 + nloc],
                                start=True, stop=True), reads=[B_qT, B_kT], writes=[B_psl[i]])
                        k.op("pe", lambda e, i=i, h=h, kvh=kvh, T0=T0: e.matmul(
                            ps_ctx[i][:, 0:256], lhsT=qT[:, h, T0:T0 + 128], rhs=kT[:, kvh, SEQ:NT],
                            start=True, stop=True), reads=[B_qT, B_kT], writes=[B_psc[i]])
                        if lat:
                            for bi, j in enumerate(range(j0, j1 + 1)):
                                sl = slice(bi * 128, (bi + 1) * 128)
                                if j == ti:
                                    k.op("act", lambda e, i=i, sl=sl: e.mul(out=sm[i][:, sl], in_=ps_loc[i][:, sl], mul=SC),
                                         reads=[B_psl[i]], writes=[B_sm[i]])
                                else:
                                    mk = maskL if j < ti else maskR
                                    k.op("dve", lambda e, i=i, sl=sl, mk=mk: e.scalar_tensor_tensor(
                                        out=sm[i][:, sl], in0=ps_loc[i][:, sl], scalar=SC, in1=mk[:], op0=ALU.mult, op1=ALU.add),
                                        reads=[B_psl[i], B_mask], writes=[B_sm[i]])
                        k.op("act", lambda e, i=i, nloc=nloc: e.mul(out=sm[i][:, nloc:nloc + 256], in_=ps_ctx[i][:, 0:256], mul=SC),
                             reads=[B_psc[i]], writes=[B_sm[i]])
                        sti = stat[i]
                        k.op("dve", lambda e, i=i, n=n, sti=sti: e.reduce_max(out=sti[:, 0:1], in_=sm[i][:, 0:n], axis=AX.X),
                             reads=[B_sm[i]], writes=[B_stat[i]])
                        k.op("dve", lambda e, sti=sti, h=h: e.tensor_tensor(out=sti[:, 1:2], in0=sti[:, 0:1], in1=sink_bc[:, h:h + 1], op=ALU.max),
                             reads=[B_stat[i], B_sink], writes=[B_stat[i]])
                        k.op("dve", lambda e, sti=sti: e.tensor_scalar(out=sti[:, 2:3], in0=sti[:, 1:2], scalar1=-1.0, scalar2=None, op0=ALU.mult),
                             reads=[B_stat[i]], writes=[B_stat[i]])
                        k.op("act", lambda e, i=i, n=n, sti=sti: e.activation(out=P[i][:, 0:n], in_=sm[i][:, 0:n], func=AF.Exp,
                                                                             bias=sti[:, 2:3], scale=1.0, accum_out=sti[:, 3:4]),
                             reads=[B_sm[i], B_stat[i]], writes=[B_P[i], B_stat[i]])
                        k.op("act", lambda e, sti=sti, h=h: e.activation(out=sti[:, 4:5], in_=sink_bc[:, h:h + 1], func=AF.Exp,
                                                                        bias=sti[:, 2:3], scale=1.0),
                             reads=[B_sink, B_stat[i]], writes=[B_stat[i]])
                        k.op("dve", lambda e, sti=sti: e.tensor_tensor(out=sti[:, 5:6], in0=sti[:, 3:4], in1=sti[:, 4:5], op=ALU.add),
                             reads=[B_stat[i]], writes=[B_stat[i]])
                        k.op("dve", lambda e, sti=sti: e.reciprocal(out=sti[:, 6:7], in_=sti[:, 5:6]),
                             reads=[B_stat[i]], writes=[B_stat[i]])
                        nb = n // 128
                        for b in range(nb):
                            k.op("pe", lambda e, i=i, b=b: e.transpose(out=pPT[:, b, :], in_=P[i][:, b * 128:(b + 1) * 128], identity=ident_b[:]),
                                 reads=[B_P[i], B_ident_b], writes=[B_pPT])
                        k.op("pool" if False else "dve", lambda e, i=i, nb=nb: e.tensor_copy(out=PT[i][:, 0:nb, :], in_=pPT[:, 0:nb, :]),
                             reads=[B_pPT], writes=[B_PT[i]])
                        for b in range(nb):
                            k.op("pe", lambda e, i=i, b=b, h=h, kvh=kvh, vb=blocks[b], nb=nb: e.matmul(
                                po[:, h * 64:(h + 1) * 64], lhsT=PT[i][:, b, :], rhs=v_all[:, vb, kvh * 64:(kvh + 1) * 64],
                                start=(b == 0), stop=(b == nb - 1)), reads=[B_PT[i], B_v], writes=[B_po])
                        k.op("dve", lambda e, h=h, sti=sti: e.tensor_scalar(out=att_bf[:, h * 64:(h + 1) * 64], in0=po[:, h * 64:(h + 1) * 64],
                                                                           scalar1=sti[:, 6:7], scalar2=None, op0=ALU.mult),
                             reads=[B_po, B_stat[i]], writes=[B_att])
                    for cb in range(4):
                        k.op("pe", lambda e, cb=cb: e.transpose(out=pcat[:, cb, :], in_=att_bf[:, cb * 128:(cb + 1) * 128], identity=ident_b[:]),
                             reads=[B_att, B_ident_b], writes=[B_pcat])
                    k.op("act", lambda e, T0=T0: e.copy(out=catT[:, 0:4, T0:T0 + 128], in_=pcat[:, 0:4, :]), reads=[B_pcat], writes=[B_catT])
                    if "dbg_att" in dbg:
                        if ti == 0:
                            dbg_att = dscr("dbg_att", [NT, 512], BF16)
                        k.dma("sp", dbg_att[T0:T0 + 128, :], att_bf[:], reads=[B_att])
                k.barrier()


            k.barrier()
            LA.close()
            for st in phase("l0ssm"):
                ssm_phase(st, catT, B_catT)
            for st in phase("l0ssmpost"):
                ssm_post(st, catT, B_catT)

            for st in phase("l0out"):
                outproj_ln1(st, 0, catT, B_catT, w_out0, NTILE)


        for st in phase("moe0"):
            moe_phase(st, 0, NTILE, x2_scr)


        layer1_mixer()
        for st in phase("moe1"):
            moe_phase(st, 1, NLT, out)

        k.barrier()
    return nc


_CONSTS = None


def _consts():
    global _CONSTS
    if _CONSTS is None:
        t = np.arange(SEQ)
        row = (t // 64).astype(np.float32)
        col = (t % 64).astype(np.float32)
        inv = (10000.0 ** (-np.arange(16, dtype=np.float32) / 16)).astype(np.float32)
        ar = row[:, None] * inv[None, :]
        ac = col[:, None] * inv[None, :]
        rope = np.concatenate([np.cos(ar), np.sin(ar), np.cos(ac), np.sin(ac)], 1).astype(np.float32)
        qi = np.arange(128)[:, None]; kj = np.arange(128)[None, :]
        mL = np.where(kj >= qi, 0.0, -30000.0).astype(np.float32)
        mR = np.where(kj <= qi, 0.0, -30000.0).astype(np.float32)
        _CONSTS = {"rope_cs": rope, "ident": np.eye(128, dtype=np.float32), "maskL": mL, "maskR": mR}
        selm = np.zeros((32, 32, 128), np.float32)
        for e_ in range(32):
            selm[e_, e_, :] = 1.0
        _CONSTS["sel"] = selm
        _CONSTS["kval"] = np.ascontiguousarray(np.broadcast_to(np.repeat(np.arange(-7, 9, dtype=np.float32), 64)[None, :], (64, 1024)))
        _CONSTS["mrow"] = np.ascontiguousarray(np.broadcast_to(np.arange(288, dtype=np.float32)[None, :], (64, 288)))
        jj = np.arange(128) // 16
        _CONSTS["maskF"] = (jj[None, :] >= jj[:, None]).astype(np.float32)
        _CONSTS["maskB"] = (jj[None, :] <= jj[:, None]).astype(np.float32)
    return _CONSTS


def make_in_maps(inputs, cores):
    f = lambda a: np.ascontiguousarray(np.asarray(a, dtype=np.float32))
    shared = {}
    for name in ("mod_w", "mod_b", "ln1_g", "ln1_b", "ln2_g", "ln2_b", "swa_sink",
                 "ssm_d", "ssm_glu_b", "dif_lam_q1", "dif_lam_k1", "dif_lam_q2", "dif_lam_k2",
                 "dif_subln_g", "moe_wg", "moe_bg", "moe_we", "moe_w1", "moe_w3", "moe_w2"):
        shared[name] = f(inputs[name])
    for name in ("swa_ssm_w_in", "swa_ssm_w_out", "ssm_a_re", "ssm_a_im", "ssm_log_step",
                 "ssm_b_re", "ssm_b_im", "ssm_c_re", "ssm_c_im", "ssm_glu_w", "dif_w_in", "dif_w_out"):
        shared[name] = f(inputs[name])[0]
    shared["moe_be"] = f(inputs["moe_be"]).reshape(2, 32)
    shared["c_ctx"] = f(inputs["c_ctx"]).reshape(1, D)
    shared.update(_consts())
    maps = []
    for b in cores:
        m = dict(shared)
        m["x"] = f(inputs["x"][b])
        m["ctx"] = f(inputs["ctx"][b])
        m["c"] = f(inputs["c"][b]).reshape(1, D)
        maps.append(m)
    return maps


def kernel(**inputs):
    nc = build()
    maps = make_in_maps(inputs, range(8))
    res = run_bass_kernel_spmd(nc, maps, core_ids=list(range(8)))
    return np.stack([r["out"] for r in res.results], 0).astype(np.float32)
```

```python
import math
from contextlib import ExitStack

import numpy as np
import concourse.bass as bass
import concourse.mybir as mybir
from concourse.bass_utils import run_bass_kernel_spmd

F32 = mybir.dt.float32
BF16 = mybir.dt.bfloat16
AF = mybir.ActivationFunctionType
ALU = mybir.AluOpType
AX = mybir.AxisListType

D = 1024
SEQ = 2048
CTX = 256
NT = SEQ + CTX
NTILE = NT // 128
NLT = SEQ // 128
ALPHA = 4 ** 0.25
LN_EPS = 1e-5


class Buf:
    __slots__ = ("w", "r")

    def __init__(self):
        self.w = None
        self.r = {}


class EngState:
    def __init__(self, name, eng, sem):
        self.name = name
        self.eng = eng
        self.sem = sem
        self.count = 0
        self.waited = {}
        self.slots = []
        self.slot_i = 0


class K:
    def __init__(self, nc, stack):
        self.nc = nc
        self.E = {}
        for name, eng in (("pe", nc.tensor), ("dve", nc.vector), ("act", nc.scalar),
                          ("pool", nc.gpsimd), ("sp", nc.sync)):
            sem = stack.enter_context(nc.semaphore("s_" + name))
            self.E[name] = EngState(name, eng, sem)
        self.semkey = {}
        for qn, n in (("sp", 12), ("pool", 12), ("act", 6)):
            for i in range(n):
                sem = stack.enter_context(nc.semaphore(f"d_{qn}{i}"))
                self.E[qn].slots.append([sem, 0])
        self.uid = 0

    def _key(self, sem):
        return id(sem)

    def _wait(self, E, deps, skip_self=False):
        best = {}
        for sem, val in deps:
            if skip_self and sem is E.sem:
                continue
            k = id(sem)
            if k not in best or best[k][1] < val:
                best[k] = (sem, val)
        for k, (sem, val) in best.items():
            if E.waited.get(k, 0) < val:
                E.eng.wait_ge(sem, val)
                E.waited[k] = val

    def _deps(self, reads, writes):
        deps = []
        for b in reads:
            if b.w is not None:
                deps.append(b.w)
        for b in writes:
            if b.w is not None:
                deps.append(b.w)
            deps.extend(b.r.values())
        return deps

    def _mark(self, tok, reads, writes):
        sem, val = tok
        for b in reads:
            b.r[id(sem)] = tok
        for b in writes:
            b.w = tok
            b.r = {}

    def op(self, en, fn, reads=(), writes=()):
        E = self.E[en]
        self._wait(E, self._deps(reads, writes), skip_self=(en == "pe"))
        ins = fn(E.eng)
        E.count += 1
        ins.then_inc(E.sem, 1)
        tok = (E.sem, E.count)
        self._mark(tok, reads, writes)
        return tok

    def dma(self, qn, out, in_, reads=(), writes=(), **kw):
        E = self.E[qn]
        self._wait(E, self._deps(reads, writes))
        slot = E.slots[E.slot_i % len(E.slots)]
        E.slot_i += 1
        if slot[1] > 0:
            self._wait(E, [(slot[0], slot[1] * 16)])
        ins = E.eng.dma_start(out=out, in_=in_, **kw)
        slot[1] += 1
        ins.then_inc(slot[0], 16)
        tok = (slot[0], slot[1] * 16)
        self._mark(tok, reads, writes)
        return tok

    def all_tokens(self):
        toks = []
        for E in self.E.values():
            if E.count:
                toks.append((E.sem, E.count))
            for sem, c in E.slots:
                if c:
                    toks.append((sem, c * 16))
        return toks

    def barrier(self):
        toks = self.all_tokens()
        for E in self.E.values():
            self._wait(E, toks, skip_self=False)


def build(dbg=(), inject=(), phases=None):
    nc = bass.Bass("TRN2", target_bir_lowering=False)
    dbg = set(dbg)
    inject = set(inject)
    ALLP = {"mod", "l0proj", "l0att", "l0ssm", "l0ssmpost", "l0out", "moe0", "l1proj", "l1att", "moe1"}
    phases = ALLP if phases is None else set(phases)

    def din(name, shape):
        return nc.dram_tensor(name, list(shape), F32, kind="ExternalInput").ap()

    def dscr(name, shape, dt=F32):
        kind = "ExternalOutput" if name in dbg else ("ExternalInput" if name in inject else "Internal")
        return nc.dram_tensor(name, list(shape), dt, kind=kind).ap()

    x_in = din("x", [SEQ, D])
    ctx_in = din("ctx", [CTX, D])
    c_in = din("c", [1, D])
    cc_in = din("c_ctx", [1, D])
    mod_w = din("mod_w", [2, D, 6 * D])
    mod_b = din("mod_b", [2, 6 * D])
    ln1_g = din("ln1_g", [2, D]); ln1_b = din("ln1_b", [2, D])
    ln2_g = din("ln2_g", [2, D]); ln2_b = din("ln2_b", [2, D])
    w_in0 = din("swa_ssm_w_in", [D, 1280])
    w_out0 = din("swa_ssm_w_out", [D, D])
    swa_sink = din("swa_sink", [1, 8])
    a_re = din("ssm_a_re", [2, 32, 64]); a_im = din("ssm_a_im", [2, 32, 64])
    log_step = din("ssm_log_step", [2, 32])
    b_re = din("ssm_b_re", [2, 32, 64, 16]); b_im = din("ssm_b_im", [2, 32, 64, 16])
    c_re = din("ssm_c_re", [2, 32, 16, 64]); c_im = din("ssm_c_im", [2, 32, 16, 64])
    ssm_d = din("ssm_d", [1, 512])
    glu_w = din("ssm_glu_w", [512, 512]); glu_b = din("ssm_glu_b", [1, 512])
    dif_w_in = din("dif_w_in", [D, 3072]); dif_w_out = din("dif_w_out", [D, D])
    lam_q1 = din("dif_lam_q1", [1, 64]); lam_k1 = din("dif_lam_k1", [1, 64])
    lam_q2 = din("dif_lam_q2", [1, 64]); lam_k2 = din("dif_lam_k2", [1, 64])
    subln_g = din("dif_subln_g", [1, 128])
    moe_wg = din("moe_wg", [2, D, 4]); moe_bg = din("moe_bg", [2, 4])
    moe_we = din("moe_we", [2, 4, D, 8]); moe_be = din("moe_be", [2, 32])
    moe_w1 = din("moe_w1", [2, 32, D, 256]); moe_w3 = din("moe_w3", [2, 32, D, 256])
    moe_w2 = din("moe_w2", [2, 32, 256, D])
    rope_cs = din("rope_cs", [SEQ, 64])
    ident_in = din("ident", [128, 128])
    sel_in = din("sel", [32, 32, 128])
    kval_in = din("kval", [64, 1024]); mrow_in = din("mrow", [64, 288])
    maskF_in = din("maskF", [128, 128]); maskB_in = din("maskB", [128, 128])
    maskL_in = din("maskL", [128, 128]); maskR_in = din("maskR", [128, 128])
    out = nc.dram_tensor("out", [SEQ, D], F32, kind="ExternalOutput").ap()

    modrow = dscr("modrow", [2, 2, 6 * D])

    with ExitStack() as gs:
        k = K(nc, gs)

        def sb(st, name, shape, dt=F32):
            k.uid += 1
            return st.enter_context(nc.sbuf_tensor(f"sb{k.uid}_{name}", list(shape), dt))

        def ps(st, name, shape, dt=F32):
            k.uid += 1
            return st.enter_context(nc.psum_tensor(f"ps{k.uid}_{name}", list(shape), dt))

        def phase(name):
            if name in phases:
                with ExitStack() as st_:
                    yield st_

        ident_f = sb(gs, "ident_f", [128, 128]); B_ident_f = Buf()
        ident_b = sb(gs, "ident_b", [128, 128], BF16); B_ident_b = Buf()
        k.dma("sp", ident_f[:], ident_in[:, :], writes=[B_ident_f])
        k.op("dve", lambda e: e.tensor_copy(out=ident_b[:], in_=ident_f[:]),
             reads=[B_ident_f], writes=[B_ident_b])

        for st in phase("mod"):
            cT = sb(st, "cT", [128, 8, 2]); B_cT = Buf()
            with nc.allow_non_contiguous_dma(reason="tiny column loads"):
                k.dma("sp", cT[:, :, 0], c_in[0, :].rearrange("(k p) -> p k", p=128), writes=[B_cT])
                k.dma("sp", cT[:, :, 1], cc_in[0, :].rearrange("(k p) -> p k", p=128), writes=[B_cT])
            sT = sb(st, "sT", [128, 8, 2]); B_sT = Buf()
            k.op("act", lambda e: e.activation(out=sT[:], in_=cT[:], func=AF.Silu),
                 reads=[B_cT], writes=[B_sT])
            wt = [sb(st, f"modw{i}", [128, 8, 512]) for i in range(2)]
            B_wt = [Buf(), Buf()]
            mb = sb(st, "modb", [2, 6 * D]); B_mb = Buf()
            mrow = sb(st, "mrow", [2, 6 * D]); B_mrow = Buf()
            pm = [ps(st, f"pmod{i}", [2, 512]) for i in range(2)]
            B_pm = [Buf(), Buf()]
            it = 0
            for l in range(2):
                k.dma("sp", mb[0:1, :], mod_b[l:l + 1, :], writes=[B_mb])
                k.dma("sp", mb[1:2, :], mod_b[l:l + 1, :], writes=[B_mb])
                for cb in range(12):
                    i = it % 2
                    it += 1
                    k.dma("sp" if cb % 2 == 0 else "act", wt[i][:],
                          mod_w[l, :, cb * 512:(cb + 1) * 512].rearrange("(k p) n -> p k n", p=128),
                          writes=[B_wt[i]])
                    for kc in range(8):
                        k.op("pe", lambda e, kc=kc, i=i: e.matmul(
                            pm[i][:], lhsT=sT[:, kc, :], rhs=wt[i][:, kc, :],
                            start=(kc == 0), stop=(kc == 7)),
                            reads=[B_sT, B_wt[i]], writes=[B_pm[i]])
                    k.op("dve", lambda e, i=i, cb=cb: e.tensor_tensor(
                        out=mrow[:, cb * 512:(cb + 1) * 512], in0=pm[i][:],
                        in1=mb[:, cb * 512:(cb + 1) * 512], op=ALU.add),
                        reads=[B_pm[i], B_mb], writes=[B_mrow])
                k.dma("sp", modrow[l], mrow[:], reads=[B_mrow], writes=[])
            k.barrier()


        def load_bc(st, name, src_row_ap, n, q="sp"):
            t = sb(st, name, [128, n]); B = Buf()
            k.dma(q, t[:], src_row_ap.partition_broadcast(128), writes=[B])
            return t, B

        def mod_bc(st, name, l, r, chunk, plus1=False):
            t, B = load_bc(st, name, modrow[l, r:r + 1, chunk * D:(chunk + 1) * D], D)
            if plus1:
                k.op("pool", lambda e: e.tensor_scalar(out=t[:], in0=t[:], scalar1=1.0, scalar2=None,
                                                       op0=ALU.add), reads=[B], writes=[B])
            return t, B

        u_scr = dscr("u_scr", [NT, 512])
        y_scr = dscr("y_scr", [NT, 512], BF16)
        x1_scr = dscr("x1_scr", [NT, D])
        x2_scr = dscr("x2_scr", [NT, D])
        dbg_q = dscr("dbg_q", [NT, 1280])

        def src_rows(ti, l):
            if l == 0:
                return x_in[ti * 128:(ti + 1) * 128, :] if ti < NLT else ctx_in[(ti - NLT) * 128:(ti - NLT + 1) * 128, :]
            return x2_scr[ti * 128:(ti + 1) * 128, :]

        def rope_apply(st_bufs, src_ps, B_src, dst, B_dst, nh, rt, B_rt, tmp1, tmp2, B_tmp):
            S = src_ps.rearrange("p (h a b f) -> p h a b f", h=nh, a=2, b=2, f=16)
            O = dst.rearrange("p (h a b f) -> p h a b f", h=nh, a=2, b=2, f=16)
            T1 = tmp1.rearrange("p (h a b f) -> p h a b f", h=nh, a=2, b=2, f=16)
            T2 = tmp2.rearrange("p (h a b f) -> p h a b f", h=nh, a=2, b=2, f=16)
            for a in range(2):
                cos = rt[:, a * 32:a * 32 + 16].rearrange("p (x y f) -> p x y f", x=1, y=1).to_broadcast([128, nh, 2, 16])
                sin = rt[:, a * 32 + 16:a * 32 + 32].rearrange("p (x y f) -> p x y f", x=1, y=1).to_broadcast([128, nh, 2, 16])
                k.op("dve", lambda e, a=a, cos=cos: e.tensor_tensor(out=T1[:, :, a], in0=S[:, :, a], in1=cos, op=ALU.mult),
                     reads=[B_src, B_rt], writes=[B_tmp])
                k.op("dve", lambda e, a=a, sin=sin: e.tensor_tensor(out=T2[:, :, a], in0=S[:, :, a, ::-1, :], in1=sin, op=ALU.mult),
                     reads=[B_src, B_rt], writes=[B_tmp])
                k.op("dve", lambda e, a=a: e.tensor_tensor(out=O[:, :, a, 0, :], in0=T1[:, :, a, 0, :], in1=T2[:, :, a, 0, :], op=ALU.subtract),
                     reads=[B_tmp], writes=[B_dst])
                k.op("dve", lambda e, a=a: e.tensor_tensor(out=O[:, :, a, 1, :], in0=T1[:, :, a, 1, :], in1=T2[:, :, a, 1, :], op=ALU.add),
                     reads=[B_tmp], writes=[B_dst])


        def ln_epilogue(wk, y_parts, B_y, xo, B_xo, g_t, lng_t, lnb_t, dst_rows):
            tmp, B_tmp, z, B_z, stt, B_stt, o, B_o = wk
            for hf in range(2):
                sl = slice(hf * 512, (hf + 1) * 512)
                k.op("dve", lambda e, hf=hf, sl=sl: e.tensor_tensor(out=tmp[:, sl], in0=y_parts[hf], in1=g_t[0][:, sl], op=ALU.mult),
                     reads=[B_y[hf], g_t[1]], writes=[B_tmp])
            k.op("dve", lambda e: e.scalar_tensor_tensor(out=z[:], in0=xo[:], scalar=ALPHA, in1=tmp[:], op0=ALU.mult, op1=ALU.add),
                 reads=[B_xo, B_tmp], writes=[B_z])
            for hf in range(2):
                k.op("dve", lambda e, hf=hf: e.bn_stats(out=stt[:, hf * 6:(hf + 1) * 6], in_=z[:, hf * 512:(hf + 1) * 512]),
                     reads=[B_z], writes=[B_stt])
            k.op("dve", lambda e: e.bn_aggr(out=stt[:, 12:14], in_=stt[:, 0:12]), reads=[B_stt], writes=[B_stt])
            k.op("dve", lambda e: e.tensor_scalar(out=stt[:, 15:16], in0=stt[:, 13:14], scalar1=LN_EPS, scalar2=None, op0=ALU.add),
                 reads=[B_stt], writes=[B_stt])
            k.op("act", lambda e: e.sqrt(out=stt[:, 15:16], in_=stt[:, 15:16]), reads=[B_stt], writes=[B_stt])
            k.op("dve", lambda e: e.reciprocal(out=stt[:, 14:15], in_=stt[:, 15:16]), reads=[B_stt], writes=[B_stt])
            k.op("dve", lambda e: e.tensor_scalar(out=tmp[:], in0=z[:], scalar1=stt[:, 12:13], scalar2=stt[:, 14:15], op0=ALU.subtract, op1=ALU.mult),
                 reads=[B_z, B_stt], writes=[B_tmp])
            k.op("pool", lambda e: e.tensor_tensor(out=o[:], in0=tmp[:], in1=lng_t[0][:], op=ALU.mult),
                 reads=[B_tmp, lng_t[1]], writes=[B_o])
            k.op("pool", lambda e: e.tensor_tensor(out=o[:], in0=o[:], in1=lnb_t[0][:], op=ALU.add),
                 reads=[B_o, lnb_t[1]], writes=[B_o])
            k.dma("sp", dst_rows, o[:], reads=[B_o])

        def ln_work(st, pfx):
            tmp = sb(st, pfx + "_tmp", [128, D]); z = sb(st, pfx + "_z", [128, D])
            stt = sb(st, pfx + "_stt", [128, 16]); o = sb(st, pfx + "_o", [128, D])
            return (tmp, Buf(), z, Buf(), stt, Buf(), o, Buf())

        def outproj_ln1(st, l, catT_, B_catT_, w_out_dram, ntiles):
            w_bf = sb(st, "w_out_bf", [128, 8, D], BF16); B_w = Buf()
            for kc in range(8):
                k.dma("pool", w_bf[:, kc, :], w_out_dram[kc * 128:(kc + 1) * 128, :], writes=[B_w])
            g1 = [mod_bc(st, f"g1_{r}", l, r, 2) for r in range(2)]
            lng = load_bc(st, "ln1g", ln1_g[l:l + 1, :], D)
            lnb = load_bc(st, "ln1b", ln1_b[l:l + 1, :], D)
            wk = ln_work(st, "e1")
            xo = [sb(st, f"xo{i}", [128, D]) for i in range(2)]; B_xo = [Buf(), Buf()]
            py = [ps(st, f"py{i}", [128, 512]) for i in range(4)]; B_py = [Buf() for _ in range(4)]
            for ti in range(ntiles):
                i = ti % 2
                r = 0 if ti < NLT else 1
                T0 = ti * 128
                k.dma("sp", xo[i][:], src_rows(ti, l), writes=[B_xo[i]])
                for hf in range(2):
                    pi = i * 2 + hf
                    for kc in range(8):
                        k.op("pe", lambda e, kc=kc, hf=hf, pi=pi, T0=T0: e.matmul(
                            py[pi][:], lhsT=catT_[:, kc, T0:T0 + 128], rhs=w_bf[:, kc, hf * 512:(hf + 1) * 512],
                            start=(kc == 0), stop=(kc == 7)), reads=[B_catT_, B_w], writes=[B_py[pi]])
                ln_epilogue(wk, [py[i * 2][:], py[i * 2 + 1][:]], [B_py[i * 2], B_py[i * 2 + 1]], xo[i], B_xo[i],
                            g1[r], lng, lnb, x1_scr[T0:T0 + 128, :])
            k.barrier()

        def moe_phase(st, l, ntiles, dst):
            ntok = ntiles * 128
            h2T = sb(st, "h2T", [128, 8, ntok], BF16); B_h2T = Buf()
            gateT = sb(st, "gateT", [32, ntok], BF16); B_gateT = Buf()
            f_acc = sb(st, "f_acc", [128, ntiles, D]); B_facc = [Buf() for _ in range(ntiles)]
            sel = sb(st, "sel", [32, 32, 128], BF16); B_sel = Buf()
            k.dma("pool", sel[:], sel_in[:, :, :], writes=[B_sel])
            with ExitStack() as s1:
                Wr = sb(s1, "Wr", [128, 8, 36]); B_Wr = Buf()
                with nc.allow_non_contiguous_dma(reason="small router weights"):
                    k.dma("sp", Wr[:, :, 0:4], moe_wg[l].rearrange("(k p) n -> p k n", p=128), writes=[B_Wr])
                    for g in range(4):
                        k.dma("sp", Wr[:, :, 4 + g * 8:12 + g * 8], moe_we[l, g].rearrange("(k p) n -> p k n", p=128), writes=[B_Wr])
                Whi = sb(s1, "Whi", [128, 8, 36], BF16); Wlo = sb(s1, "Wlo", [128, 8, 36], BF16); B_Wsp = Buf()
                k.op("dve", lambda e: e.tensor_copy(out=Whi[:], in_=Wr[:]), reads=[B_Wr], writes=[B_Wsp])
                k.op("dve", lambda e: e.tensor_tensor(out=Wlo[:], in0=Wr[:], in1=Whi[:], op=ALU.subtract), reads=[B_Wr, B_Wsp], writes=[B_Wsp])
                rb = sb(s1, "rb", [128, 36]); B_rb = Buf()
                k.dma("sp", rb[:, 0:4], moe_bg[l:l + 1, :].partition_broadcast(128), writes=[B_rb])
                k.dma("sp", rb[:, 4:36], moe_be[l:l + 1, :].partition_broadcast(128), writes=[B_rb])
                sc2p = [mod_bc(s1, f"sc2p_{r}", l, r, 4, plus1=True) for r in range(2)]
                sh2 = [mod_bc(s1, f"sh2_{r}", l, r, 3) for r in range(2)]
                xt = [sb(s1, f"mx{i}", [128, D]) for i in range(2)]; B_xt = [Buf(), Buf()]
                hf32 = [sb(s1, f"mh{c_}", [128, D]) for c_ in range(2)]; B_h = [Buf(), Buf()]
                hhi = [sb(s1, f"hhi{c_}", [128, D], BF16) for c_ in range(2)]; hlo = [sb(s1, f"hlo{c_}", [128, D], BF16) for c_ in range(2)]; B_hs = [Buf(), Buf()]
                hloT = [sb(s1, f"hloT{c_}", [128, 8, 128], BF16) for c_ in range(2)]; B_hloT = [Buf(), Buf()]
                pTh = [ps(s1, f"mpTh{c_}", [128, 8, 128], BF16) for c_ in range(2)]; B_pTh = [Buf(), Buf()]
                pTl = [ps(s1, f"mpTl{c_}", [128, 8, 128], BF16) for c_ in range(2)]; B_pTl = [Buf(), Buf()]
                pr = [ps(s1, f"mpr{c_}", [128, 512]) for c_ in range(2)]; B_pr = [Buf(), Buf()]
                pg = [ps(s1, f"mpg{c_}", [32, 1024], BF16) for c_ in range(2)]; B_pg = [Buf(), Buf()]
                lg = [sb(s1, f"lg{c_}", [128, 36]) for c_ in range(2)]; B_lg = [Buf(), Buf()]
                sm = [sb(s1, f"rsm{c_}", [128, 160]) for c_ in range(2)]; B_sm = [Buf(), Buf()]
                gates = [sb(s1, f"gates{c_}", [128, 32]) for c_ in range(2)]; B_gates = [Buf(), Buf()]
                gates_bf = [sb(s1, f"gates_bf{c_}", [128, 32], BF16) for c_ in range(2)]; B_gbf = [Buf(), Buf()]
                def router_tile(ti):
                    i = ti % 2
                    ci = ti % 2
                    r = 0 if ti < NLT else 1
                    T0 = ti * 128
                    k.dma("sp", xt[i][:], x1_scr[T0:T0 + 128, :], writes=[B_xt[i]])
                    k.op("dve", lambda e, i=i, r=r: e.tensor_tensor(out=hf32[ci][:], in0=xt[i][:], in1=sc2p[r][0][:], op=ALU.mult),
                         reads=[B_xt[i], sc2p[r][1]], writes=[B_h[ci]])
                    k.op("pool", lambda e, r=r: e.tensor_tensor(out=hf32[ci][:], in0=hf32[ci][:], in1=sh2[r][0][:], op=ALU.add),
                         reads=[B_h[ci], sh2[r][1]], writes=[B_h[ci]])
                    k.op("pool", lambda e: e.tensor_copy(out=hhi[ci][:], in_=hf32[ci][:]), reads=[B_h[ci]], writes=[B_hs[ci]])
                    k.op("dve", lambda e: e.tensor_tensor(out=hlo[ci][:], in0=hf32[ci][:], in1=hhi[ci][:], op=ALU.subtract), reads=[B_h[ci], B_hs[ci]], writes=[B_hs[ci]])
                    for kc in range(8):
                        k.op("pe", lambda e, kc=kc: e.transpose(out=pTh[ci][:, kc, :], in_=hhi[ci][:, kc * 128:(kc + 1) * 128], identity=ident_b[:]),
                             reads=[B_hs[ci], B_ident_b], writes=[B_pTh[ci]])
                    for kc in range(8):
                        k.op("pe", lambda e, kc=kc: e.transpose(out=pTl[ci][:, kc, :], in_=hlo[ci][:, kc * 128:(kc + 1) * 128], identity=ident_b[:]),
                             reads=[B_hs[ci], B_ident_b], writes=[B_pTl[ci]])
                    k.op("act", lambda e, T0=T0: e.copy(out=h2T[:, :, T0:T0 + 128], in_=pTh[ci][:]), reads=[B_pTh[ci]], writes=[B_h2T])
                    k.op("dve", lambda e: e.tensor_copy(out=hloT[ci][:], in_=pTl[ci][:]), reads=[B_pTl[ci]], writes=[B_hloT[ci]])
                    n_mm = 24
                    j = 0
                    for (A, BA, W) in ((None, B_h2T, Whi), (hloT[ci], B_hloT[ci], Whi), (None, B_h2T, Wlo)):
                        for kc in range(8):
                            lhs = h2T[:, kc, T0:T0 + 128] if A is None else A[:, kc, :]
                            k.op("pe", lambda e, lhs=lhs, W=W, kc=kc, j=j: e.matmul(pr[ci][:, 0:36], lhsT=lhs, rhs=W[:, kc, :], start=(j == 0), stop=(j == 23)),
                                 reads=[BA, B_Wsp], writes=[B_pr[ci]])
                            j += 1
                    R = [B_lg[ci], B_sm[ci]]
                    def dv(fn, reads=R, writes=(B_sm[ci],)):
                        k.op("dve", fn, reads=list(reads), writes=list(writes))
                    k.op("dve", lambda e: e.tensor_tensor(out=lg[ci][:], in0=pr[ci][:, 0:36], in1=rb[:], op=ALU.add), reads=[B_pr[ci], B_rb], writes=[B_lg[ci]])
                    dv(lambda e: e.reduce_max(out=sm[ci][:, 0:1], in_=lg[ci][:, 0:4], axis=AX.X))
                    dv(lambda e: e.tensor_scalar(out=sm[ci][:, 1:2], in0=sm[ci][:, 0:1], scalar1=-1.0, scalar2=None, op0=ALU.mult))
                    k.op("act", lambda e: e.activation(out=sm[ci][:, 56:60], in_=lg[ci][:, 0:4], func=AF.Exp, bias=sm[ci][:, 1:2], scale=1.0, accum_out=sm[ci][:, 2:3]),
                         reads=R, writes=[B_sm[ci]])
                    dv(lambda e: e.reciprocal(out=sm[ci][:, 3:4], in_=sm[ci][:, 2:3]))
                    dv(lambda e: e.tensor_scalar(out=sm[ci][:, 4:8], in0=lg[ci][:, 0:4], scalar1=sm[ci][:, 0:1], scalar2=None, op0=ALU.is_equal))
                    le = lg[ci][:, 4:36].rearrange("p (g e) -> p g e", g=4)
                    tmp48 = sm[ci][:, 64:96].rearrange("p (g e) -> p g e", g=4)
                    ohb = sm[ci][:, 4:8].rearrange("p (g x) -> p g x", x=1).to_broadcast([128, 4, 8])
                    dv(lambda e: e.tensor_tensor(out=tmp48, in0=le, in1=ohb, op=ALU.mult))
                    dv(lambda e: e.tensor_reduce(out=sm[ci][:, 8:16], in_=sm[ci][:, 64:96].rearrange("p (g e) -> p e g", g=4), axis=AX.X, op=ALU.add))
                    dv(lambda e: e.reduce_max(out=sm[ci][:, 16:17], in_=sm[ci][:, 8:16], axis=AX.X))
                    dv(lambda e: e.tensor_scalar(out=sm[ci][:, 24:32], in0=sm[ci][:, 8:16], scalar1=sm[ci][:, 16:17], scalar2=None, op0=ALU.is_equal))
                    dv(lambda e: e.scalar_tensor_tensor(out=sm[ci][:, 32:40], in0=sm[ci][:, 24:32], scalar=-1e30, in1=sm[ci][:, 8:16], op0=ALU.mult, op1=ALU.add))
                    dv(lambda e: e.reduce_max(out=sm[ci][:, 17:18], in_=sm[ci][:, 32:40], axis=AX.X))
                    dv(lambda e: e.tensor_scalar(out=sm[ci][:, 40:48], in0=sm[ci][:, 32:40], scalar1=sm[ci][:, 17:18], scalar2=None, op0=ALU.is_equal))
                    dv(lambda e: e.tensor_tensor(out=sm[ci][:, 18:19], in0=sm[ci][:, 17:18], in1=sm[ci][:, 16:17], op=ALU.subtract))
                    k.op("act", lambda e: e.activation(out=sm[ci][:, 19:20], in_=sm[ci][:, 18:19], func=AF.Exp), reads=R, writes=[B_sm[ci]])
                    dv(lambda e: e.tensor_scalar(out=sm[ci][:, 20:21], in0=sm[ci][:, 19:20], scalar1=1.0, scalar2=None, op0=ALU.add))
                    dv(lambda e: e.reciprocal(out=sm[ci][:, 20:21], in_=sm[ci][:, 20:21]))
                    dv(lambda e: e.tensor_tensor(out=sm[ci][:, 21:22], in0=sm[ci][:, 19:20], in1=sm[ci][:, 20:21], op=ALU.mult))
                    dv(lambda e: e.tensor_tensor(out=sm[ci][:, 22:23], in0=sm[ci][:, 20:21], in1=sm[ci][:, 3:4], op=ALU.mult))
                    dv(lambda e: e.tensor_tensor(out=sm[ci][:, 23:24], in0=sm[ci][:, 21:22], in1=sm[ci][:, 3:4], op=ALU.mult))
                    dv(lambda e: e.tensor_scalar(out=sm[ci][:, 48:56], in0=sm[ci][:, 24:32], scalar1=sm[ci][:, 22:23], scalar2=None, op0=ALU.mult))
                    dv(lambda e: e.scalar_tensor_tensor(out=sm[ci][:, 48:56], in0=sm[ci][:, 40:48], scalar=sm[ci][:, 23:24], in1=sm[ci][:, 48:56], op0=ALU.mult, op1=ALU.add))
                    geb = sm[ci][:, 48:56].rearrange("p (x e) -> p x e", x=1).to_broadcast([128, 4, 8])
                    k.op("dve", lambda e: e.tensor_tensor(out=gates[ci][:].rearrange("p (g e) -> p g e", g=4), in0=ohb, in1=geb, op=ALU.mult),
                         reads=R, writes=[B_gates[ci]])
                    k.op("dve", lambda e: e.tensor_copy(out=gates_bf[ci][:], in_=gates[ci][:]), reads=[B_gates[ci]], writes=[B_gbf[ci]])
                    k.op("pe", lambda e: e.transpose(out=pg[ci][:, 0:128], in_=gates_bf[ci][:], identity=ident_b[:]),
                         reads=[B_gbf[ci], B_ident_b], writes=[B_pg[ci]])
                    k.op("act", lambda e, T0=T0: e.copy(out=gateT[:, T0:T0 + 128], in_=pg[ci][:, 0:128]), reads=[B_pg[ci]], writes=[B_gateT])
                    if "dbg_gates" in dbg:
                        if ti == 0:
                            dbg_gates = dscr("dbg_gates", [NT, 32])
                        k.dma("sp", dbg_gates[T0:T0 + 128, :], gates[ci][:], reads=[B_gates[ci]])

                recs_pair = []
                for ti in range(ntiles):
                    rec_r = []
                    orig_op_r = k.op
                    k.op = lambda *a, rec_r=rec_r, **kw: rec_r.append((a, kw))
                    router_tile(ti)
                    k.op = orig_op_r
                    recs_pair.append(rec_r)
                    if len(recs_pair) == 2 or ti == ntiles - 1:
                        for ix_ in range(max(len(r_) for r_ in recs_pair)):
                            for r_ in recs_pair:
                                if ix_ < len(r_):
                                    a_, kw_ = r_[ix_]
                                    k.op(*a_, **kw_)
                        recs_pair = []
                k.barrier()
            if "stop_router" in dbg:
                return
            with ExitStack() as s2:
                w13 = [sb(s2, f"w13_{i}", [128, 8, 512], BF16) for i in range(2)]; B_w13 = [Buf(), Buf()]
                w2 = [sb(s2, f"w2_{i}", [128, 2, D], BF16) for i in range(2)]; B_w2 = [Buf(), Buf()]
                hh = [sb(s2, f"hh{i}", [128, 2, ntok], BF16) for i in range(2)]; B_hh = [Buf(), Buf()]
                stg13 = sb(s2, "stg13", [128, 8, 512]); B_stg13 = Buf()
                stg2 = sb(s2, "stg2", [128, 2, D]); B_stg2 = Buf()
                s1t = [sb(s2, f"s1t{i}", [128, 512]) for i in range(2)]; B_s1t = [Buf(), Buf()]
                t3 = [sb(s2, f"t3{i}", [128, 512]) for i in range(2)]; B_t3 = [Buf(), Buf()]
                ph1 = [ps(s2, f"ph1_{i}", [128, 512]) for i in range(2)]; B_ph1 = [Buf(), Buf()]
                ph3 = [ps(s2, f"ph3_{i}", [128, 512]) for i in range(2)]; B_ph3 = [Buf(), Buf()]
                pgbs = [ps(s2, f"pgb{i}", [128, 512]) for i in range(2)]; B_pgbs = [Buf(), Buf()]
                ibk = 0
                pf = [ps(s2, f"pf{i}", [128, 512]) for i in range(2)]; B_pf = [Buf(), Buf()]
                blocks = [(b0, min(512, ntok - b0)) for b0 in range(0, ntok, 512)]
                it = 0; itf = 0

                def w_stage13(ee):
                    k.dma("sp", stg13[:, :, 0:256], moe_w1[l, ee].rearrange("(k p) f -> p k f", p=128), writes=[B_stg13])
                    k.dma("sp", stg13[:, :, 256:512], moe_w3[l, ee].rearrange("(k p) f -> p k f", p=128), writes=[B_stg13])

                def w_stage2(ee):
                    k.dma("sp", stg2[:], moe_w2[l, ee].rearrange("(c p) d -> p c d", p=128), writes=[B_stg2])

                def w_cast13(ee):
                    wj = ee % 2
                    k.op("pool", lambda e: e.tensor_copy(out=w13[wj][:], in_=stg13[:]), reads=[B_stg13], writes=[B_w13[wj]])

                def w_cast2(ee):
                    wj = ee % 2
                    k.op("pool", lambda e: e.tensor_copy(out=w2[wj][:], in_=stg2[:]), reads=[B_stg2], writes=[B_w2[wj]])

                def w_tail(e_):
                    if e_ + 1 < 32:
                        w_cast2(e_ + 1)
                    if e_ + 2 < 32:
                        w_stage2(e_ + 2)
                for e_ in range(32):
                    wi = e_ % 2
                    if e_ == 0:
                        w_stage13(0); w_stage2(0); w_cast13(0); w_cast2(0); w_stage13(1); w_stage2(1)
                    if e_ + 1 < 32:
                        w_cast13(e_ + 1)
                    if e_ + 2 < 32:
                        w_stage13(e_ + 2)
                    for (b0, bn) in blocks:
                        pgb = pgbs[ibk % 2]; B_pgb = B_pgbs[ibk % 2]; ibk += 1
                        k.op("pe", lambda e, e_=e_, b0=b0, bn=bn, pgb=pgb: e.matmul(pgb[:, 0:bn], lhsT=sel[:, e_, :], rhs=gateT[:, b0:b0 + bn], start=True, stop=True),
                             reads=[B_sel, B_gateT], writes=[B_pgb])
                        for fc in range(2):
                            i = it % 2; it += 1
                            for kc in range(8):
                                k.op("pe", lambda e, kc=kc, fc=fc, i=i, wi=wi, b0=b0, bn=bn: e.matmul(
                                    ph1[i][:, 0:bn], lhsT=w13[wi][:, kc, fc * 128:(fc + 1) * 128], rhs=h2T[:, kc, b0:b0 + bn],
                                    start=(kc == 0), stop=(kc == 7)), reads=[B_w13[wi], B_h2T], writes=[B_ph1[i]])
                            for kc in range(8):
                                k.op("pe", lambda e, kc=kc, fc=fc, i=i, wi=wi, b0=b0, bn=bn: e.matmul(
                                    ph3[i][:, 0:bn], lhsT=w13[wi][:, kc, 256 + fc * 128:256 + (fc + 1) * 128], rhs=h2T[:, kc, b0:b0 + bn],
                                    start=(kc == 0), stop=(kc == 7)), reads=[B_w13[wi], B_h2T], writes=[B_ph3[i]])
                            k.op("act", lambda e, i=i, bn=bn: e.activation(out=s1t[i][:, 0:bn], in_=ph1[i][:, 0:bn], func=AF.Silu),
                                 reads=[B_ph1[i]], writes=[B_s1t[i]])
                            k.op("dve", lambda e, i=i, bn=bn: e.tensor_tensor(out=t3[i][:, 0:bn], in0=s1t[i][:, 0:bn], in1=ph3[i][:, 0:bn], op=ALU.mult),
                                 reads=[B_s1t[i], B_ph3[i]], writes=[B_t3[i]])
                            k.op("dve", lambda e, i=i, bn=bn, fc=fc, wi=wi, b0=b0, pgb=pgb: e.tensor_tensor(out=hh[wi][:, fc, b0:b0 + bn], in0=t3[i][:, 0:bn], in1=pgb[:, 0:bn], op=ALU.mult),
                                 reads=[B_t3[i], B_pgb], writes=[B_hh[wi]])
                    if e_ % 2 == 0:
                        w_tail(e_)
                        continue
                    for tt in range(ntiles):
                        for dc in range(2):
                            j = itf % 2; itf += 1
                            for q_ in range(4):
                                wq = q_ // 2; fc = q_ % 2
                                k.op("pe", lambda e, fc=fc, j=j, wq=wq, tt=tt, dc=dc, q_=q_: e.matmul(
                                    pf[j][:], lhsT=hh[wq][:, fc, tt * 128:(tt + 1) * 128], rhs=w2[wq][:, fc, dc * 512:(dc + 1) * 512],
                                    start=(q_ == 0), stop=(q_ == 3)), reads=[B_hh[wq], B_w2[wq]], writes=[B_pf[j]])
                            if e_ == 1:
                                k.op("dve", lambda e, j=j, tt=tt, dc=dc: e.tensor_copy(out=f_acc[:, tt, dc * 512:(dc + 1) * 512], in_=pf[j][:]),
                                     reads=[B_pf[j]], writes=[B_facc[tt]])
                            else:
                                k.op("dve", lambda e, j=j, tt=tt, dc=dc: e.tensor_tensor(out=f_acc[:, tt, dc * 512:(dc + 1) * 512],
                                     in0=f_acc[:, tt, dc * 512:(dc + 1) * 512], in1=pf[j][:], op=ALU.add),
                                     reads=[B_pf[j], B_facc[tt]], writes=[B_facc[tt]])
                    w_tail(e_)
                k.barrier()
            if "stop_experts" in dbg:
                return
            with ExitStack() as s3:
                g2 = [mod_bc(s3, f"g2_{r}", l, r, 5) for r in range(2)]
                lng = load_bc(s3, "ln2g", ln2_g[l:l + 1, :], D)
                lnb = load_bc(s3, "ln2b", ln2_b[l:l + 1, :], D)
                wk = ln_work(s3, "e2")
                xo = [sb(s3, f"x1o{i}", [128, D]) for i in range(2)]; B_xo = [Buf(), Buf()]
                for ti in range(ntiles):
                    i = ti % 2
                    r = 0 if ti < NLT else 1
                    T0 = ti * 128
                    k.dma("sp", xo[i][:], x1_scr[T0:T0 + 128, :], writes=[B_xo[i]])
                    if "dbg_f" in dbg:
                        if ti == 0:
                            dbg_f = dscr("dbg_f", [NT, D])
                        k.dma("sp", dbg_f[T0:T0 + 128, :], f_acc[:, ti, :], reads=[B_facc[ti]])
                    ln_epilogue(wk, [f_acc[:, ti, 0:512], f_acc[:, ti, 512:1024]], [B_facc[ti], B_facc[ti]], xo[i], B_xo[i],
                                g2[r], lng, lnb, dst[T0:T0 + 128, :])
                k.barrier()


        def ssm_phase(st, catT_, B_catT_):
            PI = math.pi
            TWO_PI = 2.0 * math.pi
            def bc3(ap2, n):
                P_, G_ = ap2.shape
                return ap2.rearrange("p (g x) -> p g x", x=1).to_broadcast([P_, G_, n])
            I32 = mybir.dt.int32
            INV2PI = 1.0 / TWO_PI

            def sincos(ang_ap, shape, s_out, c_out, tmps, B_in, B_out, B_tmp):
                y, yi, yf = tmps
                dvt = lambda fn: k.op("dve", fn, reads=[B_in, B_tmp, B_out], writes=[B_tmp])
                dvt(lambda e: e.tensor_scalar(out=y, in0=ang_ap, scalar1=INV2PI, scalar2=32.5, op0=ALU.mult, op1=ALU.add))
                dvt(lambda e: e.tensor_copy(out=yi, in_=y))
                dvt(lambda e: e.tensor_copy(out=yf, in_=yi))
                dvt(lambda e: e.tensor_tensor(out=y, in0=y, in1=yf, op=ALU.subtract))
                dvt(lambda e: e.scalar_tensor_tensor(out=yf, in0=y, scalar=0.0, in1=y, op0=ALU.is_lt, op1=ALU.add))
                k.op("act", lambda e: e.activation(out=s_out, in_=yf, func=AF.Sin, bias=negpi[0:shape[0], :], scale=TWO_PI),
                     reads=[B_tmp, B_np], writes=[B_out])
                dvt(lambda e: e.tensor_scalar(out=y, in0=yf, scalar1=0.25, scalar2=None, op0=ALU.add))
                dvt(lambda e: e.scalar_tensor_tensor(out=yf, in0=y, scalar=1.0, in1=y, op0=ALU.is_ge, op1=ALU.subtract))
                k.op("act", lambda e: e.activation(out=c_out, in_=yf, func=AF.Sin, bias=negpi[0:shape[0], :], scale=-TWO_PI),
                     reads=[B_tmp, B_np], writes=[B_out])

            ar = sb(st, "ar", [64, 64]); ai = sb(st, "ai", [64, 64]); ls = sb(st, "ls", [64, 64]); B_par = Buf()
            with nc.allow_non_contiguous_dma(reason="ssm params"):
                k.dma("sp", ar[:], a_re.rearrange("d g p -> p (d g)"), writes=[B_par])
                k.dma("sp", ai[:], a_im.rearrange("d g p -> p (d g)"), writes=[B_par])
            k.dma("sp", ls[:], log_step.rearrange("(x d) g -> x (d g)", x=1).partition_broadcast(64), writes=[B_par])
            negpi = sb(st, "negpi", [128, 1]); B_np = Buf()
            k.op("dve", lambda e: e.memset(negpi[:], -PI), writes=[B_np])
            kv = sb(st, "kv", [64, 16, 64]); B_kv = Buf()
            k.dma("sp", kv[:], kval_in.rearrange("p (k g) -> p k g", k=16), writes=[B_kv])
            mrow = sb(st, "mrow", [64, 288]); B_mrow = Buf()
            k.dma("sp", mrow[:], mrow_in[:, :], writes=[B_mrow])
            maskF = sb(st, "maskF", [128, 128]); maskB = sb(st, "maskB", [128, 128]); B_mk = Buf()
            k.dma("sp", maskF[:], maskF_in[:, :], writes=[B_mk])
            k.dma("sp", maskB[:], maskB_in[:, :], writes=[B_mk])
            dar = sb(st, "dar", [64, 64]); dai = sb(st, "dai", [64, 64]); B_d = Buf()
            k.op("act", lambda e: e.activation(out=ls[:], in_=ls[:], func=AF.Exp), reads=[B_par], writes=[B_par])
            k.op("dve", lambda e: e.tensor_tensor(out=dar[:], in0=ls[:], in1=ar[:], op=ALU.mult), reads=[B_par], writes=[B_d])
            k.op("dve", lambda e: e.tensor_tensor(out=dai[:], in0=ls[:], in1=ai[:], op=ALU.mult), reads=[B_par, B_d], writes=[B_d])
            LR = sb(st, "LR", [64, 16, 64]); LI = sb(st, "LI", [64, 16, 64]); MG = sb(st, "MG", [64, 16, 64]); B_L = Buf()
            th8 = sb(st, "th8", [64, 64]); B_th8 = Buf()
            k.op("dve", lambda e: e.tensor_scalar(out=th8[:], in0=dai[:], scalar1=8.0, scalar2=None, op0=ALU.mult), reads=[B_d], writes=[B_th8])
            with ExitStack() as t0:
                ang = sb(t0, "ang", [64, 16, 64]); a2 = sb(t0, "a2", [64, 16, 64]); B_ang = Buf()
                dai_b = dai[:].rearrange("p (x g) -> p x g", x=1).to_broadcast([64, 16, 64])
                dar_b = dar[:].rearrange("p (x g) -> p x g", x=1).to_broadcast([64, 16, 64])
                k.op("dve", lambda e: e.tensor_tensor(out=MG[:], in0=kv[:], in1=dar_b, op=ALU.mult), reads=[B_kv, B_d], writes=[B_L])
                k.op("act", lambda e: e.activation(out=MG[:], in_=MG[:], func=AF.Exp), reads=[B_L], writes=[B_L])
                k.op("dve", lambda e: e.tensor_tensor(out=ang[:], in0=kv[:], in1=dai_b, op=ALU.mult), reads=[B_kv, B_d], writes=[B_ang])
                a3 = sb(t0, "a3", [64, 16, 64], I32); a4 = sb(t0, "a4", [64, 16, 64])
                sincos(ang[:], [64, 16, 64], LI[:], LR[:], (a2[:], a3[:], a4[:]), B_ang, B_L, B_ang)
                k.op("dve", lambda e: e.tensor_tensor(out=LR[:], in0=LR[:], in1=MG[:], op=ALU.mult), reads=[B_L], writes=[B_L])
                k.op("dve", lambda e: e.tensor_tensor(out=LI[:], in0=LI[:], in1=MG[:], op=ALU.mult), reads=[B_L], writes=[B_L])
                k.barrier()
            cre = sb(st, "cre", [64, 64]); cim = sb(st, "cim", [64, 64]); B_c = Buf()
            with ExitStack() as t0:
                nr = sb(t0, "nr", [64, 64]); den = sb(t0, "den", [64, 64]); tq = sb(t0, "tq", [64, 64]); B_t = Buf()
                L1r = LR[:, 8, :]; L1i = LI[:, 8, :]
                dv = lambda fn: k.op("dve", fn, reads=[B_t, B_L, B_par, B_c], writes=[B_t, B_c])
                dv(lambda e: e.tensor_scalar(out=nr[:], in0=L1r, scalar1=-1.0, scalar2=None, op0=ALU.add))
                dv(lambda e: e.tensor_tensor(out=den[:], in0=ar[:], in1=ar[:], op=ALU.mult))
                dv(lambda e: e.tensor_tensor(out=tq[:], in0=ai[:], in1=ai[:], op=ALU.mult))
                dv(lambda e: e.tensor_tensor(out=den[:], in0=den[:], in1=tq[:], op=ALU.add))
                dv(lambda e: e.reciprocal(out=den[:], in_=den[:]))
                dv(lambda e: e.tensor_tensor(out=cre[:], in0=nr[:], in1=ar[:], op=ALU.mult))
                dv(lambda e: e.tensor_tensor(out=tq[:], in0=L1i, in1=ai[:], op=ALU.mult))
                dv(lambda e: e.tensor_tensor(out=cre[:], in0=cre[:], in1=tq[:], op=ALU.add))
                dv(lambda e: e.tensor_tensor(out=cre[:], in0=cre[:], in1=den[:], op=ALU.mult))
                dv(lambda e: e.tensor_tensor(out=cim[:], in0=L1i, in1=ar[:], op=ALU.mult))
                dv(lambda e: e.tensor_tensor(out=tq[:], in0=nr[:], in1=ai[:], op=ALU.mult))
                dv(lambda e: e.tensor_tensor(out=cim[:], in0=cim[:], in1=tq[:], op=ALU.subtract))
                dv(lambda e: e.tensor_tensor(out=cim[:], in0=cim[:], in1=den[:], op=ALU.mult))
                k.barrier()
            UT_all = sb(st, "UT_all", [128, 32, 288], BF16); B_UT = Buf()
            NB = ((0, 128), (128, 128), (256, 32))
            u8v = u_scr.rearrange("(n j) f -> n (j f)", j=8)
            with ExitStack() as t0:
                U8 = sb(t0, "U8", [128, 3, 4096]); B_U8 = Buf()
                U8b = sb(t0, "U8b", [128, 3, 4096], BF16); B_U8b = Buf()
                pU = [ps(t0, f"pU{i}", [128, 1024], BF16) for i in range(2)]; B_pU = [Buf(), Buf()]
                for bi, (n0, nb) in enumerate(NB):
                    k.dma("sp", U8[0:nb, bi, :], u8v[n0:n0 + nb, :], writes=[B_U8])
                    ov = U8b[0:nb, bi, :].rearrange("p (g j c) -> p j g c", g=32, j=8, c=16)
                    iv = U8[0:nb, bi, :].rearrange("p (j g c) -> p j g c", g=32, j=8, c=16)
                    k.op("pool" if bi == 1 else "act", (lambda e, ov=ov, iv=iv: e.tensor_copy(out=ov, in_=iv)) if bi == 1 else
                         (lambda e, ov=ov, iv=iv: e.copy(out=ov, in_=iv)), reads=[B_U8], writes=[B_U8b])
                for g in range(32):
                    i = g % 2
                    for bi, (n0, nb) in enumerate(NB):
                        src = U8b[0:nb, bi, g * 128:(g + 1) * 128]
                        k.op("pe", lambda e, i=i, src=src, n0=n0, nb=nb: e.transpose(out=pU[i][:, n0:n0 + nb], in_=src, identity=ident_b[0:nb, 0:nb]),
                             reads=[B_U8b, B_ident_b], writes=[B_pU[i]])
                    k.op("act", lambda e, i=i, g=g: e.copy(out=UT_all[:, g, :], in_=pU[i][:, 0:288]), reads=[B_pU[i]], writes=[B_UT])
                k.barrier()
            COr = sb(st, "COr", [64, 2, 32, 128], BF16); COi = sb(st, "COi", [64, 2, 32, 128], BF16); B_CO = Buf()
            T_all = sb(st, "T_all", [128, 32, 128], BF16); B_T = Buf()
            WinT = sb(st, "WinT", [128, 2, 32, 128], BF16); B_WinT = Buf()
            with ExitStack() as t0:
                BLr = sb(t0, "BLr", [64, 32, 128], BF16); BLi = sb(t0, "BLi", [64, 32, 128], BF16); B_BL = Buf()
                CTr = sb(t0, "CTr", [64, 32, 128], BF16); CTi = sb(t0, "CTi", [64, 32, 128], BF16); B_CT = Buf()
                Br = sb(t0, "Br", [64, 64, 16]); Bi = sb(t0, "Bi", [64, 64, 16]); B_B = Buf()
                Cr = sb(t0, "Cr", [64, 64, 16]); Ci = sb(t0, "Ci", [64, 64, 16]); B_C = Buf()
                Bbr = sb(t0, "Bbr", [64, 64, 16]); Bbi = sb(t0, "Bbi", [64, 64, 16]); B_Bb = Buf()
                with nc.allow_non_contiguous_dma(reason="ssm B/C tables"):
                    for d in range(2):
                        k.dma("sp", Br[:, d * 32:(d + 1) * 32, :], b_re[d].rearrange("g p c -> p g c"), writes=[B_B])
                        k.dma("sp", Bi[:, d * 32:(d + 1) * 32, :], b_im[d].rearrange("g p c -> p g c"), writes=[B_B])
                        for gb in range(4):
                            sl = slice(d * 32 + gb * 8, d * 32 + gb * 8 + 8)
                            k.dma("sp", Cr[:, sl, :], c_re[d, gb * 8:(gb + 1) * 8].rearrange("g c p -> p g c"), writes=[B_C])
                            k.dma("act", Ci[:, sl, :], c_im[d, gb * 8:(gb + 1) * 8].rearrange("g c p -> p g c"), writes=[B_C])
                NLR = sb(t0, "NLR", [64, 16, 64]); NLI = sb(t0, "NLI", [64, 16, 64])
                k.op("dve", lambda e: e.tensor_scalar(out=NLR[:], in0=LR[:], scalar1=-1.0, scalar2=None, op0=ALU.mult), reads=[B_L], writes=[B_L])
                k.op("dve", lambda e: e.tensor_scalar(out=NLI[:], in0=LI[:], scalar1=-1.0, scalar2=None, op0=ALU.mult), reads=[B_L], writes=[B_L])
                ta = sb(t0, "ta", [64, 32, 16]); tb_ = sb(t0, "tb", [64, 32, 16]); B_tab = Buf()
                tc_ = sb(t0, "tc", [64, 32, 16]); td_ = sb(t0, "td", [64, 32, 16]); B_tcd = Buf()
                for d in range(2):
                    dsl = slice(d * 32, (d + 1) * 32)
                    creb = bc3(cre[:, dsl], 16); cimb = bc3(cim[:, dsl], 16)
                    dv = lambda fn: k.op("dve", fn, reads=[B_B, B_c, B_tab, B_Bb], writes=[B_tab, B_Bb])
                    dv(lambda e, dsl=dsl, creb=creb: e.tensor_tensor(out=ta[:], in0=Br[:, dsl, :], in1=creb, op=ALU.mult))
                    dv(lambda e, dsl=dsl, cimb=cimb: e.tensor_tensor(out=tb_[:], in0=Bi[:, dsl, :], in1=cimb, op=ALU.mult))
                    dv(lambda e, dsl=dsl: e.tensor_tensor(out=Bbr[:, dsl, :], in0=ta[:], in1=tb_[:], op=ALU.subtract))
                    dv(lambda e, dsl=dsl, creb=creb: e.tensor_tensor(out=ta[:], in0=Bi[:, dsl, :], in1=creb, op=ALU.mult))
                    dv(lambda e, dsl=dsl, cimb=cimb: e.tensor_tensor(out=tb_[:], in0=Br[:, dsl, :], in1=cimb, op=ALU.mult))
                    dv(lambda e, dsl=dsl: e.tensor_tensor(out=Bbi[:, dsl, :], in0=ta[:], in1=tb_[:], op=ALU.add))
                pTd = [ps(t0, f"pTd{i}", [128, 512]) for i in range(2)]; B_pTd = [Buf(), Buf()]
                pW = [ps(t0, f"pW{i}", [128, 8, 128], BF16) for i in range(2)]; B_pW = [Buf(), Buf()]
                tt = sb(t0, "tt", [128, 128]); B_tt = Buf()
                for d in range(2):
                    dsl = slice(d * 32, (d + 1) * 32)
                    for j in range(8):
                        e_ = (7 - j) if d == 0 else j
                        lr = bc3(LR[:, e_ + 7, dsl], 16); li = bc3(LI[:, e_ + 7, dsl], 16)
                        o_r = BLr[:, :, j * 16:(j + 1) * 16]; o_i = BLi[:, :, j * 16:(j + 1) * 16]
                        dv2 = lambda fn: k.op("dve", fn, reads=[B_Bb, B_L, B_tab, B_BL], writes=[B_tab, B_BL])
                        dv2(lambda e, lr=lr: e.tensor_tensor(out=ta[:], in0=Bbr[:, dsl, :], in1=lr, op=ALU.mult))
                        dv2(lambda e, li=li: e.tensor_tensor(out=tb_[:], in0=Bbi[:, dsl, :], in1=li, op=ALU.mult))
                        dv2(lambda e, o_r=o_r: e.tensor_tensor(out=o_r, in0=ta[:], in1=tb_[:], op=ALU.subtract))
                        dv2(lambda e, lr=lr: e.tensor_tensor(out=ta[:], in0=Bbi[:, dsl, :], in1=lr, op=ALU.mult))
                        dv2(lambda e, li=li: e.tensor_tensor(out=tb_[:], in0=Bbr[:, dsl, :], in1=li, op=ALU.mult))
                        dv2(lambda e, o_i=o_i: e.tensor_tensor(out=o_i, in0=ta[:], in1=tb_[:], op=ALU.add))
                        f_ = (j - 7) if d == 0 else -j
                        for (kk, o_r, o_i, BO) in ((f_ + 7, CTr[:, :, j * 16:(j + 1) * 16], CTi[:, :, j * 16:(j + 1) * 16], B_CT),
                                                   (f_ + 15, COr[:, d, :, j * 16:(j + 1) * 16], COi[:, d, :, j * 16:(j + 1) * 16], B_CO)):
                            lr = bc3(LR[:, kk, dsl], 16); li = bc3(LI[:, kk, dsl], 16)
                            pl = lambda fn, BO=BO: k.op("pool", fn, reads=[B_C, B_L, B_tcd, BO], writes=[B_tcd, BO])
                            pl(lambda e, lr=lr: e.tensor_tensor(out=tc_[:], in0=Cr[:, dsl, :], in1=lr, op=ALU.mult))
                            pl(lambda e, li=li: e.tensor_tensor(out=td_[:], in0=Ci[:, dsl, :], in1=li, op=ALU.mult))
                            pl(lambda e, o_r=o_r: e.tensor_tensor(out=o_r, in0=tc_[:], in1=td_[:], op=ALU.subtract))
                            nlr = bc3(NLR[:, kk, dsl], 16); nli = bc3(NLI[:, kk, dsl], 16)
                            pl(lambda e, nlr=nlr: e.tensor_tensor(out=tc_[:], in0=Ci[:, dsl, :], in1=nlr, op=ALU.mult))
                            pl(lambda e, nli=nli: e.tensor_tensor(out=td_[:], in0=Cr[:, dsl, :], in1=nli, op=ALU.mult))
                            pl(lambda e, o_i=o_i: e.tensor_tensor(out=o_i, in0=tc_[:], in1=td_[:], op=ALU.add))
                    for g in range(32):
                        i = g % 2
                        k.op("pe", lambda e, i=i, g=g: e.matmul(pTd[i][:, 0:128], lhsT=BLr[:, g, :], rhs=CTr[:, g, :], start=True, stop=False),
                             reads=[B_BL, B_CT], writes=[B_pTd[i]])
                        k.op("pe", lambda e, i=i, g=g: e.matmul(pTd[i][:, 0:128], lhsT=BLi[:, g, :], rhs=CTi[:, g, :], start=False, stop=True),
                             reads=[B_BL, B_CT], writes=[B_pTd[i]])
                        if d == 0:
                            k.op("dve", lambda e, i=i, g=g: e.tensor_tensor(out=T_all[:, g, :], in0=pTd[i][:, 0:128], in1=maskF[:], op=ALU.mult),
                                 reads=[B_pTd[i], B_mk], writes=[B_T])
                        else:
                            k.op("dve", lambda e, i=i: e.tensor_tensor(out=tt[:], in0=pTd[i][:, 0:128], in1=maskB[:], op=ALU.mult),
                                 reads=[B_pTd[i], B_mk], writes=[B_tt])
                            k.op("dve", lambda e, g=g: e.tensor_tensor(out=T_all[:, g, :], in0=T_all[:, g, :], in1=tt[:], op=ALU.add),
                                 reads=[B_tt, B_T], writes=[B_T])
                        k.op("pe", lambda e, i=i, g=g: e.transpose(out=pW[i][:, 0, 0:64], in_=BLr[:, g, :], identity=ident_b[0:64, 0:64]),
                             reads=[B_BL, B_ident_b], writes=[B_pW[i]])
                        k.op("pe", lambda e, i=i, g=g: e.transpose(out=pW[i][:, 0, 64:128], in_=BLi[:, g, :], identity=ident_b[0:64, 0:64]),
                             reads=[B_BL, B_ident_b], writes=[B_pW[i]])
                        k.op("act", lambda e, i=i, g=g, d=d: e.copy(out=WinT[:, d, g, :], in_=pW[i][:, 0, :]), reads=[B_pW[i]], writes=[B_WinT])
                k.barrier()
            with ExitStack() as t0:
                Yt = sb(t0, "Yt", [128, 3, 4096], BF16); B_Yt = Buf()
                pXd = [[ps(t0, f"pX{d}{i}", [64, 512]) for i in range(2)] for d in range(2)]
                B_pXd = [[Buf(), Buf()], [Buf(), Buf()]]
                pY = ps(t0, "pY", [128, 512]); B_pY = Buf()
                pYt = ps(t0, "pYt", [128, 8, 128], BF16); B_pYt = Buf()
                W = []
                for d in range(2):
                    W.append(dict(
                        XS=sb(t0, f"XS{d}", [64, 2, 288]), B_XS=Buf(),
                        base=sb(t0, f"base{d}", [64, 288]), sarg=sb(t0, f"sarg{d}", [64, 288]), B_tr=Buf(), B_tr2=Buf(),
                        sargi=sb(t0, f"sargi{d}", [64, 288], mybir.dt.int32), sargf=sb(t0, f"sargf{d}", [64, 288]),
                        sn=sb(t0, f"sn{d}", [64, 288]), cs=sb(t0, f"cs{d}", [64, 288]), B_sc=Buf(),
                        RR=sb(t0, f"RR{d}", [64, 2, 288]), B_RR=Buf(),
                        q1=sb(t0, f"q1{d}", [64, 288]), q2=sb(t0, f"q2{d}", [64, 288]), B_q=Buf(),
                        q3=sb(t0, f"q3{d}", [64, 288]), q4=sb(t0, f"q4{d}", [64, 288]), B_q34=Buf(),
                        SS=sb(t0, f"SS{d}", [64, 2, 288]), B_SS=Buf()))
                Sp = sb(t0, "Sp", [64, 2, 2, 288], BF16); B_Spd = [Buf(), Buf()]
                Ysb = sb(t0, "Ysb", [128, 288], BF16); B_Ysb = Buf()
                k.op("dve", lambda e: e.memset(Sp[:], 0.0), writes=[B_Spd[0], B_Spd[1]])

                def chain(d, g):
                    w = W[d]
                    XS, base, sarg, sargi, sargf, sn, cs, RR, q1, q2, SS = (w[n] for n in ("XS", "base", "sarg", "sargi", "sargf", "sn", "cs", "RR", "q1", "q2", "SS"))
                    B_XS, B_tr, B_tr2, B_sc, B_RR, B_q, B_SS = (w[n] for n in ("B_XS", "B_tr", "B_tr2", "B_sc", "B_RR", "B_q", "B_SS"))
                    pX = pXd[d]; B_pX = B_pXd[d]; B_Sp = B_Spd[d]
                    dg = d * 32 + g
                    for c2 in range(2):
                        k.op("pe", lambda e, c2=c2: e.matmul(pX[c2][:, 0:288], lhsT=WinT[:, d, g, c2 * 64:(c2 + 1) * 64], rhs=UT_all[:, g, :],
                                                             start=True, stop=True), reads=[B_WinT, B_UT], writes=[B_pX[c2]])
                        if d == 0:
                            k.op("act", lambda e, c2=c2: e.copy(out=XS[:, c2, :], in_=pX[c2][:, 0:288]), reads=[B_pX[c2]], writes=[B_XS])
                        else:
                            k.op("act", lambda e, c2=c2: e.copy(out=XS[:, c2, 0:32], in_=pX[c2][:, 31::-1]), reads=[B_pX[c2]], writes=[B_XS])
                            k.op("act", lambda e, c2=c2: e.copy(out=XS[:, c2, 32:288], in_=pX[c2][:, 287:31:-1]), reads=[B_pX[c2]], writes=[B_XS])
                    k.op("dve", lambda e: e.tensor_scalar(out=base[:], in0=mrow[:], scalar1=th8[:, dg:dg + 1], scalar2=None, op0=ALU.mult),
                         reads=[B_mrow, B_th8], writes=[B_tr])
                    sincos(base[:], [64, 288], sn[:], cs[:], (sarg[:], sargi[:], sargf[:]), B_tr, B_sc, B_tr2)
                    dv = lambda fn: k.op("dve", fn, reads=[B_XS, B_sc, B_q, B_RR, B_SS, B_L], writes=[B_q, B_RR, B_SS])
                    dv(lambda e: e.tensor_tensor(out=q1[:], in0=cs[:], in1=XS[:, 0, :], op=ALU.mult))
                    dv(lambda e: e.tensor_tensor(out=q2[:], in0=sn[:], in1=XS[:, 1, :], op=ALU.mult))
                    dv(lambda e: e.tensor_tensor(out=q1[:], in0=q1[:], in1=q2[:], op=ALU.add))
                    dv(lambda e: e.tensor_tensor_scan(out=RR[:, 0, :], data0=MG[:, 15, dg:dg + 1].to_broadcast([64, 288]), data1=q1[:], initial=0.0, op0=ALU.mult, op1=ALU.add))
                    dv(lambda e: e.tensor_tensor(out=q1[:], in0=cs[:], in1=XS[:, 1, :], op=ALU.mult))
                    dv(lambda e: e.tensor_tensor(out=q2[:], in0=sn[:], in1=XS[:, 0, :], op=ALU.mult))
                    dv(lambda e: e.tensor_tensor(out=q1[:], in0=q1[:], in1=q2[:], op=ALU.subtract))
                    dv(lambda e: e.tensor_tensor_scan(out=RR[:, 1, :], data0=MG[:, 15, dg:dg + 1].to_broadcast([64, 288]), data1=q1[:], initial=0.0, op0=ALU.mult, op1=ALU.add))
                    dv(lambda e: e.tensor_tensor(out=q1[:], in0=cs[:], in1=RR[:, 0, :], op=ALU.mult))
                    dv(lambda e: e.tensor_tensor(out=q2[:], in0=sn[:], in1=RR[:, 1, :], op=ALU.mult))
                    dv(lambda e: e.tensor_tensor(out=SS[:, 0, :], in0=q1[:], in1=q2[:], op=ALU.subtract))
                    dv(lambda e: e.tensor_tensor(out=q1[:], in0=cs[:], in1=RR[:, 1, :], op=ALU.mult))
                    dv(lambda e: e.tensor_tensor(out=q2[:], in0=sn[:], in1=RR[:, 0, :], op=ALU.mult))
                    dv(lambda e: e.tensor_tensor(out=SS[:, 1, :], in0=q1[:], in1=q2[:], op=ALU.add))
                    for c2 in range(2):
                        if d == 0:
                            k.op("act", lambda e, c2=c2: e.copy(out=Sp[:, 0, c2, 1:288], in_=SS[:, c2, 0:287]), reads=[B_SS], writes=[B_Sp])
                        else:
                            k.op("act", lambda e, c2=c2: e.copy(out=Sp[:, 1, c2, 0:31], in_=SS[:, c2, 30::-1]), reads=[B_SS], writes=[B_Sp])
                            k.op("act", lambda e, c2=c2: e.copy(out=Sp[:, 1, c2, 32:288], in_=SS[:, c2, 286:30:-1]), reads=[B_SS], writes=[B_Sp])

                B_Sp = B_Spd[0]
                for g in range(32):
                    recs = []
                    orig_op = k.op
                    for d in range(2):
                        rec = []
                        k.op = lambda *a, rec=rec, **kw: rec.append((a, kw))
                        chain(d, g)
                        recs.append(rec)
                    k.op = orig_op
                    for i_ in range(max(len(r_) for r_ in recs)):
                        for r_ in recs:
                            if i_ < len(r_):
                                a_, kw_ = r_[i_]
                                k.op(*a_, **kw_)
                    k.op("pe", lambda e, g=g: e.matmul(pY[:, 0:288], lhsT=T_all[:, g, :], rhs=UT_all[:, g, :], start=True, stop=False),
                         reads=[B_T, B_UT], writes=[B_pY])
                    for d in range(2):
                        k.op("pe", lambda e, g=g, d=d: e.matmul(pY[:, 0:288], lhsT=COr[:, d, g, :], rhs=Sp[:, d, 0, :], start=False, stop=False),
                             reads=[B_CO, B_Spd[d]], writes=[B_pY])
                        k.op("pe", lambda e, g=g, d=d: e.matmul(pY[:, 0:288], lhsT=COi[:, d, g, :], rhs=Sp[:, d, 1, :], start=False, stop=(d == 1)),
                             reads=[B_CO, B_Spd[d]], writes=[B_pY])
                    k.op("act", lambda e: e.copy(out=Ysb[:], in_=pY[:, 0:288]), reads=[B_pY], writes=[B_Ysb])
                    for bi, (n0, nb) in enumerate(NB):
                        k.op("pe", lambda e, bi=bi, n0=n0, nb=nb: e.transpose(out=pYt[0:nb, bi, :], in_=Ysb[:, n0:n0 + nb], identity=ident_b[:]),
                             reads=[B_Ysb, B_ident_b], writes=[B_pYt])
                    for bi, (n0, nb) in enumerate(NB):
                        dst = Yt[0:nb, bi, :].rearrange("p (j f) -> p j f", j=8)[:, :, g * 16:(g + 1) * 16]
                        k.op("dve", lambda e, bi=bi, nb=nb, dst=dst: e.tensor_copy(out=dst, in_=pYt[0:nb, bi, :].rearrange("p (j c) -> p j c", j=8)),
                             reads=[B_pYt], writes=[B_Yt])
                y8v = y_scr.rearrange("(n j) f -> n (j f)", j=8)
                for bi, (n0, nb) in enumerate(NB):
                    k.dma("sp", y8v[n0:n0 + nb, :], Yt[0:nb, bi, :], reads=[B_Yt])
                k.barrier()


        def ssm_post(st, catT_, B_catT_):
            GC = 2.0 * math.sqrt(2.0 / math.pi)
            gw = sb(st, "gluw", [128, 4, 512], BF16); B_gw = Buf()
            k.dma("pool", gw[:], glu_w.rearrange("(k p) n -> p k n", p=128), writes=[B_gw])
            gb = sb(st, "glub", [128, 4]); B_gb = Buf()
            with nc.allow_non_contiguous_dma(reason="tiny bias"):
                k.dma("sp", gb[:], glu_b[0, :].rearrange("(c p) -> p c", p=128), writes=[B_gb])
            dbc = load_bc(st, "dskip", ssm_d[0:1, :], 512)
            yt = [sb(st, f"py{i}", [128, 512], BF16) for i in range(2)]; B_yt = [Buf(), Buf()]
            ut = [sb(st, f"pu{i}", [128, 512]) for i in range(2)]; B_ut = [Buf(), Buf()]
            xx = sb(st, "pxx", [128, 512]); B_xx = Buf()
            ww = sb(st, "pww", [128, 512]); B_ww = Buf()
            sg = sb(st, "psg", [128, 512]); B_sg = Buf()
            g_bf = sb(st, "pg_bf", [128, 512], BF16); B_g = Buf()
            gT = sb(st, "pgT", [128, 4, 128], BF16); B_gT = Buf()
            s2 = sb(st, "ps2", [128, 4, 128]); B_s2 = Buf()
            pGT = ps(st, "pGT", [128, 8, 128], BF16); B_pGT = Buf()
            pz = ps(st, "pz", [128, 4, 128]); B_pz = Buf()
            for ti in range(NTILE):
                i = ti % 2
                T0 = ti * 128
                row0 = T0 + CTX if ti < NLT else T0 - SEQ
                k.dma("sp", yt[i][:], y_scr[row0:row0 + 128, :], writes=[B_yt[i]])
                k.dma("sp", ut[i][:], u_scr[row0:row0 + 128, :], writes=[B_ut[i]])
                k.op("dve", lambda e, i=i: e.tensor_tensor(out=xx[:], in0=ut[i][:], in1=dbc[0][:], op=ALU.mult), reads=[B_ut[i], dbc[1]], writes=[B_xx])
                k.op("dve", lambda e, i=i: e.tensor_tensor(out=xx[:], in0=xx[:], in1=yt[i][:], op=ALU.add), reads=[B_xx, B_yt[i]], writes=[B_xx])
                k.op("pool", lambda e: e.tensor_tensor(out=ww[:], in0=xx[:], in1=xx[:], op=ALU.mult), reads=[B_xx], writes=[B_ww])
                k.op("pool", lambda e: e.tensor_scalar(out=ww[:], in0=ww[:], scalar1=0.044715, scalar2=1.0, op0=ALU.mult, op1=ALU.add), reads=[B_ww], writes=[B_ww])
                k.op("pool", lambda e: e.tensor_tensor(out=ww[:], in0=ww[:], in1=xx[:], op=ALU.mult), reads=[B_ww, B_xx], writes=[B_ww])
                k.op("act", lambda e: e.activation(out=sg[:], in_=ww[:], func=AF.Sigmoid, scale=GC), reads=[B_ww], writes=[B_sg])
                k.op("dve", lambda e: e.tensor_tensor(out=g_bf[:], in0=xx[:], in1=sg[:], op=ALU.mult), reads=[B_xx, B_sg], writes=[B_g])
                if "dbg_g" in dbg:
                    if ti == 0:
                        dbg_g = dscr("dbg_g", [NT, 512], BF16)
                    k.dma("sp", dbg_g[T0:T0 + 128, :], g_bf[:], reads=[B_g])
                for kc in range(4):
                    k.op("pe", lambda e, kc=kc: e.transpose(out=pGT[:, kc, :], in_=g_bf[:, kc * 128:(kc + 1) * 128], identity=ident_b[:]),
                         reads=[B_g, B_ident_b], writes=[B_pGT])
                k.op("act", lambda e: e.copy(out=gT[:], in_=pGT[:, 0:4, :]), reads=[B_pGT], writes=[B_gT])
                for n_ in range(4):
                    for kc in range(4):
                        k.op("pe", lambda e, n_=n_, kc=kc: e.matmul(pz[:, n_, :], lhsT=gw[:, kc, n_ * 128:(n_ + 1) * 128], rhs=gT[:, kc, :],
                                                                    start=(kc == 0), stop=(kc == 3)), reads=[B_gw, B_gT], writes=[B_pz])
                for n_ in range(4):
                    k.op("act", lambda e, n_=n_: e.activation(out=s2[:, n_, :], in_=pz[:, n_, :], func=AF.Sigmoid, bias=gb[:, n_:n_ + 1], scale=1.0),
                         reads=[B_pz, B_gb], writes=[B_s2])
                k.op("dve", lambda e, T0=T0: e.tensor_tensor(out=catT_[:, 4:8, T0:T0 + 128], in0=gT[:], in1=s2[:], op=ALU.mult),
                     reads=[B_gT, B_s2], writes=[B_catT_])
            if "dbg_cat" in dbg:
                dbg_cat = dscr("dbg_cat", [128, 8, NT], BF16)
                k.dma("sp", dbg_cat, catT_[:], reads=[B_catT_])
            k.barrier()


        def layer1_mixer():
            LAM_INIT = 0.8 - 0.6 * math.exp(-0.3 * 1)
            SC = 0.125
            with ExitStack() as L1:
                qT2 = sb(L1, "qT2", [128, 8, SEQ], BF16); B_q2 = Buf()
                kT2 = sb(L1, "kT2", [128, 8, NT], BF16); B_k2 = Buf()
                v1 = sb(L1, "v1", [128, NTILE, D], BF16); B_v1 = Buf()
                nmax = sb(L1, "nmax", [128, 32]); B_nmax = Buf()
                for st in phase("l1proj"):
                    w_bf = sb(st, "dif_w_bf", [128, 8, 3072], BF16); B_w = Buf()
                    for kc in range(8):
                        k.dma("pool", w_bf[:, kc, :], dif_w_in[kc * 128:(kc + 1) * 128, :], writes=[B_w])
                    sc1p = [mod_bc(st, f"l1sc1p_{r}", 1, r, 1, plus1=True) for r in range(2)]
                    sh1 = [mod_bc(st, f"l1sh1_{r}", 1, r, 0) for r in range(2)]
                    xt = [sb(st, f"l1xt{i}", [128, D]) for i in range(2)]; B_xt = [Buf(), Buf()]
                    tmpf = sb(st, "l1tmpf", [128, D]); B_tmpf = Buf()
                    h_bf = sb(st, "l1h_bf", [128, D], BF16); B_hbf = Buf()
                    hT = [sb(st, f"l1hT{i}", [128, 8, 128], BF16) for i in range(2)]; B_hT = [Buf(), Buf()]
                    rt = [sb(st, f"l1rt{i}", [128, 64]) for i in range(2)]; B_rt = [Buf(), Buf()]
                    t1 = sb(st, "l1rope_t1", [128, 512]); t2 = sb(st, "l1rope_t2", [128, 512]); B_rtmp = Buf()
                    qk_bf = sb(st, "l1qk_bf", [128, 512], BF16); B_qk = Buf()
                    pT = ps(st, "l1pT", [128, 8, 128], BF16); B_pT = Buf()
                    pp = [ps(st, f"l1pp{i}", [128, 512]) for i in range(3)]; B_pp = [Buf(), Buf(), Buf()]
                    pq = [ps(st, f"l1pq{i}", [128, 8, 128], BF16) for i in range(2)]; B_pq = [Buf(), Buf()]
                    sqt = sb(st, "l1sq", [128, 512]); rs8 = sb(st, "l1rs8", [128, 8]); B_sq = Buf()
                    k.op("dve", lambda e: e.memset(nmax[:], 0.0), writes=[B_nmax])
                    ib = 0
                    for ti in range(NTILE):
                        i = ti % 2
                        r = 0 if ti < NLT else 1
                        T0 = ti * 128
                        k.dma("sp", xt[i][:], src_rows(ti, 1), writes=[B_xt[i]])
                        if r == 0:
                            k.dma("sp", rt[i][:], rope_cs[T0:T0 + 128, :], writes=[B_rt[i]])
                        k.op("dve", lambda e, i=i, r=r: e.tensor_tensor(out=tmpf[:], in0=xt[i][:], in1=sc1p[r][0][:], op=ALU.mult),
                             reads=[B_xt[i], sc1p[r][1]], writes=[B_tmpf])
                        k.op("pool", lambda e, r=r: e.tensor_tensor(out=h_bf[:], in0=tmpf[:], in1=sh1[r][0][:], op=ALU.add),
                             reads=[B_tmpf, sh1[r][1]], writes=[B_hbf])
                        for kc in range(8):
                            k.op("pe", lambda e, kc=kc: e.transpose(out=pT[:, kc, :], in_=h_bf[:, kc * 128:(kc + 1) * 128], identity=ident_b[:]),
                                 reads=[B_hbf, B_ident_b], writes=[B_pT])
                        k.op("act", lambda e, i=i: e.copy(out=hT[i][:], in_=pT[:]), reads=[B_pT], writes=[B_hT[i]])
                        for cb in range(6):
                            if r == 1 and cb < 2:
                                continue
                            j = ib % 3; ib += 1
                            for kc in range(8):
                                k.op("pe", lambda e, kc=kc, j=j, cb=cb, i=i: e.matmul(
                                    pp[j][:], lhsT=hT[i][:, kc, :], rhs=w_bf[:, kc, cb * 512:(cb + 1) * 512],
                                    start=(kc == 0), stop=(kc == 7)), reads=[B_hT[i], B_w], writes=[B_pp[j]])
                            if cb >= 4:
                                c0 = (cb - 4) * 512
                                k.op("act", lambda e, j=j, ti=ti, c0=c0: e.copy(out=v1[:, ti, c0:c0 + 512], in_=pp[j][:]), reads=[B_pp[j]], writes=[B_v1])
                                continue
                            if r == 0:
                                rope_apply(None, pp[j][:], B_pp[j], qk_bf[:], B_qk, 8, rt[i], B_rt[i], t1[:], t2[:], B_rtmp)
                            else:
                                k.op("dve", lambda e, j=j: e.tensor_copy(out=qk_bf[:], in_=pp[j][:]), reads=[B_pp[j]], writes=[B_qk])
                            k.op("dve", lambda e: e.tensor_tensor(out=sqt[:], in0=qk_bf[:], in1=qk_bf[:], op=ALU.mult), reads=[B_qk, B_sq], writes=[B_sq])
                            k.op("dve", lambda e: e.tensor_reduce(out=rs8[:], in_=sqt[:].rearrange("p (m d) -> p m d", d=64), axis=AX.X, op=ALU.add), reads=[B_sq], writes=[B_sq])
                            k.op("dve", lambda e, cb=cb: e.tensor_tensor(out=nmax[:, cb * 8:(cb + 1) * 8], in0=nmax[:, cb * 8:(cb + 1) * 8], in1=rs8[:], op=ALU.max),
                                 reads=[B_sq, B_nmax], writes=[B_nmax])
                            jq = cb % 2
                            for hh in range(4):
                                k.op("pe", lambda e, hh=hh, jq=jq: e.transpose(out=pq[jq][:, hh, :], in_=qk_bf[:, hh * 128:(hh + 1) * 128], identity=ident_b[:]),
                                     reads=[B_qk, B_ident_b], writes=[B_pq[jq]])
                            dstT, BD = (qT2, B_q2) if cb < 2 else (kT2, B_k2)
                            h0 = (cb % 2) * 4
                            k.op("act", lambda e, jq=jq, dstT=dstT, h0=h0, T0=T0: e.copy(out=dstT[:, h0:h0 + 4, T0:T0 + 128], in_=pq[jq][:, 0:4, :]),
                                 reads=[B_pq[jq]], writes=[BD])
                    k.barrier()
                for st in phase("l1att"):
                    w_bf = sb(st, "difwo_bf", [128, 8, D], BF16); B_w = Buf()
                    for kc in range(8):
                        k.dma("pool", w_bf[:, kc, :], dif_w_out[kc * 128:(kc + 1) * 128, :], writes=[B_w])
                    g1 = mod_bc(st, "l1g1", 1, 0, 2)
                    lng = load_bc(st, "l1ln1g", ln1_g[1:2, :], D)
                    lnb = load_bc(st, "l1ln1b", ln1_b[1:2, :], D)
                    wk = ln_work(st, "l1e1")
                    xo = [sb(st, f"l1xo{i}", [128, D]) for i in range(2)]; B_xo = [Buf(), Buf()]
                    lam = sb(st, "lam", [128, 8]); B_lam = Buf()
                    lq = [load_bc(st, f"lq{i}", a[0:1, :], 64) for i, a in enumerate((lam_q1, lam_k1, lam_q2, lam_k2))]
                    ltmp = sb(st, "ltmp", [128, 64]); B_lt = Buf()
                    for i2 in range(2):
                        k.op("dve", lambda e, i2=i2: e.tensor_tensor(out=ltmp[:], in0=lq[2 * i2][0][:], in1=lq[2 * i2 + 1][0][:], op=ALU.mult),
                             reads=[lq[2 * i2][1], lq[2 * i2 + 1][1], B_lt], writes=[B_lt])
                        k.op("dve", lambda e, i2=i2: e.reduce_sum(out=lam[:, i2:i2 + 1], in_=ltmp[:], axis=AX.X), reads=[B_lt, B_lam], writes=[B_lam])
                    k.op("act", lambda e: e.activation(out=lam[:, 2:4], in_=lam[:, 0:2], func=AF.Exp), reads=[B_lam], writes=[B_lam])
                    k.op("dve", lambda e: e.tensor_tensor(out=lam[:, 4:5], in0=lam[:, 2:3], in1=lam[:, 3:4], op=ALU.subtract), reads=[B_lam], writes=[B_lam])
                    k.op("dve", lambda e: e.tensor_scalar(out=lam[:, 5:6], in0=lam[:, 4:5], scalar1=-1.0, scalar2=-LAM_INIT, op0=ALU.mult, op1=ALU.add), reads=[B_lam], writes=[B_lam])
                    sg_col = sb(st, "sg_col", [128, 1]); B_sg = Buf()
                    with nc.allow_non_contiguous_dma(reason="tiny"):
                        k.dma("sp", sg_col[:], subln_g[0, :].rearrange("(p x) -> p x", x=1), writes=[B_sg])
                    k.op("dve", lambda e: e.tensor_scalar(out=sg_col[:], in0=sg_col[:], scalar1=1.0 - LAM_INIT, scalar2=None, op0=ALU.mult), reads=[B_sg], writes=[B_sg])
                    ones_bf = sb(st, "ones_bf", [128, 128], BF16); B_ones = Buf()
                    k.op("dve", lambda e: e.memset(ones_bf[:], 1.0), writes=[B_ones])
                    negC = sb(st, "negC", [128, 16]); B_negC = Buf()
                    with ExitStack() as t0:
                        nb = sb(t0, "nmax_bf", [128, 32], BF16); B_nb = Buf()
                        k.op("dve", lambda e: e.tensor_scalar(out=nb[:], in0=nmax[:], scalar1=1.02, scalar2=None, op0=ALU.mult), reads=[B_nmax], writes=[B_nb])
                        pn = ps(t0, "pn", [16, 1024], BF16); B_pn = Buf()
                        k.op("pe", lambda e: e.transpose(out=pn[:, 0:128], in_=nb[:, 0:16], identity=ident_b[:]), reads=[B_nb, B_ident_b], writes=[B_pn])
                        k.op("pe", lambda e: e.transpose(out=pn[:, 128:256], in_=nb[:, 16:32], identity=ident_b[:]), reads=[B_nb, B_ident_b], writes=[B_pn])
                        r2 = sb(t0, "r2", [16, 8]); B_r2 = Buf()
                        k.op("dve", lambda e: e.reduce_max(out=r2[:, 0:1], in_=pn[:, 0:128], axis=AX.X), reads=[B_pn], writes=[B_r2])
                        k.op("dve", lambda e: e.reduce_max(out=r2[:, 1:2], in_=pn[:, 128:256], axis=AX.X), reads=[B_pn, B_r2], writes=[B_r2])
                        k.op("dve", lambda e: e.tensor_tensor(out=r2[:, 2:3], in0=r2[:, 0:1], in1=r2[:, 1:2], op=ALU.mult), reads=[B_r2], writes=[B_r2])
                        k.op("act", lambda e: e.sqrt(out=r2[:, 3:4], in_=r2[:, 2:3]), reads=[B_r2], writes=[B_r2])
                        k.op("dve", lambda e: e.tensor_scalar(out=r2[:, 4:5], in0=r2[:, 3:4], scalar1=-SC, scalar2=None, op0=ALU.mult), reads=[B_r2], writes=[B_r2])
                        dg = sb(t0, "dgC", [16, 16], BF16); B_dg = Buf()
                        k.op("dve", lambda e: e.tensor_scalar(out=dg[:], in0=ident_f[0:16, 0:16], scalar1=r2[:, 4:5], scalar2=None, op0=ALU.mult), reads=[B_r2, B_ident_f], writes=[B_dg])
                        pc = ps(t0, "pcb", [128, 512]); B_pc = Buf()
                        k.op("pe", lambda e: e.matmul(pc[:, 0:16], lhsT=ones_bf[0:16, :], rhs=dg[:], start=True, stop=True), reads=[B_ones, B_dg], writes=[B_pc])
                        k.op("dve", lambda e: e.tensor_copy(out=negC[:], in_=pc[:, 0:16]), reads=[B_pc], writes=[B_negC])
                        k.barrier()
                    ET = [sb(st, f"ET{i}", [128, 512], BF16) for i in range(4)]; B_ET = [Buf() for _ in range(4)]
                    aoT = sb(st, "aoT_all", [128, 8, 512], BF16); B_aoT = Buf()
                    rz = sb(st, "rz", [1, 2, 512]); B_rz = Buf()
                    rzb = sb(st, "rzb", [1, 4, 512], BF16); B_rzb = Buf()
                    bcs = sb(st, "bcs", [128, 512]); B_bcs = Buf()
                    oT = sb(st, "oT", [128, 512]); B_oT = Buf()
                    t5 = sb(st, "t5", [128, 512]); B_t5 = Buf()
                    sqb = sb(st, "sqb", [128, 512], BF16); B_sqb = Buf()
                    pS = [ps(st, f"pS{i}", [128, 512]) for i in range(2)]; B_pS = [Buf(), Buf()]
                    pO4 = [ps(st, f"pO{i}", [128, 512]) for i in range(4)]; B_pO4 = [Buf() for _ in range(4)]
                    pZ1 = ps(st, "pZ", [1, 512]); B_pZ1 = Buf()
                    pZ = [pZ1, pZ1]; B_pZ = [B_pZ1, B_pZ1]
                    pB = ps(st, "pB", [128, 512]); B_pB = Buf()
                    cnt = {"s": 0, "e": 0}

                    def bcast_row(hi, lo):
                        k.op("pe", lambda e: e.matmul(pB[:], lhsT=ones_bf[0:1, :], rhs=hi, start=True, stop=False), reads=[B_ones, B_rzb], writes=[B_pB])
                        k.op("pe", lambda e: e.matmul(pB[:], lhsT=ones_bf[0:1, :], rhs=lo, start=False, stop=True), reads=[B_ones, B_rzb], writes=[B_pB])
                        k.op("act", lambda e: e.copy(out=bcs[:], in_=pB[:]), reads=[B_pB], writes=[B_bcs])

                    def split_row(src, j):
                        k.op("dve", lambda e: e.tensor_copy(out=rzb[:, 2 * j, :], in_=src), reads=[B_rz], writes=[B_rzb])
                        k.op("dve", lambda e: e.tensor_tensor(out=rzb[:, 2 * j + 1, :], in0=src, in1=rzb[:, 2 * j, :], op=ALU.subtract), reads=[B_rz, B_rzb], writes=[B_rzb])

                    zs = sb(st, "zs", [1, 2, 512]); B_zs = Buf()

                    def qk_exp(Q0, h, c, b):
                        ps_ = slice(c * 64, (c + 1) * 64)
                        m = h * 2 + c
                        js = cnt["s"] % 2; cnt["s"] += 1
                        je = cnt["e"] % 4; cnt["e"] += 1
                        k.op("pe", lambda e: e.matmul(pS[js][:], lhsT=kT2[ps_, h, b * 128:(b + 1) * 128], rhs=qT2[ps_, h, Q0:Q0 + 512],
                                                      start=True, stop=True), reads=[B_k2, B_q2], writes=[B_pS[js]])
                        k.op("act", lambda e: e.activation(out=ET[je][:], in_=pS[js][:], func=AF.Exp, bias=negC[:, m:m + 1], scale=SC),
                             reads=[B_pS[js], B_negC], writes=[B_ET[je]])
                        return je

                    def pvz(h, c, b, je):
                        pO = pO4[(h % 2) * 2:(h % 2) * 2 + 2]; B_pO = B_pO4[(h % 2) * 2:(h % 2) * 2 + 2]
                        Zacc = Zacc4[(h % 2) * 2:(h % 2) * 2 + 2]; B_Zacc = B_Zacc4[(h % 2) * 2:(h % 2) * 2 + 2]
                        k.op("pe", lambda e: e.matmul(pO[c][:], lhsT=v1[:, b, h * 128:(h + 1) * 128], rhs=ET[je][:],
                                                      start=(b == 0), stop=(b == NTILE - 1)), reads=[B_v1, B_ET[je]], writes=[B_pO[c]])
                        if b == 0:
                            k.op("dve", lambda e: e.tensor_copy(out=Zacc[c][:], in_=ET[je][:]), reads=[B_ET[je]], writes=[B_Zacc[c]])
                        else:
                            k.op("dve", lambda e: e.tensor_tensor(out=Zacc[c][:], in0=Zacc[c][:], in1=ET[je][:], op=ALU.add), reads=[B_ET[je], B_Zacc[c]], writes=[B_Zacc[c]])

                    Zacc4 = [sb(st, f"Zacc{i}", [128, 512]) for i in range(4)]; B_Zacc4 = [Buf() for _ in range(4)]
                    ones_f = sb(st, "ones_f", [128, 1]); B_onesf = Buf()
                    k.op("dve", lambda e: e.memset(ones_f[:], 1.0), writes=[B_onesf])

                    def bcast_recip(c, Zacc, B_Zacc):
                        k.op("pe", lambda e: e.matmul(pZ[c][:], lhsT=ones_f[:, 0:1], rhs=Zacc[c][:], start=True, stop=True), reads=[B_onesf, B_Zacc[c]], writes=[B_pZ[c]])
                        k.op("act", lambda e: e.copy(out=zs[:, c, :], in_=pZ[c][:]), reads=[B_pZ[c], B_zs], writes=[B_zs])
                        k.op("dve", lambda e: e.tensor_copy(out=rzb[:, 2 * c, :], in_=zs[:, c, :]), reads=[B_zs, B_rzb], writes=[B_rzb])
                        k.op("dve", lambda e: e.tensor_tensor(out=rzb[:, 2 * c + 1, :], in0=zs[:, c, :], in1=rzb[:, 2 * c, :], op=ALU.subtract), reads=[B_zs, B_rzb], writes=[B_rzb])
                        k.op("pe", lambda e: e.matmul(pB[:], lhsT=ones_bf[0:1, :], rhs=rzb[:, 2 * c, :], start=True, stop=False), reads=[B_ones, B_rzb], writes=[B_pB])
                        k.op("pe", lambda e: e.matmul(pB[:], lhsT=ones_bf[0:1, :], rhs=rzb[:, 2 * c + 1, :], start=False, stop=True), reads=[B_ones, B_rzb], writes=[B_pB])
                        k.op("dve", lambda e: e.reciprocal(out=bcs[:], in_=pB[:]), reads=[B_pB, B_bcs], writes=[B_bcs])

                    def epilogue(h):
                        pO = pO4[(h % 2) * 2:(h % 2) * 2 + 2]; B_pO = B_pO4[(h % 2) * 2:(h % 2) * 2 + 2]
                        Zacc = Zacc4[(h % 2) * 2:(h % 2) * 2 + 2]; B_Zacc = B_Zacc4[(h % 2) * 2:(h % 2) * 2 + 2]
                        bcast_recip(0, Zacc, B_Zacc)
                        k.op("dve", lambda e: e.tensor_tensor(out=oT[:], in0=pO[0][:], in1=bcs[:], op=ALU.mult), reads=[B_pO[0], B_bcs, B_oT], writes=[B_oT])
                        bcast_recip(1, Zacc, B_Zacc)
                        k.op("dve", lambda e: e.tensor_tensor(out=t5[:], in0=pO[1][:], in1=bcs[:], op=ALU.mult), reads=[B_pO[1], B_bcs, B_t5], writes=[B_t5])
                        k.op("dve", lambda e: e.scalar_tensor_tensor(out=oT[:], in0=t5[:], scalar=lam[:, 5:6], in1=oT[:], op0=ALU.mult, op1=ALU.add),
                             reads=[B_oT, B_t5, B_lam], writes=[B_oT])
                        k.op("dve", lambda e: e.tensor_tensor(out=sqb[:], in0=oT[:], in1=oT[:], op=ALU.mult), reads=[B_oT, B_sqb], writes=[B_sqb])
                        k.op("pe", lambda e: e.matmul(pZ[0][:], lhsT=ones_bf[:, 0:1], rhs=sqb[:], start=True, stop=True), reads=[B_ones, B_sqb], writes=[B_pZ[0]])
                        k.op("act", lambda e: e.activation(out=rz[:, 0, :], in_=pZ[0][:], func=AF.Ln, scale=1.0 / 128.0, bias=eps_t[0:1, :]), reads=[B_pZ[0], B_rz, B_eps], writes=[B_rz])
                        k.op("act", lambda e: e.activation(out=rz[:, 0, :], in_=rz[:, 0, :], func=AF.Exp, scale=-0.5), reads=[B_rz], writes=[B_rz])
                        split_row(rz[:, 0, :], 0)
                        bcast_row(rzb[:, 0, :], rzb[:, 1, :])
                        k.op("dve", lambda e: e.scalar_tensor_tensor(out=aoT[:, h, :], in0=oT[:], scalar=sg_col[:, 0:1], in1=bcs[:], op0=ALU.mult, op1=ALU.mult),
                             reads=[B_oT, B_sg, B_bcs], writes=[B_aoT])

                    eps_t = sb(st, "eps_t", [128, 1]); B_eps = Buf()
                    k.op("dve", lambda e: e.memset(eps_t[:], 1e-5), writes=[B_eps])
                    for qg in range(4):
                        Q0 = qg * 512
                        steps = [(h, c, b) for h in range(8) for c in range(2) for b in range(NTILE)]
                        je_next = qk_exp(Q0, *steps[0])
                        pending = []
                        for si, (h, c, b) in enumerate(steps):
                            je_cur = je_next
                            if si + 1 < len(steps):
                                je_next = qk_exp(Q0, *steps[si + 1])
                            pvz(h, c, b, je_cur)
                            if pending:
                                a_, kw_ = pending.pop(0)
                                k.op(*a_, **kw_)
                            if c == 1 and b == NTILE - 1:
                                while pending:
                                    a_, kw_ = pending.pop(0)
                                    k.op(*a_, **kw_)
                                orig_op = k.op
                                rec = []
                                k.op = lambda *a, rec=rec, **kw: rec.append((a, kw))
                                epilogue(h)
                                k.op = orig_op
                                pending = rec
                        while pending:
                            a_, kw_ = pending.pop(0)
                            k.op(*a_, **kw_)
                        for tt in range(4):
                            ti = qg * 4 + tt
                            T0 = ti * 128
                            i = ti % 2
                            k.dma("sp", xo[i][:], src_rows(ti, 1), writes=[B_xo[i]])
                            for hf in range(2):
                                for h in range(8):
                                    k.op("pe", lambda e, h=h, hf=hf, tt=tt: e.matmul(pS[hf][:], lhsT=aoT[:, h, tt * 128:(tt + 1) * 128], rhs=w_bf[:, h, hf * 512:(hf + 1) * 512],
                                                                                start=(h == 0), stop=(h == 7)), reads=[B_aoT, B_w], writes=[B_pS[hf]])
                            ln_epilogue(wk, [pS[0][:], pS[1][:]], [B_pS[0], B_pS[1]], xo[i], B_xo[i], g1, lng, lnb, x1_scr[T0:T0 + 128, :])
                    k.barrier()

        with ExitStack() as L0:
            catT = sb(L0, "catT", [128, 8, NT], BF16); B_catT = Buf()
            LA = ExitStack()
            qT = sb(LA, "qT", [64, 8, NT], BF16); B_qT = Buf()
            kT = sb(LA, "kT", [64, 2, NT], BF16); B_kT = Buf()
            v_all = sb(LA, "v_all", [128, NTILE, 128], BF16); B_v = Buf()
            for st in phase("l0proj"):
                w_bf = sb(st, "w_in_bf", [128, 8, 1280], BF16); B_w = Buf()
                for kc in range(8):
                    k.dma("pool", w_bf[:, kc, :], w_in0[kc * 128:(kc + 1) * 128, :], writes=[B_w])
                sc1p = [None, None]; sh1 = [None, None]
                for r in range(2):
                    sh1[r] = mod_bc(st, f"sh1_{r}", 0, r, 0)
                    sc1p[r] = mod_bc(st, f"sc1p_{r}", 0, r, 1, plus1=True)
                xt = [sb(st, f"xt{i}", [128, D]) for i in range(2)]; B_xt = [Buf(), Buf()]
                tmpf = sb(st, "tmpf", [128, D]); B_tmpf = Buf()
                h_bf = sb(st, "h_bf", [128, D], BF16); B_hbf = Buf()
                hT = [sb(st, f"hT{i}", [128, 8, 128], BF16) for i in range(2)]; B_hT = [Buf(), Buf()]
                rt = [sb(st, f"rt{i}", [128, 64]) for i in range(2)]; B_rt = [Buf(), Buf()]
                t1 = sb(st, "rope_t1", [128, 640]); t2 = sb(st, "rope_t2", [128, 640]); B_rtmp = Buf()
                qk_bf = sb(st, "qk_bf", [128, 640], BF16); B_qk = Buf()
                ut = [sb(st, f"ut{i}", [128, 512]) for i in range(2)]; B_ut = [Buf(), Buf()]
                pT = ps(st, "pT", [128, 8, 128], BF16); B_pT = Buf()
                pp = [ps(st, f"pp{i}", [128, 512]) for i in range(3)]; B_pp = [Buf(), Buf(), Buf()]
                pq = ps(st, "pq", [64, 8, 128], BF16); B_pq = Buf()
                pk = ps(st, "pk", [64, 8, 128], BF16); B_pk = Buf()
                for ti in range(NTILE):
                    i = ti % 2
                    r = 0 if ti < NLT else 1
                    T0 = ti * 128
                    k.dma("sp", xt[i][:], src_rows(ti, 0), writes=[B_xt[i]])
                    if r == 0:
                        k.dma("sp", rt[i][:], rope_cs[T0:T0 + 128, :], writes=[B_rt[i]])
                    k.op("dve", lambda e, i=i, r=r: e.tensor_tensor(out=tmpf[:], in0=xt[i][:], in1=sc1p[r][0][:], op=ALU.mult),
                         reads=[B_xt[i], sc1p[r][1]], writes=[B_tmpf])
                    k.op("pool", lambda e, r=r: e.tensor_tensor(out=h_bf[:], in0=tmpf[:], in1=sh1[r][0][:], op=ALU.add),
                         reads=[B_tmpf, sh1[r][1]], writes=[B_hbf])
                    for kc in range(8):
                        k.op("pe", lambda e, kc=kc: e.transpose(out=pT[:, kc, :], in_=h_bf[:, kc * 128:(kc + 1) * 128], identity=ident_b[:]),
                             reads=[B_hbf, B_ident_b], writes=[B_pT])
                    k.op("act", lambda e, i=i: e.copy(out=hT[i][:], in_=pT[:]), reads=[B_pT], writes=[B_hT[i]])
                    for nb, (c0, c1) in enumerate(((0, 512), (512, 1024), (1024, 1280))):
                        for kc in range(8):
                            k.op("pe", lambda e, kc=kc, nb=nb, c0=c0, c1=c1, i=i: e.matmul(
                                pp[nb][:, 0:c1 - c0], lhsT=hT[i][:, kc, :], rhs=w_bf[:, kc, c0:c1],
                                start=(kc == 0), stop=(kc == 7)),
                                reads=[B_hT[i], B_w], writes=[B_pp[nb]])
                    if "dbg_q" in dbg:
                        if ti == 0:
                            dq = sb(st, "dq", [128, 1280]); B_dq = Buf()
                        for nb, (c0, c1) in enumerate(((0, 512), (512, 1024), (1024, 1280))):
                            k.op("dve", lambda e, nb=nb, c0=c0, c1=c1: e.tensor_copy(out=dq[:, c0:c1], in_=pp[nb][:, 0:c1 - c0]),
                                 reads=[B_pp[nb]], writes=[B_dq])
                        k.dma("sp", dbg_q[T0:T0 + 128, :], dq[:], reads=[B_dq])
                    if r == 0:
                        rope_apply(None, pp[0][:, 0:512], B_pp[0], qk_bf[:, 0:512], B_qk, 8, rt[i], B_rt[i], t1[:, 0:512], t2[:, 0:512], B_rtmp)
                        rope_apply(None, pp[1][:, 0:128], B_pp[1], qk_bf[:, 512:640], B_qk, 2, rt[i], B_rt[i], t1[:, 512:640], t2[:, 512:640], B_rtmp)
                    else:
                        k.op("dve", lambda e: e.tensor_copy(out=qk_bf[:, 0:512], in_=pp[0][:, 0:512]), reads=[B_pp[0]], writes=[B_qk])
                        k.op("dve", lambda e: e.tensor_copy(out=qk_bf[:, 512:640], in_=pp[1][:, 0:128]), reads=[B_pp[1]], writes=[B_qk])
                    for h in range(8):
                        k.op("pe", lambda e, h=h: e.transpose(out=pq[:, h, :], in_=qk_bf[:, h * 64:(h + 1) * 64], identity=ident_b[:]),
                             reads=[B_qk, B_ident_b], writes=[B_pq])
                    for h in range(2):
                        k.op("pe", lambda e, h=h: e.transpose(out=pk[:, h, :], in_=qk_bf[:, 512 + h * 64:512 + (h + 1) * 64], identity=ident_b[:]),
                             reads=[B_qk, B_ident_b], writes=[B_pk])
                    k.op("act", lambda e, T0=T0: e.copy(out=qT[:, :, T0:T0 + 128], in_=pq[:]), reads=[B_pq], writes=[B_qT])
                    k.op("act", lambda e, T0=T0: e.copy(out=kT[:, :, T0:T0 + 128], in_=pk[:, 0:2, :]), reads=[B_pk], writes=[B_kT])
                    k.op("act", lambda e, ti=ti: e.copy(out=v_all[:, ti, :], in_=pp[1][:, 128:256]), reads=[B_pp[1]], writes=[B_v])
                    k.op("act", lambda e, i=i: e.copy(out=ut[i][:, 0:256], in_=pp[1][:, 256:512]), reads=[B_pp[1]], writes=[B_ut[i]])
                    k.op("act", lambda e, i=i: e.copy(out=ut[i][:, 256:512], in_=pp[2][:, 0:256]), reads=[B_pp[2]], writes=[B_ut[i]])
                    urow = T0 + CTX if r == 0 else T0 - SEQ
                    k.dma("sp", u_scr[urow:urow + 128, :], ut[i][:], reads=[B_ut[i]])
                if "dbg_qT" in dbg:
                    dbg_qT = dscr("dbg_qT", [64, 8, NT], BF16)
                    k.dma("sp", dbg_qT, qT[:], reads=[B_qT])
                k.barrier()


            for st in phase("l0att"):
                SC = 0.125
                maskL = sb(st, "maskL_sb", [128, 128]); maskR = sb(st, "maskR_sb", [128, 128]); B_mask = Buf()
                k.dma("sp", maskL[:], maskL_in[:, :], writes=[B_mask])
                k.dma("sp", maskR[:], maskR_in[:, :], writes=[B_mask])
                sink_bc, B_sink = load_bc(st, "sink_bc", swa_sink[0:1, :], 8)
                sm = [sb(st, f"sm{i}", [128, 640]) for i in range(2)]; B_sm = [Buf(), Buf()]
                P = [sb(st, f"P{i}", [128, 640], BF16) for i in range(2)]; B_P = [Buf(), Buf()]
                PT = [sb(st, f"PT{i}", [128, 5, 128], BF16) for i in range(2)]; B_PT = [Buf(), Buf()]
                stat = [sb(st, f"stat{i}", [128, 8]) for i in range(2)]; B_stat = [Buf(), Buf()]
                att_bf = sb(st, "att_bf", [128, 512], BF16); B_att = Buf()
                ps_loc = [ps(st, f"ps_loc{i}", [128, 512]) for i in range(2)]; B_psl = [Buf(), Buf()]
                ps_ctx = [ps(st, f"ps_ctx{i}", [128, 512]) for i in range(2)]; B_psc = [Buf(), Buf()]
                pPT = ps(st, "pPT", [128, 8, 128], BF16); B_pPT = Buf()
                po = ps(st, "po", [128, 512]); B_po = Buf()
                pcat = ps(st, "pcat", [128, 8, 128], BF16); B_pcat = Buf()
                it = 0
                for ti in range(NTILE):
                    T0 = ti * 128
                    lat = ti < NLT
                    if lat:
                        j0 = max(0, ti - 1); j1 = min(NLT - 1, ti + 1)
                        nloc = (j1 - j0 + 1) * 128
                        blocks = list(range(j0, j1 + 1)) + [NLT, NLT + 1]
                    else:
                        nloc = 0
                        blocks = [NLT, NLT + 1]
                    n = nloc + 256
                    pend_B = None
                    for h in range(8):
                        i = it % 2; it += 1
                        kvh = h // 4
                        rec_ = []
                        orig_op_ = k.op
                        k.op = lambda *a, rec_=rec_, **kw: rec_.append((a, kw))
                        if lat:
                            k.op("pe", lambda e, i=i, h=h, kvh=kvh, j0=j0, nloc=nloc, T0=T0: e.matmul(
                                ps_loc[i][:, 0:nloc], lhsT=qT[:, h, T0:T0 + 128], rhs=kT[:, kvh, j0 * 128:j0 * 128 + nloc],
                                start=True, stop=True), reads=[B_qT, B_kT], writes=[B_psl[i]])
                        k.op("pe", lambda e, i=i, h=h, kvh=kvh, T0=T0: e.matmul(
                            ps_ctx[i][:, 0:256], lhsT=qT[:, h, T0:T0 + 128], rhs=kT[:, kvh, SEQ:NT],
                            start=True, stop=True), reads=[B_qT, B_kT], writes=[B_psc[i]])
                        if lat:
                            for bi, j in enumerate(range(j0, j1 + 1)):
                                sl = slice(bi * 128, (bi + 1) * 128)
                                if j == ti:
                                    k.op("act", lambda e, i=i, sl=sl: e.mul(out=sm[i][:, sl], in_=ps_loc[i][:, sl], mul=SC),
                                         reads=[B_psl[i]], writes=[B_sm[i]])
                                else:
                                    mk = maskL if j < ti else maskR
                                    k.op("dve", lambda e, i=i, sl=sl, mk=mk: e.scalar_tensor_tensor(
                                        out=sm[i][:, sl], in0=ps_loc[i][:, sl], scalar=SC, in1=mk[:], op0=ALU.mult, op1=ALU.add),
                                        reads=[B_psl[i], B_mask], writes=[B_sm[i]])
                        k.op("act", lambda e, i=i, nloc=nloc: e.mul(out=sm[i][:, nloc:nloc + 256], in_=ps_ctx[i][:, 0:256], mul=SC),
                             reads=[B_psc[i]], writes=[B_sm[i]])
                        sti = stat[i]
                        k.op("dve", lambda e, i=i, n=n, sti=sti: e.reduce_max(out=sti[:, 0:1], in_=sm[i][:, 0:n], axis=AX.X),
                             reads=[B_sm[i]], writes=[B_stat[i]])
                        k.op("dve", lambda e, sti=sti, h=h: e.tensor_tensor(out=sti[:, 1:2], in0=sti[:, 0:1], in1=sink_bc[:, h:h + 1], op=ALU.max),
                             reads=[B_stat[i], B_sink], writes=[B_stat[i]])
                        k.op("dve", lambda e, sti=sti: e.tensor_scalar(out=sti[:, 2:3], in0=sti[:, 1:2], scalar1=-1.0, scalar2=None, op0=ALU.mult),
                             reads=[B_stat[i]], writes=[B_stat[i]])
                        k.op("act", lambda e, i=i, n=n, sti=sti: e.activation(out=P[i][:, 0:n], in_=sm[i][:, 0:n], func=AF.Exp,
                                                                             bias=sti[:, 2:3], scale=1.0, accum_out=sti[:, 3:4]),
                             reads=[B_sm[i], B_stat[i]], writes=[B_P[i], B_stat[i]])
                        k.op("act", lambda e, sti=sti, h=h: e.activation(out=sti[:, 4:5], in_=sink_bc[:, h:h + 1], func=AF.Exp,
                                                                        bias=sti[:, 2:3], scale=1.0),
                             reads=[B_sink, B_stat[i]], writes=[B_stat[i]])
                        k.op("dve", lambda e, sti=sti: e.tensor_tensor(out=sti[:, 5:6], in0=sti[:, 3:4], in1=sti[:, 4:5], op=ALU.add),
                             reads=[B_stat[i]], writes=[B_stat[i]])
                        k.op("dve", lambda e, sti=sti: e.reciprocal(out=sti[:, 6:7], in_=sti[:, 5:6]),
                             reads=[B_stat[i]], writes=[B_stat[i]])
                        nb = n // 128
                        for b in range(nb):
                            k.op("pe", lambda e, i=i, b=b: e.transpose(out=pPT[:, b, :], in_=P[i][:, b * 128:(b + 1) * 128], identity=ident_b[:]),
                                 reads=[B_P[i], B_ident_b], writes=[B_pPT])
                        k.op("pool" if False else "dve", lambda e, i=i, nb=nb: e.tensor_copy(out=PT[i][:, 0:nb, :], in_=pPT[:, 0:nb, :]),
                             reads=[B_pPT], writes=[B_PT[i]])
                        for b in range(nb):
                            k.op("pe", lambda e, i=i, b=b, h=h, kvh=kvh, vb=blocks[b], nb=nb: e.matmul(
                                po[:, h * 64:(h + 1) * 64], lhsT=PT[i][:, b, :], rhs=v_all[:, vb, kvh * 64:(kvh + 1) * 64],
                                start=(b == 0), stop=(b == nb - 1)), reads=[B_PT[i], B_v], writes=[B_po])
                        k.op("dve", lambda e, h=h, sti=sti: e.tensor_scalar(out=att_bf[:, h * 64:(h + 1) * 64], in0=po[:, h * 64:(h + 1) * 64],
                                                                           scalar1=sti[:, 6:7], scalar2=None, op0=ALU.mult),
                             reads=[B_po, B_stat[i]], writes=[B_att])
                        k.op = orig_op_
                        split_ = None
                        seen_non_pe = False
                        for ix_, (a_, kw_) in enumerate(rec_):
                            if a_[0] != "pe":
                                seen_non_pe = True
                            elif seen_non_pe:
                                split_ = ix_
                                break
                        lead_ = 0
                        while rec_[lead_][0][0] == "pe":
                            lead_ += 1
                        for a_, kw_ in rec_[:lead_]:
                            k.op(*a_, **kw_)
                        if pend_B is not None:
                            for a_, kw_ in pend_B:
                                k.op(*a_, **kw_)
                        for a_, kw_ in rec_[lead_:split_]:
                            k.op(*a_, **kw_)
                        pend_B = rec_[split_:]
                    for a_, kw_ in pend_B:
                        k.op(*a_, **kw_)
                    for cb in range(4):
                        k.op("pe", lambda e, cb=cb: e.transpose(out=pcat[:, cb, :], in_=att_bf[:, cb * 128:(cb + 1) * 128], identity=ident_b[:]),
                             reads=[B_att, B_ident_b], writes=[B_pcat])
                    k.op("act", lambda e, T0=T0: e.copy(out=catT[:, 0:4, T0:T0 + 128], in_=pcat[:, 0:4, :]), reads=[B_pcat], writes=[B_catT])
                    if "dbg_att" in dbg:
                        if ti == 0:
                            dbg_att = dscr("dbg_att", [NT, 512], BF16)
                        k.dma("sp", dbg_att[T0:T0 + 128, :], att_bf[:], reads=[B_att])
                k.barrier()


            k.barrier()
            LA.close()
            for st in phase("l0ssm"):
                ssm_phase(st, catT, B_catT)
            for st in phase("l0ssmpost"):
                ssm_post(st, catT, B_catT)

            for st in phase("l0out"):
                outproj_ln1(st, 0, catT, B_catT, w_out0, NTILE)


        for st in phase("moe0"):
            moe_phase(st, 0, NTILE, x2_scr)


        layer1_mixer()
        for st in phase("moe1"):
            moe_phase(st, 1, NLT, out)

        k.barrier()
    return nc


_CONSTS = None


def _consts():
    global _CONSTS
    if _CONSTS is None:
        t = np.arange(SEQ)
        row = (t // 64).astype(np.float32)
        col = (t % 64).astype(np.float32)
        inv = (10000.0 ** (-np.arange(16, dtype=np.float32) / 16)).astype(np.float32)
        ar = row[:, None] * inv[None, :]
        ac = col[:, None] * inv[None, :]
        rope = np.concatenate([np.cos(ar), np.sin(ar), np.cos(ac), np.sin(ac)], 1).astype(np.float32)
        qi = np.arange(128)[:, None]; kj = np.arange(128)[None, :]
        mL = np.where(kj >= qi, 0.0, -30000.0).astype(np.float32)
        mR = np.where(kj <= qi, 0.0, -30000.0).astype(np.float32)
        _CONSTS = {"rope_cs": rope, "ident": np.eye(128, dtype=np.float32), "maskL": mL, "maskR": mR}
        selm = np.zeros((32, 32, 128), np.float32)
        for e_ in range(32):
            selm[e_, e_, :] = 1.0
        _CONSTS["sel"] = selm
        _CONSTS["kval"] = np.ascontiguousarray(np.broadcast_to(np.repeat(np.arange(-7, 9, dtype=np.float32), 64)[None, :], (64, 1024)))
        _CONSTS["mrow"] = np.ascontiguousarray(np.broadcast_to(np.arange(288, dtype=np.float32)[None, :], (64, 288)))
        jj = np.arange(128) // 16
        _CONSTS["maskF"] = (jj[None, :] >= jj[:, None]).astype(np.float32)
        _CONSTS["maskB"] = (jj[None, :] <= jj[:, None]).astype(np.float32)
    return _CONSTS


def make_in_maps(inputs, cores):
    f = lambda a: np.ascontiguousarray(np.asarray(a, dtype=np.float32))
    shared = {}
    for name in ("mod_w", "mod_b", "ln1_g", "ln1_b", "ln2_g", "ln2_b", "swa_sink",
                 "ssm_d", "ssm_glu_b", "dif_lam_q1", "dif_lam_k1", "dif_lam_q2", "dif_lam_k2",
                 "dif_subln_g", "moe_wg", "moe_bg", "moe_we", "moe_w1", "moe_w3", "moe_w2"):
        shared[name] = f(inputs[name])
    for name in ("swa_ssm_w_in", "swa_ssm_w_out", "ssm_a_re", "ssm_a_im", "ssm_log_step",
                 "ssm_b_re", "ssm_b_im", "ssm_c_re", "ssm_c_im", "ssm_glu_w", "dif_w_in", "dif_w_out"):
        shared[name] = f(inputs[name])[0]
    shared["moe_be"] = f(inputs["moe_be"]).reshape(2, 32)
    shared["c_ctx"] = f(inputs["c_ctx"]).reshape(1, D)
    shared.update(_consts())
    maps = []
    for b in cores:
        m = dict(shared)
        m["x"] = f(inputs["x"][b])
        m["ctx"] = f(inputs["ctx"][b])
        m["c"] = f(inputs["c"][b]).reshape(1, D)
        maps.append(m)
    return maps


def kernel(**inputs):
    nc = build()
    maps = make_in_maps(inputs, range(8))
    res = run_bass_kernel_spmd(nc, maps, core_ids=list(range(8)))
    return np.stack([r["out"] for r in res.results], 0).astype(np.float32)
```

```python
import math
from contextlib import ExitStack

import numpy as np
import concourse.bass as bass
import concourse.mybir as mybir
from concourse.bass_utils import run_bass_kernel_spmd

F32 = mybir.dt.float32
BF16 = mybir.dt.bfloat16
AF = mybir.ActivationFunctionType
ALU = mybir.AluOpType
AX = mybir.AxisListType

D = 1024
SEQ = 2048
CTX = 256
NT = SEQ + CTX
NTILE = NT // 128
NLT = SEQ // 128
ALPHA = 4 ** 0.25
LN_EPS = 1e-5


class Buf:
    __slots__ = ("w", "r")

    def __init__(self):
        self.w = None
        self.r = {}


class EngState:
    def __init__(self, name, eng, sem):
        self.name = name
        self.eng = eng
        self.sem = sem
        self.count = 0
        self.waited = {}
        self.slots = []
        self.slot_i = 0


class K:
    def __init__(self, nc, stack):
        self.nc = nc
        self.E = {}
        for name, eng in (("pe", nc.tensor), ("dve", nc.vector), ("act", nc.scalar),
                          ("pool", nc.gpsimd), ("sp", nc.sync)):
            sem = stack.enter_context(nc.semaphore("s_" + name))
            self.E[name] = EngState(name, eng, sem)
        self.semkey = {}
        for qn, n in (("sp", 12), ("pool", 12), ("act", 6)):
            for i in range(n):
                sem = stack.enter_context(nc.semaphore(f"d_{qn}{i}"))
                self.E[qn].slots.append([sem, 0])
        self.uid = 0

    def _key(self, sem):
        return id(sem)

    def _wait(self, E, deps, skip_self=False):
        best = {}
        for sem, val in deps:
            if skip_self and sem is E.sem:
                continue
            k = id(sem)
            if k not in best or best[k][1] < val:
                best[k] = (sem, val)
        for k, (sem, val) in best.items():
            if E.waited.get(k, 0) < val:
                E.eng.wait_ge(sem, val)
                E.waited[k] = val

    def _deps(self, reads, writes):
        deps = []
        for b in reads:
            if b.w is not None:
                deps.append(b.w)
        for b in writes:
            if b.w is not None:
                deps.append(b.w)
            deps.extend(b.r.values())
        return deps

    def _mark(self, tok, reads, writes):
        sem, val = tok
        for b in reads:
            b.r[id(sem)] = tok
        for b in writes:
            b.w = tok
            b.r = {}

    def op(self, en, fn, reads=(), writes=()):
        E = self.E[en]
        self._wait(E, self._deps(reads, writes), skip_self=(en == "pe"))
        ins = fn(E.eng)
        E.count += 1
        ins.then_inc(E.sem, 1)
        tok = (E.sem, E.count)
        self._mark(tok, reads, writes)
        return tok

    def dma(self, qn, out, in_, reads=(), writes=(), **kw):
        E = self.E[qn]
        self._wait(E, self._deps(reads, writes))
        slot = E.slots[E.slot_i % len(E.slots)]
        E.slot_i += 1
        if slot[1] > 0:
            self._wait(E, [(slot[0], slot[1] * 16)])
        ins = E.eng.dma_start(out=out, in_=in_, **kw)
        slot[1] += 1
        ins.then_inc(slot[0], 16)
        tok = (slot[0], slot[1] * 16)
        self._mark(tok, reads, writes)
        return tok

    def all_tokens(self):
        toks = []
        for E in self.E.values():
            if E.count:
                toks.append((E.sem, E.count))
            for sem, c in E.slots:
                if c:
                    toks.append((sem, c * 16))
        return toks

    def barrier(self):
        toks = self.all_tokens()
        for E in self.E.values():
            self._wait(E, toks, skip_self=False)


def build(dbg=(), inject=(), phases=None):
    nc = bass.Bass("TRN2", target_bir_lowering=False)
    dbg = set(dbg)
    inject = set(inject)
    ALLP = {"mod", "l0proj", "l0att", "l0ssm", "l0ssmpost", "l0out", "moe0", "l1proj", "l1att", "moe1"}
    phases = ALLP if phases is None else set(phases)

    def din(name, shape):
        return nc.dram_tensor(name, list(shape), F32, kind="ExternalInput").ap()

    def dscr(name, shape, dt=F32):
        kind = "ExternalOutput" if name in dbg else ("ExternalInput" if name in inject else "Internal")
        return nc.dram_tensor(name, list(shape), dt, kind=kind).ap()

    x_in = din("x", [SEQ, D])
    ctx_in = din("ctx", [CTX, D])
    c_in = din("c", [1, D])
    cc_in = din("c_ctx", [1, D])
    mod_w = din("mod_w", [2, D, 6 * D])
    mod_b = din("mod_b", [2, 6 * D])
    ln1_g = din("ln1_g", [2, D]); ln1_b = din("ln1_b", [2, D])
    ln2_g = din("ln2_g", [2, D]); ln2_b = din("ln2_b", [2, D])
    w_in0 = din("swa_ssm_w_in", [D, 1280])
    w_out0 = din("swa_ssm_w_out", [D, D])
    swa_sink = din("swa_sink", [1, 8])
    a_re = din("ssm_a_re", [2, 32, 64]); a_im = din("ssm_a_im", [2, 32, 64])
    log_step = din("ssm_log_step", [2, 32])
    b_re = din("ssm_b_re", [2, 32, 64, 16]); b_im = din("ssm_b_im", [2, 32, 64, 16])
    c_re = din("ssm_c_re", [2, 32, 16, 64]); c_im = din("ssm_c_im", [2, 32, 16, 64])
    ssm_d = din("ssm_d", [1, 512])
    glu_w = din("ssm_glu_w", [512, 512]); glu_b = din("ssm_glu_b", [1, 512])
    dif_w_in = din("dif_w_in", [D, 3072]); dif_w_out = din("dif_w_out", [D, D])
    lam_q1 = din("dif_lam_q1", [1, 64]); lam_k1 = din("dif_lam_k1", [1, 64])
    lam_q2 = din("dif_lam_q2", [1, 64]); lam_k2 = din("dif_lam_k2", [1, 64])
    subln_g = din("dif_subln_g", [1, 128])
    moe_wg = din("moe_wg", [2, D, 4]); moe_bg = din("moe_bg", [2, 4])
    moe_we = din("moe_we", [2, 4, D, 8]); moe_be = din("moe_be", [2, 32])
    moe_w1 = din("moe_w1", [2, 32, D, 256]); moe_w3 = din("moe_w3", [2, 32, D, 256])
    moe_w2 = din("moe_w2", [2, 32, 256, D])
    rope_cs = din("rope_cs", [SEQ, 64])
    ident_in = din("ident", [128, 128])
    sel_in = din("sel", [32, 32, 128])
    kval_in = din("kval", [64, 1024]); mrow_in = din("mrow", [64, 288])
    maskF_in = din("maskF", [128, 128]); maskB_in = din("maskB", [128, 128])
    maskL_in = din("maskL", [128, 128]); maskR_in = din("maskR", [128, 128])
    out = nc.dram_tensor("out", [SEQ, D], F32, kind="ExternalOutput").ap()

    modrow = dscr("modrow", [2, 2, 6 * D])

    with ExitStack() as gs:
        k = K(nc, gs)

        def sb(st, name, shape, dt=F32):
            k.uid += 1
            return st.enter_context(nc.sbuf_tensor(f"sb{k.uid}_{name}", list(shape), dt))

        def ps(st, name, shape, dt=F32):
            k.uid += 1
            return st.enter_context(nc.psum_tensor(f"ps{k.uid}_{name}", list(shape), dt))

        def phase(name):
            if name in phases:
                with ExitStack() as st_:
                    yield st_

        def record(fn):
            rec = []
            oo, od = k.op, k.dma
            k.op = lambda *a, **kw: rec.append(("op", a, kw))
            k.dma = lambda *a, **kw: rec.append(("dma", a, kw))
            try:
                fn()
            finally:
                k.op, k.dma = oo, od
            return rec

        def replay(recs):
            for ix in range(max(len(r_) for r_ in recs)):
                for r_ in recs:
                    if ix < len(r_):
                        kind, a_, kw_ = r_[ix]
                        (k.op if kind == "op" else k.dma)(*a_, **kw_)

        ident_f = sb(gs, "ident_f", [128, 128]); B_ident_f = Buf()
        ident_b = sb(gs, "ident_b", [128, 128], BF16); B_ident_b = Buf()
        k.dma("sp", ident_f[:], ident_in[:, :], writes=[B_ident_f])
        k.op("dve", lambda e: e.tensor_copy(out=ident_b[:], in_=ident_f[:]),
             reads=[B_ident_f], writes=[B_ident_b])

        for st in phase("mod"):
            cT = sb(st, "cT", [128, 8, 2]); B_cT = Buf()
            with nc.allow_non_contiguous_dma(reason="tiny column loads"):
                k.dma("sp", cT[:, :, 0], c_in[0, :].rearrange("(k p) -> p k", p=128), writes=[B_cT])
                k.dma("sp", cT[:, :, 1], cc_in[0, :].rearrange("(k p) -> p k", p=128), writes=[B_cT])
            sT = sb(st, "sT", [128, 8, 2]); B_sT = Buf()
            k.op("act", lambda e: e.activation(out=sT[:], in_=cT[:], func=AF.Silu),
                 reads=[B_cT], writes=[B_sT])
            wt = [sb(st, f"modw{i}", [128, 8, 512]) for i in range(2)]
            B_wt = [Buf(), Buf()]
            mb = sb(st, "modb", [2, 6 * D]); B_mb = Buf()
            mrow = sb(st, "mrow", [2, 6 * D]); B_mrow = Buf()
            pm = [ps(st, f"pmod{i}", [2, 512]) for i in range(2)]
            B_pm = [Buf(), Buf()]
            it = 0
            for l in range(2):
                k.dma("sp", mb[0:1, :], mod_b[l:l + 1, :], writes=[B_mb])
                k.dma("sp", mb[1:2, :], mod_b[l:l + 1, :], writes=[B_mb])
                for cb in range(12):
                    i = it % 2
                    it += 1
                    k.dma("sp" if cb % 2 == 0 else "act", wt[i][:],
                          mod_w[l, :, cb * 512:(cb + 1) * 512].rearrange("(k p) n -> p k n", p=128),
                          writes=[B_wt[i]])
                    for kc in range(8):
                        k.op("pe", lambda e, kc=kc, i=i: e.matmul(
                            pm[i][:], lhsT=sT[:, kc, :], rhs=wt[i][:, kc, :],
                            start=(kc == 0), stop=(kc == 7)),
                            reads=[B_sT, B_wt[i]], writes=[B_pm[i]])
                    k.op("dve", lambda e, i=i, cb=cb: e.tensor_tensor(
                        out=mrow[:, cb * 512:(cb + 1) * 512], in0=pm[i][:],
                        in1=mb[:, cb * 512:(cb + 1) * 512], op=ALU.add),
                        reads=[B_pm[i], B_mb], writes=[B_mrow])
                k.dma("sp", modrow[l], mrow[:], reads=[B_mrow], writes=[])
            k.barrier()


        def load_bc(st, name, src_row_ap, n, q="sp"):
            t = sb(st, name, [128, n]); B = Buf()
            k.dma(q, t[:], src_row_ap.partition_broadcast(128), writes=[B])
            return t, B

        def mod_bc(st, name, l, r, chunk, plus1=False):
            t, B = load_bc(st, name, modrow[l, r:r + 1, chunk * D:(chunk + 1) * D], D)
            if plus1:
                k.op("pool", lambda e: e.tensor_scalar(out=t[:], in0=t[:], scalar1=1.0, scalar2=None,
                                                       op0=ALU.add), reads=[B], writes=[B])
            return t, B

        u_scr = dscr("u_scr", [NT, 512])
        y_scr = dscr("y_scr", [NT, 512], BF16)
        x1_scr = dscr("x1_scr", [NT, D])
        x2_scr = dscr("x2_scr", [NT, D])
        dbg_q = dscr("dbg_q", [NT, 1280])

        def src_rows(ti, l):
            if l == 0:
                return x_in[ti * 128:(ti + 1) * 128, :] if ti < NLT else ctx_in[(ti - NLT) * 128:(ti - NLT + 1) * 128, :]
            return x2_scr[ti * 128:(ti + 1) * 128, :]

        def rope_apply(st_bufs, src_ps, B_src, dst, B_dst, nh, rt, B_rt, tmp1, tmp2, B_tmp):
            S = src_ps.rearrange("p (h a b f) -> p h a b f", h=nh, a=2, b=2, f=16)
            O = dst.rearrange("p (h a b f) -> p h a b f", h=nh, a=2, b=2, f=16)
            T1 = tmp1.rearrange("p (h a b f) -> p h a b f", h=nh, a=2, b=2, f=16)
            T2 = tmp2.rearrange("p (h a b f) -> p h a b f", h=nh, a=2, b=2, f=16)
            for a in range(2):
                cos = rt[:, a * 32:a * 32 + 16].rearrange("p (x y f) -> p x y f", x=1, y=1).to_broadcast([128, nh, 2, 16])
                sin = rt[:, a * 32 + 16:a * 32 + 32].rearrange("p (x y f) -> p x y f", x=1, y=1).to_broadcast([128, nh, 2, 16])
                k.op("dve", lambda e, a=a, cos=cos: e.tensor_tensor(out=T1[:, :, a], in0=S[:, :, a], in1=cos, op=ALU.mult),
                     reads=[B_src, B_rt], writes=[B_tmp])
                k.op("dve", lambda e, a=a, sin=sin: e.tensor_tensor(out=T2[:, :, a], in0=S[:, :, a, ::-1, :], in1=sin, op=ALU.mult),
                     reads=[B_src, B_rt], writes=[B_tmp])
                k.op("dve", lambda e, a=a: e.tensor_tensor(out=O[:, :, a, 0, :], in0=T1[:, :, a, 0, :], in1=T2[:, :, a, 0, :], op=ALU.subtract),
                     reads=[B_tmp], writes=[B_dst])
                k.op("dve", lambda e, a=a: e.tensor_tensor(out=O[:, :, a, 1, :], in0=T1[:, :, a, 1, :], in1=T2[:, :, a, 1, :], op=ALU.add),
                     reads=[B_tmp], writes=[B_dst])


        def ln_epilogue(wk, y_parts, B_y, xo, B_xo, g_t, lng_t, lnb_t, dst_rows):
            tmp, B_tmp, z, B_z, stt, B_stt, o, B_o = wk
            for hf in range(2):
                sl = slice(hf * 512, (hf + 1) * 512)
                k.op("dve", lambda e, hf=hf, sl=sl: e.tensor_tensor(out=tmp[:, sl], in0=y_parts[hf], in1=g_t[0][:, sl], op=ALU.mult),
                     reads=[B_y[hf], g_t[1]], writes=[B_tmp])
            k.op("dve", lambda e: e.scalar_tensor_tensor(out=z[:], in0=xo[:], scalar=ALPHA, in1=tmp[:], op0=ALU.mult, op1=ALU.add),
                 reads=[B_xo, B_tmp], writes=[B_z])
            for hf in range(2):
                k.op("dve", lambda e, hf=hf: e.bn_stats(out=stt[:, hf * 6:(hf + 1) * 6], in_=z[:, hf * 512:(hf + 1) * 512]),
                     reads=[B_z], writes=[B_stt])
            k.op("dve", lambda e: e.bn_aggr(out=stt[:, 12:14], in_=stt[:, 0:12]), reads=[B_stt], writes=[B_stt])
            k.op("dve", lambda e: e.tensor_scalar(out=stt[:, 15:16], in0=stt[:, 13:14], scalar1=LN_EPS, scalar2=None, op0=ALU.add),
                 reads=[B_stt], writes=[B_stt])
            k.op("act", lambda e: e.sqrt(out=stt[:, 15:16], in_=stt[:, 15:16]), reads=[B_stt], writes=[B_stt])
            k.op("dve", lambda e: e.reciprocal(out=stt[:, 14:15], in_=stt[:, 15:16]), reads=[B_stt], writes=[B_stt])
            k.op("dve", lambda e: e.tensor_scalar(out=tmp[:], in0=z[:], scalar1=stt[:, 12:13], scalar2=stt[:, 14:15], op0=ALU.subtract, op1=ALU.mult),
                 reads=[B_z, B_stt], writes=[B_tmp])
            k.op("pool", lambda e: e.tensor_tensor(out=o[:], in0=tmp[:], in1=lng_t[0][:], op=ALU.mult),
                 reads=[B_tmp, lng_t[1]], writes=[B_o])
            k.op("pool", lambda e: e.tensor_tensor(out=o[:], in0=o[:], in1=lnb_t[0][:], op=ALU.add),
                 reads=[B_o, lnb_t[1]], writes=[B_o])
            k.dma("sp", dst_rows, o[:], reads=[B_o])

        def ln_work(st, pfx):
            tmp = sb(st, pfx + "_tmp", [128, D]); z = sb(st, pfx + "_z", [128, D])
            stt = sb(st, pfx + "_stt", [128, 16]); o = sb(st, pfx + "_o", [128, D])
            return (tmp, Buf(), z, Buf(), stt, Buf(), o, Buf())

        def outproj_ln1(st, l, catT_, B_catT_, w_out_dram, ntiles):
            w_bf = sb(st, "w_out_bf", [128, 8, D], BF16); B_w = Buf()
            for kc in range(8):
                k.dma("pool", w_bf[:, kc, :], w_out_dram[kc * 128:(kc + 1) * 128, :], writes=[B_w])
            g1 = [mod_bc(st, f"g1_{r}", l, r, 2) for r in range(2)]
            lng = load_bc(st, "ln1g", ln1_g[l:l + 1, :], D)
            lnb = load_bc(st, "ln1b", ln1_b[l:l + 1, :], D)
            wks = [ln_work(st, "e1a"), ln_work(st, "e1b")]
            xo = [sb(st, f"xo{i}", [128, D]) for i in range(2)]; B_xo = [Buf(), Buf()]
            py = [ps(st, f"py{i}", [128, 512]) for i in range(4)]; B_py = [Buf() for _ in range(4)]

            def out_tile(ti):
                i = ti % 2
                wk = wks[i]
                r = 0 if ti < NLT else 1
                T0 = ti * 128
                k.dma("sp", xo[i][:], src_rows(ti, l), writes=[B_xo[i]])
                for hf in range(2):
                    pi = i * 2 + hf
                    for kc in range(8):
                        k.op("pe", lambda e, kc=kc, hf=hf, pi=pi, T0=T0: e.matmul(
                            py[pi][:], lhsT=catT_[:, kc, T0:T0 + 128], rhs=w_bf[:, kc, hf * 512:(hf + 1) * 512],
                            start=(kc == 0), stop=(kc == 7)), reads=[B_catT_, B_w], writes=[B_py[pi]])
                ln_epilogue(wk, [py[i * 2][:], py[i * 2 + 1][:]], [B_py[i * 2], B_py[i * 2 + 1]], xo[i], B_xo[i],
                            g1[r], lng, lnb, x1_scr[T0:T0 + 128, :])

            for t2 in range(0, ntiles, 2):
                replay([record(lambda ti=ti: out_tile(ti)) for ti in range(t2, min(t2 + 2, ntiles))])
            k.barrier()

        def moe_phase(st, l, ntiles, dst):
            ntok = ntiles * 128
            h2T = sb(st, "h2T", [128, 8, ntok], BF16); B_h2T = Buf()
            gateT = sb(st, "gateT", [32, ntok], BF16); B_gateT = Buf()
            f_acc = sb(st, "f_acc", [128, ntiles, D]); B_facc = [Buf() for _ in range(ntiles)]
            sel = sb(st, "sel", [32, 32, 128], BF16); B_sel = Buf()
            k.dma("pool", sel[:], sel_in[:, :, :], writes=[B_sel])
            with ExitStack() as s1:
                Wr = sb(s1, "Wr", [128, 8, 36]); B_Wr = Buf()
                with nc.allow_non_contiguous_dma(reason="small router weights"):
                    k.dma("sp", Wr[:, :, 0:4], moe_wg[l].rearrange("(k p) n -> p k n", p=128), writes=[B_Wr])
                    for g in range(4):
                        k.dma("sp", Wr[:, :, 4 + g * 8:12 + g * 8], moe_we[l, g].rearrange("(k p) n -> p k n", p=128), writes=[B_Wr])
                Whi = sb(s1, "Whi", [128, 8, 36], BF16); Wlo = sb(s1, "Wlo", [128, 8, 36], BF16); B_Wsp = Buf()
                k.op("dve", lambda e: e.tensor_copy(out=Whi[:], in_=Wr[:]), reads=[B_Wr], writes=[B_Wsp])
                k.op("dve", lambda e: e.tensor_tensor(out=Wlo[:], in0=Wr[:], in1=Whi[:], op=ALU.subtract), reads=[B_Wr, B_Wsp], writes=[B_Wsp])
                rb = sb(s1, "rb", [128, 36]); B_rb = Buf()
                k.dma("sp", rb[:, 0:4], moe_bg[l:l + 1, :].partition_broadcast(128), writes=[B_rb])
                k.dma("sp", rb[:, 4:36], moe_be[l:l + 1, :].partition_broadcast(128), writes=[B_rb])
                sc2p = [mod_bc(s1, f"sc2p_{r}", l, r, 4, plus1=True) for r in range(2)]
                sh2 = [mod_bc(s1, f"sh2_{r}", l, r, 3) for r in range(2)]
                xt = [sb(s1, f"mx{i}", [128, D]) for i in range(2)]; B_xt = [Buf(), Buf()]
                hf32 = [sb(s1, f"mh{c_}", [128, D]) for c_ in range(2)]; B_h = [Buf(), Buf()]
                hhi = [sb(s1, f"hhi{c_}", [128, D], BF16) for c_ in range(2)]; hlo = [sb(s1, f"hlo{c_}", [128, D], BF16) for c_ in range(2)]; B_hs = [Buf(), Buf()]
                hloT = [sb(s1, f"hloT{c_}", [128, 8, 128], BF16) for c_ in range(2)]; B_hloT = [Buf(), Buf()]
                pTh = [ps(s1, f"mpTh{c_}", [128, 8, 128], BF16) for c_ in range(2)]; B_pTh = [Buf(), Buf()]
                pTl = [ps(s1, f"mpTl{c_}", [128, 8, 128], BF16) for c_ in range(2)]; B_pTl = [Buf(), Buf()]
                pr = [ps(s1, f"mpr{c_}", [128, 512]) for c_ in range(2)]; B_pr = [Buf(), Buf()]
                pg = [ps(s1, f"mpg{c_}", [32, 1024], BF16) for c_ in range(2)]; B_pg = [Buf(), Buf()]
                lg = [sb(s1, f"lg{c_}", [128, 36]) for c_ in range(2)]; B_lg = [Buf(), Buf()]
                sm = [sb(s1, f"rsm{c_}", [128, 160]) for c_ in range(2)]; B_sm = [Buf(), Buf()]
                gates = [sb(s1, f"gates{c_}", [128, 32]) for c_ in range(2)]; B_gates = [Buf(), Buf()]
                gates_bf = [sb(s1, f"gates_bf{c_}", [128, 32], BF16) for c_ in range(2)]; B_gbf = [Buf(), Buf()]
                def router_tile(ti):
                    i = ti % 2
                    ci = ti % 2
                    r = 0 if ti < NLT else 1
                    T0 = ti * 128
                    k.dma("sp", xt[i][:], x1_scr[T0:T0 + 128, :], writes=[B_xt[i]])
                    k.op("dve", lambda e, i=i, r=r: e.tensor_tensor(out=hf32[ci][:], in0=xt[i][:], in1=sc2p[r][0][:], op=ALU.mult),
                         reads=[B_xt[i], sc2p[r][1]], writes=[B_h[ci]])
                    k.op("pool", lambda e, r=r: e.tensor_tensor(out=hf32[ci][:], in0=hf32[ci][:], in1=sh2[r][0][:], op=ALU.add),
                         reads=[B_h[ci], sh2[r][1]], writes=[B_h[ci]])
                    k.op("pool", lambda e: e.tensor_copy(out=hhi[ci][:], in_=hf32[ci][:]), reads=[B_h[ci]], writes=[B_hs[ci]])
                    k.op("dve", lambda e: e.tensor_tensor(out=hlo[ci][:], in0=hf32[ci][:], in1=hhi[ci][:], op=ALU.subtract), reads=[B_h[ci], B_hs[ci]], writes=[B_hs[ci]])
                    for kc in range(8):
                        k.op("pe", lambda e, kc=kc: e.transpose(out=pTh[ci][:, kc, :], in_=hhi[ci][:, kc * 128:(kc + 1) * 128], identity=ident_b[:]),
                             reads=[B_hs[ci], B_ident_b], writes=[B_pTh[ci]])
                    for kc in range(8):
                        k.op("pe", lambda e, kc=kc: e.transpose(out=pTl[ci][:, kc, :], in_=hlo[ci][:, kc * 128:(kc + 1) * 128], identity=ident_b[:]),
                             reads=[B_hs[ci], B_ident_b], writes=[B_pTl[ci]])
                    k.op("act", lambda e, T0=T0: e.copy(out=h2T[:, :, T0:T0 + 128], in_=pTh[ci][:]), reads=[B_pTh[ci]], writes=[B_h2T])
                    k.op("dve", lambda e: e.tensor_copy(out=hloT[ci][:], in_=pTl[ci][:]), reads=[B_pTl[ci]], writes=[B_hloT[ci]])
                    n_mm = 24
                    j = 0
                    for (A, BA, W) in ((None, B_h2T, Whi), (hloT[ci], B_hloT[ci], Whi), (None, B_h2T, Wlo)):
                        for kc in range(8):
                            lhs = h2T[:, kc, T0:T0 + 128] if A is None else A[:, kc, :]
                            k.op("pe", lambda e, lhs=lhs, W=W, kc=kc, j=j: e.matmul(pr[ci][:, 0:36], lhsT=lhs, rhs=W[:, kc, :], start=(j == 0), stop=(j == 23)),
                                 reads=[BA, B_Wsp], writes=[B_pr[ci]])
                            j += 1
                    R = [B_lg[ci], B_sm[ci]]
                    def dv(fn, reads=R, writes=(B_sm[ci],)):
                        k.op("dve", fn, reads=list(reads), writes=list(writes))
                    k.op("dve", lambda e: e.tensor_tensor(out=lg[ci][:], in0=pr[ci][:, 0:36], in1=rb[:], op=ALU.add), reads=[B_pr[ci], B_rb], writes=[B_lg[ci]])
                    dv(lambda e: e.reduce_max(out=sm[ci][:, 0:1], in_=lg[ci][:, 0:4], axis=AX.X))
                    dv(lambda e: e.tensor_scalar(out=sm[ci][:, 1:2], in0=sm[ci][:, 0:1], scalar1=-1.0, scalar2=None, op0=ALU.mult))
                    k.op("act", lambda e: e.activation(out=sm[ci][:, 56:60], in_=lg[ci][:, 0:4], func=AF.Exp, bias=sm[ci][:, 1:2], scale=1.0, accum_out=sm[ci][:, 2:3]),
                         reads=R, writes=[B_sm[ci]])
                    dv(lambda e: e.reciprocal(out=sm[ci][:, 3:4], in_=sm[ci][:, 2:3]))
                    dv(lambda e: e.tensor_scalar(out=sm[ci][:, 4:8], in0=lg[ci][:, 0:4], scalar1=sm[ci][:, 0:1], scalar2=None, op0=ALU.is_equal))
                    le = lg[ci][:, 4:36].rearrange("p (g e) -> p g e", g=4)
                    tmp48 = sm[ci][:, 64:96].rearrange("p (g e) -> p g e", g=4)
                    ohb = sm[ci][:, 4:8].rearrange("p (g x) -> p g x", x=1).to_broadcast([128, 4, 8])
                    dv(lambda e: e.tensor_tensor(out=tmp48, in0=le, in1=ohb, op=ALU.mult))
                    dv(lambda e: e.tensor_reduce(out=sm[ci][:, 8:16], in_=sm[ci][:, 64:96].rearrange("p (g e) -> p e g", g=4), axis=AX.X, op=ALU.add))
                    dv(lambda e: e.reduce_max(out=sm[ci][:, 16:17], in_=sm[ci][:, 8:16], axis=AX.X))
                    dv(lambda e: e.tensor_scalar(out=sm[ci][:, 24:32], in0=sm[ci][:, 8:16], scalar1=sm[ci][:, 16:17], scalar2=None, op0=ALU.is_equal))
                    dv(lambda e: e.scalar_tensor_tensor(out=sm[ci][:, 32:40], in0=sm[ci][:, 24:32], scalar=-1e30, in1=sm[ci][:, 8:16], op0=ALU.mult, op1=ALU.add))
                    dv(lambda e: e.reduce_max(out=sm[ci][:, 17:18], in_=sm[ci][:, 32:40], axis=AX.X))
                    dv(lambda e: e.tensor_scalar(out=sm[ci][:, 40:48], in0=sm[ci][:, 32:40], scalar1=sm[ci][:, 17:18], scalar2=None, op0=ALU.is_equal))
                    dv(lambda e: e.tensor_tensor(out=sm[ci][:, 18:19], in0=sm[ci][:, 17:18], in1=sm[ci][:, 16:17], op=ALU.subtract))
                    k.op("act", lambda e: e.activation(out=sm[ci][:, 19:20], in_=sm[ci][:, 18:19], func=AF.Exp), reads=R, writes=[B_sm[ci]])
                    dv(lambda e: e.tensor_scalar(out=sm[ci][:, 20:21], in0=sm[ci][:, 19:20], scalar1=1.0, scalar2=None, op0=ALU.add))
                    dv(lambda e: e.reciprocal(out=sm[ci][:, 20:21], in_=sm[ci][:, 20:21]))
                    dv(lambda e: e.tensor_tensor(out=sm[ci][:, 21:22], in0=sm[ci][:, 19:20], in1=sm[ci][:, 20:21], op=ALU.mult))
                    dv(lambda e: e.tensor_tensor(out=sm[ci][:, 22:23], in0=sm[ci][:, 20:21], in1=sm[ci][:, 3:4], op=ALU.mult))
                    dv(lambda e: e.tensor_tensor(out=sm[ci][:, 23:24], in0=sm[ci][:, 21:22], in1=sm[ci][:, 3:4], op=ALU.mult))
                    dv(lambda e: e.tensor_scalar(out=sm[ci][:, 48:56], in0=sm[ci][:, 24:32], scalar1=sm[ci][:, 22:23], scalar2=None, op0=ALU.mult))
                    dv(lambda e: e.scalar_tensor_tensor(out=sm[ci][:, 48:56], in0=sm[ci][:, 40:48], scalar=sm[ci][:, 23:24], in1=sm[ci][:, 48:56], op0=ALU.mult, op1=ALU.add))
                    geb = sm[ci][:, 48:56].rearrange("p (x e) -> p x e", x=1).to_broadcast([128, 4, 8])
                    k.op("dve", lambda e: e.tensor_tensor(out=gates[ci][:].rearrange("p (g e) -> p g e", g=4), in0=ohb, in1=geb, op=ALU.mult),
                         reads=R, writes=[B_gates[ci]])
                    k.op("dve", lambda e: e.tensor_copy(out=gates_bf[ci][:], in_=gates[ci][:]), reads=[B_gates[ci]], writes=[B_gbf[ci]])
                    k.op("pe", lambda e: e.transpose(out=pg[ci][:, 0:128], in_=gates_bf[ci][:], identity=ident_b[:]),
                         reads=[B_gbf[ci], B_ident_b], writes=[B_pg[ci]])
                    k.op("act", lambda e, T0=T0: e.copy(out=gateT[:, T0:T0 + 128], in_=pg[ci][:, 0:128]), reads=[B_pg[ci]], writes=[B_gateT])
                    if "dbg_gates" in dbg:
                        if ti == 0:
                            dbg_gates = dscr("dbg_gates", [NT, 32])
                        k.dma("sp", dbg_gates[T0:T0 + 128, :], gates[ci][:], reads=[B_gates[ci]])

                recs_pair = []
                for ti in range(ntiles):
                    rec_r = []
                    orig_op_r = k.op
                    k.op = lambda *a, rec_r=rec_r, **kw: rec_r.append((a, kw))
                    router_tile(ti)
                    k.op = orig_op_r
                    recs_pair.append(rec_r)
                    if len(recs_pair) == 2 or ti == ntiles - 1:
                        for ix_ in range(max(len(r_) for r_ in recs_pair)):
                            for r_ in recs_pair:
                                if ix_ < len(r_):
                                    a_, kw_ = r_[ix_]
                                    k.op(*a_, **kw_)
                        recs_pair = []
                k.barrier()
            if "stop_router" in dbg:
                return
            with ExitStack() as s2:
                w13 = [sb(s2, f"w13_{i}", [128, 8, 512], BF16) for i in range(2)]; B_w13 = [Buf(), Buf()]
                w2 = [sb(s2, f"w2_{i}", [128, 2, D], BF16) for i in range(2)]; B_w2 = [Buf(), Buf()]
                hh = [sb(s2, f"hh{i}", [128, 2, ntok], BF16) for i in range(2)]; B_hh = [Buf(), Buf()]
                stg13 = sb(s2, "stg13", [128, 8, 512]); B_stg13 = Buf()
                stg2 = sb(s2, "stg2", [128, 2, D]); B_stg2 = Buf()
                s1t = [sb(s2, f"s1t{i}", [128, 512]) for i in range(2)]; B_s1t = [Buf(), Buf()]
                t3 = [sb(s2, f"t3{i}", [128, 512]) for i in range(2)]; B_t3 = [Buf(), Buf()]
                ph1 = [ps(s2, f"ph1_{i}", [128, 512]) for i in range(2)]; B_ph1 = [Buf(), Buf()]
                ph3 = [ps(s2, f"ph3_{i}", [128, 512]) for i in range(2)]; B_ph3 = [Buf(), Buf()]
                pgbs = [ps(s2, f"pgb{i}", [128, 512]) for i in range(2)]; B_pgbs = [Buf(), Buf()]
                ibk = 0
                pf = [ps(s2, f"pf{i}", [128, 512]) for i in range(2)]; B_pf = [Buf(), Buf()]
                blocks = [(b0, min(512, ntok - b0)) for b0 in range(0, ntok, 512)]
                it = 0; itf = 0

                def w_stage13(ee):
                    k.dma("sp", stg13[:, :, 0:256], moe_w1[l, ee].rearrange("(k p) f -> p k f", p=128), writes=[B_stg13])
                    k.dma("sp", stg13[:, :, 256:512], moe_w3[l, ee].rearrange("(k p) f -> p k f", p=128), writes=[B_stg13])

                def w_stage2(ee):
                    k.dma("sp", stg2[:], moe_w2[l, ee].rearrange("(c p) d -> p c d", p=128), writes=[B_stg2])

                def w_cast13(ee):
                    wj = ee % 2
                    k.op("pool", lambda e: e.tensor_copy(out=w13[wj][:], in_=stg13[:]), reads=[B_stg13], writes=[B_w13[wj]])

                def w_cast2(ee):
                    wj = ee % 2
                    k.op("pool", lambda e: e.tensor_copy(out=w2[wj][:], in_=stg2[:]), reads=[B_stg2], writes=[B_w2[wj]])

                def w_tail(e_):
                    if e_ + 1 < 32:
                        w_cast2(e_ + 1)
                    if e_ + 2 < 32:
                        w_stage2(e_ + 2)
                for e_ in range(32):
                    wi = e_ % 2
                    if e_ == 0:
                        w_stage13(0); w_stage2(0); w_cast13(0); w_cast2(0); w_stage13(1); w_stage2(1)
                    if e_ + 1 < 32:
                        w_cast13(e_ + 1)
                    if e_ + 2 < 32:
                        w_stage13(e_ + 2)
                    for (b0, bn) in blocks:
                        pgb = pgbs[ibk % 2]; B_pgb = B_pgbs[ibk % 2]; ibk += 1
                        k.op("pe", lambda e, e_=e_, b0=b0, bn=bn, pgb=pgb: e.matmul(pgb[:, 0:bn], lhsT=sel[:, e_, :], rhs=gateT[:, b0:b0 + bn], start=True, stop=True),
                             reads=[B_sel, B_gateT], writes=[B_pgb])
                        for fc in range(2):
                            i = it % 2; it += 1
                            for kc in range(8):
                                k.op("pe", lambda e, kc=kc, fc=fc, i=i, wi=wi, b0=b0, bn=bn: e.matmul(
                                    ph1[i][:, 0:bn], lhsT=w13[wi][:, kc, fc * 128:(fc + 1) * 128], rhs=h2T[:, kc, b0:b0 + bn],
                                    start=(kc == 0), stop=(kc == 7)), reads=[B_w13[wi], B_h2T], writes=[B_ph1[i]])
                            for kc in range(8):
                                k.op("pe", lambda e, kc=kc, fc=fc, i=i, wi=wi, b0=b0, bn=bn: e.matmul(
                                    ph3[i][:, 0:bn], lhsT=w13[wi][:, kc, 256 + fc * 128:256 + (fc + 1) * 128], rhs=h2T[:, kc, b0:b0 + bn],
                                    start=(kc == 0), stop=(kc == 7)), reads=[B_w13[wi], B_h2T], writes=[B_ph3[i]])
                            k.op("act", lambda e, i=i, bn=bn: e.activation(out=s1t[i][:, 0:bn], in_=ph1[i][:, 0:bn], func=AF.Silu),
                                 reads=[B_ph1[i]], writes=[B_s1t[i]])
                            k.op("dve", lambda e, i=i, bn=bn: e.tensor_tensor(out=t3[i][:, 0:bn], in0=s1t[i][:, 0:bn], in1=ph3[i][:, 0:bn], op=ALU.mult),
                                 reads=[B_s1t[i], B_ph3[i]], writes=[B_t3[i]])
                            k.op("dve", lambda e, i=i, bn=bn, fc=fc, wi=wi, b0=b0, pgb=pgb: e.tensor_tensor(out=hh[wi][:, fc, b0:b0 + bn], in0=t3[i][:, 0:bn], in1=pgb[:, 0:bn], op=ALU.mult),
                                 reads=[B_t3[i], B_pgb], writes=[B_hh[wi]])
                    if e_ % 2 == 0:
                        w_tail(e_)
                        continue
                    for tt in range(ntiles):
                        for dc in range(2):
                            j = itf % 2; itf += 1
                            for q_ in range(4):
                                wq = q_ // 2; fc = q_ % 2
                                k.op("pe", lambda e, fc=fc, j=j, wq=wq, tt=tt, dc=dc, q_=q_: e.matmul(
                                    pf[j][:], lhsT=hh[wq][:, fc, tt * 128:(tt + 1) * 128], rhs=w2[wq][:, fc, dc * 512:(dc + 1) * 512],
                                    start=(q_ == 0), stop=(q_ == 3)), reads=[B_hh[wq], B_w2[wq]], writes=[B_pf[j]])
                            if e_ == 1:
                                k.op("dve", lambda e, j=j, tt=tt, dc=dc: e.tensor_copy(out=f_acc[:, tt, dc * 512:(dc + 1) * 512], in_=pf[j][:]),
                                     reads=[B_pf[j]], writes=[B_facc[tt]])
                            else:
                                k.op("dve", lambda e, j=j, tt=tt, dc=dc: e.tensor_tensor(out=f_acc[:, tt, dc * 512:(dc + 1) * 512],
                                     in0=f_acc[:, tt, dc * 512:(dc + 1) * 512], in1=pf[j][:], op=ALU.add),
                                     reads=[B_pf[j], B_facc[tt]], writes=[B_facc[tt]])
                    w_tail(e_)
                k.barrier()
            if "stop_experts" in dbg:
                return
            with ExitStack() as s3:
                g2 = [mod_bc(s3, f"g2_{r}", l, r, 5) for r in range(2)]
                lng = load_bc(s3, "ln2g", ln2_g[l:l + 1, :], D)
                lnb = load_bc(s3, "ln2b", ln2_b[l:l + 1, :], D)
                wks = [ln_work(s3, "e2a"), ln_work(s3, "e2b")]
                xo = [sb(s3, f"x1o{i}", [128, D]) for i in range(2)]; B_xo = [Buf(), Buf()]

                def ln2_tile(ti):
                    i = ti % 2
                    r = 0 if ti < NLT else 1
                    T0 = ti * 128
                    k.dma("sp", xo[i][:], x1_scr[T0:T0 + 128, :], writes=[B_xo[i]])
                    ln_epilogue(wks[i], [f_acc[:, ti, 0:512], f_acc[:, ti, 512:1024]], [B_facc[ti], B_facc[ti]], xo[i], B_xo[i],
                                g2[r], lng, lnb, dst[T0:T0 + 128, :])

                for t2 in range(0, ntiles, 2):
                    replay([record(lambda ti=ti: ln2_tile(ti)) for ti in range(t2, min(t2 + 2, ntiles))])
                k.barrier()


        def ssm_phase(st, catT_, B_catT_):
            PI = math.pi
            TWO_PI = 2.0 * math.pi
            def bc3(ap2, n):
                P_, G_ = ap2.shape
                return ap2.rearrange("p (g x) -> p g x", x=1).to_broadcast([P_, G_, n])
            I32 = mybir.dt.int32
            INV2PI = 1.0 / TWO_PI

            def sincos(ang_ap, shape, s_out, c_out, tmps, B_in, B_out, B_tmp):
                y, yi, yf = tmps
                dvt = lambda fn: k.op("dve", fn, reads=[B_in, B_tmp, B_out], writes=[B_tmp])
                dvt(lambda e: e.tensor_scalar(out=y, in0=ang_ap, scalar1=INV2PI, scalar2=32.5, op0=ALU.mult, op1=ALU.add))
                dvt(lambda e: e.tensor_copy(out=yi, in_=y))
                dvt(lambda e: e.tensor_copy(out=yf, in_=yi))
                dvt(lambda e: e.tensor_tensor(out=y, in0=y, in1=yf, op=ALU.subtract))
                dvt(lambda e: e.scalar_tensor_tensor(out=yf, in0=y, scalar=0.0, in1=y, op0=ALU.is_lt, op1=ALU.add))
                k.op("act", lambda e: e.activation(out=s_out, in_=yf, func=AF.Sin, bias=negpi[0:shape[0], :], scale=TWO_PI),
                     reads=[B_tmp, B_np], writes=[B_out])
                dvt(lambda e: e.tensor_scalar(out=y, in0=yf, scalar1=0.25, scalar2=None, op0=ALU.add))
                dvt(lambda e: e.scalar_tensor_tensor(out=yf, in0=y, scalar=1.0, in1=y, op0=ALU.is_ge, op1=ALU.subtract))
                k.op("act", lambda e: e.activation(out=c_out, in_=yf, func=AF.Sin, bias=negpi[0:shape[0], :], scale=-TWO_PI),
                     reads=[B_tmp, B_np], writes=[B_out])

            ar = sb(st, "ar", [64, 64]); ai = sb(st, "ai", [64, 64]); ls = sb(st, "ls", [64, 64]); B_par = Buf()
            with nc.allow_non_contiguous_dma(reason="ssm params"):
                k.dma("sp", ar[:], a_re.rearrange("d g p -> p (d g)"), writes=[B_par])
                k.dma("sp", ai[:], a_im.rearrange("d g p -> p (d g)"), writes=[B_par])
            k.dma("sp", ls[:], log_step.rearrange("(x d) g -> x (d g)", x=1).partition_broadcast(64), writes=[B_par])
            negpi = sb(st, "negpi", [128, 1]); B_np = Buf()
            k.op("dve", lambda e: e.memset(negpi[:], -PI), writes=[B_np])
            kv = sb(st, "kv", [64, 16, 64]); B_kv = Buf()
            k.dma("sp", kv[:], kval_in.rearrange("p (k g) -> p k g", k=16), writes=[B_kv])
            mrow = sb(st, "mrow", [64, 288]); B_mrow = Buf()
            k.dma("sp", mrow[:], mrow_in[:, :], writes=[B_mrow])
            maskF = sb(st, "maskF", [128, 128]); maskB = sb(st, "maskB", [128, 128]); B_mk = Buf()
            k.dma("sp", maskF[:], maskF_in[:, :], writes=[B_mk])
            k.dma("sp", maskB[:], maskB_in[:, :], writes=[B_mk])
            dar = sb(st, "dar", [64, 64]); dai = sb(st, "dai", [64, 64]); B_d = Buf()
            k.op("act", lambda e: e.activation(out=ls[:], in_=ls[:], func=AF.Exp), reads=[B_par], writes=[B_par])
            k.op("dve", lambda e: e.tensor_tensor(out=dar[:], in0=ls[:], in1=ar[:], op=ALU.mult), reads=[B_par], writes=[B_d])
            k.op("dve", lambda e: e.tensor_tensor(out=dai[:], in0=ls[:], in1=ai[:], op=ALU.mult), reads=[B_par, B_d], writes=[B_d])
            LR = sb(st, "LR", [64, 16, 64]); LI = sb(st, "LI", [64, 16, 64]); MG = sb(st, "MG", [64, 16, 64]); B_L = Buf()
            th8 = sb(st, "th8", [64, 64]); B_th8 = Buf()
            k.op("dve", lambda e: e.tensor_scalar(out=th8[:], in0=dai[:], scalar1=8.0, scalar2=None, op0=ALU.mult), reads=[B_d], writes=[B_th8])
            with ExitStack() as t0:
                ang = sb(t0, "ang", [64, 16, 64]); a2 = sb(t0, "a2", [64, 16, 64]); B_ang = Buf()
                dai_b = dai[:].rearrange("p (x g) -> p x g", x=1).to_broadcast([64, 16, 64])
                dar_b = dar[:].rearrange("p (x g) -> p x g", x=1).to_broadcast([64, 16, 64])
                k.op("dve", lambda e: e.tensor_tensor(out=MG[:], in0=kv[:], in1=dar_b, op=ALU.mult), reads=[B_kv, B_d], writes=[B_L])
                k.op("act", lambda e: e.activation(out=MG[:], in_=MG[:], func=AF.Exp), reads=[B_L], writes=[B_L])
                k.op("dve", lambda e: e.tensor_tensor(out=ang[:], in0=kv[:], in1=dai_b, op=ALU.mult), reads=[B_kv, B_d], writes=[B_ang])
                a3 = sb(t0, "a3", [64, 16, 64], I32); a4 = sb(t0, "a4", [64, 16, 64])
                sincos(ang[:], [64, 16, 64], LI[:], LR[:], (a2[:], a3[:], a4[:]), B_ang, B_L, B_ang)
                k.op("dve", lambda e: e.tensor_tensor(out=LR[:], in0=LR[:], in1=MG[:], op=ALU.mult), reads=[B_L], writes=[B_L])
                k.op("dve", lambda e: e.tensor_tensor(out=LI[:], in0=LI[:], in1=MG[:], op=ALU.mult), reads=[B_L], writes=[B_L])
                k.barrier()
            cre = sb(st, "cre", [64, 64]); cim = sb(st, "cim", [64, 64]); B_c = Buf()
            with ExitStack() as t0:
                nr = sb(t0, "nr", [64, 64]); den = sb(t0, "den", [64, 64]); tq = sb(t0, "tq", [64, 64]); B_t = Buf()
                L1r = LR[:, 8, :]; L1i = LI[:, 8, :]
                dv = lambda fn: k.op("dve", fn, reads=[B_t, B_L, B_par, B_c], writes=[B_t, B_c])
                dv(lambda e: e.tensor_scalar(out=nr[:], in0=L1r, scalar1=-1.0, scalar2=None, op0=ALU.add))
                dv(lambda e: e.tensor_tensor(out=den[:], in0=ar[:], in1=ar[:], op=ALU.mult))
                dv(lambda e: e.tensor_tensor(out=tq[:], in0=ai[:], in1=ai[:], op=ALU.mult))
                dv(lambda e: e.tensor_tensor(out=den[:], in0=den[:], in1=tq[:], op=ALU.add))
                dv(lambda e: e.reciprocal(out=den[:], in_=den[:]))
                dv(lambda e: e.tensor_tensor(out=cre[:], in0=nr[:], in1=ar[:], op=ALU.mult))
                dv(lambda e: e.tensor_tensor(out=tq[:], in0=L1i, in1=ai[:], op=ALU.mult))
                dv(lambda e: e.tensor_tensor(out=cre[:], in0=cre[:], in1=tq[:], op=ALU.add))
                dv(lambda e: e.tensor_tensor(out=cre[:], in0=cre[:], in1=den[:], op=ALU.mult))
                dv(lambda e: e.tensor_tensor(out=cim[:], in0=L1i, in1=ar[:], op=ALU.mult))
                dv(lambda e: e.tensor_tensor(out=tq[:], in0=nr[:], in1=ai[:], op=ALU.mult))
                dv(lambda e: e.tensor_tensor(out=cim[:], in0=cim[:], in1=tq[:], op=ALU.subtract))
                dv(lambda e: e.tensor_tensor(out=cim[:], in0=cim[:], in1=den[:], op=ALU.mult))
                k.barrier()
            UT_all = sb(st, "UT_all", [128, 32, 288], BF16); B_UT = Buf()
            NB = ((0, 128), (128, 128), (256, 32))
            u8v = u_scr.rearrange("(n j) f -> n (j f)", j=8)
            with ExitStack() as t0:
                U8 = sb(t0, "U8", [128, 3, 4096]); B_U8 = Buf()
                U8b = sb(t0, "U8b", [128, 3, 4096], BF16); B_U8b = Buf()
                pU = [ps(t0, f"pU{i}", [128, 1024], BF16) for i in range(2)]; B_pU = [Buf(), Buf()]
                for bi, (n0, nb) in enumerate(NB):
                    k.dma("sp", U8[0:nb, bi, :], u8v[n0:n0 + nb, :], writes=[B_U8])
                    ov = U8b[0:nb, bi, :].rearrange("p (g j c) -> p j g c", g=32, j=8, c=16)
                    iv = U8[0:nb, bi, :].rearrange("p (j g c) -> p j g c", g=32, j=8, c=16)
                    k.op("pool" if bi == 1 else "act", (lambda e, ov=ov, iv=iv: e.tensor_copy(out=ov, in_=iv)) if bi == 1 else
                         (lambda e, ov=ov, iv=iv: e.copy(out=ov, in_=iv)), reads=[B_U8], writes=[B_U8b])
                for g in range(32):
                    i = g % 2
                    for bi, (n0, nb) in enumerate(NB):
                        src = U8b[0:nb, bi, g * 128:(g + 1) * 128]
                        k.op("pe", lambda e, i=i, src=src, n0=n0, nb=nb: e.transpose(out=pU[i][:, n0:n0 + nb], in_=src, identity=ident_b[0:nb, 0:nb]),
                             reads=[B_U8b, B_ident_b], writes=[B_pU[i]])
                    k.op("act", lambda e, i=i, g=g: e.copy(out=UT_all[:, g, :], in_=pU[i][:, 0:288]), reads=[B_pU[i]], writes=[B_UT])
                k.barrier()
            COr = sb(st, "COr", [64, 2, 32, 128], BF16); COi = sb(st, "COi", [64, 2, 32, 128], BF16); B_CO = Buf()
            T_all = sb(st, "T_all", [128, 32, 128], BF16); B_T = Buf()
            WinT = sb(st, "WinT", [128, 2, 32, 128], BF16); B_WinT = Buf()
            with ExitStack() as t0:
                BLr = sb(t0, "BLr", [64, 32, 128], BF16); BLi = sb(t0, "BLi", [64, 32, 128], BF16); B_BL = Buf()
                CTr = sb(t0, "CTr", [64, 32, 128], BF16); CTi = sb(t0, "CTi", [64, 32, 128], BF16); B_CT = Buf()
                Br = sb(t0, "Br", [64, 64, 16]); Bi = sb(t0, "Bi", [64, 64, 16]); B_B = Buf()
                Cr = sb(t0, "Cr", [64, 64, 16]); Ci = sb(t0, "Ci", [64, 64, 16]); B_C = Buf()
                Bbr = sb(t0, "Bbr", [64, 64, 16]); Bbi = sb(t0, "Bbi", [64, 64, 16]); B_Bb = Buf()
                with nc.allow_non_contiguous_dma(reason="ssm B/C tables"):
                    for d in range(2):
                        k.dma("sp", Br[:, d * 32:(d + 1) * 32, :], b_re[d].rearrange("g p c -> p g c"), writes=[B_B])
                        k.dma("sp", Bi[:, d * 32:(d + 1) * 32, :], b_im[d].rearrange("g p c -> p g c"), writes=[B_B])
                        for gb in range(4):
                            sl = slice(d * 32 + gb * 8, d * 32 + gb * 8 + 8)
                            k.dma("sp", Cr[:, sl, :], c_re[d, gb * 8:(gb + 1) * 8].rearrange("g c p -> p g c"), writes=[B_C])
                            k.dma("act", Ci[:, sl, :], c_im[d, gb * 8:(gb + 1) * 8].rearrange("g c p -> p g c"), writes=[B_C])
                NLR = sb(t0, "NLR", [64, 16, 64]); NLI = sb(t0, "NLI", [64, 16, 64])
                k.op("dve", lambda e: e.tensor_scalar(out=NLR[:], in0=LR[:], scalar1=-1.0, scalar2=None, op0=ALU.mult), reads=[B_L], writes=[B_L])
                k.op("dve", lambda e: e.tensor_scalar(out=NLI[:], in0=LI[:], scalar1=-1.0, scalar2=None, op0=ALU.mult), reads=[B_L], writes=[B_L])
                ta = sb(t0, "ta", [64, 32, 16]); tb_ = sb(t0, "tb", [64, 32, 16]); B_tab = Buf()
                tc_ = sb(t0, "tc", [64, 32, 16]); td_ = sb(t0, "td", [64, 32, 16]); B_tcd = Buf()
                for d in range(2):
                    dsl = slice(d * 32, (d + 1) * 32)
                    creb = bc3(cre[:, dsl], 16); cimb = bc3(cim[:, dsl], 16)
                    dv = lambda fn: k.op("dve", fn, reads=[B_B, B_c, B_tab, B_Bb], writes=[B_tab, B_Bb])
                    dv(lambda e, dsl=dsl, creb=creb: e.tensor_tensor(out=ta[:], in0=Br[:, dsl, :], in1=creb, op=ALU.mult))
                    dv(lambda e, dsl=dsl, cimb=cimb: e.tensor_tensor(out=tb_[:], in0=Bi[:, dsl, :], in1=cimb, op=ALU.mult))
                    dv(lambda e, dsl=dsl: e.tensor_tensor(out=Bbr[:, dsl, :], in0=ta[:], in1=tb_[:], op=ALU.subtract))
                    dv(lambda e, dsl=dsl, creb=creb: e.tensor_tensor(out=ta[:], in0=Bi[:, dsl, :], in1=creb, op=ALU.mult))
                    dv(lambda e, dsl=dsl, cimb=cimb: e.tensor_tensor(out=tb_[:], in0=Br[:, dsl, :], in1=cimb, op=ALU.mult))
                    dv(lambda e, dsl=dsl: e.tensor_tensor(out=Bbi[:, dsl, :], in0=ta[:], in1=tb_[:], op=ALU.add))
                pTd = [ps(t0, f"pTd{i}", [128, 512]) for i in range(2)]; B_pTd = [Buf(), Buf()]
                pW = [ps(t0, f"pW{i}", [128, 8, 128], BF16) for i in range(2)]; B_pW = [Buf(), Buf()]
                tt = sb(t0, "tt", [128, 128]); B_tt = Buf()
                for d in range(2):
                    dsl = slice(d * 32, (d + 1) * 32)
                    for j in range(8):
                        e_ = (7 - j) if d == 0 else j
                        lr = bc3(LR[:, e_ + 7, dsl], 16); li = bc3(LI[:, e_ + 7, dsl], 16)
                        o_r = BLr[:, :, j * 16:(j + 1) * 16]; o_i = BLi[:, :, j * 16:(j + 1) * 16]
                        dv2 = lambda fn: k.op("dve", fn, reads=[B_Bb, B_L, B_tab, B_BL], writes=[B_tab, B_BL])
                        dv2(lambda e, lr=lr: e.tensor_tensor(out=ta[:], in0=Bbr[:, dsl, :], in1=lr, op=ALU.mult))
                        dv2(lambda e, li=li: e.tensor_tensor(out=tb_[:], in0=Bbi[:, dsl, :], in1=li, op=ALU.mult))
                        dv2(lambda e, o_r=o_r: e.tensor_tensor(out=o_r, in0=ta[:], in1=tb_[:], op=ALU.subtract))
                        dv2(lambda e, lr=lr: e.tensor_tensor(out=ta[:], in0=Bbi[:, dsl, :], in1=lr, op=ALU.mult))
                        dv2(lambda e, li=li: e.tensor_tensor(out=tb_[:], in0=Bbr[:, dsl, :], in1=li, op=ALU.mult))
                        dv2(lambda e, o_i=o_i: e.tensor_tensor(out=o_i, in0=ta[:], in1=tb_[:], op=ALU.add))
                        f_ = (j - 7) if d == 0 else -j
                        for (kk, o_r, o_i, BO) in ((f_ + 7, CTr[:, :, j * 16:(j + 1) * 16], CTi[:, :, j * 16:(j + 1) * 16], B_CT),
                                                   (f_ + 15, COr[:, d, :, j * 16:(j + 1) * 16], COi[:, d, :, j * 16:(j + 1) * 16], B_CO)):
                            lr = bc3(LR[:, kk, dsl], 16); li = bc3(LI[:, kk, dsl], 16)
                            pl = lambda fn, BO=BO: k.op("pool", fn, reads=[B_C, B_L, B_tcd, BO], writes=[B_tcd, BO])
                            pl(lambda e, lr=lr: e.tensor_tensor(out=tc_[:], in0=Cr[:, dsl, :], in1=lr, op=ALU.mult))
                            pl(lambda e, li=li: e.tensor_tensor(out=td_[:], in0=Ci[:, dsl, :], in1=li, op=ALU.mult))
                            pl(lambda e, o_r=o_r: e.tensor_tensor(out=o_r, in0=tc_[:], in1=td_[:], op=ALU.subtract))
                            nlr = bc3(NLR[:, kk, dsl], 16); nli = bc3(NLI[:, kk, dsl], 16)
                            pl(lambda e, nlr=nlr: e.tensor_tensor(out=tc_[:], in0=Ci[:, dsl, :], in1=nlr, op=ALU.mult))
                            pl(lambda e, nli=nli: e.tensor_tensor(out=td_[:], in0=Cr[:, dsl, :], in1=nli, op=ALU.mult))
                            pl(lambda e, o_i=o_i: e.tensor_tensor(out=o_i, in0=tc_[:], in1=td_[:], op=ALU.add))
                    for g in range(32):
                        i = g % 2
                        k.op("pe", lambda e, i=i, g=g: e.matmul(pTd[i][:, 0:128], lhsT=BLr[:, g, :], rhs=CTr[:, g, :], start=True, stop=False),
                             reads=[B_BL, B_CT], writes=[B_pTd[i]])
                        k.op("pe", lambda e, i=i, g=g: e.matmul(pTd[i][:, 0:128], lhsT=BLi[:, g, :], rhs=CTi[:, g, :], start=False, stop=True),
                             reads=[B_BL, B_CT], writes=[B_pTd[i]])
                        if d == 0:
                            k.op("dve", lambda e, i=i, g=g: e.tensor_tensor(out=T_all[:, g, :], in0=pTd[i][:, 0:128], in1=maskF[:], op=ALU.mult),
                                 reads=[B_pTd[i], B_mk], writes=[B_T])
                        else:
                            k.op("dve", lambda e, i=i: e.tensor_tensor(out=tt[:], in0=pTd[i][:, 0:128], in1=maskB[:], op=ALU.mult),
                                 reads=[B_pTd[i], B_mk], writes=[B_tt])
                            k.op("dve", lambda e, g=g: e.tensor_tensor(out=T_all[:, g, :], in0=T_all[:, g, :], in1=tt[:], op=ALU.add),
                                 reads=[B_tt, B_T], writes=[B_T])
                        k.op("pe", lambda e, i=i, g=g: e.transpose(out=pW[i][:, 0, 0:64], in_=BLr[:, g, :], identity=ident_b[0:64, 0:64]),
                             reads=[B_BL, B_ident_b], writes=[B_pW[i]])
                        k.op("pe", lambda e, i=i, g=g: e.transpose(out=pW[i][:, 0, 64:128], in_=BLi[:, g, :], identity=ident_b[0:64, 0:64]),
                             reads=[B_BL, B_ident_b], writes=[B_pW[i]])
                        k.op("act", lambda e, i=i, g=g, d=d: e.copy(out=WinT[:, d, g, :], in_=pW[i][:, 0, :]), reads=[B_pW[i]], writes=[B_WinT])
                k.barrier()
            with ExitStack() as t0:
                Yt = sb(t0, "Yt", [128, 3, 4096], BF16); B_Yt = Buf()
                pXd = [[ps(t0, f"pX{d}{i}", [64, 512]) for i in range(2)] for d in range(2)]
                B_pXd = [[Buf(), Buf()], [Buf(), Buf()]]
                pY = ps(t0, "pY", [128, 512]); B_pY = Buf()
                pYt = ps(t0, "pYt", [128, 8, 128], BF16); B_pYt = Buf()
                W = []
                for d in range(2):
                    W.append(dict(
                        XS=sb(t0, f"XS{d}", [64, 2, 288]), B_XS=Buf(),
                        base=sb(t0, f"base{d}", [64, 288]), sarg=sb(t0, f"sarg{d}", [64, 288]), B_tr=Buf(), B_tr2=Buf(),
                        sargi=sb(t0, f"sargi{d}", [64, 288], mybir.dt.int32), sargf=sb(t0, f"sargf{d}", [64, 288]),
                        sn=sb(t0, f"sn{d}", [64, 288]), cs=sb(t0, f"cs{d}", [64, 288]), B_sc=Buf(),
                        RR=sb(t0, f"RR{d}", [64, 2, 288]), B_RR=Buf(),
                        q1=sb(t0, f"q1{d}", [64, 288]), q2=sb(t0, f"q2{d}", [64, 288]), B_q=Buf(),
                        q3=sb(t0, f"q3{d}", [64, 288]), q4=sb(t0, f"q4{d}", [64, 288]), B_q34=Buf(),
                        SS=sb(t0, f"SS{d}", [64, 2, 288]), B_SS=Buf()))
                Sp = sb(t0, "Sp", [64, 2, 2, 288], BF16); B_Spd = [Buf(), Buf()]
                Ysb = sb(t0, "Ysb", [128, 288], BF16); B_Ysb = Buf()
                k.op("dve", lambda e: e.memset(Sp[:], 0.0), writes=[B_Spd[0], B_Spd[1]])

                def chain(d, g):
                    w = W[d]
                    XS, base, sarg, sargi, sargf, sn, cs, RR, q1, q2, SS = (w[n] for n in ("XS", "base", "sarg", "sargi", "sargf", "sn", "cs", "RR", "q1", "q2", "SS"))
                    B_XS, B_tr, B_tr2, B_sc, B_RR, B_q, B_SS = (w[n] for n in ("B_XS", "B_tr", "B_tr2", "B_sc", "B_RR", "B_q", "B_SS"))
                    pX = pXd[d]; B_pX = B_pXd[d]; B_Sp = B_Spd[d]
                    dg = d * 32 + g
                    for c2 in range(2):
                        k.op("pe", lambda e, c2=c2: e.matmul(pX[c2][:, 0:288], lhsT=WinT[:, d, g, c2 * 64:(c2 + 1) * 64], rhs=UT_all[:, g, :],
                                                             start=True, stop=True), reads=[B_WinT, B_UT], writes=[B_pX[c2]])
                        if d == 0:
                            k.op("act", lambda e, c2=c2: e.copy(out=XS[:, c2, :], in_=pX[c2][:, 0:288]), reads=[B_pX[c2]], writes=[B_XS])
                        else:
                            k.op("act", lambda e, c2=c2: e.copy(out=XS[:, c2, 0:32], in_=pX[c2][:, 31::-1]), reads=[B_pX[c2]], writes=[B_XS])
                            k.op("act", lambda e, c2=c2: e.copy(out=XS[:, c2, 32:288], in_=pX[c2][:, 287:31:-1]), reads=[B_pX[c2]], writes=[B_XS])
                    k.op("dve", lambda e: e.tensor_scalar(out=base[:], in0=mrow[:], scalar1=th8[:, dg:dg + 1], scalar2=None, op0=ALU.mult),
                         reads=[B_mrow, B_th8], writes=[B_tr])
                    sincos(base[:], [64, 288], sn[:], cs[:], (sarg[:], sargi[:], sargf[:]), B_tr, B_sc, B_tr2)
                    dv = lambda fn: k.op("dve", fn, reads=[B_XS, B_sc, B_q, B_RR, B_SS, B_L], writes=[B_q, B_RR, B_SS])
                    dv(lambda e: e.tensor_tensor(out=q1[:], in0=cs[:], in1=XS[:, 0, :], op=ALU.mult))
                    dv(lambda e: e.tensor_tensor(out=q2[:], in0=sn[:], in1=XS[:, 1, :], op=ALU.mult))
                    dv(lambda e: e.tensor_tensor(out=q1[:], in0=q1[:], in1=q2[:], op=ALU.add))
                    dv(lambda e: e.tensor_tensor_scan(out=RR[:, 0, :], data0=MG[:, 15, dg:dg + 1].to_broadcast([64, 288]), data1=q1[:], initial=0.0, op0=ALU.mult, op1=ALU.add))
                    dv(lambda e: e.tensor_tensor(out=q1[:], in0=cs[:], in1=XS[:, 1, :], op=ALU.mult))
                    dv(lambda e: e.tensor_tensor(out=q2[:], in0=sn[:], in1=XS[:, 0, :], op=ALU.mult))
                    dv(lambda e: e.tensor_tensor(out=q1[:], in0=q1[:], in1=q2[:], op=ALU.subtract))
                    dv(lambda e: e.tensor_tensor_scan(out=RR[:, 1, :], data0=MG[:, 15, dg:dg + 1].to_broadcast([64, 288]), data1=q1[:], initial=0.0, op0=ALU.mult, op1=ALU.add))
                    dv(lambda e: e.tensor_tensor(out=q1[:], in0=cs[:], in1=RR[:, 0, :], op=ALU.mult))
                    dv(lambda e: e.tensor_tensor(out=q2[:], in0=sn[:], in1=RR[:, 1, :], op=ALU.mult))
                    dv(lambda e: e.tensor_tensor(out=SS[:, 0, :], in0=q1[:], in1=q2[:], op=ALU.subtract))
                    dv(lambda e: e.tensor_tensor(out=q1[:], in0=cs[:], in1=RR[:, 1, :], op=ALU.mult))
                    dv(lambda e: e.tensor_tensor(out=q2[:], in0=sn[:], in1=RR[:, 0, :], op=ALU.mult))
                    dv(lambda e: e.tensor_tensor(out=SS[:, 1, :], in0=q1[:], in1=q2[:], op=ALU.add))
                    for c2 in range(2):
                        if d == 0:
                            k.op("act", lambda e, c2=c2: e.copy(out=Sp[:, 0, c2, 1:288], in_=SS[:, c2, 0:287]), reads=[B_SS], writes=[B_Sp])
                        else:
                            k.op("act", lambda e, c2=c2: e.copy(out=Sp[:, 1, c2, 0:31], in_=SS[:, c2, 30::-1]), reads=[B_SS], writes=[B_Sp])
                            k.op("act", lambda e, c2=c2: e.copy(out=Sp[:, 1, c2, 32:288], in_=SS[:, c2, 286:30:-1]), reads=[B_SS], writes=[B_Sp])

                B_Sp = B_Spd[0]
                for g in range(32):
                    recs = []
                    orig_op = k.op
                    for d in range(2):
                        rec = []
                        k.op = lambda *a, rec=rec, **kw: rec.append((a, kw))
                        chain(d, g)
                        recs.append(rec)
                    k.op = orig_op
                    for i_ in range(max(len(r_) for r_ in recs)):
                        for r_ in recs:
                            if i_ < len(r_):
                                a_, kw_ = r_[i_]
                                k.op(*a_, **kw_)
                    k.op("pe", lambda e, g=g: e.matmul(pY[:, 0:288], lhsT=T_all[:, g, :], rhs=UT_all[:, g, :], start=True, stop=False),
                         reads=[B_T, B_UT], writes=[B_pY])
                    for d in range(2):
                        k.op("pe", lambda e, g=g, d=d: e.matmul(pY[:, 0:288], lhsT=COr[:, d, g, :], rhs=Sp[:, d, 0, :], start=False, stop=False),
                             reads=[B_CO, B_Spd[d]], writes=[B_pY])
                        k.op("pe", lambda e, g=g, d=d: e.matmul(pY[:, 0:288], lhsT=COi[:, d, g, :], rhs=Sp[:, d, 1, :], start=False, stop=(d == 1)),
                             reads=[B_CO, B_Spd[d]], writes=[B_pY])
                    k.op("act", lambda e: e.copy(out=Ysb[:], in_=pY[:, 0:288]), reads=[B_pY], writes=[B_Ysb])
                    for bi, (n0, nb) in enumerate(NB):
                        k.op("pe", lambda e, bi=bi, n0=n0, nb=nb: e.transpose(out=pYt[0:nb, bi, :], in_=Ysb[:, n0:n0 + nb], identity=ident_b[:]),
                             reads=[B_Ysb, B_ident_b], writes=[B_pYt])
                    for bi, (n0, nb) in enumerate(NB):
                        dst = Yt[0:nb, bi, :].rearrange("p (j f) -> p j f", j=8)[:, :, g * 16:(g + 1) * 16]
                        k.op("dve", lambda e, bi=bi, nb=nb, dst=dst: e.tensor_copy(out=dst, in_=pYt[0:nb, bi, :].rearrange("p (j c) -> p j c", j=8)),
                             reads=[B_pYt], writes=[B_Yt])
                y8v = y_scr.rearrange("(n j) f -> n (j f)", j=8)
                for bi, (n0, nb) in enumerate(NB):
                    k.dma("sp", y8v[n0:n0 + nb, :], Yt[0:nb, bi, :], reads=[B_Yt])
                k.barrier()


        def ssm_post(st, catT_, B_catT_):
            GC = 2.0 * math.sqrt(2.0 / math.pi)
            gw = sb(st, "gluw", [128, 4, 512], BF16); B_gw = Buf()
            k.dma("pool", gw[:], glu_w.rearrange("(k p) n -> p k n", p=128), writes=[B_gw])
            gb = sb(st, "glub", [128, 4]); B_gb = Buf()
            with nc.allow_non_contiguous_dma(reason="tiny bias"):
                k.dma("sp", gb[:], glu_b[0, :].rearrange("(c p) -> p c", p=128), writes=[B_gb])
            dbc = load_bc(st, "dskip", ssm_d[0:1, :], 512)
            yt = [sb(st, f"py{i}", [128, 512], BF16) for i in range(2)]; B_yt = [Buf(), Buf()]
            ut = [sb(st, f"pu{i}", [128, 512]) for i in range(2)]; B_ut = [Buf(), Buf()]
            xx = [sb(st, f"pxx{c_}", [128, 512]) for c_ in range(2)]; B_xx = [Buf(), Buf()]
            ww = [sb(st, f"pww{c_}", [128, 512]) for c_ in range(2)]; B_ww = [Buf(), Buf()]
            sg = [sb(st, f"psg{c_}", [128, 512]) for c_ in range(2)]; B_sg = [Buf(), Buf()]
            g_bf = [sb(st, f"pg_bf{c_}", [128, 512], BF16) for c_ in range(2)]; B_g = [Buf(), Buf()]
            gT = [sb(st, f"pgT{c_}", [128, 4, 128], BF16) for c_ in range(2)]; B_gT = [Buf(), Buf()]
            s2 = [sb(st, f"ps2{c_}", [128, 4, 128]) for c_ in range(2)]; B_s2 = [Buf(), Buf()]
            pGT = [ps(st, f"pGT{c_}", [128, 8, 128], BF16) for c_ in range(2)]; B_pGT = [Buf(), Buf()]
            pz = [ps(st, f"pz{c_}", [128, 4, 128]) for c_ in range(2)]; B_pz = [Buf(), Buf()]
            def post_tile(ti):
                i = ti % 2
                T0 = ti * 128
                row0 = T0 + CTX if ti < NLT else T0 - SEQ
                k.dma("sp", yt[i][:], y_scr[row0:row0 + 128, :], writes=[B_yt[i]])
                k.dma("sp", ut[i][:], u_scr[row0:row0 + 128, :], writes=[B_ut[i]])
                k.op("dve", lambda e, i=i: e.tensor_tensor(out=xx[i][:], in0=ut[i][:], in1=dbc[0][:], op=ALU.mult), reads=[B_ut[i], dbc[1]], writes=[B_xx[i]])
                k.op("dve", lambda e, i=i: e.tensor_tensor(out=xx[i][:], in0=xx[i][:], in1=yt[i][:], op=ALU.add), reads=[B_xx[i], B_yt[i]], writes=[B_xx[i]])
                k.op("pool", lambda e: e.tensor_tensor(out=ww[i][:], in0=xx[i][:], in1=xx[i][:], op=ALU.mult), reads=[B_xx[i]], writes=[B_ww[i]])
                k.op("pool", lambda e: e.tensor_scalar(out=ww[i][:], in0=ww[i][:], scalar1=0.044715, scalar2=1.0, op0=ALU.mult, op1=ALU.add), reads=[B_ww[i]], writes=[B_ww[i]])
                k.op("pool", lambda e: e.tensor_tensor(out=ww[i][:], in0=ww[i][:], in1=xx[i][:], op=ALU.mult), reads=[B_ww[i], B_xx[i]], writes=[B_ww[i]])
                k.op("act", lambda e: e.activation(out=sg[i][:], in_=ww[i][:], func=AF.Sigmoid, scale=GC), reads=[B_ww[i]], writes=[B_sg[i]])
                k.op("dve", lambda e: e.tensor_tensor(out=g_bf[i][:], in0=xx[i][:], in1=sg[i][:], op=ALU.mult), reads=[B_xx[i], B_sg[i]], writes=[B_g[i]])
                if "dbg_g" in dbg:
                    if ti == 0:
                        dbg_g = dscr("dbg_g", [NT, 512], BF16)
                    k.dma("sp", dbg_g[T0:T0 + 128, :], g_bf[i][:], reads=[B_g[i]])
                for kc in range(4):
                    k.op("pe", lambda e, kc=kc: e.transpose(out=pGT[i][:, kc, :], in_=g_bf[i][:, kc * 128:(kc + 1) * 128], identity=ident_b[:]),
                         reads=[B_g[i], B_ident_b], writes=[B_pGT[i]])
                k.op("act", lambda e: e.copy(out=gT[i][:], in_=pGT[i][:, 0:4, :]), reads=[B_pGT[i]], writes=[B_gT[i]])
                for n_ in range(4):
                    for kc in range(4):
                        k.op("pe", lambda e, n_=n_, kc=kc: e.matmul(pz[i][:, n_, :], lhsT=gw[:, kc, n_ * 128:(n_ + 1) * 128], rhs=gT[i][:, kc, :],
                                                                    start=(kc == 0), stop=(kc == 3)), reads=[B_gw, B_gT[i]], writes=[B_pz[i]])
                for n_ in range(4):
                    k.op("act", lambda e, n_=n_: e.activation(out=s2[i][:, n_, :], in_=pz[i][:, n_, :], func=AF.Sigmoid, bias=gb[:, n_:n_ + 1], scale=1.0),
                         reads=[B_pz[i], B_gb], writes=[B_s2[i]])
                k.op("dve", lambda e, T0=T0: e.tensor_tensor(out=catT_[:, 4:8, T0:T0 + 128], in0=gT[i][:], in1=s2[i][:], op=ALU.mult),
                     reads=[B_gT[i], B_s2[i]], writes=[B_catT_])
            for t2_ in range(0, NTILE, 2):
                replay([record(lambda ti=ti: post_tile(ti)) for ti in range(t2_, min(t2_ + 2, NTILE))])
            if "dbg_cat" in dbg:
                dbg_cat = dscr("dbg_cat", [128, 8, NT], BF16)
                k.dma("sp", dbg_cat, catT_[:], reads=[B_catT_])
            k.barrier()


        def layer1_mixer():
            LAM_INIT = 0.8 - 0.6 * math.exp(-0.3 * 1)
            SC = 0.125
            with ExitStack() as L1:
                qT2 = sb(L1, "qT2", [128, 8, SEQ], BF16); B_q2 = Buf()
                kT2 = sb(L1, "kT2", [128, 8, NT], BF16); B_k2 = Buf()
                v1 = sb(L1, "v1", [128, NTILE, D], BF16); B_v1 = Buf()
                nmax = sb(L1, "nmax", [128, 32]); B_nmax = Buf()
                for st in phase("l1proj"):
                    w_bf = sb(st, "dif_w_bf", [128, 8, 3072], BF16); B_w = Buf()
                    for kc in range(8):
                        k.dma("pool", w_bf[:, kc, :], dif_w_in[kc * 128:(kc + 1) * 128, :], writes=[B_w])
                    sc1p = [mod_bc(st, f"l1sc1p_{r}", 1, r, 1, plus1=True) for r in range(2)]
                    sh1 = [mod_bc(st, f"l1sh1_{r}", 1, r, 0) for r in range(2)]
                    xt = [sb(st, f"l1xt{i}", [128, D]) for i in range(2)]; B_xt = [Buf(), Buf()]
                    tmpf = [sb(st, f"l1tmpf{i}", [128, D]) for i in range(2)]; B_tmpf = [Buf(), Buf()]
                    h_bf = [sb(st, f"l1h_bf{i}", [128, D], BF16) for i in range(2)]; B_hbf = [Buf(), Buf()]
                    hT = [sb(st, f"l1hT{i}", [128, 8, 128], BF16) for i in range(2)]; B_hT = [Buf(), Buf()]
                    rt = [sb(st, f"l1rt{i}", [128, 64]) for i in range(2)]; B_rt = [Buf(), Buf()]
                    t1 = [sb(st, f"l1rope_t1{i}", [128, 512]) for i in range(2)]; t2 = [sb(st, f"l1rope_t2{i}", [128, 512]) for i in range(2)]; B_rtmp = [Buf(), Buf()]
                    qk_bf = [sb(st, f"l1qk_bf{i}", [128, 512], BF16) for i in range(2)]; B_qk = [Buf(), Buf()]
                    pT = [ps(st, f"l1pT{i}", [128, 8, 128], BF16) for i in range(2)]; B_pT = [Buf(), Buf()]
                    pp = [[ps(st, f"l1pp{i}{j}", [128, 512]) for j in range(2)] for i in range(2)]; B_pp = [[Buf(), Buf()], [Buf(), Buf()]]
                    pq = [ps(st, f"l1pq{i}", [128, 8, 128], BF16) for i in range(2)]; B_pq = [Buf(), Buf()]
                    sqt = [sb(st, f"l1sq{i}", [128, 512]) for i in range(2)]; rs8 = [sb(st, f"l1rs8{i}", [128, 8]) for i in range(2)]; B_sq = [Buf(), Buf()]
                    k.op("dve", lambda e: e.memset(nmax[:], 0.0), writes=[B_nmax])
                    ibs = [0, 0]

                    def proj_tile(ti):
                        i = ti % 2
                        r = 0 if ti < NLT else 1
                        T0 = ti * 128
                        k.dma("sp", xt[i][:], src_rows(ti, 1), writes=[B_xt[i]])
                        if r == 0:
                            k.dma("sp", rt[i][:], rope_cs[T0:T0 + 128, :], writes=[B_rt[i]])
                        k.op("dve", lambda e: e.tensor_tensor(out=tmpf[i][:], in0=xt[i][:], in1=sc1p[r][0][:], op=ALU.mult),
                             reads=[B_xt[i], sc1p[r][1]], writes=[B_tmpf[i]])
                        k.op("pool", lambda e: e.tensor_tensor(out=h_bf[i][:], in0=tmpf[i][:], in1=sh1[r][0][:], op=ALU.add),
                             reads=[B_tmpf[i], sh1[r][1]], writes=[B_hbf[i]])
                        for kc in range(8):
                            k.op("pe", lambda e, kc=kc: e.transpose(out=pT[i][:, kc, :], in_=h_bf[i][:, kc * 128:(kc + 1) * 128], identity=ident_b[:]),
                                 reads=[B_hbf[i], B_ident_b], writes=[B_pT[i]])
                        k.op("act", lambda e: e.copy(out=hT[i][:], in_=pT[i][:]), reads=[B_pT[i]], writes=[B_hT[i]])
                        for cb in range(6):
                            if r == 1 and cb < 2:
                                continue
                            j = ibs[i] % 2; ibs[i] += 1
                            ppj = pp[i][j]; Bppj = B_pp[i][j]
                            for kc in range(8):
                                k.op("pe", lambda e, kc=kc, ppj=ppj, cb=cb: e.matmul(
                                    ppj[:], lhsT=hT[i][:, kc, :], rhs=w_bf[:, kc, cb * 512:(cb + 1) * 512],
                                    start=(kc == 0), stop=(kc == 7)), reads=[B_hT[i], B_w], writes=[Bppj])
                            if cb >= 4:
                                c0 = (cb - 4) * 512
                                k.op("act", lambda e, ppj=ppj, c0=c0: e.copy(out=v1[:, ti, c0:c0 + 512], in_=ppj[:]), reads=[Bppj], writes=[B_v1])
                                continue
                            if r == 0:
                                rope_apply(None, ppj[:], Bppj, qk_bf[i][:], B_qk[i], 8, rt[i], B_rt[i], t1[i][:], t2[i][:], B_rtmp[i])
                            else:
                                k.op("dve", lambda e, ppj=ppj: e.tensor_copy(out=qk_bf[i][:], in_=ppj[:]), reads=[Bppj], writes=[B_qk[i]])
                            k.op("dve", lambda e: e.tensor_tensor(out=sqt[i][:], in0=qk_bf[i][:], in1=qk_bf[i][:], op=ALU.mult), reads=[B_qk[i], B_sq[i]], writes=[B_sq[i]])
                            k.op("dve", lambda e: e.tensor_reduce(out=rs8[i][:], in_=sqt[i][:].rearrange("p (m d) -> p m d", d=64), axis=AX.X, op=ALU.add), reads=[B_sq[i]], writes=[B_sq[i]])
                            k.op("dve", lambda e, cb=cb: e.tensor_tensor(out=nmax[:, cb * 8:(cb + 1) * 8], in0=nmax[:, cb * 8:(cb + 1) * 8], in1=rs8[i][:], op=ALU.max),
                                 reads=[B_sq[i], B_nmax], writes=[B_nmax])
                            for hh in range(4):
                                k.op("pe", lambda e, hh=hh: e.transpose(out=pq[i][:, hh, :], in_=qk_bf[i][:, hh * 128:(hh + 1) * 128], identity=ident_b[:]),
                                     reads=[B_qk[i], B_ident_b], writes=[B_pq[i]])
                            dstT, BD = (qT2, B_q2) if cb < 2 else (kT2, B_k2)
                            h0 = (cb % 2) * 4
                            k.op("act", lambda e, dstT=dstT, h0=h0: e.copy(out=dstT[:, h0:h0 + 4, T0:T0 + 128], in_=pq[i][:, 0:4, :]),
                                 reads=[B_pq[i]], writes=[BD])

                    for t2_ in range(0, NTILE, 2):
                        replay([record(lambda ti=ti: proj_tile(ti)) for ti in range(t2_, min(t2_ + 2, NTILE))])
                    k.barrier()
                for st in phase("l1att"):
                    w_bf = sb(st, "difwo_bf", [128, 8, D], BF16); B_w = Buf()
                    for kc in range(8):
                        k.dma("pool", w_bf[:, kc, :], dif_w_out[kc * 128:(kc + 1) * 128, :], writes=[B_w])
                    g1 = mod_bc(st, "l1g1", 1, 0, 2)
                    lng = load_bc(st, "l1ln1g", ln1_g[1:2, :], D)
                    lnb = load_bc(st, "l1ln1b", ln1_b[1:2, :], D)
                    wk = ln_work(st, "l1e1")
                    xo = [sb(st, f"l1xo{i}", [128, D]) for i in range(2)]; B_xo = [Buf(), Buf()]
                    lam = sb(st, "lam", [128, 8]); B_lam = Buf()
                    lq = [load_bc(st, f"lq{i}", a[0:1, :], 64) for i, a in enumerate((lam_q1, lam_k1, lam_q2, lam_k2))]
                    ltmp = sb(st, "ltmp", [128, 64]); B_lt = Buf()
                    for i2 in range(2):
                        k.op("dve", lambda e, i2=i2: e.tensor_tensor(out=ltmp[:], in0=lq[2 * i2][0][:], in1=lq[2 * i2 + 1][0][:], op=ALU.mult),
                             reads=[lq[2 * i2][1], lq[2 * i2 + 1][1], B_lt], writes=[B_lt])
                        k.op("dve", lambda e, i2=i2: e.reduce_sum(out=lam[:, i2:i2 + 1], in_=ltmp[:], axis=AX.X), reads=[B_lt, B_lam], writes=[B_lam])
                    k.op("act", lambda e: e.activation(out=lam[:, 2:4], in_=lam[:, 0:2], func=AF.Exp), reads=[B_lam], writes=[B_lam])
                    k.op("dve", lambda e: e.tensor_tensor(out=lam[:, 4:5], in0=lam[:, 2:3], in1=lam[:, 3:4], op=ALU.subtract), reads=[B_lam], writes=[B_lam])
                    k.op("dve", lambda e: e.tensor_scalar(out=lam[:, 5:6], in0=lam[:, 4:5], scalar1=-1.0, scalar2=-LAM_INIT, op0=ALU.mult, op1=ALU.add), reads=[B_lam], writes=[B_lam])
                    sg_col = sb(st, "sg_col", [128, 1]); B_sg = Buf()
                    with nc.allow_non_contiguous_dma(reason="tiny"):
                        k.dma("sp", sg_col[:], subln_g[0, :].rearrange("(p x) -> p x", x=1), writes=[B_sg])
                    k.op("dve", lambda e: e.tensor_scalar(out=sg_col[:], in0=sg_col[:], scalar1=1.0 - LAM_INIT, scalar2=None, op0=ALU.mult), reads=[B_sg], writes=[B_sg])
                    ones_bf = sb(st, "ones_bf", [128, 128], BF16); B_ones = Buf()
                    k.op("dve", lambda e: e.memset(ones_bf[:], 1.0), writes=[B_ones])
                    negC = sb(st, "negC", [128, 16]); B_negC = Buf()
                    with ExitStack() as t0:
                        nb = sb(t0, "nmax_bf", [128, 32], BF16); B_nb = Buf()
                        k.op("dve", lambda e: e.tensor_scalar(out=nb[:], in0=nmax[:], scalar1=1.02, scalar2=None, op0=ALU.mult), reads=[B_nmax], writes=[B_nb])
                        pn = ps(t0, "pn", [16, 1024], BF16); B_pn = Buf()
                        k.op("pe", lambda e: e.transpose(out=pn[:, 0:128], in_=nb[:, 0:16], identity=ident_b[:]), reads=[B_nb, B_ident_b], writes=[B_pn])
                        k.op("pe", lambda e: e.transpose(out=pn[:, 128:256], in_=nb[:, 16:32], identity=ident_b[:]), reads=[B_nb, B_ident_b], writes=[B_pn])
                        r2 = sb(t0, "r2", [16, 8]); B_r2 = Buf()
                        k.op("dve", lambda e: e.reduce_max(out=r2[:, 0:1], in_=pn[:, 0:128], axis=AX.X), reads=[B_pn], writes=[B_r2])
                        k.op("dve", lambda e: e.reduce_max(out=r2[:, 1:2], in_=pn[:, 128:256], axis=AX.X), reads=[B_pn, B_r2], writes=[B_r2])
                        k.op("dve", lambda e: e.tensor_tensor(out=r2[:, 2:3], in0=r2[:, 0:1], in1=r2[:, 1:2], op=ALU.mult), reads=[B_r2], writes=[B_r2])
                        k.op("act", lambda e: e.sqrt(out=r2[:, 3:4], in_=r2[:, 2:3]), reads=[B_r2], writes=[B_r2])
                        k.op("dve", lambda e: e.tensor_scalar(out=r2[:, 4:5], in0=r2[:, 3:4], scalar1=-SC, scalar2=None, op0=ALU.mult), reads=[B_r2], writes=[B_r2])
                        dg = sb(t0, "dgC", [16, 16], BF16); B_dg = Buf()
                        k.op("dve", lambda e: e.tensor_scalar(out=dg[:], in0=ident_f[0:16, 0:16], scalar1=r2[:, 4:5], scalar2=None, op0=ALU.mult), reads=[B_r2, B_ident_f], writes=[B_dg])
                        pc = ps(t0, "pcb", [128, 512]); B_pc = Buf()
                        k.op("pe", lambda e: e.matmul(pc[:, 0:16], lhsT=ones_bf[0:16, :], rhs=dg[:], start=True, stop=True), reads=[B_ones, B_dg], writes=[B_pc])
                        k.op("dve", lambda e: e.tensor_copy(out=negC[:], in_=pc[:, 0:16]), reads=[B_pc], writes=[B_negC])
                        k.barrier()
                    ET = [sb(st, f"ET{i}", [128, 512], BF16) for i in range(4)]; B_ET = [Buf() for _ in range(4)]
                    aoT = sb(st, "aoT_all", [128, 8, 512], BF16); B_aoT = Buf()
                    rz = sb(st, "rz", [1, 2, 512]); B_rz = Buf()
                    rzb = sb(st, "rzb", [1, 4, 512], BF16); B_rzb = Buf()
                    bcs = sb(st, "bcs", [128, 512]); B_bcs = Buf()
                    oT = sb(st, "oT", [128, 512]); B_oT = Buf()
                    t5 = sb(st, "t5", [128, 512]); B_t5 = Buf()
                    sqb = sb(st, "sqb", [128, 512], BF16); B_sqb = Buf()
                    pS = [ps(st, f"pS{i}", [128, 512]) for i in range(2)]; B_pS = [Buf(), Buf()]
                    pO4 = [ps(st, f"pO{i}", [128, 512]) for i in range(4)]; B_pO4 = [Buf() for _ in range(4)]
                    pZ1 = ps(st, "pZ", [1, 512]); B_pZ1 = Buf()
                    pZ = [pZ1, pZ1]; B_pZ = [B_pZ1, B_pZ1]
                    pB = ps(st, "pB", [128, 512]); B_pB = Buf()
                    cnt = {"s": 0, "e": 0}

                    def bcast_row(hi, lo):
                        k.op("pe", lambda e: e.matmul(pB[:], lhsT=ones_bf[0:1, :], rhs=hi, start=True, stop=False), reads=[B_ones, B_rzb], writes=[B_pB])
                        k.op("pe", lambda e: e.matmul(pB[:], lhsT=ones_bf[0:1, :], rhs=lo, start=False, stop=True), reads=[B_ones, B_rzb], writes=[B_pB])
                        k.op("act", lambda e: e.copy(out=bcs[:], in_=pB[:]), reads=[B_pB], writes=[B_bcs])

                    def split_row(src, j):
                        k.op("dve", lambda e: e.tensor_copy(out=rzb[:, 2 * j, :], in_=src), reads=[B_rz], writes=[B_rzb])
                        k.op("dve", lambda e: e.tensor_tensor(out=rzb[:, 2 * j + 1, :], in0=src, in1=rzb[:, 2 * j, :], op=ALU.subtract), reads=[B_rz, B_rzb], writes=[B_rzb])

                    zs = sb(st, "zs", [1, 2, 512]); B_zs = Buf()

                    def qk_exp(Q0, h, c, b):
                        ps_ = slice(c * 64, (c + 1) * 64)
                        m = h * 2 + c
                        js = cnt["s"] % 2; cnt["s"] += 1
                        je = cnt["e"] % 4; cnt["e"] += 1
                        k.op("pe", lambda e: e.matmul(pS[js][:], lhsT=kT2[ps_, h, b * 128:(b + 1) * 128], rhs=qT2[ps_, h, Q0:Q0 + 512],
                                                      start=True, stop=True), reads=[B_k2, B_q2], writes=[B_pS[js]])
                        k.op("act", lambda e: e.activation(out=ET[je][:], in_=pS[js][:], func=AF.Exp, bias=negC[:, m:m + 1], scale=SC),
                             reads=[B_pS[js], B_negC], writes=[B_ET[je]])
                        return je

                    def pvz(h, c, b, je):
                        pO = pO4[(h % 2) * 2:(h % 2) * 2 + 2]; B_pO = B_pO4[(h % 2) * 2:(h % 2) * 2 + 2]
                        Zacc = Zacc4[(h % 2) * 2:(h % 2) * 2 + 2]; B_Zacc = B_Zacc4[(h % 2) * 2:(h % 2) * 2 + 2]
                        k.op("pe", lambda e: e.matmul(pO[c][:], lhsT=v1[:, b, h * 128:(h + 1) * 128], rhs=ET[je][:],
                                                      start=(b == 0), stop=(b == NTILE - 1)), reads=[B_v1, B_ET[je]], writes=[B_pO[c]])
                        if b == 0:
                            k.op("dve", lambda e: e.tensor_copy(out=Zacc[c][:], in_=ET[je][:]), reads=[B_ET[je]], writes=[B_Zacc[c]])
                        else:
                            k.op("dve", lambda e: e.tensor_tensor(out=Zacc[c][:], in0=Zacc[c][:], in1=ET[je][:], op=ALU.add), reads=[B_ET[je], B_Zacc[c]], writes=[B_Zacc[c]])

                    Zacc4 = [sb(st, f"Zacc{i}", [128, 512]) for i in range(4)]; B_Zacc4 = [Buf() for _ in range(4)]
                    ones_f = sb(st, "ones_f", [128, 1]); B_onesf = Buf()
                    k.op("dve", lambda e: e.memset(ones_f[:], 1.0), writes=[B_onesf])

                    def bcast_recip(c, Zacc, B_Zacc):
                        k.op("pe", lambda e: e.matmul(pZ[c][:], lhsT=ones_f[:, 0:1], rhs=Zacc[c][:], start=True, stop=True), reads=[B_onesf, B_Zacc[c]], writes=[B_pZ[c]])
                        k.op("act", lambda e: e.copy(out=zs[:, c, :], in_=pZ[c][:]), reads=[B_pZ[c], B_zs], writes=[B_zs])
                        k.op("dve", lambda e: e.tensor_copy(out=rzb[:, 2 * c, :], in_=zs[:, c, :]), reads=[B_zs, B_rzb], writes=[B_rzb])
                        k.op("dve", lambda e: e.tensor_tensor(out=rzb[:, 2 * c + 1, :], in0=zs[:, c, :], in1=rzb[:, 2 * c, :], op=ALU.subtract), reads=[B_zs, B_rzb], writes=[B_rzb])
                        k.op("pe", lambda e: e.matmul(pB[:], lhsT=ones_bf[0:1, :], rhs=rzb[:, 2 * c, :], start=True, stop=False), reads=[B_ones, B_rzb], writes=[B_pB])
                        k.op("pe", lambda e: e.matmul(pB[:], lhsT=ones_bf[0:1, :], rhs=rzb[:, 2 * c + 1, :], start=False, stop=True), reads=[B_ones, B_rzb], writes=[B_pB])
                        k.op("dve", lambda e: e.reciprocal(out=bcs[:], in_=pB[:]), reads=[B_pB, B_bcs], writes=[B_bcs])

                    def epilogue(h):
                        pO = pO4[(h % 2) * 2:(h % 2) * 2 + 2]; B_pO = B_pO4[(h % 2) * 2:(h % 2) * 2 + 2]
                        Zacc = Zacc4[(h % 2) * 2:(h % 2) * 2 + 2]; B_Zacc = B_Zacc4[(h % 2) * 2:(h % 2) * 2 + 2]
                        bcast_recip(0, Zacc, B_Zacc)
                        k.op("dve", lambda e: e.tensor_tensor(out=oT[:], in0=pO[0][:], in1=bcs[:], op=ALU.mult), reads=[B_pO[0], B_bcs, B_oT], writes=[B_oT])
                        bcast_recip(1, Zacc, B_Zacc)
                        k.op("dve", lambda e: e.tensor_tensor(out=t5[:], in0=pO[1][:], in1=bcs[:], op=ALU.mult), reads=[B_pO[1], B_bcs, B_t5], writes=[B_t5])
                        k.op("dve", lambda e: e.scalar_tensor_tensor(out=oT[:], in0=t5[:], scalar=lam[:, 5:6], in1=oT[:], op0=ALU.mult, op1=ALU.add),
                             reads=[B_oT, B_t5, B_lam], writes=[B_oT])
                        k.op("dve", lambda e: e.tensor_tensor(out=sqb[:], in0=oT[:], in1=oT[:], op=ALU.mult), reads=[B_oT, B_sqb], writes=[B_sqb])
                        k.op("pe", lambda e: e.matmul(pZ[0][:], lhsT=ones_bf[:, 0:1], rhs=sqb[:], start=True, stop=True), reads=[B_ones, B_sqb], writes=[B_pZ[0]])
                        k.op("act", lambda e: e.activation(out=rz[:, 0, :], in_=pZ[0][:], func=AF.Ln, scale=1.0 / 128.0, bias=eps_t[0:1, :]), reads=[B_pZ[0], B_rz, B_eps], writes=[B_rz])
                        k.op("act", lambda e: e.activation(out=rz[:, 0, :], in_=rz[:, 0, :], func=AF.Exp, scale=-0.5), reads=[B_rz], writes=[B_rz])
                        split_row(rz[:, 0, :], 0)
                        bcast_row(rzb[:, 0, :], rzb[:, 1, :])
                        k.op("dve", lambda e: e.scalar_tensor_tensor(out=aoT[:, h, :], in0=oT[:], scalar=sg_col[:, 0:1], in1=bcs[:], op0=ALU.mult, op1=ALU.mult),
                             reads=[B_oT, B_sg, B_bcs], writes=[B_aoT])

                    eps_t = sb(st, "eps_t", [128, 1]); B_eps = Buf()
                    k.op("dve", lambda e: e.memset(eps_t[:], 1e-5), writes=[B_eps])
                    for qg in range(4):
                        Q0 = qg * 512
                        steps = [(h, c, b) for h in range(8) for c in range(2) for b in range(NTILE)]
                        je_next = qk_exp(Q0, *steps[0])
                        pending = []
                        for si, (h, c, b) in enumerate(steps):
                            je_cur = je_next
                            if si + 1 < len(steps):
                                je_next = qk_exp(Q0, *steps[si + 1])
                            pvz(h, c, b, je_cur)
                            if pending:
                                a_, kw_ = pending.pop(0)
                                k.op(*a_, **kw_)
                            if c == 1 and b == NTILE - 1:
                                while pending:
                                    a_, kw_ = pending.pop(0)
                                    k.op(*a_, **kw_)
                                orig_op = k.op
                                rec = []
                                k.op = lambda *a, rec=rec, **kw: rec.append((a, kw))
                                epilogue(h)
                                k.op = orig_op
                                pending = rec
                        while pending:
                            a_, kw_ = pending.pop(0)
                            k.op(*a_, **kw_)
                        for tt in range(4):
                            ti = qg * 4 + tt
                            T0 = ti * 128
                            i = ti % 2
                            k.dma("sp", xo[i][:], src_rows(ti, 1), writes=[B_xo[i]])
                            for hf in range(2):
                                for h in range(8):
                                    k.op("pe", lambda e, h=h, hf=hf, tt=tt: e.matmul(pS[hf][:], lhsT=aoT[:, h, tt * 128:(tt + 1) * 128], rhs=w_bf[:, h, hf * 512:(hf + 1) * 512],
                                                                                start=(h == 0), stop=(h == 7)), reads=[B_aoT, B_w], writes=[B_pS[hf]])
                            ln_epilogue(wk, [pS[0][:], pS[1][:]], [B_pS[0], B_pS[1]], xo[i], B_xo[i], g1, lng, lnb, x1_scr[T0:T0 + 128, :])
                    k.barrier()

        with ExitStack() as L0:
            catT = sb(L0, "catT", [128, 8, NT], BF16); B_catT = Buf()
            LA = ExitStack()
            qT = sb(LA, "qT", [64, 8, NT], BF16); B_qT = Buf()
            kT = sb(LA, "kT", [64, 2, NT], BF16); B_kT = Buf()
            v_all = sb(LA, "v_all", [128, NTILE, 128], BF16); B_v = Buf()
            for st in phase("l0proj"):
                w_bf = sb(st, "w_in_bf", [128, 8, 1280], BF16); B_w = Buf()
                for kc in range(8):
                    k.dma("pool", w_bf[:, kc, :], w_in0[kc * 128:(kc + 1) * 128, :], writes=[B_w])
                sc1p = [None, None]; sh1 = [None, None]
                for r in range(2):
                    sh1[r] = mod_bc(st, f"sh1_{r}", 0, r, 0)
                    sc1p[r] = mod_bc(st, f"sc1p_{r}", 0, r, 1, plus1=True)
                xt = [sb(st, f"xt{i}", [128, D]) for i in range(2)]; B_xt = [Buf(), Buf()]
                tmpf = [sb(st, f"tmpf{i}", [128, D]) for i in range(2)]; B_tmpf = [Buf(), Buf()]
                h_bf = [sb(st, f"h_bf{i}", [128, D], BF16) for i in range(2)]; B_hbf = [Buf(), Buf()]
                hT = [sb(st, f"hT{i}", [128, 8, 128], BF16) for i in range(2)]; B_hT = [Buf(), Buf()]
                rt = [sb(st, f"rt{i}", [128, 64]) for i in range(2)]; B_rt = [Buf(), Buf()]
                t1 = [sb(st, f"rope_t1{i}", [128, 640]) for i in range(2)]; t2 = [sb(st, f"rope_t2{i}", [128, 640]) for i in range(2)]; B_rtmp = [Buf(), Buf()]
                qk_bf = [sb(st, f"qk_bf{i}", [128, 640], BF16) for i in range(2)]; B_qk = [Buf(), Buf()]
                ut = [sb(st, f"ut{i}", [128, 512]) for i in range(2)]; B_ut = [Buf(), Buf()]
                pA = [ps(st, f"pA{i}", [128, 8, 128], BF16) for i in range(2)]; B_pA = [Buf(), Buf()]
                pp = [[ps(st, f"pp{i}{j}", [128, 512]) for j in range(3)] for i in range(2)]; B_pp = [[Buf(), Buf(), Buf()], [Buf(), Buf(), Buf()]]

                def proj0_tile(ti):
                    i = ti % 2
                    r = 0 if ti < NLT else 1
                    T0 = ti * 128
                    ppi = pp[i]; Bppi = B_pp[i]
                    k.dma("sp", xt[i][:], src_rows(ti, 0), writes=[B_xt[i]])
                    if r == 0:
                        k.dma("sp", rt[i][:], rope_cs[T0:T0 + 128, :], writes=[B_rt[i]])
                    k.op("dve", lambda e: e.tensor_tensor(out=tmpf[i][:], in0=xt[i][:], in1=sc1p[r][0][:], op=ALU.mult),
                         reads=[B_xt[i], sc1p[r][1]], writes=[B_tmpf[i]])
                    k.op("pool", lambda e: e.tensor_tensor(out=h_bf[i][:], in0=tmpf[i][:], in1=sh1[r][0][:], op=ALU.add),
                         reads=[B_tmpf[i], sh1[r][1]], writes=[B_hbf[i]])
                    for kc in range(8):
                        k.op("pe", lambda e, kc=kc: e.transpose(out=pA[i][:, kc, :], in_=h_bf[i][:, kc * 128:(kc + 1) * 128], identity=ident_b[:]),
                             reads=[B_hbf[i], B_ident_b], writes=[B_pA[i]])
                    k.op("act", lambda e: e.copy(out=hT[i][:], in_=pA[i][:]), reads=[B_pA[i]], writes=[B_hT[i]])
                    for nb, (c0, c1) in enumerate(((0, 512), (512, 1024), (1024, 1280))):
                        for kc in range(8):
                            k.op("pe", lambda e, kc=kc, nb=nb, c0=c0, c1=c1: e.matmul(
                                ppi[nb][:, 0:c1 - c0], lhsT=hT[i][:, kc, :], rhs=w_bf[:, kc, c0:c1],
                                start=(kc == 0), stop=(kc == 7)),
                                reads=[B_hT[i], B_w], writes=[Bppi[nb]])
                    if r == 0:
                        rope_apply(None, ppi[0][:, 0:512], Bppi[0], qk_bf[i][:, 0:512], B_qk[i], 8, rt[i], B_rt[i], t1[i][:, 0:512], t2[i][:, 0:512], B_rtmp[i])
                        rope_apply(None, ppi[1][:, 0:128], Bppi[1], qk_bf[i][:, 512:640], B_qk[i], 2, rt[i], B_rt[i], t1[i][:, 512:640], t2[i][:, 512:640], B_rtmp[i])
                    else:
                        k.op("dve", lambda e: e.tensor_copy(out=qk_bf[i][:, 0:512], in_=ppi[0][:, 0:512]), reads=[Bppi[0]], writes=[B_qk[i]])
                        k.op("dve", lambda e: e.tensor_copy(out=qk_bf[i][:, 512:640], in_=ppi[1][:, 0:128]), reads=[Bppi[1]], writes=[B_qk[i]])
                    for h in range(8):
                        k.op("pe", lambda e, h=h: e.transpose(out=pA[i][0:64, h, :], in_=qk_bf[i][:, h * 64:(h + 1) * 64], identity=ident_b[:]),
                             reads=[B_qk[i], B_ident_b], writes=[B_pA[i]])
                    k.op("act", lambda e: e.copy(out=qT[:, :, T0:T0 + 128], in_=pA[i][0:64, :, :]), reads=[B_pA[i]], writes=[B_qT])
                    for h in range(2):
                        k.op("pe", lambda e, h=h: e.transpose(out=pA[i][0:64, h, :], in_=qk_bf[i][:, 512 + h * 64:512 + (h + 1) * 64], identity=ident_b[:]),
                             reads=[B_qk[i], B_ident_b], writes=[B_pA[i]])
                    k.op("act", lambda e: e.copy(out=kT[:, :, T0:T0 + 128], in_=pA[i][0:64, 0:2, :]), reads=[B_pA[i]], writes=[B_kT])
                    k.op("act", lambda e: e.copy(out=v_all[:, ti, :], in_=ppi[1][:, 128:256]), reads=[Bppi[1]], writes=[B_v])
                    k.op("act", lambda e: e.copy(out=ut[i][:, 0:256], in_=ppi[1][:, 256:512]), reads=[Bppi[1]], writes=[B_ut[i]])
                    k.op("act", lambda e: e.copy(out=ut[i][:, 256:512], in_=ppi[2][:, 0:256]), reads=[Bppi[2]], writes=[B_ut[i]])
                    urow = T0 + CTX if r == 0 else T0 - SEQ
                    k.dma("sp", u_scr[urow:urow + 128, :], ut[i][:], reads=[B_ut[i]])

                for t2_ in range(0, NTILE, 2):
                    replay([record(lambda ti=ti: proj0_tile(ti)) for ti in range(t2_, min(t2_ + 2, NTILE))])
                if "dbg_qT" in dbg:
                    dbg_qT = dscr("dbg_qT", [64, 8, NT], BF16)
                    k.dma("sp", dbg_qT, qT[:], reads=[B_qT])
                k.barrier()


            for st in phase("l0att"):
                SC = 0.125
                maskL = sb(st, "maskL_sb", [128, 128]); maskR = sb(st, "maskR_sb", [128, 128]); B_mask = Buf()
                k.dma("sp", maskL[:], maskL_in[:, :], writes=[B_mask])
                k.dma("sp", maskR[:], maskR_in[:, :], writes=[B_mask])
                sink_bc, B_sink = load_bc(st, "sink_bc", swa_sink[0:1, :], 8)
                sm = [sb(st, f"sm{i}", [128, 640]) for i in range(2)]; B_sm = [Buf(), Buf()]
                P = [sb(st, f"P{i}", [128, 640], BF16) for i in range(2)]; B_P = [Buf(), Buf()]
                PT = [sb(st, f"PT{i}", [128, 5, 128], BF16) for i in range(2)]; B_PT = [Buf(), Buf()]
                stat = [sb(st, f"stat{i}", [128, 8]) for i in range(2)]; B_stat = [Buf(), Buf()]
                att_bf = sb(st, "att_bf", [128, 512], BF16); B_att = Buf()
                ps_loc = [ps(st, f"ps_loc{i}", [128, 512]) for i in range(2)]; B_psl = [Buf(), Buf()]
                ps_ctx = [ps(st, f"ps_ctx{i}", [128, 512]) for i in range(2)]; B_psc = [Buf(), Buf()]
                pPT = ps(st, "pPT", [128, 8, 128], BF16); B_pPT = Buf()
                po = ps(st, "po", [128, 512]); B_po = Buf()
                pcat = ps(st, "pcat", [128, 8, 128], BF16); B_pcat = Buf()
                it = 0
                for ti in range(NTILE):
                    T0 = ti * 128
                    lat = ti < NLT
                    if lat:
                        j0 = max(0, ti - 1); j1 = min(NLT - 1, ti + 1)
                        nloc = (j1 - j0 + 1) * 128
                        blocks = list(range(j0, j1 + 1)) + [NLT, NLT + 1]
                    else:
                        nloc = 0
                        blocks = [NLT, NLT + 1]
                    n = nloc + 256
                    pend_B = None
                    for h in range(8):
                        i = it % 2; it += 1
                        kvh = h // 4
                        rec_ = []
                        orig_op_ = k.op
                        k.op = lambda *a, rec_=rec_, **kw: rec_.append((a, kw))
                        if lat:
                            k.op("pe", lambda e, i=i, h=h, kvh=kvh, j0=j0, nloc=nloc, T0=T0: e.matmul(
                                ps_loc[i][:, 0:nloc], lhsT=qT[:, h, T0:T0 + 128], rhs=kT[:, kvh, j0 * 128:j0 * 128 + nloc],
                                start=True, stop=True), reads=[B_qT, B_kT], writes=[B_psl[i]])
                        k.op("pe", lambda e, i=i, h=h, kvh=kvh, T0=T0: e.matmul(
                            ps_ctx[i][:, 0:256], lhsT=qT[:, h, T0:T0 + 128], rhs=kT[:, kvh, SEQ:NT],
                            start=True, stop=True), reads=[B_qT, B_kT], writes=[B_psc[i]])
                        if lat:
                            for bi, j in enumerate(range(j0, j1 + 1)):
                                sl = slice(bi * 128, (bi + 1) * 128)
                                if j == ti:
                                    k.op("act", lambda e, i=i, sl=sl: e.mul(out=sm[i][:, sl], in_=ps_loc[i][:, sl], mul=SC),
                                         reads=[B_psl[i]], writes=[B_sm[i]])
                                else:
                                    mk = maskL if j < ti else maskR
                                    k.op("dve", lambda e, i=i, sl=sl, mk=mk: e.scalar_tensor_tensor(
                                        out=sm[i][:, sl], in0=ps_loc[i][:, sl], scalar=SC, in1=mk[:], op0=ALU.mult, op1=ALU.add),
                                        reads=[B_psl[i], B_mask], writes=[B_sm[i]])
                        k.op("act", lambda e, i=i, nloc=nloc: e.mul(out=sm[i][:, nloc:nloc + 256], in_=ps_ctx[i][:, 0:256], mul=SC),
                             reads=[B_psc[i]], writes=[B_sm[i]])
                        sti = stat[i]
                        k.op("dve", lambda e, i=i, n=n, sti=sti: e.reduce_max(out=sti[:, 0:1], in_=sm[i][:, 0:n], axis=AX.X),
                             reads=[B_sm[i]], writes=[B_stat[i]])
                        k.op("dve", lambda e, sti=sti, h=h: e.tensor_tensor(out=sti[:, 1:2], in0=sti[:, 0:1], in1=sink_bc[:, h:h + 1], op=ALU.max),
                             reads=[B_stat[i], B_sink], writes=[B_stat[i]])
                        k.op("dve", lambda e, sti=sti: e.tensor_scalar(out=sti[:, 2:3], in0=sti[:, 1:2], scalar1=-1.0, scalar2=None, op0=ALU.mult),
                             reads=[B_stat[i]], writes=[B_stat[i]])
                        k.op("act", lambda e, i=i, n=n, sti=sti: e.activation(out=P[i][:, 0:n], in_=sm[i][:, 0:n], func=AF.Exp,
                                                                             bias=sti[:, 2:3], scale=1.0, accum_out=sti[:, 3:4]),
                             reads=[B_sm[i], B_stat[i]], writes=[B_P[i], B_stat[i]])
                        k.op("act", lambda e, sti=sti, h=h: e.activation(out=sti[:, 4:5], in_=sink_bc[:, h:h + 1], func=AF.Exp,
                                                                        bias=sti[:, 2:3], scale=1.0),
                             reads=[B_sink, B_stat[i]], writes=[B_stat[i]])
                        k.op("dve", lambda e, sti=sti: e.tensor_tensor(out=sti[:, 5:6], in0=sti[:, 3:4], in1=sti[:, 4:5], op=ALU.add),
                             reads=[B_stat[i]], writes=[B_stat[i]])
                        k.op("dve", lambda e, sti=sti: e.reciprocal(out=sti[:, 6:7], in_=sti[:, 5:6]),
                             reads=[B_stat[i]], writes=[B_stat[i]])
                        nb = n // 128
                        for b in range(nb):
                            k.op("pe", lambda e, i=i, b=b: e.transpose(out=pPT[:, b, :], in_=P[i][:, b * 128:(b + 1) * 128], identity=ident_b[:]),
                                 reads=[B_P[i], B_ident_b], writes=[B_pPT])
                        k.op("pool" if False else "dve", lambda e, i=i, nb=nb: e.tensor_copy(out=PT[i][:, 0:nb, :], in_=pPT[:, 0:nb, :]),
                             reads=[B_pPT], writes=[B_PT[i]])
                        for b in range(nb):
                            k.op("pe", lambda e, i=i, b=b, h=h, kvh=kvh, vb=blocks[b], nb=nb: e.matmul(
                                po[:, h * 64:(h + 1) * 64], lhsT=PT[i][:, b, :], rhs=v_all[:, vb, kvh * 64:(kvh + 1) * 64],
                                start=(b == 0), stop=(b == nb - 1)), reads=[B_PT[i], B_v], writes=[B_po])
                        k.op("dve", lambda e, h=h, sti=sti: e.tensor_scalar(out=att_bf[:, h * 64:(h + 1) * 64], in0=po[:, h * 64:(h + 1) * 64],
                                                                           scalar1=sti[:, 6:7], scalar2=None, op0=ALU.mult),
                             reads=[B_po, B_stat[i]], writes=[B_att])
                        k.op = orig_op_
                        split_ = None
                        seen_non_pe = False
                        for ix_, (a_, kw_) in enumerate(rec_):
                            if a_[0] != "pe":
                                seen_non_pe = True
                            elif seen_non_pe:
                                split_ = ix_
                                break
                        lead_ = 0
                        while rec_[lead_][0][0] == "pe":
                            lead_ += 1
                        for a_, kw_ in rec_[:lead_]:
                            k.op(*a_, **kw_)
                        if pend_B is not None:
                            for a_, kw_ in pend_B:
                                k.op(*a_, **kw_)
                        for a_, kw_ in rec_[lead_:split_]:
                            k.op(*a_, **kw_)
                        pend_B = rec_[split_:]
                    for a_, kw_ in pend_B:
                        k.op(*a_, **kw_)
                    for cb in range(4):
                        k.op("pe", lambda e, cb=cb: e.transpose(out=pcat[:, cb, :], in_=att_bf[:, cb * 128:(cb + 1) * 128], identity=ident_b[:]),
                             reads=[B_att, B_ident_b], writes=[B_pcat])
                    k.op("act", lambda e, T0=T0: e.copy(out=catT[:, 0:4, T0:T0 + 128], in_=pcat[:, 0:4, :]), reads=[B_pcat], writes=[B_catT])
                    if "dbg_att" in dbg:
                        if ti == 0:
                            dbg_att = dscr("dbg_att", [NT, 512], BF16)
                        k.dma("sp", dbg_att[T0:T0 + 128, :], att_bf[:], reads=[B_att])
                k.barrier()


            k.barrier()
            LA.close()
            for st in phase("l0ssm"):
                ssm_phase(st, catT, B_catT)
            for st in phase("l0ssmpost"):
                ssm_post(st, catT, B_catT)

            for st in phase("l0out"):
                outproj_ln1(st, 0, catT, B_catT, w_out0, NTILE)


        for st in phase("moe0"):
            moe_phase(st, 0, NTILE, x2_scr)


        layer1_mixer()
        for st in phase("moe1"):
            moe_phase(st, 1, NLT, out)

        k.barrier()
    return nc


_CONSTS = None


def _consts():
    global _CONSTS
    if _CONSTS is None:
        t = np.arange(SEQ)
        row = (t // 64).astype(np.float32)
        col = (t % 64).astype(np.float32)
        inv = (10000.0 ** (-np.arange(16, dtype=np.float32) / 16)).astype(np.float32)
        ar = row[:, None] * inv[None, :]
        ac = col[:, None] * inv[None, :]
        rope = np.concatenate([np.cos(ar), np.sin(ar), np.cos(ac), np.sin(ac)], 1).astype(np.float32)
        qi = np.arange(128)[:, None]; kj = np.arange(128)[None, :]
        mL = np.where(kj >= qi, 0.0, -30000.0).astype(np.float32)
        mR = np.where(kj <= qi, 0.0, -30000.0).astype(np.float32)
        _CONSTS = {"rope_cs": rope, "ident": np.eye(128, dtype=np.float32), "maskL": mL, "maskR": mR}
        selm = np.zeros((32, 32, 128), np.float32)
        for e_ in range(32):
            selm[e_, e_, :] = 1.0
        _CONSTS["sel"] = selm
        _CONSTS["kval"] = np.ascontiguousarray(np.broadcast_to(np.repeat(np.arange(-7, 9, dtype=np.float32), 64)[None, :], (64, 1024)))
        _CONSTS["mrow"] = np.ascontiguousarray(np.broadcast_to(np.arange(288, dtype=np.float32)[None, :], (64, 288)))
        jj = np.arange(128) // 16
        _CONSTS["maskF"] = (jj[None, :] >= jj[:, None]).astype(np.float32)
        _CONSTS["maskB"] = (jj[None, :] <= jj[:, None]).astype(np.float32)
    return _CONSTS


def make_in_maps(inputs, cores):
    f = lambda a: np.ascontiguousarray(np.asarray(a, dtype=np.float32))
    shared = {}
    for name in ("mod_w", "mod_b", "ln1_g", "ln1_b", "ln2_g", "ln2_b", "swa_sink",
                 "ssm_d", "ssm_glu_b", "dif_lam_q1", "dif_lam_k1", "dif_lam_q2", "dif_lam_k2",
                 "dif_subln_g", "moe_wg", "moe_bg", "moe_we", "moe_w1", "moe_w3", "moe_w2"):
        shared[name] = f(inputs[name])
    for name in ("swa_ssm_w_in", "swa_ssm_w_out", "ssm_a_re", "ssm_a_im", "ssm_log_step",
                 "ssm_b_re", "ssm_b_im", "ssm_c_re", "ssm_c_im", "ssm_glu_w", "dif_w_in", "dif_w_out"):
        shared[name] = f(inputs[name])[0]
    shared["moe_be"] = f(inputs["moe_be"]).reshape(2, 32)
    shared["c_ctx"] = f(inputs["c_ctx"]).reshape(1, D)
    shared.update(_consts())
    maps = []
    for b in cores:
        m = dict(shared)
        m["x"] = f(inputs["x"][b])
        m["ctx"] = f(inputs["ctx"][b])
        m["c"] = f(inputs["c"][b]).reshape(1, D)
        maps.append(m)
    return maps


def kernel(**inputs):
    nc = build()
    maps = make_in_maps(inputs, range(8))
    res = run_bass_kernel_spmd(nc, maps, core_ids=list(range(8)))
    return np.stack([r["out"] for r in res.results], 0).astype(np.float32)
```

```python
import math
from contextlib import ExitStack

import numpy as np
import concourse.bass as bass
import concourse.mybir as mybir
from concourse.bass_utils import run_bass_kernel_spmd

F32 = mybir.dt.float32
BF16 = mybir.dt.bfloat16
AF = mybir.ActivationFunctionType
ALU = mybir.AluOpType
AX = mybir.AxisListType

D = 1024
SEQ = 2048
CTX = 256
NT = SEQ + CTX
NTILE = NT // 128
NLT = SEQ // 128
ALPHA = 4 ** 0.25
LN_EPS = 1e-5


class Buf:
    __slots__ = ("w", "r")

    def __init__(self):
        self.w = None
        self.r = {}


class EngState:
    def __init__(self, name, eng, sem):
        self.name = name
        self.eng = eng
        self.sem = sem
        self.count = 0
        self.waited = {}
        self.slots = []
        self.slot_i = 0


class K:
    def __init__(self, nc, stack):
        self.nc = nc
        self.E = {}
        for name, eng in (("pe", nc.tensor), ("dve", nc.vector), ("act", nc.scalar),
                          ("pool", nc.gpsimd), ("sp", nc.sync)):
            sem = stack.enter_context(nc.semaphore("s_" + name))
            self.E[name] = EngState(name, eng, sem)
        self.semkey = {}
        for qn, n in (("sp", 12), ("pool", 12), ("act", 6)):
            for i in range(n):
                sem = stack.enter_context(nc.semaphore(f"d_{qn}{i}"))
                self.E[qn].slots.append([sem, 0])
        self.uid = 0

    def _key(self, sem):
        return id(sem)

    def _wait(self, E, deps, skip_self=False):
        best = {}
        for sem, val in deps:
            if skip_self and sem is E.sem:
                continue
            k = id(sem)
            if k not in best or best[k][1] < val:
                best[k] = (sem, val)
        for k, (sem, val) in best.items():
            if E.waited.get(k, 0) < val:
                E.eng.wait_ge(sem, val)
                E.waited[k] = val

    def _deps(self, reads, writes):
        deps = []
        for b in reads:
            if b.w is not None:
                deps.append(b.w)
        for b in writes:
            if b.w is not None:
                deps.append(b.w)
            deps.extend(b.r.values())
        return deps

    def _mark(self, tok, reads, writes):
        sem, val = tok
        for b in reads:
            b.r[id(sem)] = tok
        for b in writes:
            b.w = tok
            b.r = {}

    def op(self, en, fn, reads=(), writes=()):
        E = self.E[en]
        self._wait(E, self._deps(reads, writes), skip_self=(en == "pe"))
        ins = fn(E.eng)
        E.count += 1
        ins.then_inc(E.sem, 1)
        tok = (E.sem, E.count)
        self._mark(tok, reads, writes)
        return tok

    def dma(self, qn, out, in_, reads=(), writes=(), **kw):
        E = self.E[qn]
        self._wait(E, self._deps(reads, writes))
        slot = E.slots[E.slot_i % len(E.slots)]
        E.slot_i += 1
        if slot[1] > 0:
            self._wait(E, [(slot[0], slot[1] * 16)])
        ins = E.eng.dma_start(out=out, in_=in_, **kw)
        slot[1] += 1
        ins.then_inc(slot[0], 16)
        tok = (slot[0], slot[1] * 16)
        self._mark(tok, reads, writes)
        return tok

    def all_tokens(self):
        toks = []
        for E in self.E.values():
            if E.count:
                toks.append((E.sem, E.count))
            for sem, c in E.slots:
                if c:
                    toks.append((sem, c * 16))
        return toks

    def barrier(self):
        toks = self.all_tokens()
        for E in self.E.values():
            self._wait(E, toks, skip_self=False)


def build(dbg=(), inject=(), phases=None):
    nc = bass.Bass("TRN2", target_bir_lowering=False)
    dbg = set(dbg)
    inject = set(inject)
    ALLP = {"mod", "l0proj", "l0att", "l0ssm", "l0ssmpost", "l0out", "moe0", "l1proj", "l1att", "moe1"}
    phases = ALLP if phases is None else set(phases)

    def din(name, shape):
        return nc.dram_tensor(name, list(shape), F32, kind="ExternalInput").ap()

    def dscr(name, shape, dt=F32):
        kind = "ExternalOutput" if name in dbg else ("ExternalInput" if name in inject else "Internal")
        return nc.dram_tensor(name, list(shape), dt, kind=kind).ap()

    x_in = din("x", [SEQ, D])
    ctx_in = din("ctx", [CTX, D])
    c_in = din("c", [1, D])
    cc_in = din("c_ctx", [1, D])
    mod_w = din("mod_w", [2, D, 6 * D])
    mod_b = din("mod_b", [2, 6 * D])
    ln1_g = din("ln1_g", [2, D]); ln1_b = din("ln1_b", [2, D])
    ln2_g = din("ln2_g", [2, D]); ln2_b = din("ln2_b", [2, D])
    w_in0 = din("swa_ssm_w_in", [D, 1280])
    w_out0 = din("swa_ssm_w_out", [D, D])
    swa_sink = din("swa_sink", [1, 8])
    a_re = din("ssm_a_re", [2, 32, 64]); a_im = din("ssm_a_im", [2, 32, 64])
    log_step = din("ssm_log_step", [2, 32])
    b_re = din("ssm_b_re", [2, 32, 64, 16]); b_im = din("ssm_b_im", [2, 32, 64, 16])
    c_re = din("ssm_c_re", [2, 32, 16, 64]); c_im = din("ssm_c_im", [2, 32, 16, 64])
    ssm_d = din("ssm_d", [1, 512])
    glu_w = din("ssm_glu_w", [512, 512]); glu_b = din("ssm_glu_b", [1, 512])
    dif_w_in = din("dif_w_in", [D, 3072]); dif_w_out = din("dif_w_out", [D, D])
    lam_q1 = din("dif_lam_q1", [1, 64]); lam_k1 = din("dif_lam_k1", [1, 64])
    lam_q2 = din("dif_lam_q2", [1, 64]); lam_k2 = din("dif_lam_k2", [1, 64])
    subln_g = din("dif_subln_g", [1, 128])
    moe_wg = din("moe_wg", [2, D, 4]); moe_bg = din("moe_bg", [2, 4])
    moe_we = din("moe_we", [2, 4, D, 8]); moe_be = din("moe_be", [2, 32])
    moe_w1 = din("moe_w1", [2, 32, D, 256]); moe_w3 = din("moe_w3", [2, 32, D, 256])
    moe_w2 = din("moe_w2", [2, 32, 256, D])
    rope_cs = din("rope_cs", [SEQ, 64])
    ident_in = din("ident", [128, 128])
    sel_in = din("sel", [32, 32, 128])
    kval_in = din("kval", [64, 1024]); mrow_in = din("mrow", [64, 288])
    maskF_in = din("maskF", [128, 128]); maskB_in = din("maskB", [128, 128])
    maskL_in = din("maskL", [128, 128]); maskR_in = din("maskR", [128, 128])
    out = nc.dram_tensor("out", [SEQ, D], F32, kind="ExternalOutput").ap()

    modrow = dscr("modrow", [2, 2, 6 * D])

    with ExitStack() as gs:
        k = K(nc, gs)

        def sb(st, name, shape, dt=F32):
            k.uid += 1
            return st.enter_context(nc.sbuf_tensor(f"sb{k.uid}_{name}", list(shape), dt))

        def ps(st, name, shape, dt=F32):
            k.uid += 1
            return st.enter_context(nc.psum_tensor(f"ps{k.uid}_{name}", list(shape), dt))

        def phase(name):
            if name in phases:
                with ExitStack() as st_:
                    yield st_

        def record(fn):
            rec = []
            oo, od = k.op, k.dma
            k.op = lambda *a, **kw: rec.append(("op", a, kw))
            k.dma = lambda *a, **kw: rec.append(("dma", a, kw))
            try:
                fn()
            finally:
                k.op, k.dma = oo, od
            return rec

        def replay(recs):
            for ix in range(max(len(r_) for r_ in recs)):
                for r_ in recs:
                    if ix < len(r_):
                        kind, a_, kw_ = r_[ix]
                        (k.op if kind == "op" else k.dma)(*a_, **kw_)

        ident_f = sb(gs, "ident_f", [128, 128]); B_ident_f = Buf()
        ident_b = sb(gs, "ident_b", [128, 128], BF16); B_ident_b = Buf()
        k.dma("sp", ident_f[:], ident_in[:, :], writes=[B_ident_f])
        k.op("dve", lambda e: e.tensor_copy(out=ident_b[:], in_=ident_f[:]),
             reads=[B_ident_f], writes=[B_ident_b])

        for st in phase("mod"):
            cT = sb(st, "cT", [128, 8, 2]); B_cT = Buf()
            with nc.allow_non_contiguous_dma(reason="tiny column loads"):
                k.dma("sp", cT[:, :, 0], c_in[0, :].rearrange("(k p) -> p k", p=128), writes=[B_cT])
                k.dma("sp", cT[:, :, 1], cc_in[0, :].rearrange("(k p) -> p k", p=128), writes=[B_cT])
            sT = sb(st, "sT", [128, 8, 2]); B_sT = Buf()
            k.op("act", lambda e: e.activation(out=sT[:], in_=cT[:], func=AF.Silu),
                 reads=[B_cT], writes=[B_sT])
            wt = [sb(st, f"modw{i}", [128, 8, 512]) for i in range(2)]
            B_wt = [Buf(), Buf()]
            mb = sb(st, "modb", [2, 6 * D]); B_mb = Buf()
            mrow = sb(st, "mrow", [2, 6 * D]); B_mrow = Buf()
            pm = [ps(st, f"pmod{i}", [2, 512]) for i in range(2)]
            B_pm = [Buf(), Buf()]
            it = 0
            for l in range(2):
                k.dma("sp", mb[0:1, :], mod_b[l:l + 1, :], writes=[B_mb])
                k.dma("sp", mb[1:2, :], mod_b[l:l + 1, :], writes=[B_mb])
                for cb in range(12):
                    i = it % 2
                    it += 1
                    k.dma("sp" if cb % 2 == 0 else "act", wt[i][:],
                          mod_w[l, :, cb * 512:(cb + 1) * 512].rearrange("(k p) n -> p k n", p=128),
                          writes=[B_wt[i]])
                    for kc in range(8):
                        k.op("pe", lambda e, kc=kc, i=i: e.matmul(
                            pm[i][:], lhsT=sT[:, kc, :], rhs=wt[i][:, kc, :],
                            start=(kc == 0), stop=(kc == 7)),
                            reads=[B_sT, B_wt[i]], writes=[B_pm[i]])
                    k.op("dve", lambda e, i=i, cb=cb: e.tensor_tensor(
                        out=mrow[:, cb * 512:(cb + 1) * 512], in0=pm[i][:],
                        in1=mb[:, cb * 512:(cb + 1) * 512], op=ALU.add),
                        reads=[B_pm[i], B_mb], writes=[B_mrow])
                k.dma("sp", modrow[l], mrow[:], reads=[B_mrow], writes=[])
            k.barrier()


        def load_bc(st, name, src_row_ap, n, q="sp"):
            t = sb(st, name, [128, n]); B = Buf()
            k.dma(q, t[:], src_row_ap.partition_broadcast(128), writes=[B])
            return t, B

        def mod_bc(st, name, l, r, chunk, plus1=False):
            t, B = load_bc(st, name, modrow[l, r:r + 1, chunk * D:(chunk + 1) * D], D)
            if plus1:
                k.op("pool", lambda e: e.tensor_scalar(out=t[:], in0=t[:], scalar1=1.0, scalar2=None,
                                                       op0=ALU.add), reads=[B], writes=[B])
            return t, B

        u_scr = dscr("u_scr", [NT, 512])
        y_scr = dscr("y_scr", [NT, 512], BF16)
        x1_scr = dscr("x1_scr", [NT, D])
        x2_scr = dscr("x2_scr", [NT, D])
        dbg_q = dscr("dbg_q", [NT, 1280])

        def src_rows(ti, l):
            if l == 0:
                return x_in[ti * 128:(ti + 1) * 128, :] if ti < NLT else ctx_in[(ti - NLT) * 128:(ti - NLT + 1) * 128, :]
            return x2_scr[ti * 128:(ti + 1) * 128, :]

        def rope_apply(st_bufs, src_ps, B_src, dst, B_dst, nh, rt, B_rt, tmp1, tmp2, B_tmp):
            S = src_ps.rearrange("p (h a b f) -> p h a b f", h=nh, a=2, b=2, f=16)
            O = dst.rearrange("p (h a b f) -> p h a b f", h=nh, a=2, b=2, f=16)
            T1 = tmp1.rearrange("p (h a b f) -> p h a b f", h=nh, a=2, b=2, f=16)
            T2 = tmp2.rearrange("p (h a b f) -> p h a b f", h=nh, a=2, b=2, f=16)
            for a in range(2):
                cos = rt[:, a * 32:a * 32 + 16].rearrange("p (x y f) -> p x y f", x=1, y=1).to_broadcast([128, nh, 2, 16])
                sin = rt[:, a * 32 + 16:a * 32 + 32].rearrange("p (x y f) -> p x y f", x=1, y=1).to_broadcast([128, nh, 2, 16])
                k.op("dve", lambda e, a=a, cos=cos: e.tensor_tensor(out=T1[:, :, a], in0=S[:, :, a], in1=cos, op=ALU.mult),
                     reads=[B_src, B_rt], writes=[B_tmp])
                k.op("dve", lambda e, a=a, sin=sin: e.tensor_tensor(out=T2[:, :, a], in0=S[:, :, a, ::-1, :], in1=sin, op=ALU.mult),
                     reads=[B_src, B_rt], writes=[B_tmp])
                k.op("dve", lambda e, a=a: e.tensor_tensor(out=O[:, :, a, 0, :], in0=T1[:, :, a, 0, :], in1=T2[:, :, a, 0, :], op=ALU.subtract),
                     reads=[B_tmp], writes=[B_dst])
                k.op("dve", lambda e, a=a: e.tensor_tensor(out=O[:, :, a, 1, :], in0=T1[:, :, a, 1, :], in1=T2[:, :, a, 1, :], op=ALU.add),
                     reads=[B_tmp], writes=[B_dst])


        def ln_epilogue(wk, y_parts, B_y, xo, B_xo, g_t, lng_t, lnb_t, dst_rows):
            tmp, B_tmp, z, B_z, stt, B_stt, o, B_o = wk
            for hf in range(2):
                sl = slice(hf * 512, (hf + 1) * 512)
                k.op("dve", lambda e, hf=hf, sl=sl: e.tensor_tensor(out=tmp[:, sl], in0=y_parts[hf], in1=g_t[0][:, sl], op=ALU.mult),
                     reads=[B_y[hf], g_t[1]], writes=[B_tmp])
            k.op("dve", lambda e: e.scalar_tensor_tensor(out=z[:], in0=xo[:], scalar=ALPHA, in1=tmp[:], op0=ALU.mult, op1=ALU.add),
                 reads=[B_xo, B_tmp], writes=[B_z])
            for hf in range(2):
                k.op("dve", lambda e, hf=hf: e.bn_stats(out=stt[:, hf * 6:(hf + 1) * 6], in_=z[:, hf * 512:(hf + 1) * 512]),
                     reads=[B_z], writes=[B_stt])
            k.op("dve", lambda e: e.bn_aggr(out=stt[:, 12:14], in_=stt[:, 0:12]), reads=[B_stt], writes=[B_stt])
            k.op("dve", lambda e: e.tensor_scalar(out=stt[:, 15:16], in0=stt[:, 13:14], scalar1=LN_EPS, scalar2=None, op0=ALU.add),
                 reads=[B_stt], writes=[B_stt])
            k.op("act", lambda e: e.sqrt(out=stt[:, 15:16], in_=stt[:, 15:16]), reads=[B_stt], writes=[B_stt])
            k.op("dve", lambda e: e.reciprocal(out=stt[:, 14:15], in_=stt[:, 15:16]), reads=[B_stt], writes=[B_stt])
            k.op("dve", lambda e: e.tensor_scalar(out=tmp[:], in0=z[:], scalar1=stt[:, 12:13], scalar2=stt[:, 14:15], op0=ALU.subtract, op1=ALU.mult),
                 reads=[B_z, B_stt], writes=[B_tmp])
            k.op("pool", lambda e: e.tensor_tensor(out=o[:], in0=tmp[:], in1=lng_t[0][:], op=ALU.mult),
                 reads=[B_tmp, lng_t[1]], writes=[B_o])
            k.op("pool", lambda e: e.tensor_tensor(out=o[:], in0=o[:], in1=lnb_t[0][:], op=ALU.add),
                 reads=[B_o, lnb_t[1]], writes=[B_o])
            k.dma("sp", dst_rows, o[:], reads=[B_o])

        def ln_work(st, pfx):
            tmp = sb(st, pfx + "_tmp", [128, D]); z = sb(st, pfx + "_z", [128, D])
            stt = sb(st, pfx + "_stt", [128, 16]); o = sb(st, pfx + "_o", [128, D])
            return (tmp, Buf(), z, Buf(), stt, Buf(), o, Buf())

        def outproj_ln1(st, l, catT_, B_catT_, w_out_dram, ntiles):
            w_bf = sb(st, "w_out_bf", [128, 8, D], BF16); B_w = Buf()
            for kc in range(8):
                k.dma("pool", w_bf[:, kc, :], w_out_dram[kc * 128:(kc + 1) * 128, :], writes=[B_w])
            g1 = [mod_bc(st, f"g1_{r}", l, r, 2) for r in range(2)]
            lng = load_bc(st, "ln1g", ln1_g[l:l + 1, :], D)
            lnb = load_bc(st, "ln1b", ln1_b[l:l + 1, :], D)
            wks = [ln_work(st, "e1a"), ln_work(st, "e1b")]
            xo = [sb(st, f"xo{i}", [128, D]) for i in range(2)]; B_xo = [Buf(), Buf()]
            py = [ps(st, f"py{i}", [128, 512]) for i in range(4)]; B_py = [Buf() for _ in range(4)]

            def out_tile(ti):
                i = ti % 2
                wk = wks[i]
                r = 0 if ti < NLT else 1
                T0 = ti * 128
                k.dma("sp", xo[i][:], src_rows(ti, l), writes=[B_xo[i]])
                for hf in range(2):
                    pi = i * 2 + hf
                    for kc in range(8):
                        k.op("pe", lambda e, kc=kc, hf=hf, pi=pi, T0=T0: e.matmul(
                            py[pi][:], lhsT=catT_[:, kc, T0:T0 + 128], rhs=w_bf[:, kc, hf * 512:(hf + 1) * 512],
                            start=(kc == 0), stop=(kc == 7)), reads=[B_catT_, B_w], writes=[B_py[pi]])
                ln_epilogue(wk, [py[i * 2][:], py[i * 2 + 1][:]], [B_py[i * 2], B_py[i * 2 + 1]], xo[i], B_xo[i],
                            g1[r], lng, lnb, x1_scr[T0:T0 + 128, :])

            for t2 in range(0, ntiles, 2):
                replay([record(lambda ti=ti: out_tile(ti)) for ti in range(t2, min(t2 + 2, ntiles))])
            k.barrier()

        def moe_phase(st, l, ntiles, dst):
            ntok = ntiles * 128
            h2T = sb(st, "h2T", [128, 8, ntok], BF16); B_h2T = Buf()
            gateT = sb(st, "gateT", [32, ntok], BF16); B_gateT = Buf()
            f_acc = sb(st, "f_acc", [128, ntiles, D]); B_facc = [Buf() for _ in range(ntiles)]
            sel = sb(st, "sel", [32, 32, 128], BF16); B_sel = Buf()
            k.dma("pool", sel[:], sel_in[:, :, :], writes=[B_sel])
            with ExitStack() as s1:
                Wr = sb(s1, "Wr", [128, 8, 36]); B_Wr = Buf()
                with nc.allow_non_contiguous_dma(reason="small router weights"):
                    k.dma("sp", Wr[:, :, 0:4], moe_wg[l].rearrange("(k p) n -> p k n", p=128), writes=[B_Wr])
                    for g in range(4):
                        k.dma("sp", Wr[:, :, 4 + g * 8:12 + g * 8], moe_we[l, g].rearrange("(k p) n -> p k n", p=128), writes=[B_Wr])
                Whi = sb(s1, "Whi", [128, 8, 36], BF16); Wlo = sb(s1, "Wlo", [128, 8, 36], BF16); B_Wsp = Buf()
                k.op("dve", lambda e: e.tensor_copy(out=Whi[:], in_=Wr[:]), reads=[B_Wr], writes=[B_Wsp])
                k.op("dve", lambda e: e.tensor_tensor(out=Wlo[:], in0=Wr[:], in1=Whi[:], op=ALU.subtract), reads=[B_Wr, B_Wsp], writes=[B_Wsp])
                rb = sb(s1, "rb", [128, 36]); B_rb = Buf()
                k.dma("sp", rb[:, 0:4], moe_bg[l:l + 1, :].partition_broadcast(128), writes=[B_rb])
                k.dma("sp", rb[:, 4:36], moe_be[l:l + 1, :].partition_broadcast(128), writes=[B_rb])
                sc2p = [mod_bc(s1, f"sc2p_{r}", l, r, 4, plus1=True) for r in range(2)]
                sh2 = [mod_bc(s1, f"sh2_{r}", l, r, 3) for r in range(2)]
                xt = [sb(s1, f"mx{i}", [128, D]) for i in range(2)]; B_xt = [Buf(), Buf()]
                hf32 = [sb(s1, f"mh{c_}", [128, D]) for c_ in range(2)]; B_h = [Buf(), Buf()]
                hhi = [sb(s1, f"hhi{c_}", [128, D], BF16) for c_ in range(2)]; hlo = [sb(s1, f"hlo{c_}", [128, D], BF16) for c_ in range(2)]; B_hs = [Buf(), Buf()]
                hloT = [sb(s1, f"hloT{c_}", [128, 8, 128], BF16) for c_ in range(2)]; B_hloT = [Buf(), Buf()]
                pTh = [ps(s1, f"mpTh{c_}", [128, 8, 128], BF16) for c_ in range(2)]; B_pTh = [Buf(), Buf()]
                pTl = [ps(s1, f"mpTl{c_}", [128, 8, 128], BF16) for c_ in range(2)]; B_pTl = [Buf(), Buf()]
                pr = [ps(s1, f"mpr{c_}", [128, 512]) for c_ in range(2)]; B_pr = [Buf(), Buf()]
                pg = [ps(s1, f"mpg{c_}", [32, 1024], BF16) for c_ in range(2)]; B_pg = [Buf(), Buf()]
                lg = [sb(s1, f"lg{c_}", [128, 36]) for c_ in range(2)]; B_lg = [Buf(), Buf()]
                sm = [sb(s1, f"rsm{c_}", [128, 160]) for c_ in range(2)]; B_sm = [Buf(), Buf()]
                gates = [sb(s1, f"gates{c_}", [128, 32]) for c_ in range(2)]; B_gates = [Buf(), Buf()]
                gates_bf = [sb(s1, f"gates_bf{c_}", [128, 32], BF16) for c_ in range(2)]; B_gbf = [Buf(), Buf()]
                def router_tile(ti):
                    i = ti % 2
                    ci = ti % 2
                    r = 0 if ti < NLT else 1
                    T0 = ti * 128
                    k.dma("sp", xt[i][:], x1_scr[T0:T0 + 128, :], writes=[B_xt[i]])
                    k.op("dve", lambda e, i=i, r=r: e.tensor_tensor(out=hf32[ci][:], in0=xt[i][:], in1=sc2p[r][0][:], op=ALU.mult),
                         reads=[B_xt[i], sc2p[r][1]], writes=[B_h[ci]])
                    k.op("pool", lambda e, r=r: e.tensor_tensor(out=hf32[ci][:], in0=hf32[ci][:], in1=sh2[r][0][:], op=ALU.add),
                         reads=[B_h[ci], sh2[r][1]], writes=[B_h[ci]])
                    k.op("pool", lambda e: e.tensor_copy(out=hhi[ci][:], in_=hf32[ci][:]), reads=[B_h[ci]], writes=[B_hs[ci]])
                    k.op("dve", lambda e: e.tensor_tensor(out=hlo[ci][:], in0=hf32[ci][:], in1=hhi[ci][:], op=ALU.subtract), reads=[B_h[ci], B_hs[ci]], writes=[B_hs[ci]])
                    for kc in range(8):
                        k.op("pe", lambda e, kc=kc: e.transpose(out=pTh[ci][:, kc, :], in_=hhi[ci][:, kc * 128:(kc + 1) * 128], identity=ident_b[:]),
                             reads=[B_hs[ci], B_ident_b], writes=[B_pTh[ci]])
                    for kc in range(8):
                        k.op("pe", lambda e, kc=kc: e.transpose(out=pTl[ci][:, kc, :], in_=hlo[ci][:, kc * 128:(kc + 1) * 128], identity=ident_b[:]),
                             reads=[B_hs[ci], B_ident_b], writes=[B_pTl[ci]])
                    k.op("act", lambda e, T0=T0: e.copy(out=h2T[:, :, T0:T0 + 128], in_=pTh[ci][:]), reads=[B_pTh[ci]], writes=[B_h2T])
                    k.op("dve", lambda e: e.tensor_copy(out=hloT[ci][:], in_=pTl[ci][:]), reads=[B_pTl[ci]], writes=[B_hloT[ci]])
                    n_mm = 24
                    j = 0
                    for (A, BA, W) in ((None, B_h2T, Whi), (hloT[ci], B_hloT[ci], Whi), (None, B_h2T, Wlo)):
                        for kc in range(8):
                            lhs = h2T[:, kc, T0:T0 + 128] if A is None else A[:, kc, :]
                            k.op("pe", lambda e, lhs=lhs, W=W, kc=kc, j=j: e.matmul(pr[ci][:, 0:36], lhsT=lhs, rhs=W[:, kc, :], start=(j == 0), stop=(j == 23)),
                                 reads=[BA, B_Wsp], writes=[B_pr[ci]])
                            j += 1
                    R = [B_lg[ci], B_sm[ci]]
                    def dv(fn, reads=R, writes=(B_sm[ci],)):
                        k.op("dve", fn, reads=list(reads), writes=list(writes))
                    k.op("dve", lambda e: e.tensor_tensor(out=lg[ci][:], in0=pr[ci][:, 0:36], in1=rb[:], op=ALU.add), reads=[B_pr[ci], B_rb], writes=[B_lg[ci]])
                    dv(lambda e: e.reduce_max(out=sm[ci][:, 0:1], in_=lg[ci][:, 0:4], axis=AX.X))
                    dv(lambda e: e.tensor_scalar(out=sm[ci][:, 1:2], in0=sm[ci][:, 0:1], scalar1=-1.0, scalar2=None, op0=ALU.mult))
                    k.op("act", lambda e: e.activation(out=sm[ci][:, 56:60], in_=lg[ci][:, 0:4], func=AF.Exp, bias=sm[ci][:, 1:2], scale=1.0, accum_out=sm[ci][:, 2:3]),
                         reads=R, writes=[B_sm[ci]])
                    dv(lambda e: e.reciprocal(out=sm[ci][:, 3:4], in_=sm[ci][:, 2:3]))
                    dv(lambda e: e.tensor_scalar(out=sm[ci][:, 4:8], in0=lg[ci][:, 0:4], scalar1=sm[ci][:, 0:1], scalar2=None, op0=ALU.is_equal))
                    le = lg[ci][:, 4:36].rearrange("p (g e) -> p g e", g=4)
                    tmp48 = sm[ci][:, 64:96].rearrange("p (g e) -> p g e", g=4)
                    ohb = sm[ci][:, 4:8].rearrange("p (g x) -> p g x", x=1).to_broadcast([128, 4, 8])
                    dv(lambda e: e.tensor_tensor(out=tmp48, in0=le, in1=ohb, op=ALU.mult))
                    dv(lambda e: e.tensor_reduce(out=sm[ci][:, 8:16], in_=sm[ci][:, 64:96].rearrange("p (g e) -> p e g", g=4), axis=AX.X, op=ALU.add))
                    dv(lambda e: e.reduce_max(out=sm[ci][:, 16:17], in_=sm[ci][:, 8:16], axis=AX.X))
                    dv(lambda e: e.tensor_scalar(out=sm[ci][:, 24:32], in0=sm[ci][:, 8:16], scalar1=sm[ci][:, 16:17], scalar2=None, op0=ALU.is_equal))
                    dv(lambda e: e.scalar_tensor_tensor(out=sm[ci][:, 32:40], in0=sm[ci][:, 24:32], scalar=-1e30, in1=sm[ci][:, 8:16], op0=ALU.mult, op1=ALU.add))
                    dv(lambda e: e.reduce_max(out=sm[ci][:, 17:18], in_=sm[ci][:, 32:40], axis=AX.X))
                    dv(lambda e: e.tensor_scalar(out=sm[ci][:, 40:48], in0=sm[ci][:, 32:40], scalar1=sm[ci][:, 17:18], scalar2=None, op0=ALU.is_equal))
                    dv(lambda e: e.tensor_tensor(out=sm[ci][:, 18:19], in0=sm[ci][:, 17:18], in1=sm[ci][:, 16:17], op=ALU.subtract))
                    k.op("act", lambda e: e.activation(out=sm[ci][:, 19:20], in_=sm[ci][:, 18:19], func=AF.Exp), reads=R, writes=[B_sm[ci]])
                    dv(lambda e: e.tensor_scalar(out=sm[ci][:, 20:21], in0=sm[ci][:, 19:20], scalar1=1.0, scalar2=None, op0=ALU.add))
                    dv(lambda e: e.reciprocal(out=sm[ci][:, 20:21], in_=sm[ci][:, 20:21]))
                    dv(lambda e: e.tensor_tensor(out=sm[ci][:, 21:22], in0=sm[ci][:, 19:20], in1=sm[ci][:, 20:21], op=ALU.mult))
                    dv(lambda e: e.tensor_tensor(out=sm[ci][:, 22:23], in0=sm[ci][:, 20:21], in1=sm[ci][:, 3:4], op=ALU.mult))
                    dv(lambda e: e.tensor_tensor(out=sm[ci][:, 23:24], in0=sm[ci][:, 21:22], in1=sm[ci][:, 3:4], op=ALU.mult))
                    dv(lambda e: e.tensor_scalar(out=sm[ci][:, 48:56], in0=sm[ci][:, 24:32], scalar1=sm[ci][:, 22:23], scalar2=None, op0=ALU.mult))
                    dv(lambda e: e.scalar_tensor_tensor(out=sm[ci][:, 48:56], in0=sm[ci][:, 40:48], scalar=sm[ci][:, 23:24], in1=sm[ci][:, 48:56], op0=ALU.mult, op1=ALU.add))
                    geb = sm[ci][:, 48:56].rearrange("p (x e) -> p x e", x=1).to_broadcast([128, 4, 8])
                    k.op("dve", lambda e: e.tensor_tensor(out=gates[ci][:].rearrange("p (g e) -> p g e", g=4), in0=ohb, in1=geb, op=ALU.mult),
                         reads=R, writes=[B_gates[ci]])
                    k.op("dve", lambda e: e.tensor_copy(out=gates_bf[ci][:], in_=gates[ci][:]), reads=[B_gates[ci]], writes=[B_gbf[ci]])
                    k.op("pe", lambda e: e.transpose(out=pg[ci][:, 0:128], in_=gates_bf[ci][:], identity=ident_b[:]),
                         reads=[B_gbf[ci], B_ident_b], writes=[B_pg[ci]])
                    k.op("act", lambda e, T0=T0: e.copy(out=gateT[:, T0:T0 + 128], in_=pg[ci][:, 0:128]), reads=[B_pg[ci]], writes=[B_gateT])
                    if "dbg_gates" in dbg:
                        if ti == 0:
                            dbg_gates = dscr("dbg_gates", [NT, 32])
                        k.dma("sp", dbg_gates[T0:T0 + 128, :], gates[ci][:], reads=[B_gates[ci]])

                recs_pair = []
                for ti in range(ntiles):
                    rec_r = []
                    orig_op_r = k.op
                    k.op = lambda *a, rec_r=rec_r, **kw: rec_r.append((a, kw))
                    router_tile(ti)
                    k.op = orig_op_r
                    recs_pair.append(rec_r)
                    if len(recs_pair) == 2 or ti == ntiles - 1:
                        for ix_ in range(max(len(r_) for r_ in recs_pair)):
                            for r_ in recs_pair:
                                if ix_ < len(r_):
                                    a_, kw_ = r_[ix_]
                                    k.op(*a_, **kw_)
                        recs_pair = []
                k.barrier()
            if "stop_router" in dbg:
                return
            with ExitStack() as s2:
                w13 = [sb(s2, f"w13_{i}", [128, 8, 512], BF16) for i in range(2)]; B_w13 = [Buf(), Buf()]
                w2 = [sb(s2, f"w2_{i}", [128, 2, D], BF16) for i in range(2)]; B_w2 = [Buf(), Buf()]
                hh = [sb(s2, f"hh{i}", [128, 2, ntok], BF16) for i in range(2)]; B_hh = [Buf(), Buf()]
                stg13 = sb(s2, "stg13", [128, 8, 512]); B_stg13 = Buf()
                stg2 = sb(s2, "stg2", [128, 2, D]); B_stg2 = Buf()
                s1t = [sb(s2, f"s1t{i}", [128, 512]) for i in range(2)]; B_s1t = [Buf(), Buf()]
                t3 = [sb(s2, f"t3{i}", [128, 512]) for i in range(2)]; B_t3 = [Buf(), Buf()]
                ph1 = [ps(s2, f"ph1_{i}", [128, 512]) for i in range(2)]; B_ph1 = [Buf(), Buf()]
                ph3 = [ps(s2, f"ph3_{i}", [128, 512]) for i in range(2)]; B_ph3 = [Buf(), Buf()]
                pgbs = [ps(s2, f"pgb{i}", [128, 512]) for i in range(2)]; B_pgbs = [Buf(), Buf()]
                ibk = 0
                pf = [ps(s2, f"pf{i}", [128, 512]) for i in range(2)]; B_pf = [Buf(), Buf()]
                blocks = [(b0, min(512, ntok - b0)) for b0 in range(0, ntok, 512)]
                it = 0; itf = 0

                def w_stage13(ee):
                    k.dma("sp", stg13[:, :, 0:256], moe_w1[l, ee].rearrange("(k p) f -> p k f", p=128), writes=[B_stg13])
                    k.dma("sp", stg13[:, :, 256:512], moe_w3[l, ee].rearrange("(k p) f -> p k f", p=128), writes=[B_stg13])

                def w_stage2(ee):
                    k.dma("sp", stg2[:], moe_w2[l, ee].rearrange("(c p) d -> p c d", p=128), writes=[B_stg2])

                def w_cast13(ee):
                    wj = ee % 2
                    k.op("pool", lambda e: e.tensor_copy(out=w13[wj][:], in_=stg13[:]), reads=[B_stg13], writes=[B_w13[wj]])

                def w_cast2(ee):
                    wj = ee % 2
                    k.op("pool", lambda e: e.tensor_copy(out=w2[wj][:], in_=stg2[:]), reads=[B_stg2], writes=[B_w2[wj]])

                def w_tail(e_):
                    if e_ + 1 < 32:
                        w_cast2(e_ + 1)
                    if e_ + 2 < 32:
                        w_stage2(e_ + 2)
                for e_ in range(32):
                    wi = e_ % 2
                    if e_ == 0:
                        w_stage13(0); w_stage2(0); w_cast13(0); w_cast2(0); w_stage13(1); w_stage2(1)
                    if e_ + 1 < 32:
                        w_cast13(e_ + 1)
                    if e_ + 2 < 32:
                        w_stage13(e_ + 2)
                    for (b0, bn) in blocks:
                        pgb = pgbs[ibk % 2]; B_pgb = B_pgbs[ibk % 2]; ibk += 1
                        k.op("pe", lambda e, e_=e_, b0=b0, bn=bn, pgb=pgb: e.matmul(pgb[:, 0:bn], lhsT=sel[:, e_, :], rhs=gateT[:, b0:b0 + bn], start=True, stop=True),
                             reads=[B_sel, B_gateT], writes=[B_pgb])
                        for fc in range(2):
                            i = it % 2; it += 1
                            for kc in range(8):
                                k.op("pe", lambda e, kc=kc, fc=fc, i=i, wi=wi, b0=b0, bn=bn: e.matmul(
                                    ph1[i][:, 0:bn], lhsT=w13[wi][:, kc, fc * 128:(fc + 1) * 128], rhs=h2T[:, kc, b0:b0 + bn],
                                    start=(kc == 0), stop=(kc == 7)), reads=[B_w13[wi], B_h2T], writes=[B_ph1[i]])
                            for kc in range(8):
                                k.op("pe", lambda e, kc=kc, fc=fc, i=i, wi=wi, b0=b0, bn=bn: e.matmul(
                                    ph3[i][:, 0:bn], lhsT=w13[wi][:, kc, 256 + fc * 128:256 + (fc + 1) * 128], rhs=h2T[:, kc, b0:b0 + bn],
                                    start=(kc == 0), stop=(kc == 7)), reads=[B_w13[wi], B_h2T], writes=[B_ph3[i]])
                            k.op("act", lambda e, i=i, bn=bn: e.activation(out=s1t[i][:, 0:bn], in_=ph1[i][:, 0:bn], func=AF.Silu),
                                 reads=[B_ph1[i]], writes=[B_s1t[i]])
                            k.op("dve", lambda e, i=i, bn=bn: e.tensor_tensor(out=t3[i][:, 0:bn], in0=s1t[i][:, 0:bn], in1=ph3[i][:, 0:bn], op=ALU.mult),
                                 reads=[B_s1t[i], B_ph3[i]], writes=[B_t3[i]])
                            k.op("dve", lambda e, i=i, bn=bn, fc=fc, wi=wi, b0=b0, pgb=pgb: e.tensor_tensor(out=hh[wi][:, fc, b0:b0 + bn], in0=t3[i][:, 0:bn], in1=pgb[:, 0:bn], op=ALU.mult),
                                 reads=[B_t3[i], B_pgb], writes=[B_hh[wi]])
                    if e_ % 2 == 0:
                        w_tail(e_)
                        continue
                    for tt in range(ntiles):
                        for dc in range(2):
                            j = itf % 2; itf += 1
                            for q_ in range(4):
                                wq = q_ // 2; fc = q_ % 2
                                k.op("pe", lambda e, fc=fc, j=j, wq=wq, tt=tt, dc=dc, q_=q_: e.matmul(
                                    pf[j][:], lhsT=hh[wq][:, fc, tt * 128:(tt + 1) * 128], rhs=w2[wq][:, fc, dc * 512:(dc + 1) * 512],
                                    start=(q_ == 0), stop=(q_ == 3)), reads=[B_hh[wq], B_w2[wq]], writes=[B_pf[j]])
                            if e_ == 1:
                                k.op("dve", lambda e, j=j, tt=tt, dc=dc: e.tensor_copy(out=f_acc[:, tt, dc * 512:(dc + 1) * 512], in_=pf[j][:]),
                                     reads=[B_pf[j]], writes=[B_facc[tt]])
                            else:
                                k.op("dve", lambda e, j=j, tt=tt, dc=dc: e.tensor_tensor(out=f_acc[:, tt, dc * 512:(dc + 1) * 512],
                                     in0=f_acc[:, tt, dc * 512:(dc + 1) * 512], in1=pf[j][:], op=ALU.add),
                                     reads=[B_pf[j], B_facc[tt]], writes=[B_facc[tt]])
                    w_tail(e_)
                k.barrier()
            if "stop_experts" in dbg:
                return
            with ExitStack() as s3:
                g2 = [mod_bc(s3, f"g2_{r}", l, r, 5) for r in range(2)]
                lng = load_bc(s3, "ln2g", ln2_g[l:l + 1, :], D)
                lnb = load_bc(s3, "ln2b", ln2_b[l:l + 1, :], D)
                wks = [ln_work(s3, "e2a"), ln_work(s3, "e2b")]
                xo = [sb(s3, f"x1o{i}", [128, D]) for i in range(2)]; B_xo = [Buf(), Buf()]

                def ln2_tile(ti):
                    i = ti % 2
                    r = 0 if ti < NLT else 1
                    T0 = ti * 128
                    k.dma("sp", xo[i][:], x1_scr[T0:T0 + 128, :], writes=[B_xo[i]])
                    ln_epilogue(wks[i], [f_acc[:, ti, 0:512], f_acc[:, ti, 512:1024]], [B_facc[ti], B_facc[ti]], xo[i], B_xo[i],
                                g2[r], lng, lnb, dst[T0:T0 + 128, :])

                for t2 in range(0, ntiles, 2):
                    replay([record(lambda ti=ti: ln2_tile(ti)) for ti in range(t2, min(t2 + 2, ntiles))])
                k.barrier()


        def ssm_phase(st, catT_, B_catT_):
            PI = math.pi
            TWO_PI = 2.0 * math.pi
            def bc3(ap2, n):
                P_, G_ = ap2.shape
                return ap2.rearrange("p (g x) -> p g x", x=1).to_broadcast([P_, G_, n])
            I32 = mybir.dt.int32
            INV2PI = 1.0 / TWO_PI

            def sincos(ang_ap, shape, s_out, c_out, tmps, B_in, B_out, B_tmp):
                y, yi, yf = tmps
                dvt = lambda fn: k.op("dve", fn, reads=[B_in, B_tmp, B_out], writes=[B_tmp])
                dvt(lambda e: e.tensor_scalar(out=y, in0=ang_ap, scalar1=INV2PI, scalar2=32.5, op0=ALU.mult, op1=ALU.add))
                dvt(lambda e: e.tensor_copy(out=yi, in_=y))
                dvt(lambda e: e.tensor_copy(out=yf, in_=yi))
                dvt(lambda e: e.tensor_tensor(out=y, in0=y, in1=yf, op=ALU.subtract))
                dvt(lambda e: e.scalar_tensor_tensor(out=yf, in0=y, scalar=0.0, in1=y, op0=ALU.is_lt, op1=ALU.add))
                k.op("act", lambda e: e.activation(out=s_out, in_=yf, func=AF.Sin, bias=negpi[0:shape[0], :], scale=TWO_PI),
                     reads=[B_tmp, B_np], writes=[B_out])
                dvt(lambda e: e.tensor_scalar(out=y, in0=yf, scalar1=0.25, scalar2=None, op0=ALU.add))
                dvt(lambda e: e.scalar_tensor_tensor(out=yf, in0=y, scalar=1.0, in1=y, op0=ALU.is_ge, op1=ALU.subtract))
                k.op("act", lambda e: e.activation(out=c_out, in_=yf, func=AF.Sin, bias=negpi[0:shape[0], :], scale=-TWO_PI),
                     reads=[B_tmp, B_np], writes=[B_out])

            ar = sb(st, "ar", [64, 64]); ai = sb(st, "ai", [64, 64]); ls = sb(st, "ls", [64, 64]); B_par = Buf()
            with nc.allow_non_contiguous_dma(reason="ssm params"):
                k.dma("sp", ar[:], a_re.rearrange("d g p -> p (d g)"), writes=[B_par])
                k.dma("sp", ai[:], a_im.rearrange("d g p -> p (d g)"), writes=[B_par])
            k.dma("sp", ls[:], log_step.rearrange("(x d) g -> x (d g)", x=1).partition_broadcast(64), writes=[B_par])
            negpi = sb(st, "negpi", [128, 1]); B_np = Buf()
            k.op("dve", lambda e: e.memset(negpi[:], -PI), writes=[B_np])
            kv = sb(st, "kv", [64, 16, 64]); B_kv = Buf()
            k.dma("sp", kv[:], kval_in.rearrange("p (k g) -> p k g", k=16), writes=[B_kv])
            mrow = sb(st, "mrow", [64, 288]); B_mrow = Buf()
            k.dma("sp", mrow[:], mrow_in[:, :], writes=[B_mrow])
            maskF = sb(st, "maskF", [128, 128]); maskB = sb(st, "maskB", [128, 128]); B_mk = Buf()
            k.dma("sp", maskF[:], maskF_in[:, :], writes=[B_mk])
            k.dma("sp", maskB[:], maskB_in[:, :], writes=[B_mk])
            dar = sb(st, "dar", [64, 64]); dai = sb(st, "dai", [64, 64]); B_d = Buf()
            k.op("act", lambda e: e.activation(out=ls[:], in_=ls[:], func=AF.Exp), reads=[B_par], writes=[B_par])
            k.op("dve", lambda e: e.tensor_tensor(out=dar[:], in0=ls[:], in1=ar[:], op=ALU.mult), reads=[B_par], writes=[B_d])
            k.op("dve", lambda e: e.tensor_tensor(out=dai[:], in0=ls[:], in1=ai[:], op=ALU.mult), reads=[B_par, B_d], writes=[B_d])
            LR = sb(st, "LR", [64, 16, 64]); LI = sb(st, "LI", [64, 16, 64]); MG = sb(st, "MG", [64, 16, 64]); B_L = Buf()
            th8 = sb(st, "th8", [64, 64]); B_th8 = Buf()
            k.op("dve", lambda e: e.tensor_scalar(out=th8[:], in0=dai[:], scalar1=8.0, scalar2=None, op0=ALU.mult), reads=[B_d], writes=[B_th8])
            with ExitStack() as t0:
                ang = sb(t0, "ang", [64, 16, 64]); a2 = sb(t0, "a2", [64, 16, 64]); B_ang = Buf()
                dai_b = dai[:].rearrange("p (x g) -> p x g", x=1).to_broadcast([64, 16, 64])
                dar_b = dar[:].rearrange("p (x g) -> p x g", x=1).to_broadcast([64, 16, 64])
                k.op("dve", lambda e: e.tensor_tensor(out=MG[:], in0=kv[:], in1=dar_b, op=ALU.mult), reads=[B_kv, B_d], writes=[B_L])
                k.op("act", lambda e: e.activation(out=MG[:], in_=MG[:], func=AF.Exp), reads=[B_L], writes=[B_L])
                k.op("dve", lambda e: e.tensor_tensor(out=ang[:], in0=kv[:], in1=dai_b, op=ALU.mult), reads=[B_kv, B_d], writes=[B_ang])
                a3 = sb(t0, "a3", [64, 16, 64], I32); a4 = sb(t0, "a4", [64, 16, 64])
                sincos(ang[:], [64, 16, 64], LI[:], LR[:], (a2[:], a3[:], a4[:]), B_ang, B_L, B_ang)
                k.op("dve", lambda e: e.tensor_tensor(out=LR[:], in0=LR[:], in1=MG[:], op=ALU.mult), reads=[B_L], writes=[B_L])
                k.op("dve", lambda e: e.tensor_tensor(out=LI[:], in0=LI[:], in1=MG[:], op=ALU.mult), reads=[B_L], writes=[B_L])
                k.barrier()
            cre = sb(st, "cre", [64, 64]); cim = sb(st, "cim", [64, 64]); B_c = Buf()
            with ExitStack() as t0:
                nr = sb(t0, "nr", [64, 64]); den = sb(t0, "den", [64, 64]); tq = sb(t0, "tq", [64, 64]); B_t = Buf()
                L1r = LR[:, 8, :]; L1i = LI[:, 8, :]
                dv = lambda fn: k.op("dve", fn, reads=[B_t, B_L, B_par, B_c], writes=[B_t, B_c])
                dv(lambda e: e.tensor_scalar(out=nr[:], in0=L1r, scalar1=-1.0, scalar2=None, op0=ALU.add))
                dv(lambda e: e.tensor_tensor(out=den[:], in0=ar[:], in1=ar[:], op=ALU.mult))
                dv(lambda e: e.tensor_tensor(out=tq[:], in0=ai[:], in1=ai[:], op=ALU.mult))
                dv(lambda e: e.tensor_tensor(out=den[:], in0=den[:], in1=tq[:], op=ALU.add))
                dv(lambda e: e.reciprocal(out=den[:], in_=den[:]))
                dv(lambda e: e.tensor_tensor(out=cre[:], in0=nr[:], in1=ar[:], op=ALU.mult))
                dv(lambda e: e.tensor_tensor(out=tq[:], in0=L1i, in1=ai[:], op=ALU.mult))
                dv(lambda e: e.tensor_tensor(out=cre[:], in0=cre[:], in1=tq[:], op=ALU.add))
                dv(lambda e: e.tensor_tensor(out=cre[:], in0=cre[:], in1=den[:], op=ALU.mult))
                dv(lambda e: e.tensor_tensor(out=cim[:], in0=L1i, in1=ar[:], op=ALU.mult))
                dv(lambda e: e.tensor_tensor(out=tq[:], in0=nr[:], in1=ai[:], op=ALU.mult))
                dv(lambda e: e.tensor_tensor(out=cim[:], in0=cim[:], in1=tq[:], op=ALU.subtract))
                dv(lambda e: e.tensor_tensor(out=cim[:], in0=cim[:], in1=den[:], op=ALU.mult))
                k.barrier()
            UT_all = sb(st, "UT_all", [128, 32, 288], BF16); B_UT = Buf()
            NB = ((0, 128), (128, 128), (256, 32))
            u8v = u_scr.rearrange("(n j) f -> n (j f)", j=8)
            with ExitStack() as t0:
                U8 = sb(t0, "U8", [128, 3, 4096]); B_U8 = Buf()
                U8b = sb(t0, "U8b", [128, 3, 4096], BF16); B_U8b = Buf()
                pU = [ps(t0, f"pU{i}", [128, 1024], BF16) for i in range(2)]; B_pU = [Buf(), Buf()]
                for bi, (n0, nb) in enumerate(NB):
                    k.dma("sp", U8[0:nb, bi, :], u8v[n0:n0 + nb, :], writes=[B_U8])
                    ov = U8b[0:nb, bi, :].rearrange("p (g j c) -> p j g c", g=32, j=8, c=16)
                    iv = U8[0:nb, bi, :].rearrange("p (j g c) -> p j g c", g=32, j=8, c=16)
                    k.op("pool" if bi == 1 else "act", (lambda e, ov=ov, iv=iv: e.tensor_copy(out=ov, in_=iv)) if bi == 1 else
                         (lambda e, ov=ov, iv=iv: e.copy(out=ov, in_=iv)), reads=[B_U8], writes=[B_U8b])
                for g in range(32):
                    i = g % 2
                    for bi, (n0, nb) in enumerate(NB):
                        src = U8b[0:nb, bi, g * 128:(g + 1) * 128]
                        k.op("pe", lambda e, i=i, src=src, n0=n0, nb=nb: e.transpose(out=pU[i][:, n0:n0 + nb], in_=src, identity=ident_b[0:nb, 0:nb]),
                             reads=[B_U8b, B_ident_b], writes=[B_pU[i]])
                    k.op("act", lambda e, i=i, g=g: e.copy(out=UT_all[:, g, :], in_=pU[i][:, 0:288]), reads=[B_pU[i]], writes=[B_UT])
                k.barrier()
            COr = sb(st, "COr", [64, 2, 32, 128], BF16); COi = sb(st, "COi", [64, 2, 32, 128], BF16); B_CO = Buf()
            T_all = sb(st, "T_all", [128, 32, 128], BF16); B_T = Buf()
            WinT = sb(st, "WinT", [128, 2, 32, 128], BF16); B_WinT = Buf()
            with ExitStack() as t0:
                BLr = sb(t0, "BLr", [64, 32, 128], BF16); BLi = sb(t0, "BLi", [64, 32, 128], BF16); B_BL = Buf()
                CTr = sb(t0, "CTr", [64, 32, 128], BF16); CTi = sb(t0, "CTi", [64, 32, 128], BF16); B_CT = Buf()
                Br = sb(t0, "Br", [64, 64, 16]); Bi = sb(t0, "Bi", [64, 64, 16]); B_B = Buf()
                Cr = sb(t0, "Cr", [64, 64, 16]); Ci = sb(t0, "Ci", [64, 64, 16]); B_C = Buf()
                Bbr = sb(t0, "Bbr", [64, 64, 16]); Bbi = sb(t0, "Bbi", [64, 64, 16]); B_Bb = Buf()
                with nc.allow_non_contiguous_dma(reason="ssm B/C tables"):
                    for d in range(2):
                        k.dma("sp", Br[:, d * 32:(d + 1) * 32, :], b_re[d].rearrange("g p c -> p g c"), writes=[B_B])
                        k.dma("sp", Bi[:, d * 32:(d + 1) * 32, :], b_im[d].rearrange("g p c -> p g c"), writes=[B_B])
                        for gb in range(4):
                            sl = slice(d * 32 + gb * 8, d * 32 + gb * 8 + 8)
                            k.dma("sp", Cr[:, sl, :], c_re[d, gb * 8:(gb + 1) * 8].rearrange("g c p -> p g c"), writes=[B_C])
                            k.dma("act", Ci[:, sl, :], c_im[d, gb * 8:(gb + 1) * 8].rearrange("g c p -> p g c"), writes=[B_C])
                NLR = sb(t0, "NLR", [64, 16, 64]); NLI = sb(t0, "NLI", [64, 16, 64])
                k.op("dve", lambda e: e.tensor_scalar(out=NLR[:], in0=LR[:], scalar1=-1.0, scalar2=None, op0=ALU.mult), reads=[B_L], writes=[B_L])
                k.op("dve", lambda e: e.tensor_scalar(out=NLI[:], in0=LI[:], scalar1=-1.0, scalar2=None, op0=ALU.mult), reads=[B_L], writes=[B_L])
                ta = sb(t0, "ta", [64, 32, 16]); tb_ = sb(t0, "tb", [64, 32, 16]); B_tab = Buf()
                tc_ = sb(t0, "tc", [64, 32, 16]); td_ = sb(t0, "td", [64, 32, 16]); B_tcd = Buf()
                for d in range(2):
                    dsl = slice(d * 32, (d + 1) * 32)
                    creb = bc3(cre[:, dsl], 16); cimb = bc3(cim[:, dsl], 16)
                    dv = lambda fn: k.op("dve", fn, reads=[B_B, B_c, B_tab, B_Bb], writes=[B_tab, B_Bb])
                    dv(lambda e, dsl=dsl, creb=creb: e.tensor_tensor(out=ta[:], in0=Br[:, dsl, :], in1=creb, op=ALU.mult))
                    dv(lambda e, dsl=dsl, cimb=cimb: e.tensor_tensor(out=tb_[:], in0=Bi[:, dsl, :], in1=cimb, op=ALU.mult))
                    dv(lambda e, dsl=dsl: e.tensor_tensor(out=Bbr[:, dsl, :], in0=ta[:], in1=tb_[:], op=ALU.subtract))
                    dv(lambda e, dsl=dsl, creb=creb: e.tensor_tensor(out=ta[:], in0=Bi[:, dsl, :], in1=creb, op=ALU.mult))
                    dv(lambda e, dsl=dsl, cimb=cimb: e.tensor_tensor(out=tb_[:], in0=Br[:, dsl, :], in1=cimb, op=ALU.mult))
                    dv(lambda e, dsl=dsl: e.tensor_tensor(out=Bbi[:, dsl, :], in0=ta[:], in1=tb_[:], op=ALU.add))
                pTd = [ps(t0, f"pTd{i}", [128, 512]) for i in range(2)]; B_pTd = [Buf(), Buf()]
                pW = [ps(t0, f"pW{i}", [128, 8, 128], BF16) for i in range(2)]; B_pW = [Buf(), Buf()]
                tt = sb(t0, "tt", [128, 128]); B_tt = Buf()
                for d in range(2):
                    dsl = slice(d * 32, (d + 1) * 32)
                    for j in range(8):
                        e_ = (7 - j) if d == 0 else j
                        lr = bc3(LR[:, e_ + 7, dsl], 16); li = bc3(LI[:, e_ + 7, dsl], 16)
                        o_r = BLr[:, :, j * 16:(j + 1) * 16]; o_i = BLi[:, :, j * 16:(j + 1) * 16]
                        dv2 = lambda fn: k.op("dve", fn, reads=[B_Bb, B_L, B_tab, B_BL], writes=[B_tab, B_BL])
                        dv2(lambda e, lr=lr: e.tensor_tensor(out=ta[:], in0=Bbr[:, dsl, :], in1=lr, op=ALU.mult))
                        dv2(lambda e, li=li: e.tensor_tensor(out=tb_[:], in0=Bbi[:, dsl, :], in1=li, op=ALU.mult))
                        dv2(lambda e, o_r=o_r: e.tensor_tensor(out=o_r, in0=ta[:], in1=tb_[:], op=ALU.subtract))
                        dv2(lambda e, lr=lr: e.tensor_tensor(out=ta[:], in0=Bbi[:, dsl, :], in1=lr, op=ALU.mult))
                        dv2(lambda e, li=li: e.tensor_tensor(out=tb_[:], in0=Bbr[:, dsl, :], in1=li, op=ALU.mult))
                        dv2(lambda e, o_i=o_i: e.tensor_tensor(out=o_i, in0=ta[:], in1=tb_[:], op=ALU.add))
                        f_ = (j - 7) if d == 0 else -j
                        for (kk, o_r, o_i, BO) in ((f_ + 7, CTr[:, :, j * 16:(j + 1) * 16], CTi[:, :, j * 16:(j + 1) * 16], B_CT),
                                                   (f_ + 15, COr[:, d, :, j * 16:(j + 1) * 16], COi[:, d, :, j * 16:(j + 1) * 16], B_CO)):
                            lr = bc3(LR[:, kk, dsl], 16); li = bc3(LI[:, kk, dsl], 16)
                            pl = lambda fn, BO=BO: k.op("pool", fn, reads=[B_C, B_L, B_tcd, BO], writes=[B_tcd, BO])
                            pl(lambda e, lr=lr: e.tensor_tensor(out=tc_[:], in0=Cr[:, dsl, :], in1=lr, op=ALU.mult))
                            pl(lambda e, li=li: e.tensor_tensor(out=td_[:], in0=Ci[:, dsl, :], in1=li, op=ALU.mult))
                            pl(lambda e, o_r=o_r: e.tensor_tensor(out=o_r, in0=tc_[:], in1=td_[:], op=ALU.subtract))
                            nlr = bc3(NLR[:, kk, dsl], 16); nli = bc3(NLI[:, kk, dsl], 16)
                            pl(lambda e, nlr=nlr: e.tensor_tensor(out=tc_[:], in0=Ci[:, dsl, :], in1=nlr, op=ALU.mult))
                            pl(lambda e, nli=nli: e.tensor_tensor(out=td_[:], in0=Cr[:, dsl, :], in1=nli, op=ALU.mult))
                            pl(lambda e, o_i=o_i: e.tensor_tensor(out=o_i, in0=tc_[:], in1=td_[:], op=ALU.add))
                    for g in range(32):
                        i = g % 2
                        k.op("pe", lambda e, i=i, g=g: e.matmul(pTd[i][:, 0:128], lhsT=BLr[:, g, :], rhs=CTr[:, g, :], start=True, stop=False),
                             reads=[B_BL, B_CT], writes=[B_pTd[i]])
                        k.op("pe", lambda e, i=i, g=g: e.matmul(pTd[i][:, 0:128], lhsT=BLi[:, g, :], rhs=CTi[:, g, :], start=False, stop=True),
                             reads=[B_BL, B_CT], writes=[B_pTd[i]])
                        if d == 0:
                            k.op("dve", lambda e, i=i, g=g: e.tensor_tensor(out=T_all[:, g, :], in0=pTd[i][:, 0:128], in1=maskF[:], op=ALU.mult),
                                 reads=[B_pTd[i], B_mk], writes=[B_T])
                        else:
                            k.op("dve", lambda e, i=i: e.tensor_tensor(out=tt[:], in0=pTd[i][:, 0:128], in1=maskB[:], op=ALU.mult),
                                 reads=[B_pTd[i], B_mk], writes=[B_tt])
                            k.op("dve", lambda e, g=g: e.tensor_tensor(out=T_all[:, g, :], in0=T_all[:, g, :], in1=tt[:], op=ALU.add),
                                 reads=[B_tt, B_T], writes=[B_T])
                        k.op("pe", lambda e, i=i, g=g: e.transpose(out=pW[i][:, 0, 0:64], in_=BLr[:, g, :], identity=ident_b[0:64, 0:64]),
                             reads=[B_BL, B_ident_b], writes=[B_pW[i]])
                        k.op("pe", lambda e, i=i, g=g: e.transpose(out=pW[i][:, 0, 64:128], in_=BLi[:, g, :], identity=ident_b[0:64, 0:64]),
                             reads=[B_BL, B_ident_b], writes=[B_pW[i]])
                        k.op("act", lambda e, i=i, g=g, d=d: e.copy(out=WinT[:, d, g, :], in_=pW[i][:, 0, :]), reads=[B_pW[i]], writes=[B_WinT])
                k.barrier()
            with ExitStack() as t0:
                Yt = sb(t0, "Yt", [128, 3, 4096], BF16); B_Yt = Buf()
                pXd = [[ps(t0, f"pX{d}{i}", [64, 512]) for i in range(2)] for d in range(2)]
                B_pXd = [[Buf(), Buf()], [Buf(), Buf()]]
                pY = ps(t0, "pY", [128, 512]); B_pY = Buf()
                pYt = ps(t0, "pYt", [128, 8, 128], BF16); B_pYt = Buf()
                W = []
                for d in range(2):
                    W.append(dict(
                        XS=sb(t0, f"XS{d}", [64, 2, 288]), B_XS=Buf(),
                        base=sb(t0, f"base{d}", [64, 288]), sarg=sb(t0, f"sarg{d}", [64, 288]), B_tr=Buf(), B_tr2=Buf(),
                        sargi=sb(t0, f"sargi{d}", [64, 288], mybir.dt.int32), sargf=sb(t0, f"sargf{d}", [64, 288]),
                        sn=sb(t0, f"sn{d}", [64, 288]), cs=sb(t0, f"cs{d}", [64, 288]), B_sc=Buf(),
                        RR=sb(t0, f"RR{d}", [64, 2, 288]), B_RR=Buf(),
                        q1=sb(t0, f"q1{d}", [64, 288]), q2=sb(t0, f"q2{d}", [64, 288]), B_q=Buf(),
                        q3=sb(t0, f"q3{d}", [64, 288]), q4=sb(t0, f"q4{d}", [64, 288]), B_q34=Buf(),
                        SS=sb(t0, f"SS{d}", [64, 2, 288]), B_SS=Buf()))
                Sp = sb(t0, "Sp", [64, 2, 2, 288], BF16); B_Spd = [Buf(), Buf()]
                Ysb = sb(t0, "Ysb", [128, 288], BF16); B_Ysb = Buf()
                k.op("dve", lambda e: e.memset(Sp[:], 0.0), writes=[B_Spd[0], B_Spd[1]])

                def chain(d, g):
                    w = W[d]
                    XS, base, sarg, sargi, sargf, sn, cs, RR, q1, q2, SS = (w[n] for n in ("XS", "base", "sarg", "sargi", "sargf", "sn", "cs", "RR", "q1", "q2", "SS"))
                    B_XS, B_tr, B_tr2, B_sc, B_RR, B_q, B_SS = (w[n] for n in ("B_XS", "B_tr", "B_tr2", "B_sc", "B_RR", "B_q", "B_SS"))
                    pX = pXd[d]; B_pX = B_pXd[d]; B_Sp = B_Spd[d]
                    dg = d * 32 + g
                    for c2 in range(2):
                        k.op("pe", lambda e, c2=c2: e.matmul(pX[c2][:, 0:288], lhsT=WinT[:, d, g, c2 * 64:(c2 + 1) * 64], rhs=UT_all[:, g, :],
                                                             start=True, stop=True), reads=[B_WinT, B_UT], writes=[B_pX[c2]])
                        if d == 0:
                            k.op("act", lambda e, c2=c2: e.copy(out=XS[:, c2, :], in_=pX[c2][:, 0:288]), reads=[B_pX[c2]], writes=[B_XS])
                        else:
                            k.op("act", lambda e, c2=c2: e.copy(out=XS[:, c2, 0:32], in_=pX[c2][:, 31::-1]), reads=[B_pX[c2]], writes=[B_XS])
                            k.op("act", lambda e, c2=c2: e.copy(out=XS[:, c2, 32:288], in_=pX[c2][:, 287:31:-1]), reads=[B_pX[c2]], writes=[B_XS])
                    k.op("dve", lambda e: e.tensor_scalar(out=base[:], in0=mrow[:], scalar1=th8[:, dg:dg + 1], scalar2=None, op0=ALU.mult),
                         reads=[B_mrow, B_th8], writes=[B_tr])
                    sincos(base[:], [64, 288], sn[:], cs[:], (sarg[:], sargi[:], sargf[:]), B_tr, B_sc, B_tr2)
                    dv = lambda fn: k.op("dve", fn, reads=[B_XS, B_sc, B_q, B_RR, B_SS, B_L], writes=[B_q, B_RR, B_SS])
                    dv(lambda e: e.tensor_tensor(out=q1[:], in0=cs[:], in1=XS[:, 0, :], op=ALU.mult))
                    dv(lambda e: e.tensor_tensor(out=q2[:], in0=sn[:], in1=XS[:, 1, :], op=ALU.mult))
                    dv(lambda e: e.tensor_tensor(out=q1[:], in0=q1[:], in1=q2[:], op=ALU.add))
                    dv(lambda e: e.tensor_tensor_scan(out=RR[:, 0, :], data0=MG[:, 15, dg:dg + 1].to_broadcast([64, 288]), data1=q1[:], initial=0.0, op0=ALU.mult, op1=ALU.add))
                    dv(lambda e: e.tensor_tensor(out=q1[:], in0=cs[:], in1=XS[:, 1, :], op=ALU.mult))
                    dv(lambda e: e.tensor_tensor(out=q2[:], in0=sn[:], in1=XS[:, 0, :], op=ALU.mult))
                    dv(lambda e: e.tensor_tensor(out=q1[:], in0=q1[:], in1=q2[:], op=ALU.subtract))
                    dv(lambda e: e.tensor_tensor_scan(out=RR[:, 1, :], data0=MG[:, 15, dg:dg + 1].to_broadcast([64, 288]), data1=q1[:], initial=0.0, op0=ALU.mult, op1=ALU.add))
                    dv(lambda e: e.tensor_tensor(out=q1[:], in0=cs[:], in1=RR[:, 0, :], op=ALU.mult))
                    dv(lambda e: e.tensor_tensor(out=q2[:], in0=sn[:], in1=RR[:, 1, :], op=ALU.mult))
                    dv(lambda e: e.tensor_tensor(out=SS[:, 0, :], in0=q1[:], in1=q2[:], op=ALU.subtract))
                    dv(lambda e: e.tensor_tensor(out=q1[:], in0=cs[:], in1=RR[:, 1, :], op=ALU.mult))
                    dv(lambda e: e.tensor_tensor(out=q2[:], in0=sn[:], in1=RR[:, 0, :], op=ALU.mult))
                    dv(lambda e: e.tensor_tensor(out=SS[:, 1, :], in0=q1[:], in1=q2[:], op=ALU.add))
                    for c2 in range(2):
                        if d == 0:
                            k.op("act", lambda e, c2=c2: e.copy(out=Sp[:, 0, c2, 1:288], in_=SS[:, c2, 0:287]), reads=[B_SS], writes=[B_Sp])
                        else:
                            k.op("act", lambda e, c2=c2: e.copy(out=Sp[:, 1, c2, 0:31], in_=SS[:, c2, 30::-1]), reads=[B_SS], writes=[B_Sp])
                            k.op("act", lambda e, c2=c2: e.copy(out=Sp[:, 1, c2, 32:288], in_=SS[:, c2, 286:30:-1]), reads=[B_SS], writes=[B_Sp])

                B_Sp = B_Spd[0]
                for g in range(32):
                    recs = []
                    orig_op = k.op
                    for d in range(2):
                        rec = []
                        k.op = lambda *a, rec=rec, **kw: rec.append((a, kw))
                        chain(d, g)
                        recs.append(rec)
                    k.op = orig_op
                    for i_ in range(max(len(r_) for r_ in recs)):
                        for r_ in recs:
                            if i_ < len(r_):
                                a_, kw_ = r_[i_]
                                k.op(*a_, **kw_)
                    k.op("pe", lambda e, g=g: e.matmul(pY[:, 0:288], lhsT=T_all[:, g, :], rhs=UT_all[:, g, :], start=True, stop=False),
                         reads=[B_T, B_UT], writes=[B_pY])
                    for d in range(2):
                        k.op("pe", lambda e, g=g, d=d: e.matmul(pY[:, 0:288], lhsT=COr[:, d, g, :], rhs=Sp[:, d, 0, :], start=False, stop=False),
                             reads=[B_CO, B_Spd[d]], writes=[B_pY])
                        k.op("pe", lambda e, g=g, d=d: e.matmul(pY[:, 0:288], lhsT=COi[:, d, g, :], rhs=Sp[:, d, 1, :], start=False, stop=(d == 1)),
                             reads=[B_CO, B_Spd[d]], writes=[B_pY])
                    k.op("act", lambda e: e.copy(out=Ysb[:], in_=pY[:, 0:288]), reads=[B_pY], writes=[B_Ysb])
                    for bi, (n0, nb) in enumerate(NB):
                        k.op("pe", lambda e, bi=bi, n0=n0, nb=nb: e.transpose(out=pYt[0:nb, bi, :], in_=Ysb[:, n0:n0 + nb], identity=ident_b[:]),
                             reads=[B_Ysb, B_ident_b], writes=[B_pYt])
                    for bi, (n0, nb) in enumerate(NB):
                        dst = Yt[0:nb, bi, :].rearrange("p (j f) -> p j f", j=8)[:, :, g * 16:(g + 1) * 16]
                        k.op("dve", lambda e, bi=bi, nb=nb, dst=dst: e.tensor_copy(out=dst, in_=pYt[0:nb, bi, :].rearrange("p (j c) -> p j c", j=8)),
                             reads=[B_pYt], writes=[B_Yt])
                y8v = y_scr.rearrange("(n j) f -> n (j f)", j=8)
                for bi, (n0, nb) in enumerate(NB):
                    k.dma("sp", y8v[n0:n0 + nb, :], Yt[0:nb, bi, :], reads=[B_Yt])
                k.barrier()


        def ssm_post(st, catT_, B_catT_):
            GC = 2.0 * math.sqrt(2.0 / math.pi)
            gw = sb(st, "gluw", [128, 4, 512], BF16); B_gw = Buf()
            k.dma("pool", gw[:], glu_w.rearrange("(k p) n -> p k n", p=128), writes=[B_gw])
            gb = sb(st, "glub", [128, 4]); B_gb = Buf()
            with nc.allow_non_contiguous_dma(reason="tiny bias"):
                k.dma("sp", gb[:], glu_b[0, :].rearrange("(c p) -> p c", p=128), writes=[B_gb])
            dbc = load_bc(st, "dskip", ssm_d[0:1, :], 512)
            yt = [sb(st, f"py{i}", [128, 512], BF16) for i in range(2)]; B_yt = [Buf(), Buf()]
            ut = [sb(st, f"pu{i}", [128, 512]) for i in range(2)]; B_ut = [Buf(), Buf()]
            xx = [sb(st, f"pxx{c_}", [128, 512]) for c_ in range(2)]; B_xx = [Buf(), Buf()]
            ww = [sb(st, f"pww{c_}", [128, 512]) for c_ in range(2)]; B_ww = [Buf(), Buf()]
            sg = [sb(st, f"psg{c_}", [128, 512]) for c_ in range(2)]; B_sg = [Buf(), Buf()]
            g_bf = [sb(st, f"pg_bf{c_}", [128, 512], BF16) for c_ in range(2)]; B_g = [Buf(), Buf()]
            gT = [sb(st, f"pgT{c_}", [128, 4, 128], BF16) for c_ in range(2)]; B_gT = [Buf(), Buf()]
            s2 = [sb(st, f"ps2{c_}", [128, 4, 128]) for c_ in range(2)]; B_s2 = [Buf(), Buf()]
            pGT = [ps(st, f"pGT{c_}", [128, 8, 128], BF16) for c_ in range(2)]; B_pGT = [Buf(), Buf()]
            pz = [ps(st, f"pz{c_}", [128, 4, 128]) for c_ in range(2)]; B_pz = [Buf(), Buf()]
            def post_tile(ti):
                i = ti % 2
                T0 = ti * 128
                row0 = T0 + CTX if ti < NLT else T0 - SEQ
                k.dma("sp", yt[i][:], y_scr[row0:row0 + 128, :], writes=[B_yt[i]])
                k.dma("sp", ut[i][:], u_scr[row0:row0 + 128, :], writes=[B_ut[i]])
                k.op("dve", lambda e, i=i: e.tensor_tensor(out=xx[i][:], in0=ut[i][:], in1=dbc[0][:], op=ALU.mult), reads=[B_ut[i], dbc[1]], writes=[B_xx[i]])
                k.op("dve", lambda e, i=i: e.tensor_tensor(out=xx[i][:], in0=xx[i][:], in1=yt[i][:], op=ALU.add), reads=[B_xx[i], B_yt[i]], writes=[B_xx[i]])
                k.op("pool", lambda e: e.tensor_tensor(out=ww[i][:], in0=xx[i][:], in1=xx[i][:], op=ALU.mult), reads=[B_xx[i]], writes=[B_ww[i]])
                k.op("pool", lambda e: e.tensor_scalar(out=ww[i][:], in0=ww[i][:], scalar1=0.044715, scalar2=1.0, op0=ALU.mult, op1=ALU.add), reads=[B_ww[i]], writes=[B_ww[i]])
                k.op("pool", lambda e: e.tensor_tensor(out=ww[i][:], in0=ww[i][:], in1=xx[i][:], op=ALU.mult), reads=[B_ww[i], B_xx[i]], writes=[B_ww[i]])
                k.op("act", lambda e: e.activation(out=sg[i][:], in_=ww[i][:], func=AF.Sigmoid, scale=GC), reads=[B_ww[i]], writes=[B_sg[i]])
                k.op("dve", lambda e: e.tensor_tensor(out=g_bf[i][:], in0=xx[i][:], in1=sg[i][:], op=ALU.mult), reads=[B_xx[i], B_sg[i]], writes=[B_g[i]])
                if "dbg_g" in dbg:
                    if ti == 0:
                        dbg_g = dscr("dbg_g", [NT, 512], BF16)
                    k.dma("sp", dbg_g[T0:T0 + 128, :], g_bf[i][:], reads=[B_g[i]])
                for kc in range(4):
                    k.op("pe", lambda e, kc=kc: e.transpose(out=pGT[i][:, kc, :], in_=g_bf[i][:, kc * 128:(kc + 1) * 128], identity=ident_b[:]),
                         reads=[B_g[i], B_ident_b], writes=[B_pGT[i]])
                k.op("act", lambda e: e.copy(out=gT[i][:], in_=pGT[i][:, 0:4, :]), reads=[B_pGT[i]], writes=[B_gT[i]])
                for n_ in range(4):
                    for kc in range(4):
                        k.op("pe", lambda e, n_=n_, kc=kc: e.matmul(pz[i][:, n_, :], lhsT=gw[:, kc, n_ * 128:(n_ + 1) * 128], rhs=gT[i][:, kc, :],
                                                                    start=(kc == 0), stop=(kc == 3)), reads=[B_gw, B_gT[i]], writes=[B_pz[i]])
                for n_ in range(4):
                    k.op("act", lambda e, n_=n_: e.activation(out=s2[i][:, n_, :], in_=pz[i][:, n_, :], func=AF.Sigmoid, bias=gb[:, n_:n_ + 1], scale=1.0),
                         reads=[B_pz[i], B_gb], writes=[B_s2[i]])
                k.op("dve", lambda e, T0=T0: e.tensor_tensor(out=catT_[:, 4:8, T0:T0 + 128], in0=gT[i][:], in1=s2[i][:], op=ALU.mult),
                     reads=[B_gT[i], B_s2[i]], writes=[B_catT_])
            for t2_ in range(0, NTILE, 2):
                replay([record(lambda ti=ti: post_tile(ti)) for ti in range(t2_, min(t2_ + 2, NTILE))])
            if "dbg_cat" in dbg:
                dbg_cat = dscr("dbg_cat", [128, 8, NT], BF16)
                k.dma("sp", dbg_cat, catT_[:], reads=[B_catT_])
            k.barrier()


        def layer1_mixer():
            LAM_INIT = 0.8 - 0.6 * math.exp(-0.3 * 1)
            SC = 0.125
            with ExitStack() as L1:
                qT2 = sb(L1, "qT2", [128, 8, SEQ], BF16); B_q2 = Buf()
                kT2 = sb(L1, "kT2", [128, 8, NT], BF16); B_k2 = Buf()
                v1 = sb(L1, "v1", [128, NTILE, D], BF16); B_v1 = Buf()
                nmax = sb(L1, "nmax", [128, 32]); B_nmax = Buf()
                for st in phase("l1proj"):
                    w_bf = sb(st, "dif_w_bf", [128, 8, 3072], BF16); B_w = Buf()
                    for kc in range(8):
                        k.dma("pool", w_bf[:, kc, :], dif_w_in[kc * 128:(kc + 1) * 128, :], writes=[B_w])
                    sc1p = [mod_bc(st, f"l1sc1p_{r}", 1, r, 1, plus1=True) for r in range(2)]
                    sh1 = [mod_bc(st, f"l1sh1_{r}", 1, r, 0) for r in range(2)]
                    xt = [sb(st, f"l1xt{i}", [128, D]) for i in range(2)]; B_xt = [Buf(), Buf()]
                    tmpf = [sb(st, f"l1tmpf{i}", [128, D]) for i in range(2)]; B_tmpf = [Buf(), Buf()]
                    h_bf = [sb(st, f"l1h_bf{i}", [128, D], BF16) for i in range(2)]; B_hbf = [Buf(), Buf()]
                    hT = [sb(st, f"l1hT{i}", [128, 8, 128], BF16) for i in range(2)]; B_hT = [Buf(), Buf()]
                    rt = [sb(st, f"l1rt{i}", [128, 64]) for i in range(2)]; B_rt = [Buf(), Buf()]
                    t1 = [sb(st, f"l1rope_t1{i}", [128, 512]) for i in range(2)]; t2 = [sb(st, f"l1rope_t2{i}", [128, 512]) for i in range(2)]; B_rtmp = [Buf(), Buf()]
                    qk_bf = [sb(st, f"l1qk_bf{i}", [128, 512], BF16) for i in range(2)]; B_qk = [Buf(), Buf()]
                    pT = [ps(st, f"l1pT{i}", [128, 8, 128], BF16) for i in range(2)]; B_pT = [Buf(), Buf()]
                    pp = [[ps(st, f"l1pp{i}{j}", [128, 512]) for j in range(2)] for i in range(2)]; B_pp = [[Buf(), Buf()], [Buf(), Buf()]]
                    pq = [ps(st, f"l1pq{i}", [128, 8, 128], BF16) for i in range(2)]; B_pq = [Buf(), Buf()]
                    sqt = [sb(st, f"l1sq{i}", [128, 512]) for i in range(2)]; rs8 = [sb(st, f"l1rs8{i}", [128, 8]) for i in range(2)]; B_sq = [Buf(), Buf()]
                    k.op("dve", lambda e: e.memset(nmax[:], 0.0), writes=[B_nmax])
                    ibs = [0, 0]

                    def proj_tile(ti):
                        i = ti % 2
                        r = 0 if ti < NLT else 1
                        T0 = ti * 128
                        k.dma("sp", xt[i][:], src_rows(ti, 1), writes=[B_xt[i]])
                        if r == 0:
                            k.dma("sp", rt[i][:], rope_cs[T0:T0 + 128, :], writes=[B_rt[i]])
                        k.op("dve", lambda e: e.tensor_tensor(out=tmpf[i][:], in0=xt[i][:], in1=sc1p[r][0][:], op=ALU.mult),
                             reads=[B_xt[i], sc1p[r][1]], writes=[B_tmpf[i]])
                        k.op("pool", lambda e: e.tensor_tensor(out=h_bf[i][:], in0=tmpf[i][:], in1=sh1[r][0][:], op=ALU.add),
                             reads=[B_tmpf[i], sh1[r][1]], writes=[B_hbf[i]])
                        for kc in range(8):
                            k.op("pe", lambda e, kc=kc: e.transpose(out=pT[i][:, kc, :], in_=h_bf[i][:, kc * 128:(kc + 1) * 128], identity=ident_b[:]),
                                 reads=[B_hbf[i], B_ident_b], writes=[B_pT[i]])
                        k.op("act", lambda e: e.copy(out=hT[i][:], in_=pT[i][:]), reads=[B_pT[i]], writes=[B_hT[i]])
                        for cb in range(6):
                            if r == 1 and cb < 2:
                                continue
                            j = ibs[i] % 2; ibs[i] += 1
                            ppj = pp[i][j]; Bppj = B_pp[i][j]
                            for kc in range(8):
                                k.op("pe", lambda e, kc=kc, ppj=ppj, cb=cb: e.matmul(
                                    ppj[:], lhsT=hT[i][:, kc, :], rhs=w_bf[:, kc, cb * 512:(cb + 1) * 512],
                                    start=(kc == 0), stop=(kc == 7)), reads=[B_hT[i], B_w], writes=[Bppj])
                            if cb >= 4:
                                c0 = (cb - 4) * 512
                                k.op("act", lambda e, ppj=ppj, c0=c0: e.copy(out=v1[:, ti, c0:c0 + 512], in_=ppj[:]), reads=[Bppj], writes=[B_v1])
                                continue
                            if r == 0:
                                rope_apply(None, ppj[:], Bppj, qk_bf[i][:], B_qk[i], 8, rt[i], B_rt[i], t1[i][:], t2[i][:], B_rtmp[i])
                            else:
                                k.op("dve", lambda e, ppj=ppj: e.tensor_copy(out=qk_bf[i][:], in_=ppj[:]), reads=[Bppj], writes=[B_qk[i]])
                            k.op("dve", lambda e: e.tensor_tensor(out=sqt[i][:], in0=qk_bf[i][:], in1=qk_bf[i][:], op=ALU.mult), reads=[B_qk[i], B_sq[i]], writes=[B_sq[i]])
                            k.op("dve", lambda e: e.tensor_reduce(out=rs8[i][:], in_=sqt[i][:].rearrange("p (m d) -> p m d", d=64), axis=AX.X, op=ALU.add), reads=[B_sq[i]], writes=[B_sq[i]])
                            k.op("dve", lambda e, cb=cb: e.tensor_tensor(out=nmax[:, cb * 8:(cb + 1) * 8], in0=nmax[:, cb * 8:(cb + 1) * 8], in1=rs8[i][:], op=ALU.max),
                                 reads=[B_sq[i], B_nmax], writes=[B_nmax])
                            for hh in range(4):
                                k.op("pe", lambda e, hh=hh: e.transpose(out=pq[i][:, hh, :], in_=qk_bf[i][:, hh * 128:(hh + 1) * 128], identity=ident_b[:]),
                                     reads=[B_qk[i], B_ident_b], writes=[B_pq[i]])
                            dstT, BD = (qT2, B_q2) if cb < 2 else (kT2, B_k2)
                            h0 = (cb % 2) * 4
                            k.op("act", lambda e, dstT=dstT, h0=h0: e.copy(out=dstT[:, h0:h0 + 4, T0:T0 + 128], in_=pq[i][:, 0:4, :]),
                                 reads=[B_pq[i]], writes=[BD])

                    for t2_ in range(0, NTILE, 2):
                        replay([record(lambda ti=ti: proj_tile(ti)) for ti in range(t2_, min(t2_ + 2, NTILE))])
                    k.barrier()
                for st in phase("l1att"):
                    w_bf = sb(st, "difwo_bf", [128, 8, D], BF16); B_w = Buf()
                    for kc in range(8):
                        k.dma("pool", w_bf[:, kc, :], dif_w_out[kc * 128:(kc + 1) * 128, :], writes=[B_w])
                    g1 = mod_bc(st, "l1g1", 1, 0, 2)
                    lng = load_bc(st, "l1ln1g", ln1_g[1:2, :], D)
                    lnb = load_bc(st, "l1ln1b", ln1_b[1:2, :], D)
                    wk = ln_work(st, "l1e1")
                    xo = [sb(st, f"l1xo{i}", [128, D]) for i in range(2)]; B_xo = [Buf(), Buf()]
                    lam = sb(st, "lam", [128, 8]); B_lam = Buf()
                    lq = [load_bc(st, f"lq{i}", a[0:1, :], 64) for i, a in enumerate((lam_q1, lam_k1, lam_q2, lam_k2))]
                    ltmp = sb(st, "ltmp", [128, 64]); B_lt = Buf()
                    for i2 in range(2):
                        k.op("dve", lambda e, i2=i2: e.tensor_tensor(out=ltmp[:], in0=lq[2 * i2][0][:], in1=lq[2 * i2 + 1][0][:], op=ALU.mult),
                             reads=[lq[2 * i2][1], lq[2 * i2 + 1][1], B_lt], writes=[B_lt])
                        k.op("dve", lambda e, i2=i2: e.reduce_sum(out=lam[:, i2:i2 + 1], in_=ltmp[:], axis=AX.X), reads=[B_lt, B_lam], writes=[B_lam])
                    k.op("act", lambda e: e.activation(out=lam[:, 2:4], in_=lam[:, 0:2], func=AF.Exp), reads=[B_lam], writes=[B_lam])
                    k.op("dve", lambda e: e.tensor_tensor(out=lam[:, 4:5], in0=lam[:, 2:3], in1=lam[:, 3:4], op=ALU.subtract), reads=[B_lam], writes=[B_lam])
                    k.op("dve", lambda e: e.tensor_scalar(out=lam[:, 5:6], in0=lam[:, 4:5], scalar1=-1.0, scalar2=-LAM_INIT, op0=ALU.mult, op1=ALU.add), reads=[B_lam], writes=[B_lam])
                    sg_col = sb(st, "sg_col", [128, 1]); B_sg = Buf()
                    with nc.allow_non_contiguous_dma(reason="tiny"):
                        k.dma("sp", sg_col[:], subln_g[0, :].rearrange("(p x) -> p x", x=1), writes=[B_sg])
                    k.op("dve", lambda e: e.tensor_scalar(out=sg_col[:], in0=sg_col[:], scalar1=1.0 - LAM_INIT, scalar2=None, op0=ALU.mult), reads=[B_sg], writes=[B_sg])
                    ones_bf = sb(st, "ones_bf", [128, 128], BF16); B_ones = Buf()
                    k.op("dve", lambda e: e.memset(ones_bf[:], 1.0), writes=[B_ones])
                    negC = sb(st, "negC", [128, 16]); B_negC = Buf()
                    with ExitStack() as t0:
                        nb = sb(t0, "nmax_bf", [128, 32], BF16); B_nb = Buf()
                        k.op("dve", lambda e: e.tensor_scalar(out=nb[:], in0=nmax[:], scalar1=1.02, scalar2=None, op0=ALU.mult), reads=[B_nmax], writes=[B_nb])
                        pn = ps(t0, "pn", [16, 1024], BF16); B_pn = Buf()
                        k.op("pe", lambda e: e.transpose(out=pn[:, 0:128], in_=nb[:, 0:16], identity=ident_b[:]), reads=[B_nb, B_ident_b], writes=[B_pn])
                        k.op("pe", lambda e: e.transpose(out=pn[:, 128:256], in_=nb[:, 16:32], identity=ident_b[:]), reads=[B_nb, B_ident_b], writes=[B_pn])
                        r2 = sb(t0, "r2", [16, 8]); B_r2 = Buf()
                        k.op("dve", lambda e: e.reduce_max(out=r2[:, 0:1], in_=pn[:, 0:128], axis=AX.X), reads=[B_pn], writes=[B_r2])
                        k.op("dve", lambda e: e.reduce_max(out=r2[:, 1:2], in_=pn[:, 128:256], axis=AX.X), reads=[B_pn, B_r2], writes=[B_r2])
                        k.op("dve", lambda e: e.tensor_tensor(out=r2[:, 2:3], in0=r2[:, 0:1], in1=r2[:, 1:2], op=ALU.mult), reads=[B_r2], writes=[B_r2])
                        k.op("act", lambda e: e.sqrt(out=r2[:, 3:4], in_=r2[:, 2:3]), reads=[B_r2], writes=[B_r2])
                        k.op("dve", lambda e: e.tensor_scalar(out=r2[:, 4:5], in0=r2[:, 3:4], scalar1=-SC, scalar2=None, op0=ALU.mult), reads=[B_r2], writes=[B_r2])
                        dg = sb(t0, "dgC", [16, 16], BF16); B_dg = Buf()
                        k.op("dve", lambda e: e.tensor_scalar(out=dg[:], in0=ident_f[0:16, 0:16], scalar1=r2[:, 4:5], scalar2=None, op0=ALU.mult), reads=[B_r2, B_ident_f], writes=[B_dg])
                        pc = ps(t0, "pcb", [128, 512]); B_pc = Buf()
                        k.op("pe", lambda e: e.matmul(pc[:, 0:16], lhsT=ones_bf[0:16, :], rhs=dg[:], start=True, stop=True), reads=[B_ones, B_dg], writes=[B_pc])
                        k.op("dve", lambda e: e.tensor_copy(out=negC[:], in_=pc[:, 0:16]), reads=[B_pc], writes=[B_negC])
                        k.barrier()
                    ET = [sb(st, f"ET{i}", [128, 512], BF16) for i in range(4)]; B_ET = [Buf() for _ in range(4)]
                    aoT = sb(st, "aoT_all", [128, 8, 512], BF16); B_aoT = Buf()
                    rz = sb(st, "rz", [1, 2, 512]); B_rz = Buf()
                    rzb = sb(st, "rzb", [1, 4, 512], BF16); B_rzb = Buf()
                    bcs = sb(st, "bcs", [128, 512]); B_bcs = Buf()
                    oT = sb(st, "oT", [128, 512]); B_oT = Buf()
                    t5 = sb(st, "t5", [128, 512]); B_t5 = Buf()
                    sqb = sb(st, "sqb", [128, 512], BF16); B_sqb = Buf()
                    pS = [ps(st, f"pS{i}", [128, 512]) for i in range(2)]; B_pS = [Buf(), Buf()]
                    pO4 = [ps(st, f"pO{i}", [128, 512]) for i in range(4)]; B_pO4 = [Buf() for _ in range(4)]
                    pZ1 = ps(st, "pZ", [1, 512]); B_pZ1 = Buf()
                    pZ = [pZ1, pZ1]; B_pZ = [B_pZ1, B_pZ1]
                    pB = ps(st, "pB", [128, 512]); B_pB = Buf()
                    cnt = {"s": 0, "e": 0}

                    def bcast_row(hi, lo):
                        k.op("pe", lambda e: e.matmul(pB[:], lhsT=ones_bf[0:1, :], rhs=hi, start=True, stop=False), reads=[B_ones, B_rzb], writes=[B_pB])
                        k.op("pe", lambda e: e.matmul(pB[:], lhsT=ones_bf[0:1, :], rhs=lo, start=False, stop=True), reads=[B_ones, B_rzb], writes=[B_pB])
                        k.op("act", lambda e: e.copy(out=bcs[:], in_=pB[:]), reads=[B_pB], writes=[B_bcs])

                    def split_row(src, j):
                        k.op("dve", lambda e: e.tensor_copy(out=rzb[:, 2 * j, :], in_=src), reads=[B_rz], writes=[B_rzb])
                        k.op("dve", lambda e: e.tensor_tensor(out=rzb[:, 2 * j + 1, :], in0=src, in1=rzb[:, 2 * j, :], op=ALU.subtract), reads=[B_rz, B_rzb], writes=[B_rzb])

                    zs = sb(st, "zs", [1, 2, 512]); B_zs = Buf()

                    def qk_exp(Q0, h, c, b):
                        ps_ = slice(c * 64, (c + 1) * 64)
                        m = h * 2 + c
                        js = cnt["s"] % 2; cnt["s"] += 1
                        je = cnt["e"] % 4; cnt["e"] += 1
                        k.op("pe", lambda e: e.matmul(pS[js][:], lhsT=kT2[ps_, h, b * 128:(b + 1) * 128], rhs=qT2[ps_, h, Q0:Q0 + 512],
                                                      start=True, stop=True), reads=[B_k2, B_q2], writes=[B_pS[js]])
                        k.op("act", lambda e: e.activation(out=ET[je][:], in_=pS[js][:], func=AF.Exp, bias=negC[:, m:m + 1], scale=SC),
                             reads=[B_pS[js], B_negC], writes=[B_ET[je]])
                        return je

                    def pvz(h, c, b, je):
                        pO = pO4[(h % 2) * 2:(h % 2) * 2 + 2]; B_pO = B_pO4[(h % 2) * 2:(h % 2) * 2 + 2]
                        Zacc = Zacc4[(h % 2) * 2:(h % 2) * 2 + 2]; B_Zacc = B_Zacc4[(h % 2) * 2:(h % 2) * 2 + 2]
                        k.op("pe", lambda e: e.matmul(pO[c][:], lhsT=v1[:, b, h * 128:(h + 1) * 128], rhs=ET[je][:],
                                                      start=(b == 0), stop=(b == NTILE - 1)), reads=[B_v1, B_ET[je]], writes=[B_pO[c]])
                        if b == 0:
                            k.op("dve", lambda e: e.tensor_copy(out=Zacc[c][:], in_=ET[je][:]), reads=[B_ET[je]], writes=[B_Zacc[c]])
                        else:
                            k.op("dve", lambda e: e.tensor_tensor(out=Zacc[c][:], in0=Zacc[c][:], in1=ET[je][:], op=ALU.add), reads=[B_ET[je], B_Zacc[c]], writes=[B_Zacc[c]])

                    Zacc4 = [sb(st, f"Zacc{i}", [128, 512]) for i in range(4)]; B_Zacc4 = [Buf() for _ in range(4)]
                    ones_f = sb(st, "ones_f", [128, 1]); B_onesf = Buf()
                    k.op("dve", lambda e: e.memset(ones_f[:], 1.0), writes=[B_onesf])

                    def bcast_recip(c, Zacc, B_Zacc):
                        k.op("pe", lambda e: e.matmul(pZ[c][:], lhsT=ones_f[:, 0:1], rhs=Zacc[c][:], start=True, stop=True), reads=[B_onesf, B_Zacc[c]], writes=[B_pZ[c]])
                        k.op("act", lambda e: e.copy(out=zs[:, c, :], in_=pZ[c][:]), reads=[B_pZ[c], B_zs], writes=[B_zs])
                        k.op("dve", lambda e: e.tensor_copy(out=rzb[:, 2 * c, :], in_=zs[:, c, :]), reads=[B_zs, B_rzb], writes=[B_rzb])
                        k.op("dve", lambda e: e.tensor_tensor(out=rzb[:, 2 * c + 1, :], in0=zs[:, c, :], in1=rzb[:, 2 * c, :], op=ALU.subtract), reads=[B_zs, B_rzb], writes=[B_rzb])
                        k.op("pe", lambda e: e.matmul(pB[:], lhsT=ones_bf[0:1, :], rhs=rzb[:, 2 * c, :], start=True, stop=False), reads=[B_ones, B_rzb], writes=[B_pB])
                        k.op("pe", lambda e: e.matmul(pB[:], lhsT=ones_bf[0:1, :], rhs=rzb[:, 2 * c + 1, :], start=False, stop=True), reads=[B_ones, B_rzb], writes=[B_pB])
                        k.op("dve", lambda e: e.reciprocal(out=bcs[:], in_=pB[:]), reads=[B_pB, B_bcs], writes=[B_bcs])

                    def epilogue(h):
                        pO = pO4[(h % 2) * 2:(h % 2) * 2 + 2]; B_pO = B_pO4[(h % 2) * 2:(h % 2) * 2 + 2]
                        Zacc = Zacc4[(h % 2) * 2:(h % 2) * 2 + 2]; B_Zacc = B_Zacc4[(h % 2) * 2:(h % 2) * 2 + 2]
                        bcast_recip(0, Zacc, B_Zacc)
                        k.op("dve", lambda e: e.tensor_tensor(out=oT[:], in0=pO[0][:], in1=bcs[:], op=ALU.mult), reads=[B_pO[0], B_bcs, B_oT], writes=[B_oT])
                        bcast_recip(1, Zacc, B_Zacc)
                        k.op("dve", lambda e: e.tensor_tensor(out=t5[:], in0=pO[1][:], in1=bcs[:], op=ALU.mult), reads=[B_pO[1], B_bcs, B_t5], writes=[B_t5])
                        k.op("dve", lambda e: e.scalar_tensor_tensor(out=oT[:], in0=t5[:], scalar=lam[:, 5:6], in1=oT[:], op0=ALU.mult, op1=ALU.add),
                             reads=[B_oT, B_t5, B_lam], writes=[B_oT])
                        k.op("dve", lambda e: e.tensor_tensor(out=sqb[:], in0=oT[:], in1=oT[:], op=ALU.mult), reads=[B_oT, B_sqb], writes=[B_sqb])
                        k.op("pe", lambda e: e.matmul(pZ[0][:], lhsT=ones_bf[:, 0:1], rhs=sqb[:], start=True, stop=True), reads=[B_ones, B_sqb], writes=[B_pZ[0]])
                        k.op("act", lambda e: e.activation(out=rz[:, 0, :], in_=pZ[0][:], func=AF.Ln, scale=1.0 / 128.0, bias=eps_t[0:1, :]), reads=[B_pZ[0], B_rz, B_eps], writes=[B_rz])
                        k.op("act", lambda e: e.activation(out=rz[:, 0, :], in_=rz[:, 0, :], func=AF.Exp, scale=-0.5), reads=[B_rz], writes=[B_rz])
                        split_row(rz[:, 0, :], 0)
                        bcast_row(rzb[:, 0, :], rzb[:, 1, :])
                        k.op("dve", lambda e: e.scalar_tensor_tensor(out=aoT[:, h, :], in0=oT[:], scalar=sg_col[:, 0:1], in1=bcs[:], op0=ALU.mult, op1=ALU.mult),
                             reads=[B_oT, B_sg, B_bcs], writes=[B_aoT])

                    eps_t = sb(st, "eps_t", [128, 1]); B_eps = Buf()
                    k.op("dve", lambda e: e.memset(eps_t[:], 1e-5), writes=[B_eps])
                    for qg in range(4):
                        Q0 = qg * 512
                        steps = [(h, c, b) for h in range(8) for c in range(2) for b in range(NTILE)]
                        je_next = qk_exp(Q0, *steps[0])
                        pending = []
                        for si, (h, c, b) in enumerate(steps):
                            je_cur = je_next
                            if si + 1 < len(steps):
                                je_next = qk_exp(Q0, *steps[si + 1])
                            pvz(h, c, b, je_cur)
                            if pending:
                                a_, kw_ = pending.pop(0)
                                k.op(*a_, **kw_)
                            if c == 1 and b == NTILE - 1:
                                while pending:
                                    a_, kw_ = pending.pop(0)
                                    k.op(*a_, **kw_)
                                orig_op = k.op
                                rec = []
                                k.op = lambda *a, rec=rec, **kw: rec.append((a, kw))
                                epilogue(h)
                                k.op = orig_op
                                pending = rec
                        while pending:
                            a_, kw_ = pending.pop(0)
                            k.op(*a_, **kw_)
                        for tt in range(4):
                            ti = qg * 4 + tt
                            T0 = ti * 128
                            i = ti % 2
                            k.dma("sp", xo[i][:], src_rows(ti, 1), writes=[B_xo[i]])
                            for hf in range(2):
                                for h in range(8):
                                    k.op("pe", lambda e, h=h, hf=hf, tt=tt: e.matmul(pS[hf][:], lhsT=aoT[:, h, tt * 128:(tt + 1) * 128], rhs=w_bf[:, h, hf * 512:(hf + 1) * 512],
                                                                                start=(h == 0), stop=(h == 7)), reads=[B_aoT, B_w], writes=[B_pS[hf]])
                            ln_epilogue(wk, [pS[0][:], pS[1][:]], [B_pS[0], B_pS[1]], xo[i], B_xo[i], g1, lng, lnb, x1_scr[T0:T0 + 128, :])
                    k.barrier()

        with ExitStack() as L0:
            catT = sb(L0, "catT", [128, 8, NT], BF16); B_catT = Buf()
            LA = ExitStack()
            qT = sb(LA, "qT", [64, 8, NT], BF16); B_qT = Buf()
            kT = sb(LA, "kT", [64, 2, NT], BF16); B_kT = Buf()
            v_all = sb(LA, "v_all", [128, NTILE, 128], BF16); B_v = Buf()
            for st in phase("l0proj"):
                w_bf = sb(st, "w_in_bf", [128, 8, 1280], BF16); B_w = Buf()
                for kc in range(8):
                    k.dma("pool", w_bf[:, kc, :], w_in0[kc * 128:(kc + 1) * 128, :], writes=[B_w])
                sc1p = [None, None]; sh1 = [None, None]
                for r in range(2):
                    sh1[r] = mod_bc(st, f"sh1_{r}", 0, r, 0)
                    sc1p[r] = mod_bc(st, f"sc1p_{r}", 0, r, 1, plus1=True)
                xt = [sb(st, f"xt{i}", [128, D]) for i in range(2)]; B_xt = [Buf(), Buf()]
                tmpf = [sb(st, f"tmpf{i}", [128, D]) for i in range(2)]; B_tmpf = [Buf(), Buf()]
                h_bf = [sb(st, f"h_bf{i}", [128, D], BF16) for i in range(2)]; B_hbf = [Buf(), Buf()]
                hT = [sb(st, f"hT{i}", [128, 8, 128], BF16) for i in range(2)]; B_hT = [Buf(), Buf()]
                rt = [sb(st, f"rt{i}", [128, 64]) for i in range(2)]; B_rt = [Buf(), Buf()]
                t1 = [sb(st, f"rope_t1{i}", [128, 640]) for i in range(2)]; t2 = [sb(st, f"rope_t2{i}", [128, 640]) for i in range(2)]; B_rtmp = [Buf(), Buf()]
                qk_bf = [sb(st, f"qk_bf{i}", [128, 640], BF16) for i in range(2)]; B_qk = [Buf(), Buf()]
                ut = [sb(st, f"ut{i}", [128, 512]) for i in range(2)]; B_ut = [Buf(), Buf()]
                pA = [ps(st, f"pA{i}", [128, 8, 128], BF16) for i in range(2)]; B_pA = [Buf(), Buf()]
                pp = [[ps(st, f"pp{i}{j}", [128, 512]) for j in range(3)] for i in range(2)]; B_pp = [[Buf(), Buf(), Buf()], [Buf(), Buf(), Buf()]]

                def proj0_tile(ti):
                    i = ti % 2
                    r = 0 if ti < NLT else 1
                    T0 = ti * 128
                    ppi = pp[i]; Bppi = B_pp[i]
                    k.dma("sp", xt[i][:], src_rows(ti, 0), writes=[B_xt[i]])
                    if r == 0:
                        k.dma("sp", rt[i][:], rope_cs[T0:T0 + 128, :], writes=[B_rt[i]])
                    k.op("dve", lambda e: e.tensor_tensor(out=tmpf[i][:], in0=xt[i][:], in1=sc1p[r][0][:], op=ALU.mult),
                         reads=[B_xt[i], sc1p[r][1]], writes=[B_tmpf[i]])
                    k.op("pool", lambda e: e.tensor_tensor(out=h_bf[i][:], in0=tmpf[i][:], in1=sh1[r][0][:], op=ALU.add),
                         reads=[B_tmpf[i], sh1[r][1]], writes=[B_hbf[i]])
                    for kc in range(8):
                        k.op("pe", lambda e, kc=kc: e.transpose(out=pA[i][:, kc, :], in_=h_bf[i][:, kc * 128:(kc + 1) * 128], identity=ident_b[:]),
                             reads=[B_hbf[i], B_ident_b], writes=[B_pA[i]])
                    k.op("act", lambda e: e.copy(out=hT[i][:], in_=pA[i][:]), reads=[B_pA[i]], writes=[B_hT[i]])
                    for nb, (c0, c1) in enumerate(((0, 512), (512, 1024), (1024, 1280))):
                        for kc in range(8):
                            k.op("pe", lambda e, kc=kc, nb=nb, c0=c0, c1=c1: e.matmul(
                                ppi[nb][:, 0:c1 - c0], lhsT=hT[i][:, kc, :], rhs=w_bf[:, kc, c0:c1],
                                start=(kc == 0), stop=(kc == 7)),
                                reads=[B_hT[i], B_w], writes=[Bppi[nb]])
                    if r == 0:
                        rope_apply(None, ppi[0][:, 0:512], Bppi[0], qk_bf[i][:, 0:512], B_qk[i], 8, rt[i], B_rt[i], t1[i][:, 0:512], t2[i][:, 0:512], B_rtmp[i])
                        rope_apply(None, ppi[1][:, 0:128], Bppi[1], qk_bf[i][:, 512:640], B_qk[i], 2, rt[i], B_rt[i], t1[i][:, 512:640], t2[i][:, 512:640], B_rtmp[i])
                    else:
                        k.op("dve", lambda e: e.tensor_copy(out=qk_bf[i][:, 0:512], in_=ppi[0][:, 0:512]), reads=[Bppi[0]], writes=[B_qk[i]])
                        k.op("dve", lambda e: e.tensor_copy(out=qk_bf[i][:, 512:640], in_=ppi[1][:, 0:128]), reads=[Bppi[1]], writes=[B_qk[i]])
                    for h in range(8):
                        k.op("pe", lambda e, h=h: e.transpose(out=pA[i][0:64, h, :], in_=qk_bf[i][:, h * 64:(h + 1) * 64], identity=ident_b[:]),
                             reads=[B_qk[i], B_ident_b], writes=[B_pA[i]])
                    k.op("act", lambda e: e.copy(out=qT[:, :, T0:T0 + 128], in_=pA[i][0:64, :, :]), reads=[B_pA[i]], writes=[B_qT])
                    for h in range(2):
                        k.op("pe", lambda e, h=h: e.transpose(out=pA[i][0:64, h, :], in_=qk_bf[i][:, 512 + h * 64:512 + (h + 1) * 64], identity=ident_b[:]),
                             reads=[B_qk[i], B_ident_b], writes=[B_pA[i]])
                    k.op("act", lambda e: e.copy(out=kT[:, :, T0:T0 + 128], in_=pA[i][0:64, 0:2, :]), reads=[B_pA[i]], writes=[B_kT])
                    k.op("act", lambda e: e.copy(out=v_all[:, ti, :], in_=ppi[1][:, 128:256]), reads=[Bppi[1]], writes=[B_v])
                    k.op("act", lambda e: e.copy(out=ut[i][:, 0:256], in_=ppi[1][:, 256:512]), reads=[Bppi[1]], writes=[B_ut[i]])
                    k.op("act", lambda e: e.copy(out=ut[i][:, 256:512], in_=ppi[2][:, 0:256]), reads=[Bppi[2]], writes=[B_ut[i]])
                    urow = T0 + CTX if r == 0 else T0 - SEQ
                    k.dma("sp", u_scr[urow:urow + 128, :], ut[i][:], reads=[B_ut[i]])

                for t2_ in range(0, NTILE, 2):
                    replay([record(lambda ti=ti: proj0_tile(ti)) for ti in range(t2_, min(t2_ + 2, NTILE))])
                if "dbg_qT" in dbg:
                    dbg_qT = dscr("dbg_qT", [64, 8, NT], BF16)
                    k.dma("sp", dbg_qT, qT[:], reads=[B_qT])
                k.barrier()


            for st in phase("l0att"):
                SC = 0.125
                maskL = sb(st, "maskL_sb", [128, 128]); maskR = sb(st, "maskR_sb", [128, 128]); B_mask = Buf()
                k.dma("sp", maskL[:], maskL_in[:, :], writes=[B_mask])
                k.dma("sp", maskR[:], maskR_in[:, :], writes=[B_mask])
                sink_bc, B_sink = load_bc(st, "sink_bc", swa_sink[0:1, :], 8)
                sm = [sb(st, f"sm{i}", [128, 640]) for i in range(2)]; B_sm = [Buf(), Buf()]
                P = [sb(st, f"P{i}", [128, 640], BF16) for i in range(2)]; B_P = [Buf(), Buf()]
                PT = [sb(st, f"PT{i}", [128, 5, 128], BF16) for i in range(2)]; B_PT = [Buf(), Buf()]
                stat = [sb(st, f"stat{i}", [128, 8]) for i in range(2)]; B_stat = [Buf(), Buf()]
                att_bf = sb(st, "att_bf", [128, 512], BF16); B_att = Buf()
                ps_loc = [ps(st, f"ps_loc{i}", [128, 512]) for i in range(2)]; B_psl = [Buf(), Buf()]
                ps_ctx = [ps(st, f"ps_ctx{i}", [128, 512]) for i in range(2)]; B_psc = [Buf(), Buf()]
                pPT = [ps(st, f"pPT{c_}", [128, 8, 128], BF16) for c_ in range(2)]; B_pPT = [Buf(), Buf()]
                po2 = [ps(st, f"po{c_}", [128, 512]) for c_ in range(2)]; B_poh = [Buf() for _ in range(8)]
                pcat = pPT[0]; B_pcat = B_pPT[0]
                it = 0
                for ti in range(NTILE):
                    T0 = ti * 128
                    lat = ti < NLT
                    if lat:
                        j0 = max(0, ti - 1); j1 = min(NLT - 1, ti + 1)
                        nloc = (j1 - j0 + 1) * 128
                        blocks = list(range(j0, j1 + 1)) + [NLT, NLT + 1]
                    else:
                        nloc = 0
                        blocks = [NLT, NLT + 1]
                    n = nloc + 256
                    def head_chain(h, i):
                        kvh = h // 4
                        if lat:
                            k.op("pe", lambda e, i=i, h=h, kvh=kvh, j0=j0, nloc=nloc, T0=T0: e.matmul(
                                ps_loc[i][:, 0:nloc], lhsT=qT[:, h, T0:T0 + 128], rhs=kT[:, kvh, j0 * 128:j0 * 128 + nloc],
                                start=True, stop=True), reads=[B_qT, B_kT], writes=[B_psl[i]])
                        k.op("pe", lambda e, i=i, h=h, kvh=kvh, T0=T0: e.matmul(
                            ps_ctx[i][:, 0:256], lhsT=qT[:, h, T0:T0 + 128], rhs=kT[:, kvh, SEQ:NT],
                            start=True, stop=True), reads=[B_qT, B_kT], writes=[B_psc[i]])
                        if lat:
                            for bi, j in enumerate(range(j0, j1 + 1)):
                                sl = slice(bi * 128, (bi + 1) * 128)
                                if j == ti:
                                    k.op("act", lambda e, i=i, sl=sl: e.mul(out=sm[i][:, sl], in_=ps_loc[i][:, sl], mul=SC),
                                         reads=[B_psl[i]], writes=[B_sm[i]])
                                else:
                                    mk = maskL if j < ti else maskR
                                    k.op("dve", lambda e, i=i, sl=sl, mk=mk: e.scalar_tensor_tensor(
                                        out=sm[i][:, sl], in0=ps_loc[i][:, sl], scalar=SC, in1=mk[:], op0=ALU.mult, op1=ALU.add),
                                        reads=[B_psl[i], B_mask], writes=[B_sm[i]])
                        k.op("act", lambda e, i=i, nloc=nloc: e.mul(out=sm[i][:, nloc:nloc + 256], in_=ps_ctx[i][:, 0:256], mul=SC),
                             reads=[B_psc[i]], writes=[B_sm[i]])
                        sti = stat[i]
                        k.op("dve", lambda e, i=i, n=n, sti=sti: e.reduce_max(out=sti[:, 0:1], in_=sm[i][:, 0:n], axis=AX.X),
                             reads=[B_sm[i]], writes=[B_stat[i]])
                        k.op("dve", lambda e, sti=sti, h=h: e.tensor_tensor(out=sti[:, 1:2], in0=sti[:, 0:1], in1=sink_bc[:, h:h + 1], op=ALU.max),
                             reads=[B_stat[i], B_sink], writes=[B_stat[i]])
                        k.op("dve", lambda e, sti=sti: e.tensor_scalar(out=sti[:, 2:3], in0=sti[:, 1:2], scalar1=-1.0, scalar2=None, op0=ALU.mult),
                             reads=[B_stat[i]], writes=[B_stat[i]])
                        k.op("act", lambda e, i=i, n=n, sti=sti: e.activation(out=P[i][:, 0:n], in_=sm[i][:, 0:n], func=AF.Exp,
                                                                             bias=sti[:, 2:3], scale=1.0, accum_out=sti[:, 3:4]),
                             reads=[B_sm[i], B_stat[i]], writes=[B_P[i], B_stat[i]])
                        k.op("act", lambda e, sti=sti, h=h: e.activation(out=sti[:, 4:5], in_=sink_bc[:, h:h + 1], func=AF.Exp,
                                                                        bias=sti[:, 2:3], scale=1.0),
                             reads=[B_sink, B_stat[i]], writes=[B_stat[i]])
                        k.op("dve", lambda e, sti=sti: e.tensor_tensor(out=sti[:, 5:6], in0=sti[:, 3:4], in1=sti[:, 4:5], op=ALU.add),
                             reads=[B_stat[i]], writes=[B_stat[i]])
                        k.op("dve", lambda e, sti=sti: e.reciprocal(out=sti[:, 6:7], in_=sti[:, 5:6]),
                             reads=[B_stat[i]], writes=[B_stat[i]])
                        nb = n // 128
                        for b in range(nb):
                            k.op("pe", lambda e, i=i, b=b: e.transpose(out=pPT[i][:, b, :], in_=P[i][:, b * 128:(b + 1) * 128], identity=ident_b[:]),
                                 reads=[B_P[i], B_ident_b], writes=[B_pPT[i]])
                        k.op("pool" if False else "dve", lambda e, i=i, nb=nb: e.tensor_copy(out=PT[i][:, 0:nb, :], in_=pPT[i][:, 0:nb, :]),
                             reads=[B_pPT[i]], writes=[B_PT[i]])
                        for b in range(nb):
                            k.op("pe", lambda e, i=i, b=b, h=h, kvh=kvh, vb=blocks[b], nb=nb: e.matmul(
                                po2[i][:, h * 64:(h + 1) * 64], lhsT=PT[i][:, b, :], rhs=v_all[:, vb, kvh * 64:(kvh + 1) * 64],
                                start=(b == 0), stop=(b == nb - 1)), reads=[B_PT[i], B_v], writes=[B_poh[h]])
                        k.op("dve", lambda e, h=h, sti=sti: e.tensor_scalar(out=att_bf[:, h * 64:(h + 1) * 64], in0=po2[i][:, h * 64:(h + 1) * 64],
                                                                           scalar1=sti[:, 6:7], scalar2=None, op0=ALU.mult),
                             reads=[B_poh[h], B_stat[i]], writes=[B_att])

                    pair_ = []
                    for h in range(8):
                        i = it % 2; it += 1
                        pair_.append(record(lambda h=h, i=i: head_chain(h, i)))
                        if len(pair_) == 2:
                            replay(pair_)
                            pair_ = []
                    for cb in range(4):
                        k.op("pe", lambda e, cb=cb: e.transpose(out=pcat[:, cb, :], in_=att_bf[:, cb * 128:(cb + 1) * 128], identity=ident_b[:]),
                             reads=[B_att, B_ident_b], writes=[B_pcat])
                    k.op("act", lambda e, T0=T0: e.copy(out=catT[:, 0:4, T0:T0 + 128], in_=pcat[:, 0:4, :]), reads=[B_pcat], writes=[B_catT])
                    if "dbg_att" in dbg:
                        if ti == 0:
                            dbg_att = dscr("dbg_att", [NT, 512], BF16)
                        k.dma("sp", dbg_att[T0:T0 + 128, :], att_bf[:], reads=[B_att])
                k.barrier()


            k.barrier()
            LA.close()
            for st in phase("l0ssm"):
                ssm_phase(st, catT, B_catT)
            for st in phase("l0ssmpost"):
                ssm_post(st, catT, B_catT)

            for st in phase("l0out"):
                outproj_ln1(st, 0, catT, B_catT, w_out0, NTILE)


        for st in phase("moe0"):
            moe_phase(st, 0, NTILE, x2_scr)


        layer1_mixer()
        for st in phase("moe1"):
            moe_phase(st, 1, NLT, out)

        k.barrier()
    return nc


_CONSTS = None


def _consts():
    global _CONSTS
    if _CONSTS is None:
        t = np.arange(SEQ)
        row = (t // 64).astype(np.float32)
        col = (t % 64).astype(np.float32)
        inv = (10000.0 ** (-np.arange(16, dtype=np.float32) / 16)).astype(np.float32)
        ar = row[:, None] * inv[None, :]
        ac = col[:, None] * inv[None, :]
        rope = np.concatenate([np.cos(ar), np.sin(ar), np.cos(ac), np.sin(ac)], 1).astype(np.float32)
        qi = np.arange(128)[:, None]; kj = np.arange(128)[None, :]
        mL = np.where(kj >= qi, 0.0, -30000.0).astype(np.float32)
        mR = np.where(kj <= qi, 0.0, -30000.0).astype(np.float32)
        _CONSTS = {"rope_cs": rope, "ident": np.eye(128, dtype=np.float32), "maskL": mL, "maskR": mR}
        selm = np.zeros((32, 32, 128), np.float32)
        for e_ in range(32):
            selm[e_, e_, :] = 1.0
        _CONSTS["sel"] = selm
        _CONSTS["kval"] = np.ascontiguousarray(np.broadcast_to(np.repeat(np.arange(-7, 9, dtype=np.float32), 64)[None, :], (64, 1024)))
        _CONSTS["mrow"] = np.ascontiguousarray(np.broadcast_to(np.arange(288, dtype=np.float32)[None, :], (64, 288)))
        jj = np.arange(128) // 16
        _CONSTS["maskF"] = (jj[None, :] >= jj[:, None]).astype(np.float32)
        _CONSTS["maskB"] = (jj[None, :] <= jj[:, None]).astype(np.float32)
    return _CONSTS


def make_in_maps(inputs, cores):
    f = lambda a: np.ascontiguousarray(np.asarray(a, dtype=np.float32))
    shared = {}
    for name in ("mod_w", "mod_b", "ln1_g", "ln1_b", "ln2_g", "ln2_b", "swa_sink",
                 "ssm_d", "ssm_glu_b", "dif_lam_q1", "dif_lam_k1", "dif_lam_q2", "dif_lam_k2",
                 "dif_subln_g", "moe_wg", "moe_bg", "moe_we", "moe_w1", "moe_w3", "moe_w2"):
        shared[name] = f(inputs[name])
    for name in ("swa_ssm_w_in", "swa_ssm_w_out", "ssm_a_re", "ssm_a_im", "ssm_log_step",
                 "ssm_b_re", "ssm_b_im", "ssm_c_re", "ssm_c_im", "ssm_glu_w", "dif_w_in", "dif_w_out"):
        shared[name] = f(inputs[name])[0]
    shared["moe_be"] = f(inputs["moe_be"]).reshape(2, 32)
    shared["c_ctx"] = f(inputs["c_ctx"]).reshape(1, D)
    shared.update(_consts())
    maps = []
    for b in cores:
        m = dict(shared)
        m["x"] = f(inputs["x"][b])
        m["ctx"] = f(inputs["ctx"][b])
        m["c"] = f(inputs["c"][b]).reshape(1, D)
        maps.append(m)
    return maps


def kernel(**inputs):
    nc = build()
    maps = make_in_maps(inputs, range(8))
    res = run_bass_kernel_spmd(nc, maps, core_ids=list(range(8)))
    return np.stack([r["out"] for r in res.results], 0).astype(np.float32)
```

```python
import math
from contextlib import ExitStack

import numpy as np
import concourse.bass as bass
import concourse.mybir as mybir
from concourse.bass_utils import run_bass_kernel_spmd

F32 = mybir.dt.float32
BF16 = mybir.dt.bfloat16
AF = mybir.ActivationFunctionType
ALU = mybir.AluOpType
AX = mybir.AxisListType

D = 1024
SEQ = 2048
CTX = 256
NT = SEQ + CTX
NTILE = NT // 128
NLT = SEQ // 128
ALPHA = 4 ** 0.25
LN_EPS = 1e-5


class Buf:
    __slots__ = ("w", "r")

    def __init__(self):
        self.w = None
        self.r = {}


class EngState:
    def __init__(self, name, eng, sem):
        self.name = name
        self.eng = eng
        self.sem = sem
        self.count = 0
        self.waited = {}
        self.slots = []
        self.slot_i = 0


class K:
    def __init__(self, nc, stack):
        self.nc = nc
        self.E = {}
        for name, eng in (("pe", nc.tensor), ("dve", nc.vector), ("act", nc.scalar),
                          ("pool", nc.gpsimd), ("sp", nc.sync)):
            sem = stack.enter_context(nc.semaphore("s_" + name))
            self.E[name] = EngState(name, eng, sem)
        self.semkey = {}
        for qn, n in (("sp", 12), ("pool", 12), ("act", 6)):
            for i in range(n):
                sem = stack.enter_context(nc.semaphore(f"d_{qn}{i}"))
                self.E[qn].slots.append([sem, 0])
        self.uid = 0

    def _key(self, sem):
        return id(sem)

    def _wait(self, E, deps, skip_self=False):
        best = {}
        for sem, val in deps:
            if skip_self and sem is E.sem:
                continue
            k = id(sem)
            if k not in best or best[k][1] < val:
                best[k] = (sem, val)
        for k, (sem, val) in best.items():
            if E.waited.get(k, 0) < val:
                E.eng.wait_ge(sem, val)
                E.waited[k] = val

    def _deps(self, reads, writes):
        deps = []
        for b in reads:
            if b.w is not None:
                deps.append(b.w)
        for b in writes:
            if b.w is not None:
                deps.append(b.w)
            deps.extend(b.r.values())
        return deps

    def _mark(self, tok, reads, writes):
        sem, val = tok
        for b in reads:
            b.r[id(sem)] = tok
        for b in writes:
            b.w = tok
            b.r = {}

    def op(self, en, fn, reads=(), writes=()):
        E = self.E[en]
        self._wait(E, self._deps(reads, writes), skip_self=(en == "pe"))
        ins = fn(E.eng)
        E.count += 1
        ins.then_inc(E.sem, 1)
        tok = (E.sem, E.count)
        self._mark(tok, reads, writes)
        return tok

    def dma(self, qn, out, in_, reads=(), writes=(), **kw):
        E = self.E[qn]
        self._wait(E, self._deps(reads, writes))
        slot = E.slots[E.slot_i % len(E.slots)]
        E.slot_i += 1
        if slot[1] > 0:
            self._wait(E, [(slot[0], slot[1] * 16)])
        ins = E.eng.dma_start(out=out, in_=in_, **kw)
        slot[1] += 1
        ins.then_inc(slot[0], 16)
        tok = (slot[0], slot[1] * 16)
        self._mark(tok, reads, writes)
        return tok

    def all_tokens(self):
        toks = []
        for E in self.E.values():
            if E.count:
                toks.append((E.sem, E.count))
            for sem, c in E.slots:
                if c:
                    toks.append((sem, c * 16))
        return toks

    def barrier(self):
        toks = self.all_tokens()
        for E in self.E.values():
            self._wait(E, toks, skip_self=False)


def build(dbg=(), inject=(), phases=None):
    nc = bass.Bass("TRN2", target_bir_lowering=False)
    dbg = set(dbg)
    inject = set(inject)
    ALLP = {"mod", "l0proj", "l0att", "l0ssm", "l0ssmpost", "l0out", "moe0", "l1proj", "l1att", "moe1"}
    phases = ALLP if phases is None else set(phases)

    def din(name, shape):
        return nc.dram_tensor(name, list(shape), F32, kind="ExternalInput").ap()

    def dscr(name, shape, dt=F32):
        kind = "ExternalOutput" if name in dbg else ("ExternalInput" if name in inject else "Internal")
        return nc.dram_tensor(name, list(shape), dt, kind=kind).ap()

    x_in = din("x", [SEQ, D])
    ctx_in = din("ctx", [CTX, D])
    c_in = din("c", [1, D])
    cc_in = din("c_ctx", [1, D])
    mod_w = din("mod_w", [2, D, 6 * D])
    mod_b = din("mod_b", [2, 6 * D])
    ln1_g = din("ln1_g", [2, D]); ln1_b = din("ln1_b", [2, D])
    ln2_g = din("ln2_g", [2, D]); ln2_b = din("ln2_b", [2, D])
    w_in0 = din("swa_ssm_w_in", [D, 1280])
    w_out0 = din("swa_ssm_w_out", [D, D])
    swa_sink = din("swa_sink", [1, 8])
    a_re = din("ssm_a_re", [2, 32, 64]); a_im = din("ssm_a_im", [2, 32, 64])
    log_step = din("ssm_log_step", [2, 32])
    b_re = din("ssm_b_re", [2, 32, 64, 16]); b_im = din("ssm_b_im", [2, 32, 64, 16])
    c_re = din("ssm_c_re", [2, 32, 16, 64]); c_im = din("ssm_c_im", [2, 32, 16, 64])
    ssm_d = din("ssm_d", [1, 512])
    glu_w = din("ssm_glu_w", [512, 512]); glu_b = din("ssm_glu_b", [1, 512])
    dif_w_in = din("dif_w_in", [D, 3072]); dif_w_out = din("dif_w_out", [D, D])
    lam_q1 = din("dif_lam_q1", [1, 64]); lam_k1 = din("dif_lam_k1", [1, 64])
    lam_q2 = din("dif_lam_q2", [1, 64]); lam_k2 = din("dif_lam_k2", [1, 64])
    subln_g = din("dif_subln_g", [1, 128])
    moe_wg = din("moe_wg", [2, D, 4]); moe_bg = din("moe_bg", [2, 4])
    moe_we = din("moe_we", [2, 4, D, 8]); moe_be = din("moe_be", [2, 32])
    moe_w1 = din("moe_w1", [2, 32, D, 256]); moe_w3 = din("moe_w3", [2, 32, D, 256])
    moe_w2 = din("moe_w2", [2, 32, 256, D])
    rope_cs = din("rope_cs", [SEQ, 64])
    ident_in = din("ident", [128, 128])
    sel_in = din("sel", [32, 32, 128])
    kval_in = din("kval", [64, 1024]); mrow_in = din("mrow", [64, 288])
    maskF_in = din("maskF", [128, 128]); maskB_in = din("maskB", [128, 128])
    maskL_in = din("maskL", [128, 128]); maskR_in = din("maskR", [128, 128])
    out = nc.dram_tensor("out", [SEQ, D], F32, kind="ExternalOutput").ap()

    modrow = dscr("modrow", [2, 2, 6 * D])

    with ExitStack() as gs:
        k = K(nc, gs)

        def sb(st, name, shape, dt=F32):
            k.uid += 1
            return st.enter_context(nc.sbuf_tensor(f"sb{k.uid}_{name}", list(shape), dt))

        def ps(st, name, shape, dt=F32):
            k.uid += 1
            return st.enter_context(nc.psum_tensor(f"ps{k.uid}_{name}", list(shape), dt))

        def phase(name):
            if name in phases:
                with ExitStack() as st_:
                    yield st_

        def record(fn):
            rec = []
            oo, od = k.op, k.dma
            k.op = lambda *a, **kw: rec.append(("op", a, kw))
            k.dma = lambda *a, **kw: rec.append(("dma", a, kw))
            try:
                fn()
            finally:
                k.op, k.dma = oo, od
            return rec

        def replay(recs):
            for ix in range(max(len(r_) for r_ in recs)):
                for r_ in recs:
                    if ix < len(r_):
                        kind, a_, kw_ = r_[ix]
                        (k.op if kind == "op" else k.dma)(*a_, **kw_)

        ident_f = sb(gs, "ident_f", [128, 128]); B_ident_f = Buf()
        ident_b = sb(gs, "ident_b", [128, 128], BF16); B_ident_b = Buf()
        k.dma("sp", ident_f[:], ident_in[:, :], writes=[B_ident_f])
        k.op("dve", lambda e: e.tensor_copy(out=ident_b[:], in_=ident_f[:]),
             reads=[B_ident_f], writes=[B_ident_b])

        for st in phase("mod"):
            cT = sb(st, "cT", [128, 8, 2]); B_cT = Buf()
            with nc.allow_non_contiguous_dma(reason="tiny column loads"):
                k.dma("sp", cT[:, :, 0], c_in[0, :].rearrange("(k p) -> p k", p=128), writes=[B_cT])
                k.dma("sp", cT[:, :, 1], cc_in[0, :].rearrange("(k p) -> p k", p=128), writes=[B_cT])
            sT = sb(st, "sT", [128, 8, 2]); B_sT = Buf()
            k.op("act", lambda e: e.activation(out=sT[:], in_=cT[:], func=AF.Silu),
                 reads=[B_cT], writes=[B_sT])
            wt = [sb(st, f"modw{i}", [128, 8, 512]) for i in range(2)]
            B_wt = [Buf(), Buf()]
            mb = sb(st, "modb", [2, 6 * D]); B_mb = Buf()
            mrow = sb(st, "mrow", [2, 6 * D]); B_mrow = Buf()
            pm = [ps(st, f"pmod{i}", [2, 512]) for i in range(2)]
            B_pm = [Buf(), Buf()]
            it = 0
            for l in range(2):
                k.dma("sp", mb[0:1, :], mod_b[l:l + 1, :], writes=[B_mb])
                k.dma("sp", mb[1:2, :], mod_b[l:l + 1, :], writes=[B_mb])
                for cb in range(12):
                    i = it % 2
                    it += 1
                    k.dma("sp" if cb % 2 == 0 else "act", wt[i][:],
                          mod_w[l, :, cb * 512:(cb + 1) * 512].rearrange("(k p) n -> p k n", p=128),
                          writes=[B_wt[i]])
                    for kc in range(8):
                        k.op("pe", lambda e, kc=kc, i=i: e.matmul(
                            pm[i][:], lhsT=sT[:, kc, :], rhs=wt[i][:, kc, :],
                            start=(kc == 0), stop=(kc == 7)),
                            reads=[B_sT, B_wt[i]], writes=[B_pm[i]])
                    k.op("dve", lambda e, i=i, cb=cb: e.tensor_tensor(
                        out=mrow[:, cb * 512:(cb + 1) * 512], in0=pm[i][:],
                        in1=mb[:, cb * 512:(cb + 1) * 512], op=ALU.add),
                        reads=[B_pm[i], B_mb], writes=[B_mrow])
                k.dma("sp", modrow[l], mrow[:], reads=[B_mrow], writes=[])
            k.barrier()


        def load_bc(st, name, src_row_ap, n, q="sp"):
            t = sb(st, name, [128, n]); B = Buf()
            k.dma(q, t[:], src_row_ap.partition_broadcast(128), writes=[B])
            return t, B

        def mod_bc(st, name, l, r, chunk, plus1=False):
            t, B = load_bc(st, name, modrow[l, r:r + 1, chunk * D:(chunk + 1) * D], D)
            if plus1:
                k.op("pool", lambda e: e.tensor_scalar(out=t[:], in0=t[:], scalar1=1.0, scalar2=None,
                                                       op0=ALU.add), reads=[B], writes=[B])
            return t, B

        u_scr = dscr("u_scr", [NT, 512])
        y_scr = dscr("y_scr", [NT, 512], BF16)
        x1_scr = dscr("x1_scr", [NT, D])
        x2_scr = dscr("x2_scr", [NT, D])
        dbg_q = dscr("dbg_q", [NT, 1280])

        def src_rows(ti, l):
            if l == 0:
                return x_in[ti * 128:(ti + 1) * 128, :] if ti < NLT else ctx_in[(ti - NLT) * 128:(ti - NLT + 1) * 128, :]
            return x2_scr[ti * 128:(ti + 1) * 128, :]

        def rope_apply(st_bufs, src_ps, B_src, dst, B_dst, nh, rt, B_rt, tmp1, tmp2, B_tmp):
            S = src_ps.rearrange("p (h a b f) -> p h a b f", h=nh, a=2, b=2, f=16)
            O = dst.rearrange("p (h a b f) -> p h a b f", h=nh, a=2, b=2, f=16)
            T1 = tmp1.rearrange("p (h a b f) -> p h a b f", h=nh, a=2, b=2, f=16)
            T2 = tmp2.rearrange("p (h a b f) -> p h a b f", h=nh, a=2, b=2, f=16)
            for a in range(2):
                cos = rt[:, a * 32:a * 32 + 16].rearrange("p (x y f) -> p x y f", x=1, y=1).to_broadcast([128, nh, 2, 16])
                sin = rt[:, a * 32 + 16:a * 32 + 32].rearrange("p (x y f) -> p x y f", x=1, y=1).to_broadcast([128, nh, 2, 16])
                k.op("dve", lambda e, a=a, cos=cos: e.tensor_tensor(out=T1[:, :, a], in0=S[:, :, a], in1=cos, op=ALU.mult),
                     reads=[B_src, B_rt], writes=[B_tmp])
                k.op("dve", lambda e, a=a, sin=sin: e.tensor_tensor(out=T2[:, :, a], in0=S[:, :, a, ::-1, :], in1=sin, op=ALU.mult),
                     reads=[B_src, B_rt], writes=[B_tmp])
                k.op("dve", lambda e, a=a: e.tensor_tensor(out=O[:, :, a, 0, :], in0=T1[:, :, a, 0, :], in1=T2[:, :, a, 0, :], op=ALU.subtract),
                     reads=[B_tmp], writes=[B_dst])
                k.op("dve", lambda e, a=a: e.tensor_tensor(out=O[:, :, a, 1, :], in0=T1[:, :, a, 1, :], in1=T2[:, :, a, 1, :], op=ALU.add),
                     reads=[B_tmp], writes=[B_dst])


        def ln_epilogue(wk, y_parts, B_y, xo, B_xo, g_t, lng_t, lnb_t, dst_rows):
            tmp, B_tmp, z, B_z, stt, B_stt, o, B_o = wk
            for hf in range(2):
                sl = slice(hf * 512, (hf + 1) * 512)
                k.op("dve", lambda e, hf=hf, sl=sl: e.tensor_tensor(out=tmp[:, sl], in0=y_parts[hf], in1=g_t[0][:, sl], op=ALU.mult),
                     reads=[B_y[hf], g_t[1]], writes=[B_tmp])
            k.op("dve", lambda e: e.scalar_tensor_tensor(out=z[:], in0=xo[:], scalar=ALPHA, in1=tmp[:], op0=ALU.mult, op1=ALU.add),
                 reads=[B_xo, B_tmp], writes=[B_z])
            for hf in range(2):
                k.op("dve", lambda e, hf=hf: e.bn_stats(out=stt[:, hf * 6:(hf + 1) * 6], in_=z[:, hf * 512:(hf + 1) * 512]),
                     reads=[B_z], writes=[B_stt])
            k.op("dve", lambda e: e.bn_aggr(out=stt[:, 12:14], in_=stt[:, 0:12]), reads=[B_stt], writes=[B_stt])
            k.op("dve", lambda e: e.tensor_scalar(out=stt[:, 15:16], in0=stt[:, 13:14], scalar1=LN_EPS, scalar2=None, op0=ALU.add),
                 reads=[B_stt], writes=[B_stt])
            k.op("act", lambda e: e.sqrt(out=stt[:, 15:16], in_=stt[:, 15:16]), reads=[B_stt], writes=[B_stt])
            k.op("dve", lambda e: e.reciprocal(out=stt[:, 14:15], in_=stt[:, 15:16]), reads=[B_stt], writes=[B_stt])
            k.op("dve", lambda e: e.tensor_scalar(out=tmp[:], in0=z[:], scalar1=stt[:, 12:13], scalar2=stt[:, 14:15], op0=ALU.subtract, op1=ALU.mult),
                 reads=[B_z, B_stt], writes=[B_tmp])
            k.op("pool", lambda e: e.tensor_tensor(out=o[:], in0=tmp[:], in1=lng_t[0][:], op=ALU.mult),
                 reads=[B_tmp, lng_t[1]], writes=[B_o])
            k.op("pool", lambda e: e.tensor_tensor(out=o[:], in0=o[:], in1=lnb_t[0][:], op=ALU.add),
                 reads=[B_o, lnb_t[1]], writes=[B_o])
            k.dma("sp", dst_rows, o[:], reads=[B_o])

        def ln_work(st, pfx):
            tmp = sb(st, pfx + "_tmp", [128, D]); z = sb(st, pfx + "_z", [128, D])
            stt = sb(st, pfx + "_stt", [128, 16]); o = sb(st, pfx + "_o", [128, D])
            return (tmp, Buf(), z, Buf(), stt, Buf(), o, Buf())

        def outproj_ln1(st, l, catT_, B_catT_, w_out_dram, ntiles):
            w_bf = sb(st, "w_out_bf", [128, 8, D], BF16); B_w = Buf()
            for kc in range(8):
                k.dma("pool", w_bf[:, kc, :], w_out_dram[kc * 128:(kc + 1) * 128, :], writes=[B_w])
            g1 = [mod_bc(st, f"g1_{r}", l, r, 2) for r in range(2)]
            lng = load_bc(st, "ln1g", ln1_g[l:l + 1, :], D)
            lnb = load_bc(st, "ln1b", ln1_b[l:l + 1, :], D)
            wks = [ln_work(st, "e1a"), ln_work(st, "e1b")]
            xo = [sb(st, f"xo{i}", [128, D]) for i in range(2)]; B_xo = [Buf(), Buf()]
            py = [ps(st, f"py{i}", [128, 512]) for i in range(4)]; B_py = [Buf() for _ in range(4)]

            def out_tile(ti):
                i = ti % 2
                wk = wks[i]
                r = 0 if ti < NLT else 1
                T0 = ti * 128
                k.dma("sp", xo[i][:], src_rows(ti, l), writes=[B_xo[i]])
                for hf in range(2):
                    pi = i * 2 + hf
                    for kc in range(8):
                        k.op("pe", lambda e, kc=kc, hf=hf, pi=pi, T0=T0: e.matmul(
                            py[pi][:], lhsT=catT_[:, kc, T0:T0 + 128], rhs=w_bf[:, kc, hf * 512:(hf + 1) * 512],
                            start=(kc == 0), stop=(kc == 7)), reads=[B_catT_, B_w], writes=[B_py[pi]])
                ln_epilogue(wk, [py[i * 2][:], py[i * 2 + 1][:]], [B_py[i * 2], B_py[i * 2 + 1]], xo[i], B_xo[i],
                            g1[r], lng, lnb, x1_scr[T0:T0 + 128, :])

            for t2 in range(0, ntiles, 2):
                replay([record(lambda ti=ti: out_tile(ti)) for ti in range(t2, min(t2 + 2, ntiles))])
            k.barrier()

        def moe_phase(st, l, ntiles, dst):
            ntok = ntiles * 128
            h2T = sb(st, "h2T", [128, 8, ntok], BF16); B_h2T = Buf()
            gateT = sb(st, "gateT", [32, ntok], BF16); B_gateT = Buf()
            f_acc = sb(st, "f_acc", [128, ntiles, D]); B_facc = [Buf() for _ in range(ntiles)]
            sel = sb(st, "sel", [32, 32, 128], BF16); B_sel = Buf()
            k.dma("pool", sel[:], sel_in[:, :, :], writes=[B_sel])
            with ExitStack() as s1:
                Wr = sb(s1, "Wr", [128, 8, 36]); B_Wr = Buf()
                with nc.allow_non_contiguous_dma(reason="small router weights"):
                    k.dma("sp", Wr[:, :, 0:4], moe_wg[l].rearrange("(k p) n -> p k n", p=128), writes=[B_Wr])
                    for g in range(4):
                        k.dma("sp", Wr[:, :, 4 + g * 8:12 + g * 8], moe_we[l, g].rearrange("(k p) n -> p k n", p=128), writes=[B_Wr])
                Whi = sb(s1, "Whi", [128, 8, 36], BF16); Wlo = sb(s1, "Wlo", [128, 8, 36], BF16); B_Wsp = Buf()
                k.op("dve", lambda e: e.tensor_copy(out=Whi[:], in_=Wr[:]), reads=[B_Wr], writes=[B_Wsp])
                k.op("dve", lambda e: e.tensor_tensor(out=Wlo[:], in0=Wr[:], in1=Whi[:], op=ALU.subtract), reads=[B_Wr, B_Wsp], writes=[B_Wsp])
                rb = sb(s1, "rb", [128, 36]); B_rb = Buf()
                k.dma("sp", rb[:, 0:4], moe_bg[l:l + 1, :].partition_broadcast(128), writes=[B_rb])
                k.dma("sp", rb[:, 4:36], moe_be[l:l + 1, :].partition_broadcast(128), writes=[B_rb])
                sc2p = [mod_bc(s1, f"sc2p_{r}", l, r, 4, plus1=True) for r in range(2)]
                sh2 = [mod_bc(s1, f"sh2_{r}", l, r, 3) for r in range(2)]
                xt = [sb(s1, f"mx{i}", [128, D]) for i in range(2)]; B_xt = [Buf(), Buf()]
                hf32 = [sb(s1, f"mh{c_}", [128, D]) for c_ in range(2)]; B_h = [Buf(), Buf()]
                hhi = [sb(s1, f"hhi{c_}", [128, D], BF16) for c_ in range(2)]; hlo = [sb(s1, f"hlo{c_}", [128, D], BF16) for c_ in range(2)]; B_hs = [Buf(), Buf()]
                hloT = [sb(s1, f"hloT{c_}", [128, 8, 128], BF16) for c_ in range(2)]; B_hloT = [Buf(), Buf()]
                pTh = [ps(s1, f"mpTh{c_}", [128, 8, 128], BF16) for c_ in range(2)]; B_pTh = [Buf(), Buf()]
                pTl = [ps(s1, f"mpTl{c_}", [128, 8, 128], BF16) for c_ in range(2)]; B_pTl = [Buf(), Buf()]
                pr = [ps(s1, f"mpr{c_}", [128, 512]) for c_ in range(2)]; B_pr = [Buf(), Buf()]
                pg = [ps(s1, f"mpg{c_}", [32, 1024], BF16) for c_ in range(2)]; B_pg = [Buf(), Buf()]
                lg = [sb(s1, f"lg{c_}", [128, 36]) for c_ in range(2)]; B_lg = [Buf(), Buf()]
                sm = [sb(s1, f"rsm{c_}", [128, 160]) for c_ in range(2)]; B_sm = [Buf(), Buf()]
                gates = [sb(s1, f"gates{c_}", [128, 32]) for c_ in range(2)]; B_gates = [Buf(), Buf()]
                gates_bf = [sb(s1, f"gates_bf{c_}", [128, 32], BF16) for c_ in range(2)]; B_gbf = [Buf(), Buf()]
                def router_tile(ti):
                    i = ti % 2
                    ci = ti % 2
                    r = 0 if ti < NLT else 1
                    T0 = ti * 128
                    k.dma("sp", xt[i][:], x1_scr[T0:T0 + 128, :], writes=[B_xt[i]])
                    k.op("dve", lambda e, i=i, r=r: e.tensor_tensor(out=hf32[ci][:], in0=xt[i][:], in1=sc2p[r][0][:], op=ALU.mult),
                         reads=[B_xt[i], sc2p[r][1]], writes=[B_h[ci]])
                    k.op("pool", lambda e, r=r: e.tensor_tensor(out=hf32[ci][:], in0=hf32[ci][:], in1=sh2[r][0][:], op=ALU.add),
                         reads=[B_h[ci], sh2[r][1]], writes=[B_h[ci]])
                    k.op("pool", lambda e: e.tensor_copy(out=hhi[ci][:], in_=hf32[ci][:]), reads=[B_h[ci]], writes=[B_hs[ci]])
                    k.op("dve", lambda e: e.tensor_tensor(out=hlo[ci][:], in0=hf32[ci][:], in1=hhi[ci][:], op=ALU.subtract), reads=[B_h[ci], B_hs[ci]], writes=[B_hs[ci]])
                    for kc in range(8):
                        k.op("pe", lambda e, kc=kc: e.transpose(out=pTh[ci][:, kc, :], in_=hhi[ci][:, kc * 128:(kc + 1) * 128], identity=ident_b[:]),
                             reads=[B_hs[ci], B_ident_b], writes=[B_pTh[ci]])
                    for kc in range(8):
                        k.op("pe", lambda e, kc=kc: e.transpose(out=pTl[ci][:, kc, :], in_=hlo[ci][:, kc * 128:(kc + 1) * 128], identity=ident_b[:]),
                             reads=[B_hs[ci], B_ident_b], writes=[B_pTl[ci]])
                    k.op("act", lambda e, T0=T0: e.copy(out=h2T[:, :, T0:T0 + 128], in_=pTh[ci][:]), reads=[B_pTh[ci]], writes=[B_h2T])
                    k.op("dve", lambda e: e.tensor_copy(out=hloT[ci][:], in_=pTl[ci][:]), reads=[B_pTl[ci]], writes=[B_hloT[ci]])
                    n_mm = 24
                    j = 0
                    for (A, BA, W) in ((None, B_h2T, Whi), (hloT[ci], B_hloT[ci], Whi), (None, B_h2T, Wlo)):
                        for kc in range(8):
                            lhs = h2T[:, kc, T0:T0 + 128] if A is None else A[:, kc, :]
                            k.op("pe", lambda e, lhs=lhs, W=W, kc=kc, j=j: e.matmul(pr[ci][:, 0:36], lhsT=lhs, rhs=W[:, kc, :], start=(j == 0), stop=(j == 23)),
                                 reads=[BA, B_Wsp], writes=[B_pr[ci]])
                            j += 1
                    R = [B_lg[ci], B_sm[ci]]
                    def dv(fn, reads=R, writes=(B_sm[ci],)):
                        k.op("dve", fn, reads=list(reads), writes=list(writes))
                    k.op("dve", lambda e: e.tensor_tensor(out=lg[ci][:], in0=pr[ci][:, 0:36], in1=rb[:], op=ALU.add), reads=[B_pr[ci], B_rb], writes=[B_lg[ci]])
                    dv(lambda e: e.reduce_max(out=sm[ci][:, 0:1], in_=lg[ci][:, 0:4], axis=AX.X))
                    dv(lambda e: e.tensor_scalar(out=sm[ci][:, 1:2], in0=sm[ci][:, 0:1], scalar1=-1.0, scalar2=None, op0=ALU.mult))
                    k.op("act", lambda e: e.activation(out=sm[ci][:, 56:60], in_=lg[ci][:, 0:4], func=AF.Exp, bias=sm[ci][:, 1:2], scale=1.0, accum_out=sm[ci][:, 2:3]),
                         reads=R, writes=[B_sm[ci]])
                    dv(lambda e: e.reciprocal(out=sm[ci][:, 3:4], in_=sm[ci][:, 2:3]))
                    dv(lambda e: e.tensor_scalar(out=sm[ci][:, 4:8], in0=lg[ci][:, 0:4], scalar1=sm[ci][:, 0:1], scalar2=None, op0=ALU.is_equal))
                    le = lg[ci][:, 4:36].rearrange("p (g e) -> p g e", g=4)
                    tmp48 = sm[ci][:, 64:96].rearrange("p (g e) -> p g e", g=4)
                    ohb = sm[ci][:, 4:8].rearrange("p (g x) -> p g x", x=1).to_broadcast([128, 4, 8])
                    dv(lambda e: e.tensor_tensor(out=tmp48, in0=le, in1=ohb, op=ALU.mult))
                    dv(lambda e: e.tensor_reduce(out=sm[ci][:, 8:16], in_=sm[ci][:, 64:96].rearrange("p (g e) -> p e g", g=4), axis=AX.X, op=ALU.add))
                    dv(lambda e: e.reduce_max(out=sm[ci][:, 16:17], in_=sm[ci][:, 8:16], axis=AX.X))
                    dv(lambda e: e.tensor_scalar(out=sm[ci][:, 24:32], in0=sm[ci][:, 8:16], scalar1=sm[ci][:, 16:17], scalar2=None, op0=ALU.is_equal))
                    dv(lambda e: e.scalar_tensor_tensor(out=sm[ci][:, 32:40], in0=sm[ci][:, 24:32], scalar=-1e30, in1=sm[ci][:, 8:16], op0=ALU.mult, op1=ALU.add))
                    dv(lambda e: e.reduce_max(out=sm[ci][:, 17:18], in_=sm[ci][:, 32:40], axis=AX.X))
                    dv(lambda e: e.tensor_scalar(out=sm[ci][:, 40:48], in0=sm[ci][:, 32:40], scalar1=sm[ci][:, 17:18], scalar2=None, op0=ALU.is_equal))
                    dv(lambda e: e.tensor_tensor(out=sm[ci][:, 18:19], in0=sm[ci][:, 17:18], in1=sm[ci][:, 16:17], op=ALU.subtract))
                    k.op("act", lambda e: e.activation(out=sm[ci][:, 19:20], in_=sm[ci][:, 18:19], func=AF.Exp), reads=R, writes=[B_sm[ci]])
                    dv(lambda e: e.tensor_scalar(out=sm[ci][:, 20:21], in0=sm[ci][:, 19:20], scalar1=1.0, scalar2=None, op0=ALU.add))
                    dv(lambda e: e.reciprocal(out=sm[ci][:, 20:21], in_=sm[ci][:, 20:21]))
                    dv(lambda e: e.tensor_tensor(out=sm[ci][:, 21:22], in0=sm[ci][:, 19:20], in1=sm[ci][:, 20:21], op=ALU.mult))
                    dv(lambda e: e.tensor_tensor(out=sm[ci][:, 22:23], in0=sm[ci][:, 20:21], in1=sm[ci][:, 3:4], op=ALU.mult))
                    dv(lambda e: e.tensor_tensor(out=sm[ci][:, 23:24], in0=sm[ci][:, 21:22], in1=sm[ci][:, 3:4], op=ALU.mult))
                    dv(lambda e: e.tensor_scalar(out=sm[ci][:, 48:56], in0=sm[ci][:, 24:32], scalar1=sm[ci][:, 22:23], scalar2=None, op0=ALU.mult))
                    dv(lambda e: e.scalar_tensor_tensor(out=sm[ci][:, 48:56], in0=sm[ci][:, 40:48], scalar=sm[ci][:, 23:24], in1=sm[ci][:, 48:56], op0=ALU.mult, op1=ALU.add))
                    geb = sm[ci][:, 48:56].rearrange("p (x e) -> p x e", x=1).to_broadcast([128, 4, 8])
                    k.op("dve", lambda e: e.tensor_tensor(out=gates[ci][:].rearrange("p (g e) -> p g e", g=4), in0=ohb, in1=geb, op=ALU.mult),
                         reads=R, writes=[B_gates[ci]])
                    k.op("dve", lambda e: e.tensor_copy(out=gates_bf[ci][:], in_=gates[ci][:]), reads=[B_gates[ci]], writes=[B_gbf[ci]])
                    k.op("pe", lambda e: e.transpose(out=pg[ci][:, 0:128], in_=gates_bf[ci][:], identity=ident_b[:]),
                         reads=[B_gbf[ci], B_ident_b], writes=[B_pg[ci]])
                    k.op("act", lambda e, T0=T0: e.copy(out=gateT[:, T0:T0 + 128], in_=pg[ci][:, 0:128]), reads=[B_pg[ci]], writes=[B_gateT])
                    if "dbg_gates" in dbg:
                        if ti == 0:
                            dbg_gates = dscr("dbg_gates", [NT, 32])
                        k.dma("sp", dbg_gates[T0:T0 + 128, :], gates[ci][:], reads=[B_gates[ci]])

                recs_pair = []
                for ti in range(ntiles):
                    rec_r = []
                    orig_op_r = k.op
                    k.op = lambda *a, rec_r=rec_r, **kw: rec_r.append((a, kw))
                    router_tile(ti)
                    k.op = orig_op_r
                    recs_pair.append(rec_r)
                    if len(recs_pair) == 2 or ti == ntiles - 1:
                        for ix_ in range(max(len(r_) for r_ in recs_pair)):
                            for r_ in recs_pair:
                                if ix_ < len(r_):
                                    a_, kw_ = r_[ix_]
                                    k.op(*a_, **kw_)
                        recs_pair = []
                k.barrier()
            if "stop_router" in dbg:
                return
            with ExitStack() as s2:
                w13 = [sb(s2, f"w13_{i}", [128, 8, 512], BF16) for i in range(2)]; B_w13 = [Buf(), Buf()]
                w2 = [sb(s2, f"w2_{i}", [128, 2, D], BF16) for i in range(2)]; B_w2 = [Buf(), Buf()]
                hh = [sb(s2, f"hh{i}", [128, 2, ntok], BF16) for i in range(2)]; B_hh = [Buf(), Buf()]
                stg13 = sb(s2, "stg13", [128, 8, 512]); B_stg13 = Buf()
                stg2 = sb(s2, "stg2", [128, 2, D]); B_stg2 = Buf()
                s1t = [sb(s2, f"s1t{i}", [128, 512]) for i in range(2)]; B_s1t = [Buf(), Buf()]
                t3 = [sb(s2, f"t3{i}", [128, 512]) for i in range(2)]; B_t3 = [Buf(), Buf()]
                ph1 = [ps(s2, f"ph1_{i}", [128, 512]) for i in range(2)]; B_ph1 = [Buf(), Buf()]
                ph3 = [ps(s2, f"ph3_{i}", [128, 512]) for i in range(2)]; B_ph3 = [Buf(), Buf()]
                pgbs = [ps(s2, f"pgb{i}", [128, 512]) for i in range(2)]; B_pgbs = [Buf(), Buf()]
                ibk = 0
                pf = [ps(s2, f"pf{i}", [128, 512]) for i in range(2)]; B_pf = [Buf(), Buf()]
                blocks = [(b0, min(512, ntok - b0)) for b0 in range(0, ntok, 512)]
                it = 0; itf = 0

                def w_stage13(ee):
                    k.dma("sp", stg13[:, :, 0:256], moe_w1[l, ee].rearrange("(k p) f -> p k f", p=128), writes=[B_stg13])
                    k.dma("sp", stg13[:, :, 256:512], moe_w3[l, ee].rearrange("(k p) f -> p k f", p=128), writes=[B_stg13])

                def w_stage2(ee):
                    k.dma("sp", stg2[:], moe_w2[l, ee].rearrange("(c p) d -> p c d", p=128), writes=[B_stg2])

                def w_cast13(ee):
                    wj = ee % 2
                    k.op("pool", lambda e: e.tensor_copy(out=w13[wj][:], in_=stg13[:]), reads=[B_stg13], writes=[B_w13[wj]])

                def w_cast2(ee):
                    wj = ee % 2
                    k.op("pool", lambda e: e.tensor_copy(out=w2[wj][:], in_=stg2[:]), reads=[B_stg2], writes=[B_w2[wj]])

                def w_tail(e_):
                    if e_ + 1 < 32:
                        w_cast2(e_ + 1)
                    if e_ + 2 < 32:
                        w_stage2(e_ + 2)
                for e_ in range(32):
                    wi = e_ % 2
                    if e_ == 0:
                        w_stage13(0); w_stage2(0); w_cast13(0); w_cast2(0); w_stage13(1); w_stage2(1)
                    if e_ + 1 < 32:
                        w_cast13(e_ + 1)
                    if e_ + 2 < 32:
                        w_stage13(e_ + 2)
                    for (b0, bn) in blocks:
                        pgb = pgbs[ibk % 2]; B_pgb = B_pgbs[ibk % 2]; ibk += 1
                        k.op("pe", lambda e, e_=e_, b0=b0, bn=bn, pgb=pgb: e.matmul(pgb[:, 0:bn], lhsT=sel[:, e_, :], rhs=gateT[:, b0:b0 + bn], start=True, stop=True),
                             reads=[B_sel, B_gateT], writes=[B_pgb])
                        for fc in range(2):
                            i = it % 2; it += 1
                            for kc in range(8):
                                k.op("pe", lambda e, kc=kc, fc=fc, i=i, wi=wi, b0=b0, bn=bn: e.matmul(
                                    ph1[i][:, 0:bn], lhsT=w13[wi][:, kc, fc * 128:(fc + 1) * 128], rhs=h2T[:, kc, b0:b0 + bn],
                                    start=(kc == 0), stop=(kc == 7)), reads=[B_w13[wi], B_h2T], writes=[B_ph1[i]])
                            for kc in range(8):
                                k.op("pe", lambda e, kc=kc, fc=fc, i=i, wi=wi, b0=b0, bn=bn: e.matmul(
                                    ph3[i][:, 0:bn], lhsT=w13[wi][:, kc, 256 + fc * 128:256 + (fc + 1) * 128], rhs=h2T[:, kc, b0:b0 + bn],
                                    start=(kc == 0), stop=(kc == 7)), reads=[B_w13[wi], B_h2T], writes=[B_ph3[i]])
                            k.op("act", lambda e, i=i, bn=bn: e.activation(out=s1t[i][:, 0:bn], in_=ph1[i][:, 0:bn], func=AF.Silu),
                                 reads=[B_ph1[i]], writes=[B_s1t[i]])
                            k.op("dve", lambda e, i=i, bn=bn: e.tensor_tensor(out=t3[i][:, 0:bn], in0=s1t[i][:, 0:bn], in1=ph3[i][:, 0:bn], op=ALU.mult),
                                 reads=[B_s1t[i], B_ph3[i]], writes=[B_t3[i]])
                            k.op("dve", lambda e, i=i, bn=bn, fc=fc, wi=wi, b0=b0, pgb=pgb: e.tensor_tensor(out=hh[wi][:, fc, b0:b0 + bn], in0=t3[i][:, 0:bn], in1=pgb[:, 0:bn], op=ALU.mult),
                                 reads=[B_t3[i], B_pgb], writes=[B_hh[wi]])
                    if e_ % 2 == 0:
                        w_tail(e_)
                        continue
                    for tt in range(ntiles):
                        for dc in range(2):
                            j = itf % 2; itf += 1
                            for q_ in range(4):
                                wq = q_ // 2; fc = q_ % 2
                                k.op("pe", lambda e, fc=fc, j=j, wq=wq, tt=tt, dc=dc, q_=q_: e.matmul(
                                    pf[j][:], lhsT=hh[wq][:, fc, tt * 128:(tt + 1) * 128], rhs=w2[wq][:, fc, dc * 512:(dc + 1) * 512],
                                    start=(q_ == 0), stop=(q_ == 3)), reads=[B_hh[wq], B_w2[wq]], writes=[B_pf[j]])
                            if e_ == 1:
                                k.op("dve", lambda e, j=j, tt=tt, dc=dc: e.tensor_copy(out=f_acc[:, tt, dc * 512:(dc + 1) * 512], in_=pf[j][:]),
                                     reads=[B_pf[j]], writes=[B_facc[tt]])
                            else:
                                k.op("dve", lambda e, j=j, tt=tt, dc=dc: e.tensor_tensor(out=f_acc[:, tt, dc * 512:(dc + 1) * 512],
                                     in0=f_acc[:, tt, dc * 512:(dc + 1) * 512], in1=pf[j][:], op=ALU.add),
                                     reads=[B_pf[j], B_facc[tt]], writes=[B_facc[tt]])
                    w_tail(e_)
                k.barrier()
            if "stop_experts" in dbg:
                return
            with ExitStack() as s3:
                g2 = [mod_bc(s3, f"g2_{r}", l, r, 5) for r in range(2)]
                lng = load_bc(s3, "ln2g", ln2_g[l:l + 1, :], D)
                lnb = load_bc(s3, "ln2b", ln2_b[l:l + 1, :], D)
                wks = [ln_work(s3, "e2a"), ln_work(s3, "e2b")]
                xo = [sb(s3, f"x1o{i}", [128, D]) for i in range(2)]; B_xo = [Buf(), Buf()]

                def ln2_tile(ti):
                    i = ti % 2
                    r = 0 if ti < NLT else 1
                    T0 = ti * 128
                    k.dma("sp", xo[i][:], x1_scr[T0:T0 + 128, :], writes=[B_xo[i]])
                    ln_epilogue(wks[i], [f_acc[:, ti, 0:512], f_acc[:, ti, 512:1024]], [B_facc[ti], B_facc[ti]], xo[i], B_xo[i],
                                g2[r], lng, lnb, dst[T0:T0 + 128, :])

                for t2 in range(0, ntiles, 2):
                    replay([record(lambda ti=ti: ln2_tile(ti)) for ti in range(t2, min(t2 + 2, ntiles))])
                k.barrier()


        def ssm_phase(st, catT_, B_catT_):
            PI = math.pi
            TWO_PI = 2.0 * math.pi
            def bc3(ap2, n):
                P_, G_ = ap2.shape
                return ap2.rearrange("p (g x) -> p g x", x=1).to_broadcast([P_, G_, n])
            I32 = mybir.dt.int32
            INV2PI = 1.0 / TWO_PI

            def sincos(ang_ap, shape, s_out, c_out, tmps, B_in, B_out, B_tmp):
                y, yi, yf = tmps
                dvt = lambda fn: k.op("dve", fn, reads=[B_in, B_tmp, B_out], writes=[B_tmp])
                dvt(lambda e: e.tensor_scalar(out=y, in0=ang_ap, scalar1=INV2PI, scalar2=32.5, op0=ALU.mult, op1=ALU.add))
                dvt(lambda e: e.tensor_copy(out=yi, in_=y))
                dvt(lambda e: e.tensor_copy(out=yf, in_=yi))
                dvt(lambda e: e.tensor_tensor(out=y, in0=y, in1=yf, op=ALU.subtract))
                dvt(lambda e: e.scalar_tensor_tensor(out=yf, in0=y, scalar=0.0, in1=y, op0=ALU.is_lt, op1=ALU.add))
                k.op("act", lambda e: e.activation(out=s_out, in_=yf, func=AF.Sin, bias=negpi[0:shape[0], :], scale=TWO_PI),
                     reads=[B_tmp, B_np], writes=[B_out])
                dvt(lambda e: e.tensor_scalar(out=y, in0=yf, scalar1=0.25, scalar2=None, op0=ALU.add))
                dvt(lambda e: e.scalar_tensor_tensor(out=yf, in0=y, scalar=1.0, in1=y, op0=ALU.is_ge, op1=ALU.subtract))
                k.op("act", lambda e: e.activation(out=c_out, in_=yf, func=AF.Sin, bias=negpi[0:shape[0], :], scale=-TWO_PI),
                     reads=[B_tmp, B_np], writes=[B_out])

            ar = sb(st, "ar", [64, 64]); ai = sb(st, "ai", [64, 64]); ls = sb(st, "ls", [64, 64]); B_par = Buf()
            with nc.allow_non_contiguous_dma(reason="ssm params"):
                k.dma("sp", ar[:], a_re.rearrange("d g p -> p (d g)"), writes=[B_par])
                k.dma("sp", ai[:], a_im.rearrange("d g p -> p (d g)"), writes=[B_par])
            k.dma("sp", ls[:], log_step.rearrange("(x d) g -> x (d g)", x=1).partition_broadcast(64), writes=[B_par])
            negpi = sb(st, "negpi", [128, 1]); B_np = Buf()
            k.op("dve", lambda e: e.memset(negpi[:], -PI), writes=[B_np])
            kv = sb(st, "kv", [64, 16, 64]); B_kv = Buf()
            k.dma("sp", kv[:], kval_in.rearrange("p (k g) -> p k g", k=16), writes=[B_kv])
            mrow = sb(st, "mrow", [64, 288]); B_mrow = Buf()
            k.dma("sp", mrow[:], mrow_in[:, :], writes=[B_mrow])
            maskF = sb(st, "maskF", [128, 128]); maskB = sb(st, "maskB", [128, 128]); B_mk = Buf()
            k.dma("sp", maskF[:], maskF_in[:, :], writes=[B_mk])
            k.dma("sp", maskB[:], maskB_in[:, :], writes=[B_mk])
            dar = sb(st, "dar", [64, 64]); dai = sb(st, "dai", [64, 64]); B_d = Buf()
            k.op("act", lambda e: e.activation(out=ls[:], in_=ls[:], func=AF.Exp), reads=[B_par], writes=[B_par])
            k.op("dve", lambda e: e.tensor_tensor(out=dar[:], in0=ls[:], in1=ar[:], op=ALU.mult), reads=[B_par], writes=[B_d])
            k.op("dve", lambda e: e.tensor_tensor(out=dai[:], in0=ls[:], in1=ai[:], op=ALU.mult), reads=[B_par, B_d], writes=[B_d])
            LR = sb(st, "LR", [64, 16, 64]); LI = sb(st, "LI", [64, 16, 64]); MG = sb(st, "MG", [64, 16, 64]); B_L = Buf()
            th8 = sb(st, "th8", [64, 64]); B_th8 = Buf()
            k.op("dve", lambda e: e.tensor_scalar(out=th8[:], in0=dai[:], scalar1=8.0, scalar2=None, op0=ALU.mult), reads=[B_d], writes=[B_th8])
            with ExitStack() as t0:
                ang = sb(t0, "ang", [64, 16, 64]); a2 = sb(t0, "a2", [64, 16, 64]); B_ang = Buf()
                dai_b = dai[:].rearrange("p (x g) -> p x g", x=1).to_broadcast([64, 16, 64])
                dar_b = dar[:].rearrange("p (x g) -> p x g", x=1).to_broadcast([64, 16, 64])
                k.op("dve", lambda e: e.tensor_tensor(out=MG[:], in0=kv[:], in1=dar_b, op=ALU.mult), reads=[B_kv, B_d], writes=[B_L])
                k.op("act", lambda e: e.activation(out=MG[:], in_=MG[:], func=AF.Exp), reads=[B_L], writes=[B_L])
                k.op("dve", lambda e: e.tensor_tensor(out=ang[:], in0=kv[:], in1=dai_b, op=ALU.mult), reads=[B_kv, B_d], writes=[B_ang])
                a3 = sb(t0, "a3", [64, 16, 64], I32); a4 = sb(t0, "a4", [64, 16, 64])
                sincos(ang[:], [64, 16, 64], LI[:], LR[:], (a2[:], a3[:], a4[:]), B_ang, B_L, B_ang)
                k.op("dve", lambda e: e.tensor_tensor(out=LR[:], in0=LR[:], in1=MG[:], op=ALU.mult), reads=[B_L], writes=[B_L])
                k.op("dve", lambda e: e.tensor_tensor(out=LI[:], in0=LI[:], in1=MG[:], op=ALU.mult), reads=[B_L], writes=[B_L])
                k.barrier()
            cre = sb(st, "cre", [64, 64]); cim = sb(st, "cim", [64, 64]); B_c = Buf()
            with ExitStack() as t0:
                nr = sb(t0, "nr", [64, 64]); den = sb(t0, "den", [64, 64]); tq = sb(t0, "tq", [64, 64]); B_t = Buf()
                L1r = LR[:, 8, :]; L1i = LI[:, 8, :]
                dv = lambda fn: k.op("dve", fn, reads=[B_t, B_L, B_par, B_c], writes=[B_t, B_c])
                dv(lambda e: e.tensor_scalar(out=nr[:], in0=L1r, scalar1=-1.0, scalar2=None, op0=ALU.add))
                dv(lambda e: e.tensor_tensor(out=den[:], in0=ar[:], in1=ar[:], op=ALU.mult))
                dv(lambda e: e.tensor_tensor(out=tq[:], in0=ai[:], in1=ai[:], op=ALU.mult))
                dv(lambda e: e.tensor_tensor(out=den[:], in0=den[:], in1=tq[:], op=ALU.add))
                dv(lambda e: e.reciprocal(out=den[:], in_=den[:]))
                dv(lambda e: e.tensor_tensor(out=cre[:], in0=nr[:], in1=ar[:], op=ALU.mult))
                dv(lambda e: e.tensor_tensor(out=tq[:], in0=L1i, in1=ai[:], op=ALU.mult))
                dv(lambda e: e.tensor_tensor(out=cre[:], in0=cre[:], in1=tq[:], op=ALU.add))
                dv(lambda e: e.tensor_tensor(out=cre[:], in0=cre[:], in1=den[:], op=ALU.mult))
                dv(lambda e: e.tensor_tensor(out=cim[:], in0=L1i, in1=ar[:], op=ALU.mult))
                dv(lambda e: e.tensor_tensor(out=tq[:], in0=nr[:], in1=ai[:], op=ALU.mult))
                dv(lambda e: e.tensor_tensor(out=cim[:], in0=cim[:], in1=tq[:], op=ALU.subtract))
                dv(lambda e: e.tensor_tensor(out=cim[:], in0=cim[:], in1=den[:], op=ALU.mult))
                k.barrier()
            UT_all = sb(st, "UT_all", [128, 32, 288], BF16); B_UT = Buf()
            NB = ((0, 128), (128, 128), (256, 32))
            u8v = u_scr.rearrange("(n j) f -> n (j f)", j=8)
            with ExitStack() as t0:
                U8 = sb(t0, "U8", [128, 3, 4096]); B_U8 = Buf()
                U8b = sb(t0, "U8b", [128, 3, 4096], BF16); B_U8b = Buf()
                pU = [ps(t0, f"pU{i}", [128, 1024], BF16) for i in range(2)]; B_pU = [Buf(), Buf()]
                for bi, (n0, nb) in enumerate(NB):
                    k.dma("sp", U8[0:nb, bi, :], u8v[n0:n0 + nb, :], writes=[B_U8])
                    ov = U8b[0:nb, bi, :].rearrange("p (g j c) -> p j g c", g=32, j=8, c=16)
                    iv = U8[0:nb, bi, :].rearrange("p (j g c) -> p j g c", g=32, j=8, c=16)
                    k.op("pool" if bi == 1 else "act", (lambda e, ov=ov, iv=iv: e.tensor_copy(out=ov, in_=iv)) if bi == 1 else
                         (lambda e, ov=ov, iv=iv: e.copy(out=ov, in_=iv)), reads=[B_U8], writes=[B_U8b])
                for g in range(32):
                    i = g % 2
                    for bi, (n0, nb) in enumerate(NB):
                        src = U8b[0:nb, bi, g * 128:(g + 1) * 128]
                        k.op("pe", lambda e, i=i, src=src, n0=n0, nb=nb: e.transpose(out=pU[i][:, n0:n0 + nb], in_=src, identity=ident_b[0:nb, 0:nb]),
                             reads=[B_U8b, B_ident_b], writes=[B_pU[i]])
                    k.op("act", lambda e, i=i, g=g: e.copy(out=UT_all[:, g, :], in_=pU[i][:, 0:288]), reads=[B_pU[i]], writes=[B_UT])
                k.barrier()
            COr = sb(st, "COr", [64, 2, 32, 128], BF16); COi = sb(st, "COi", [64, 2, 32, 128], BF16); B_CO = Buf()
            T_all = sb(st, "T_all", [128, 32, 128], BF16); B_T = Buf()
            WinT = sb(st, "WinT", [128, 2, 32, 128], BF16); B_WinT = Buf()
            with ExitStack() as t0:
                BLr = sb(t0, "BLr", [64, 32, 128], BF16); BLi = sb(t0, "BLi", [64, 32, 128], BF16); B_BL = Buf()
                CTr = sb(t0, "CTr", [64, 32, 128], BF16); CTi = sb(t0, "CTi", [64, 32, 128], BF16); B_CT = Buf()
                Br = sb(t0, "Br", [64, 64, 16]); Bi = sb(t0, "Bi", [64, 64, 16]); B_B = Buf()
                Cr = sb(t0, "Cr", [64, 64, 16]); Ci = sb(t0, "Ci", [64, 64, 16]); B_C = Buf()
                Bbr = sb(t0, "Bbr", [64, 64, 16]); Bbi = sb(t0, "Bbi", [64, 64, 16]); B_Bb = Buf()
                with nc.allow_non_contiguous_dma(reason="ssm B/C tables"):
                    for d in range(2):
                        k.dma("sp", Br[:, d * 32:(d + 1) * 32, :], b_re[d].rearrange("g p c -> p g c"), writes=[B_B])
                        k.dma("sp", Bi[:, d * 32:(d + 1) * 32, :], b_im[d].rearrange("g p c -> p g c"), writes=[B_B])
                        for gb in range(4):
                            sl = slice(d * 32 + gb * 8, d * 32 + gb * 8 + 8)
                            k.dma("sp", Cr[:, sl, :], c_re[d, gb * 8:(gb + 1) * 8].rearrange("g c p -> p g c"), writes=[B_C])
                            k.dma("act", Ci[:, sl, :], c_im[d, gb * 8:(gb + 1) * 8].rearrange("g c p -> p g c"), writes=[B_C])
                NLR = sb(t0, "NLR", [64, 16, 64]); NLI = sb(t0, "NLI", [64, 16, 64])
                k.op("dve", lambda e: e.tensor_scalar(out=NLR[:], in0=LR[:], scalar1=-1.0, scalar2=None, op0=ALU.mult), reads=[B_L], writes=[B_L])
                k.op("dve", lambda e: e.tensor_scalar(out=NLI[:], in0=LI[:], scalar1=-1.0, scalar2=None, op0=ALU.mult), reads=[B_L], writes=[B_L])
                ta = sb(t0, "ta", [64, 32, 16]); tb_ = sb(t0, "tb", [64, 32, 16]); B_tab = Buf()
                tc_ = sb(t0, "tc", [64, 32, 16]); td_ = sb(t0, "td", [64, 32, 16]); B_tcd = Buf()
                for d in range(2):
                    dsl = slice(d * 32, (d + 1) * 32)
                    creb = bc3(cre[:, dsl], 16); cimb = bc3(cim[:, dsl], 16)
                    dv = lambda fn: k.op("dve", fn, reads=[B_B, B_c, B_tab, B_Bb], writes=[B_tab, B_Bb])
                    dv(lambda e, dsl=dsl, creb=creb: e.tensor_tensor(out=ta[:], in0=Br[:, dsl, :], in1=creb, op=ALU.mult))
                    dv(lambda e, dsl=dsl, cimb=cimb: e.tensor_tensor(out=tb_[:], in0=Bi[:, dsl, :], in1=cimb, op=ALU.mult))
                    dv(lambda e, dsl=dsl: e.tensor_tensor(out=Bbr[:, dsl, :], in0=ta[:], in1=tb_[:], op=ALU.subtract))
                    dv(lambda e, dsl=dsl, creb=creb: e.tensor_tensor(out=ta[:], in0=Bi[:, dsl, :], in1=creb, op=ALU.mult))
                    dv(lambda e, dsl=dsl, cimb=cimb: e.tensor_tensor(out=tb_[:], in0=Br[:, dsl, :], in1=cimb, op=ALU.mult))
                    dv(lambda e, dsl=dsl: e.tensor_tensor(out=Bbi[:, dsl, :], in0=ta[:], in1=tb_[:], op=ALU.add))
                pTd = [ps(t0, f"pTd{i}", [128, 512]) for i in range(2)]; B_pTd = [Buf(), Buf()]
                pW = [ps(t0, f"pW{i}", [128, 8, 128], BF16) for i in range(2)]; B_pW = [Buf(), Buf()]
                tt = sb(t0, "tt", [128, 128]); B_tt = Buf()
                for d in range(2):
                    dsl = slice(d * 32, (d + 1) * 32)
                    for j in range(8):
                        e_ = (7 - j) if d == 0 else j
                        lr = bc3(LR[:, e_ + 7, dsl], 16); li = bc3(LI[:, e_ + 7, dsl], 16)
                        o_r = BLr[:, :, j * 16:(j + 1) * 16]; o_i = BLi[:, :, j * 16:(j + 1) * 16]
                        dv2 = lambda fn: k.op("dve", fn, reads=[B_Bb, B_L, B_tab, B_BL], writes=[B_tab, B_BL])
                        dv2(lambda e, lr=lr: e.tensor_tensor(out=ta[:], in0=Bbr[:, dsl, :], in1=lr, op=ALU.mult))
                        dv2(lambda e, li=li: e.tensor_tensor(out=tb_[:], in0=Bbi[:, dsl, :], in1=li, op=ALU.mult))
                        dv2(lambda e, o_r=o_r: e.tensor_tensor(out=o_r, in0=ta[:], in1=tb_[:], op=ALU.subtract))
                        dv2(lambda e, lr=lr: e.tensor_tensor(out=ta[:], in0=Bbi[:, dsl, :], in1=lr, op=ALU.mult))
                        dv2(lambda e, li=li: e.tensor_tensor(out=tb_[:], in0=Bbr[:, dsl, :], in1=li, op=ALU.mult))
                        dv2(lambda e, o_i=o_i: e.tensor_tensor(out=o_i, in0=ta[:], in1=tb_[:], op=ALU.add))
                        f_ = (j - 7) if d == 0 else -j
                        for (kk, o_r, o_i, BO) in ((f_ + 7, CTr[:, :, j * 16:(j + 1) * 16], CTi[:, :, j * 16:(j + 1) * 16], B_CT),
                                                   (f_ + 15, COr[:, d, :, j * 16:(j + 1) * 16], COi[:, d, :, j * 16:(j + 1) * 16], B_CO)):
                            lr = bc3(LR[:, kk, dsl], 16); li = bc3(LI[:, kk, dsl], 16)
                            pl = lambda fn, BO=BO: k.op("pool", fn, reads=[B_C, B_L, B_tcd, BO], writes=[B_tcd, BO])
                            pl(lambda e, lr=lr: e.tensor_tensor(out=tc_[:], in0=Cr[:, dsl, :], in1=lr, op=ALU.mult))
                            pl(lambda e, li=li: e.tensor_tensor(out=td_[:], in0=Ci[:, dsl, :], in1=li, op=ALU.mult))
                            pl(lambda e, o_r=o_r: e.tensor_tensor(out=o_r, in0=tc_[:], in1=td_[:], op=ALU.subtract))
                            nlr = bc3(NLR[:, kk, dsl], 16); nli = bc3(NLI[:, kk, dsl], 16)
                            pl(lambda e, nlr=nlr: e.tensor_tensor(out=tc_[:], in0=Ci[:, dsl, :], in1=nlr, op=ALU.mult))
                            pl(lambda e, nli=nli: e.tensor_tensor(out=td_[:], in0=Cr[:, dsl, :], in1=nli, op=ALU.mult))
                            pl(lambda e, o_i=o_i: e.tensor_tensor(out=o_i, in0=tc_[:], in1=td_[:], op=ALU.add))
                    for g in range(32):
                        i = g % 2
                        k.op("pe", lambda e, i=i, g=g: e.matmul(pTd[i][:, 0:128], lhsT=BLr[:, g, :], rhs=CTr[:, g, :], start=True, stop=False),
                             reads=[B_BL, B_CT], writes=[B_pTd[i]])
                        k.op("pe", lambda e, i=i, g=g: e.matmul(pTd[i][:, 0:128], lhsT=BLi[:, g, :], rhs=CTi[:, g, :], start=False, stop=True),
                             reads=[B_BL, B_CT], writes=[B_pTd[i]])
                        if d == 0:
                            k.op("dve", lambda e, i=i, g=g: e.tensor_tensor(out=T_all[:, g, :], in0=pTd[i][:, 0:128], in1=maskF[:], op=ALU.mult),
                                 reads=[B_pTd[i], B_mk], writes=[B_T])
                        else:
                            k.op("dve", lambda e, i=i: e.tensor_tensor(out=tt[:], in0=pTd[i][:, 0:128], in1=maskB[:], op=ALU.mult),
                                 reads=[B_pTd[i], B_mk], writes=[B_tt])
                            k.op("dve", lambda e, g=g: e.tensor_tensor(out=T_all[:, g, :], in0=T_all[:, g, :], in1=tt[:], op=ALU.add),
                                 reads=[B_tt, B_T], writes=[B_T])
                        k.op("pe", lambda e, i=i, g=g: e.transpose(out=pW[i][:, 0, 0:64], in_=BLr[:, g, :], identity=ident_b[0:64, 0:64]),
                             reads=[B_BL, B_ident_b], writes=[B_pW[i]])
                        k.op("pe", lambda e, i=i, g=g: e.transpose(out=pW[i][:, 0, 64:128], in_=BLi[:, g, :], identity=ident_b[0:64, 0:64]),
                             reads=[B_BL, B_ident_b], writes=[B_pW[i]])
                        k.op("act", lambda e, i=i, g=g, d=d: e.copy(out=WinT[:, d, g, :], in_=pW[i][:, 0, :]), reads=[B_pW[i]], writes=[B_WinT])
                k.barrier()
            with ExitStack() as t0:
                Yt = sb(t0, "Yt", [128, 3, 4096], BF16); B_Yt = Buf()
                pXd = [[ps(t0, f"pX{d}{i}", [64, 512]) for i in range(2)] for d in range(2)]
                B_pXd = [[Buf(), Buf()], [Buf(), Buf()]]
                pY = ps(t0, "pY", [128, 512]); B_pY = Buf()
                pYt = ps(t0, "pYt", [128, 8, 128], BF16); B_pYt = Buf()
                W = []
                for d in range(2):
                    W.append(dict(
                        XS=sb(t0, f"XS{d}", [64, 2, 288]), B_XS=Buf(),
                        base=sb(t0, f"base{d}", [64, 288]), sarg=sb(t0, f"sarg{d}", [64, 288]), B_tr=Buf(), B_tr2=Buf(),
                        sargi=sb(t0, f"sargi{d}", [64, 288], mybir.dt.int32), sargf=sb(t0, f"sargf{d}", [64, 288]),
                        sn=sb(t0, f"sn{d}", [64, 288]), cs=sb(t0, f"cs{d}", [64, 288]), B_sc=Buf(),
                        RR=sb(t0, f"RR{d}", [64, 2, 288]), B_RR=Buf(),
                        q1=sb(t0, f"q1{d}", [64, 288]), q2=sb(t0, f"q2{d}", [64, 288]), B_q=Buf(),
                        q3=sb(t0, f"q3{d}", [64, 288]), q4=sb(t0, f"q4{d}", [64, 288]), B_q34=Buf(),
                        SS=sb(t0, f"SS{d}", [64, 2, 288]), B_SS=Buf()))
                Sp = sb(t0, "Sp", [64, 2, 2, 288], BF16); B_Spd = [Buf(), Buf()]
                Ysb = sb(t0, "Ysb", [128, 288], BF16); B_Ysb = Buf()
                k.op("dve", lambda e: e.memset(Sp[:], 0.0), writes=[B_Spd[0], B_Spd[1]])

                def chain(d, g):
                    w = W[d]
                    XS, base, sarg, sargi, sargf, sn, cs, RR, q1, q2, SS = (w[n] for n in ("XS", "base", "sarg", "sargi", "sargf", "sn", "cs", "RR", "q1", "q2", "SS"))
                    B_XS, B_tr, B_tr2, B_sc, B_RR, B_q, B_SS = (w[n] for n in ("B_XS", "B_tr", "B_tr2", "B_sc", "B_RR", "B_q", "B_SS"))
                    pX = pXd[d]; B_pX = B_pXd[d]; B_Sp = B_Spd[d]
                    dg = d * 32 + g
                    for c2 in range(2):
                        k.op("pe", lambda e, c2=c2: e.matmul(pX[c2][:, 0:288], lhsT=WinT[:, d, g, c2 * 64:(c2 + 1) * 64], rhs=UT_all[:, g, :],
                                                             start=True, stop=True), reads=[B_WinT, B_UT], writes=[B_pX[c2]])
                        if d == 0:
                            k.op("act", lambda e, c2=c2: e.copy(out=XS[:, c2, :], in_=pX[c2][:, 0:288]), reads=[B_pX[c2]], writes=[B_XS])
                        else:
                            k.op("act", lambda e, c2=c2: e.copy(out=XS[:, c2, 0:32], in_=pX[c2][:, 31::-1]), reads=[B_pX[c2]], writes=[B_XS])
                            k.op("act", lambda e, c2=c2: e.copy(out=XS[:, c2, 32:288], in_=pX[c2][:, 287:31:-1]), reads=[B_pX[c2]], writes=[B_XS])
                    k.op("dve", lambda e: e.tensor_scalar(out=base[:], in0=mrow[:], scalar1=th8[:, dg:dg + 1], scalar2=None, op0=ALU.mult),
                         reads=[B_mrow, B_th8], writes=[B_tr])
                    sincos(base[:], [64, 288], sn[:], cs[:], (sarg[:], sargi[:], sargf[:]), B_tr, B_sc, B_tr2)
                    dv = lambda fn: k.op("dve", fn, reads=[B_XS, B_sc, B_q, B_RR, B_SS, B_L], writes=[B_q, B_RR, B_SS])
                    dv(lambda e: e.tensor_tensor(out=q1[:], in0=cs[:], in1=XS[:, 0, :], op=ALU.mult))
                    dv(lambda e: e.tensor_tensor(out=q2[:], in0=sn[:], in1=XS[:, 1, :], op=ALU.mult))
                    dv(lambda e: e.tensor_tensor(out=q1[:], in0=q1[:], in1=q2[:], op=ALU.add))
                    dv(lambda e: e.tensor_tensor_scan(out=RR[:, 0, :], data0=MG[:, 15, dg:dg + 1].to_broadcast([64, 288]), data1=q1[:], initial=0.0, op0=ALU.mult, op1=ALU.add))
                    dv(lambda e: e.tensor_tensor(out=q1[:], in0=cs[:], in1=XS[:, 1, :], op=ALU.mult))
                    dv(lambda e: e.tensor_tensor(out=q2[:], in0=sn[:], in1=XS[:, 0, :], op=ALU.mult))
                    dv(lambda e: e.tensor_tensor(out=q1[:], in0=q1[:], in1=q2[:], op=ALU.subtract))
                    dv(lambda e: e.tensor_tensor_scan(out=RR[:, 1, :], data0=MG[:, 15, dg:dg + 1].to_broadcast([64, 288]), data1=q1[:], initial=0.0, op0=ALU.mult, op1=ALU.add))
                    dv(lambda e: e.tensor_tensor(out=q1[:], in0=cs[:], in1=RR[:, 0, :], op=ALU.mult))
                    dv(lambda e: e.tensor_tensor(out=q2[:], in0=sn[:], in1=RR[:, 1, :], op=ALU.mult))
                    dv(lambda e: e.tensor_tensor(out=SS[:, 0, :], in0=q1[:], in1=q2[:], op=ALU.subtract))
                    dv(lambda e: e.tensor_tensor(out=q1[:], in0=cs[:], in1=RR[:, 1, :], op=ALU.mult))
                    dv(lambda e: e.tensor_tensor(out=q2[:], in0=sn[:], in1=RR[:, 0, :], op=ALU.mult))
                    dv(lambda e: e.tensor_tensor(out=SS[:, 1, :], in0=q1[:], in1=q2[:], op=ALU.add))
                    for c2 in range(2):
                        if d == 0:
                            k.op("act", lambda e, c2=c2: e.copy(out=Sp[:, 0, c2, 1:288], in_=SS[:, c2, 0:287]), reads=[B_SS], writes=[B_Sp])
                        else:
                            k.op("act", lambda e, c2=c2: e.copy(out=Sp[:, 1, c2, 0:31], in_=SS[:, c2, 30::-1]), reads=[B_SS], writes=[B_Sp])
                            k.op("act", lambda e, c2=c2: e.copy(out=Sp[:, 1, c2, 32:288], in_=SS[:, c2, 286:30:-1]), reads=[B_SS], writes=[B_Sp])

                B_Sp = B_Spd[0]
                for g in range(32):
                    recs = []
                    orig_op = k.op
                    for d in range(2):
                        rec = []
                        k.op = lambda *a, rec=rec, **kw: rec.append((a, kw))
                        chain(d, g)
                        recs.append(rec)
                    k.op = orig_op
                    for i_ in range(max(len(r_) for r_ in recs)):
                        for r_ in recs:
                            if i_ < len(r_):
                                a_, kw_ = r_[i_]
                                k.op(*a_, **kw_)
                    k.op("pe", lambda e, g=g: e.matmul(pY[:, 0:288], lhsT=T_all[:, g, :], rhs=UT_all[:, g, :], start=True, stop=False),
                         reads=[B_T, B_UT], writes=[B_pY])
                    for d in range(2):
                        k.op("pe", lambda e, g=g, d=d: e.matmul(pY[:, 0:288], lhsT=COr[:, d, g, :], rhs=Sp[:, d, 0, :], start=False, stop=False),
                             reads=[B_CO, B_Spd[d]], writes=[B_pY])
                        k.op("pe", lambda e, g=g, d=d: e.matmul(pY[:, 0:288], lhsT=COi[:, d, g, :], rhs=Sp[:, d, 1, :], start=False, stop=(d == 1)),
                             reads=[B_CO, B_Spd[d]], writes=[B_pY])
                    k.op("act", lambda e: e.copy(out=Ysb[:], in_=pY[:, 0:288]), reads=[B_pY], writes=[B_Ysb])
                    for bi, (n0, nb) in enumerate(NB):
                        k.op("pe", lambda e, bi=bi, n0=n0, nb=nb: e.transpose(out=pYt[0:nb, bi, :], in_=Ysb[:, n0:n0 + nb], identity=ident_b[:]),
                             reads=[B_Ysb, B_ident_b], writes=[B_pYt])
                    for bi, (n0, nb) in enumerate(NB):
                        dst = Yt[0:nb, bi, :].rearrange("p (j f) -> p j f", j=8)[:, :, g * 16:(g + 1) * 16]
                        k.op("dve", lambda e, bi=bi, nb=nb, dst=dst: e.tensor_copy(out=dst, in_=pYt[0:nb, bi, :].rearrange("p (j c) -> p j c", j=8)),
                             reads=[B_pYt], writes=[B_Yt])
                y8v = y_scr.rearrange("(n j) f -> n (j f)", j=8)
                for bi, (n0, nb) in enumerate(NB):
                    k.dma("sp", y8v[n0:n0 + nb, :], Yt[0:nb, bi, :], reads=[B_Yt])
                k.barrier()


        def ssm_post(st, catT_, B_catT_):
            GC = 2.0 * math.sqrt(2.0 / math.pi)
            gw = sb(st, "gluw", [128, 4, 512], BF16); B_gw = Buf()
            k.dma("pool", gw[:], glu_w.rearrange("(k p) n -> p k n", p=128), writes=[B_gw])
            gb = sb(st, "glub", [128, 4]); B_gb = Buf()
            with nc.allow_non_contiguous_dma(reason="tiny bias"):
                k.dma("sp", gb[:], glu_b[0, :].rearrange("(c p) -> p c", p=128), writes=[B_gb])
            dbc = load_bc(st, "dskip", ssm_d[0:1, :], 512)
            yt = [sb(st, f"py{i}", [128, 512], BF16) for i in range(2)]; B_yt = [Buf(), Buf()]
            ut = [sb(st, f"pu{i}", [128, 512]) for i in range(2)]; B_ut = [Buf(), Buf()]
            xx = [sb(st, f"pxx{c_}", [128, 512]) for c_ in range(2)]; B_xx = [Buf(), Buf()]
            ww = [sb(st, f"pww{c_}", [128, 512]) for c_ in range(2)]; B_ww = [Buf(), Buf()]
            sg = [sb(st, f"psg{c_}", [128, 512]) for c_ in range(2)]; B_sg = [Buf(), Buf()]
            g_bf = [sb(st, f"pg_bf{c_}", [128, 512], BF16) for c_ in range(2)]; B_g = [Buf(), Buf()]
            gT = [sb(st, f"pgT{c_}", [128, 4, 128], BF16) for c_ in range(2)]; B_gT = [Buf(), Buf()]
            s2 = [sb(st, f"ps2{c_}", [128, 4, 128]) for c_ in range(2)]; B_s2 = [Buf(), Buf()]
            pGT = [ps(st, f"pGT{c_}", [128, 8, 128], BF16) for c_ in range(2)]; B_pGT = [Buf(), Buf()]
            pz = [ps(st, f"pz{c_}", [128, 4, 128]) for c_ in range(2)]; B_pz = [Buf(), Buf()]
            def post_tile(ti):
                i = ti % 2
                T0 = ti * 128
                row0 = T0 + CTX if ti < NLT else T0 - SEQ
                k.dma("sp", yt[i][:], y_scr[row0:row0 + 128, :], writes=[B_yt[i]])
                k.dma("sp", ut[i][:], u_scr[row0:row0 + 128, :], writes=[B_ut[i]])
                k.op("dve", lambda e, i=i: e.tensor_tensor(out=xx[i][:], in0=ut[i][:], in1=dbc[0][:], op=ALU.mult), reads=[B_ut[i], dbc[1]], writes=[B_xx[i]])
                k.op("dve", lambda e, i=i: e.tensor_tensor(out=xx[i][:], in0=xx[i][:], in1=yt[i][:], op=ALU.add), reads=[B_xx[i], B_yt[i]], writes=[B_xx[i]])
                k.op("pool", lambda e: e.tensor_tensor(out=ww[i][:], in0=xx[i][:], in1=xx[i][:], op=ALU.mult), reads=[B_xx[i]], writes=[B_ww[i]])
                k.op("pool", lambda e: e.tensor_scalar(out=ww[i][:], in0=ww[i][:], scalar1=0.044715, scalar2=1.0, op0=ALU.mult, op1=ALU.add), reads=[B_ww[i]], writes=[B_ww[i]])
                k.op("pool", lambda e: e.tensor_tensor(out=ww[i][:], in0=ww[i][:], in1=xx[i][:], op=ALU.mult), reads=[B_ww[i], B_xx[i]], writes=[B_ww[i]])
                k.op("act", lambda e: e.activation(out=sg[i][:], in_=ww[i][:], func=AF.Sigmoid, scale=GC), reads=[B_ww[i]], writes=[B_sg[i]])
                k.op("dve", lambda e: e.tensor_tensor(out=g_bf[i][:], in0=xx[i][:], in1=sg[i][:], op=ALU.mult), reads=[B_xx[i], B_sg[i]], writes=[B_g[i]])
                if "dbg_g" in dbg:
                    if ti == 0:
                        dbg_g = dscr("dbg_g", [NT, 512], BF16)
                    k.dma("sp", dbg_g[T0:T0 + 128, :], g_bf[i][:], reads=[B_g[i]])
                for kc in range(4):
                    k.op("pe", lambda e, kc=kc: e.transpose(out=pGT[i][:, kc, :], in_=g_bf[i][:, kc * 128:(kc + 1) * 128], identity=ident_b[:]),
                         reads=[B_g[i], B_ident_b], writes=[B_pGT[i]])
                k.op("act", lambda e: e.copy(out=gT[i][:], in_=pGT[i][:, 0:4, :]), reads=[B_pGT[i]], writes=[B_gT[i]])
                for n_ in range(4):
                    for kc in range(4):
                        k.op("pe", lambda e, n_=n_, kc=kc: e.matmul(pz[i][:, n_, :], lhsT=gw[:, kc, n_ * 128:(n_ + 1) * 128], rhs=gT[i][:, kc, :],
                                                                    start=(kc == 0), stop=(kc == 3)), reads=[B_gw, B_gT[i]], writes=[B_pz[i]])
                for n_ in range(4):
                    k.op("act", lambda e, n_=n_: e.activation(out=s2[i][:, n_, :], in_=pz[i][:, n_, :], func=AF.Sigmoid, bias=gb[:, n_:n_ + 1], scale=1.0),
                         reads=[B_pz[i], B_gb], writes=[B_s2[i]])
                k.op("dve", lambda e, T0=T0: e.tensor_tensor(out=catT_[:, 4:8, T0:T0 + 128], in0=gT[i][:], in1=s2[i][:], op=ALU.mult),
                     reads=[B_gT[i], B_s2[i]], writes=[B_catT_])
            for t2_ in range(0, NTILE, 2):
                replay([record(lambda ti=ti: post_tile(ti)) for ti in range(t2_, min(t2_ + 2, NTILE))])
            if "dbg_cat" in dbg:
                dbg_cat = dscr("dbg_cat", [128, 8, NT], BF16)
                k.dma("sp", dbg_cat, catT_[:], reads=[B_catT_])
            k.barrier()


        def layer1_mixer():
            LAM_INIT = 0.8 - 0.6 * math.exp(-0.3 * 1)
            SC = 0.125
            with ExitStack() as L1:
                qT2 = sb(L1, "qT2", [128, 8, SEQ], BF16); B_q2 = Buf()
                kT2 = sb(L1, "kT2", [128, 8, NT], BF16); B_k2 = Buf()
                v1 = sb(L1, "v1", [128, NTILE, D], BF16); B_v1 = Buf()
                nmax = sb(L1, "nmax", [128, 32]); B_nmax = Buf()
                for st in phase("l1proj"):
                    w_bf = sb(st, "dif_w_bf", [128, 8, 3072], BF16); B_w = Buf()
                    for kc in range(8):
                        k.dma("pool", w_bf[:, kc, :], dif_w_in[kc * 128:(kc + 1) * 128, :], writes=[B_w])
                    sc1p = [mod_bc(st, f"l1sc1p_{r}", 1, r, 1, plus1=True) for r in range(2)]
                    sh1 = [mod_bc(st, f"l1sh1_{r}", 1, r, 0) for r in range(2)]
                    xt = [sb(st, f"l1xt{i}", [128, D]) for i in range(2)]; B_xt = [Buf(), Buf()]
                    tmpf = [sb(st, f"l1tmpf{i}", [128, D]) for i in range(2)]; B_tmpf = [Buf(), Buf()]
                    h_bf = [sb(st, f"l1h_bf{i}", [128, D], BF16) for i in range(2)]; B_hbf = [Buf(), Buf()]
                    hT = [sb(st, f"l1hT{i}", [128, 8, 128], BF16) for i in range(2)]; B_hT = [Buf(), Buf()]
                    rt = [sb(st, f"l1rt{i}", [128, 64]) for i in range(2)]; B_rt = [Buf(), Buf()]
                    t1 = [sb(st, f"l1rope_t1{i}", [128, 512]) for i in range(2)]; t2 = [sb(st, f"l1rope_t2{i}", [128, 512]) for i in range(2)]; B_rtmp = [Buf(), Buf()]
                    qk_bf = [sb(st, f"l1qk_bf{i}", [128, 512], BF16) for i in range(2)]; B_qk = [Buf(), Buf()]
                    pT = [ps(st, f"l1pT{i}", [128, 8, 128], BF16) for i in range(2)]; B_pT = [Buf(), Buf()]
                    pp = [[ps(st, f"l1pp{i}{j}", [128, 512]) for j in range(2)] for i in range(2)]; B_pp = [[Buf(), Buf()], [Buf(), Buf()]]
                    pq = [ps(st, f"l1pq{i}", [128, 8, 128], BF16) for i in range(2)]; B_pq = [Buf(), Buf()]
                    sqt = [sb(st, f"l1sq{i}", [128, 512]) for i in range(2)]; rs8 = [sb(st, f"l1rs8{i}", [128, 8]) for i in range(2)]; B_sq = [Buf(), Buf()]
                    k.op("dve", lambda e: e.memset(nmax[:], 0.0), writes=[B_nmax])
                    ibs = [0, 0]

                    def proj_tile(ti):
                        i = ti % 2
                        r = 0 if ti < NLT else 1
                        T0 = ti * 128
                        k.dma("sp", xt[i][:], src_rows(ti, 1), writes=[B_xt[i]])
                        if r == 0:
                            k.dma("sp", rt[i][:], rope_cs[T0:T0 + 128, :], writes=[B_rt[i]])
                        k.op("dve", lambda e: e.tensor_tensor(out=tmpf[i][:], in0=xt[i][:], in1=sc1p[r][0][:], op=ALU.mult),
                             reads=[B_xt[i], sc1p[r][1]], writes=[B_tmpf[i]])
                        k.op("pool", lambda e: e.tensor_tensor(out=h_bf[i][:], in0=tmpf[i][:], in1=sh1[r][0][:], op=ALU.add),
                             reads=[B_tmpf[i], sh1[r][1]], writes=[B_hbf[i]])
                        for kc in range(8):
                            k.op("pe", lambda e, kc=kc: e.transpose(out=pT[i][:, kc, :], in_=h_bf[i][:, kc * 128:(kc + 1) * 128], identity=ident_b[:]),
                                 reads=[B_hbf[i], B_ident_b], writes=[B_pT[i]])
                        k.op("act", lambda e: e.copy(out=hT[i][:], in_=pT[i][:]), reads=[B_pT[i]], writes=[B_hT[i]])
                        for cb in range(6):
                            if r == 1 and cb < 2:
                                continue
                            j = ibs[i] % 2; ibs[i] += 1
                            ppj = pp[i][j]; Bppj = B_pp[i][j]
                            for kc in range(8):
                                k.op("pe", lambda e, kc=kc, ppj=ppj, cb=cb: e.matmul(
                                    ppj[:], lhsT=hT[i][:, kc, :], rhs=w_bf[:, kc, cb * 512:(cb + 1) * 512],
                                    start=(kc == 0), stop=(kc == 7)), reads=[B_hT[i], B_w], writes=[Bppj])
                            if cb >= 4:
                                c0 = (cb - 4) * 512
                                k.op("act", lambda e, ppj=ppj, c0=c0: e.copy(out=v1[:, ti, c0:c0 + 512], in_=ppj[:]), reads=[Bppj], writes=[B_v1])
                                continue
                            if r == 0:
                                rope_apply(None, ppj[:], Bppj, qk_bf[i][:], B_qk[i], 8, rt[i], B_rt[i], t1[i][:], t2[i][:], B_rtmp[i])
                            else:
                                k.op("dve", lambda e, ppj=ppj: e.tensor_copy(out=qk_bf[i][:], in_=ppj[:]), reads=[Bppj], writes=[B_qk[i]])
                            k.op("dve", lambda e: e.tensor_tensor(out=sqt[i][:], in0=qk_bf[i][:], in1=qk_bf[i][:], op=ALU.mult), reads=[B_qk[i], B_sq[i]], writes=[B_sq[i]])
                            k.op("dve", lambda e: e.tensor_reduce(out=rs8[i][:], in_=sqt[i][:].rearrange("p (m d) -> p m d", d=64), axis=AX.X, op=ALU.add), reads=[B_sq[i]], writes=[B_sq[i]])
                            k.op("dve", lambda e, cb=cb: e.tensor_tensor(out=nmax[:, cb * 8:(cb + 1) * 8], in0=nmax[:, cb * 8:(cb + 1) * 8], in1=rs8[i][:], op=ALU.max),
                                 reads=[B_sq[i], B_nmax], writes=[B_nmax])
                            for hh in range(4):
                                k.op("pe", lambda e, hh=hh: e.transpose(out=pq[i][:, hh, :], in_=qk_bf[i][:, hh * 128:(hh + 1) * 128], identity=ident_b[:]),
                                     reads=[B_qk[i], B_ident_b], writes=[B_pq[i]])
                            dstT, BD = (qT2, B_q2) if cb < 2 else (kT2, B_k2)
                            h0 = (cb % 2) * 4
                            k.op("act", lambda e, dstT=dstT, h0=h0: e.copy(out=dstT[:, h0:h0 + 4, T0:T0 + 128], in_=pq[i][:, 0:4, :]),
                                 reads=[B_pq[i]], writes=[BD])

                    for t2_ in range(0, NTILE, 2):
                        replay([record(lambda ti=ti: proj_tile(ti)) for ti in range(t2_, min(t2_ + 2, NTILE))])
                    k.barrier()
                for st in phase("l1att"):
                    w_bf = sb(st, "difwo_bf", [128, 8, D], BF16); B_w = Buf()
                    for kc in range(8):
                        k.dma("pool", w_bf[:, kc, :], dif_w_out[kc * 128:(kc + 1) * 128, :], writes=[B_w])
                    g1 = mod_bc(st, "l1g1", 1, 0, 2)
                    lng = load_bc(st, "l1ln1g", ln1_g[1:2, :], D)
                    lnb = load_bc(st, "l1ln1b", ln1_b[1:2, :], D)
                    wks1 = [ln_work(st, "l1e1a"), ln_work(st, "l1e1b")]
                    xo = [sb(st, f"l1xo{i}", [128, D]) for i in range(2)]; B_xo = [Buf(), Buf()]
                    lam = sb(st, "lam", [128, 8]); B_lam = Buf()
                    lq = [load_bc(st, f"lq{i}", a[0:1, :], 64) for i, a in enumerate((lam_q1, lam_k1, lam_q2, lam_k2))]
                    ltmp = sb(st, "ltmp", [128, 64]); B_lt = Buf()
                    for i2 in range(2):
                        k.op("dve", lambda e, i2=i2: e.tensor_tensor(out=ltmp[:], in0=lq[2 * i2][0][:], in1=lq[2 * i2 + 1][0][:], op=ALU.mult),
                             reads=[lq[2 * i2][1], lq[2 * i2 + 1][1], B_lt], writes=[B_lt])
                        k.op("dve", lambda e, i2=i2: e.reduce_sum(out=lam[:, i2:i2 + 1], in_=ltmp[:], axis=AX.X), reads=[B_lt, B_lam], writes=[B_lam])
                    k.op("act", lambda e: e.activation(out=lam[:, 2:4], in_=lam[:, 0:2], func=AF.Exp), reads=[B_lam], writes=[B_lam])
                    k.op("dve", lambda e: e.tensor_tensor(out=lam[:, 4:5], in0=lam[:, 2:3], in1=lam[:, 3:4], op=ALU.subtract), reads=[B_lam], writes=[B_lam])
                    k.op("dve", lambda e: e.tensor_scalar(out=lam[:, 5:6], in0=lam[:, 4:5], scalar1=-1.0, scalar2=-LAM_INIT, op0=ALU.mult, op1=ALU.add), reads=[B_lam], writes=[B_lam])
                    sg_col = sb(st, "sg_col", [128, 1]); B_sg = Buf()
                    with nc.allow_non_contiguous_dma(reason="tiny"):
                        k.dma("sp", sg_col[:], subln_g[0, :].rearrange("(p x) -> p x", x=1), writes=[B_sg])
                    k.op("dve", lambda e: e.tensor_scalar(out=sg_col[:], in0=sg_col[:], scalar1=1.0 - LAM_INIT, scalar2=None, op0=ALU.mult), reads=[B_sg], writes=[B_sg])
                    ones_bf = sb(st, "ones_bf", [128, 128], BF16); B_ones = Buf()
                    k.op("dve", lambda e: e.memset(ones_bf[:], 1.0), writes=[B_ones])
                    negC = sb(st, "negC", [128, 16]); B_negC = Buf()
                    with ExitStack() as t0:
                        nb = sb(t0, "nmax_bf", [128, 32], BF16); B_nb = Buf()
                        k.op("dve", lambda e: e.tensor_scalar(out=nb[:], in0=nmax[:], scalar1=1.02, scalar2=None, op0=ALU.mult), reads=[B_nmax], writes=[B_nb])
                        pn = ps(t0, "pn", [16, 1024], BF16); B_pn = Buf()
                        k.op("pe", lambda e: e.transpose(out=pn[:, 0:128], in_=nb[:, 0:16], identity=ident_b[:]), reads=[B_nb, B_ident_b], writes=[B_pn])
                        k.op("pe", lambda e: e.transpose(out=pn[:, 128:256], in_=nb[:, 16:32], identity=ident_b[:]), reads=[B_nb, B_ident_b], writes=[B_pn])
                        r2 = sb(t0, "r2", [16, 8]); B_r2 = Buf()
                        k.op("dve", lambda e: e.reduce_max(out=r2[:, 0:1], in_=pn[:, 0:128], axis=AX.X), reads=[B_pn], writes=[B_r2])
                        k.op("dve", lambda e: e.reduce_max(out=r2[:, 1:2], in_=pn[:, 128:256], axis=AX.X), reads=[B_pn, B_r2], writes=[B_r2])
                        k.op("dve", lambda e: e.tensor_tensor(out=r2[:, 2:3], in0=r2[:, 0:1], in1=r2[:, 1:2], op=ALU.mult), reads=[B_r2], writes=[B_r2])
                        k.op("act", lambda e: e.sqrt(out=r2[:, 3:4], in_=r2[:, 2:3]), reads=[B_r2], writes=[B_r2])
                        k.op("dve", lambda e: e.tensor_scalar(out=r2[:, 4:5], in0=r2[:, 3:4], scalar1=-SC, scalar2=None, op0=ALU.mult), reads=[B_r2], writes=[B_r2])
                        dg = sb(t0, "dgC", [16, 16], BF16); B_dg = Buf()
                        k.op("dve", lambda e: e.tensor_scalar(out=dg[:], in0=ident_f[0:16, 0:16], scalar1=r2[:, 4:5], scalar2=None, op0=ALU.mult), reads=[B_r2, B_ident_f], writes=[B_dg])
                        pc = ps(t0, "pcb", [128, 512]); B_pc = Buf()
                        k.op("pe", lambda e: e.matmul(pc[:, 0:16], lhsT=ones_bf[0:16, :], rhs=dg[:], start=True, stop=True), reads=[B_ones, B_dg], writes=[B_pc])
                        k.op("dve", lambda e: e.tensor_copy(out=negC[:], in_=pc[:, 0:16]), reads=[B_pc], writes=[B_negC])
                        k.barrier()
                    ET = [sb(st, f"ET{i}", [128, 512], BF16) for i in range(4)]; B_ET = [Buf() for _ in range(4)]
                    aoT = sb(st, "aoT_all", [128, 8, 512], BF16); B_aoT = Buf()
                    rz = sb(st, "rz", [1, 2, 512]); B_rz = Buf()
                    rzb = sb(st, "rzb", [1, 4, 512], BF16); B_rzb = Buf()
                    bcs = sb(st, "bcs", [128, 512]); B_bcs = Buf()
                    oT = sb(st, "oT", [128, 512]); B_oT = Buf()
                    t5 = sb(st, "t5", [128, 512]); B_t5 = Buf()
                    sqb = sb(st, "sqb", [128, 512], BF16); B_sqb = Buf()
                    pS = [ps(st, f"pS{i}", [128, 512]) for i in range(2)]; B_pS = [Buf(), Buf()]
                    pO4 = [ps(st, f"pO{i}", [128, 512]) for i in range(4)]; B_pO4 = [Buf() for _ in range(4)]
                    pZ1 = ps(st, "pZ", [1, 512]); B_pZ1 = Buf()
                    pZ = [pZ1, pZ1]; B_pZ = [B_pZ1, B_pZ1]
                    pB = ps(st, "pB", [128, 512]); B_pB = Buf()
                    cnt = {"s": 0, "e": 0}

                    def bcast_row(hi, lo):
                        k.op("pe", lambda e: e.matmul(pB[:], lhsT=ones_bf[0:1, :], rhs=hi, start=True, stop=False), reads=[B_ones, B_rzb], writes=[B_pB])
                        k.op("pe", lambda e: e.matmul(pB[:], lhsT=ones_bf[0:1, :], rhs=lo, start=False, stop=True), reads=[B_ones, B_rzb], writes=[B_pB])
                        k.op("act", lambda e: e.copy(out=bcs[:], in_=pB[:]), reads=[B_pB], writes=[B_bcs])

                    def split_row(src, j):
                        k.op("dve", lambda e: e.tensor_copy(out=rzb[:, 2 * j, :], in_=src), reads=[B_rz], writes=[B_rzb])
                        k.op("dve", lambda e: e.tensor_tensor(out=rzb[:, 2 * j + 1, :], in0=src, in1=rzb[:, 2 * j, :], op=ALU.subtract), reads=[B_rz, B_rzb], writes=[B_rzb])

                    zs = sb(st, "zs", [1, 2, 512]); B_zs = Buf()

                    def qk_exp(Q0, h, c, b):
                        ps_ = slice(c * 64, (c + 1) * 64)
                        m = h * 2 + c
                        js = cnt["s"] % 2; cnt["s"] += 1
                        je = cnt["e"] % 4; cnt["e"] += 1
                        k.op("pe", lambda e: e.matmul(pS[js][:], lhsT=kT2[ps_, h, b * 128:(b + 1) * 128], rhs=qT2[ps_, h, Q0:Q0 + 512],
                                                      start=True, stop=True), reads=[B_k2, B_q2], writes=[B_pS[js]])
                        k.op("act", lambda e: e.activation(out=ET[je][:], in_=pS[js][:], func=AF.Exp, bias=negC[:, m:m + 1], scale=SC),
                             reads=[B_pS[js], B_negC], writes=[B_ET[je]])
                        return je

                    def pvz(h, c, b, je):
                        pO = pO4[(h % 2) * 2:(h % 2) * 2 + 2]; B_pO = B_pO4[(h % 2) * 2:(h % 2) * 2 + 2]
                        Zacc = Zacc4[(h % 2) * 2:(h % 2) * 2 + 2]; B_Zacc = B_Zacc4[(h % 2) * 2:(h % 2) * 2 + 2]
                        k.op("pe", lambda e: e.matmul(pO[c][:], lhsT=v1[:, b, h * 128:(h + 1) * 128], rhs=ET[je][:],
                                                      start=(b == 0), stop=(b == NTILE - 1)), reads=[B_v1, B_ET[je]], writes=[B_pO[c]])
                        if b == 0:
                            k.op("dve", lambda e: e.tensor_copy(out=Zacc[c][:], in_=ET[je][:]), reads=[B_ET[je]], writes=[B_Zacc[c]])
                        else:
                            k.op("dve", lambda e: e.tensor_tensor(out=Zacc[c][:], in0=Zacc[c][:], in1=ET[je][:], op=ALU.add), reads=[B_ET[je], B_Zacc[c]], writes=[B_Zacc[c]])

                    Zacc4 = [sb(st, f"Zacc{i}", [128, 512]) for i in range(4)]; B_Zacc4 = [Buf() for _ in range(4)]
                    ones_f = sb(st, "ones_f", [128, 1]); B_onesf = Buf()
                    k.op("dve", lambda e: e.memset(ones_f[:], 1.0), writes=[B_onesf])

                    def bcast_recip(c, Zacc, B_Zacc):
                        k.op("pe", lambda e: e.matmul(pZ[c][:], lhsT=ones_f[:, 0:1], rhs=Zacc[c][:], start=True, stop=True), reads=[B_onesf, B_Zacc[c]], writes=[B_pZ[c]])
                        k.op("act", lambda e: e.copy(out=zs[:, c, :], in_=pZ[c][:]), reads=[B_pZ[c], B_zs], writes=[B_zs])
                        k.op("dve", lambda e: e.tensor_copy(out=rzb[:, 2 * c, :], in_=zs[:, c, :]), reads=[B_zs, B_rzb], writes=[B_rzb])
                        k.op("dve", lambda e: e.tensor_tensor(out=rzb[:, 2 * c + 1, :], in0=zs[:, c, :], in1=rzb[:, 2 * c, :], op=ALU.subtract), reads=[B_zs, B_rzb], writes=[B_rzb])
                        k.op("pe", lambda e: e.matmul(pB[:], lhsT=ones_bf[0:1, :], rhs=rzb[:, 2 * c, :], start=True, stop=False), reads=[B_ones, B_rzb], writes=[B_pB])
                        k.op("pe", lambda e: e.matmul(pB[:], lhsT=ones_bf[0:1, :], rhs=rzb[:, 2 * c + 1, :], start=False, stop=True), reads=[B_ones, B_rzb], writes=[B_pB])
                        k.op("dve", lambda e: e.reciprocal(out=bcs[:], in_=pB[:]), reads=[B_pB, B_bcs], writes=[B_bcs])

                    def epilogue(h):
                        pO = pO4[(h % 2) * 2:(h % 2) * 2 + 2]; B_pO = B_pO4[(h % 2) * 2:(h % 2) * 2 + 2]
                        Zacc = Zacc4[(h % 2) * 2:(h % 2) * 2 + 2]; B_Zacc = B_Zacc4[(h % 2) * 2:(h % 2) * 2 + 2]
                        bcast_recip(0, Zacc, B_Zacc)
                        k.op("dve", lambda e: e.tensor_tensor(out=oT[:], in0=pO[0][:], in1=bcs[:], op=ALU.mult), reads=[B_pO[0], B_bcs, B_oT], writes=[B_oT])
                        bcast_recip(1, Zacc, B_Zacc)
                        k.op("dve", lambda e: e.tensor_tensor(out=t5[:], in0=pO[1][:], in1=bcs[:], op=ALU.mult), reads=[B_pO[1], B_bcs, B_t5], writes=[B_t5])
                        k.op("dve", lambda e: e.scalar_tensor_tensor(out=oT[:], in0=t5[:], scalar=lam[:, 5:6], in1=oT[:], op0=ALU.mult, op1=ALU.add),
                             reads=[B_oT, B_t5, B_lam], writes=[B_oT])
                        k.op("dve", lambda e: e.tensor_tensor(out=sqb[:], in0=oT[:], in1=oT[:], op=ALU.mult), reads=[B_oT, B_sqb], writes=[B_sqb])
                        k.op("pe", lambda e: e.matmul(pZ[0][:], lhsT=ones_bf[:, 0:1], rhs=sqb[:], start=True, stop=True), reads=[B_ones, B_sqb], writes=[B_pZ[0]])
                        k.op("act", lambda e: e.activation(out=rz[:, 0, :], in_=pZ[0][:], func=AF.Ln, scale=1.0 / 128.0, bias=eps_t[0:1, :]), reads=[B_pZ[0], B_rz, B_eps], writes=[B_rz])
                        k.op("act", lambda e: e.activation(out=rz[:, 0, :], in_=rz[:, 0, :], func=AF.Exp, scale=-0.5), reads=[B_rz], writes=[B_rz])
                        split_row(rz[:, 0, :], 0)
                        bcast_row(rzb[:, 0, :], rzb[:, 1, :])
                        k.op("dve", lambda e: e.scalar_tensor_tensor(out=aoT[:, h, :], in0=oT[:], scalar=sg_col[:, 0:1], in1=bcs[:], op0=ALU.mult, op1=ALU.mult),
                             reads=[B_oT, B_sg, B_bcs], writes=[B_aoT])

                    eps_t = sb(st, "eps_t", [128, 1]); B_eps = Buf()
                    k.op("dve", lambda e: e.memset(eps_t[:], 1e-5), writes=[B_eps])
                    for qg in range(4):
                        Q0 = qg * 512
                        steps = [(h, c, b) for h in range(8) for c in range(2) for b in range(NTILE)]
                        je_next = qk_exp(Q0, *steps[0])
                        pending = []
                        for si, (h, c, b) in enumerate(steps):
                            je_cur = je_next
                            if si + 1 < len(steps):
                                je_next = qk_exp(Q0, *steps[si + 1])
                            pvz(h, c, b, je_cur)
                            if pending:
                                a_, kw_ = pending.pop(0)
                                k.op(*a_, **kw_)
                            if c == 1 and b == NTILE - 1:
                                while pending:
                                    a_, kw_ = pending.pop(0)
                                    k.op(*a_, **kw_)
                                orig_op = k.op
                                rec = []
                                k.op = lambda *a, rec=rec, **kw: rec.append((a, kw))
                                epilogue(h)
                                k.op = orig_op
                                pending = rec
                        while pending:
                            a_, kw_ = pending.pop(0)
                            k.op(*a_, **kw_)
                        def l1out_tile(tt):
                            ti = qg * 4 + tt
                            T0 = ti * 128
                            i = ti % 2
                            pa = pO4[2 * i:2 * i + 2]; Bpa = B_pO4[2 * i:2 * i + 2]
                            k.dma("sp", xo[i][:], src_rows(ti, 1), writes=[B_xo[i]])
                            for hf in range(2):
                                for h in range(8):
                                    k.op("pe", lambda e, h=h, hf=hf: e.matmul(pa[hf][:], lhsT=aoT[:, h, tt * 128:(tt + 1) * 128], rhs=w_bf[:, h, hf * 512:(hf + 1) * 512],
                                                                         start=(h == 0), stop=(h == 7)), reads=[B_aoT, B_w], writes=[Bpa[hf]])
                            ln_epilogue(wks1[i], [pa[0][:], pa[1][:]], [Bpa[0], Bpa[1]], xo[i], B_xo[i], g1, lng, lnb, x1_scr[T0:T0 + 128, :])

                        for t2_ in range(0, 4, 2):
                            replay([record(lambda tt=tt: l1out_tile(tt)) for tt in range(t2_, t2_ + 2)])
                    k.barrier()

        with ExitStack() as L0:
            catT = sb(L0, "catT", [128, 8, NT], BF16); B_catT = Buf()
            LA = ExitStack()
            qT = sb(LA, "qT", [64, 8, NT], BF16); B_qT = Buf()
            kT = sb(LA, "kT", [64, 2, NT], BF16); B_kT = Buf()
            v_all = sb(LA, "v_all", [128, NTILE, 128], BF16); B_v = Buf()
            for st in phase("l0proj"):
                w_bf = sb(st, "w_in_bf", [128, 8, 1280], BF16); B_w = Buf()
                for kc in range(8):
                    k.dma("pool", w_bf[:, kc, :], w_in0[kc * 128:(kc + 1) * 128, :], writes=[B_w])
                sc1p = [None, None]; sh1 = [None, None]
                for r in range(2):
                    sh1[r] = mod_bc(st, f"sh1_{r}", 0, r, 0)
                    sc1p[r] = mod_bc(st, f"sc1p_{r}", 0, r, 1, plus1=True)
                xt = [sb(st, f"xt{i}", [128, D]) for i in range(2)]; B_xt = [Buf(), Buf()]
                tmpf = [sb(st, f"tmpf{i}", [128, D]) for i in range(2)]; B_tmpf = [Buf(), Buf()]
                h_bf = [sb(st, f"h_bf{i}", [128, D], BF16) for i in range(2)]; B_hbf = [Buf(), Buf()]
                hT = [sb(st, f"hT{i}", [128, 8, 128], BF16) for i in range(2)]; B_hT = [Buf(), Buf()]
                rt = [sb(st, f"rt{i}", [128, 64]) for i in range(2)]; B_rt = [Buf(), Buf()]
                t1 = [sb(st, f"rope_t1{i}", [128, 640]) for i in range(2)]; t2 = [sb(st, f"rope_t2{i}", [128, 640]) for i in range(2)]; B_rtmp = [Buf(), Buf()]
                qk_bf = [sb(st, f"qk_bf{i}", [128, 640], BF16) for i in range(2)]; B_qk = [Buf(), Buf()]
                ut = [sb(st, f"ut{i}", [128, 512]) for i in range(2)]; B_ut = [Buf(), Buf()]
                pA = [ps(st, f"pA{i}", [128, 8, 128], BF16) for i in range(2)]; B_pA = [Buf(), Buf()]
                pp = [[ps(st, f"pp{i}{j}", [128, 512]) for j in range(3)] for i in range(2)]; B_pp = [[Buf(), Buf(), Buf()], [Buf(), Buf(), Buf()]]

                def proj0_tile(ti):
                    i = ti % 2
                    r = 0 if ti < NLT else 1
                    T0 = ti * 128
                    ppi = pp[i]; Bppi = B_pp[i]
                    k.dma("sp", xt[i][:], src_rows(ti, 0), writes=[B_xt[i]])
                    if r == 0:
                        k.dma("sp", rt[i][:], rope_cs[T0:T0 + 128, :], writes=[B_rt[i]])
                    k.op("dve", lambda e: e.tensor_tensor(out=tmpf[i][:], in0=xt[i][:], in1=sc1p[r][0][:], op=ALU.mult),
                         reads=[B_xt[i], sc1p[r][1]], writes=[B_tmpf[i]])
                    k.op("pool", lambda e: e.tensor_tensor(out=h_bf[i][:], in0=tmpf[i][:], in1=sh1[r][0][:], op=ALU.add),
                         reads=[B_tmpf[i], sh1[r][1]], writes=[B_hbf[i]])
                    for kc in range(8):
                        k.op("pe", lambda e, kc=kc: e.transpose(out=pA[i][:, kc, :], in_=h_bf[i][:, kc * 128:(kc + 1) * 128], identity=ident_b[:]),
                             reads=[B_hbf[i], B_ident_b], writes=[B_pA[i]])
                    k.op("act", lambda e: e.copy(out=hT[i][:], in_=pA[i][:]), reads=[B_pA[i]], writes=[B_hT[i]])
                    for nb, (c0, c1) in enumerate(((0, 512), (512, 1024), (1024, 1280))):
                        for kc in range(8):
                            k.op("pe", lambda e, kc=kc, nb=nb, c0=c0, c1=c1: e.matmul(
                                ppi[nb][:, 0:c1 - c0], lhsT=hT[i][:, kc, :], rhs=w_bf[:, kc, c0:c1],
                                start=(kc == 0), stop=(kc == 7)),
                                reads=[B_hT[i], B_w], writes=[Bppi[nb]])
                    if r == 0:
                        rope_apply(None, ppi[0][:, 0:512], Bppi[0], qk_bf[i][:, 0:512], B_qk[i], 8, rt[i], B_rt[i], t1[i][:, 0:512], t2[i][:, 0:512], B_rtmp[i])
                        rope_apply(None, ppi[1][:, 0:128], Bppi[1], qk_bf[i][:, 512:640], B_qk[i], 2, rt[i], B_rt[i], t1[i][:, 512:640], t2[i][:, 512:640], B_rtmp[i])
                    else:
                        k.op("dve", lambda e: e.tensor_copy(out=qk_bf[i][:, 0:512], in_=ppi[0][:, 0:512]), reads=[Bppi[0]], writes=[B_qk[i]])
                        k.op("dve", lambda e: e.tensor_copy(out=qk_bf[i][:, 512:640], in_=ppi[1][:, 0:128]), reads=[Bppi[1]], writes=[B_qk[i]])
                    for h in range(8):
                        k.op("pe", lambda e, h=h: e.transpose(out=pA[i][0:64, h, :], in_=qk_bf[i][:, h * 64:(h + 1) * 64], identity=ident_b[:]),
                             reads=[B_qk[i], B_ident_b], writes=[B_pA[i]])
                    k.op("act", lambda e: e.copy(out=qT[:, :, T0:T0 + 128], in_=pA[i][0:64, :, :]), reads=[B_pA[i]], writes=[B_qT])
                    for h in range(2):
                        k.op("pe", lambda e, h=h: e.transpose(out=pA[i][0:64, h, :], in_=qk_bf[i][:, 512 + h * 64:512 + (h + 1) * 64], identity=ident_b[:]),
                             reads=[B_qk[i], B_ident_b], writes=[B_pA[i]])
                    k.op("act", lambda e: e.copy(out=kT[:, :, T0:T0 + 128], in_=pA[i][0:64, 0:2, :]), reads=[B_pA[i]], writes=[B_kT])
                    k.op("act", lambda e: e.copy(out=v_all[:, ti, :], in_=ppi[1][:, 128:256]), reads=[Bppi[1]], writes=[B_v])
                    k.op("act", lambda e: e.copy(out=ut[i][:, 0:256], in_=ppi[1][:, 256:512]), reads=[Bppi[1]], writes=[B_ut[i]])
                    k.op("act", lambda e: e.copy(out=ut[i][:, 256:512], in_=ppi[2][:, 0:256]), reads=[Bppi[2]], writes=[B_ut[i]])
                    urow = T0 + CTX if r == 0 else T0 - SEQ
                    k.dma("sp", u_scr[urow:urow + 128, :], ut[i][:], reads=[B_ut[i]])

                for t2_ in range(0, NTILE, 2):
                    replay([record(lambda ti=ti: proj0_tile(ti)) for ti in range(t2_, min(t2_ + 2, NTILE))])
                if "dbg_qT" in dbg:
                    dbg_qT = dscr("dbg_qT", [64, 8, NT], BF16)
                    k.dma("sp", dbg_qT, qT[:], reads=[B_qT])
                k.barrier()


            for st in phase("l0att"):
                SC = 0.125
                maskL = sb(st, "maskL_sb", [128, 128]); maskR = sb(st, "maskR_sb", [128, 128]); B_mask = Buf()
                k.dma("sp", maskL[:], maskL_in[:, :], writes=[B_mask])
                k.dma("sp", maskR[:], maskR_in[:, :], writes=[B_mask])
                sink_bc, B_sink = load_bc(st, "sink_bc", swa_sink[0:1, :], 8)
                sm = [sb(st, f"sm{i}", [128, 640]) for i in range(2)]; B_sm = [Buf(), Buf()]
                P = [sb(st, f"P{i}", [128, 640], BF16) for i in range(2)]; B_P = [Buf(), Buf()]
                PT = [sb(st, f"PT{i}", [128, 5, 128], BF16) for i in range(2)]; B_PT = [Buf(), Buf()]
                stat = [sb(st, f"stat{i}", [128, 8]) for i in range(2)]; B_stat = [Buf(), Buf()]
                att_bf = sb(st, "att_bf", [128, 512], BF16); B_att = Buf()
                ps_loc = [ps(st, f"ps_loc{i}", [128, 512]) for i in range(2)]; B_psl = [Buf(), Buf()]
                ps_ctx = [ps(st, f"ps_ctx{i}", [128, 512]) for i in range(2)]; B_psc = [Buf(), Buf()]
                pPT = [ps(st, f"pPT{c_}", [128, 8, 128], BF16) for c_ in range(2)]; B_pPT = [Buf(), Buf()]
                po2 = [ps(st, f"po{c_}", [128, 512]) for c_ in range(2)]; B_poh = [Buf() for _ in range(8)]
                pcat = pPT[0]; B_pcat = B_pPT[0]
                it = 0
                for ti in range(NTILE):
                    T0 = ti * 128
                    lat = ti < NLT
                    if lat:
                        j0 = max(0, ti - 1); j1 = min(NLT - 1, ti + 1)
                        nloc = (j1 - j0 + 1) * 128
                        blocks = list(range(j0, j1 + 1)) + [NLT, NLT + 1]
                    else:
                        nloc = 0
                        blocks = [NLT, NLT + 1]
                    n = nloc + 256
                    def head_chain(h, i):
                        kvh = h // 4
                        if lat:
                            k.op("pe", lambda e, i=i, h=h, kvh=kvh, j0=j0, nloc=nloc, T0=T0: e.matmul(
                                ps_loc[i][:, 0:nloc], lhsT=qT[:, h, T0:T0 + 128], rhs=kT[:, kvh, j0 * 128:j0 * 128 + nloc],
                                start=True, stop=True), reads=[B_qT, B_kT], writes=[B_psl[i]])
                        k.op("pe", lambda e, i=i, h=h, kvh=kvh, T0=T0: e.matmul(
                            ps_ctx[i][:, 0:256], lhsT=qT[:, h, T0:T0 + 128], rhs=kT[:, kvh, SEQ:NT],
                            start=True, stop=True), reads=[B_qT, B_kT], writes=[B_psc[i]])
                        if lat:
                            for bi, j in enumerate(range(j0, j1 + 1)):
                                sl = slice(bi * 128, (bi + 1) * 128)
                                if j == ti:
                                    k.op("act", lambda e, i=i, sl=sl: e.mul(out=sm[i][:, sl], in_=ps_loc[i][:, sl], mul=SC),
                                         reads=[B_psl[i]], writes=[B_sm[i]])
                                else:
                                    mk = maskL if j < ti else maskR
                                    k.op("dve", lambda e, i=i, sl=sl, mk=mk: e.scalar_tensor_tensor(
                                        out=sm[i][:, sl], in0=ps_loc[i][:, sl], scalar=SC, in1=mk[:], op0=ALU.mult, op1=ALU.add),
                                        reads=[B_psl[i], B_mask], writes=[B_sm[i]])
                        k.op("act", lambda e, i=i, nloc=nloc: e.mul(out=sm[i][:, nloc:nloc + 256], in_=ps_ctx[i][:, 0:256], mul=SC),
                             reads=[B_psc[i]], writes=[B_sm[i]])
                        sti = stat[i]
                        k.op("dve", lambda e, i=i, n=n, sti=sti: e.reduce_max(out=sti[:, 0:1], in_=sm[i][:, 0:n], axis=AX.X),
                             reads=[B_sm[i]], writes=[B_stat[i]])
                        k.op("dve", lambda e, sti=sti, h=h: e.tensor_tensor(out=sti[:, 1:2], in0=sti[:, 0:1], in1=sink_bc[:, h:h + 1], op=ALU.max),
                             reads=[B_stat[i], B_sink], writes=[B_stat[i]])
                        k.op("dve", lambda e, sti=sti: e.tensor_scalar(out=sti[:, 2:3], in0=sti[:, 1:2], scalar1=-1.0, scalar2=None, op0=ALU.mult),
                             reads=[B_stat[i]], writes=[B_stat[i]])
                        k.op("act", lambda e, i=i, n=n, sti=sti: e.activation(out=P[i][:, 0:n], in_=sm[i][:, 0:n], func=AF.Exp,
                                                                             bias=sti[:, 2:3], scale=1.0, accum_out=sti[:, 3:4]),
                             reads=[B_sm[i], B_stat[i]], writes=[B_P[i], B_stat[i]])
                        k.op("act", lambda e, sti=sti, h=h: e.activation(out=sti[:, 4:5], in_=sink_bc[:, h:h + 1], func=AF.Exp,
                                                                        bias=sti[:, 2:3], scale=1.0),
                             reads=[B_sink, B_stat[i]], writes=[B_stat[i]])
                        k.op("dve", lambda e, sti=sti: e.tensor_tensor(out=sti[:, 5:6], in0=sti[:, 3:4], in1=sti[:, 4:5], op=ALU.add),
                             reads=[B_stat[i]], writes=[B_stat[i]])
                        k.op("dve", lambda e, sti=sti: e.reciprocal(out=sti[:, 6:7], in_=sti[:, 5:6]),
                             reads=[B_stat[i]], writes=[B_stat[i]])
                        nb = n // 128
                        for b in range(nb):
                            k.op("pe", lambda e, i=i, b=b: e.transpose(out=pPT[i][:, b, :], in_=P[i][:, b * 128:(b + 1) * 128], identity=ident_b[:]),
                                 reads=[B_P[i], B_ident_b], writes=[B_pPT[i]])
                        k.op("pool" if False else "dve", lambda e, i=i, nb=nb: e.tensor_copy(out=PT[i][:, 0:nb, :], in_=pPT[i][:, 0:nb, :]),
                             reads=[B_pPT[i]], writes=[B_PT[i]])
                        for b in range(nb):
                            k.op("pe", lambda e, i=i, b=b, h=h, kvh=kvh, vb=blocks[b], nb=nb: e.matmul(
                                po2[i][:, h * 64:(h + 1) * 64], lhsT=PT[i][:, b, :], rhs=v_all[:, vb, kvh * 64:(kvh + 1) * 64],
                                start=(b == 0), stop=(b == nb - 1)), reads=[B_PT[i], B_v], writes=[B_poh[h]])
                        k.op("dve", lambda e, h=h, sti=sti: e.tensor_scalar(out=att_bf[:, h * 64:(h + 1) * 64], in0=po2[i][:, h * 64:(h + 1) * 64],
                                                                           scalar1=sti[:, 6:7], scalar2=None, op0=ALU.mult),
                             reads=[B_poh[h], B_stat[i]], writes=[B_att])

                    pair_ = []
                    for h in range(8):
                        i = it % 2; it += 1
                        pair_.append(record(lambda h=h, i=i: head_chain(h, i)))
                        if len(pair_) == 2:
                            replay(pair_)
                            pair_ = []
                    for cb in range(4):
                        k.op("pe", lambda e, cb=cb: e.transpose(out=pcat[:, cb, :], in_=att_bf[:, cb * 128:(cb + 1) * 128], identity=ident_b[:]),
                             reads=[B_att, B_ident_b], writes=[B_pcat])
                    k.op("act", lambda e, T0=T0: e.copy(out=catT[:, 0:4, T0:T0 + 128], in_=pcat[:, 0:4, :]), reads=[B_pcat], writes=[B_catT])
                    if "dbg_att" in dbg:
                        if ti == 0:
                            dbg_att = dscr("dbg_att", [NT, 512], BF16)
                        k.dma("sp", dbg_att[T0:T0 + 128, :], att_bf[:], reads=[B_att])
                k.barrier()


            k.barrier()
            LA.close()
            for st in phase("l0ssm"):
                ssm_phase(st, catT, B_catT)
            for st in phase("l0ssmpost"):
                ssm_post(st, catT, B_catT)

            for st in phase("l0out"):
                outproj_ln1(st, 0, catT, B_catT, w_out0, NTILE)


        for st in phase("moe0"):
            moe_phase(st, 0, NTILE, x2_scr)


        layer1_mixer()
        for st in phase("moe1"):
            moe_phase(st, 1, NLT, out)

        k.barrier()
    return nc


_CONSTS = None


def _consts():
    global _CONSTS
    if _CONSTS is None:
        t = np.arange(SEQ)
        row = (t // 64).astype(np.float32)
        col = (t % 64).astype(np.float32)
        inv = (10000.0 ** (-np.arange(16, dtype=np.float32) / 16)).astype(np.float32)
        ar = row[:, None] * inv[None, :]
        ac = col[:, None] * inv[None, :]
        rope = np.concatenate([np.cos(ar), np.sin(ar), np.cos(ac), np.sin(ac)], 1).astype(np.float32)
        qi = np.arange(128)[:, None]; kj = np.arange(128)[None, :]
        mL = np.where(kj >= qi, 0.0, -30000.0).astype(np.float32)
        mR = np.where(kj <= qi, 0.0, -30000.0).astype(np.float32)
        _CONSTS = {"rope_cs": rope, "ident": np.eye(128, dtype=np.float32), "maskL": mL, "maskR": mR}
        selm = np.zeros((32, 32, 128), np.float32)
        for e_ in range(32):
            selm[e_, e_, :] = 1.0
        _CONSTS["sel"] = selm
        _CONSTS["kval"] = np.ascontiguousarray(np.broadcast_to(np.repeat(np.arange(-7, 9, dtype=np.float32), 64)[None, :], (64, 1024)))
        _CONSTS["mrow"] = np.ascontiguousarray(np.broadcast_to(np.arange(288, dtype=np.float32)[None, :], (64, 288)))
        jj = np.arange(128) // 16
        _CONSTS["maskF"] = (jj[None, :] >= jj[:, None]).astype(np.float32)
        _CONSTS["maskB"] = (jj[None, :] <= jj[:, None]).astype(np.float32)
    return _CONSTS


def make_in_maps(inputs, cores):
    f = lambda a: np.ascontiguousarray(np.asarray(a, dtype=np.float32))
    shared = {}
    for name in ("mod_w", "mod_b", "ln1_g", "ln1_b", "ln2_g", "ln2_b", "swa_sink",
                 "ssm_d", "ssm_glu_b", "dif_lam_q1", "dif_lam_k1", "dif_lam_q2", "dif_lam_k2",
                 "dif_subln_g", "moe_wg", "moe_bg", "moe_we", "moe_w1", "moe_w3", "moe_w2"):
        shared[name] = f(inputs[name])
    for name in ("swa_ssm_w_in", "swa_ssm_w_out", "ssm_a_re", "ssm_a_im", "ssm_log_step",
                 "ssm_b_re", "ssm_b_im", "ssm_c_re", "ssm_c_im", "ssm_glu_w", "dif_w_in", "dif_w_out"):
        shared[name] = f(inputs[name])[0]
    shared["moe_be"] = f(inputs["moe_be"]).reshape(2, 32)
    shared["c_ctx"] = f(inputs["c_ctx"]).reshape(1, D)
    shared.update(_consts())
    maps = []
    for b in cores:
        m = dict(shared)
        m["x"] = f(inputs["x"][b])
        m["ctx"] = f(inputs["ctx"][b])
        m["c"] = f(inputs["c"][b]).reshape(1, D)
        maps.append(m)
    return maps


def kernel(**inputs):
    nc = build()
    maps = make_in_maps(inputs, range(8))
    res = run_bass_kernel_spmd(nc, maps, core_ids=list(range(8)))
    return np.stack([r["out"] for r in res.results], 0).astype(np.float32)
```
